# Optimizing a Trainium2 kernel written in Bass

```python
import jax, jax.numpy as jnp
from jax import lax
import numpy as np

D_MODEL = 1024
BATCH = 16
SEQ = 2048
DEPTH = 1

CHUNK = 64
D_MIX = D_MODEL
SSD_HEADS = 8
SSD_HEAD_DIM = 64
SSD_INNER = SSD_HEADS * SSD_HEAD_DIM
SSD_GROUPS = 2
SSD_STATE = 128
CONV_WIDTH = 4
SSD_CONV_DIM = SSD_INNER + 2 * SSD_GROUPS * SSD_STATE
ATT_HEADS = 8
ATT_HEAD_DIM = 64
ATT_INNER = ATT_HEADS * ATT_HEAD_DIM
Q_BLOCK = 128
N_EXPERT_GROUPS = 4
EXPERTS_PER_GROUP = 4
N_EXPERTS = N_EXPERT_GROUPS * EXPERTS_PER_GROUP
TOP_K = 2
D_FF_EXPERT = 512
DEEPNORM_ALPHA = (2.0 * DEPTH) ** 0.25
DEEPNORM_BETA = (8.0 * DEPTH) ** -0.25
LN_EPS = 1e-5
RMS_EPS = 1e-5
IN_SIZES = (SSD_INNER, SSD_CONV_DIM, SSD_HEADS, ATT_INNER, ATT_INNER, ATT_INNER, ATT_HEADS)
D_IN_PROJ = SSD_INNER + SSD_CONV_DIM + SSD_HEADS + 3 * ATT_INNER + ATT_HEADS

kernel_name = "hybrid_ssd_fox_hiermoe_deepnorm"


def _split_cols(u, sizes):
    idx = np.cumsum(np.array(sizes))[:-1].tolist()
    return jnp.split(u, idx, axis=-1)


def _layer_norm(u, g, b):
    uf = u.astype(jnp.float32)
    mu = jnp.mean(uf, axis=-1, keepdims=True)
    var = jnp.mean(jnp.square(uf - mu), axis=-1, keepdims=True)
    return ((uf - mu) * lax.rsqrt(var + LN_EPS) * g + b).astype(u.dtype)


def _causal_dwconv(u, w, b):
    s = u.shape[1]
    up = jnp.pad(u, ((0, 0), (CONV_WIDTH - 1, 0), (0, 0)))
    out = b
    for k in range(CONV_WIDTH):
        out = out + up[:, k:k + s, :] * w[k]
    return out


def _ssd_scan(xs, dt, a, bm, cm, d_skip):
    f32 = jnp.float32
    xs, dt, bm, cm = xs.astype(f32), dt.astype(f32), bm.astype(f32), cm.astype(f32)
    b, s, h, p = xs.shape
    nc = s // CHUNK
    r = h // SSD_GROUPS
    xdt = (xs * dt[..., None]).reshape(b, nc, CHUNK, SSD_GROUPS, r, p)
    da = (dt * a.astype(f32)).reshape(b, nc, CHUNK, SSD_GROUPS, r)
    bc = bm.reshape(b, nc, CHUNK, SSD_GROUPS, SSD_STATE)
    cc = cm.reshape(b, nc, CHUNK, SSD_GROUPS, SSD_STATE)
    a_cum = jnp.cumsum(da, axis=2)
    seg = a_cum[:, :, :, None] - a_cum[:, :, None, :]
    tri = jnp.tril(jnp.ones((CHUNK, CHUNK), dtype=bool))[None, None, :, :, None, None]
    lmat = jnp.exp(jnp.where(tri, seg, -jnp.inf))
    cb = jnp.einsum('bclgn,bcsgn->bclsg', cc, bc)
    y_diag = jnp.einsum('bclsg,bclsgr,bcsgrp->bclgrp', cb, lmat, xdt)
    decay_to_end = jnp.exp(a_cum[:, :, -1:] - a_cum)
    chunk_states = jnp.einsum('bclgn,bclgr,bclgrp->bcgrpn', bc, decay_to_end, xdt)
    chunk_decay = jnp.exp(a_cum[:, :, -1])

    def step(state, inp):
        st, dec = inp
        return state * dec[..., None, None] + st, state

    init = jnp.zeros((b, SSD_GROUPS, r, p, SSD_STATE), f32)
    _, states_in = lax.scan(step, init, (jnp.moveaxis(chunk_states, 1, 0),
                                         jnp.moveaxis(chunk_decay, 1, 0)))
    states_in = jnp.moveaxis(states_in, 0, 1)
    y_off = jnp.einsum('bclgn,bcgrpn,bclgr->bclgrp', cc, states_in, jnp.exp(a_cum))
    y = (y_diag + y_off).reshape(b, s, h, p)
    return y + xs * d_skip.astype(f32)[:, None]


def _forgetting_attention(q, k, v, log_f):
    b, s, h, d = q.shape
    fcum = jnp.transpose(jnp.cumsum(log_f, axis=1), (0, 2, 1))
    scale = d ** -0.5
    outs = []
    for blk in range(s // Q_BLOCK):
        q0, q1 = blk * Q_BLOCK, (blk + 1) * Q_BLOCK
        logits = jnp.einsum('bqhd,bkhd->bhqk', q[:, q0:q1], k[:, :q1]).astype(jnp.float32) * scale
        logits = logits + fcum[:, :, q0:q1, None] - fcum[:, :, None, :q1]
        qpos = jnp.arange(q0, q1)[:, None]
        kpos = jnp.arange(q1)[None, :]
        logits = jnp.where(qpos >= kpos, logits, -jnp.inf)
        probs = jax.nn.softmax(logits, axis=-1)
        outs.append(jnp.einsum('bhqk,bkhd->bqhd', probs.astype(v.dtype), v[:, :q1]))
    return jnp.concatenate(outs, axis=1)


def _hybrid_mixer(h, w_in, b_in, conv_w, conv_b, a_log, d_skip, ssd_norm_g, w_out):
    bsz, s, _ = h.shape
    proj = jnp.einsum('bsd,de->bse', h, w_in) + b_in
    z, xbc, dt_raw, q, k, v, f_raw = _split_cols(proj, IN_SIZES)
    xbc = jax.nn.silu(_causal_dwconv(xbc, conv_w, conv_b))
    xs, bm, cm = _split_cols(xbc, (SSD_INNER, SSD_GROUPS * SSD_STATE, SSD_GROUPS * SSD_STATE))
    xs = xs.reshape(bsz, s, SSD_HEADS, SSD_HEAD_DIM)
    bm = bm.reshape(bsz, s, SSD_GROUPS, SSD_STATE)
    cm = cm.reshape(bsz, s, SSD_GROUPS, SSD_STATE)
    dt = jax.nn.softplus(dt_raw.astype(jnp.float32))
    a = -jnp.exp(a_log.astype(jnp.float32))
    y = _ssd_scan(xs, dt, a, bm, cm, d_skip).reshape(bsz, s, SSD_INNER)
    y = (y * jax.nn.silu(z.astype(jnp.float32))).reshape(bsz, s, SSD_GROUPS, SSD_INNER // SSD_GROUPS)
    y = y * lax.rsqrt(jnp.mean(jnp.square(y), axis=-1, keepdims=True) + RMS_EPS)
    y_ssd = (y.reshape(bsz, s, SSD_INNER) * ssd_norm_g).astype(h.dtype)
    q = q.reshape(bsz, s, ATT_HEADS, ATT_HEAD_DIM)
    k = k.reshape(bsz, s, ATT_HEADS, ATT_HEAD_DIM)
    v = v.reshape(bsz, s, ATT_HEADS, ATT_HEAD_DIM)
    log_f = jax.nn.log_sigmoid(f_raw.astype(jnp.float32))
    y_att = _forgetting_attention(q, k, v, log_f).reshape(bsz, s, ATT_INNER)
    merged = jnp.concatenate([y_ssd, y_att.astype(h.dtype)], axis=-1)
    return jnp.einsum('bse,ed->bsd', merged, w_out)


def _hier_moe(h, rg_w, rg_b, re_w, re_b, w_gate, w_up, w_down):
    bsz, s, d = h.shape
    hf = h.reshape(bsz * s, d)
    g_logits = (hf @ rg_w + rg_b).astype(jnp.float32)
    g_val, g_idx = lax.top_k(jax.nn.softmax(g_logits, axis=-1), 1)
    g_val, g_idx = g_val[:, 0], g_idx[:, 0]
    e_all = (jnp.einsum('td,gde->tge', hf, re_w) + re_b).astype(jnp.float32)
    e_logits = jnp.take_along_axis(e_all, g_idx[:, None, None], axis=1)[:, 0]
    e_val, e_idx = lax.top_k(jax.nn.softmax(e_logits, axis=-1), TOP_K)
    e_val = e_val / jnp.sum(e_val, axis=-1, keepdims=True)
    weights = g_val[:, None] * e_val
    expert_id = g_idx[:, None] * EXPERTS_PER_GROUP + e_idx
    combine = jnp.sum(jax.nn.one_hot(expert_id, N_EXPERTS, dtype=jnp.float32) * weights[..., None], axis=1)
    out = jnp.zeros((bsz * s, d), jnp.float32)
    for e in range(N_EXPERTS):
        act = jax.nn.silu(hf @ w_gate[e]) * (hf @ w_up[e])
        out = out + combine[:, e:e + 1] * (act @ w_down[e]).astype(jnp.float32)
    return out.astype(h.dtype).reshape(bsz, s, d)


def setup_inputs(seed: int = 0) -> dict:
    key = jax.random.key(seed)
    ks = jax.random.split(key, 20)
    f32 = jnp.float32
    x = jax.random.normal(ks[0], (BATCH, SEQ, D_MODEL), f32)
    col_scale = np.concatenate([
        np.ones(SSD_INNER + SSD_CONV_DIM), np.full(SSD_HEADS, 0.1),
        np.ones(2 * ATT_INNER), np.full(ATT_INNER, DEEPNORM_BETA), np.full(ATT_HEADS, 0.1)]).astype(np.float32)
    w_in = jax.random.normal(ks[1], (DEPTH, D_MODEL, D_IN_PROJ), f32) * (D_MODEL ** -0.5) * jnp.asarray(col_scale)
    b_in = 0.01 * jax.random.normal(ks[2], (DEPTH, D_IN_PROJ), f32)
    dt0 = jnp.exp(jax.random.uniform(ks[3], (DEPTH, SSD_HEADS), f32, np.log(1e-3), np.log(1e-1)))
    dt_bias = dt0 + jnp.log(-jnp.expm1(-dt0))
    off_dt = SSD_INNER + SSD_CONV_DIM
    b_in = b_in.at[:, off_dt:off_dt + SSD_HEADS].set(dt_bias)
    f_bias = jax.random.uniform(ks[4], (DEPTH, ATT_HEADS), f32, 1.0, 5.0)
    b_in = b_in.at[:, D_IN_PROJ - ATT_HEADS:].set(f_bias)
    conv_w = jax.random.normal(ks[5], (DEPTH, CONV_WIDTH, SSD_CONV_DIM), f32) * (CONV_WIDTH ** -0.5)
    conv_b = 0.01 * jax.random.normal(ks[6], (DEPTH, SSD_CONV_DIM), f32)
    a_log = jnp.log(jax.random.uniform(ks[7], (DEPTH, SSD_HEADS), f32, 1.0, 16.0))
    d_skip = 1.0 + 0.1 * jax.random.normal(ks[8], (DEPTH, SSD_HEADS), f32)
    ssd_norm_g = 1.0 + 0.05 * jax.random.normal(ks[9], (DEPTH, SSD_INNER), f32)
    w_out = jax.random.normal(ks[10], (DEPTH, D_MIX, D_MODEL), f32) * (D_MIX ** -0.5) * DEEPNORM_BETA
    ln1_g = 1.0 + 0.05 * jax.random.normal(ks[11], (DEPTH, D_MODEL), f32)
    ln1_b = 0.02 * jax.random.normal(ks[12], (DEPTH, D_MODEL), f32)
    router_group_w = jax.random.normal(ks[13], (DEPTH, D_MODEL, N_EXPERT_GROUPS), f32) * (D_MODEL ** -0.5)
    router_group_b = 0.01 * jax.random.normal(ks[14], (DEPTH, N_EXPERT_GROUPS), f32)
    router_expert_w = jax.random.normal(ks[15], (DEPTH, N_EXPERT_GROUPS, D_MODEL, EXPERTS_PER_GROUP), f32) * (D_MODEL ** -0.5)
    router_expert_b = 0.01 * jax.random.normal(ks[16], (DEPTH, N_EXPERT_GROUPS, EXPERTS_PER_GROUP), f32)
    kw = jax.random.split(ks[17], 3)
    w_gate = jax.random.normal(kw[0], (DEPTH, N_EXPERTS, D_MODEL, D_FF_EXPERT), f32) * (D_MODEL ** -0.5)
    w_up = jax.random.normal(kw[1], (DEPTH, N_EXPERTS, D_MODEL, D_FF_EXPERT), f32) * (D_MODEL ** -0.5)
    w_down = jax.random.normal(kw[2], (DEPTH, N_EXPERTS, D_FF_EXPERT, D_MODEL), f32) * (D_FF_EXPERT ** -0.5) * DEEPNORM_BETA
    ln2_g = 1.0 + 0.05 * jax.random.normal(ks[18], (DEPTH, D_MODEL), f32)
    ln2_b = 0.02 * jax.random.normal(ks[19], (DEPTH, D_MODEL), f32)
    return {"x": x, "w_in": w_in, "b_in": b_in, "conv_w": conv_w, "conv_b": conv_b,
            "a_log": a_log, "d_skip": d_skip, "ssd_norm_g": ssd_norm_g, "w_out": w_out,
            "ln1_g": ln1_g, "ln1_b": ln1_b, "router_group_w": router_group_w,
            "router_group_b": router_group_b, "router_expert_w": router_expert_w,
            "router_expert_b": router_expert_b, "w_gate": w_gate, "w_up": w_up,
            "w_down": w_down, "ln2_g": ln2_g, "ln2_b": ln2_b}


def reference(x, w_in, b_in, conv_w, conv_b, a_log, d_skip, ssd_norm_g, w_out,
              ln1_g, ln1_b, router_group_w, router_group_b, router_expert_w,
              router_expert_b, w_gate, w_up, w_down, ln2_g, ln2_b):
    h = x
    for l in range(DEPTH):
        mix = _hybrid_mixer(h, w_in[l], b_in[l], conv_w[l], conv_b[l], a_log[l],
                            d_skip[l], ssd_norm_g[l], w_out[l])
        h = _layer_norm(DEEPNORM_ALPHA * h + mix, ln1_g[l], ln1_b[l])
        ffn = _hier_moe(h, router_group_w[l], router_group_b[l], router_expert_w[l],
                        router_expert_b[l], w_gate[l], w_up[l], w_down[l])
        h = _layer_norm(DEEPNORM_ALPHA * h + ffn, ln2_g[l], ln2_b[l])
    return h
```

```python
import os
import numpy as np
import concourse.bass as bass
import concourse.mybir as mybir
from concourse.bass_utils import run_bass_kernel_spmd

F32 = mybir.dt.float32
BF16 = mybir.dt.bfloat16
U8 = mybir.dt.uint8
AF = mybir.ActivationFunctionType
ALU = mybir.AluOpType
AX = mybir.AxisListType

NCORES = 8
NSEQ = 2
SEQ = 2048
NT = 16
DM = 1024
DIN = 3088
ALPHA = float(2.0 ** 0.25)
LN_EPS = 1e-5
RMS_EPS = 1e-5
ATT_SCALE = 0.125
NEG = -30000.0


class Op:
    __slots__ = ("eng", "fn", "reads", "writes", "deps", "signal", "sigval", "dma", "gi", "nofence")


class Sched:
    COMPUTE = ("pe", "act", "dve", "pool")

    def __init__(self, nc, n_dma_sems=40):
        self.nc = nc
        self.h = {"pe": nc.tensor, "act": nc.scalar, "dve": nc.vector, "pool": nc.gpsimd, "sp": nc.sync}
        self.ops = []
        self.last_w = {}
        self.readers = {}
        self.n_dma_sems = n_dma_sems
        self.live_dma = []
        self.nfence = 0

    def add(self, eng, fn, reads=(), writes=(), dma=False, nofence=False):
        o = Op()
        o.eng = eng; o.fn = fn; o.reads = tuple(reads); o.writes = tuple(writes)
        o.deps = []; o.signal = False; o.sigval = None; o.dma = dma; o.gi = len(self.ops); o.nofence = nofence
        for r in o.reads:
            p = self.last_w.get(r)
            if p is not None:
                self._dep(o, p, True)
            if r.startswith("ps"):
                rd = self.readers.get(r)
                if rd:
                    for q in rd.values():
                        if q.eng != eng:
                            self._dep(o, q, True)
        for w in o.writes:
            p = self.last_w.get(w)
            if p is not None:
                self._dep(o, p, False)
            rd = self.readers.get(w)
            if rd:
                for q in rd.values():
                    self._dep(o, q, False)
        for r in o.reads:
            d = self.readers.setdefault(r, {})
            d[("dma", o.gi) if dma else eng] = o
        for w in o.writes:
            self.last_w[w] = o
            self.readers[w] = {}
        self.ops.append(o)
        if dma and not nofence:
            self.live_dma.append(o)
        return o

    def _dep(self, o, p, raw):
        if p is o:
            return
        if (not p.dma) and (not o.dma) and p.eng == o.eng:
            if o.eng == "pe":
                return
        o.deps.append(p)
        p.signal = True

    def pe(self, fn, reads=(), writes=()): return self.add("pe", fn, reads, writes)
    def act(self, fn, reads=(), writes=()): return self.add("act", fn, reads, writes)
    def dve(self, fn, reads=(), writes=()): return self.add("dve", fn, reads, writes)
    def pool(self, fn, reads=(), writes=()): return self.add("pool", fn, reads, writes)
    def dma(self, q, fn, reads=(), writes=(), nofence=False):
        return self.add(q, fn, reads, writes, dma=True, nofence=nofence)

    def fence(self, scratch):
        n = self.nfence; self.nfence += 1
        a_keys = []
        col = {"pe": None, "act": 0, "dve": 1, "pool": 2}
        for e in ("act", "dve", "pool"):
            k = "fenceA.%d.%s" % (n, e)
            c = col[e]
            if e == "act":
                self.add(e, (lambda eh, c=c: eh.activation(out=scratch[:, c:c + 1], in_=scratch[:, 8:9], func=AF.Copy)), reads=(), writes=(k,))
            else:
                self.add(e, (lambda eh, c=c: eh.memset(scratch[:, c:c + 1], 0.0)), reads=(), writes=(k,))
            a_keys.append(k)
        k = "fenceA.%d.pe" % n
        self.add("pe", (lambda eh: eh.matmul(self.fence_ps[0:1, 0:1], lhsT=self.fence_w[0:1, 0:1], rhs=self.fence_w[0:1, 0:1], start=True, stop=True)),
                 reads=(), writes=(k, "ps7"))
        a_keys.append(k)
        dmas = self.live_dma
        self.live_dma = []
        for e in ("act", "dve", "pool", "pe", "sp"):
            kb = "fenceB.%d.%s" % (n, e)
            if e == "act":
                o = self.add(e, (lambda eh: eh.activation(out=scratch[:, 3:4], in_=scratch[:, 8:9], func=AF.Copy)), reads=a_keys, writes=(kb,))
            elif e == "pe":
                o = self.add(e, (lambda eh: eh.matmul(self.fence_ps[0:1, 1:2], lhsT=self.fence_w[0:1, 0:1], rhs=self.fence_w[0:1, 0:1], start=True, stop=True)),
                             reads=a_keys, writes=(kb, "ps7"))
            elif e == "sp":
                o = self.add(e, (lambda eh: eh.nop()), reads=a_keys, writes=(kb,))
            else:
                c = 4 if e == "dve" else 5
                o = self.add(e, (lambda eh, c=c: eh.memset(scratch[:, c:c + 1], 0.0)), reads=a_keys, writes=(kb,))
            for d in dmas:
                o.deps.append(d)

    def emit(self, final_wait_ops=()):
        nc = self.nc
        esem = {e: nc.alloc_semaphore("s_" + e) for e in self.COMPUTE}
        dsems = [nc.alloc_semaphore("s_dma%d" % i) for i in range(self.n_dma_sems)]
        dtotal = [0] * self.n_dma_sems
        dlast = [None] * self.n_dma_sems
        ecount = {e: 0 for e in self.COMPUTE}
        nd = 0
        nq = {"sp": 0, "pool": 0}
        half = self.n_dma_sems // 2
        for o in self.ops:
            if o.dma:
                qi = nq[o.eng]; nq[o.eng] += 1; nd += 1
                i = (qi % half) + (0 if o.eng == "sp" else half)
                prev = dlast[i]
                if prev is not None:
                    o.deps.append(prev)
                dtotal[i] += 16
                o.sigval = (dsems[i], dtotal[i], 1000 + i)
                dlast[i] = o
            elif o.signal:
                ecount[o.eng] += 1
                o.sigval = (esem[o.eng], ecount[o.eng], o.eng)
        known = {e: {} for e in self.h}
        nwaits = 0
        for o in self.ops:
            eh = self.h[o.eng]
            kn = known[o.eng]
            need = {}
            for p in o.deps:
                s, v, key = p.sigval
                if kn.get(key, 0) >= v:
                    continue
                if key not in need or need[key][1] < v:
                    need[key] = (s, v)
            for key, (s, v) in need.items():
                eh.wait_ge(s, v)
                kn[key] = v
                nwaits += 1
            ins = o.fn(eh)
            if o.dma:
                ins.then_inc(o.sigval[0], 16)
            elif o.signal:
                ins.then_inc(o.sigval[0], 1)
        eh = self.h["sp"]
        for o in final_wait_ops:
            s, v, key = o.sigval
            eh.wait_ge(s, v)
        self.stats = dict(n_ops=len(self.ops), n_waits=nwaits, counts=dict(ecount), n_dma=nd)
        return self.stats


class Arena:
    def __init__(self, nc, name, nbytes):
        self.t = nc.alloc_sbuf_tensor(name, [128, nbytes], U8)
        self.n = nbytes
        self.off = 0

    def reset(self, off=0):
        self.off = off

    def alloc(self, shape, dtype, parts=128):
        esz = 2 if dtype == BF16 else 4
        n = esz
        for s in shape:
            n *= s
        off = (self.off + 31) // 32 * 32
        assert off + n <= self.n, (off, n, self.n)
        self.off = off + n
        flat = self.t[0:parts, off:off + n].bitcast(dtype)
        if len(shape) == 1:
            return flat
        names = " ".join("a%d" % i for i in range(len(shape)))
        kw = {"a%d" % i: shape[i] for i in range(1, len(shape))}
        return flat.rearrange("p (%s) -> p %s" % (names, names), **kw)


def build_program(dbg=False):
    nc = bass.Bass("TRN2", target_bir_lowering=False)
    S = Sched(nc)
    D = {}

    def din(name, shape):
        D[name] = nc.dram_tensor(name, list(shape), F32, kind="ExternalInput").ap()
        return D[name]

    x = din("x", [NSEQ, SEQ, DM])
    w_in = din("w_in", [DM, DIN])
    b_in = din("b_in", [1, DIN])
    conv_w = din("conv_w", [4, 1024])
    conv_b = din("conv_b", [1, 1024])
    a_log = din("a_log", [1, 8])
    d_skip = din("d_skip", [1, 8])
    ssd_g = din("ssd_norm_g", [1, 512])
    w_out = din("w_out", [DM, DM])
    ln1_g = din("ln1_g", [1, DM]); ln1_b = din("ln1_b", [1, DM])
    rg_w = din("router_group_w", [DM, 4]); rg_b = din("router_group_b", [1, 4])
    re_w = din("router_expert_w", [4, DM, 4]); re_b = din("router_expert_b", [1, 16])
    w_gate = din("w_gate", [16, DM, 512]); w_up = din("w_up", [16, DM, 512]); w_down = din("w_down", [16, 512, DM])
    ln2_g = din("ln2_g", [1, DM]); ln2_b = din("ln2_b", [1, DM])
    out = nc.dram_tensor("out", [NSEQ, SEQ, DM], F32, kind="ExternalOutput").ap()
    h_scr = nc.dram_tensor("h_scr", [NSEQ, SEQ, DM], F32).ap()
    dbg_t = {}

    def dbg_out(name, shape):
        dbg_t[name] = nc.dram_tensor(name, list(shape), F32, kind="ExternalOutput").ap()
        return dbg_t[name]

    CONST = Arena(nc, "CONST", 22 * 1024)
    RW = Arena(nc, "RW", 49408)
    RX = Arena(nc, "RX", 32768)
    RACC = Arena(nc, "RACC", 65536)
    MSSD = Arena(nc, "MSSD", 16384)
    TT = Arena(nc, "TT", nc.sbuf_bytes_remaining - 256)

    identB = CONST.alloc([128], BF16); identF = CONST.alloc([128], F32)
    Uf = CONST.alloc([128], F32); triU = CONST.alloc([128], BF16); maskneg = CONST.alloc([128], BF16)
    onesF = CONST.alloc([128], F32)
    ones_row = CONST.alloc([128], BF16, parts=1)
    bz_row = CONST.alloc([512], BF16, parts=1); bv_row = CONST.alloc([512], BF16, parts=1)
    bdtf = CONST.alloc([16], F32)
    bxbc = CONST.alloc([8], F32); bq = CONST.alloc([4], F32); bk = CONST.alloc([4], F32)
    convw = CONST.alloc([8, 4], F32); convb = CONST.alloc([8], F32)
    a_bc = CONST.alloc([8], F32); dskip_bc = CONST.alloc([8], F32)
    gssd_bc = CONST.alloc([512], F32)
    lnG = CONST.alloc([1024], F32); lnB = CONST.alloc([1024], F32)
    rw = CONST.alloc([8, 20], F32); rb_bc = CONST.alloc([20], F32)
    logits = CONST.alloc([NT, 20], F32); comb = CONST.alloc([NT, 16], F32)
    fsc = CONST.alloc([16], F32)
    S.fence_w = CONST.alloc([8], BF16)
    pd = [nc.alloc_psum_tensor("pd%d" % i, [128, 1024], F32) for i in range(4)]
    def bank(i):
        return pd[i // 2][:, (i % 2) * 512:(i % 2) * 512 + 512]
    def bankb(i):
        return bank(i).bitcast(BF16)
    PSK = ["ps%d" % i for i in range(8)]
    S.fence_ps = nc.alloc_sbuf_tensor("fence_dummy", [1, 8], F32)
    S.fence_ps = bank(7)[:, 504:512]

    def fence():
        S.fence(fsc)

    S.pool(lambda e: e.memset(fsc[:, :], 0.0), writes=["fsc"])
    S.pool(lambda e: e.memset(S.fence_w[:, :], 0.0), writes=["fence_w"])
    S.pool(lambda e: e.memset(identB[:, :], 1.0), writes=["identB"])
    S.pool(lambda e: e.affine_select(out=identB[:, :], in_=identB[:, :], pattern=[[-1, 128]], compare_op=ALU.is_equal, fill=0.0, base=0, channel_multiplier=1), reads=["identB"], writes=["identB"])
    S.pool(lambda e: e.memset(identF[:, :], 1.0), writes=["identF"])
    S.pool(lambda e: e.affine_select(out=identF[:, :], in_=identF[:, :], pattern=[[-1, 128]], compare_op=ALU.is_equal, fill=0.0, base=0, channel_multiplier=1), reads=["identF"], writes=["identF"])
    S.pool(lambda e: e.memset(Uf[:, :], 1.0), writes=["Uf"])
    S.pool(lambda e: e.affine_select(out=Uf[:, :], in_=Uf[:, :], pattern=[[1, 128]], compare_op=ALU.is_ge, fill=0.0, base=0, channel_multiplier=-1), reads=["Uf"], writes=["Uf"])
    S.pool(lambda e: e.memset(triU[:, :], 1.0), writes=["triU"])
    S.pool(lambda e: e.affine_select(out=triU[:, :], in_=triU[:, :], pattern=[[1, 128]], compare_op=ALU.is_ge, fill=0.0, base=0, channel_multiplier=-1), reads=["triU"], writes=["triU"])
    S.pool(lambda e: e.memset(maskneg[:, :], NEG), writes=["maskneg"])
    S.pool(lambda e: e.affine_select(out=maskneg[:, :], in_=maskneg[:, :], pattern=[[-1, 128]], compare_op=ALU.is_gt, fill=0.0, base=0, channel_multiplier=1), reads=["maskneg"], writes=["maskneg"])
    S.pool(lambda e: e.memset(onesF[:, :], 1.0), writes=["onesF"])
    S.pool(lambda e: e.memset(ones_row[:, :], 1.0), writes=["ones_row"])
    S.dma("pool", lambda e: e.dma_start(out=bz_row[:, :], in_=b_in[0:1, 0:512]), writes=["bz_row"])
    S.dma("pool", lambda e: e.dma_start(out=bv_row[:, :], in_=b_in[0:1, 2568:3080]), writes=["bv_row"])
    S.dma("sp", lambda e: e.dma_start(out=bdtf[:, 0:8], in_=b_in[0:1, 1536:1544].to_broadcast([128, 8])), writes=["bdtf0"])
    S.dma("sp", lambda e: e.dma_start(out=bdtf[:, 8:16], in_=b_in[0:1, 3080:3088].to_broadcast([128, 8])), writes=["bdtf1"])
    S.dma("sp", lambda e: e.dma_start(out=bxbc[:, :], in_=b_in[0, 512:1536].rearrange("(c p) -> p c", p=128)), writes=["bxbc"])
    S.dma("sp", lambda e: e.dma_start(out=bq[:, :], in_=b_in[0, 1544:2056].rearrange("(c p) -> p c", p=128)), writes=["bq"])
    S.dma("sp", lambda e: e.dma_start(out=bk[:, :], in_=b_in[0, 2056:2568].rearrange("(c p) -> p c", p=128)), writes=["bk"])
    for k in range(4):
        S.dma("sp", lambda e, k=k: e.dma_start(out=convw[:, :, k], in_=conv_w[k, :].rearrange("(c p) -> p c", p=128)), writes=["convw%d" % k])
    CONVW = ["convw%d" % k for k in range(4)]
    S.dma("sp", lambda e: e.dma_start(out=convb[:, :], in_=conv_b[0, :].rearrange("(c p) -> p c", p=128)), writes=["convb"])
    S.dma("sp", lambda e: e.dma_start(out=a_bc[:, :], in_=a_log[0:1, :].to_broadcast([128, 8])), writes=["a_bc"])
    S.act(lambda e: e.activation(out=a_bc[:, :], in_=a_bc[:, :], func=AF.Exp), reads=["a_bc"], writes=["a_bc"])
    S.dve(lambda e: e.tensor_scalar(out=a_bc[:, :], in0=a_bc[:, :], scalar1=-1.0, scalar2=None, op0=ALU.mult), reads=["a_bc"], writes=["a_bc"])
    S.dma("sp", lambda e: e.dma_start(out=dskip_bc[:, :], in_=d_skip[0:1, :].to_broadcast([128, 8])), writes=["dskip_bc"])
    S.dma("sp", lambda e: e.dma_start(out=gssd_bc[:, :], in_=ssd_g[0:1, :].to_broadcast([128, 512])), writes=["gssd_bc"])
    S.dma("sp", lambda e: e.dma_start(out=rw[:, :, 0:4], in_=rg_w.rearrange("(k p) j -> p k j", p=128)), writes=["rw0"])
    for g in range(4):
        S.dma("sp", lambda e, g=g: e.dma_start(out=rw[:, :, 4 + 4 * g:8 + 4 * g], in_=re_w[g].rearrange("(k p) j -> p k j", p=128)), writes=["rw%d" % (g + 1)])
    RWK = ["rw%d" % i for i in range(5)]
    S.dma("sp", lambda e: e.dma_start(out=rb_bc[:, 0:4], in_=rg_b[0:1, :].to_broadcast([128, 4])), writes=["rb0"])
    S.dma("sp", lambda e: e.dma_start(out=rb_bc[:, 4:20], in_=re_b[0:1, :].to_broadcast([128, 16])), writes=["rb1"])

    outs = []

    for s in range(NSEQ):
        RW.reset(); RX.reset(); RACC.reset(); MSSD.reset(); TT.reset()
        wA = RW.alloc([8, 1544], BF16)
        xb = [RW.alloc([4, 1024], BF16) for _ in range(2)]
        xT = RX.alloc([8, SEQ], BF16)
        sz = RACC.alloc([NT, 512], BF16)
        xsB = RACC.alloc([NT, 768], BF16)
        BT = RACC.alloc([2, SEQ], BF16)
        CT = RACC.alloc([2, SEQ], BF16)
        xsT = MSSD.alloc([4, SEQ], BF16)
        dt_t = TT.alloc([NT, 8], F32)
        tt_mark = TT.off
        pre = [TT.alloc([SEQ + 3], BF16) for _ in range(2)]
        cacc = [TT.alloc([SEQ], F32) for _ in range(2)]
        dt_raw = TT.alloc([NT, 8], F32)

        for half in range(2):
            S.dma("pool", lambda e, half=half: e.dma_start(out=wA[:, 4 * half:4 * half + 4, :], in_=w_in.rearrange("(k p) c -> p k c", p=128)[:, 4 * half:4 * half + 4, 0:1544]),
                  writes=["wA"])
        for b in range(2):
            S.pool(lambda e, b=b: e.memset(pre[b][:, 0:3], 0.0), writes=["pre%d" % b])
        ev = 0
        for blk in range(4):
            S.dma("pool", lambda e, blk=blk, s=s: e.dma_start(out=xb[blk % 2][:, :, :], in_=x[s, blk * 512:(blk + 1) * 512, :].rearrange("(t p) d -> p t d", p=128)),
                  writes=["xb%d" % (blk % 2)])
            for k in range(8):
                pb = (blk * 8 + k) % 2
                for t in range(4):
                    S.pe(lambda e, pb=pb, t=t, k=k, blk=blk: e.transpose(bankb(pb)[:, t * 128:(t + 1) * 128], xb[blk % 2][:, t, k * 128:(k + 1) * 128], identB[:, :]),
                         reads=["xb%d" % (blk % 2), "identB"], writes=[PSK[pb]])
                if ev % 2 == 0:
                    S.act(lambda e, pb=pb, k=k, blk=blk: e.copy(xT[:, k, blk * 512:(blk + 1) * 512], bankb(pb)[:, 0:512]), reads=[PSK[pb]], writes=["xT.%d.%d" % (k, blk)])
                else:
                    S.dve(lambda e, pb=pb, k=k, blk=blk: e.tensor_copy(xT[:, k, blk * 512:(blk + 1) * 512], bankb(pb)[:, 0:512]), reads=[PSK[pb]], writes=["xT.%d.%d" % (k, blk)])
                ev += 1
            for tt in range(4):
                t = blk * 4 + tt
                pz = 2 + (t % 2)
                xk = ["xT.%d.%d" % (k, blk) for k in range(8)]
                for k in range(8):
                    S.pe(lambda e, pz=pz, t=t, k=k: e.matmul(bank(pz)[:, :], lhsT=xT[:, k, t * 128:(t + 1) * 128], rhs=wA[:, k, 0:512], start=(k == 0), stop=False),
                         reads=[xk[k], "wA"], writes=[PSK[pz]])
                S.pe(lambda e, pz=pz: e.matmul(bank(pz)[:, :], lhsT=ones_row[0:1, :], rhs=bz_row[0:1, :], start=False, stop=True),
                     reads=["ones_row", "bz_row"], writes=[PSK[pz]])
                S.act(lambda e, pz=pz, t=t: e.activation(out=sz[:, t, :], in_=bank(pz)[:, :], func=AF.Silu), reads=[PSK[pz]], writes=["sz.%d" % t])
                for k in range(8):
                    S.pe(lambda e, t=t, k=k: e.matmul(bank(4)[:, t * 8:(t + 1) * 8], lhsT=xT[:, k, t * 128:(t + 1) * 128], rhs=wA[:, k, 1536:1544], start=(k == 0), stop=(k == 7)),
                         reads=[xk[k], "wA"], writes=[PSK[4]])
        S.dve(lambda e: e.tensor_tensor(out=dt_raw[:, :, :], in0=bank(4)[:, 0:128].rearrange("p (t h) -> p t h", h=8), in1=bdtf[:, 0:8].unsqueeze(1).to_broadcast([128, NT, 8]), op=ALU.add),
              reads=[PSK[4], "bdtf0"], writes=["dt_raw"])
        XT_ALL = lambda blk: ["xT.%d.%d" % (k, blk) for k in range(8)]
        for c in range(8):
            pb_ = c % 2
            for blk in range(4):
                pc = 5 + (c * 4 + blk) % 2
                for k in range(8):
                    S.pe(lambda e, pc=pc, c=c, k=k, blk=blk: e.matmul(bank(pc)[:, :], lhsT=wA[:, k, 512 + c * 128:512 + (c + 1) * 128], rhs=xT[:, k, blk * 512:(blk + 1) * 512], start=(k == 0), stop=(k == 7)),
                         reads=["xT.%d.%d" % (k, blk), "wA"], writes=[PSK[pc]])
                S.act(lambda e, pc=pc, c=c, blk=blk, pb_=pb_: e.activation(out=pre[pb_][:, 3 + blk * 512:3 + (blk + 1) * 512], in_=bank(pc)[:, :], func=AF.Identity, bias=bxbc[:, c:c + 1], scale=1.0),
                      reads=[PSK[pc], "bxbc"], writes=["pre%d" % pb_])
            eng = S.dve
            ca = cacc[pb_]; pr = pre[pb_]
            eng(lambda e, ca=ca, pr=pr, c=c: e.tensor_scalar(out=ca[:, :], in0=pr[:, 3:SEQ + 3], scalar1=convw[:, c, 3:4], scalar2=None, op0=ALU.mult),
                reads=["pre%d" % pb_] + CONVW, writes=["cacc%d" % pb_])
            for kk in (2, 1, 0):
                eng(lambda e, ca=ca, pr=pr, c=c, kk=kk: e.scalar_tensor_tensor(out=ca[:, :], in0=pr[:, kk:SEQ + kk], scalar=convw[:, c, kk:kk + 1], in1=ca[:, :], op0=ALU.mult, op1=ALU.add),
                    reads=["pre%d" % pb_, "cacc%d" % pb_] + CONVW, writes=["cacc%d" % pb_])
            if c < 4:
                dst = xsT[:, c, :]; dk = "xsT.%d" % c
            elif c < 6:
                dst = BT[:, c - 4, :]; dk = "BT.%d" % (c - 4)
            else:
                dst = CT[:, c - 6, :]; dk = "CT.%d" % (c - 6)
            S.act(lambda e, ca=ca, dst=dst, c=c: e.activation(out=dst, in_=ca[:, :], func=AF.Silu, bias=convb[:, c:c + 1], scale=1.0),
                  reads=["cacc%d" % pb_, "convb"], writes=[dk])
        for t in range(NT):
            pb = t % 2
            for c in range(6):
                src = xsT[:, c, t * 128:(t + 1) * 128] if c < 4 else BT[:, c - 4, t * 128:(t + 1) * 128]
                sk = "xsT.%d" % c if c < 4 else "BT.%d" % (c - 4)
                S.pe(lambda e, pb=pb, c=c, src=src: e.transpose(bankb(pb)[:, c * 128:(c + 1) * 128], src, identB[:, :]), reads=[sk, "identB"], writes=[PSK[pb]])
            if t % 2 == 0:
                S.dve(lambda e, pb=pb, t=t: e.tensor_copy(xsB[:, t, :], bankb(pb)[:, 0:768]), reads=[PSK[pb]], writes=["xsB.%d" % t])
            else:
                S.act(lambda e, pb=pb, t=t: e.copy(xsB[:, t, :], bankb(pb)[:, 0:768]), reads=[PSK[pb]], writes=["xsB.%d" % t])
        S.act(lambda e: e.activation(out=dt_t[:, :, :], in_=dt_raw[:, :, :], func=AF.Exp), reads=["dt_raw"], writes=["dt_t"])
        S.act(lambda e: e.activation(out=dt_t[:, :, :], in_=dt_t[:, :, :], func=AF.Ln, bias=1.0, scale=1.0), reads=["dt_t"], writes=["dt_t"])

        fence()
        MSSD.reset(); TT.reset(tt_mark); RW.reset()
        m_ssd = MSSD.alloc([NT, 512], BF16)
        da = TT.alloc([NT, 8], F32); acum = TT.alloc([NT, 8], F32); nacum = TT.alloc([NT, 8], F32)
        alast = TT.alloc([NT, 8], F32); dte = TT.alloc([NT, 8], F32); ea = TT.alloc([NT, 8], F32)
        cdec = TT.alloc([NT, 8], F32); dtdte = TT.alloc([NT, 8], F32)
        stT = TT.alloc([8, 64], F32); stTb = TT.alloc([8, 64], BF16)
        LT = [TT.alloc([128], BF16) for _ in range(4)]
        MT = [TT.alloc([128], BF16) for _ in range(4)]
        xdt = [RW.alloc([8, 64], BF16) for _ in range(2)]
        xdtd = [RW.alloc([8, 64], BF16) for _ in range(2)]
        t1 = [RW.alloc([8, 64], F32) for _ in range(2)]
        t2 = [RW.alloc([8, 64], F32) for _ in range(2)]
        yg = [RW.alloc([512], F32) for _ in range(2)]
        junk = TT.alloc([256], F32)
        ss = [TT.alloc([2], F32) for _ in range(2)]
        rstd = [TT.alloc([2], F32) for _ in range(2)]

        S.dve(lambda e: e.tensor_tensor(out=da[:, :, :], in0=dt_t[:, :, :], in1=a_bc[:, :].unsqueeze(1).to_broadcast([128, NT, 8]), op=ALU.mult), reads=["dt_t", "a_bc"], writes=["da"])
        daf = da.rearrange("p t h -> p (t h)")
        S.pe(lambda e: e.matmul(bank(5)[:, 0:128], lhsT=Uf[:, :], rhs=daf, start=True, stop=True), reads=["Uf", "da"], writes=[PSK[5]])
        S.pe(lambda e: e.matmul(bank(6)[:, 0:128], lhsT=onesF[:, :], rhs=daf, start=True, stop=True), reads=["onesF", "da"], writes=[PSK[6]])
        S.dve(lambda e: e.tensor_copy(acum.rearrange("p t h -> p (t h)"), bank(5)[:, 0:128]), reads=[PSK[5]], writes=["acum"])
        S.dve(lambda e: e.tensor_scalar(out=nacum.rearrange("p t h -> p (t h)"), in0=bank(5)[:, 0:128], scalar1=-1.0, scalar2=None, op0=ALU.mult), reads=[PSK[5]], writes=["nacum"])
        S.dve(lambda e: e.tensor_copy(alast.rearrange("p t h -> p (t h)"), bank(6)[:, 0:128]), reads=[PSK[6]], writes=["alast"])
        S.dve(lambda e: e.tensor_tensor(out=dte[:, :, :], in0=alast[:, :, :], in1=acum[:, :, :], op=ALU.subtract), reads=["alast", "acum"], writes=["dte"])
        S.act(lambda e: e.activation(out=dte[:, :, :], in_=dte[:, :, :], func=AF.Exp), reads=["dte"], writes=["dte"])
        S.act(lambda e: e.activation(out=ea[:, :, :], in_=acum[:, :, :], func=AF.Exp), reads=["acum"], writes=["ea"])
        S.act(lambda e: e.activation(out=cdec[:, :, :], in_=alast[:, :, :], func=AF.Exp), reads=["alast"], writes=["cdec"])
        S.dve(lambda e: e.tensor_tensor(out=dtdte[:, :, :], in0=dt_t[:, :, :], in1=dte[:, :, :], op=ALU.mult), reads=["dt_t", "dte"], writes=["dtdte"])
        S.pool(lambda e: e.memset(stT[:, :, :], 0.0), writes=["stT"])
        S.pool(lambda e: e.memset(stTb[:, :, :], 0.0), writes=["stTb"])

        for c in range(NT):
            cs = slice(c * 128, (c + 1) * 128)
            b2 = c % 2
            xs_c = xsB[:, c, 0:512].rearrange("p (h d) -> p h d", h=8)
            S.pool(lambda e, c=c, b2=b2, xs_c=xs_c: e.tensor_tensor(out=xdt[b2][:, :, :], in0=xs_c, in1=dt_t[:, c, :].unsqueeze(2).to_broadcast([128, 8, 64]), op=ALU.mult),
                   reads=["xsB.%d" % c, "dt_t"], writes=["xdt%d" % b2])
            S.pool(lambda e, c=c, b2=b2, xs_c=xs_c: e.tensor_tensor(out=xdtd[b2][:, :, :], in0=xs_c, in1=dtdte[:, c, :].unsqueeze(2).to_broadcast([128, 8, 64]), op=ALU.mult),
                   reads=["xsB.%d" % c, "dtdte"], writes=["xdtd%d" % b2])
            S.pool(lambda e, c=c, b2=b2, xs_c=xs_c: e.tensor_tensor(out=t2[b2][:, :, :], in0=xs_c, in1=dskip_bc[:, :].unsqueeze(2).to_broadcast([128, 8, 64]), op=ALU.mult),
                   reads=["xsB.%d" % c, "dskip_bc"], writes=["t2%d" % b2])
            for g in range(2):
                S.pe(lambda e, g=g, cs=cs: e.matmul(bank(4)[:, g * 128:(g + 1) * 128], lhsT=BT[:, g, cs], rhs=CT[:, g, cs], start=True, stop=True),
                     reads=["BT.%d" % g, "CT.%d" % g], writes=[PSK[4]])
            for hh in range(2):
                pl = (c * 2 + hh) % 4
                for h4 in range(4):
                    h = hh * 4 + h4
                    S.pe(lambda e, pl=pl, h4=h4, c=c, h=h: e.matmul(bank(pl)[:, h4 * 128:(h4 + 1) * 128], lhsT=da[:, c, h:h + 1].to_broadcast([128, 128]), rhs=Uf[:, :], start=True, stop=False),
                         reads=["da", "Uf"], writes=[PSK[pl]])
                    S.pe(lambda e, pl=pl, h4=h4: e.matmul(bank(pl)[:, h4 * 128:(h4 + 1) * 128], lhsT=identB[:, :], rhs=maskneg[:, :], start=False, stop=True),
                         reads=["identB", "maskneg"], writes=[PSK[pl]])
                for h4 in range(4):
                    h = hh * 4 + h4
                    S.act(lambda e, pl=pl, h4=h4, c=c, h=h: e.activation(out=LT[h4][:, :], in_=bank(pl)[:, h4 * 128:(h4 + 1) * 128], func=AF.Exp, bias=nacum[:, c, h:h + 1], scale=1.0),
                          reads=[PSK[pl], "nacum"], writes=["LT%d" % h4])
                    g = h // 4
                    S.dve(lambda e, h4=h4, g=g: e.tensor_tensor(out=MT[h4][:, :], in0=bank(4)[:, g * 128:(g + 1) * 128], in1=LT[h4][:, :], op=ALU.mult),
                          reads=[PSK[4], "LT%d" % h4], writes=["MT%d" % h4])
                    S.pe(lambda e, h4=h4, h=h, b2=b2: e.matmul(bank(5)[:, h * 64:(h + 1) * 64], lhsT=MT[h4][:, :], rhs=xdt[b2][:, h, :], start=True, stop=True),
                         reads=["MT%d" % h4, "xdt%d" % b2], writes=[PSK[5]])
            if c > 0:
                for g in range(2):
                    S.pe(lambda e, g=g, cs=cs: e.matmul(bank(6)[:, g * 256:(g + 1) * 256], lhsT=CT[:, g, cs], rhs=stTb[:, 4 * g:4 * g + 4, :].rearrange("p h d -> p (h d)"), start=True, stop=True),
                         reads=["CT.%d" % g, "stTb"], writes=[PSK[6]])
                S.dve(lambda e, c=c, b2=b2: e.tensor_tensor(out=t1[b2][:, :, :], in0=bank(6)[:, :].rearrange("p (h d) -> p h d", h=8), in1=ea[:, c, :].unsqueeze(2).to_broadcast([128, 8, 64]), op=ALU.mult),
                      reads=[PSK[6], "ea"], writes=["t1%d" % b2])
                S.dve(lambda e, b2=b2: e.tensor_tensor(out=t1[b2][:, :, :], in0=bank(5)[:, :].rearrange("p (h d) -> p h d", h=8), in1=t1[b2][:, :, :], op=ALU.add),
                      reads=[PSK[5], "t1%d" % b2], writes=["t1%d" % b2])
                S.dve(lambda e, b2=b2: e.tensor_tensor(out=t1[b2][:, :, :], in0=t1[b2][:, :, :], in1=t2[b2][:, :, :], op=ALU.add),
                      reads=["t1%d" % b2, "t2%d" % b2], writes=["t1%d" % b2])
            else:
                S.dve(lambda e, b2=b2: e.tensor_tensor(out=t1[b2][:, :, :], in0=bank(5)[:, :].rearrange("p (h d) -> p h d", h=8), in1=t2[b2][:, :, :], op=ALU.add),
                      reads=[PSK[5], "t2%d" % b2], writes=["t1%d" % b2])
            S.dve(lambda e, c=c, b2=b2: e.tensor_tensor(out=yg[b2][:, :], in0=t1[b2].rearrange("p h d -> p (h d)"), in1=sz[:, c, :], op=ALU.mult),
                  reads=["t1%d" % b2, "sz.%d" % c], writes=["yg%d" % b2])
            for g in range(2):
                S.act(lambda e, g=g, b2=b2: e.activation(out=junk[:, :], in_=yg[b2][:, g * 256:(g + 1) * 256], func=AF.Square, accum_out=ss[b2][:, g:g + 1]),
                      reads=["yg%d" % b2], writes=["junk", "ss%d.%d" % (b2, g)])
            S.act(lambda e, b2=b2: e.activation(out=rstd[b2][:, :], in_=ss[b2][:, :], func=AF.Ln, bias=RMS_EPS, scale=1.0 / 256.0),
                  reads=["ss%d.0" % b2, "ss%d.1" % b2], writes=["rstd%d" % b2])
            S.act(lambda e, b2=b2: e.activation(out=rstd[b2][:, :], in_=rstd[b2][:, :], func=AF.Exp, scale=-0.5), reads=["rstd%d" % b2], writes=["rstd%d" % b2])
            for g in range(2):
                S.dve(lambda e, g=g, b2=b2, c=c: e.scalar_tensor_tensor(out=m_ssd[:, c, g * 256:(g + 1) * 256], in0=yg[b2][:, g * 256:(g + 1) * 256], scalar=rstd[b2][:, g:g + 1], in1=gssd_bc[:, g * 256:(g + 1) * 256], op0=ALU.mult, op1=ALU.mult),
                      reads=["yg%d" % b2, "rstd%d" % b2, "gssd_bc"], writes=["m_ssd.%d" % c])
            if c < NT - 1:
                for g in range(2):
                    S.pe(lambda e, g=g, c=c, b2=b2: e.matmul(bank(7)[:, g * 256:(g + 1) * 256], lhsT=xsB[:, c, 512 + g * 128:512 + (g + 1) * 128], rhs=xdtd[b2][:, 4 * g:4 * g + 4, :].rearrange("p h d -> p (h d)"), start=True, stop=True),
                         reads=["xsB.%d" % c, "xdtd%d" % b2], writes=[PSK[7]])
                S.dve(lambda e, c=c: e.tensor_tensor(out=stT[:, :, :], in0=stT[:, :, :], in1=cdec[:, c, :].unsqueeze(2).to_broadcast([128, 8, 64]), op=ALU.mult),
                      reads=["stT", "cdec"], writes=["stT"])
                S.dve(lambda e: e.tensor_tensor(out=stT[:, :, :], in0=bank(7)[:, 0:512].rearrange("p (h d) -> p h d", h=8), in1=stT[:, :, :], op=ALU.add),
                      reads=[PSK[7], "stT"], writes=["stT"])
                S.pool(lambda e: e.tensor_copy(stTb[:, :, :], stT[:, :, :]), reads=["stT"], writes=["stTb"])

        if dbg == 'ssd' and s == 0:
            RX.reset()
            dm = dbg_out("dbg_mssd", [128, NT * 512])
            cvt = RX.alloc([NT * 512], F32) if dbg == 'ssd' else None
            S.dve(lambda e: e.tensor_copy(cvt[:, :], m_ssd.rearrange("p t d -> p (t d)")), reads=["m_ssd.%d" % c for c in range(NT)], writes=["cvt"])
            outs.append(S.dma("sp", lambda e: e.dma_start(out=dm[:, :], in_=cvt[:, :]), reads=["cvt"]))
        if dbg == "ssd":
            break

        fence()
        RW.reset(); RACC.reset(); TT.reset()
        wB = RW.alloc([8, 1544], BF16)
        wo = RW.alloc([8, 1024], BF16)
        ah = RW.alloc([1024], F32)
        hTf = RW.alloc([8, 128], F32)
        qT = RACC.alloc([4, SEQ], BF16)
        kT = RACC.alloc([4, SEQ], BF16)
        v_aug = RACC.alloc([NT, 8, 65], BF16)
        xres = [RACC.alloc([1024], F32) for _ in range(2)]
        hpre = [RACC.alloc([1024], F32), TT.alloc([1024], F32)]
        f_raw = TT.alloc([NT, 8], F32); Gc = TT.alloc([NT, 8], F32); tot = TT.alloc([NT, 8], F32)
        Pp = TT.alloc([NT, 8], F32); Gf = TT.alloc([NT, 8], F32); Gend = TT.alloc([NT, 8], F32)
        biasT = TT.alloc([8, NT, NT], F32)
        PT = [TT.alloc([4, 128], BF16) for _ in range(3)]
        yatt = [TT.alloc([8, 64], BF16) for _ in range(2)]
        rden = [TT.alloc([8], F32) for _ in range(2)]
        mT = [TT.alloc([8, 128], BF16) for _ in range(2)]
        bst = [TT.alloc([2, 6], F32) for _ in range(2)]
        mv = [TT.alloc([2], F32) for _ in range(2)]
        rs1 = [TT.alloc([1], F32) for _ in range(2)]

        for half in range(2):
            S.dma("pool", lambda e, half=half: e.dma_start(out=wB[:, 4 * half:4 * half + 4, :], in_=w_in.rearrange("(k p) c -> p k c", p=128)[:, 4 * half:4 * half + 4, 1544:3088]),
                  writes=["wB"])
            S.dma("pool", lambda e, half=half: e.dma_start(out=wo[:, 4 * half:4 * half + 4, :], in_=w_out.rearrange("(k p) c -> p k c", p=128)[:, 4 * half:4 * half + 4, :]),
                  writes=["wo"])
        S.dma("sp", lambda e: e.dma_start(out=lnG[:, :], in_=ln1_g[0:1, :].to_broadcast([128, 1024])), writes=["lnG"])
        S.dma("sp", lambda e: e.dma_start(out=lnB[:, :], in_=ln1_b[0:1, :].to_broadcast([128, 1024])), writes=["lnB"])
        S.pool(lambda e: e.memset(v_aug[:, :, :, 64:65], 1.0), writes=["v_ones"])
        evq = 0
        for qk in range(2):
            dstT = qT if qk == 0 else kT
            bcol = bq if qk == 0 else bk
            nm = "qT" if qk == 0 else "kT"
            for p in range(4):
                for blk in range(4):
                    pq = evq % 4
                    for k in range(8):
                        S.pe(lambda e, pq=pq, k=k, p=p, blk=blk, qk=qk: e.matmul(bank(pq)[:, :], lhsT=wB[:, k, qk * 512 + p * 128:qk * 512 + (p + 1) * 128], rhs=xT[:, k, blk * 512:(blk + 1) * 512], start=(k == 0), stop=(k == 7)),
                             reads=["xT.%d.%d" % (k, blk), "wB"], writes=[PSK[pq]])
                    if evq % 2 == 0:
                        S.act(lambda e, pq=pq, p=p, blk=blk, dstT=dstT, bcol=bcol: e.activation(out=dstT[:, p, blk * 512:(blk + 1) * 512], in_=bank(pq)[:, :], func=AF.Identity, bias=bcol[:, p:p + 1], scale=1.0),
                              reads=[PSK[pq], "bq", "bk"], writes=["%s.%d.%d" % (nm, p, blk)])
                    else:
                        S.dve(lambda e, pq=pq, p=p, blk=blk, dstT=dstT, bcol=bcol: e.tensor_scalar(out=dstT[:, p, blk * 512:(blk + 1) * 512], in0=bank(pq)[:, :], scalar1=bcol[:, p:p + 1], scalar2=None, op0=ALU.add),
                              reads=[PSK[pq], "bq", "bk"], writes=["%s.%d.%d" % (nm, p, blk)])
                    evq += 1
        for t in range(NT):
            pv = 4 + (t % 2)
            blk = t // 4
            for k in range(8):
                S.pe(lambda e, pv=pv, t=t, k=k: e.matmul(bank(pv)[:, :], lhsT=xT[:, k, t * 128:(t + 1) * 128], rhs=wB[:, k, 1024:1536], start=(k == 0), stop=False),
                     reads=["xT.%d.%d" % (k, blk), "wB"], writes=[PSK[pv]])
            S.pe(lambda e, pv=pv: e.matmul(bank(pv)[:, :], lhsT=ones_row[0:1, :], rhs=bv_row[0:1, :], start=False, stop=True),
                 reads=["ones_row", "bv_row"], writes=[PSK[pv]])
            if t % 2 == 0:
                S.act(lambda e, pv=pv, t=t: e.copy(v_aug[:, t, :, 0:64], bank(pv)[:, :].rearrange("p (h d) -> p h d", h=8)), reads=[PSK[pv]], writes=["v.%d" % t])
            else:
                S.dve(lambda e, pv=pv, t=t: e.tensor_copy(v_aug[:, t, :, 0:64], bank(pv)[:, :].rearrange("p (h d) -> p h d", h=8)), reads=[PSK[pv]], writes=["v.%d" % t])
            for k in range(8):
                S.pe(lambda e, t=t, k=k: e.matmul(bank(6)[:, t * 8:(t + 1) * 8], lhsT=xT[:, k, t * 128:(t + 1) * 128], rhs=wB[:, k, 1536:1544], start=(k == 0), stop=(k == 7)),
                     reads=["xT.%d.%d" % (k, blk), "wB"], writes=[PSK[6]])
        S.dve(lambda e: e.tensor_tensor(out=f_raw[:, :, :], in0=bank(6)[:, 0:128].rearrange("p (t h) -> p t h", h=8), in1=bdtf[:, 8:16].unsqueeze(1).to_broadcast([128, NT, 8]), op=ALU.add),
              reads=[PSK[6], "bdtf1"], writes=["f_raw"])
        S.act(lambda e: e.activation(out=f_raw[:, :, :], in_=f_raw[:, :, :], func=AF.Exp, scale=-1.0), reads=["f_raw"], writes=["f_raw"])
        S.act(lambda e: e.activation(out=f_raw[:, :, :], in_=f_raw[:, :, :], func=AF.Ln, bias=1.0, scale=1.0), reads=["f_raw"], writes=["f_raw"])
        frf = f_raw.rearrange("p t h -> p (t h)")
        S.pe(lambda e: e.matmul(bank(7)[:, 0:128], lhsT=Uf[:, :], rhs=frf, start=True, stop=True), reads=["Uf", "f_raw"], writes=[PSK[7]])
        S.pe(lambda e: e.matmul(bank(7)[:, 128:256], lhsT=onesF[:, :], rhs=frf, start=True, stop=True), reads=["onesF", "f_raw"], writes=[PSK[7]])
        S.dve(lambda e: e.tensor_copy(Gc.rearrange("p t h -> p (t h)"), bank(7)[:, 0:128]), reads=[PSK[7]], writes=["Gc"])
        S.dve(lambda e: e.tensor_copy(tot.rearrange("p t h -> p (t h)"), bank(7)[:, 128:256]), reads=[PSK[7]], writes=["tot"])
        S.pool(lambda e: e.memset(Pp[:, 0, :], 0.0), writes=["Pp"])
        for t in range(1, NT):
            S.dve(lambda e, t=t: e.tensor_tensor(out=Pp[:, t, :], in0=Pp[:, t - 1, :], in1=tot[:, t - 1, :], op=ALU.add), reads=["Pp", "tot"], writes=["Pp"])
        S.dve(lambda e: e.tensor_tensor(out=Gf[:, :, :], in0=Gc[:, :, :], in1=Pp[:, :, :], op=ALU.add), reads=["Gc", "Pp"], writes=["Gf"])
        S.dve(lambda e: e.tensor_tensor(out=Gend[:, :, :], in0=tot[:, :, :], in1=Pp[:, :, :], op=ALU.add), reads=["tot", "Pp"], writes=["Gend"])
        for h in range(8):
            S.dve(lambda e, h=h: e.tensor_tensor(out=biasT[:, h, :, :], in0=Gf[:, :, h].unsqueeze(1).to_broadcast([128, NT, NT]), in1=Gend[:, :, h].unsqueeze(2).to_broadcast([128, NT, NT]), op=ALU.subtract),
                  reads=["Gf", "Gend"], writes=["biasT"])

        fence()
        RX.reset()
        hT = RX.alloc([8, SEQ], BF16)
        gi = 0
        for i in range(int(os.environ.get("KATT_TILES", NT))):
            b2 = i % 2
            S.dma("sp", lambda e, i=i, b2=b2, s=s: e.dma_start(out=xres[b2][:, :], in_=x[s, i * 128:(i + 1) * 128, :]), writes=["xres%d" % b2])
            for h in range(8):
                p = h // 2; r0 = (h % 2) * 64
                po = 2 + h // 4; oc = (h % 4) * 65
                for j0 in range(0, i + 1, 4):
                    js = list(range(j0, min(j0 + 4, i + 1)))
                    ps = gi % 2; pb = gi % 3; gi += 1
                    for jj, j in enumerate(js):
                        S.pe(lambda e, ps=ps, jj=jj, j=j, p=p, r0=r0, i=i: e.matmul(bank(ps)[:, jj * 128:(jj + 1) * 128], lhsT=kT[r0:r0 + 64, p, j * 128:(j + 1) * 128], rhs=qT[r0:r0 + 64, p, i * 128:(i + 1) * 128], start=True, stop=True),
                             reads=["kT.%d.%d" % (p, j // 4), "qT.%d.%d" % (p, i // 4)], writes=[PSK[ps]])
                    for jj, j in enumerate(js):
                        S.act(lambda e, ps=ps, pb=pb, jj=jj, j=j, h=h, i=i: e.activation(out=PT[pb][:, jj, :], in_=bank(ps)[:, jj * 128:(jj + 1) * 128], func=AF.Exp, bias=biasT[:, h, i, j:j + 1], scale=ATT_SCALE),
                              reads=[PSK[ps], "biasT"], writes=["PT%d.%d" % (pb, jj)])
                        if j == i:
                            S.pool(lambda e, pb=pb, jj=jj: e.tensor_tensor(out=PT[pb][:, jj, :], in0=PT[pb][:, jj, :], in1=triU[:, :], op=ALU.mult),
                                   reads=["PT%d.%d" % (pb, jj), "triU"], writes=["PT%d.%d" % (pb, jj)])
                        S.pe(lambda e, pb=pb, jj=jj, j=j, h=h, po=po, oc=oc, i=i: e.matmul(bank(po)[:, oc:oc + 65], lhsT=PT[pb][:, jj, :], rhs=v_aug[:, j, h, :], start=(j == 0), stop=(j == i)),
                             reads=["PT%d.%d" % (pb, jj), "v.%d" % j, "v_ones"], writes=[PSK[po]])
            STG = int(os.environ.get("KATT_STAGE", 9))
            if STG < 2: continue
            for hb in range(2):
                ov = bank(2 + hb)[:, 0:260].rearrange("p (h d) -> p h d", h=4)
                S.dve(lambda e, hb=hb, ov=ov, b2=b2: e.reciprocal(rden[b2][:, 4 * hb:4 * hb + 4], ov[:, :, 64]), reads=[PSK[2 + hb]], writes=["rden%d.%d" % (b2, hb)])
                S.dve(lambda e, hb=hb, ov=ov, b2=b2: e.tensor_tensor(out=yatt[b2][:, 4 * hb:4 * hb + 4, :], in0=ov[:, :, 0:64], in1=rden[b2][:, 4 * hb:4 * hb + 4].unsqueeze(2).to_broadcast([128, 4, 64]), op=ALU.mult),
                      reads=[PSK[2 + hb], "rden%d.%d" % (b2, hb)], writes=["yatt%d.%d" % (b2, hb)])
            if dbg == "att" and s == 0:
                pass
            if STG < 3: continue
            yf = yatt[b2].rearrange("p h d -> p (h d)")
            for ec in range(8):
                src = m_ssd[:, i, ec * 128:(ec + 1) * 128] if ec < 4 else yf[:, (ec - 4) * 128:(ec - 3) * 128]
                rk = ["m_ssd.%d" % i] if ec < 4 else ["yatt%d.%d" % (b2, (ec - 4) // 2)]
                S.pe(lambda e, ec=ec, src=src: e.transpose(bankb(4)[:, ec * 128:(ec + 1) * 128], src, identB[:, :]), reads=rk + ["identB"], writes=[PSK[4]])
            S.act(lambda e, b2=b2: e.copy(mT[b2].rearrange("p a b -> p (a b)"), bankb(4)[:, 0:1024]), reads=[PSK[4]], writes=["mT%d" % b2])
            for half in range(2):
                for ec in range(8):
                    S.pe(lambda e, half=half, ec=ec, b2=b2: e.matmul(pd[3][:, half * 512:(half + 1) * 512], lhsT=mT[b2][:, ec, :], rhs=wo[:, ec, half * 512:(half + 1) * 512], start=(ec == 0), stop=(ec == 7)),
                         reads=["mT%d" % b2, "wo"], writes=[PSK[6 + half]])
            if STG < 4: continue
            hp = hpre[b2]
            S.dve(lambda e, hp=hp, b2=b2: e.scalar_tensor_tensor(out=hp[:, :], in0=xres[b2][:, :], scalar=ALPHA, in1=pd[3][:, :], op0=ALU.mult, op1=ALU.add),
                  reads=["xres%d" % b2, PSK[6], PSK[7]], writes=["hpre%d" % b2])
            for c2 in range(2):
                S.dve(lambda e, hp=hp, c2=c2, b2=b2: e.bn_stats(bst[b2][:, c2, :], hp[:, c2 * 512:(c2 + 1) * 512]), reads=["hpre%d" % b2], writes=["bst%d.%d" % (b2, c2)])
            S.dve(lambda e, b2=b2: e.bn_aggr(mv[b2][:, :], bst[b2][:, :, :]), reads=["bst%d.0" % b2, "bst%d.1" % b2], writes=["mv%d" % b2])
            S.act(lambda e, b2=b2: e.activation(out=rs1[b2][:, :], in_=mv[b2][:, 1:2], func=AF.Ln, bias=LN_EPS, scale=1.0), reads=["mv%d" % b2], writes=["rs1%d" % b2])
            S.act(lambda e, b2=b2: e.activation(out=rs1[b2][:, :], in_=rs1[b2][:, :], func=AF.Exp, scale=-0.5), reads=["rs1%d" % b2], writes=["rs1%d" % b2])
            S.dve(lambda e, hp=hp, b2=b2: e.tensor_scalar(out=hp[:, :], in0=hp[:, :], scalar1=mv[b2][:, 0:1], scalar2=rs1[b2][:, 0:1], op0=ALU.subtract, op1=ALU.mult),
                  reads=["hpre%d" % b2, "mv%d" % b2, "rs1%d" % b2], writes=["hpre%d" % b2])
            S.pool(lambda e, hp=hp: e.tensor_tensor(out=hp[:, :], in0=hp[:, :], in1=lnG[:, :], op=ALU.mult), reads=["hpre%d" % b2, "lnG"], writes=["hpre%d" % b2])
            S.pool(lambda e, hp=hp: e.tensor_tensor(out=hp[:, :], in0=hp[:, :], in1=lnB[:, :], op=ALU.add), reads=["hpre%d" % b2, "lnB"], writes=["hpre%d" % b2])
            if STG < 5: continue
            S.act(lambda e, hp=hp: e.mul(ah[:, :], hp[:, :], ALPHA), reads=["hpre%d" % b2], writes=["ah"])
            S.dma("sp", lambda e, i=i, s=s: e.dma_start(out=h_scr[s, i * 128:(i + 1) * 128, :], in_=ah[:, :]), reads=["ah"], writes=["h_scr.%d" % i])
            if STG < 6: continue
            for half in range(2):
                for q4 in range(4):
                    ec = half * 4 + q4
                    S.pe(lambda e, q4=q4, ec=ec, hp=hp: e.transpose(bank(5)[:, q4 * 128:(q4 + 1) * 128], hp[:, ec * 128:(ec + 1) * 128], identF[:, :]), reads=["hpre%d" % b2, "identF"], writes=[PSK[5]])
                KSUB = os.environ.get("KSUB", "ab")
                if "a" in KSUB:
                    S.act(lambda e, half=half, i=i: e.copy(hT[:, 4 * half:4 * half + 4, i * 128:(i + 1) * 128], bank(5)[:, :].rearrange("p (a b) -> p a b", a=4)), reads=[PSK[5]], writes=["hT.%d.%d" % (i, half)])
                if "b" in KSUB:
                    S.dve(lambda e, half=half: e.tensor_copy(hTf[:, 4 * half:4 * half + 4, :], bank(5)[:, :].rearrange("p (a b) -> p a b", a=4)), reads=[PSK[5]], writes=["hTf.%d" % half])
            if STG < 7: continue
            for ec in range(8):
                S.pe(lambda e, ec=ec: e.matmul(bank(4)[:, 0:20], lhsT=hTf[:, ec, :], rhs=rw[:, ec, :], start=(ec == 0), stop=(ec == 7)), reads=["hTf.%d" % (ec // 4)] + RWK, writes=[PSK[4]])
            S.dve(lambda e, i=i: e.tensor_tensor(out=logits[:, i, :], in0=bank(4)[:, 0:20], in1=rb_bc[:, :], op=ALU.add), reads=[PSK[4], "rb0", "rb1"], writes=["logits.%d" % i])

        if dbg == "att" and s == 0:
            fence()
            RACC.reset()
            cvt = RACC.alloc([NT * 1024], BF16)
            d1 = dbg_out("dbg_hT", [128, 8 * SEQ]); d2 = dbg_out("dbg_logits", [128, NT * 20])
            cv2 = RACC.alloc([2 * SEQ], F32)
            for q in range(4):
                S.dve(lambda e, q=q: e.tensor_copy(cv2[:, :], hT[:, 2 * q:2 * q + 2, :].rearrange("p a b -> p (a b)")), reads=[], writes=["cv2"])
                outs.append(S.dma("sp", lambda e, q=q: e.dma_start(out=d1[:, 2 * q * SEQ:(2 * q + 2) * SEQ], in_=cv2[:, :]), reads=["cv2"]))
            outs.append(S.dma("sp", lambda e: e.dma_start(out=d2[:, :], in_=logits.rearrange("p t j -> p (t j)")), reads=["logits.%d" % i for i in range(int(os.environ.get("KATT_TILES", NT)))] if int(os.environ.get("KATT_STAGE", 9)) >= 7 else []))
            break


        fence()
        TT.reset(); RW.reset(); RACC.reset()
        S.dma("sp", lambda e: e.dma_start(out=lnG[:, :], in_=ln2_g[0:1, :].to_broadcast([128, 1024])), writes=["lnG"])
        S.dma("sp", lambda e: e.dma_start(out=lnB[:, :], in_=ln2_b[0:1, :].to_broadcast([128, 1024])), writes=["lnB"])
        LOGK = ["logits.%d" % i for i in range(NT)]
        lg = logits[:, :, 0:4]
        le4 = logits[:, :, 4:20].rearrange("p t (g j) -> p t g j", g=4)
        gmax = TT.alloc([NT], F32); goh = TT.alloc([NT, 4], F32); gex = TT.alloc([NT, 4], F32)
        gsum = TT.alloc([NT], F32); gval = TT.alloc([NT], F32)
        tmp16 = TT.alloc([NT, 4, 4], F32); esel = TT.alloc([NT, 4], F32)
        m1 = TT.alloc([NT], F32); oh1 = TT.alloc([NT, 4], F32); e2 = TT.alloc([NT, 4], F32)
        m2 = TT.alloc([NT], F32); oh2 = TT.alloc([NT, 4], F32); dd = TT.alloc([NT], F32)
        w1 = TT.alloc([NT], F32); w2 = TT.alloc([NT], F32); cw1 = TT.alloc([NT], F32); cw2 = TT.alloc([NT], F32)
        cj = TT.alloc([NT, 4], F32); cj2 = TT.alloc([NT, 4], F32)
        bc4 = lambda a: a.unsqueeze(2).to_broadcast([128, NT, 4])
        S.dve(lambda e: e.tensor_reduce(out=gmax[:, :], in_=lg, axis=AX.X, op=ALU.max), reads=LOGK, writes=["gmax"])
        S.dve(lambda e: e.tensor_tensor(out=goh[:, :, :], in0=lg, in1=bc4(gmax[:, :]), op=ALU.is_equal), reads=LOGK + ["gmax"], writes=["goh"])
        S.dve(lambda e: e.tensor_tensor(out=gex[:, :, :], in0=lg, in1=bc4(gmax[:, :]), op=ALU.subtract), reads=LOGK + ["gmax"], writes=["gex"])
        S.act(lambda e: e.activation(out=gex[:, :, :], in_=gex[:, :, :], func=AF.Exp), reads=["gex"], writes=["gex"])
        S.dve(lambda e: e.tensor_reduce(out=gsum[:, :], in_=gex[:, :, :], axis=AX.X, op=ALU.add), reads=["gex"], writes=["gsum"])
        S.dve(lambda e: e.reciprocal(gval[:, :], gsum[:, :]), reads=["gsum"], writes=["gval"])
        S.dve(lambda e: e.tensor_tensor(out=tmp16[:, :, :, :], in0=le4, in1=goh[:, :, :].unsqueeze(3).to_broadcast([128, NT, 4, 4]), op=ALU.mult), reads=LOGK + ["goh"], writes=["tmp16"])
        S.dve(lambda e: e.tensor_reduce(out=esel[:, :, :], in_=tmp16.rearrange("p t g j -> p t j g"), axis=AX.X, op=ALU.add), reads=["tmp16"], writes=["esel"])
        S.dve(lambda e: e.tensor_reduce(out=m1[:, :], in_=esel[:, :, :], axis=AX.X, op=ALU.max), reads=["esel"], writes=["m1"])
        S.dve(lambda e: e.tensor_tensor(out=oh1[:, :, :], in0=esel[:, :, :], in1=bc4(m1[:, :]), op=ALU.is_equal), reads=["esel", "m1"], writes=["oh1"])
        S.dve(lambda e: e.scalar_tensor_tensor(out=e2[:, :, :], in0=oh1[:, :, :], scalar=-1e30, in1=esel[:, :, :], op0=ALU.mult, op1=ALU.add), reads=["oh1", "esel"], writes=["e2"])
        S.dve(lambda e: e.tensor_reduce(out=m2[:, :], in_=e2[:, :, :], axis=AX.X, op=ALU.max), reads=["e2"], writes=["m2"])
        S.dve(lambda e: e.tensor_tensor(out=oh2[:, :, :], in0=e2[:, :, :], in1=bc4(m2[:, :]), op=ALU.is_equal), reads=["e2", "m2"], writes=["oh2"])
        S.dve(lambda e: e.tensor_tensor(out=dd[:, :], in0=m2[:, :], in1=m1[:, :], op=ALU.subtract), reads=["m1", "m2"], writes=["dd"])
        S.act(lambda e: e.activation(out=dd[:, :], in_=dd[:, :], func=AF.Exp), reads=["dd"], writes=["dd"])
        S.dve(lambda e: e.tensor_scalar(out=w1[:, :], in0=dd[:, :], scalar1=1.0, scalar2=None, op0=ALU.add), reads=["dd"], writes=["w1"])
        S.dve(lambda e: e.reciprocal(w1[:, :], w1[:, :]), reads=["w1"], writes=["w1"])
        S.dve(lambda e: e.tensor_tensor(out=w2[:, :], in0=dd[:, :], in1=w1[:, :], op=ALU.mult), reads=["dd", "w1"], writes=["w2"])
        S.dve(lambda e: e.tensor_tensor(out=cw1[:, :], in0=gval[:, :], in1=w1[:, :], op=ALU.mult), reads=["gval", "w1"], writes=["cw1"])
        S.dve(lambda e: e.tensor_tensor(out=cw2[:, :], in0=gval[:, :], in1=w2[:, :], op=ALU.mult), reads=["gval", "w2"], writes=["cw2"])
        S.dve(lambda e: e.tensor_tensor(out=cj[:, :, :], in0=oh1[:, :, :], in1=bc4(cw1[:, :]), op=ALU.mult), reads=["oh1", "cw1"], writes=["cj"])
        S.dve(lambda e: e.tensor_tensor(out=cj2[:, :, :], in0=oh2[:, :, :], in1=bc4(cw2[:, :]), op=ALU.mult), reads=["oh2", "cw2"], writes=["cj2"])
        S.dve(lambda e: e.tensor_tensor(out=cj[:, :, :], in0=cj[:, :, :], in1=cj2[:, :, :], op=ALU.add), reads=["cj", "cj2"], writes=["cj"])
        S.dve(lambda e: e.tensor_tensor(out=comb.rearrange("p t (g j) -> p t g j", g=4), in0=goh[:, :, :].unsqueeze(3).to_broadcast([128, NT, 4, 4]), in1=cj[:, :, :].unsqueeze(2).to_broadcast([128, NT, 4, 4]), op=ALU.mult),
              reads=["goh", "cj"], writes=["comb"])

        acc = RACC.alloc([NT, 1024], F32)
        wgu = [RW.alloc([8, 1024], BF16) for _ in range(2)]
        wdn = [RW.alloc([4, 1024], BF16) for _ in range(2)]
        sg = [TT.alloc([512], BF16) for _ in range(2)]
        actT = [TT.alloc([4, 512], BF16) for _ in range(2)]
        obuf = [TT.alloc([1024], F32) for _ in range(2)]
        bst2 = [TT.alloc([2, 6], F32) for _ in range(2)]
        mv2 = [TT.alloc([2], F32) for _ in range(2)]
        rs2 = [TT.alloc([1], F32) for _ in range(2)]
        for q in range(4):
            S.dma("sp", lambda e, q=q, s=s: e.dma_start(out=acc[:, 4 * q:4 * q + 4, :], in_=h_scr[s, q * 512:(q + 1) * 512, :].rearrange("(t p) d -> p t d", p=128)),
                  reads=["h_scr.%d" % t for t in range(4 * q, 4 * q + 4)], writes=["acc.%d" % t for t in range(4 * q, 4 * q + 4)])
        NEXP = int(os.environ.get("KNEXP", 16))

        def load_expert(ex):
            sl = ex % 2
            S.dma("pool", lambda e, ex=ex, sl=sl: e.dma_start(out=wgu[sl][:, :, 0:512], in_=w_gate[ex].rearrange("(k p) f -> p k f", p=128)), writes=["wg.%d" % sl])
            S.dma("pool", lambda e, ex=ex, sl=sl: e.dma_start(out=wgu[sl][:, :, 512:1024], in_=w_up[ex].rearrange("(k p) f -> p k f", p=128)), writes=["wu.%d" % sl])
            S.dma("pool", lambda e, ex=ex, sl=sl: e.dma_start(out=wdn[sl][:, :, :], in_=w_down[ex].rearrange("(k p) d -> p k d", p=128)), writes=["wd.%d" % sl])

        load_expert(0)
        if NEXP > 1:
            load_expert(1)
        cg = 0; cd = 0
        for ex in range(NEXP):
            sl = ex % 2
            for tb in range(4):
                ab = (ex * 4 + tb) % 2
                hk = ["hT.%d.%d" % (i, hf) for i in range(4 * tb, 4 * tb + 4) for hf in range(2)]
                for fc in range(4):
                    pg = cg % 2; cg += 1
                    for k in range(8):
                        S.pe(lambda e, pg=pg, k=k, fc=fc, tb=tb, sl=sl: e.matmul(bank(pg)[:, :], lhsT=wgu[sl][:, k, fc * 128:(fc + 1) * 128], rhs=hT[:, k, tb * 512:(tb + 1) * 512], start=(k == 0), stop=(k == 7)),
                             reads=["wg.%d" % sl] + hk, writes=[PSK[pg]])
                    for k in range(8):
                        S.pe(lambda e, pg=pg, k=k, fc=fc, tb=tb, sl=sl: e.matmul(bank(2 + pg)[:, :], lhsT=wgu[sl][:, k, 512 + fc * 128:512 + (fc + 1) * 128], rhs=hT[:, k, tb * 512:(tb + 1) * 512], start=(k == 0), stop=(k == 7)),
                             reads=["wu.%d" % sl] + hk, writes=[PSK[2 + pg]])
                    S.act(lambda e, pg=pg: e.activation(out=sg[pg][:, :], in_=bank(pg)[:, :], func=AF.Silu), reads=[PSK[pg]], writes=["sg%d" % pg])
                    S.dve(lambda e, pg=pg, ab=ab, fc=fc: e.tensor_tensor(out=actT[ab][:, fc, :], in0=bank(2 + pg)[:, :], in1=sg[pg][:, :], op=ALU.mult),
                          reads=[PSK[2 + pg], "sg%d" % pg], writes=["actT%d.%d" % (ab, fc)])
                for tt in range(4):
                    t = tb * 4 + tt
                    pdi = 2 + (cd % 2); cd += 1
                    for half in range(2):
                        for fc in range(4):
                            S.pe(lambda e, pdi=pdi, half=half, fc=fc, ab=ab, tt=tt, sl=sl: e.matmul(pd[pdi][:, half * 512:(half + 1) * 512], lhsT=actT[ab][:, fc, tt * 128:(tt + 1) * 128], rhs=wdn[sl][:, fc, half * 512:(half + 1) * 512], start=(fc == 0), stop=(fc == 3)),
                                 reads=["actT%d.%d" % (ab, fc), "wd.%d" % sl], writes=[PSK[2 * pdi + half]])
                    S.dve(lambda e, pdi=pdi, t=t, ex=ex: e.scalar_tensor_tensor(out=acc[:, t, :], in0=pd[pdi][:, :], scalar=comb[:, t, ex:ex + 1], in1=acc[:, t, :], op0=ALU.mult, op1=ALU.add),
                          reads=[PSK[2 * pdi], PSK[2 * pdi + 1], "comb", "acc.%d" % t], writes=["acc.%d" % t])
            if ex + 2 < NEXP:
                load_expert(ex + 2)
        for t in range(NT):
            b2 = t % 2
            for c2 in range(2):
                S.dve(lambda e, c2=c2, t=t, b2=b2: e.bn_stats(bst2[b2][:, c2, :], acc[:, t, c2 * 512:(c2 + 1) * 512]), reads=["acc.%d" % t], writes=["bst2%d.%d" % (b2, c2)])
            S.dve(lambda e, b2=b2: e.bn_aggr(mv2[b2][:, :], bst2[b2][:, :, :]), reads=["bst2%d.0" % b2, "bst2%d.1" % b2], writes=["mv2%d" % b2])
            S.act(lambda e, b2=b2: e.activation(out=rs2[b2][:, :], in_=mv2[b2][:, 1:2], func=AF.Ln, bias=LN_EPS, scale=1.0), reads=["mv2%d" % b2], writes=["rs2%d" % b2])
            S.act(lambda e, b2=b2: e.activation(out=rs2[b2][:, :], in_=rs2[b2][:, :], func=AF.Exp, scale=-0.5), reads=["rs2%d" % b2], writes=["rs2%d" % b2])
            S.dve(lambda e, t=t, b2=b2: e.tensor_scalar(out=obuf[b2][:, :], in0=acc[:, t, :], scalar1=mv2[b2][:, 0:1], scalar2=rs2[b2][:, 0:1], op0=ALU.subtract, op1=ALU.mult),
                  reads=["acc.%d" % t, "mv2%d" % b2, "rs2%d" % b2], writes=["obuf%d" % b2])
            S.pool(lambda e, b2=b2: e.tensor_tensor(out=obuf[b2][:, :], in0=obuf[b2][:, :], in1=lnG[:, :], op=ALU.mult), reads=["obuf%d" % b2, "lnG"], writes=["obuf%d" % b2])
            S.pool(lambda e, b2=b2: e.tensor_tensor(out=obuf[b2][:, :], in0=obuf[b2][:, :], in1=lnB[:, :], op=ALU.add), reads=["obuf%d" % b2, "lnB"], writes=["obuf%d" % b2])
            outs.append(S.dma("sp", lambda e, t=t, b2=b2, s=s: e.dma_start(out=out[s, t * 128:(t + 1) * 128, :], in_=obuf[b2][:, :]), reads=["obuf%d" % b2], writes=["out.%d.%d" % (s, t)]))
        if s + 1 < NSEQ:
            fence()
        if dbg == "one":
            break

    with nc.allow_non_contiguous_dma(reason="tiny constant loads"):
        st = S.emit(outs)
    return nc, st, dbg_t


_CACHE = {}


def _get_program():
    if "p" not in _CACHE:
        _CACHE["p"] = build_program(dbg=os.environ.get("KDBG", ""))
    return _CACHE["p"]


def kernel(**inputs):
    nc, st, dbg_t = _get_program()
    f = lambda a: np.ascontiguousarray(np.asarray(a, dtype=np.float32))
    x = f(inputs["x"])
    shared = {
        "w_in": f(inputs["w_in"])[0], "b_in": f(inputs["b_in"]).reshape(1, DIN),
        "conv_w": f(inputs["conv_w"])[0], "conv_b": f(inputs["conv_b"]).reshape(1, 1024),
        "a_log": f(inputs["a_log"]).reshape(1, 8), "d_skip": f(inputs["d_skip"]).reshape(1, 8),
        "ssd_norm_g": f(inputs["ssd_norm_g"]).reshape(1, 512), "w_out": f(inputs["w_out"])[0],
        "ln1_g": f(inputs["ln1_g"]).reshape(1, DM), "ln1_b": f(inputs["ln1_b"]).reshape(1, DM),
        "router_group_w": f(inputs["router_group_w"])[0], "router_group_b": f(inputs["router_group_b"]).reshape(1, 4),
        "router_expert_w": f(inputs["router_expert_w"])[0], "router_expert_b": f(inputs["router_expert_b"]).reshape(1, 16),
        "w_gate": f(inputs["w_gate"])[0], "w_up": f(inputs["w_up"])[0], "w_down": f(inputs["w_down"])[0],
        "ln2_g": f(inputs["ln2_g"]).reshape(1, DM), "ln2_b": f(inputs["ln2_b"]).reshape(1, DM),
    }
    ncores = int(os.environ.get("KCORES", NCORES))
    in_maps = []
    for c in range(ncores):
        m = dict(shared)
        m["x"] = np.ascontiguousarray(x[c * NSEQ:(c + 1) * NSEQ])
        in_maps.append(m)
    res = run_bass_kernel_spmd(nc, in_maps, core_ids=list(range(ncores)))
    if os.environ.get("KDBG", ""):
        _CACHE["dbg"] = res.results
    outp = np.concatenate([r["out"] for r in res.results], axis=0)
    return outp.astype(np.float32)
```

```python
import os
import numpy as np
import concourse.bass as bass
import concourse.mybir as mybir
from concourse.bass_utils import run_bass_kernel_spmd

F32 = mybir.dt.float32
BF16 = mybir.dt.bfloat16
U8 = mybir.dt.uint8
AF = mybir.ActivationFunctionType
ALU = mybir.AluOpType
AX = mybir.AxisListType

NCORES = 8
NSEQ = 2
SEQ = 2048
NT = 16
DM = 1024
DIN = 3088
ALPHA = float(2.0 ** 0.25)
LN_EPS = 1e-5
RMS_EPS = 1e-5
ATT_SCALE = 0.125
NEG = -30000.0


class Op:
    __slots__ = ("eng", "fn", "reads", "writes", "deps", "signal", "sigval", "dma", "gi", "nofence")


class Sched:
    COMPUTE = ("pe", "act", "dve", "pool")

    def __init__(self, nc, n_dma_sems=40):
        self.nc = nc
        self.h = {"pe": nc.tensor, "act": nc.scalar, "dve": nc.vector, "pool": nc.gpsimd, "sp": nc.sync}
        self.ops = []
        self.last_w = {}
        self.readers = {}
        self.n_dma_sems = n_dma_sems
        self.live_dma = []
        self.nfence = 0

    def add(self, eng, fn, reads=(), writes=(), dma=False, nofence=False):
        o = Op()
        o.eng = eng; o.fn = fn; o.reads = tuple(reads); o.writes = tuple(writes)
        o.deps = []; o.signal = False; o.sigval = None; o.dma = dma; o.gi = len(self.ops); o.nofence = nofence
        for r in o.reads:
            p = self.last_w.get(r)
            if p is not None:
                self._dep(o, p, True)
            if r.startswith("ps"):
                rd = self.readers.get(r)
                if rd:
                    for q in rd.values():
                        if q.eng != eng:
                            self._dep(o, q, True)
        for w in o.writes:
            p = self.last_w.get(w)
            if p is not None:
                self._dep(o, p, False)
            rd = self.readers.get(w)
            if rd:
                for q in rd.values():
                    self._dep(o, q, False)
        for r in o.reads:
            d = self.readers.setdefault(r, {})
            d[("dma", o.gi) if dma else eng] = o
        for w in o.writes:
            self.last_w[w] = o
            self.readers[w] = {}
        self.ops.append(o)
        if dma and not nofence:
            self.live_dma.append(o)
        return o

    def _dep(self, o, p, raw):
        if p is o:
            return
        if (not p.dma) and (not o.dma) and p.eng == o.eng:
            if o.eng == "pe":
                return
        o.deps.append(p)
        p.signal = True

    def pe(self, fn, reads=(), writes=()): return self.add("pe", fn, reads, writes)
    def act(self, fn, reads=(), writes=()): return self.add("act", fn, reads, writes)
    def dve(self, fn, reads=(), writes=()): return self.add("dve", fn, reads, writes)
    def pool(self, fn, reads=(), writes=()): return self.add("pool", fn, reads, writes)
    def dma(self, q, fn, reads=(), writes=(), nofence=False):
        return self.add(q, fn, reads, writes, dma=True, nofence=nofence)

    def fence(self, scratch):
        n = self.nfence; self.nfence += 1
        a_keys = []
        col = {"pe": None, "act": 0, "dve": 1, "pool": 2}
        for e in ("act", "dve", "pool"):
            k = "fenceA.%d.%s" % (n, e)
            c = col[e]
            if e == "act":
                self.add(e, (lambda eh, c=c: eh.activation(out=scratch[:, c:c + 1], in_=scratch[:, 8:9], func=AF.Copy)), reads=(), writes=(k,))
            else:
                self.add(e, (lambda eh, c=c: eh.memset(scratch[:, c:c + 1], 0.0)), reads=(), writes=(k,))
            a_keys.append(k)
        k = "fenceA.%d.pe" % n
        self.add("pe", (lambda eh: eh.matmul(self.fence_ps[0:1, 0:1], lhsT=self.fence_w[0:1, 0:1], rhs=self.fence_w[0:1, 0:1], start=True, stop=True)),
                 reads=(), writes=(k, "ps7"))
        a_keys.append(k)
        dmas = self.live_dma
        self.live_dma = []
        for e in ("act", "dve", "pool", "pe", "sp"):
            kb = "fenceB.%d.%s" % (n, e)
            if e == "act":
                o = self.add(e, (lambda eh: eh.activation(out=scratch[:, 3:4], in_=scratch[:, 8:9], func=AF.Copy)), reads=a_keys, writes=(kb,))
            elif e == "pe":
                o = self.add(e, (lambda eh: eh.matmul(self.fence_ps[0:1, 1:2], lhsT=self.fence_w[0:1, 0:1], rhs=self.fence_w[0:1, 0:1], start=True, stop=True)),
                             reads=a_keys, writes=(kb, "ps7"))
            elif e == "sp":
                o = self.add(e, (lambda eh: eh.nop()), reads=a_keys, writes=(kb,))
            else:
                c = 4 if e == "dve" else 5
                o = self.add(e, (lambda eh, c=c: eh.memset(scratch[:, c:c + 1], 0.0)), reads=a_keys, writes=(kb,))
            for d in dmas:
                o.deps.append(d)

    def emit(self, final_wait_ops=()):
        nc = self.nc
        esem = {e: nc.alloc_semaphore("s_" + e) for e in self.COMPUTE}
        dsems = [nc.alloc_semaphore("s_dma%d" % i) for i in range(self.n_dma_sems)]
        dtotal = [0] * self.n_dma_sems
        dlast = [None] * self.n_dma_sems
        ecount = {e: 0 for e in self.COMPUTE}
        nd = 0
        nq = {"sp": 0, "pool": 0}
        half = self.n_dma_sems // 2
        for o in self.ops:
            if o.dma:
                qi = nq[o.eng]; nq[o.eng] += 1; nd += 1
                i = (qi % half) + (0 if o.eng == "sp" else half)
                prev = dlast[i]
                if prev is not None:
                    o.deps.append(prev)
                dtotal[i] += 16
                o.sigval = (dsems[i], dtotal[i], 1000 + i)
                dlast[i] = o
            elif o.signal:
                ecount[o.eng] += 1
                o.sigval = (esem[o.eng], ecount[o.eng], o.eng)
        known = {e: {} for e in self.h}
        nwaits = 0
        for o in self.ops:
            eh = self.h[o.eng]
            kn = known[o.eng]
            need = {}
            for p in o.deps:
                s, v, key = p.sigval
                if kn.get(key, 0) >= v:
                    continue
                if key not in need or need[key][1] < v:
                    need[key] = (s, v)
            for key, (s, v) in need.items():
                eh.wait_ge(s, v)
                kn[key] = v
                nwaits += 1
            ins = o.fn(eh)
            if o.dma:
                ins.then_inc(o.sigval[0], 16)
            elif o.signal:
                ins.then_inc(o.sigval[0], 1)
        eh = self.h["sp"]
        for o in final_wait_ops:
            s, v, key = o.sigval
            eh.wait_ge(s, v)
        self.stats = dict(n_ops=len(self.ops), n_waits=nwaits, counts=dict(ecount), n_dma=nd)
        return self.stats


class Arena:
    def __init__(self, nc, name, nbytes):
        self.t = nc.alloc_sbuf_tensor(name, [128, nbytes], U8)
        self.n = nbytes
        self.off = 0

    def reset(self, off=0):
        self.off = off

    def alloc(self, shape, dtype, parts=128):
        esz = 2 if dtype == BF16 else 4
        n = esz
        for s in shape:
            n *= s
        off = (self.off + 31) // 32 * 32
        assert off + n <= self.n, (off, n, self.n)
        self.off = off + n
        flat = self.t[0:parts, off:off + n].bitcast(dtype)
        if len(shape) == 1:
            return flat
        names = " ".join("a%d" % i for i in range(len(shape)))
        kw = {"a%d" % i: shape[i] for i in range(1, len(shape))}
        return flat.rearrange("p (%s) -> p %s" % (names, names), **kw)


def build_program(dbg=False):
    nc = bass.Bass("TRN2", target_bir_lowering=False)
    S = Sched(nc)
    D = {}

    def din(name, shape):
        D[name] = nc.dram_tensor(name, list(shape), F32, kind="ExternalInput").ap()
        return D[name]

    x = din("x", [NSEQ, SEQ, DM])
    w_in = din("w_in", [DM, DIN])
    b_in = din("b_in", [1, DIN])
    conv_w = din("conv_w", [4, 1024])
    conv_b = din("conv_b", [1, 1024])
    a_log = din("a_log", [1, 8])
    d_skip = din("d_skip", [1, 8])
    ssd_g = din("ssd_norm_g", [1, 512])
    w_out = din("w_out", [DM, DM])
    ln1_g = din("ln1_g", [1, DM]); ln1_b = din("ln1_b", [1, DM])
    rg_w = din("router_group_w", [DM, 4]); rg_b = din("router_group_b", [1, 4])
    re_w = din("router_expert_w", [4, DM, 4]); re_b = din("router_expert_b", [1, 16])
    w_gate = din("w_gate", [16, DM, 512]); w_up = din("w_up", [16, DM, 512]); w_down = din("w_down", [16, 512, DM])
    ln2_g = din("ln2_g", [1, DM]); ln2_b = din("ln2_b", [1, DM])
    out = nc.dram_tensor("out", [NSEQ, SEQ, DM], F32, kind="ExternalOutput").ap()
    h_scr = nc.dram_tensor("h_scr", [NSEQ, SEQ, DM], F32).ap()
    dbg_t = {}

    def dbg_out(name, shape):
        dbg_t[name] = nc.dram_tensor(name, list(shape), F32, kind="ExternalOutput").ap()
        return dbg_t[name]

    CONST = Arena(nc, "CONST", 22 * 1024)
    RW = Arena(nc, "RW", 49408)
    RX = Arena(nc, "RX", 32768)
    RACC = Arena(nc, "RACC", 65536)
    MSSD = Arena(nc, "MSSD", 16384)
    TT = Arena(nc, "TT", nc.sbuf_bytes_remaining - 256)

    identB = CONST.alloc([128], BF16); identF = CONST.alloc([128], F32)
    Uf = CONST.alloc([128], F32); triU = CONST.alloc([128], BF16); maskneg = CONST.alloc([128], BF16)
    onesF = CONST.alloc([128], F32)
    ones_row = CONST.alloc([128], BF16, parts=1)
    bz_row = CONST.alloc([512], BF16, parts=1); bv_row = CONST.alloc([512], BF16, parts=1)
    bdtf = CONST.alloc([16], F32)
    bxbc = CONST.alloc([8], F32); bq = CONST.alloc([4], F32); bk = CONST.alloc([4], F32)
    convw = CONST.alloc([8, 4], F32); convb = CONST.alloc([8], F32)
    a_bc = CONST.alloc([8], F32); dskip_bc = CONST.alloc([8], F32)
    gssd_bc = CONST.alloc([512], F32)
    lnG = CONST.alloc([1024], F32); lnB = CONST.alloc([1024], F32)
    rw = CONST.alloc([8, 20], F32); rb_bc = CONST.alloc([20], F32)
    logits = CONST.alloc([NT, 20], F32); comb = CONST.alloc([NT, 16], F32)
    fsc = CONST.alloc([16], F32)
    S.fence_w = CONST.alloc([8], BF16)
    pd = [nc.alloc_psum_tensor("pd%d" % i, [128, 1024], F32) for i in range(4)]
    def bank(i):
        return pd[i // 2][:, (i % 2) * 512:(i % 2) * 512 + 512]
    def bankb(i):
        return bank(i).bitcast(BF16)
    PSK = ["ps%d" % i for i in range(8)]
    S.fence_ps = nc.alloc_sbuf_tensor("fence_dummy", [1, 8], F32)
    S.fence_ps = bank(7)[:, 504:512]

    def fence():
        S.fence(fsc)

    S.pool(lambda e: e.memset(fsc[:, :], 0.0), writes=["fsc"])
    S.pool(lambda e: e.memset(S.fence_w[:, :], 0.0), writes=["fence_w"])
    S.pool(lambda e: e.memset(identB[:, :], 1.0), writes=["identB"])
    S.pool(lambda e: e.affine_select(out=identB[:, :], in_=identB[:, :], pattern=[[-1, 128]], compare_op=ALU.is_equal, fill=0.0, base=0, channel_multiplier=1), reads=["identB"], writes=["identB"])
    S.pool(lambda e: e.memset(identF[:, :], 1.0), writes=["identF"])
    S.pool(lambda e: e.affine_select(out=identF[:, :], in_=identF[:, :], pattern=[[-1, 128]], compare_op=ALU.is_equal, fill=0.0, base=0, channel_multiplier=1), reads=["identF"], writes=["identF"])
    S.pool(lambda e: e.memset(Uf[:, :], 1.0), writes=["Uf"])
    S.pool(lambda e: e.affine_select(out=Uf[:, :], in_=Uf[:, :], pattern=[[1, 128]], compare_op=ALU.is_ge, fill=0.0, base=0, channel_multiplier=-1), reads=["Uf"], writes=["Uf"])
    S.pool(lambda e: e.memset(triU[:, :], 1.0), writes=["triU"])
    S.pool(lambda e: e.affine_select(out=triU[:, :], in_=triU[:, :], pattern=[[1, 128]], compare_op=ALU.is_ge, fill=0.0, base=0, channel_multiplier=-1), reads=["triU"], writes=["triU"])
    S.pool(lambda e: e.memset(maskneg[:, :], NEG), writes=["maskneg"])
    S.pool(lambda e: e.affine_select(out=maskneg[:, :], in_=maskneg[:, :], pattern=[[-1, 128]], compare_op=ALU.is_gt, fill=0.0, base=0, channel_multiplier=1), reads=["maskneg"], writes=["maskneg"])
    S.pool(lambda e: e.memset(onesF[:, :], 1.0), writes=["onesF"])
    S.pool(lambda e: e.memset(ones_row[:, :], 1.0), writes=["ones_row"])
    S.dma("pool", lambda e: e.dma_start(out=bz_row[:, :], in_=b_in[0:1, 0:512]), writes=["bz_row"])
    S.dma("pool", lambda e: e.dma_start(out=bv_row[:, :], in_=b_in[0:1, 2568:3080]), writes=["bv_row"])
    S.dma("sp", lambda e: e.dma_start(out=bdtf[:, 0:8], in_=b_in[0:1, 1536:1544].to_broadcast([128, 8])), writes=["bdtf0"])
    S.dma("sp", lambda e: e.dma_start(out=bdtf[:, 8:16], in_=b_in[0:1, 3080:3088].to_broadcast([128, 8])), writes=["bdtf1"])
    S.dma("sp", lambda e: e.dma_start(out=bxbc[:, :], in_=b_in[0, 512:1536].rearrange("(c p) -> p c", p=128)), writes=["bxbc"])
    S.dma("sp", lambda e: e.dma_start(out=bq[:, :], in_=b_in[0, 1544:2056].rearrange("(c p) -> p c", p=128)), writes=["bq"])
    S.dma("sp", lambda e: e.dma_start(out=bk[:, :], in_=b_in[0, 2056:2568].rearrange("(c p) -> p c", p=128)), writes=["bk"])
    for k in range(4):
        S.dma("sp", lambda e, k=k: e.dma_start(out=convw[:, :, k], in_=conv_w[k, :].rearrange("(c p) -> p c", p=128)), writes=["convw%d" % k])
    CONVW = ["convw%d" % k for k in range(4)]
    S.dma("sp", lambda e: e.dma_start(out=convb[:, :], in_=conv_b[0, :].rearrange("(c p) -> p c", p=128)), writes=["convb"])
    S.dma("sp", lambda e: e.dma_start(out=a_bc[:, :], in_=a_log[0:1, :].to_broadcast([128, 8])), writes=["a_bc"])
    S.act(lambda e: e.activation(out=a_bc[:, :], in_=a_bc[:, :], func=AF.Exp), reads=["a_bc"], writes=["a_bc"])
    S.dve(lambda e: e.tensor_scalar(out=a_bc[:, :], in0=a_bc[:, :], scalar1=-1.0, scalar2=None, op0=ALU.mult), reads=["a_bc"], writes=["a_bc"])
    S.dma("sp", lambda e: e.dma_start(out=dskip_bc[:, :], in_=d_skip[0:1, :].to_broadcast([128, 8])), writes=["dskip_bc"])
    S.dma("sp", lambda e: e.dma_start(out=gssd_bc[:, :], in_=ssd_g[0:1, :].to_broadcast([128, 512])), writes=["gssd_bc"])
    S.dma("sp", lambda e: e.dma_start(out=rw[:, :, 0:4], in_=rg_w.rearrange("(k p) j -> p k j", p=128)), writes=["rw0"])
    for g in range(4):
        S.dma("sp", lambda e, g=g: e.dma_start(out=rw[:, :, 4 + 4 * g:8 + 4 * g], in_=re_w[g].rearrange("(k p) j -> p k j", p=128)), writes=["rw%d" % (g + 1)])
    RWK = ["rw%d" % i for i in range(5)]
    S.dma("sp", lambda e: e.dma_start(out=rb_bc[:, 0:4], in_=rg_b[0:1, :].to_broadcast([128, 4])), writes=["rb0"])
    S.dma("sp", lambda e: e.dma_start(out=rb_bc[:, 4:20], in_=re_b[0:1, :].to_broadcast([128, 16])), writes=["rb1"])

    outs = []

    for s in range(NSEQ):
        RW.reset(); RX.reset(); RACC.reset(); MSSD.reset(); TT.reset()
        wA = RW.alloc([8, 1544], BF16)
        xb = [RW.alloc([4, 1024], BF16) for _ in range(2)]
        xT = RX.alloc([8, SEQ], BF16)
        sz = RACC.alloc([NT, 512], BF16)
        xsB = RACC.alloc([NT, 768], BF16)
        BT = RACC.alloc([2, SEQ], BF16)
        CT = RACC.alloc([2, SEQ], BF16)
        xsT = MSSD.alloc([4, SEQ], BF16)
        dt_t = TT.alloc([NT, 8], F32)
        tt_mark = TT.off
        pre = [TT.alloc([SEQ + 3], BF16) for _ in range(2)]
        cacc = [TT.alloc([SEQ], F32) for _ in range(2)]
        dt_raw = TT.alloc([NT, 8], F32)

        for half in range(2):
            S.dma("pool", lambda e, half=half: e.dma_start(out=wA[:, 4 * half:4 * half + 4, :], in_=w_in.rearrange("(k p) c -> p k c", p=128)[:, 4 * half:4 * half + 4, 0:1544]),
                  writes=["wA"])
        for b in range(2):
            S.pool(lambda e, b=b: e.memset(pre[b][:, 0:3], 0.0), writes=["pre%d" % b])
        ev = 0
        for blk in range(4):
            S.dma("pool", lambda e, blk=blk, s=s: e.dma_start(out=xb[blk % 2][:, :, :], in_=x[s, blk * 512:(blk + 1) * 512, :].rearrange("(t p) d -> p t d", p=128)),
                  writes=["xb%d" % (blk % 2)])
            for k in range(8):
                pb = (blk * 8 + k) % 2
                for t in range(4):
                    S.pe(lambda e, pb=pb, t=t, k=k, blk=blk: e.transpose(bankb(pb)[:, t * 128:(t + 1) * 128], xb[blk % 2][:, t, k * 128:(k + 1) * 128], identB[:, :]),
                         reads=["xb%d" % (blk % 2), "identB"], writes=[PSK[pb]])
                if ev % 2 == 0:
                    S.act(lambda e, pb=pb, k=k, blk=blk: e.copy(xT[:, k, blk * 512:(blk + 1) * 512], bankb(pb)[:, 0:512]), reads=[PSK[pb]], writes=["xT.%d.%d" % (k, blk)])
                else:
                    S.dve(lambda e, pb=pb, k=k, blk=blk: e.tensor_copy(xT[:, k, blk * 512:(blk + 1) * 512], bankb(pb)[:, 0:512]), reads=[PSK[pb]], writes=["xT.%d.%d" % (k, blk)])
                ev += 1
            for tt in range(4):
                t = blk * 4 + tt
                pz = 2 + (t % 2)
                xk = ["xT.%d.%d" % (k, blk) for k in range(8)]
                for k in range(8):
                    S.pe(lambda e, pz=pz, t=t, k=k: e.matmul(bank(pz)[:, :], lhsT=xT[:, k, t * 128:(t + 1) * 128], rhs=wA[:, k, 0:512], start=(k == 0), stop=False),
                         reads=[xk[k], "wA"], writes=[PSK[pz]])
                S.pe(lambda e, pz=pz: e.matmul(bank(pz)[:, :], lhsT=ones_row[0:1, :], rhs=bz_row[0:1, :], start=False, stop=True),
                     reads=["ones_row", "bz_row"], writes=[PSK[pz]])
                S.act(lambda e, pz=pz, t=t: e.activation(out=sz[:, t, :], in_=bank(pz)[:, :], func=AF.Silu), reads=[PSK[pz]], writes=["sz.%d" % t])
                for k in range(8):
                    S.pe(lambda e, t=t, k=k: e.matmul(bank(4)[:, t * 8:(t + 1) * 8], lhsT=xT[:, k, t * 128:(t + 1) * 128], rhs=wA[:, k, 1536:1544], start=(k == 0), stop=(k == 7)),
                         reads=[xk[k], "wA"], writes=[PSK[4]])
        S.dve(lambda e: e.tensor_tensor(out=dt_raw[:, :, :], in0=bank(4)[:, 0:128].rearrange("p (t h) -> p t h", h=8), in1=bdtf[:, 0:8].unsqueeze(1).to_broadcast([128, NT, 8]), op=ALU.add),
              reads=[PSK[4], "bdtf0"], writes=["dt_raw"])
        XT_ALL = lambda blk: ["xT.%d.%d" % (k, blk) for k in range(8)]
        for c in range(8):
            pb_ = c % 2
            for blk in range(4):
                pc = 5 + (c * 4 + blk) % 2
                for k in range(8):
                    S.pe(lambda e, pc=pc, c=c, k=k, blk=blk: e.matmul(bank(pc)[:, :], lhsT=wA[:, k, 512 + c * 128:512 + (c + 1) * 128], rhs=xT[:, k, blk * 512:(blk + 1) * 512], start=(k == 0), stop=(k == 7)),
                         reads=["xT.%d.%d" % (k, blk), "wA"], writes=[PSK[pc]])
                S.act(lambda e, pc=pc, c=c, blk=blk, pb_=pb_: e.activation(out=pre[pb_][:, 3 + blk * 512:3 + (blk + 1) * 512], in_=bank(pc)[:, :], func=AF.Identity, bias=bxbc[:, c:c + 1], scale=1.0),
                      reads=[PSK[pc], "bxbc"], writes=["pre%d" % pb_])
            eng = S.dve
            ca = cacc[pb_]; pr = pre[pb_]
            eng(lambda e, ca=ca, pr=pr, c=c: e.tensor_scalar(out=ca[:, :], in0=pr[:, 3:SEQ + 3], scalar1=convw[:, c, 3:4], scalar2=None, op0=ALU.mult),
                reads=["pre%d" % pb_] + CONVW, writes=["cacc%d" % pb_])
            for kk in (2, 1, 0):
                eng(lambda e, ca=ca, pr=pr, c=c, kk=kk: e.scalar_tensor_tensor(out=ca[:, :], in0=pr[:, kk:SEQ + kk], scalar=convw[:, c, kk:kk + 1], in1=ca[:, :], op0=ALU.mult, op1=ALU.add),
                    reads=["pre%d" % pb_, "cacc%d" % pb_] + CONVW, writes=["cacc%d" % pb_])
            if c < 4:
                dst = xsT[:, c, :]; dk = "xsT.%d" % c
            elif c < 6:
                dst = BT[:, c - 4, :]; dk = "BT.%d" % (c - 4)
            else:
                dst = CT[:, c - 6, :]; dk = "CT.%d" % (c - 6)
            S.act(lambda e, ca=ca, dst=dst, c=c: e.activation(out=dst, in_=ca[:, :], func=AF.Silu, bias=convb[:, c:c + 1], scale=1.0),
                  reads=["cacc%d" % pb_, "convb"], writes=[dk])
        for t in range(NT):
            pb = t % 2
            for c in range(6):
                src = xsT[:, c, t * 128:(t + 1) * 128] if c < 4 else BT[:, c - 4, t * 128:(t + 1) * 128]
                sk = "xsT.%d" % c if c < 4 else "BT.%d" % (c - 4)
                S.pe(lambda e, pb=pb, c=c, src=src: e.transpose(bankb(pb)[:, c * 128:(c + 1) * 128], src, identB[:, :]), reads=[sk, "identB"], writes=[PSK[pb]])
            if t % 2 == 0:
                S.dve(lambda e, pb=pb, t=t: e.tensor_copy(xsB[:, t, :], bankb(pb)[:, 0:768]), reads=[PSK[pb]], writes=["xsB.%d" % t])
            else:
                S.act(lambda e, pb=pb, t=t: e.copy(xsB[:, t, :], bankb(pb)[:, 0:768]), reads=[PSK[pb]], writes=["xsB.%d" % t])
        S.act(lambda e: e.activation(out=dt_t[:, :, :], in_=dt_raw[:, :, :], func=AF.Exp), reads=["dt_raw"], writes=["dt_t"])
        S.act(lambda e: e.activation(out=dt_t[:, :, :], in_=dt_t[:, :, :], func=AF.Ln, bias=1.0, scale=1.0), reads=["dt_t"], writes=["dt_t"])

        fence()
        MSSD.reset(); TT.reset(tt_mark); RW.reset()
        m_ssd = MSSD.alloc([NT, 512], BF16)
        da = TT.alloc([NT, 8], F32); acum = TT.alloc([NT, 8], F32); nacum = TT.alloc([NT, 8], F32)
        alast = TT.alloc([NT, 8], F32); dte = TT.alloc([NT, 8], F32); ea = TT.alloc([NT, 8], F32)
        cdec = TT.alloc([NT, 8], F32); dtdte = TT.alloc([NT, 8], F32)
        stT = TT.alloc([8, 64], F32); stTb = TT.alloc([8, 64], BF16)
        LT = [TT.alloc([128], BF16) for _ in range(4)]
        MT = [TT.alloc([128], BF16) for _ in range(4)]
        xdt = [RW.alloc([8, 64], BF16) for _ in range(2)]
        xdtd = [RW.alloc([8, 64], BF16) for _ in range(2)]
        t1 = [RW.alloc([8, 64], F32) for _ in range(2)]
        t2 = [RW.alloc([8, 64], F32) for _ in range(2)]
        yg = [RW.alloc([512], F32) for _ in range(2)]
        junk = TT.alloc([256], F32)
        ss = [TT.alloc([2], F32) for _ in range(2)]
        rstd = [TT.alloc([2], F32) for _ in range(2)]

        S.dve(lambda e: e.tensor_tensor(out=da[:, :, :], in0=dt_t[:, :, :], in1=a_bc[:, :].unsqueeze(1).to_broadcast([128, NT, 8]), op=ALU.mult), reads=["dt_t", "a_bc"], writes=["da"])
        daf = da.rearrange("p t h -> p (t h)")
        S.pe(lambda e: e.matmul(bank(5)[:, 0:128], lhsT=Uf[:, :], rhs=daf, start=True, stop=True), reads=["Uf", "da"], writes=[PSK[5]])
        S.pe(lambda e: e.matmul(bank(6)[:, 0:128], lhsT=onesF[:, :], rhs=daf, start=True, stop=True), reads=["onesF", "da"], writes=[PSK[6]])
        S.dve(lambda e: e.tensor_copy(acum.rearrange("p t h -> p (t h)"), bank(5)[:, 0:128]), reads=[PSK[5]], writes=["acum"])
        S.dve(lambda e: e.tensor_scalar(out=nacum.rearrange("p t h -> p (t h)"), in0=bank(5)[:, 0:128], scalar1=-1.0, scalar2=None, op0=ALU.mult), reads=[PSK[5]], writes=["nacum"])
        S.dve(lambda e: e.tensor_copy(alast.rearrange("p t h -> p (t h)"), bank(6)[:, 0:128]), reads=[PSK[6]], writes=["alast"])
        S.dve(lambda e: e.tensor_tensor(out=dte[:, :, :], in0=alast[:, :, :], in1=acum[:, :, :], op=ALU.subtract), reads=["alast", "acum"], writes=["dte"])
        S.act(lambda e: e.activation(out=dte[:, :, :], in_=dte[:, :, :], func=AF.Exp), reads=["dte"], writes=["dte"])
        S.act(lambda e: e.activation(out=ea[:, :, :], in_=acum[:, :, :], func=AF.Exp), reads=["acum"], writes=["ea"])
        S.act(lambda e: e.activation(out=cdec[:, :, :], in_=alast[:, :, :], func=AF.Exp), reads=["alast"], writes=["cdec"])
        S.dve(lambda e: e.tensor_tensor(out=dtdte[:, :, :], in0=dt_t[:, :, :], in1=dte[:, :, :], op=ALU.mult), reads=["dt_t", "dte"], writes=["dtdte"])
        S.pool(lambda e: e.memset(stT[:, :, :], 0.0), writes=["stT"])
        S.pool(lambda e: e.memset(stTb[:, :, :], 0.0), writes=["stTb"])

        for c in range(NT):
            cs = slice(c * 128, (c + 1) * 128)
            b2 = c % 2
            xs_c = xsB[:, c, 0:512].rearrange("p (h d) -> p h d", h=8)
            S.pool(lambda e, c=c, b2=b2, xs_c=xs_c: e.tensor_tensor(out=xdt[b2][:, :, :], in0=xs_c, in1=dt_t[:, c, :].unsqueeze(2).to_broadcast([128, 8, 64]), op=ALU.mult),
                   reads=["xsB.%d" % c, "dt_t"], writes=["xdt%d" % b2])
            S.pool(lambda e, c=c, b2=b2, xs_c=xs_c: e.tensor_tensor(out=xdtd[b2][:, :, :], in0=xs_c, in1=dtdte[:, c, :].unsqueeze(2).to_broadcast([128, 8, 64]), op=ALU.mult),
                   reads=["xsB.%d" % c, "dtdte"], writes=["xdtd%d" % b2])
            S.pool(lambda e, c=c, b2=b2, xs_c=xs_c: e.tensor_tensor(out=t2[b2][:, :, :], in0=xs_c, in1=dskip_bc[:, :].unsqueeze(2).to_broadcast([128, 8, 64]), op=ALU.mult),
                   reads=["xsB.%d" % c, "dskip_bc"], writes=["t2%d" % b2])
            for g in range(2):
                S.pe(lambda e, g=g, cs=cs: e.matmul(bank(4)[:, g * 128:(g + 1) * 128], lhsT=BT[:, g, cs], rhs=CT[:, g, cs], start=True, stop=True),
                     reads=["BT.%d" % g, "CT.%d" % g], writes=[PSK[4]])
            for hh in range(2):
                pl = (c * 2 + hh) % 4
                for h4 in range(4):
                    h = hh * 4 + h4
                    S.pe(lambda e, pl=pl, h4=h4, c=c, h=h: e.matmul(bank(pl)[:, h4 * 128:(h4 + 1) * 128], lhsT=da[:, c, h:h + 1].to_broadcast([128, 128]), rhs=Uf[:, :], start=True, stop=False),
                         reads=["da", "Uf"], writes=[PSK[pl]])
                    S.pe(lambda e, pl=pl, h4=h4: e.matmul(bank(pl)[:, h4 * 128:(h4 + 1) * 128], lhsT=identB[:, :], rhs=maskneg[:, :], start=False, stop=True),
                         reads=["identB", "maskneg"], writes=[PSK[pl]])
                for h4 in range(4):
                    h = hh * 4 + h4
                    S.act(lambda e, pl=pl, h4=h4, c=c, h=h: e.activation(out=LT[h4][:, :], in_=bank(pl)[:, h4 * 128:(h4 + 1) * 128], func=AF.Exp, bias=nacum[:, c, h:h + 1], scale=1.0),
                          reads=[PSK[pl], "nacum"], writes=["LT%d" % h4])
                    g = h // 4
                    S.dve(lambda e, h4=h4, g=g: e.tensor_tensor(out=MT[h4][:, :], in0=bank(4)[:, g * 128:(g + 1) * 128], in1=LT[h4][:, :], op=ALU.mult),
                          reads=[PSK[4], "LT%d" % h4], writes=["MT%d" % h4])
                    S.pe(lambda e, h4=h4, h=h, b2=b2: e.matmul(bank(5)[:, h * 64:(h + 1) * 64], lhsT=MT[h4][:, :], rhs=xdt[b2][:, h, :], start=True, stop=True),
                         reads=["MT%d" % h4, "xdt%d" % b2], writes=[PSK[5]])
            if c > 0:
                for g in range(2):
                    S.pe(lambda e, g=g, cs=cs: e.matmul(bank(6)[:, g * 256:(g + 1) * 256], lhsT=CT[:, g, cs], rhs=stTb[:, 4 * g:4 * g + 4, :].rearrange("p h d -> p (h d)"), start=True, stop=True),
                         reads=["CT.%d" % g, "stTb"], writes=[PSK[6]])
                S.dve(lambda e, c=c, b2=b2: e.tensor_tensor(out=t1[b2][:, :, :], in0=bank(6)[:, :].rearrange("p (h d) -> p h d", h=8), in1=ea[:, c, :].unsqueeze(2).to_broadcast([128, 8, 64]), op=ALU.mult),
                      reads=[PSK[6], "ea"], writes=["t1%d" % b2])
                S.dve(lambda e, b2=b2: e.tensor_tensor(out=t1[b2][:, :, :], in0=bank(5)[:, :].rearrange("p (h d) -> p h d", h=8), in1=t1[b2][:, :, :], op=ALU.add),
                      reads=[PSK[5], "t1%d" % b2], writes=["t1%d" % b2])
                S.dve(lambda e, b2=b2: e.tensor_tensor(out=t1[b2][:, :, :], in0=t1[b2][:, :, :], in1=t2[b2][:, :, :], op=ALU.add),
                      reads=["t1%d" % b2, "t2%d" % b2], writes=["t1%d" % b2])
            else:
                S.dve(lambda e, b2=b2: e.tensor_tensor(out=t1[b2][:, :, :], in0=bank(5)[:, :].rearrange("p (h d) -> p h d", h=8), in1=t2[b2][:, :, :], op=ALU.add),
                      reads=[PSK[5], "t2%d" % b2], writes=["t1%d" % b2])
            S.dve(lambda e, c=c, b2=b2: e.tensor_tensor(out=yg[b2][:, :], in0=t1[b2].rearrange("p h d -> p (h d)"), in1=sz[:, c, :], op=ALU.mult),
                  reads=["t1%d" % b2, "sz.%d" % c], writes=["yg%d" % b2])
            for g in range(2):
                S.act(lambda e, g=g, b2=b2: e.activation(out=junk[:, :], in_=yg[b2][:, g * 256:(g + 1) * 256], func=AF.Square, accum_out=ss[b2][:, g:g + 1]),
                      reads=["yg%d" % b2], writes=["junk", "ss%d.%d" % (b2, g)])
            S.act(lambda e, b2=b2: e.activation(out=rstd[b2][:, :], in_=ss[b2][:, :], func=AF.Ln, bias=RMS_EPS, scale=1.0 / 256.0),
                  reads=["ss%d.0" % b2, "ss%d.1" % b2], writes=["rstd%d" % b2])
            S.act(lambda e, b2=b2: e.activation(out=rstd[b2][:, :], in_=rstd[b2][:, :], func=AF.Exp, scale=-0.5), reads=["rstd%d" % b2], writes=["rstd%d" % b2])
            for g in range(2):
                S.dve(lambda e, g=g, b2=b2, c=c: e.scalar_tensor_tensor(out=m_ssd[:, c, g * 256:(g + 1) * 256], in0=yg[b2][:, g * 256:(g + 1) * 256], scalar=rstd[b2][:, g:g + 1], in1=gssd_bc[:, g * 256:(g + 1) * 256], op0=ALU.mult, op1=ALU.mult),
                      reads=["yg%d" % b2, "rstd%d" % b2, "gssd_bc"], writes=["m_ssd.%d" % c])
            if c < NT - 1:
                for g in range(2):
                    S.pe(lambda e, g=g, c=c, b2=b2: e.matmul(bank(7)[:, g * 256:(g + 1) * 256], lhsT=xsB[:, c, 512 + g * 128:512 + (g + 1) * 128], rhs=xdtd[b2][:, 4 * g:4 * g + 4, :].rearrange("p h d -> p (h d)"), start=True, stop=True),
                         reads=["xsB.%d" % c, "xdtd%d" % b2], writes=[PSK[7]])
                S.dve(lambda e, c=c: e.tensor_tensor(out=stT[:, :, :], in0=stT[:, :, :], in1=cdec[:, c, :].unsqueeze(2).to_broadcast([128, 8, 64]), op=ALU.mult),
                      reads=["stT", "cdec"], writes=["stT"])
                S.dve(lambda e: e.tensor_tensor(out=stT[:, :, :], in0=bank(7)[:, 0:512].rearrange("p (h d) -> p h d", h=8), in1=stT[:, :, :], op=ALU.add),
                      reads=[PSK[7], "stT"], writes=["stT"])
                S.act(lambda e: e.copy(stTb[:, :, :], stT[:, :, :]), reads=["stT"], writes=["stTb"])

        if dbg == 'ssd' and s == 0:
            RX.reset()
            dm = dbg_out("dbg_mssd", [128, NT * 512])
            cvt = RX.alloc([NT * 512], F32) if dbg == 'ssd' else None
            S.dve(lambda e: e.tensor_copy(cvt[:, :], m_ssd.rearrange("p t d -> p (t d)")), reads=["m_ssd.%d" % c for c in range(NT)], writes=["cvt"])
            outs.append(S.dma("sp", lambda e: e.dma_start(out=dm[:, :], in_=cvt[:, :]), reads=["cvt"]))
        if dbg == "ssd":
            break

        fence()
        RW.reset(); RACC.reset(); TT.reset()
        wB = RW.alloc([8, 1544], BF16)
        wo = RW.alloc([8, 1024], BF16)
        ah = RW.alloc([1024], F32)
        hTf = RW.alloc([8, 128], F32)
        qT = RACC.alloc([4, SEQ], BF16)
        kT = RACC.alloc([4, SEQ], BF16)
        v_aug = RACC.alloc([NT, 8, 65], BF16)
        xres = [RACC.alloc([1024], F32) for _ in range(2)]
        hpre = [RACC.alloc([1024], F32), TT.alloc([1024], F32)]
        f_raw = TT.alloc([NT, 8], F32); Gc = TT.alloc([NT, 8], F32); tot = TT.alloc([NT, 8], F32)
        Pp = TT.alloc([NT, 8], F32); Gf = TT.alloc([NT, 8], F32); Gend = TT.alloc([NT, 8], F32)
        biasT = TT.alloc([8, NT, NT], F32)
        PT = [TT.alloc([4, 128], BF16) for _ in range(3)]
        yatt = [TT.alloc([8, 64], BF16) for _ in range(2)]
        rden = [TT.alloc([8], F32) for _ in range(2)]
        mT = [TT.alloc([8, 128], BF16) for _ in range(2)]
        bst = [TT.alloc([2, 6], F32) for _ in range(2)]
        mv = [TT.alloc([2], F32) for _ in range(2)]
        rs1 = [TT.alloc([1], F32) for _ in range(2)]

        for half in range(2):
            S.dma("pool", lambda e, half=half: e.dma_start(out=wB[:, 4 * half:4 * half + 4, :], in_=w_in.rearrange("(k p) c -> p k c", p=128)[:, 4 * half:4 * half + 4, 1544:3088]),
                  writes=["wB"])
            S.dma("pool", lambda e, half=half: e.dma_start(out=wo[:, 4 * half:4 * half + 4, :], in_=w_out.rearrange("(k p) c -> p k c", p=128)[:, 4 * half:4 * half + 4, :]),
                  writes=["wo"])
        S.dma("sp", lambda e: e.dma_start(out=lnG[:, :], in_=ln1_g[0:1, :].to_broadcast([128, 1024])), writes=["lnG"])
        S.dma("sp", lambda e: e.dma_start(out=lnB[:, :], in_=ln1_b[0:1, :].to_broadcast([128, 1024])), writes=["lnB"])
        S.pool(lambda e: e.memset(v_aug[:, :, :, 64:65], 1.0), writes=["v_ones"])
        evq = 0
        for qk in range(2):
            dstT = qT if qk == 0 else kT
            bcol = bq if qk == 0 else bk
            nm = "qT" if qk == 0 else "kT"
            for p in range(4):
                for blk in range(4):
                    pq = evq % 4
                    for k in range(8):
                        S.pe(lambda e, pq=pq, k=k, p=p, blk=blk, qk=qk: e.matmul(bank(pq)[:, :], lhsT=wB[:, k, qk * 512 + p * 128:qk * 512 + (p + 1) * 128], rhs=xT[:, k, blk * 512:(blk + 1) * 512], start=(k == 0), stop=(k == 7)),
                             reads=["xT.%d.%d" % (k, blk), "wB"], writes=[PSK[pq]])
                    if evq % 2 == 0:
                        S.act(lambda e, pq=pq, p=p, blk=blk, dstT=dstT, bcol=bcol: e.activation(out=dstT[:, p, blk * 512:(blk + 1) * 512], in_=bank(pq)[:, :], func=AF.Identity, bias=bcol[:, p:p + 1], scale=1.0),
                              reads=[PSK[pq], "bq", "bk"], writes=["%s.%d.%d" % (nm, p, blk)])
                    else:
                        S.dve(lambda e, pq=pq, p=p, blk=blk, dstT=dstT, bcol=bcol: e.tensor_scalar(out=dstT[:, p, blk * 512:(blk + 1) * 512], in0=bank(pq)[:, :], scalar1=bcol[:, p:p + 1], scalar2=None, op0=ALU.add),
                              reads=[PSK[pq], "bq", "bk"], writes=["%s.%d.%d" % (nm, p, blk)])
                    evq += 1
        for t in range(NT):
            pv = 4 + (t % 2)
            blk = t // 4
            for k in range(8):
                S.pe(lambda e, pv=pv, t=t, k=k: e.matmul(bank(pv)[:, :], lhsT=xT[:, k, t * 128:(t + 1) * 128], rhs=wB[:, k, 1024:1536], start=(k == 0), stop=False),
                     reads=["xT.%d.%d" % (k, blk), "wB"], writes=[PSK[pv]])
            S.pe(lambda e, pv=pv: e.matmul(bank(pv)[:, :], lhsT=ones_row[0:1, :], rhs=bv_row[0:1, :], start=False, stop=True),
                 reads=["ones_row", "bv_row"], writes=[PSK[pv]])
            if t % 2 == 0:
                S.act(lambda e, pv=pv, t=t: e.copy(v_aug[:, t, :, 0:64], bank(pv)[:, :].rearrange("p (h d) -> p h d", h=8)), reads=[PSK[pv]], writes=["v.%d" % t])
            else:
                S.dve(lambda e, pv=pv, t=t: e.tensor_copy(v_aug[:, t, :, 0:64], bank(pv)[:, :].rearrange("p (h d) -> p h d", h=8)), reads=[PSK[pv]], writes=["v.%d" % t])
            for k in range(8):
                S.pe(lambda e, t=t, k=k: e.matmul(bank(6)[:, t * 8:(t + 1) * 8], lhsT=xT[:, k, t * 128:(t + 1) * 128], rhs=wB[:, k, 1536:1544], start=(k == 0), stop=(k == 7)),
                     reads=["xT.%d.%d" % (k, blk), "wB"], writes=[PSK[6]])
        S.dve(lambda e: e.tensor_tensor(out=f_raw[:, :, :], in0=bank(6)[:, 0:128].rearrange("p (t h) -> p t h", h=8), in1=bdtf[:, 8:16].unsqueeze(1).to_broadcast([128, NT, 8]), op=ALU.add),
              reads=[PSK[6], "bdtf1"], writes=["f_raw"])
        S.act(lambda e: e.activation(out=f_raw[:, :, :], in_=f_raw[:, :, :], func=AF.Exp, scale=-1.0), reads=["f_raw"], writes=["f_raw"])
        S.act(lambda e: e.activation(out=f_raw[:, :, :], in_=f_raw[:, :, :], func=AF.Ln, bias=1.0, scale=1.0), reads=["f_raw"], writes=["f_raw"])
        frf = f_raw.rearrange("p t h -> p (t h)")
        S.pe(lambda e: e.matmul(bank(7)[:, 0:128], lhsT=Uf[:, :], rhs=frf, start=True, stop=True), reads=["Uf", "f_raw"], writes=[PSK[7]])
        S.pe(lambda e: e.matmul(bank(7)[:, 128:256], lhsT=onesF[:, :], rhs=frf, start=True, stop=True), reads=["onesF", "f_raw"], writes=[PSK[7]])
        S.dve(lambda e: e.tensor_copy(Gc.rearrange("p t h -> p (t h)"), bank(7)[:, 0:128]), reads=[PSK[7]], writes=["Gc"])
        S.dve(lambda e: e.tensor_copy(tot.rearrange("p t h -> p (t h)"), bank(7)[:, 128:256]), reads=[PSK[7]], writes=["tot"])
        S.pool(lambda e: e.memset(Pp[:, 0, :], 0.0), writes=["Pp"])
        for t in range(1, NT):
            S.dve(lambda e, t=t: e.tensor_tensor(out=Pp[:, t, :], in0=Pp[:, t - 1, :], in1=tot[:, t - 1, :], op=ALU.add), reads=["Pp", "tot"], writes=["Pp"])
        S.dve(lambda e: e.tensor_tensor(out=Gf[:, :, :], in0=Gc[:, :, :], in1=Pp[:, :, :], op=ALU.add), reads=["Gc", "Pp"], writes=["Gf"])
        S.dve(lambda e: e.tensor_tensor(out=Gend[:, :, :], in0=tot[:, :, :], in1=Pp[:, :, :], op=ALU.add), reads=["tot", "Pp"], writes=["Gend"])
        for h in range(8):
            S.dve(lambda e, h=h: e.tensor_tensor(out=biasT[:, h, :, :], in0=Gf[:, :, h].unsqueeze(1).to_broadcast([128, NT, NT]), in1=Gend[:, :, h].unsqueeze(2).to_broadcast([128, NT, NT]), op=ALU.subtract),
                  reads=["Gf", "Gend"], writes=["biasT"])

        fence()
        RX.reset()
        hT = RX.alloc([8, SEQ], BF16)
        NTI = int(os.environ.get("KATT_TILES", NT))
        units = []
        for i in range(NTI):
            for h in range(8):
                for j0 in range(0, i + 1, 4):
                    units.append((i, h, list(range(j0, min(j0 + 4, i + 1)))))

        def emit_S(u, gi):
            i, h, js = u
            p = h // 2; r0 = (h % 2) * 64
            ps = gi % 2
            for jj, j in enumerate(js):
                S.pe(lambda e, ps=ps, jj=jj, j=j, p=p, r0=r0, i=i: e.matmul(bank(ps)[:, jj * 128:(jj + 1) * 128], lhsT=kT[r0:r0 + 64, p, j * 128:(j + 1) * 128], rhs=qT[r0:r0 + 64, p, i * 128:(i + 1) * 128], start=True, stop=True),
                     reads=["kT.%d.%d" % (p, j // 4), "qT.%d.%d" % (p, i // 4)], writes=[PSK[ps]])

        def emit_E(u, gi):
            i, h, js = u
            ps = gi % 2; pb = gi % 3
            for jj, j in enumerate(js):
                S.act(lambda e, ps=ps, pb=pb, jj=jj, j=j, h=h, i=i: e.activation(out=PT[pb][:, jj, :], in_=bank(ps)[:, jj * 128:(jj + 1) * 128], func=AF.Exp, bias=biasT[:, h, i, j:j + 1], scale=ATT_SCALE),
                      reads=[PSK[ps], "biasT"], writes=["PT%d.%d" % (pb, jj)])
                if j == i:
                    S.dve(lambda e, pb=pb, jj=jj: e.tensor_tensor(out=PT[pb][:, jj, :], in0=PT[pb][:, jj, :], in1=triU[:, :], op=ALU.mult),
                          reads=["PT%d.%d" % (pb, jj), "triU"], writes=["PT%d.%d" % (pb, jj)])

        def emit_V(u, gi):
            i, h, js = u
            pb = gi % 3
            po = 2 + h // 4; oc = (h % 4) * 65
            for jj, j in enumerate(js):
                S.pe(lambda e, pb=pb, jj=jj, j=j, h=h, po=po, oc=oc, i=i: e.matmul(bank(po)[:, oc:oc + 65], lhsT=PT[pb][:, jj, :], rhs=v_aug[:, j, h, :], start=(j == 0), stop=(j == i)),
                     reads=["PT%d.%d" % (pb, jj), "v.%d" % j, "v_ones"], writes=[PSK[po]])

        def tail_N(i):
            b2 = i % 2
            S.dma("sp", lambda e, i=i, b2=b2, s=s: e.dma_start(out=xres[b2][:, :], in_=x[s, i * 128:(i + 1) * 128, :]), writes=["xres%d" % b2])
            for hb in range(2):
                ov = bank(2 + hb)[:, 0:260].rearrange("p (h d) -> p h d", h=4)
                S.dve(lambda e, hb=hb, ov=ov, b2=b2: e.reciprocal(rden[b2][:, 4 * hb:4 * hb + 4], ov[:, :, 64]), reads=[PSK[2 + hb]], writes=["rden%d.%d" % (b2, hb)])
                S.dve(lambda e, hb=hb, ov=ov, b2=b2: e.tensor_tensor(out=yatt[b2][:, 4 * hb:4 * hb + 4, :], in0=ov[:, :, 0:64], in1=rden[b2][:, 4 * hb:4 * hb + 4].unsqueeze(2).to_broadcast([128, 4, 64]), op=ALU.mult),
                      reads=[PSK[2 + hb], "rden%d.%d" % (b2, hb)], writes=["yatt%d.%d" % (b2, hb)])

        def tail_T(i):
            b2 = i % 2
            yf = yatt[b2].rearrange("p h d -> p (h d)")
            for ec in range(8):
                src = m_ssd[:, i, ec * 128:(ec + 1) * 128] if ec < 4 else yf[:, (ec - 4) * 128:(ec - 3) * 128]
                rk = ["m_ssd.%d" % i] if ec < 4 else ["yatt%d.%d" % (b2, (ec - 4) // 2)]
                S.pe(lambda e, ec=ec, src=src: e.transpose(bankb(4)[:, ec * 128:(ec + 1) * 128], src, identB[:, :]), reads=rk + ["identB"], writes=[PSK[4]])
            S.dve(lambda e, b2=b2: e.tensor_copy(mT[b2].rearrange("p a b -> p (a b)"), bankb(4)[:, 0:1024]), reads=[PSK[4]], writes=["mT%d" % b2])

        def tail_O(i):
            b2 = i % 2
            for half in range(2):
                for ec in range(8):
                    S.pe(lambda e, half=half, ec=ec, b2=b2: e.matmul(pd[3][:, half * 512:(half + 1) * 512], lhsT=mT[b2][:, ec, :], rhs=wo[:, ec, half * 512:(half + 1) * 512], start=(ec == 0), stop=(ec == 7)),
                         reads=["mT%d" % b2, "wo"], writes=[PSK[6 + half]])
            hp = hpre[b2]
            S.dve(lambda e, hp=hp, b2=b2: e.scalar_tensor_tensor(out=hp[:, :], in0=xres[b2][:, :], scalar=ALPHA, in1=pd[3][:, :], op0=ALU.mult, op1=ALU.add),
                  reads=["xres%d" % b2, PSK[6], PSK[7]], writes=["hpre%d" % b2])
            for c2 in range(2):
                S.dve(lambda e, hp=hp, c2=c2, b2=b2: e.bn_stats(bst[b2][:, c2, :], hp[:, c2 * 512:(c2 + 1) * 512]), reads=["hpre%d" % b2], writes=["bst%d.%d" % (b2, c2)])
            S.dve(lambda e, b2=b2: e.bn_aggr(mv[b2][:, :], bst[b2][:, :, :]), reads=["bst%d.0" % b2, "bst%d.1" % b2], writes=["mv%d" % b2])
            S.act(lambda e, b2=b2: e.activation(out=rs1[b2][:, :], in_=mv[b2][:, 1:2], func=AF.Ln, bias=LN_EPS, scale=1.0), reads=["mv%d" % b2], writes=["rs1%d" % b2])
            S.act(lambda e, b2=b2: e.activation(out=rs1[b2][:, :], in_=rs1[b2][:, :], func=AF.Exp, scale=-0.5), reads=["rs1%d" % b2], writes=["rs1%d" % b2])
            S.dve(lambda e, hp=hp, b2=b2: e.tensor_scalar(out=hp[:, :], in0=hp[:, :], scalar1=mv[b2][:, 0:1], scalar2=rs1[b2][:, 0:1], op0=ALU.subtract, op1=ALU.mult),
                  reads=["hpre%d" % b2, "mv%d" % b2, "rs1%d" % b2], writes=["hpre%d" % b2])
            S.dve(lambda e, hp=hp: e.tensor_tensor(out=hp[:, :], in0=hp[:, :], in1=lnG[:, :], op=ALU.mult), reads=["hpre%d" % b2, "lnG"], writes=["hpre%d" % b2])
            S.dve(lambda e, hp=hp: e.tensor_tensor(out=hp[:, :], in0=hp[:, :], in1=lnB[:, :], op=ALU.add), reads=["hpre%d" % b2, "lnB"], writes=["hpre%d" % b2])
            S.act(lambda e, hp=hp: e.mul(ah[:, :], hp[:, :], ALPHA), reads=["hpre%d" % b2], writes=["ah"])
            S.dma("sp", lambda e, i=i, s=s: e.dma_start(out=h_scr[s, i * 128:(i + 1) * 128, :], in_=ah[:, :]), reads=["ah"], writes=["h_scr.%d" % i])

        def tail_H(i, half):
            b2 = i % 2
            hp = hpre[b2]
            for q4 in range(4):
                ec = half * 4 + q4
                S.pe(lambda e, q4=q4, ec=ec, hp=hp: e.transpose(bank(5)[:, q4 * 128:(q4 + 1) * 128], hp[:, ec * 128:(ec + 1) * 128], identF[:, :]), reads=["hpre%d" % b2, "identF"], writes=[PSK[5]])
            S.dve(lambda e, half=half, i=i: e.tensor_copy(hT[:, 4 * half:4 * half + 4, i * 128:(i + 1) * 128], bank(5)[:, :].rearrange("p (a b) -> p a b", a=4)), reads=[PSK[5]], writes=["hT.%d.%d" % (i, half)])
            S.dve(lambda e, half=half: e.tensor_copy(hTf[:, 4 * half:4 * half + 4, :], bank(5)[:, :].rearrange("p (a b) -> p a b", a=4)), reads=[PSK[5]], writes=["hTf.%d" % half])

        def tail_R(i):
            for ec in range(8):
                S.pe(lambda e, ec=ec: e.matmul(bank(4)[:, 0:20], lhsT=hTf[:, ec, :], rhs=rw[:, ec, :], start=(ec == 0), stop=(ec == 7)), reads=["hTf.%d" % (ec // 4)] + RWK, writes=[PSK[4]])
            S.dve(lambda e, i=i: e.tensor_tensor(out=logits[:, i, :], in0=bank(4)[:, 0:20], in1=rb_bc[:, :], op=ALU.add), reads=[PSK[4], "rb0", "rb1"], writes=["logits.%d" % i])

        TAIL = [(0, tail_N), (1, tail_T), (2, tail_O), (5, lambda i: tail_H(i, 0)), (6, lambda i: tail_H(i, 1)), (7, tail_R)]
        pending = []
        def tick():
            keep = []
            for ent in pending:
                if ent[0] <= 0:
                    ent[1](ent[2])
                else:
                    ent[0] -= 1
                    keep.append(ent)
            pending[:] = keep
        prev = None
        for gi, u in enumerate(units):
            emit_S(u, gi)
            emit_E(u, gi)
            if prev is not None:
                emit_V(prev[0], prev[1])
                if prev[0][0] != u[0]:
                    for dly, fn in TAIL:
                        pending.append([dly, fn, prev[0][0]])
            tick()
            prev = (u, gi)
        if prev is not None:
            emit_V(prev[0], prev[1])
            for dly, fn in TAIL:
                pending.append([dly, fn, prev[0][0]])
        while pending:
            tick()

        if dbg == "att" and s == 0:
            fence()
            RACC.reset()
            cvt = RACC.alloc([NT * 1024], BF16)
            d1 = dbg_out("dbg_hT", [128, 8 * SEQ]); d2 = dbg_out("dbg_logits", [128, NT * 20])
            cv2 = RACC.alloc([2 * SEQ], F32)
            for q in range(4):
                S.dve(lambda e, q=q: e.tensor_copy(cv2[:, :], hT[:, 2 * q:2 * q + 2, :].rearrange("p a b -> p (a b)")), reads=[], writes=["cv2"])
                outs.append(S.dma("sp", lambda e, q=q: e.dma_start(out=d1[:, 2 * q * SEQ:(2 * q + 2) * SEQ], in_=cv2[:, :]), reads=["cv2"]))
            outs.append(S.dma("sp", lambda e: e.dma_start(out=d2[:, :], in_=logits.rearrange("p t j -> p (t j)")), reads=["logits.%d" % i for i in range(int(os.environ.get("KATT_TILES", NT)))] if int(os.environ.get("KATT_STAGE", 9)) >= 7 else []))
            break


        fence()
        TT.reset(); RW.reset(); RACC.reset()
        S.dma("sp", lambda e: e.dma_start(out=lnG[:, :], in_=ln2_g[0:1, :].to_broadcast([128, 1024])), writes=["lnG"])
        S.dma("sp", lambda e: e.dma_start(out=lnB[:, :], in_=ln2_b[0:1, :].to_broadcast([128, 1024])), writes=["lnB"])
        LOGK = ["logits.%d" % i for i in range(NT)]
        lg = logits[:, :, 0:4]
        le4 = logits[:, :, 4:20].rearrange("p t (g j) -> p t g j", g=4)
        gmax = TT.alloc([NT], F32); goh = TT.alloc([NT, 4], F32); gex = TT.alloc([NT, 4], F32)
        gsum = TT.alloc([NT], F32); gval = TT.alloc([NT], F32)
        tmp16 = TT.alloc([NT, 4, 4], F32); esel = TT.alloc([NT, 4], F32)
        m1 = TT.alloc([NT], F32); oh1 = TT.alloc([NT, 4], F32); e2 = TT.alloc([NT, 4], F32)
        m2 = TT.alloc([NT], F32); oh2 = TT.alloc([NT, 4], F32); dd = TT.alloc([NT], F32)
        w1 = TT.alloc([NT], F32); w2 = TT.alloc([NT], F32); cw1 = TT.alloc([NT], F32); cw2 = TT.alloc([NT], F32)
        cj = TT.alloc([NT, 4], F32); cj2 = TT.alloc([NT, 4], F32)
        bc4 = lambda a: a.unsqueeze(2).to_broadcast([128, NT, 4])
        S.dve(lambda e: e.tensor_reduce(out=gmax[:, :], in_=lg, axis=AX.X, op=ALU.max), reads=LOGK, writes=["gmax"])
        S.dve(lambda e: e.tensor_tensor(out=goh[:, :, :], in0=lg, in1=bc4(gmax[:, :]), op=ALU.is_equal), reads=LOGK + ["gmax"], writes=["goh"])
        S.dve(lambda e: e.tensor_tensor(out=gex[:, :, :], in0=lg, in1=bc4(gmax[:, :]), op=ALU.subtract), reads=LOGK + ["gmax"], writes=["gex"])
        S.act(lambda e: e.activation(out=gex[:, :, :], in_=gex[:, :, :], func=AF.Exp), reads=["gex"], writes=["gex"])
        S.dve(lambda e: e.tensor_reduce(out=gsum[:, :], in_=gex[:, :, :], axis=AX.X, op=ALU.add), reads=["gex"], writes=["gsum"])
        S.dve(lambda e: e.reciprocal(gval[:, :], gsum[:, :]), reads=["gsum"], writes=["gval"])
        S.dve(lambda e: e.tensor_tensor(out=tmp16[:, :, :, :], in0=le4, in1=goh[:, :, :].unsqueeze(3).to_broadcast([128, NT, 4, 4]), op=ALU.mult), reads=LOGK + ["goh"], writes=["tmp16"])
        S.dve(lambda e: e.tensor_reduce(out=esel[:, :, :], in_=tmp16.rearrange("p t g j -> p t j g"), axis=AX.X, op=ALU.add), reads=["tmp16"], writes=["esel"])
        S.dve(lambda e: e.tensor_reduce(out=m1[:, :], in_=esel[:, :, :], axis=AX.X, op=ALU.max), reads=["esel"], writes=["m1"])
        S.dve(lambda e: e.tensor_tensor(out=oh1[:, :, :], in0=esel[:, :, :], in1=bc4(m1[:, :]), op=ALU.is_equal), reads=["esel", "m1"], writes=["oh1"])
        S.dve(lambda e: e.scalar_tensor_tensor(out=e2[:, :, :], in0=oh1[:, :, :], scalar=-1e30, in1=esel[:, :, :], op0=ALU.mult, op1=ALU.add), reads=["oh1", "esel"], writes=["e2"])
        S.dve(lambda e: e.tensor_reduce(out=m2[:, :], in_=e2[:, :, :], axis=AX.X, op=ALU.max), reads=["e2"], writes=["m2"])
        S.dve(lambda e: e.tensor_tensor(out=oh2[:, :, :], in0=e2[:, :, :], in1=bc4(m2[:, :]), op=ALU.is_equal), reads=["e2", "m2"], writes=["oh2"])
        S.dve(lambda e: e.tensor_tensor(out=dd[:, :], in0=m2[:, :], in1=m1[:, :], op=ALU.subtract), reads=["m1", "m2"], writes=["dd"])
        S.act(lambda e: e.activation(out=dd[:, :], in_=dd[:, :], func=AF.Exp), reads=["dd"], writes=["dd"])
        S.dve(lambda e: e.tensor_scalar(out=w1[:, :], in0=dd[:, :], scalar1=1.0, scalar2=None, op0=ALU.add), reads=["dd"], writes=["w1"])
        S.dve(lambda e: e.reciprocal(w1[:, :], w1[:, :]), reads=["w1"], writes=["w1"])
        S.dve(lambda e: e.tensor_tensor(out=w2[:, :], in0=dd[:, :], in1=w1[:, :], op=ALU.mult), reads=["dd", "w1"], writes=["w2"])
        S.dve(lambda e: e.tensor_tensor(out=cw1[:, :], in0=gval[:, :], in1=w1[:, :], op=ALU.mult), reads=["gval", "w1"], writes=["cw1"])
        S.dve(lambda e: e.tensor_tensor(out=cw2[:, :], in0=gval[:, :], in1=w2[:, :], op=ALU.mult), reads=["gval", "w2"], writes=["cw2"])
        S.dve(lambda e: e.tensor_tensor(out=cj[:, :, :], in0=oh1[:, :, :], in1=bc4(cw1[:, :]), op=ALU.mult), reads=["oh1", "cw1"], writes=["cj"])
        S.dve(lambda e: e.tensor_tensor(out=cj2[:, :, :], in0=oh2[:, :, :], in1=bc4(cw2[:, :]), op=ALU.mult), reads=["oh2", "cw2"], writes=["cj2"])
        S.dve(lambda e: e.tensor_tensor(out=cj[:, :, :], in0=cj[:, :, :], in1=cj2[:, :, :], op=ALU.add), reads=["cj", "cj2"], writes=["cj"])
        S.dve(lambda e: e.tensor_tensor(out=comb.rearrange("p t (g j) -> p t g j", g=4), in0=goh[:, :, :].unsqueeze(3).to_broadcast([128, NT, 4, 4]), in1=cj[:, :, :].unsqueeze(2).to_broadcast([128, NT, 4, 4]), op=ALU.mult),
              reads=["goh", "cj"], writes=["comb"])

        acc = RACC.alloc([NT, 1024], F32)
        wgu = [RW.alloc([8, 1024], BF16) for _ in range(2)]
        wdn = [RW.alloc([4, 1024], BF16) for _ in range(2)]
        sg = [TT.alloc([512], BF16) for _ in range(2)]
        actT = [TT.alloc([4, 512], BF16) for _ in range(2)]
        obuf = [TT.alloc([1024], F32) for _ in range(2)]
        bst2 = [TT.alloc([2, 6], F32) for _ in range(2)]
        mv2 = [TT.alloc([2], F32) for _ in range(2)]
        rs2 = [TT.alloc([1], F32) for _ in range(2)]
        for q in range(4):
            S.dma("sp", lambda e, q=q, s=s: e.dma_start(out=acc[:, 4 * q:4 * q + 4, :], in_=h_scr[s, q * 512:(q + 1) * 512, :].rearrange("(t p) d -> p t d", p=128)),
                  reads=["h_scr.%d" % t for t in range(4 * q, 4 * q + 4)], writes=["acc.%d" % t for t in range(4 * q, 4 * q + 4)])
        NEXP = int(os.environ.get("KNEXP", 16))

        def load_expert(ex):
            sl = ex % 2
            S.dma("pool", lambda e, ex=ex, sl=sl: e.dma_start(out=wgu[sl][:, :, 0:512], in_=w_gate[ex].rearrange("(k p) f -> p k f", p=128)), writes=["wg.%d" % sl])
            S.dma("pool", lambda e, ex=ex, sl=sl: e.dma_start(out=wgu[sl][:, :, 512:1024], in_=w_up[ex].rearrange("(k p) f -> p k f", p=128)), writes=["wu.%d" % sl])
            S.dma("pool", lambda e, ex=ex, sl=sl: e.dma_start(out=wdn[sl][:, :, :], in_=w_down[ex].rearrange("(k p) d -> p k d", p=128)), writes=["wd.%d" % sl])

        load_expert(0)
        if NEXP > 1:
            load_expert(1)
        cg = 0; cd = 0
        for ex in range(NEXP):
            sl = ex % 2
            for tb in range(4):
                ab = (ex * 4 + tb) % 2
                hk = ["hT.%d.%d" % (i, hf) for i in range(4 * tb, 4 * tb + 4) for hf in range(2)]
                for fc in range(4):
                    pg = cg % 2; cg += 1
                    for k in range(8):
                        S.pe(lambda e, pg=pg, k=k, fc=fc, tb=tb, sl=sl: e.matmul(bank(pg)[:, :], lhsT=wgu[sl][:, k, fc * 128:(fc + 1) * 128], rhs=hT[:, k, tb * 512:(tb + 1) * 512], start=(k == 0), stop=(k == 7)),
                             reads=["wg.%d" % sl] + hk, writes=[PSK[pg]])
                    for k in range(8):
                        S.pe(lambda e, pg=pg, k=k, fc=fc, tb=tb, sl=sl: e.matmul(bank(2 + pg)[:, :], lhsT=wgu[sl][:, k, 512 + fc * 128:512 + (fc + 1) * 128], rhs=hT[:, k, tb * 512:(tb + 1) * 512], start=(k == 0), stop=(k == 7)),
                             reads=["wu.%d" % sl] + hk, writes=[PSK[2 + pg]])
                    S.act(lambda e, pg=pg: e.activation(out=sg[pg][:, :], in_=bank(pg)[:, :], func=AF.Silu), reads=[PSK[pg]], writes=["sg%d" % pg])
                    S.dve(lambda e, pg=pg, ab=ab, fc=fc: e.tensor_tensor(out=actT[ab][:, fc, :], in0=bank(2 + pg)[:, :], in1=sg[pg][:, :], op=ALU.mult),
                          reads=[PSK[2 + pg], "sg%d" % pg], writes=["actT%d.%d" % (ab, fc)])
                for tt in range(4):
                    t = tb * 4 + tt
                    pdi = 2 + (cd % 2); cd += 1
                    for half in range(2):
                        for fc in range(4):
                            S.pe(lambda e, pdi=pdi, half=half, fc=fc, ab=ab, tt=tt, sl=sl: e.matmul(pd[pdi][:, half * 512:(half + 1) * 512], lhsT=actT[ab][:, fc, tt * 128:(tt + 1) * 128], rhs=wdn[sl][:, fc, half * 512:(half + 1) * 512], start=(fc == 0), stop=(fc == 3)),
                                 reads=["actT%d.%d" % (ab, fc), "wd.%d" % sl], writes=[PSK[2 * pdi + half]])
                    S.dve(lambda e, pdi=pdi, t=t, ex=ex: e.scalar_tensor_tensor(out=acc[:, t, :], in0=pd[pdi][:, :], scalar=comb[:, t, ex:ex + 1], in1=acc[:, t, :], op0=ALU.mult, op1=ALU.add),
                          reads=[PSK[2 * pdi], PSK[2 * pdi + 1], "comb", "acc.%d" % t], writes=["acc.%d" % t])
            if ex + 2 < NEXP:
                load_expert(ex + 2)
        for t in range(NT):
            b2 = t % 2
            for c2 in range(2):
                S.dve(lambda e, c2=c2, t=t, b2=b2: e.bn_stats(bst2[b2][:, c2, :], acc[:, t, c2 * 512:(c2 + 1) * 512]), reads=["acc.%d" % t], writes=["bst2%d.%d" % (b2, c2)])
            S.dve(lambda e, b2=b2: e.bn_aggr(mv2[b2][:, :], bst2[b2][:, :, :]), reads=["bst2%d.0" % b2, "bst2%d.1" % b2], writes=["mv2%d" % b2])
            S.act(lambda e, b2=b2: e.activation(out=rs2[b2][:, :], in_=mv2[b2][:, 1:2], func=AF.Ln, bias=LN_EPS, scale=1.0), reads=["mv2%d" % b2], writes=["rs2%d" % b2])
            S.act(lambda e, b2=b2: e.activation(out=rs2[b2][:, :], in_=rs2[b2][:, :], func=AF.Exp, scale=-0.5), reads=["rs2%d" % b2], writes=["rs2%d" % b2])
            S.dve(lambda e, t=t, b2=b2: e.tensor_scalar(out=obuf[b2][:, :], in0=acc[:, t, :], scalar1=mv2[b2][:, 0:1], scalar2=rs2[b2][:, 0:1], op0=ALU.subtract, op1=ALU.mult),
                  reads=["acc.%d" % t, "mv2%d" % b2, "rs2%d" % b2], writes=["obuf%d" % b2])
            S.dve(lambda e, b2=b2: e.tensor_tensor(out=obuf[b2][:, :], in0=obuf[b2][:, :], in1=lnG[:, :], op=ALU.mult), reads=["obuf%d" % b2, "lnG"], writes=["obuf%d" % b2])
            S.dve(lambda e, b2=b2: e.tensor_tensor(out=obuf[b2][:, :], in0=obuf[b2][:, :], in1=lnB[:, :], op=ALU.add), reads=["obuf%d" % b2, "lnB"], writes=["obuf%d" % b2])
            outs.append(S.dma("sp", lambda e, t=t, b2=b2, s=s: e.dma_start(out=out[s, t * 128:(t + 1) * 128, :], in_=obuf[b2][:, :]), reads=["obuf%d" % b2], writes=["out.%d.%d" % (s, t)]))
        if s + 1 < NSEQ:
            fence()
        if dbg == "one":
            break

    with nc.allow_non_contiguous_dma(reason="tiny constant loads"):
        st = S.emit(outs)
    return nc, st, dbg_t


_CACHE = {}


def _get_program():
    if "p" not in _CACHE:
        _CACHE["p"] = build_program(dbg=os.environ.get("KDBG", ""))
    return _CACHE["p"]


def kernel(**inputs):
    nc, st, dbg_t = _get_program()
    f = lambda a: np.ascontiguousarray(np.asarray(a, dtype=np.float32))
    x = f(inputs["x"])
    shared = {
        "w_in": f(inputs["w_in"])[0], "b_in": f(inputs["b_in"]).reshape(1, DIN),
        "conv_w": f(inputs["conv_w"])[0], "conv_b": f(inputs["conv_b"]).reshape(1, 1024),
        "a_log": f(inputs["a_log"]).reshape(1, 8), "d_skip": f(inputs["d_skip"]).reshape(1, 8),
        "ssd_norm_g": f(inputs["ssd_norm_g"]).reshape(1, 512), "w_out": f(inputs["w_out"])[0],
        "ln1_g": f(inputs["ln1_g"]).reshape(1, DM), "ln1_b": f(inputs["ln1_b"]).reshape(1, DM),
        "router_group_w": f(inputs["router_group_w"])[0], "router_group_b": f(inputs["router_group_b"]).reshape(1, 4),
        "router_expert_w": f(inputs["router_expert_w"])[0], "router_expert_b": f(inputs["router_expert_b"]).reshape(1, 16),
        "w_gate": f(inputs["w_gate"])[0], "w_up": f(inputs["w_up"])[0], "w_down": f(inputs["w_down"])[0],
        "ln2_g": f(inputs["ln2_g"]).reshape(1, DM), "ln2_b": f(inputs["ln2_b"]).reshape(1, DM),
    }
    ncores = int(os.environ.get("KCORES", NCORES))
    in_maps = []
    for c in range(ncores):
        m = dict(shared)
        m["x"] = np.ascontiguousarray(x[c * NSEQ:(c + 1) * NSEQ])
        in_maps.append(m)
    res = run_bass_kernel_spmd(nc, in_maps, core_ids=list(range(ncores)))
    if os.environ.get("KDBG", ""):
        _CACHE["dbg"] = res.results
    outp = np.concatenate([r["out"] for r in res.results], axis=0)
    return outp.astype(np.float32)
```

```python
import os
import numpy as np
import concourse.bass as bass
import concourse.mybir as mybir
from concourse.bass_utils import run_bass_kernel_spmd

F32 = mybir.dt.float32
BF16 = mybir.dt.bfloat16
U8 = mybir.dt.uint8
AF = mybir.ActivationFunctionType
ALU = mybir.AluOpType
AX = mybir.AxisListType

NCORES = 8
NSEQ = 2
SEQ = 2048
NT = 16
DM = 1024
DIN = 3088
ALPHA = float(2.0 ** 0.25)
LN_EPS = 1e-5
RMS_EPS = 1e-5
ATT_SCALE = 0.125
NEG = -30000.0


class Op:
    __slots__ = ("eng", "fn", "reads", "writes", "deps", "signal", "sigval", "dma", "gi", "nofence")


class Sched:
    COMPUTE = ("pe", "act", "dve", "pool")

    def __init__(self, nc, n_dma_sems=40):
        self.nc = nc
        self.h = {"pe": nc.tensor, "act": nc.scalar, "dve": nc.vector, "pool": nc.gpsimd, "sp": nc.sync}
        self.ops = []
        self.last_w = {}
        self.readers = {}
        self.n_dma_sems = n_dma_sems
        self.live_dma = []
        self.nfence = 0

    def add(self, eng, fn, reads=(), writes=(), dma=False, nofence=False):
        o = Op()
        o.eng = eng; o.fn = fn; o.reads = tuple(reads); o.writes = tuple(writes)
        o.deps = []; o.signal = False; o.sigval = None; o.dma = dma; o.gi = len(self.ops); o.nofence = nofence
        for r in o.reads:
            p = self.last_w.get(r)
            if p is not None:
                self._dep(o, p, True)
            if r.startswith("ps"):
                rd = self.readers.get(r)
                if rd:
                    for q in rd.values():
                        if q.eng != eng:
                            self._dep(o, q, True)
        for w in o.writes:
            p = self.last_w.get(w)
            if p is not None:
                self._dep(o, p, False)
            rd = self.readers.get(w)
            if rd:
                for q in rd.values():
                    self._dep(o, q, False)
        for r in o.reads:
            d = self.readers.setdefault(r, {})
            d[("dma", o.gi) if dma else eng] = o
        for w in o.writes:
            self.last_w[w] = o
            self.readers[w] = {}
        self.ops.append(o)
        if dma and not nofence:
            self.live_dma.append(o)
        return o

    def _dep(self, o, p, raw):
        if p is o:
            return
        if (not p.dma) and (not o.dma) and p.eng == o.eng:
            if o.eng == "pe":
                return
        o.deps.append(p)
        p.signal = True

    def pe(self, fn, reads=(), writes=()): return self.add("pe", fn, reads, writes)
    def act(self, fn, reads=(), writes=()): return self.add("act", fn, reads, writes)
    def dve(self, fn, reads=(), writes=()): return self.add("dve", fn, reads, writes)
    def pool(self, fn, reads=(), writes=()): return self.add("pool", fn, reads, writes)
    def dma(self, q, fn, reads=(), writes=(), nofence=False):
        return self.add(q, fn, reads, writes, dma=True, nofence=nofence)

    def fence(self, scratch):
        n = self.nfence; self.nfence += 1
        a_keys = []
        col = {"pe": None, "act": 0, "dve": 1, "pool": 2}
        for e in ("act", "dve", "pool"):
            k = "fenceA.%d.%s" % (n, e)
            c = col[e]
            if e == "act":
                self.add(e, (lambda eh, c=c: eh.activation(out=scratch[:, c:c + 1], in_=scratch[:, 8:9], func=AF.Copy)), reads=(), writes=(k,))
            else:
                self.add(e, (lambda eh, c=c: eh.memset(scratch[:, c:c + 1], 0.0)), reads=(), writes=(k,))
            a_keys.append(k)
        k = "fenceA.%d.pe" % n
        self.add("pe", (lambda eh: eh.matmul(self.fence_ps[0:1, 0:1], lhsT=self.fence_w[0:1, 0:1], rhs=self.fence_w[0:1, 0:1], start=True, stop=True)),
                 reads=(), writes=(k, "ps7"))
        a_keys.append(k)
        dmas = self.live_dma
        self.live_dma = []
        for e in ("act", "dve", "pool", "pe", "sp"):
            kb = "fenceB.%d.%s" % (n, e)
            if e == "act":
                o = self.add(e, (lambda eh: eh.activation(out=scratch[:, 3:4], in_=scratch[:, 8:9], func=AF.Copy)), reads=a_keys, writes=(kb,))
            elif e == "pe":
                o = self.add(e, (lambda eh: eh.matmul(self.fence_ps[0:1, 1:2], lhsT=self.fence_w[0:1, 0:1], rhs=self.fence_w[0:1, 0:1], start=True, stop=True)),
                             reads=a_keys, writes=(kb, "ps7"))
            elif e == "sp":
                o = self.add(e, (lambda eh: eh.nop()), reads=a_keys, writes=(kb,))
            else:
                c = 4 if e == "dve" else 5
                o = self.add(e, (lambda eh, c=c: eh.memset(scratch[:, c:c + 1], 0.0)), reads=a_keys, writes=(kb,))
            for d in dmas:
                o.deps.append(d)

    def emit(self, final_wait_ops=()):
        nc = self.nc
        esem = {e: nc.alloc_semaphore("s_" + e) for e in self.COMPUTE}
        dsems = [nc.alloc_semaphore("s_dma%d" % i) for i in range(self.n_dma_sems)]
        dtotal = [0] * self.n_dma_sems
        dlast = [None] * self.n_dma_sems
        ecount = {e: 0 for e in self.COMPUTE}
        nd = 0
        nq = {"sp": 0, "pool": 0}
        half = self.n_dma_sems // 2
        for o in self.ops:
            if o.dma:
                qi = nq[o.eng]; nq[o.eng] += 1; nd += 1
                i = (qi % half) + (0 if o.eng == "sp" else half)
                prev = dlast[i]
                if prev is not None:
                    o.deps.append(prev)
                dtotal[i] += 16
                o.sigval = (dsems[i], dtotal[i], 1000 + i)
                dlast[i] = o
            elif o.signal:
                ecount[o.eng] += 1
                o.sigval = (esem[o.eng], ecount[o.eng], o.eng)
        known = {e: {} for e in self.h}
        nwaits = 0
        for o in self.ops:
            eh = self.h[o.eng]
            kn = known[o.eng]
            need = {}
            for p in o.deps:
                s, v, key = p.sigval
                if kn.get(key, 0) >= v:
                    continue
                if key not in need or need[key][1] < v:
                    need[key] = (s, v)
            for key, (s, v) in need.items():
                eh.wait_ge(s, v)
                kn[key] = v
                nwaits += 1
            ins = o.fn(eh)
            if o.dma:
                ins.then_inc(o.sigval[0], 16)
            elif o.signal:
                ins.then_inc(o.sigval[0], 1)
        eh = self.h["sp"]
        for o in final_wait_ops:
            s, v, key = o.sigval
            eh.wait_ge(s, v)
        self.stats = dict(n_ops=len(self.ops), n_waits=nwaits, counts=dict(ecount), n_dma=nd)
        return self.stats


class Arena:
    def __init__(self, nc, name, nbytes):
        self.t = nc.alloc_sbuf_tensor(name, [128, nbytes], U8)
        self.n = nbytes
        self.off = 0

    def reset(self, off=0):
        self.off = off

    def alloc(self, shape, dtype, parts=128):
        esz = 2 if dtype == BF16 else 4
        n = esz
        for s in shape:
            n *= s
        off = (self.off + 31) // 32 * 32
        assert off + n <= self.n, (off, n, self.n)
        self.off = off + n
        flat = self.t[0:parts, off:off + n].bitcast(dtype)
        if len(shape) == 1:
            return flat
        names = " ".join("a%d" % i for i in range(len(shape)))
        kw = {"a%d" % i: shape[i] for i in range(1, len(shape))}
        return flat.rearrange("p (%s) -> p %s" % (names, names), **kw)


def build_program(dbg=False):
    nc = bass.Bass("TRN2", target_bir_lowering=False)
    S = Sched(nc)
    D = {}

    def din(name, shape):
        D[name] = nc.dram_tensor(name, list(shape), F32, kind="ExternalInput").ap()
        return D[name]

    x = din("x", [NSEQ, SEQ, DM])
    w_in = din("w_in", [DM, DIN])
    b_in = din("b_in", [1, DIN])
    conv_w = din("conv_w", [4, 1024])
    conv_b = din("conv_b", [1, 1024])
    a_log = din("a_log", [1, 8])
    d_skip = din("d_skip", [1, 8])
    ssd_g = din("ssd_norm_g", [1, 512])
    w_out = din("w_out", [DM, DM])
    ln1_g = din("ln1_g", [1, DM]); ln1_b = din("ln1_b", [1, DM])
    rg_w = din("router_group_w", [DM, 4]); rg_b = din("router_group_b", [1, 4])
    re_w = din("router_expert_w", [4, DM, 4]); re_b = din("router_expert_b", [1, 16])
    w_gate = din("w_gate", [16, DM, 512]); w_up = din("w_up", [16, DM, 512]); w_down = din("w_down", [16, 512, DM])
    ln2_g = din("ln2_g", [1, DM]); ln2_b = din("ln2_b", [1, DM])
    out = nc.dram_tensor("out", [NSEQ, SEQ, DM], F32, kind="ExternalOutput").ap()
    h_scr = nc.dram_tensor("h_scr", [NSEQ, SEQ, DM], F32).ap()
    dbg_t = {}

    def dbg_out(name, shape):
        dbg_t[name] = nc.dram_tensor(name, list(shape), F32, kind="ExternalOutput").ap()
        return dbg_t[name]

    CONST = Arena(nc, "CONST", 22 * 1024)
    RW = Arena(nc, "RW", 49408)
    RX = Arena(nc, "RX", 32768)
    RACC = Arena(nc, "RACC", 65536)
    MSSD = Arena(nc, "MSSD", 16384)
    TT = Arena(nc, "TT", nc.sbuf_bytes_remaining - 256)

    identB = CONST.alloc([128], BF16); identF = CONST.alloc([128], F32)
    Uf = CONST.alloc([128], F32); triU = CONST.alloc([128], BF16); maskneg = CONST.alloc([128], BF16)
    onesF = CONST.alloc([128], F32)
    ones_row = CONST.alloc([128], BF16, parts=1)
    bz_row = CONST.alloc([512], BF16, parts=1); bv_row = CONST.alloc([512], BF16, parts=1)
    bdtf = CONST.alloc([16], F32)
    bxbc = CONST.alloc([8], F32); bq = CONST.alloc([4], F32); bk = CONST.alloc([4], F32)
    convw = CONST.alloc([8, 4], F32); convb = CONST.alloc([8], F32)
    a_bc = CONST.alloc([8], F32); dskip_bc = CONST.alloc([8], F32)
    gssd_bc = CONST.alloc([512], F32)
    lnG = CONST.alloc([1024], F32); lnB = CONST.alloc([1024], F32)
    rw = CONST.alloc([8, 20], F32); rb_bc = CONST.alloc([20], F32)
    logits = CONST.alloc([NT, 20], F32); comb = CONST.alloc([NT, 16], F32)
    fsc = CONST.alloc([16], F32)
    S.fence_w = CONST.alloc([8], BF16)
    pd = [nc.alloc_psum_tensor("pd%d" % i, [128, 1024], F32) for i in range(4)]
    def bank(i):
        return pd[i // 2][:, (i % 2) * 512:(i % 2) * 512 + 512]
    def bankb(i):
        return bank(i).bitcast(BF16)
    PSK = ["ps%d" % i for i in range(8)]
    S.fence_ps = nc.alloc_sbuf_tensor("fence_dummy", [1, 8], F32)
    S.fence_ps = bank(7)[:, 504:512]

    def fence():
        S.fence(fsc)

    S.pool(lambda e: e.memset(fsc[:, :], 0.0), writes=["fsc"])
    S.pool(lambda e: e.memset(S.fence_w[:, :], 0.0), writes=["fence_w"])
    S.pool(lambda e: e.memset(identB[:, :], 1.0), writes=["identB"])
    S.pool(lambda e: e.affine_select(out=identB[:, :], in_=identB[:, :], pattern=[[-1, 128]], compare_op=ALU.is_equal, fill=0.0, base=0, channel_multiplier=1), reads=["identB"], writes=["identB"])
    S.pool(lambda e: e.memset(identF[:, :], 1.0), writes=["identF"])
    S.pool(lambda e: e.affine_select(out=identF[:, :], in_=identF[:, :], pattern=[[-1, 128]], compare_op=ALU.is_equal, fill=0.0, base=0, channel_multiplier=1), reads=["identF"], writes=["identF"])
    S.pool(lambda e: e.memset(Uf[:, :], 1.0), writes=["Uf"])
    S.pool(lambda e: e.affine_select(out=Uf[:, :], in_=Uf[:, :], pattern=[[1, 128]], compare_op=ALU.is_ge, fill=0.0, base=0, channel_multiplier=-1), reads=["Uf"], writes=["Uf"])
    S.pool(lambda e: e.memset(triU[:, :], 1.0), writes=["triU"])
    S.pool(lambda e: e.affine_select(out=triU[:, :], in_=triU[:, :], pattern=[[1, 128]], compare_op=ALU.is_ge, fill=0.0, base=0, channel_multiplier=-1), reads=["triU"], writes=["triU"])
    S.pool(lambda e: e.memset(maskneg[:, :], NEG), writes=["maskneg"])
    S.pool(lambda e: e.affine_select(out=maskneg[:, :], in_=maskneg[:, :], pattern=[[-1, 128]], compare_op=ALU.is_gt, fill=0.0, base=0, channel_multiplier=1), reads=["maskneg"], writes=["maskneg"])
    S.pool(lambda e: e.memset(onesF[:, :], 1.0), writes=["onesF"])
    S.pool(lambda e: e.memset(ones_row[:, :], 1.0), writes=["ones_row"])
    S.dma("pool", lambda e: e.dma_start(out=bz_row[:, :], in_=b_in[0:1, 0:512]), writes=["bz_row"])
    S.dma("pool", lambda e: e.dma_start(out=bv_row[:, :], in_=b_in[0:1, 2568:3080]), writes=["bv_row"])
    S.dma("sp", lambda e: e.dma_start(out=bdtf[:, 0:8], in_=b_in[0:1, 1536:1544].to_broadcast([128, 8])), writes=["bdtf0"])
    S.dma("sp", lambda e: e.dma_start(out=bdtf[:, 8:16], in_=b_in[0:1, 3080:3088].to_broadcast([128, 8])), writes=["bdtf1"])
    S.dma("sp", lambda e: e.dma_start(out=bxbc[:, :], in_=b_in[0, 512:1536].rearrange("(c p) -> p c", p=128)), writes=["bxbc"])
    S.dma("sp", lambda e: e.dma_start(out=bq[:, :], in_=b_in[0, 1544:2056].rearrange("(c p) -> p c", p=128)), writes=["bq"])
    S.dma("sp", lambda e: e.dma_start(out=bk[:, :], in_=b_in[0, 2056:2568].rearrange("(c p) -> p c", p=128)), writes=["bk"])
    for k in range(4):
        S.dma("sp", lambda e, k=k: e.dma_start(out=convw[:, :, k], in_=conv_w[k, :].rearrange("(c p) -> p c", p=128)), writes=["convw%d" % k])
    CONVW = ["convw%d" % k for k in range(4)]
    S.dma("sp", lambda e: e.dma_start(out=convb[:, :], in_=conv_b[0, :].rearrange("(c p) -> p c", p=128)), writes=["convb"])
    S.dma("sp", lambda e: e.dma_start(out=a_bc[:, :], in_=a_log[0:1, :].to_broadcast([128, 8])), writes=["a_bc"])
    S.act(lambda e: e.activation(out=a_bc[:, :], in_=a_bc[:, :], func=AF.Exp), reads=["a_bc"], writes=["a_bc"])
    S.dve(lambda e: e.tensor_scalar(out=a_bc[:, :], in0=a_bc[:, :], scalar1=-1.0, scalar2=None, op0=ALU.mult), reads=["a_bc"], writes=["a_bc"])
    S.dma("sp", lambda e: e.dma_start(out=dskip_bc[:, :], in_=d_skip[0:1, :].to_broadcast([128, 8])), writes=["dskip_bc"])
    S.dma("sp", lambda e: e.dma_start(out=gssd_bc[:, :], in_=ssd_g[0:1, :].to_broadcast([128, 512])), writes=["gssd_bc"])
    S.dma("sp", lambda e: e.dma_start(out=rw[:, :, 0:4], in_=rg_w.rearrange("(k p) j -> p k j", p=128)), writes=["rw0"])
    for g in range(4):
        S.dma("sp", lambda e, g=g: e.dma_start(out=rw[:, :, 4 + 4 * g:8 + 4 * g], in_=re_w[g].rearrange("(k p) j -> p k j", p=128)), writes=["rw%d" % (g + 1)])
    RWK = ["rw%d" % i for i in range(5)]
    S.dma("sp", lambda e: e.dma_start(out=rb_bc[:, 0:4], in_=rg_b[0:1, :].to_broadcast([128, 4])), writes=["rb0"])
    S.dma("sp", lambda e: e.dma_start(out=rb_bc[:, 4:20], in_=re_b[0:1, :].to_broadcast([128, 16])), writes=["rb1"])

    outs = []

    for s in range(NSEQ):
        RW.reset(); RX.reset(); RACC.reset(); MSSD.reset(); TT.reset()
        wA = RW.alloc([8, 1544], BF16)
        xb = [RW.alloc([4, 1024], BF16) for _ in range(2)]
        xT = RX.alloc([8, SEQ], BF16)
        sz = RACC.alloc([NT, 512], BF16)
        xsB = RACC.alloc([NT, 768], BF16)
        BT = RACC.alloc([2, SEQ], BF16)
        CT = RACC.alloc([2, SEQ], BF16)
        xsT = MSSD.alloc([4, SEQ], BF16)
        dt_t = TT.alloc([NT, 8], F32)
        tt_mark = TT.off
        pre = [TT.alloc([SEQ + 3], BF16) for _ in range(2)]
        cacc = [TT.alloc([SEQ], F32) for _ in range(2)]
        dt_raw = TT.alloc([NT, 8], F32)

        for half in range(2):
            S.dma("pool", lambda e, half=half: e.dma_start(out=wA[:, 4 * half:4 * half + 4, :], in_=w_in.rearrange("(k p) c -> p k c", p=128)[:, 4 * half:4 * half + 4, 0:1544]),
                  writes=["wA"])
        for b in range(2):
            S.pool(lambda e, b=b: e.memset(pre[b][:, 0:3], 0.0), writes=["pre%d" % b])
        ev = 0
        for blk in range(4):
            S.dma("pool", lambda e, blk=blk, s=s: e.dma_start(out=xb[blk % 2][:, :, :], in_=x[s, blk * 512:(blk + 1) * 512, :].rearrange("(t p) d -> p t d", p=128)),
                  writes=["xb%d" % (blk % 2)])
            for k in range(8):
                pb = (blk * 8 + k) % 2
                for t in range(4):
                    S.pe(lambda e, pb=pb, t=t, k=k, blk=blk: e.transpose(bankb(pb)[:, t * 128:(t + 1) * 128], xb[blk % 2][:, t, k * 128:(k + 1) * 128], identB[:, :]),
                         reads=["xb%d" % (blk % 2), "identB"], writes=[PSK[pb]])
                if ev % 2 == 0:
                    S.act(lambda e, pb=pb, k=k, blk=blk: e.copy(xT[:, k, blk * 512:(blk + 1) * 512], bankb(pb)[:, 0:512]), reads=[PSK[pb]], writes=["xT.%d.%d" % (k, blk)])
                else:
                    S.dve(lambda e, pb=pb, k=k, blk=blk: e.tensor_copy(xT[:, k, blk * 512:(blk + 1) * 512], bankb(pb)[:, 0:512]), reads=[PSK[pb]], writes=["xT.%d.%d" % (k, blk)])
                ev += 1
            for tt in range(4):
                t = blk * 4 + tt
                pz = 2 + (t % 2)
                xk = ["xT.%d.%d" % (k, blk) for k in range(8)]
                for k in range(8):
                    S.pe(lambda e, pz=pz, t=t, k=k: e.matmul(bank(pz)[:, :], lhsT=xT[:, k, t * 128:(t + 1) * 128], rhs=wA[:, k, 0:512], start=(k == 0), stop=False),
                         reads=[xk[k], "wA"], writes=[PSK[pz]])
                S.pe(lambda e, pz=pz: e.matmul(bank(pz)[:, :], lhsT=ones_row[0:1, :], rhs=bz_row[0:1, :], start=False, stop=True),
                     reads=["ones_row", "bz_row"], writes=[PSK[pz]])
                S.act(lambda e, pz=pz, t=t: e.activation(out=sz[:, t, :], in_=bank(pz)[:, :], func=AF.Silu), reads=[PSK[pz]], writes=["sz.%d" % t])
                for k in range(8):
                    S.pe(lambda e, t=t, k=k: e.matmul(bank(4)[:, t * 8:(t + 1) * 8], lhsT=xT[:, k, t * 128:(t + 1) * 128], rhs=wA[:, k, 1536:1544], start=(k == 0), stop=(k == 7)),
                         reads=[xk[k], "wA"], writes=[PSK[4]])
        S.dve(lambda e: e.tensor_tensor(out=dt_raw[:, :, :], in0=bank(4)[:, 0:128].rearrange("p (t h) -> p t h", h=8), in1=bdtf[:, 0:8].unsqueeze(1).to_broadcast([128, NT, 8]), op=ALU.add),
              reads=[PSK[4], "bdtf0"], writes=["dt_raw"])
        XT_ALL = lambda blk: ["xT.%d.%d" % (k, blk) for k in range(8)]
        for c in range(8):
            pb_ = c % 2
            for blk in range(4):
                pc = 5 + (c * 4 + blk) % 2
                for k in range(8):
                    S.pe(lambda e, pc=pc, c=c, k=k, blk=blk: e.matmul(bank(pc)[:, :], lhsT=wA[:, k, 512 + c * 128:512 + (c + 1) * 128], rhs=xT[:, k, blk * 512:(blk + 1) * 512], start=(k == 0), stop=(k == 7)),
                         reads=["xT.%d.%d" % (k, blk), "wA"], writes=[PSK[pc]])
                S.act(lambda e, pc=pc, c=c, blk=blk, pb_=pb_: e.activation(out=pre[pb_][:, 3 + blk * 512:3 + (blk + 1) * 512], in_=bank(pc)[:, :], func=AF.Identity, bias=bxbc[:, c:c + 1], scale=1.0),
                      reads=[PSK[pc], "bxbc"], writes=["pre%d" % pb_])
            eng = S.dve
            ca = cacc[pb_]; pr = pre[pb_]
            eng(lambda e, ca=ca, pr=pr, c=c: e.tensor_scalar(out=ca[:, :], in0=pr[:, 3:SEQ + 3], scalar1=convw[:, c, 3:4], scalar2=None, op0=ALU.mult),
                reads=["pre%d" % pb_] + CONVW, writes=["cacc%d" % pb_])
            for kk in (2, 1, 0):
                eng(lambda e, ca=ca, pr=pr, c=c, kk=kk: e.scalar_tensor_tensor(out=ca[:, :], in0=pr[:, kk:SEQ + kk], scalar=convw[:, c, kk:kk + 1], in1=ca[:, :], op0=ALU.mult, op1=ALU.add),
                    reads=["pre%d" % pb_, "cacc%d" % pb_] + CONVW, writes=["cacc%d" % pb_])
            if c < 4:
                dst = xsT[:, c, :]; dk = "xsT.%d" % c
            elif c < 6:
                dst = BT[:, c - 4, :]; dk = "BT.%d" % (c - 4)
            else:
                dst = CT[:, c - 6, :]; dk = "CT.%d" % (c - 6)
            S.act(lambda e, ca=ca, dst=dst, c=c: e.activation(out=dst, in_=ca[:, :], func=AF.Silu, bias=convb[:, c:c + 1], scale=1.0),
                  reads=["cacc%d" % pb_, "convb"], writes=[dk])
        for t in range(NT):
            pb = t % 2
            for c in range(6):
                src = xsT[:, c, t * 128:(t + 1) * 128] if c < 4 else BT[:, c - 4, t * 128:(t + 1) * 128]
                sk = "xsT.%d" % c if c < 4 else "BT.%d" % (c - 4)
                S.pe(lambda e, pb=pb, c=c, src=src: e.transpose(bankb(pb)[:, c * 128:(c + 1) * 128], src, identB[:, :]), reads=[sk, "identB"], writes=[PSK[pb]])
            if t % 2 == 0:
                S.dve(lambda e, pb=pb, t=t: e.tensor_copy(xsB[:, t, :], bankb(pb)[:, 0:768]), reads=[PSK[pb]], writes=["xsB.%d" % t])
            else:
                S.act(lambda e, pb=pb, t=t: e.copy(xsB[:, t, :], bankb(pb)[:, 0:768]), reads=[PSK[pb]], writes=["xsB.%d" % t])
        S.act(lambda e: e.activation(out=dt_t[:, :, :], in_=dt_raw[:, :, :], func=AF.Exp), reads=["dt_raw"], writes=["dt_t"])
        S.act(lambda e: e.activation(out=dt_t[:, :, :], in_=dt_t[:, :, :], func=AF.Ln, bias=1.0, scale=1.0), reads=["dt_t"], writes=["dt_t"])

        fence()
        MSSD.reset(); TT.reset(tt_mark); RW.reset()
        m_ssd = MSSD.alloc([NT, 512], BF16)
        da = TT.alloc([NT, 8], F32); acum = TT.alloc([NT, 8], F32); nacum = TT.alloc([NT, 8], F32)
        alast = TT.alloc([NT, 8], F32); dte = TT.alloc([NT, 8], F32); ea = TT.alloc([NT, 8], F32)
        cdec = TT.alloc([NT, 8], F32); dtdte = TT.alloc([NT, 8], F32)
        stT = TT.alloc([8, 64], F32); stTb = TT.alloc([8, 64], BF16)
        LT = [TT.alloc([128], BF16) for _ in range(4)]
        MT = [TT.alloc([128], BF16) for _ in range(4)]
        xdt = [RW.alloc([8, 64], BF16) for _ in range(2)]
        xdtd = [RW.alloc([8, 64], BF16) for _ in range(2)]
        t1 = [RW.alloc([8, 64], F32) for _ in range(2)]
        t2 = [RW.alloc([8, 64], F32) for _ in range(2)]
        yg = [RW.alloc([512], F32) for _ in range(2)]
        junk = TT.alloc([256], F32)
        ss = [TT.alloc([2], F32) for _ in range(2)]
        rstd = [TT.alloc([2], F32) for _ in range(2)]

        S.dve(lambda e: e.tensor_tensor(out=da[:, :, :], in0=dt_t[:, :, :], in1=a_bc[:, :].unsqueeze(1).to_broadcast([128, NT, 8]), op=ALU.mult), reads=["dt_t", "a_bc"], writes=["da"])
        daf = da.rearrange("p t h -> p (t h)")
        S.pe(lambda e: e.matmul(bank(5)[:, 0:128], lhsT=Uf[:, :], rhs=daf, start=True, stop=True), reads=["Uf", "da"], writes=[PSK[5]])
        S.pe(lambda e: e.matmul(bank(6)[:, 0:128], lhsT=onesF[:, :], rhs=daf, start=True, stop=True), reads=["onesF", "da"], writes=[PSK[6]])
        S.dve(lambda e: e.tensor_copy(acum.rearrange("p t h -> p (t h)"), bank(5)[:, 0:128]), reads=[PSK[5]], writes=["acum"])
        S.dve(lambda e: e.tensor_scalar(out=nacum.rearrange("p t h -> p (t h)"), in0=bank(5)[:, 0:128], scalar1=-1.0, scalar2=None, op0=ALU.mult), reads=[PSK[5]], writes=["nacum"])
        S.dve(lambda e: e.tensor_copy(alast.rearrange("p t h -> p (t h)"), bank(6)[:, 0:128]), reads=[PSK[6]], writes=["alast"])
        S.dve(lambda e: e.tensor_tensor(out=dte[:, :, :], in0=alast[:, :, :], in1=acum[:, :, :], op=ALU.subtract), reads=["alast", "acum"], writes=["dte"])
        S.act(lambda e: e.activation(out=dte[:, :, :], in_=dte[:, :, :], func=AF.Exp), reads=["dte"], writes=["dte"])
        S.act(lambda e: e.activation(out=ea[:, :, :], in_=acum[:, :, :], func=AF.Exp), reads=["acum"], writes=["ea"])
        S.act(lambda e: e.activation(out=cdec[:, :, :], in_=alast[:, :, :], func=AF.Exp), reads=["alast"], writes=["cdec"])
        S.dve(lambda e: e.tensor_tensor(out=dtdte[:, :, :], in0=dt_t[:, :, :], in1=dte[:, :, :], op=ALU.mult), reads=["dt_t", "dte"], writes=["dtdte"])
        S.pool(lambda e: e.memset(stT[:, :, :], 0.0), writes=["stT"])
        S.pool(lambda e: e.memset(stTb[:, :, :], 0.0), writes=["stTb"])

        for c in range(NT):
            cs = slice(c * 128, (c + 1) * 128)
            b2 = c % 2
            xs_c = xsB[:, c, 0:512].rearrange("p (h d) -> p h d", h=8)
            S.pool(lambda e, c=c, b2=b2, xs_c=xs_c: e.tensor_tensor(out=xdt[b2][:, :, :], in0=xs_c, in1=dt_t[:, c, :].unsqueeze(2).to_broadcast([128, 8, 64]), op=ALU.mult),
                   reads=["xsB.%d" % c, "dt_t"], writes=["xdt%d" % b2])
            S.pool(lambda e, c=c, b2=b2, xs_c=xs_c: e.tensor_tensor(out=xdtd[b2][:, :, :], in0=xs_c, in1=dtdte[:, c, :].unsqueeze(2).to_broadcast([128, 8, 64]), op=ALU.mult),
                   reads=["xsB.%d" % c, "dtdte"], writes=["xdtd%d" % b2])
            S.pool(lambda e, c=c, b2=b2, xs_c=xs_c: e.tensor_tensor(out=t2[b2][:, :, :], in0=xs_c, in1=dskip_bc[:, :].unsqueeze(2).to_broadcast([128, 8, 64]), op=ALU.mult),
                   reads=["xsB.%d" % c, "dskip_bc"], writes=["t2%d" % b2])
            for g in range(2):
                S.pe(lambda e, g=g, cs=cs: e.matmul(bank(4)[:, g * 128:(g + 1) * 128], lhsT=BT[:, g, cs], rhs=CT[:, g, cs], start=True, stop=True),
                     reads=["BT.%d" % g, "CT.%d" % g], writes=[PSK[4]])
            for hh in range(2):
                pl = (c * 2 + hh) % 4
                for h4 in range(4):
                    h = hh * 4 + h4
                    S.pe(lambda e, pl=pl, h4=h4, c=c, h=h: e.matmul(bank(pl)[:, h4 * 128:(h4 + 1) * 128], lhsT=da[:, c, h:h + 1].to_broadcast([128, 128]), rhs=Uf[:, :], start=True, stop=False),
                         reads=["da", "Uf"], writes=[PSK[pl]])
                    S.pe(lambda e, pl=pl, h4=h4: e.matmul(bank(pl)[:, h4 * 128:(h4 + 1) * 128], lhsT=identB[:, :], rhs=maskneg[:, :], start=False, stop=True),
                         reads=["identB", "maskneg"], writes=[PSK[pl]])
                for h4 in range(4):
                    h = hh * 4 + h4
                    S.act(lambda e, pl=pl, h4=h4, c=c, h=h: e.activation(out=LT[h4][:, :], in_=bank(pl)[:, h4 * 128:(h4 + 1) * 128], func=AF.Exp, bias=nacum[:, c, h:h + 1], scale=1.0),
                          reads=[PSK[pl], "nacum"], writes=["LT%d" % h4])
                    g = h // 4
                    S.dve(lambda e, h4=h4, g=g: e.tensor_tensor(out=MT[h4][:, :], in0=bank(4)[:, g * 128:(g + 1) * 128], in1=LT[h4][:, :], op=ALU.mult),
                          reads=[PSK[4], "LT%d" % h4], writes=["MT%d" % h4])
                    S.pe(lambda e, h4=h4, h=h, b2=b2: e.matmul(bank(5)[:, h * 64:(h + 1) * 64], lhsT=MT[h4][:, :], rhs=xdt[b2][:, h, :], start=True, stop=True),
                         reads=["MT%d" % h4, "xdt%d" % b2], writes=[PSK[5]])
            if c > 0:
                for g in range(2):
                    S.pe(lambda e, g=g, cs=cs: e.matmul(bank(6)[:, g * 256:(g + 1) * 256], lhsT=CT[:, g, cs], rhs=stTb[:, 4 * g:4 * g + 4, :].rearrange("p h d -> p (h d)"), start=True, stop=True),
                         reads=["CT.%d" % g, "stTb"], writes=[PSK[6]])
                S.dve(lambda e, c=c, b2=b2: e.tensor_tensor(out=t1[b2][:, :, :], in0=bank(6)[:, :].rearrange("p (h d) -> p h d", h=8), in1=ea[:, c, :].unsqueeze(2).to_broadcast([128, 8, 64]), op=ALU.mult),
                      reads=[PSK[6], "ea"], writes=["t1%d" % b2])
                S.dve(lambda e, b2=b2: e.tensor_tensor(out=t1[b2][:, :, :], in0=bank(5)[:, :].rearrange("p (h d) -> p h d", h=8), in1=t1[b2][:, :, :], op=ALU.add),
                      reads=[PSK[5], "t1%d" % b2], writes=["t1%d" % b2])
                S.dve(lambda e, b2=b2: e.tensor_tensor(out=t1[b2][:, :, :], in0=t1[b2][:, :, :], in1=t2[b2][:, :, :], op=ALU.add),
                      reads=["t1%d" % b2, "t2%d" % b2], writes=["t1%d" % b2])
            else:
                S.dve(lambda e, b2=b2: e.tensor_tensor(out=t1[b2][:, :, :], in0=bank(5)[:, :].rearrange("p (h d) -> p h d", h=8), in1=t2[b2][:, :, :], op=ALU.add),
                      reads=[PSK[5], "t2%d" % b2], writes=["t1%d" % b2])
            S.dve(lambda e, c=c, b2=b2: e.tensor_tensor(out=yg[b2][:, :], in0=t1[b2].rearrange("p h d -> p (h d)"), in1=sz[:, c, :], op=ALU.mult),
                  reads=["t1%d" % b2, "sz.%d" % c], writes=["yg%d" % b2])
            for g in range(2):
                S.act(lambda e, g=g, b2=b2: e.activation(out=junk[:, :], in_=yg[b2][:, g * 256:(g + 1) * 256], func=AF.Square, accum_out=ss[b2][:, g:g + 1]),
                      reads=["yg%d" % b2], writes=["junk", "ss%d.%d" % (b2, g)])
            S.act(lambda e, b2=b2: e.activation(out=rstd[b2][:, :], in_=ss[b2][:, :], func=AF.Ln, bias=RMS_EPS, scale=1.0 / 256.0),
                  reads=["ss%d.0" % b2, "ss%d.1" % b2], writes=["rstd%d" % b2])
            S.act(lambda e, b2=b2: e.activation(out=rstd[b2][:, :], in_=rstd[b2][:, :], func=AF.Exp, scale=-0.5), reads=["rstd%d" % b2], writes=["rstd%d" % b2])
            for g in range(2):
                S.dve(lambda e, g=g, b2=b2, c=c: e.scalar_tensor_tensor(out=m_ssd[:, c, g * 256:(g + 1) * 256], in0=yg[b2][:, g * 256:(g + 1) * 256], scalar=rstd[b2][:, g:g + 1], in1=gssd_bc[:, g * 256:(g + 1) * 256], op0=ALU.mult, op1=ALU.mult),
                      reads=["yg%d" % b2, "rstd%d" % b2, "gssd_bc"], writes=["m_ssd.%d" % c])
            if c < NT - 1:
                for g in range(2):
                    S.pe(lambda e, g=g, c=c, b2=b2: e.matmul(bank(7)[:, g * 256:(g + 1) * 256], lhsT=xsB[:, c, 512 + g * 128:512 + (g + 1) * 128], rhs=xdtd[b2][:, 4 * g:4 * g + 4, :].rearrange("p h d -> p (h d)"), start=True, stop=True),
                         reads=["xsB.%d" % c, "xdtd%d" % b2], writes=[PSK[7]])
                S.dve(lambda e, c=c: e.tensor_tensor(out=stT[:, :, :], in0=stT[:, :, :], in1=cdec[:, c, :].unsqueeze(2).to_broadcast([128, 8, 64]), op=ALU.mult),
                      reads=["stT", "cdec"], writes=["stT"])
                S.dve(lambda e: e.tensor_tensor(out=stT[:, :, :], in0=bank(7)[:, 0:512].rearrange("p (h d) -> p h d", h=8), in1=stT[:, :, :], op=ALU.add),
                      reads=[PSK[7], "stT"], writes=["stT"])
                S.act(lambda e: e.copy(stTb[:, :, :], stT[:, :, :]), reads=["stT"], writes=["stTb"])

        if dbg == 'ssd' and s == 0:
            RX.reset()
            dm = dbg_out("dbg_mssd", [128, NT * 512])
            cvt = RX.alloc([NT * 512], F32) if dbg == 'ssd' else None
            S.dve(lambda e: e.tensor_copy(cvt[:, :], m_ssd.rearrange("p t d -> p (t d)")), reads=["m_ssd.%d" % c for c in range(NT)], writes=["cvt"])
            outs.append(S.dma("sp", lambda e: e.dma_start(out=dm[:, :], in_=cvt[:, :]), reads=["cvt"]))
        if dbg == "ssd":
            break

        fence()
        RW.reset(); RACC.reset(); TT.reset()
        wB = RW.alloc([8, 1544], BF16)
        wo = RW.alloc([8, 1024], BF16)
        ah = RW.alloc([1024], F32)
        hTf = RW.alloc([8, 128], F32)
        qT = RACC.alloc([4, SEQ], BF16)
        kT = RACC.alloc([4, SEQ], BF16)
        v_aug = RACC.alloc([NT, 8, 65], BF16)
        xres = [RACC.alloc([1024], F32) for _ in range(2)]
        hpre = [RACC.alloc([1024], F32), TT.alloc([1024], F32)]
        f_raw = TT.alloc([NT, 8], F32); Gc = TT.alloc([NT, 8], F32); tot = TT.alloc([NT, 8], F32)
        Pp = TT.alloc([NT, 8], F32); Gf = TT.alloc([NT, 8], F32); Gend = TT.alloc([NT, 8], F32)
        biasT = TT.alloc([8, NT, NT], F32)
        PT = [TT.alloc([8, 128], BF16) for _ in range(2)]
        yatt = [TT.alloc([8, 64], BF16) for _ in range(2)]
        rden = [TT.alloc([8], F32) for _ in range(2)]
        mT = [TT.alloc([8, 128], BF16)] * 2
        bst = [TT.alloc([2, 6], F32) for _ in range(2)]
        mv = [TT.alloc([2], F32) for _ in range(2)]
        rs1 = [TT.alloc([1], F32) for _ in range(2)]

        for half in range(2):
            S.dma("pool", lambda e, half=half: e.dma_start(out=wB[:, 4 * half:4 * half + 4, :], in_=w_in.rearrange("(k p) c -> p k c", p=128)[:, 4 * half:4 * half + 4, 1544:3088]),
                  writes=["wB"])
            S.dma("pool", lambda e, half=half: e.dma_start(out=wo[:, 4 * half:4 * half + 4, :], in_=w_out.rearrange("(k p) c -> p k c", p=128)[:, 4 * half:4 * half + 4, :]),
                  writes=["wo"])
        S.dma("sp", lambda e: e.dma_start(out=lnG[:, :], in_=ln1_g[0:1, :].to_broadcast([128, 1024])), writes=["lnG"])
        S.dma("sp", lambda e: e.dma_start(out=lnB[:, :], in_=ln1_b[0:1, :].to_broadcast([128, 1024])), writes=["lnB"])
        S.pool(lambda e: e.memset(v_aug[:, :, :, 64:65], 1.0), writes=["v_ones"])
        evq = 0
        for qk in range(2):
            dstT = qT if qk == 0 else kT
            bcol = bq if qk == 0 else bk
            nm = "qT" if qk == 0 else "kT"
            for p in range(4):
                for blk in range(4):
                    pq = evq % 4
                    for k in range(8):
                        S.pe(lambda e, pq=pq, k=k, p=p, blk=blk, qk=qk: e.matmul(bank(pq)[:, :], lhsT=wB[:, k, qk * 512 + p * 128:qk * 512 + (p + 1) * 128], rhs=xT[:, k, blk * 512:(blk + 1) * 512], start=(k == 0), stop=(k == 7)),
                             reads=["xT.%d.%d" % (k, blk), "wB"], writes=[PSK[pq]])
                    if evq % 2 == 0:
                        S.act(lambda e, pq=pq, p=p, blk=blk, dstT=dstT, bcol=bcol: e.activation(out=dstT[:, p, blk * 512:(blk + 1) * 512], in_=bank(pq)[:, :], func=AF.Identity, bias=bcol[:, p:p + 1], scale=1.0),
                              reads=[PSK[pq], "bq", "bk"], writes=["%s.%d.%d" % (nm, p, blk)])
                    else:
                        S.dve(lambda e, pq=pq, p=p, blk=blk, dstT=dstT, bcol=bcol: e.tensor_scalar(out=dstT[:, p, blk * 512:(blk + 1) * 512], in0=bank(pq)[:, :], scalar1=bcol[:, p:p + 1], scalar2=None, op0=ALU.add),
                              reads=[PSK[pq], "bq", "bk"], writes=["%s.%d.%d" % (nm, p, blk)])
                    evq += 1
        for t in range(NT):
            pv = 4 + (t % 2)
            blk = t // 4
            for k in range(8):
                S.pe(lambda e, pv=pv, t=t, k=k: e.matmul(bank(pv)[:, :], lhsT=xT[:, k, t * 128:(t + 1) * 128], rhs=wB[:, k, 1024:1536], start=(k == 0), stop=False),
                     reads=["xT.%d.%d" % (k, blk), "wB"], writes=[PSK[pv]])
            S.pe(lambda e, pv=pv: e.matmul(bank(pv)[:, :], lhsT=ones_row[0:1, :], rhs=bv_row[0:1, :], start=False, stop=True),
                 reads=["ones_row", "bv_row"], writes=[PSK[pv]])
            if t % 2 == 0:
                S.act(lambda e, pv=pv, t=t: e.copy(v_aug[:, t, :, 0:64], bank(pv)[:, :].rearrange("p (h d) -> p h d", h=8)), reads=[PSK[pv]], writes=["v.%d" % t])
            else:
                S.dve(lambda e, pv=pv, t=t: e.tensor_copy(v_aug[:, t, :, 0:64], bank(pv)[:, :].rearrange("p (h d) -> p h d", h=8)), reads=[PSK[pv]], writes=["v.%d" % t])
            for k in range(8):
                S.pe(lambda e, t=t, k=k: e.matmul(bank(6)[:, t * 8:(t + 1) * 8], lhsT=xT[:, k, t * 128:(t + 1) * 128], rhs=wB[:, k, 1536:1544], start=(k == 0), stop=(k == 7)),
                     reads=["xT.%d.%d" % (k, blk), "wB"], writes=[PSK[6]])
        S.dve(lambda e: e.tensor_tensor(out=f_raw[:, :, :], in0=bank(6)[:, 0:128].rearrange("p (t h) -> p t h", h=8), in1=bdtf[:, 8:16].unsqueeze(1).to_broadcast([128, NT, 8]), op=ALU.add),
              reads=[PSK[6], "bdtf1"], writes=["f_raw"])
        S.act(lambda e: e.activation(out=f_raw[:, :, :], in_=f_raw[:, :, :], func=AF.Exp, scale=-1.0), reads=["f_raw"], writes=["f_raw"])
        S.act(lambda e: e.activation(out=f_raw[:, :, :], in_=f_raw[:, :, :], func=AF.Ln, bias=1.0, scale=1.0), reads=["f_raw"], writes=["f_raw"])
        frf = f_raw.rearrange("p t h -> p (t h)")
        S.pe(lambda e: e.matmul(bank(7)[:, 0:128], lhsT=Uf[:, :], rhs=frf, start=True, stop=True), reads=["Uf", "f_raw"], writes=[PSK[7]])
        S.pe(lambda e: e.matmul(bank(7)[:, 128:256], lhsT=onesF[:, :], rhs=frf, start=True, stop=True), reads=["onesF", "f_raw"], writes=[PSK[7]])
        S.dve(lambda e: e.tensor_copy(Gc.rearrange("p t h -> p (t h)"), bank(7)[:, 0:128]), reads=[PSK[7]], writes=["Gc"])
        S.dve(lambda e: e.tensor_copy(tot.rearrange("p t h -> p (t h)"), bank(7)[:, 128:256]), reads=[PSK[7]], writes=["tot"])
        S.pool(lambda e: e.memset(Pp[:, 0, :], 0.0), writes=["Pp"])
        for t in range(1, NT):
            S.dve(lambda e, t=t: e.tensor_tensor(out=Pp[:, t, :], in0=Pp[:, t - 1, :], in1=tot[:, t - 1, :], op=ALU.add), reads=["Pp", "tot"], writes=["Pp"])
        S.dve(lambda e: e.tensor_tensor(out=Gf[:, :, :], in0=Gc[:, :, :], in1=Pp[:, :, :], op=ALU.add), reads=["Gc", "Pp"], writes=["Gf"])
        S.dve(lambda e: e.tensor_tensor(out=Gend[:, :, :], in0=tot[:, :, :], in1=Pp[:, :, :], op=ALU.add), reads=["tot", "Pp"], writes=["Gend"])
        for h in range(8):
            S.dve(lambda e, h=h: e.tensor_tensor(out=biasT[:, h, :, :], in0=Gf[:, :, h].unsqueeze(1).to_broadcast([128, NT, NT]), in1=Gend[:, :, h].unsqueeze(2).to_broadcast([128, NT, NT]), op=ALU.subtract),
                  reads=["Gf", "Gend"], writes=["biasT"])

        fence()
        RX.reset()
        hT = RX.alloc([8, SEQ], BF16)
        NTI = int(os.environ.get("KATT_TILES", NT))
        units = []
        for i in range(NTI):
            for p in range(4):
                for j0 in range(0, i + 1, 4):
                    units.append((i, p, list(range(j0, min(j0 + 4, i + 1)))))

        def emit_S(u, gi):
            i, p, js = u
            pb0 = 2 * (gi % 2)
            for jj, j in enumerate(js):
                for hh in range(2):
                    r0 = hh * 64
                    S.pe(lambda e, bk=pb0 + hh, jj=jj, j=j, p=p, r0=r0, i=i: e.matmul(bank(bk)[:, jj * 128:(jj + 1) * 128], lhsT=kT[r0:r0 + 64, p, j * 128:(j + 1) * 128], rhs=qT[r0:r0 + 64, p, i * 128:(i + 1) * 128], start=True, stop=True),
                         reads=["kT.%d.%d" % (p, j // 4), "qT.%d.%d" % (p, i // 4)], writes=[PSK[pb0 + hh]])

        def emit_E(u, gi):
            i, p, js = u
            pb0 = 2 * (gi % 2); pb = gi % 2
            for jj, j in enumerate(js):
                for hh in range(2):
                    h = 2 * p + hh
                    c8 = jj * 2 + hh
                    S.act(lambda e, bk=pb0 + hh, pb=pb, c8=c8, jj=jj, j=j, h=h, i=i: e.activation(out=PT[pb][:, c8, :], in_=bank(bk)[:, jj * 128:(jj + 1) * 128], func=AF.Exp, bias=biasT[:, h, i, j:j + 1], scale=ATT_SCALE),
                          reads=[PSK[pb0 + hh], "biasT"], writes=["PT%d.%d" % (pb, c8)])
                    if j == i:
                        S.dve(lambda e, pb=pb, c8=c8: e.tensor_tensor(out=PT[pb][:, c8, :], in0=PT[pb][:, c8, :], in1=triU[:, :], op=ALU.mult),
                              reads=["PT%d.%d" % (pb, c8), "triU"], writes=["PT%d.%d" % (pb, c8)])

        def emit_V(u, gi):
            i, p, js = u
            pb = gi % 2
            for jj, j in enumerate(js):
                for hh in range(2):
                    h = 2 * p + hh
                    c8 = jj * 2 + hh
                    po = 4 + h // 4; oc = (h % 4) * 65
                    S.pe(lambda e, pb=pb, c8=c8, j=j, h=h, po=po, oc=oc, i=i: e.matmul(bank(po)[:, oc:oc + 65], lhsT=PT[pb][:, c8, :], rhs=v_aug[:, j, h, :], start=(j == 0 and h % 4 == 0), stop=(j == i and h % 4 == 3)),
                         reads=["PT%d.%d" % (pb, c8), "v.%d" % j, "v_ones"], writes=[PSK[po]])

        def tail_N(i):
            b2 = i % 2
            S.dma("sp", lambda e, i=i, b2=b2, s=s: e.dma_start(out=xres[b2][:, :], in_=x[s, i * 128:(i + 1) * 128, :]), writes=["xres%d" % b2])
            for hb in range(2):
                ov = bank(4 + hb)[:, 0:260].rearrange("p (h d) -> p h d", h=4)
                S.dve(lambda e, hb=hb, ov=ov, b2=b2: e.reciprocal(rden[b2][:, 4 * hb:4 * hb + 4], ov[:, :, 64]), reads=[PSK[4 + hb]], writes=["rden%d.%d" % (b2, hb)])
                S.dve(lambda e, hb=hb, ov=ov, b2=b2: e.tensor_tensor(out=yatt[b2][:, 4 * hb:4 * hb + 4, :], in0=ov[:, :, 0:64], in1=rden[b2][:, 4 * hb:4 * hb + 4].unsqueeze(2).to_broadcast([128, 4, 64]), op=ALU.mult),
                      reads=[PSK[4 + hb], "rden%d.%d" % (b2, hb)], writes=["yatt%d.%d" % (b2, hb)])

        def tail_T(i):
            b2 = i % 2
            yf = yatt[b2].rearrange("p h d -> p (h d)")
            for ec in range(8):
                src = m_ssd[:, i, ec * 128:(ec + 1) * 128] if ec < 4 else yf[:, (ec - 4) * 128:(ec - 3) * 128]
                rk = ["m_ssd.%d" % i] if ec < 4 else ["yatt%d.%d" % (b2, (ec - 4) // 2)]
                S.pe(lambda e, ec=ec, src=src: e.transpose(bankb(6)[:, ec * 128:(ec + 1) * 128], src, identB[:, :]), reads=rk + ["identB"], writes=[PSK[6]])
            S.dve(lambda e, b2=b2: e.tensor_copy(mT[b2].rearrange("p a b -> p (a b)"), bankb(6)[:, 0:1024]), reads=[PSK[6]], writes=["mT"])

        def tail_Oq(i, q):
            b2 = i % 2
            half = q // 2
            hp = hpre[b2]
            for ec in range(4 * (q % 2), 4 * (q % 2) + 4):
                S.pe(lambda e, ec=ec, b2=b2, half=half: e.matmul(bank(7)[:, :], lhsT=mT[b2][:, ec, :], rhs=wo[:, ec, half * 512:(half + 1) * 512], start=(ec == 0), stop=(ec == 7)),
                     reads=["mT", "wo"], writes=[PSK[7]])
            if q % 2 == 1:
                S.dve(lambda e, hp=hp, b2=b2, half=half: e.scalar_tensor_tensor(out=hp[:, half * 512:(half + 1) * 512], in0=xres[b2][:, half * 512:(half + 1) * 512], scalar=ALPHA, in1=bank(7)[:, :], op0=ALU.mult, op1=ALU.add),
                      reads=["xres%d" % b2, PSK[7]], writes=["hpre%d.%d" % (b2, half)])
                S.dve(lambda e, hp=hp, half=half, b2=b2: e.bn_stats(bst[b2][:, half, :], hp[:, half * 512:(half + 1) * 512]), reads=["hpre%d.%d" % (b2, half)], writes=["bst%d.%d" % (b2, half)])
            if q == 3:
                S.dve(lambda e, b2=b2: e.bn_aggr(mv[b2][:, :], bst[b2][:, :, :]), reads=["bst%d.0" % b2, "bst%d.1" % b2], writes=["mv%d" % b2])

        def tail_L(i):
            b2 = i % 2
            hp = hpre[b2]
            HK = ["hpre%d.0" % b2, "hpre%d.1" % b2]
            S.act(lambda e, b2=b2: e.activation(out=rs1[b2][:, :], in_=mv[b2][:, 1:2], func=AF.Ln, bias=LN_EPS, scale=1.0), reads=["mv%d" % b2], writes=["rs1%d" % b2])
            S.act(lambda e, b2=b2: e.activation(out=rs1[b2][:, :], in_=rs1[b2][:, :], func=AF.Exp, scale=-0.5), reads=["rs1%d" % b2], writes=["rs1%d" % b2])
            S.dve(lambda e, hp=hp, b2=b2: e.tensor_scalar(out=hp[:, :], in0=hp[:, :], scalar1=mv[b2][:, 0:1], scalar2=rs1[b2][:, 0:1], op0=ALU.subtract, op1=ALU.mult),
                  reads=HK + ["mv%d" % b2, "rs1%d" % b2], writes=HK)
            S.dve(lambda e, hp=hp: e.tensor_tensor(out=hp[:, :], in0=hp[:, :], in1=lnG[:, :], op=ALU.mult), reads=HK + ["lnG"], writes=HK)
            S.dve(lambda e, hp=hp: e.tensor_tensor(out=hp[:, :], in0=hp[:, :], in1=lnB[:, :], op=ALU.add), reads=HK + ["lnB"], writes=HK)
            S.dve(lambda e, hp=hp: e.tensor_scalar(out=ah[:, :], in0=hp[:, :], scalar1=ALPHA, scalar2=None, op0=ALU.mult), reads=HK, writes=["ah"])
            S.dma("sp", lambda e, i=i, s=s: e.dma_start(out=h_scr[s, i * 128:(i + 1) * 128, :], in_=ah[:, :]), reads=["ah"], writes=["h_scr.%d" % i])

        def tail_H(i, half):
            b2 = i % 2
            hp = hpre[b2]
            for q4 in range(4):
                ec = half * 4 + q4
                S.pe(lambda e, q4=q4, ec=ec, hp=hp: e.transpose(bank(6)[:, q4 * 128:(q4 + 1) * 128], hp[:, ec * 128:(ec + 1) * 128], identF[:, :]), reads=["hpre%d.%d" % (b2, half), "identF"], writes=[PSK[6]])
            S.dve(lambda e, half=half, i=i: e.tensor_copy(hT[:, 4 * half:4 * half + 4, i * 128:(i + 1) * 128], bank(6)[:, :].rearrange("p (a b) -> p a b", a=4)), reads=[PSK[6]], writes=["hT.%d.%d" % (i, half)])
            S.dve(lambda e, half=half: e.tensor_copy(hTf[:, 4 * half:4 * half + 4, :], bank(6)[:, :].rearrange("p (a b) -> p a b", a=4)), reads=[PSK[6]], writes=["hTf.%d" % half])

        def tail_R(i, part):
            for ec in range(4 * part, 4 * part + 4):
                S.pe(lambda e, ec=ec: e.matmul(bank(6)[:, 0:20], lhsT=hTf[:, ec, :], rhs=rw[:, ec, :], start=(ec == 0), stop=(ec == 7)), reads=["hTf.%d" % (ec // 4)] + RWK, writes=[PSK[6]])
            if part == 1:
                S.dve(lambda e, i=i: e.tensor_tensor(out=logits[:, i, :], in0=bank(6)[:, 0:20], in1=rb_bc[:, :], op=ALU.add), reads=[PSK[6], "rb0", "rb1"], writes=["logits.%d" % i])

        TD = [int(v) for v in os.environ.get("KTAIL", "0,1,2,3,4,5,9,11,12,14").split(",")]
        TAIL = [(TD[0], tail_N), (TD[1], tail_T), (TD[2], lambda i: tail_Oq(i, 0)), (TD[3], lambda i: tail_Oq(i, 1)), (TD[4], lambda i: tail_Oq(i, 2)), (TD[5], lambda i: tail_Oq(i, 3)),
                (TD[6], tail_L), (TD[7], lambda i: tail_H(i, 0)), (TD[8], lambda i: tail_H(i, 1)), (TD[9], lambda i: (tail_R(i, 0), tail_R(i, 1)))]
        NSTEP = len(TAIL)
        done_steps = set()

        def emit_step(t, k):
            if t < 0 or (t, k) in done_steps:
                return
            for kk in range(k):
                emit_step(t, kk)
            emit_step(t - 1, k)
            if k == 1:
                emit_step(t - 1, 5)
            if k == 7:
                emit_step(t - 1, NSTEP - 1)
            if k == 0:
                for kk in range(NSTEP):
                    emit_step(t - 2, kk)
            done_steps.add((t, k))
            TAIL[k][1](t)

        pending = []
        def tick():
            keep = []
            for ent in pending:
                if ent[0] <= 0:
                    emit_step(ent[1], ent[2])
                else:
                    ent[0] -= 1
                    keep.append(ent)
            pending[:] = keep
        prev = None
        for gi, u in enumerate(units):
            emit_S(u, gi)
            emit_E(u, gi)
            if prev is not None:
                emit_V(prev[0], prev[1])
                if prev[0][0] != u[0]:
                    for k, (dly, fn) in enumerate(TAIL):
                        pending.append([dly, prev[0][0], k])
            tick()
            prev = (u, gi)
        if prev is not None:
            emit_V(prev[0], prev[1])
            for k, (dly, fn) in enumerate(TAIL):
                pending.append([dly, prev[0][0], k])
        while pending:
            tick()

        if dbg == "att" and s == 0:
            fence()
            RACC.reset()
            cvt = RACC.alloc([NT * 1024], BF16)
            d1 = dbg_out("dbg_hT", [128, 8 * SEQ]); d2 = dbg_out("dbg_logits", [128, NT * 20])
            cv2 = RACC.alloc([2 * SEQ], F32)
            for q in range(4):
                S.dve(lambda e, q=q: e.tensor_copy(cv2[:, :], hT[:, 2 * q:2 * q + 2, :].rearrange("p a b -> p (a b)")), reads=[], writes=["cv2"])
                outs.append(S.dma("sp", lambda e, q=q: e.dma_start(out=d1[:, 2 * q * SEQ:(2 * q + 2) * SEQ], in_=cv2[:, :]), reads=["cv2"]))
            outs.append(S.dma("sp", lambda e: e.dma_start(out=d2[:, :], in_=logits.rearrange("p t j -> p (t j)")), reads=["logits.%d" % i for i in range(int(os.environ.get("KATT_TILES", NT)))] if int(os.environ.get("KATT_STAGE", 9)) >= 7 else []))
            break


        fence()
        TT.reset(); RW.reset(); RACC.reset()
        S.dma("sp", lambda e: e.dma_start(out=lnG[:, :], in_=ln2_g[0:1, :].to_broadcast([128, 1024])), writes=["lnG"])
        S.dma("sp", lambda e: e.dma_start(out=lnB[:, :], in_=ln2_b[0:1, :].to_broadcast([128, 1024])), writes=["lnB"])
        LOGK = ["logits.%d" % i for i in range(NT)]
        lg = logits[:, :, 0:4]
        le4 = logits[:, :, 4:20].rearrange("p t (g j) -> p t g j", g=4)
        gmax = TT.alloc([NT], F32); goh = TT.alloc([NT, 4], F32); gex = TT.alloc([NT, 4], F32)
        gsum = TT.alloc([NT], F32); gval = TT.alloc([NT], F32)
        tmp16 = TT.alloc([NT, 4, 4], F32); esel = TT.alloc([NT, 4], F32)
        m1 = TT.alloc([NT], F32); oh1 = TT.alloc([NT, 4], F32); e2 = TT.alloc([NT, 4], F32)
        m2 = TT.alloc([NT], F32); oh2 = TT.alloc([NT, 4], F32); dd = TT.alloc([NT], F32)
        w1 = TT.alloc([NT], F32); w2 = TT.alloc([NT], F32); cw1 = TT.alloc([NT], F32); cw2 = TT.alloc([NT], F32)
        cj = TT.alloc([NT, 4], F32); cj2 = TT.alloc([NT, 4], F32)
        bc4 = lambda a: a.unsqueeze(2).to_broadcast([128, NT, 4])
        S.dve(lambda e: e.tensor_reduce(out=gmax[:, :], in_=lg, axis=AX.X, op=ALU.max), reads=LOGK, writes=["gmax"])
        S.dve(lambda e: e.tensor_tensor(out=goh[:, :, :], in0=lg, in1=bc4(gmax[:, :]), op=ALU.is_equal), reads=LOGK + ["gmax"], writes=["goh"])
        S.dve(lambda e: e.tensor_tensor(out=gex[:, :, :], in0=lg, in1=bc4(gmax[:, :]), op=ALU.subtract), reads=LOGK + ["gmax"], writes=["gex"])
        S.act(lambda e: e.activation(out=gex[:, :, :], in_=gex[:, :, :], func=AF.Exp), reads=["gex"], writes=["gex"])
        S.dve(lambda e: e.tensor_reduce(out=gsum[:, :], in_=gex[:, :, :], axis=AX.X, op=ALU.add), reads=["gex"], writes=["gsum"])
        S.dve(lambda e: e.reciprocal(gval[:, :], gsum[:, :]), reads=["gsum"], writes=["gval"])
        S.dve(lambda e: e.tensor_tensor(out=tmp16[:, :, :, :], in0=le4, in1=goh[:, :, :].unsqueeze(3).to_broadcast([128, NT, 4, 4]), op=ALU.mult), reads=LOGK + ["goh"], writes=["tmp16"])
        S.dve(lambda e: e.tensor_reduce(out=esel[:, :, :], in_=tmp16.rearrange("p t g j -> p t j g"), axis=AX.X, op=ALU.add), reads=["tmp16"], writes=["esel"])
        S.dve(lambda e: e.tensor_reduce(out=m1[:, :], in_=esel[:, :, :], axis=AX.X, op=ALU.max), reads=["esel"], writes=["m1"])
        S.dve(lambda e: e.tensor_tensor(out=oh1[:, :, :], in0=esel[:, :, :], in1=bc4(m1[:, :]), op=ALU.is_equal), reads=["esel", "m1"], writes=["oh1"])
        S.dve(lambda e: e.scalar_tensor_tensor(out=e2[:, :, :], in0=oh1[:, :, :], scalar=-1e30, in1=esel[:, :, :], op0=ALU.mult, op1=ALU.add), reads=["oh1", "esel"], writes=["e2"])
        S.dve(lambda e: e.tensor_reduce(out=m2[:, :], in_=e2[:, :, :], axis=AX.X, op=ALU.max), reads=["e2"], writes=["m2"])
        S.dve(lambda e: e.tensor_tensor(out=oh2[:, :, :], in0=e2[:, :, :], in1=bc4(m2[:, :]), op=ALU.is_equal), reads=["e2", "m2"], writes=["oh2"])
        S.dve(lambda e: e.tensor_tensor(out=dd[:, :], in0=m2[:, :], in1=m1[:, :], op=ALU.subtract), reads=["m1", "m2"], writes=["dd"])
        S.act(lambda e: e.activation(out=dd[:, :], in_=dd[:, :], func=AF.Exp), reads=["dd"], writes=["dd"])
        S.dve(lambda e: e.tensor_scalar(out=w1[:, :], in0=dd[:, :], scalar1=1.0, scalar2=None, op0=ALU.add), reads=["dd"], writes=["w1"])
        S.dve(lambda e: e.reciprocal(w1[:, :], w1[:, :]), reads=["w1"], writes=["w1"])
        S.dve(lambda e: e.tensor_tensor(out=w2[:, :], in0=dd[:, :], in1=w1[:, :], op=ALU.mult), reads=["dd", "w1"], writes=["w2"])
        S.dve(lambda e: e.tensor_tensor(out=cw1[:, :], in0=gval[:, :], in1=w1[:, :], op=ALU.mult), reads=["gval", "w1"], writes=["cw1"])
        S.dve(lambda e: e.tensor_tensor(out=cw2[:, :], in0=gval[:, :], in1=w2[:, :], op=ALU.mult), reads=["gval", "w2"], writes=["cw2"])
        S.dve(lambda e: e.tensor_tensor(out=cj[:, :, :], in0=oh1[:, :, :], in1=bc4(cw1[:, :]), op=ALU.mult), reads=["oh1", "cw1"], writes=["cj"])
        S.dve(lambda e: e.tensor_tensor(out=cj2[:, :, :], in0=oh2[:, :, :], in1=bc4(cw2[:, :]), op=ALU.mult), reads=["oh2", "cw2"], writes=["cj2"])
        S.dve(lambda e: e.tensor_tensor(out=cj[:, :, :], in0=cj[:, :, :], in1=cj2[:, :, :], op=ALU.add), reads=["cj", "cj2"], writes=["cj"])
        S.dve(lambda e: e.tensor_tensor(out=comb.rearrange("p t (g j) -> p t g j", g=4), in0=goh[:, :, :].unsqueeze(3).to_broadcast([128, NT, 4, 4]), in1=cj[:, :, :].unsqueeze(2).to_broadcast([128, NT, 4, 4]), op=ALU.mult),
              reads=["goh", "cj"], writes=["comb"])

        acc = RACC.alloc([NT, 1024], F32)
        wgu = [RW.alloc([8, 1024], BF16) for _ in range(2)]
        wdn = [RW.alloc([4, 1024], BF16) for _ in range(2)]
        sg = [TT.alloc([512], BF16) for _ in range(2)]
        actT = [TT.alloc([4, 512], BF16) for _ in range(2)]
        obuf = [TT.alloc([1024], F32) for _ in range(2)]
        bst2 = [TT.alloc([2, 6], F32) for _ in range(2)]
        mv2 = [TT.alloc([2], F32) for _ in range(2)]
        rs2 = [TT.alloc([1], F32) for _ in range(2)]
        for q in range(4):
            S.dma("sp", lambda e, q=q, s=s: e.dma_start(out=acc[:, 4 * q:4 * q + 4, :], in_=h_scr[s, q * 512:(q + 1) * 512, :].rearrange("(t p) d -> p t d", p=128)),
                  reads=["h_scr.%d" % t for t in range(4 * q, 4 * q + 4)], writes=["acc.%d" % t for t in range(4 * q, 4 * q + 4)])
        NEXP = int(os.environ.get("KNEXP", 16))

        def load_expert(ex):
            sl = ex % 2
            S.dma("pool", lambda e, ex=ex, sl=sl: e.dma_start(out=wgu[sl][:, :, 0:512], in_=w_gate[ex].rearrange("(k p) f -> p k f", p=128)), writes=["wg.%d" % sl])
            S.dma("pool", lambda e, ex=ex, sl=sl: e.dma_start(out=wgu[sl][:, :, 512:1024], in_=w_up[ex].rearrange("(k p) f -> p k f", p=128)), writes=["wu.%d" % sl])
            S.dma("pool", lambda e, ex=ex, sl=sl: e.dma_start(out=wdn[sl][:, :, :], in_=w_down[ex].rearrange("(k p) d -> p k d", p=128)), writes=["wd.%d" % sl])

        load_expert(0)
        if NEXP > 1:
            load_expert(1)
        cg = 0; cd = 0
        for ex in range(NEXP):
            sl = ex % 2
            for tb in range(4):
                ab = (ex * 4 + tb) % 2
                hk = ["hT.%d.%d" % (i, hf) for i in range(4 * tb, 4 * tb + 4) for hf in range(2)]
                for fc in range(4):
                    pg = cg % 2; cg += 1
                    for k in range(8):
                        S.pe(lambda e, pg=pg, k=k, fc=fc, tb=tb, sl=sl: e.matmul(bank(pg)[:, :], lhsT=wgu[sl][:, k, fc * 128:(fc + 1) * 128], rhs=hT[:, k, tb * 512:(tb + 1) * 512], start=(k == 0), stop=(k == 7)),
                             reads=["wg.%d" % sl] + hk, writes=[PSK[pg]])
                    for k in range(8):
                        S.pe(lambda e, pg=pg, k=k, fc=fc, tb=tb, sl=sl: e.matmul(bank(2 + pg)[:, :], lhsT=wgu[sl][:, k, 512 + fc * 128:512 + (fc + 1) * 128], rhs=hT[:, k, tb * 512:(tb + 1) * 512], start=(k == 0), stop=(k == 7)),
                             reads=["wu.%d" % sl] + hk, writes=[PSK[2 + pg]])
                    S.act(lambda e, pg=pg: e.activation(out=sg[pg][:, :], in_=bank(pg)[:, :], func=AF.Silu), reads=[PSK[pg]], writes=["sg%d" % pg])
                    S.dve(lambda e, pg=pg, ab=ab, fc=fc: e.tensor_tensor(out=actT[ab][:, fc, :], in0=bank(2 + pg)[:, :], in1=sg[pg][:, :], op=ALU.mult),
                          reads=[PSK[2 + pg], "sg%d" % pg], writes=["actT%d.%d" % (ab, fc)])
                for tt in range(4):
                    t = tb * 4 + tt
                    pdi = 2 + (cd % 2); cd += 1
                    for half in range(2):
                        for fc in range(4):
                            S.pe(lambda e, pdi=pdi, half=half, fc=fc, ab=ab, tt=tt, sl=sl: e.matmul(pd[pdi][:, half * 512:(half + 1) * 512], lhsT=actT[ab][:, fc, tt * 128:(tt + 1) * 128], rhs=wdn[sl][:, fc, half * 512:(half + 1) * 512], start=(fc == 0), stop=(fc == 3)),
                                 reads=["actT%d.%d" % (ab, fc), "wd.%d" % sl], writes=[PSK[2 * pdi + half]])
                    S.dve(lambda e, pdi=pdi, t=t, ex=ex: e.scalar_tensor_tensor(out=acc[:, t, :], in0=pd[pdi][:, :], scalar=comb[:, t, ex:ex + 1], in1=acc[:, t, :], op0=ALU.mult, op1=ALU.add),
                          reads=[PSK[2 * pdi], PSK[2 * pdi + 1], "comb", "acc.%d" % t], writes=["acc.%d" % t])
            if ex + 2 < NEXP:
                load_expert(ex + 2)
        for t in range(NT):
            b2 = t % 2
            for c2 in range(2):
                S.dve(lambda e, c2=c2, t=t, b2=b2: e.bn_stats(bst2[b2][:, c2, :], acc[:, t, c2 * 512:(c2 + 1) * 512]), reads=["acc.%d" % t], writes=["bst2%d.%d" % (b2, c2)])
            S.dve(lambda e, b2=b2: e.bn_aggr(mv2[b2][:, :], bst2[b2][:, :, :]), reads=["bst2%d.0" % b2, "bst2%d.1" % b2], writes=["mv2%d" % b2])
            S.act(lambda e, b2=b2: e.activation(out=rs2[b2][:, :], in_=mv2[b2][:, 1:2], func=AF.Ln, bias=LN_EPS, scale=1.0), reads=["mv2%d" % b2], writes=["rs2%d" % b2])
            S.act(lambda e, b2=b2: e.activation(out=rs2[b2][:, :], in_=rs2[b2][:, :], func=AF.Exp, scale=-0.5), reads=["rs2%d" % b2], writes=["rs2%d" % b2])
            S.dve(lambda e, t=t, b2=b2: e.tensor_scalar(out=obuf[b2][:, :], in0=acc[:, t, :], scalar1=mv2[b2][:, 0:1], scalar2=rs2[b2][:, 0:1], op0=ALU.subtract, op1=ALU.mult),
                  reads=["acc.%d" % t, "mv2%d" % b2, "rs2%d" % b2], writes=["obuf%d" % b2])
            S.dve(lambda e, b2=b2: e.tensor_tensor(out=obuf[b2][:, :], in0=obuf[b2][:, :], in1=lnG[:, :], op=ALU.mult), reads=["obuf%d" % b2, "lnG"], writes=["obuf%d" % b2])
            S.dve(lambda e, b2=b2: e.tensor_tensor(out=obuf[b2][:, :], in0=obuf[b2][:, :], in1=lnB[:, :], op=ALU.add), reads=["obuf%d" % b2, "lnB"], writes=["obuf%d" % b2])
            outs.append(S.dma("sp", lambda e, t=t, b2=b2, s=s: e.dma_start(out=out[s, t * 128:(t + 1) * 128, :], in_=obuf[b2][:, :]), reads=["obuf%d" % b2], writes=["out.%d.%d" % (s, t)]))
        if s + 1 < NSEQ:
            fence()
        if dbg == "one":
            break

    with nc.allow_non_contiguous_dma(reason="tiny constant loads"):
        st = S.emit(outs)
    return nc, st, dbg_t


_CACHE = {}


def _get_program():
    if "p" not in _CACHE:
        _CACHE["p"] = build_program(dbg=os.environ.get("KDBG", ""))
    return _CACHE["p"]


def kernel(**inputs):
    nc, st, dbg_t = _get_program()
    f = lambda a: np.ascontiguousarray(np.asarray(a, dtype=np.float32))
    x = f(inputs["x"])
    shared = {
        "w_in": f(inputs["w_in"])[0], "b_in": f(inputs["b_in"]).reshape(1, DIN),
        "conv_w": f(inputs["conv_w"])[0], "conv_b": f(inputs["conv_b"]).reshape(1, 1024),
        "a_log": f(inputs["a_log"]).reshape(1, 8), "d_skip": f(inputs["d_skip"]).reshape(1, 8),
        "ssd_norm_g": f(inputs["ssd_norm_g"]).reshape(1, 512), "w_out": f(inputs["w_out"])[0],
        "ln1_g": f(inputs["ln1_g"]).reshape(1, DM), "ln1_b": f(inputs["ln1_b"]).reshape(1, DM),
        "router_group_w": f(inputs["router_group_w"])[0], "router_group_b": f(inputs["router_group_b"]).reshape(1, 4),
        "router_expert_w": f(inputs["router_expert_w"])[0], "router_expert_b": f(inputs["router_expert_b"]).reshape(1, 16),
        "w_gate": f(inputs["w_gate"])[0], "w_up": f(inputs["w_up"])[0], "w_down": f(inputs["w_down"])[0],
        "ln2_g": f(inputs["ln2_g"]).reshape(1, DM), "ln2_b": f(inputs["ln2_b"]).reshape(1, DM),
    }
    ncores = int(os.environ.get("KCORES", NCORES))
    in_maps = []
    for c in range(ncores):
        m = dict(shared)
        m["x"] = np.ascontiguousarray(x[c * NSEQ:(c + 1) * NSEQ])
        in_maps.append(m)
    res = run_bass_kernel_spmd(nc, in_maps, core_ids=list(range(ncores)))
    if os.environ.get("KDBG", ""):
        _CACHE["dbg"] = res.results
    outp = np.concatenate([r["out"] for r in res.results], axis=0)
    return outp.astype(np.float32)
```

```python
import os
import numpy as np
import concourse.bass as bass
import concourse.mybir as mybir
from concourse.bass_utils import run_bass_kernel_spmd

F32 = mybir.dt.float32
BF16 = mybir.dt.bfloat16
U8 = mybir.dt.uint8
AF = mybir.ActivationFunctionType
ALU = mybir.AluOpType
AX = mybir.AxisListType

NCORES = 8
NSEQ = 2
SEQ = 2048
NT = 16
DM = 1024
DIN = 3088
ALPHA = float(2.0 ** 0.25)
LN_EPS = 1e-5
RMS_EPS = 1e-5
ATT_SCALE = 0.125
NEG = -30000.0


class Op:
    __slots__ = ("eng", "fn", "reads", "writes", "deps", "signal", "sigval", "dma", "gi", "nofence")


class Sched:
    COMPUTE = ("pe", "act", "dve", "pool")

    def __init__(self, nc, n_dma_sems=40):
        self.nc = nc
        self.h = {"pe": nc.tensor, "act": nc.scalar, "dve": nc.vector, "pool": nc.gpsimd, "sp": nc.sync}
        self.ops = []
        self.last_w = {}
        self.readers = {}
        self.n_dma_sems = n_dma_sems
        self.live_dma = []
        self.nfence = 0

    def add(self, eng, fn, reads=(), writes=(), dma=False, nofence=False):
        o = Op()
        o.eng = eng; o.fn = fn; o.reads = tuple(reads); o.writes = tuple(writes)
        o.deps = []; o.signal = False; o.sigval = None; o.dma = dma; o.gi = len(self.ops); o.nofence = nofence
        for r in o.reads:
            p = self.last_w.get(r)
            if p is not None:
                self._dep(o, p, True)
            if r.startswith("ps"):
                rd = self.readers.get(r)
                if rd:
                    for q in rd.values():
                        if q.eng != eng:
                            self._dep(o, q, True)
        for w in o.writes:
            p = self.last_w.get(w)
            if p is not None:
                self._dep(o, p, False)
            rd = self.readers.get(w)
            if rd:
                for q in rd.values():
                    self._dep(o, q, False)
        for r in o.reads:
            d = self.readers.setdefault(r, {})
            d[("dma", o.gi) if dma else eng] = o
        for w in o.writes:
            self.last_w[w] = o
            self.readers[w] = {}
        self.ops.append(o)
        if dma and not nofence:
            self.live_dma.append(o)
        return o

    def _dep(self, o, p, raw):
        if p is o:
            return
        if (not p.dma) and (not o.dma) and p.eng == o.eng:
            if o.eng == "pe":
                return
        o.deps.append(p)
        p.signal = True

    def pe(self, fn, reads=(), writes=()): return self.add("pe", fn, reads, writes)
    def act(self, fn, reads=(), writes=()): return self.add("act", fn, reads, writes)
    def dve(self, fn, reads=(), writes=()): return self.add("dve", fn, reads, writes)
    def pool(self, fn, reads=(), writes=()): return self.add("pool", fn, reads, writes)
    def dma(self, q, fn, reads=(), writes=(), nofence=False):
        return self.add(q, fn, reads, writes, dma=True, nofence=nofence)

    def fence(self, scratch):
        n = self.nfence; self.nfence += 1
        a_keys = []
        col = {"pe": None, "act": 0, "dve": 1, "pool": 2}
        for e in ("act", "dve", "pool"):
            k = "fenceA.%d.%s" % (n, e)
            c = col[e]
            if e == "act":
                self.add(e, (lambda eh, c=c: eh.activation(out=scratch[:, c:c + 1], in_=scratch[:, 8:9], func=AF.Copy)), reads=(), writes=(k,))
            else:
                self.add(e, (lambda eh, c=c: eh.memset(scratch[:, c:c + 1], 0.0)), reads=(), writes=(k,))
            a_keys.append(k)
        k = "fenceA.%d.pe" % n
        self.add("pe", (lambda eh: eh.matmul(self.fence_ps[0:1, 0:1], lhsT=self.fence_w[0:1, 0:1], rhs=self.fence_w[0:1, 0:1], start=True, stop=True)),
                 reads=(), writes=(k, "ps7"))
        a_keys.append(k)
        dmas = self.live_dma
        self.live_dma = []
        for e in ("act", "dve", "pool", "pe", "sp"):
            kb = "fenceB.%d.%s" % (n, e)
            if e == "act":
                o = self.add(e, (lambda eh: eh.activation(out=scratch[:, 3:4], in_=scratch[:, 8:9], func=AF.Copy)), reads=a_keys, writes=(kb,))
            elif e == "pe":
                o = self.add(e, (lambda eh: eh.matmul(self.fence_ps[0:1, 1:2], lhsT=self.fence_w[0:1, 0:1], rhs=self.fence_w[0:1, 0:1], start=True, stop=True)),
                             reads=a_keys, writes=(kb, "ps7"))
            elif e == "sp":
                o = self.add(e, (lambda eh: eh.nop()), reads=a_keys, writes=(kb,))
            else:
                c = 4 if e == "dve" else 5
                o = self.add(e, (lambda eh, c=c: eh.memset(scratch[:, c:c + 1], 0.0)), reads=a_keys, writes=(kb,))
            for d in dmas:
                o.deps.append(d)

    def emit(self, final_wait_ops=()):
        nc = self.nc
        esem = {e: nc.alloc_semaphore("s_" + e) for e in self.COMPUTE}
        dsems = [nc.alloc_semaphore("s_dma%d" % i) for i in range(self.n_dma_sems)]
        dtotal = [0] * self.n_dma_sems
        dlast = [None] * self.n_dma_sems
        ecount = {e: 0 for e in self.COMPUTE}
        nd = 0
        nq = {"sp": 0, "pool": 0}
        half = self.n_dma_sems // 2
        for o in self.ops:
            if o.dma:
                qi = nq[o.eng]; nq[o.eng] += 1; nd += 1
                i = (qi % half) + (0 if o.eng == "sp" else half)
                prev = dlast[i]
                if prev is not None:
                    o.deps.append(prev)
                dtotal[i] += 16
                o.sigval = (dsems[i], dtotal[i], 1000 + i)
                dlast[i] = o
            elif o.signal:
                ecount[o.eng] += 1
                o.sigval = (esem[o.eng], ecount[o.eng], o.eng)
        known = {e: {} for e in self.h}
        nwaits = 0
        for o in self.ops:
            eh = self.h[o.eng]
            kn = known[o.eng]
            need = {}
            for p in o.deps:
                s, v, key = p.sigval
                if kn.get(key, 0) >= v:
                    continue
                if key not in need or need[key][1] < v:
                    need[key] = (s, v)
            for key, (s, v) in need.items():
                eh.wait_ge(s, v)
                kn[key] = v
                nwaits += 1
            ins = o.fn(eh)
            if o.dma:
                ins.then_inc(o.sigval[0], 16)
            elif o.signal:
                ins.then_inc(o.sigval[0], 1)
        eh = self.h["sp"]
        for o in final_wait_ops:
            s, v, key = o.sigval
            eh.wait_ge(s, v)
        self.stats = dict(n_ops=len(self.ops), n_waits=nwaits, counts=dict(ecount), n_dma=nd)
        return self.stats


class Arena:
    def __init__(self, nc, name, nbytes):
        self.t = nc.alloc_sbuf_tensor(name, [128, nbytes], U8)
        self.n = nbytes
        self.off = 0

    def reset(self, off=0):
        self.off = off

    def alloc(self, shape, dtype, parts=128):
        esz = 2 if dtype == BF16 else 4
        n = esz
        for s in shape:
            n *= s
        off = (self.off + 31) // 32 * 32
        assert off + n <= self.n, (off, n, self.n)
        self.off = off + n
        flat = self.t[0:parts, off:off + n].bitcast(dtype)
        if len(shape) == 1:
            return flat
        names = " ".join("a%d" % i for i in range(len(shape)))
        kw = {"a%d" % i: shape[i] for i in range(1, len(shape))}
        return flat.rearrange("p (%s) -> p %s" % (names, names), **kw)


def build_program(dbg=False):
    nc = bass.Bass("TRN2", target_bir_lowering=False)
    S = Sched(nc)
    D = {}

    def din(name, shape):
        D[name] = nc.dram_tensor(name, list(shape), F32, kind="ExternalInput").ap()
        return D[name]

    x = din("x", [NSEQ, SEQ, DM])
    w_in = din("w_in", [DM, DIN])
    b_in = din("b_in", [1, DIN])
    conv_w = din("conv_w", [4, 1024])
    conv_b = din("conv_b", [1, 1024])
    a_log = din("a_log", [1, 8])
    d_skip = din("d_skip", [1, 8])
    ssd_g = din("ssd_norm_g", [1, 512])
    w_out = din("w_out", [DM, DM])
    ln1_g = din("ln1_g", [1, DM]); ln1_b = din("ln1_b", [1, DM])
    rg_w = din("router_group_w", [DM, 4]); rg_b = din("router_group_b", [1, 4])
    re_w = din("router_expert_w", [4, DM, 4]); re_b = din("router_expert_b", [1, 16])
    w_gate = din("w_gate", [16, DM, 512]); w_up = din("w_up", [16, DM, 512]); w_down = din("w_down", [16, 512, DM])
    ln2_g = din("ln2_g", [1, DM]); ln2_b = din("ln2_b", [1, DM])
    out = nc.dram_tensor("out", [NSEQ, SEQ, DM], F32, kind="ExternalOutput").ap()
    h_scr = nc.dram_tensor("h_scr", [NSEQ, SEQ, DM], F32).ap()
    dbg_t = {}

    def dbg_out(name, shape):
        dbg_t[name] = nc.dram_tensor(name, list(shape), F32, kind="ExternalOutput").ap()
        return dbg_t[name]

    CONST = Arena(nc, "CONST", 22 * 1024)
    RW = Arena(nc, "RW", 49408)
    RX = Arena(nc, "RX", 32768)
    RACC = Arena(nc, "RACC", 65536)
    MSSD = Arena(nc, "MSSD", 16384)
    TT = Arena(nc, "TT", nc.sbuf_bytes_remaining - 256)

    identB = CONST.alloc([128], BF16); identF = CONST.alloc([128], F32)
    Uf = CONST.alloc([128], F32); triU = CONST.alloc([128], BF16); maskneg = CONST.alloc([128], BF16)
    onesF = CONST.alloc([128], F32)
    ones_row = CONST.alloc([128], BF16, parts=1)
    bz_row = CONST.alloc([512], BF16, parts=1); bv_row = CONST.alloc([512], BF16, parts=1)
    bdtf = CONST.alloc([16], F32)
    bxbc = CONST.alloc([8], F32); bq = CONST.alloc([4], F32); bk = CONST.alloc([4], F32)
    convw = CONST.alloc([8, 4], F32); convb = CONST.alloc([8], F32)
    a_bc = CONST.alloc([8], F32); dskip_bc = CONST.alloc([8], F32)
    gssd_bc = CONST.alloc([512], F32)
    lnG = CONST.alloc([1024], F32); lnB = CONST.alloc([1024], F32)
    rw = CONST.alloc([8, 20], F32); rb_bc = CONST.alloc([20], F32)
    logits = CONST.alloc([NT, 20], F32); comb = CONST.alloc([NT, 16], F32)
    fsc = CONST.alloc([16], F32)
    S.fence_w = CONST.alloc([8], BF16)
    pd = [nc.alloc_psum_tensor("pd%d" % i, [128, 1024], F32) for i in range(4)]
    def bank(i):
        return pd[i // 2][:, (i % 2) * 512:(i % 2) * 512 + 512]
    def bankb(i):
        return bank(i).bitcast(BF16)
    PSK = ["ps%d" % i for i in range(8)]
    S.fence_ps = nc.alloc_sbuf_tensor("fence_dummy", [1, 8], F32)
    S.fence_ps = bank(7)[:, 504:512]

    def fence():
        S.fence(fsc)

    S.pool(lambda e: e.memset(fsc[:, :], 0.0), writes=["fsc"])
    S.pool(lambda e: e.memset(S.fence_w[:, :], 0.0), writes=["fence_w"])
    S.pool(lambda e: e.memset(identB[:, :], 1.0), writes=["identB"])
    S.pool(lambda e: e.affine_select(out=identB[:, :], in_=identB[:, :], pattern=[[-1, 128]], compare_op=ALU.is_equal, fill=0.0, base=0, channel_multiplier=1), reads=["identB"], writes=["identB"])
    S.pool(lambda e: e.memset(identF[:, :], 1.0), writes=["identF"])
    S.pool(lambda e: e.affine_select(out=identF[:, :], in_=identF[:, :], pattern=[[-1, 128]], compare_op=ALU.is_equal, fill=0.0, base=0, channel_multiplier=1), reads=["identF"], writes=["identF"])
    S.pool(lambda e: e.memset(Uf[:, :], 1.0), writes=["Uf"])
    S.pool(lambda e: e.affine_select(out=Uf[:, :], in_=Uf[:, :], pattern=[[1, 128]], compare_op=ALU.is_ge, fill=0.0, base=0, channel_multiplier=-1), reads=["Uf"], writes=["Uf"])
    S.pool(lambda e: e.memset(triU[:, :], 1.0), writes=["triU"])
    S.pool(lambda e: e.affine_select(out=triU[:, :], in_=triU[:, :], pattern=[[1, 128]], compare_op=ALU.is_ge, fill=0.0, base=0, channel_multiplier=-1), reads=["triU"], writes=["triU"])
    S.pool(lambda e: e.memset(maskneg[:, :], NEG), writes=["maskneg"])
    S.pool(lambda e: e.affine_select(out=maskneg[:, :], in_=maskneg[:, :], pattern=[[-1, 128]], compare_op=ALU.is_gt, fill=0.0, base=0, channel_multiplier=1), reads=["maskneg"], writes=["maskneg"])
    S.pool(lambda e: e.memset(onesF[:, :], 1.0), writes=["onesF"])
    S.pool(lambda e: e.memset(ones_row[:, :], 1.0), writes=["ones_row"])
    S.dma("pool", lambda e: e.dma_start(out=bz_row[:, :], in_=b_in[0:1, 0:512]), writes=["bz_row"])
    S.dma("pool", lambda e: e.dma_start(out=bv_row[:, :], in_=b_in[0:1, 2568:3080]), writes=["bv_row"])
    S.dma("sp", lambda e: e.dma_start(out=bdtf[:, 0:8], in_=b_in[0:1, 1536:1544].to_broadcast([128, 8])), writes=["bdtf0"])
    S.dma("sp", lambda e: e.dma_start(out=bdtf[:, 8:16], in_=b_in[0:1, 3080:3088].to_broadcast([128, 8])), writes=["bdtf1"])
    S.dma("sp", lambda e: e.dma_start(out=bxbc[:, :], in_=b_in[0, 512:1536].rearrange("(c p) -> p c", p=128)), writes=["bxbc"])
    S.dma("sp", lambda e: e.dma_start(out=bq[:, :], in_=b_in[0, 1544:2056].rearrange("(c p) -> p c", p=128)), writes=["bq"])
    S.dma("sp", lambda e: e.dma_start(out=bk[:, :], in_=b_in[0, 2056:2568].rearrange("(c p) -> p c", p=128)), writes=["bk"])
    for k in range(4):
        S.dma("sp", lambda e, k=k: e.dma_start(out=convw[:, :, k], in_=conv_w[k, :].rearrange("(c p) -> p c", p=128)), writes=["convw%d" % k])
    CONVW = ["convw%d" % k for k in range(4)]
    S.dma("sp", lambda e: e.dma_start(out=convb[:, :], in_=conv_b[0, :].rearrange("(c p) -> p c", p=128)), writes=["convb"])
    S.dma("sp", lambda e: e.dma_start(out=a_bc[:, :], in_=a_log[0:1, :].to_broadcast([128, 8])), writes=["a_bc"])
    S.act(lambda e: e.activation(out=a_bc[:, :], in_=a_bc[:, :], func=AF.Exp), reads=["a_bc"], writes=["a_bc"])
    S.dve(lambda e: e.tensor_scalar(out=a_bc[:, :], in0=a_bc[:, :], scalar1=-1.0, scalar2=None, op0=ALU.mult), reads=["a_bc"], writes=["a_bc"])
    S.dma("sp", lambda e: e.dma_start(out=dskip_bc[:, :], in_=d_skip[0:1, :].to_broadcast([128, 8])), writes=["dskip_bc"])
    S.dma("sp", lambda e: e.dma_start(out=gssd_bc[:, :], in_=ssd_g[0:1, :].to_broadcast([128, 512])), writes=["gssd_bc"])
    S.dma("sp", lambda e: e.dma_start(out=rw[:, :, 0:4], in_=rg_w.rearrange("(k p) j -> p k j", p=128)), writes=["rw0"])
    for g in range(4):
        S.dma("sp", lambda e, g=g: e.dma_start(out=rw[:, :, 4 + 4 * g:8 + 4 * g], in_=re_w[g].rearrange("(k p) j -> p k j", p=128)), writes=["rw%d" % (g + 1)])
    RWK = ["rw%d" % i for i in range(5)]
    S.dma("sp", lambda e: e.dma_start(out=rb_bc[:, 0:4], in_=rg_b[0:1, :].to_broadcast([128, 4])), writes=["rb0"])
    S.dma("sp", lambda e: e.dma_start(out=rb_bc[:, 4:20], in_=re_b[0:1, :].to_broadcast([128, 16])), writes=["rb1"])

    outs = []

    for s in range(NSEQ):
        RW.reset(); RX.reset(); RACC.reset(); MSSD.reset(); TT.reset()
        wgu = [None, None]; wdn = [None, None]
        wgu[0] = RW.alloc([8, 1024], BF16); wdn[0] = RW.alloc([4, 1024], BF16)
        wgu[1] = RW.alloc([8, 1024], BF16); wdn[1] = RW.alloc([4, 1024], BF16)
        RW.reset()
        wA = RW.alloc([8, 1544], BF16)
        xb = [RW.alloc([4, 1024], BF16) for _ in range(2)]
        xT = RX.alloc([8, SEQ], BF16)
        sz = RACC.alloc([NT, 512], BF16)
        xsB = RACC.alloc([NT, 768], BF16)
        BT = RACC.alloc([2, SEQ], BF16)
        CT = RACC.alloc([2, SEQ], BF16)
        xsT = MSSD.alloc([4, SEQ], BF16)
        dt_t = TT.alloc([NT, 8], F32)
        tt_mark = TT.off
        pre = [TT.alloc([SEQ + 3], BF16) for _ in range(2)]
        cacc = [TT.alloc([SEQ], F32) for _ in range(2)]
        dt_raw = TT.alloc([NT, 8], F32)

        for half in range(2):
            S.dma("pool", lambda e, half=half: e.dma_start(out=wA[:, 4 * half:4 * half + 4, :], in_=w_in.rearrange("(k p) c -> p k c", p=128)[:, 4 * half:4 * half + 4, 0:1544]),
                  writes=["wA"])
        for b in range(2):
            S.pool(lambda e, b=b: e.memset(pre[b][:, 0:3], 0.0), writes=["pre%d" % b])
        ev = 0
        for blk in range(4):
            S.dma("pool", lambda e, blk=blk, s=s: e.dma_start(out=xb[blk % 2][:, :, :], in_=x[s, blk * 512:(blk + 1) * 512, :].rearrange("(t p) d -> p t d", p=128)),
                  writes=["xb%d" % (blk % 2)])
            for k in range(8):
                pb = (blk * 8 + k) % 2
                for t in range(4):
                    S.pe(lambda e, pb=pb, t=t, k=k, blk=blk: e.transpose(bankb(pb)[:, t * 128:(t + 1) * 128], xb[blk % 2][:, t, k * 128:(k + 1) * 128], identB[:, :]),
                         reads=["xb%d" % (blk % 2), "identB"], writes=[PSK[pb]])
                if ev % 2 == 0:
                    S.act(lambda e, pb=pb, k=k, blk=blk: e.copy(xT[:, k, blk * 512:(blk + 1) * 512], bankb(pb)[:, 0:512]), reads=[PSK[pb]], writes=["xT.%d.%d" % (k, blk)])
                else:
                    S.dve(lambda e, pb=pb, k=k, blk=blk: e.tensor_copy(xT[:, k, blk * 512:(blk + 1) * 512], bankb(pb)[:, 0:512]), reads=[PSK[pb]], writes=["xT.%d.%d" % (k, blk)])
                ev += 1
            for tt in range(4):
                t = blk * 4 + tt
                pz = 2 + (t % 2)
                xk = ["xT.%d.%d" % (k, blk) for k in range(8)]
                for k in range(8):
                    S.pe(lambda e, pz=pz, t=t, k=k: e.matmul(bank(pz)[:, :], lhsT=xT[:, k, t * 128:(t + 1) * 128], rhs=wA[:, k, 0:512], start=(k == 0), stop=False),
                         reads=[xk[k], "wA"], writes=[PSK[pz]])
                S.pe(lambda e, pz=pz: e.matmul(bank(pz)[:, :], lhsT=ones_row[0:1, :], rhs=bz_row[0:1, :], start=False, stop=True),
                     reads=["ones_row", "bz_row"], writes=[PSK[pz]])
                S.act(lambda e, pz=pz, t=t: e.activation(out=sz[:, t, :], in_=bank(pz)[:, :], func=AF.Silu), reads=[PSK[pz]], writes=["sz.%d" % t])
                for k in range(8):
                    S.pe(lambda e, t=t, k=k: e.matmul(bank(4)[:, t * 8:(t + 1) * 8], lhsT=xT[:, k, t * 128:(t + 1) * 128], rhs=wA[:, k, 1536:1544], start=(k == 0), stop=(k == 7)),
                         reads=[xk[k], "wA"], writes=[PSK[4]])
        S.dve(lambda e: e.tensor_tensor(out=dt_raw[:, :, :], in0=bank(4)[:, 0:128].rearrange("p (t h) -> p t h", h=8), in1=bdtf[:, 0:8].unsqueeze(1).to_broadcast([128, NT, 8]), op=ALU.add),
              reads=[PSK[4], "bdtf0"], writes=["dt_raw"])
        XT_ALL = lambda blk: ["xT.%d.%d" % (k, blk) for k in range(8)]
        for c in range(8):
            pb_ = c % 2
            for blk in range(4):
                pc = 5 + (c * 4 + blk) % 2
                for k in range(8):
                    S.pe(lambda e, pc=pc, c=c, k=k, blk=blk: e.matmul(bank(pc)[:, :], lhsT=wA[:, k, 512 + c * 128:512 + (c + 1) * 128], rhs=xT[:, k, blk * 512:(blk + 1) * 512], start=(k == 0), stop=(k == 7)),
                         reads=["xT.%d.%d" % (k, blk), "wA"], writes=[PSK[pc]])
                S.act(lambda e, pc=pc, c=c, blk=blk, pb_=pb_: e.activation(out=pre[pb_][:, 3 + blk * 512:3 + (blk + 1) * 512], in_=bank(pc)[:, :], func=AF.Identity, bias=bxbc[:, c:c + 1], scale=1.0),
                      reads=[PSK[pc], "bxbc"], writes=["pre%d" % pb_])
            eng = S.dve
            ca = cacc[pb_]; pr = pre[pb_]
            eng(lambda e, ca=ca, pr=pr, c=c: e.tensor_scalar(out=ca[:, :], in0=pr[:, 3:SEQ + 3], scalar1=convw[:, c, 3:4], scalar2=None, op0=ALU.mult),
                reads=["pre%d" % pb_] + CONVW, writes=["cacc%d" % pb_])
            for kk in (2, 1, 0):
                eng(lambda e, ca=ca, pr=pr, c=c, kk=kk: e.scalar_tensor_tensor(out=ca[:, :], in0=pr[:, kk:SEQ + kk], scalar=convw[:, c, kk:kk + 1], in1=ca[:, :], op0=ALU.mult, op1=ALU.add),
                    reads=["pre%d" % pb_, "cacc%d" % pb_] + CONVW, writes=["cacc%d" % pb_])
            if c < 4:
                dst = xsT[:, c, :]; dk = "xsT.%d" % c
            elif c < 6:
                dst = BT[:, c - 4, :]; dk = "BT.%d" % (c - 4)
            else:
                dst = CT[:, c - 6, :]; dk = "CT.%d" % (c - 6)
            S.act(lambda e, ca=ca, dst=dst, c=c: e.activation(out=dst, in_=ca[:, :], func=AF.Silu, bias=convb[:, c:c + 1], scale=1.0),
                  reads=["cacc%d" % pb_, "convb"], writes=[dk])
        for t in range(NT):
            pb = t % 2
            for c in range(6):
                src = xsT[:, c, t * 128:(t + 1) * 128] if c < 4 else BT[:, c - 4, t * 128:(t + 1) * 128]
                sk = "xsT.%d" % c if c < 4 else "BT.%d" % (c - 4)
                S.pe(lambda e, pb=pb, c=c, src=src: e.transpose(bankb(pb)[:, c * 128:(c + 1) * 128], src, identB[:, :]), reads=[sk, "identB"], writes=[PSK[pb]])
            if t % 2 == 0:
                S.dve(lambda e, pb=pb, t=t: e.tensor_copy(xsB[:, t, :], bankb(pb)[:, 0:768]), reads=[PSK[pb]], writes=["xsB.%d" % t])
            else:
                S.act(lambda e, pb=pb, t=t: e.copy(xsB[:, t, :], bankb(pb)[:, 0:768]), reads=[PSK[pb]], writes=["xsB.%d" % t])
        S.act(lambda e: e.activation(out=dt_t[:, :, :], in_=dt_raw[:, :, :], func=AF.Exp), reads=["dt_raw"], writes=["dt_t"])
        S.act(lambda e: e.activation(out=dt_t[:, :, :], in_=dt_t[:, :, :], func=AF.Ln, bias=1.0, scale=1.0), reads=["dt_t"], writes=["dt_t"])

        fence()
        MSSD.reset(); TT.reset(tt_mark); RW.reset()
        m_ssd = MSSD.alloc([NT, 512], BF16)
        da = TT.alloc([NT, 8], F32); acum = TT.alloc([NT, 8], F32); nacum = TT.alloc([NT, 8], F32)
        alast = TT.alloc([NT, 8], F32); dte = TT.alloc([NT, 8], F32); ea = TT.alloc([NT, 8], F32)
        cdec = TT.alloc([NT, 8], F32); dtdte = TT.alloc([NT, 8], F32)
        stT = TT.alloc([8, 64], F32); stTb = TT.alloc([8, 64], BF16)
        LT = [TT.alloc([128], BF16) for _ in range(4)]
        MT = [TT.alloc([128], BF16) for _ in range(4)]
        xdt = [RW.alloc([8, 64], BF16) for _ in range(2)]
        xdtd = [RW.alloc([8, 64], BF16) for _ in range(2)]
        t1 = [RW.alloc([8, 64], F32) for _ in range(2)]
        t2 = [RW.alloc([8, 64], F32) for _ in range(2)]
        yg = [RW.alloc([512], F32) for _ in range(2)]
        junk = TT.alloc([256], F32)
        ss = [TT.alloc([2], F32) for _ in range(2)]
        rstd = [TT.alloc([2], F32) for _ in range(2)]

        S.dve(lambda e: e.tensor_tensor(out=da[:, :, :], in0=dt_t[:, :, :], in1=a_bc[:, :].unsqueeze(1).to_broadcast([128, NT, 8]), op=ALU.mult), reads=["dt_t", "a_bc"], writes=["da"])
        daf = da.rearrange("p t h -> p (t h)")
        S.pe(lambda e: e.matmul(bank(5)[:, 0:128], lhsT=Uf[:, :], rhs=daf, start=True, stop=True), reads=["Uf", "da"], writes=[PSK[5]])
        S.pe(lambda e: e.matmul(bank(6)[:, 0:128], lhsT=onesF[:, :], rhs=daf, start=True, stop=True), reads=["onesF", "da"], writes=[PSK[6]])
        S.dve(lambda e: e.tensor_copy(acum.rearrange("p t h -> p (t h)"), bank(5)[:, 0:128]), reads=[PSK[5]], writes=["acum"])
        S.dve(lambda e: e.tensor_scalar(out=nacum.rearrange("p t h -> p (t h)"), in0=bank(5)[:, 0:128], scalar1=-1.0, scalar2=None, op0=ALU.mult), reads=[PSK[5]], writes=["nacum"])
        S.dve(lambda e: e.tensor_copy(alast.rearrange("p t h -> p (t h)"), bank(6)[:, 0:128]), reads=[PSK[6]], writes=["alast"])
        S.dve(lambda e: e.tensor_tensor(out=dte[:, :, :], in0=alast[:, :, :], in1=acum[:, :, :], op=ALU.subtract), reads=["alast", "acum"], writes=["dte"])
        S.act(lambda e: e.activation(out=dte[:, :, :], in_=dte[:, :, :], func=AF.Exp), reads=["dte"], writes=["dte"])
        S.act(lambda e: e.activation(out=ea[:, :, :], in_=acum[:, :, :], func=AF.Exp), reads=["acum"], writes=["ea"])
        S.act(lambda e: e.activation(out=cdec[:, :, :], in_=alast[:, :, :], func=AF.Exp), reads=["alast"], writes=["cdec"])
        S.dve(lambda e: e.tensor_tensor(out=dtdte[:, :, :], in0=dt_t[:, :, :], in1=dte[:, :, :], op=ALU.mult), reads=["dt_t", "dte"], writes=["dtdte"])
        S.pool(lambda e: e.memset(stT[:, :, :], 0.0), writes=["stT"])
        S.pool(lambda e: e.memset(stTb[:, :, :], 0.0), writes=["stTb"])

        def ssd_F(c):
            cs = slice(c * 128, (c + 1) * 128)
            b2 = c % 2
            py = 2 + b2
            xs_c = xsB[:, c, 0:512].rearrange("p (h d) -> p h d", h=8)
            S.pool(lambda e, c=c, b2=b2, xs_c=xs_c: e.tensor_tensor(out=xdt[b2][:, :, :], in0=xs_c, in1=dt_t[:, c, :].unsqueeze(2).to_broadcast([128, 8, 64]), op=ALU.mult),
                   reads=["xsB.%d" % c, "dt_t"], writes=["xdt%d" % b2])
            S.pool(lambda e, c=c, b2=b2, xs_c=xs_c: e.tensor_tensor(out=xdtd[b2][:, :, :], in0=xs_c, in1=dtdte[:, c, :].unsqueeze(2).to_broadcast([128, 8, 64]), op=ALU.mult),
                   reads=["xsB.%d" % c, "dtdte"], writes=["xdtd%d" % b2])
            S.pool(lambda e, c=c, b2=b2, xs_c=xs_c: e.tensor_tensor(out=t2[b2][:, :, :], in0=xs_c, in1=dskip_bc[:, :].unsqueeze(2).to_broadcast([128, 8, 64]), op=ALU.mult),
                   reads=["xsB.%d" % c, "dskip_bc"], writes=["t2%d" % b2])
            for g in range(2):
                S.pe(lambda e, g=g, cs=cs: e.matmul(bank(4)[:, g * 128:(g + 1) * 128], lhsT=BT[:, g, cs], rhs=CT[:, g, cs], start=True, stop=True),
                     reads=["BT.%d" % g, "CT.%d" % g], writes=[PSK[4]])
            for hh in range(2):
                pl = hh
                for h4 in range(4):
                    h = hh * 4 + h4
                    S.pe(lambda e, pl=pl, h4=h4, c=c, h=h: e.matmul(bank(pl)[:, h4 * 128:(h4 + 1) * 128], lhsT=da[:, c, h:h + 1].to_broadcast([128, 128]), rhs=Uf[:, :], start=True, stop=False),
                         reads=["da", "Uf"], writes=[PSK[pl]])
                    S.pe(lambda e, pl=pl, h4=h4: e.matmul(bank(pl)[:, h4 * 128:(h4 + 1) * 128], lhsT=identB[:, :], rhs=maskneg[:, :], start=False, stop=True),
                         reads=["identB", "maskneg"], writes=[PSK[pl]])
            for hh in range(2):
                pl = hh
                for h4 in range(4):
                    h = hh * 4 + h4
                    S.act(lambda e, pl=pl, h4=h4, c=c, h=h: e.activation(out=LT[h4][:, :], in_=bank(pl)[:, h4 * 128:(h4 + 1) * 128], func=AF.Exp, bias=nacum[:, c, h:h + 1], scale=1.0),
                          reads=[PSK[pl], "nacum"], writes=["LT%d" % h4])
                    g = h // 4
                    S.dve(lambda e, h4=h4, g=g: e.tensor_tensor(out=MT[h4][:, :], in0=bank(4)[:, g * 128:(g + 1) * 128], in1=LT[h4][:, :], op=ALU.mult),
                          reads=[PSK[4], "LT%d" % h4], writes=["MT%d" % h4])
                    S.pe(lambda e, h4=h4, h=h, b2=b2, py=py: e.matmul(bank(py)[:, h * 64:(h + 1) * 64], lhsT=MT[h4][:, :], rhs=xdt[b2][:, h, :], start=True, stop=True),
                         reads=["MT%d" % h4, "xdt%d" % b2], writes=[PSK[py]])

        def ssd_B(c):
            cs = slice(c * 128, (c + 1) * 128)
            b2 = c % 2
            py = 2 + b2
            if c > 0:
                for g in range(2):
                    S.pe(lambda e, g=g, cs=cs: e.matmul(bank(6)[:, g * 256:(g + 1) * 256], lhsT=CT[:, g, cs], rhs=stTb[:, 4 * g:4 * g + 4, :].rearrange("p h d -> p (h d)"), start=True, stop=True),
                         reads=["CT.%d" % g, "stTb"], writes=[PSK[6]])
            if c < NT - 1:
                for g in range(2):
                    S.pe(lambda e, g=g, c=c, b2=b2: e.matmul(bank(7)[:, g * 256:(g + 1) * 256], lhsT=xsB[:, c, 512 + g * 128:512 + (g + 1) * 128], rhs=xdtd[b2][:, 4 * g:4 * g + 4, :].rearrange("p h d -> p (h d)"), start=True, stop=True),
                         reads=["xsB.%d" % c, "xdtd%d" % b2], writes=[PSK[7]])
                S.dve(lambda e, c=c: e.tensor_tensor(out=stT[:, :, :], in0=stT[:, :, :], in1=cdec[:, c, :].unsqueeze(2).to_broadcast([128, 8, 64]), op=ALU.mult),
                      reads=["stT", "cdec"], writes=["stT"])
                S.dve(lambda e: e.tensor_tensor(out=stT[:, :, :], in0=bank(7)[:, 0:512].rearrange("p (h d) -> p h d", h=8), in1=stT[:, :, :], op=ALU.add),
                      reads=[PSK[7], "stT"], writes=["stT"])
            if c > 0:
                S.dve(lambda e, c=c, b2=b2: e.tensor_tensor(out=t1[b2][:, :, :], in0=bank(6)[:, :].rearrange("p (h d) -> p h d", h=8), in1=ea[:, c, :].unsqueeze(2).to_broadcast([128, 8, 64]), op=ALU.mult),
                      reads=[PSK[6], "ea"], writes=["t1%d" % b2])
            if c < NT - 1:
                S.act(lambda e: e.copy(stTb[:, :, :], stT[:, :, :]), reads=["stT"], writes=["stTb"])
            if c > 0:
                S.dve(lambda e, b2=b2, py=py: e.tensor_tensor(out=t1[b2][:, :, :], in0=bank(py)[:, :].rearrange("p (h d) -> p h d", h=8), in1=t1[b2][:, :, :], op=ALU.add),
                      reads=[PSK[py], "t1%d" % b2], writes=["t1%d" % b2])
                S.dve(lambda e, b2=b2: e.tensor_tensor(out=t1[b2][:, :, :], in0=t1[b2][:, :, :], in1=t2[b2][:, :, :], op=ALU.add),
                      reads=["t1%d" % b2, "t2%d" % b2], writes=["t1%d" % b2])
            else:
                S.dve(lambda e, b2=b2, py=py: e.tensor_tensor(out=t1[b2][:, :, :], in0=bank(py)[:, :].rearrange("p (h d) -> p h d", h=8), in1=t2[b2][:, :, :], op=ALU.add),
                      reads=[PSK[py], "t2%d" % b2], writes=["t1%d" % b2])
            S.dve(lambda e, c=c, b2=b2: e.tensor_tensor(out=yg[b2][:, :], in0=t1[b2].rearrange("p h d -> p (h d)"), in1=sz[:, c, :], op=ALU.mult),
                  reads=["t1%d" % b2, "sz.%d" % c], writes=["yg%d" % b2])
            for g in range(2):
                S.act(lambda e, g=g, b2=b2: e.activation(out=junk[:, :], in_=yg[b2][:, g * 256:(g + 1) * 256], func=AF.Square, accum_out=ss[b2][:, g:g + 1]),
                      reads=["yg%d" % b2], writes=["junk", "ss%d.%d" % (b2, g)])
            S.act(lambda e, b2=b2: e.activation(out=rstd[b2][:, :], in_=ss[b2][:, :], func=AF.Ln, bias=RMS_EPS, scale=1.0 / 256.0),
                  reads=["ss%d.0" % b2, "ss%d.1" % b2], writes=["rstd%d" % b2])
            S.act(lambda e, b2=b2: e.activation(out=rstd[b2][:, :], in_=rstd[b2][:, :], func=AF.Exp, scale=-0.5), reads=["rstd%d" % b2], writes=["rstd%d" % b2])
            for g in range(2):
                S.dve(lambda e, g=g, b2=b2, c=c: e.scalar_tensor_tensor(out=m_ssd[:, c, g * 256:(g + 1) * 256], in0=yg[b2][:, g * 256:(g + 1) * 256], scalar=rstd[b2][:, g:g + 1], in1=gssd_bc[:, g * 256:(g + 1) * 256], op0=ALU.mult, op1=ALU.mult),
                      reads=["yg%d" % b2, "rstd%d" % b2, "gssd_bc"], writes=["m_ssd.%d" % c])

        ssd_F(0)
        for c in range(NT):
            if c + 1 < NT:
                ssd_F(c + 1)
            ssd_B(c)

        if dbg == 'ssd' and s == 0:
            RX.reset()
            dm = dbg_out("dbg_mssd", [128, NT * 512])
            cvt = RX.alloc([NT * 512], F32) if dbg == 'ssd' else None
            S.dve(lambda e: e.tensor_copy(cvt[:, :], m_ssd.rearrange("p t d -> p (t d)")), reads=["m_ssd.%d" % c for c in range(NT)], writes=["cvt"])
            outs.append(S.dma("sp", lambda e: e.dma_start(out=dm[:, :], in_=cvt[:, :]), reads=["cvt"]))
        if dbg == "ssd":
            break

        fence()
        RW.reset(); RACC.reset(); TT.reset()
        wB = RW.alloc([8, 1544], BF16)
        wo = RW.alloc([8, 1024], BF16)
        ah = RW.alloc([1024], F32)
        hTf = RW.alloc([8, 128], F32)
        qT = RACC.alloc([4, SEQ], BF16)
        kT = RACC.alloc([4, SEQ], BF16)
        v_aug = RACC.alloc([NT, 8, 65], BF16)
        xres = [RACC.alloc([1024], F32) for _ in range(2)]
        hpre = [RACC.alloc([1024], F32), TT.alloc([1024], F32)]
        f_raw = TT.alloc([NT, 8], F32); Gc = TT.alloc([NT, 8], F32); tot = TT.alloc([NT, 8], F32)
        Pp = TT.alloc([NT, 8], F32); Gf = TT.alloc([NT, 8], F32); Gend = TT.alloc([NT, 8], F32)
        biasT = TT.alloc([8, NT, NT], F32)
        PT = [TT.alloc([8, 128], BF16) for _ in range(2)]
        yatt = [TT.alloc([8, 64], BF16) for _ in range(2)]
        rden = [TT.alloc([8], F32) for _ in range(2)]
        mT = [TT.alloc([8, 128], BF16)] * 2
        bst = [TT.alloc([2, 6], F32) for _ in range(2)]
        mv = [TT.alloc([2], F32) for _ in range(2)]
        rs1 = [TT.alloc([1], F32) for _ in range(2)]

        for half in range(2):
            S.dma("pool", lambda e, half=half: e.dma_start(out=wB[:, 4 * half:4 * half + 4, :], in_=w_in.rearrange("(k p) c -> p k c", p=128)[:, 4 * half:4 * half + 4, 1544:3088]),
                  writes=["wB"])
            S.dma("pool", lambda e, half=half: e.dma_start(out=wo[:, 4 * half:4 * half + 4, :], in_=w_out.rearrange("(k p) c -> p k c", p=128)[:, 4 * half:4 * half + 4, :]),
                  writes=["wo"])
        S.dma("sp", lambda e: e.dma_start(out=lnG[:, :], in_=ln1_g[0:1, :].to_broadcast([128, 1024])), writes=["lnG"])
        S.dma("sp", lambda e: e.dma_start(out=lnB[:, :], in_=ln1_b[0:1, :].to_broadcast([128, 1024])), writes=["lnB"])
        S.pool(lambda e: e.memset(v_aug[:, :, :, 64:65], 1.0), writes=["v_ones"])
        evq = 0
        for qk in range(2):
            dstT = qT if qk == 0 else kT
            bcol = bq if qk == 0 else bk
            nm = "qT" if qk == 0 else "kT"
            for p in range(4):
                for blk in range(4):
                    pq = evq % 4
                    for k in range(8):
                        S.pe(lambda e, pq=pq, k=k, p=p, blk=blk, qk=qk: e.matmul(bank(pq)[:, :], lhsT=wB[:, k, qk * 512 + p * 128:qk * 512 + (p + 1) * 128], rhs=xT[:, k, blk * 512:(blk + 1) * 512], start=(k == 0), stop=(k == 7)),
                             reads=["xT.%d.%d" % (k, blk), "wB"], writes=[PSK[pq]])
                    if evq % 2 == 0:
                        S.act(lambda e, pq=pq, p=p, blk=blk, dstT=dstT, bcol=bcol: e.activation(out=dstT[:, p, blk * 512:(blk + 1) * 512], in_=bank(pq)[:, :], func=AF.Identity, bias=bcol[:, p:p + 1], scale=1.0),
                              reads=[PSK[pq], "bq", "bk"], writes=["%s.%d.%d" % (nm, p, blk)])
                    else:
                        S.dve(lambda e, pq=pq, p=p, blk=blk, dstT=dstT, bcol=bcol: e.tensor_scalar(out=dstT[:, p, blk * 512:(blk + 1) * 512], in0=bank(pq)[:, :], scalar1=bcol[:, p:p + 1], scalar2=None, op0=ALU.add),
                              reads=[PSK[pq], "bq", "bk"], writes=["%s.%d.%d" % (nm, p, blk)])
                    evq += 1
        for t in range(NT):
            pv = 4 + (t % 2)
            blk = t // 4
            for k in range(8):
                S.pe(lambda e, pv=pv, t=t, k=k: e.matmul(bank(pv)[:, :], lhsT=xT[:, k, t * 128:(t + 1) * 128], rhs=wB[:, k, 1024:1536], start=(k == 0), stop=False),
                     reads=["xT.%d.%d" % (k, blk), "wB"], writes=[PSK[pv]])
            S.pe(lambda e, pv=pv: e.matmul(bank(pv)[:, :], lhsT=ones_row[0:1, :], rhs=bv_row[0:1, :], start=False, stop=True),
                 reads=["ones_row", "bv_row"], writes=[PSK[pv]])
            if t % 2 == 0:
                S.act(lambda e, pv=pv, t=t: e.copy(v_aug[:, t, :, 0:64], bank(pv)[:, :].rearrange("p (h d) -> p h d", h=8)), reads=[PSK[pv]], writes=["v.%d" % t])
            else:
                S.dve(lambda e, pv=pv, t=t: e.tensor_copy(v_aug[:, t, :, 0:64], bank(pv)[:, :].rearrange("p (h d) -> p h d", h=8)), reads=[PSK[pv]], writes=["v.%d" % t])
            for k in range(8):
                S.pe(lambda e, t=t, k=k: e.matmul(bank(6)[:, t * 8:(t + 1) * 8], lhsT=xT[:, k, t * 128:(t + 1) * 128], rhs=wB[:, k, 1536:1544], start=(k == 0), stop=(k == 7)),
                     reads=["xT.%d.%d" % (k, blk), "wB"], writes=[PSK[6]])
        S.dve(lambda e: e.tensor_tensor(out=f_raw[:, :, :], in0=bank(6)[:, 0:128].rearrange("p (t h) -> p t h", h=8), in1=bdtf[:, 8:16].unsqueeze(1).to_broadcast([128, NT, 8]), op=ALU.add),
              reads=[PSK[6], "bdtf1"], writes=["f_raw"])
        S.act(lambda e: e.activation(out=f_raw[:, :, :], in_=f_raw[:, :, :], func=AF.Exp, scale=-1.0), reads=["f_raw"], writes=["f_raw"])
        S.act(lambda e: e.activation(out=f_raw[:, :, :], in_=f_raw[:, :, :], func=AF.Ln, bias=1.0, scale=1.0), reads=["f_raw"], writes=["f_raw"])
        frf = f_raw.rearrange("p t h -> p (t h)")
        S.pe(lambda e: e.matmul(bank(7)[:, 0:128], lhsT=Uf[:, :], rhs=frf, start=True, stop=True), reads=["Uf", "f_raw"], writes=[PSK[7]])
        S.pe(lambda e: e.matmul(bank(7)[:, 128:256], lhsT=onesF[:, :], rhs=frf, start=True, stop=True), reads=["onesF", "f_raw"], writes=[PSK[7]])
        S.dve(lambda e: e.tensor_copy(Gc.rearrange("p t h -> p (t h)"), bank(7)[:, 0:128]), reads=[PSK[7]], writes=["Gc"])
        S.dve(lambda e: e.tensor_copy(tot.rearrange("p t h -> p (t h)"), bank(7)[:, 128:256]), reads=[PSK[7]], writes=["tot"])
        S.pool(lambda e: e.memset(Pp[:, 0, :], 0.0), writes=["Pp"])
        for t in range(1, NT):
            S.dve(lambda e, t=t: e.tensor_tensor(out=Pp[:, t, :], in0=Pp[:, t - 1, :], in1=tot[:, t - 1, :], op=ALU.add), reads=["Pp", "tot"], writes=["Pp"])
        S.dve(lambda e: e.tensor_tensor(out=Gf[:, :, :], in0=Gc[:, :, :], in1=Pp[:, :, :], op=ALU.add), reads=["Gc", "Pp"], writes=["Gf"])
        S.dve(lambda e: e.tensor_tensor(out=Gend[:, :, :], in0=tot[:, :, :], in1=Pp[:, :, :], op=ALU.add), reads=["tot", "Pp"], writes=["Gend"])
        for h in range(8):
            S.dve(lambda e, h=h: e.tensor_tensor(out=biasT[:, h, :, :], in0=Gf[:, :, h].unsqueeze(1).to_broadcast([128, NT, NT]), in1=Gend[:, :, h].unsqueeze(2).to_broadcast([128, NT, NT]), op=ALU.subtract),
                  reads=["Gf", "Gend"], writes=["biasT"])

        fence()
        RX.reset()
        hT = RX.alloc([8, SEQ], BF16)
        NEXP = int(os.environ.get("KNEXP", 16))

        def load_expert(ex):
            sl = ex % 2
            S.dma("pool", lambda e, ex=ex, sl=sl: e.dma_start(out=wgu[sl][:, :, 0:512], in_=w_gate[ex].rearrange("(k p) f -> p k f", p=128)), writes=["wg.%d" % sl], nofence=(ex == 0))
            S.dma("pool", lambda e, ex=ex, sl=sl: e.dma_start(out=wgu[sl][:, :, 512:1024], in_=w_up[ex].rearrange("(k p) f -> p k f", p=128)), writes=["wu.%d" % sl], nofence=(ex == 0))
            S.dma("pool", lambda e, ex=ex, sl=sl: e.dma_start(out=wdn[sl][:, :, :], in_=w_down[ex].rearrange("(k p) d -> p k d", p=128)), writes=["wd.%d" % sl], nofence=(ex == 0))


        load_expert(0)
        NTI = int(os.environ.get("KATT_TILES", NT))
        units = []
        for i in range(NTI):
            for p in range(4):
                for j0 in range(0, i + 1, 4):
                    units.append((i, p, list(range(j0, min(j0 + 4, i + 1)))))

        def emit_S(u, gi):
            i, p, js = u
            pb0 = 2 * (gi % 2)
            for jj, j in enumerate(js):
                for hh in range(2):
                    r0 = hh * 64
                    S.pe(lambda e, bk=pb0 + hh, jj=jj, j=j, p=p, r0=r0, i=i: e.matmul(bank(bk)[:, jj * 128:(jj + 1) * 128], lhsT=kT[r0:r0 + 64, p, j * 128:(j + 1) * 128], rhs=qT[r0:r0 + 64, p, i * 128:(i + 1) * 128], start=True, stop=True),
                         reads=["kT.%d.%d" % (p, j // 4), "qT.%d.%d" % (p, i // 4)], writes=[PSK[pb0 + hh]])

        def emit_E(u, gi):
            i, p, js = u
            pb0 = 2 * (gi % 2); pb = gi % 2
            for jj, j in enumerate(js):
                for hh in range(2):
                    h = 2 * p + hh
                    c8 = jj * 2 + hh
                    S.act(lambda e, bk=pb0 + hh, pb=pb, c8=c8, jj=jj, j=j, h=h, i=i: e.activation(out=PT[pb][:, c8, :], in_=bank(bk)[:, jj * 128:(jj + 1) * 128], func=AF.Exp, bias=biasT[:, h, i, j:j + 1], scale=ATT_SCALE),
                          reads=[PSK[pb0 + hh], "biasT"], writes=["PT%d.%d" % (pb, c8)])
                    if j == i:
                        S.dve(lambda e, pb=pb, c8=c8: e.tensor_tensor(out=PT[pb][:, c8, :], in0=PT[pb][:, c8, :], in1=triU[:, :], op=ALU.mult),
                              reads=["PT%d.%d" % (pb, c8), "triU"], writes=["PT%d.%d" % (pb, c8)])

        def emit_V(u, gi):
            i, p, js = u
            pb = gi % 2
            for jj, j in enumerate(js):
                for hh in range(2):
                    h = 2 * p + hh
                    c8 = jj * 2 + hh
                    po = 4 + h // 4; oc = (h % 4) * 65
                    S.pe(lambda e, pb=pb, c8=c8, j=j, h=h, po=po, oc=oc, i=i: e.matmul(bank(po)[:, oc:oc + 65], lhsT=PT[pb][:, c8, :], rhs=v_aug[:, j, h, :], start=(j == 0 and h % 4 == 0), stop=(j == i and h % 4 == 3)),
                         reads=["PT%d.%d" % (pb, c8), "v.%d" % j, "v_ones"], writes=[PSK[po]])

        def tail_N(i):
            b2 = i % 2
            S.dma("sp", lambda e, i=i, b2=b2, s=s: e.dma_start(out=xres[b2][:, :], in_=x[s, i * 128:(i + 1) * 128, :]), writes=["xres%d" % b2])
            for hb in range(2):
                ov = bank(4 + hb)[:, 0:260].rearrange("p (h d) -> p h d", h=4)
                S.dve(lambda e, hb=hb, ov=ov, b2=b2: e.reciprocal(rden[b2][:, 4 * hb:4 * hb + 4], ov[:, :, 64]), reads=[PSK[4 + hb]], writes=["rden%d.%d" % (b2, hb)])
                S.dve(lambda e, hb=hb, ov=ov, b2=b2: e.tensor_tensor(out=yatt[b2][:, 4 * hb:4 * hb + 4, :], in0=ov[:, :, 0:64], in1=rden[b2][:, 4 * hb:4 * hb + 4].unsqueeze(2).to_broadcast([128, 4, 64]), op=ALU.mult),
                      reads=[PSK[4 + hb], "rden%d.%d" % (b2, hb)], writes=["yatt%d.%d" % (b2, hb)])

        def tail_T(i):
            b2 = i % 2
            yf = yatt[b2].rearrange("p h d -> p (h d)")
            for ec in range(8):
                src = m_ssd[:, i, ec * 128:(ec + 1) * 128] if ec < 4 else yf[:, (ec - 4) * 128:(ec - 3) * 128]
                rk = ["m_ssd.%d" % i] if ec < 4 else ["yatt%d.%d" % (b2, (ec - 4) // 2)]
                S.pe(lambda e, ec=ec, src=src: e.transpose(bankb(6)[:, ec * 128:(ec + 1) * 128], src, identB[:, :]), reads=rk + ["identB"], writes=[PSK[6]])
            S.dve(lambda e, b2=b2: e.tensor_copy(mT[b2].rearrange("p a b -> p (a b)"), bankb(6)[:, 0:1024]), reads=[PSK[6]], writes=["mT"])

        def tail_Oq(i, q):
            b2 = i % 2
            half = q // 2
            hp = hpre[b2]
            for ec in range(4 * (q % 2), 4 * (q % 2) + 4):
                S.pe(lambda e, ec=ec, b2=b2, half=half: e.matmul(bank(7)[:, :], lhsT=mT[b2][:, ec, :], rhs=wo[:, ec, half * 512:(half + 1) * 512], start=(ec == 0), stop=(ec == 7)),
                     reads=["mT", "wo"], writes=[PSK[7]])
            if q % 2 == 1:
                S.dve(lambda e, hp=hp, b2=b2, half=half: e.scalar_tensor_tensor(out=hp[:, half * 512:(half + 1) * 512], in0=xres[b2][:, half * 512:(half + 1) * 512], scalar=ALPHA, in1=bank(7)[:, :], op0=ALU.mult, op1=ALU.add),
                      reads=["xres%d" % b2, PSK[7]], writes=["hpre%d.%d" % (b2, half)])
                S.dve(lambda e, hp=hp, half=half, b2=b2: e.bn_stats(bst[b2][:, half, :], hp[:, half * 512:(half + 1) * 512]), reads=["hpre%d.%d" % (b2, half)], writes=["bst%d.%d" % (b2, half)])
            if q == 3:
                S.dve(lambda e, b2=b2: e.bn_aggr(mv[b2][:, :], bst[b2][:, :, :]), reads=["bst%d.0" % b2, "bst%d.1" % b2], writes=["mv%d" % b2])

        def tail_L(i):
            b2 = i % 2
            hp = hpre[b2]
            HK = ["hpre%d.0" % b2, "hpre%d.1" % b2]
            S.act(lambda e, b2=b2: e.activation(out=rs1[b2][:, :], in_=mv[b2][:, 1:2], func=AF.Ln, bias=LN_EPS, scale=1.0), reads=["mv%d" % b2], writes=["rs1%d" % b2])
            S.act(lambda e, b2=b2: e.activation(out=rs1[b2][:, :], in_=rs1[b2][:, :], func=AF.Exp, scale=-0.5), reads=["rs1%d" % b2], writes=["rs1%d" % b2])
            S.dve(lambda e, hp=hp, b2=b2: e.tensor_scalar(out=hp[:, :], in0=hp[:, :], scalar1=mv[b2][:, 0:1], scalar2=rs1[b2][:, 0:1], op0=ALU.subtract, op1=ALU.mult),
                  reads=HK + ["mv%d" % b2, "rs1%d" % b2], writes=HK)
            S.dve(lambda e, hp=hp: e.tensor_tensor(out=hp[:, :], in0=hp[:, :], in1=lnG[:, :], op=ALU.mult), reads=HK + ["lnG"], writes=HK)
            S.dve(lambda e, hp=hp: e.tensor_tensor(out=hp[:, :], in0=hp[:, :], in1=lnB[:, :], op=ALU.add), reads=HK + ["lnB"], writes=HK)
            S.dve(lambda e, hp=hp: e.tensor_scalar(out=ah[:, :], in0=hp[:, :], scalar1=ALPHA, scalar2=None, op0=ALU.mult), reads=HK, writes=["ah"])
            S.dma("sp", lambda e, i=i, s=s: e.dma_start(out=h_scr[s, i * 128:(i + 1) * 128, :], in_=ah[:, :]), reads=["ah"], writes=["h_scr.%d" % i])

        def tail_H(i, half):
            b2 = i % 2
            hp = hpre[b2]
            for q4 in range(4):
                ec = half * 4 + q4
                S.pe(lambda e, q4=q4, ec=ec, hp=hp: e.transpose(bank(6)[:, q4 * 128:(q4 + 1) * 128], hp[:, ec * 128:(ec + 1) * 128], identF[:, :]), reads=["hpre%d.%d" % (b2, half), "identF"], writes=[PSK[6]])
            S.dve(lambda e, half=half, i=i: e.tensor_copy(hT[:, 4 * half:4 * half + 4, i * 128:(i + 1) * 128], bank(6)[:, :].rearrange("p (a b) -> p a b", a=4)), reads=[PSK[6]], writes=["hT.%d.%d" % (i, half)])
            S.dve(lambda e, half=half: e.tensor_copy(hTf[:, 4 * half:4 * half + 4, :], bank(6)[:, :].rearrange("p (a b) -> p a b", a=4)), reads=[PSK[6]], writes=["hTf.%d" % half])

        def tail_R(i, part):
            for ec in range(4 * part, 4 * part + 4):
                S.pe(lambda e, ec=ec: e.matmul(bank(6)[:, 0:20], lhsT=hTf[:, ec, :], rhs=rw[:, ec, :], start=(ec == 0), stop=(ec == 7)), reads=["hTf.%d" % (ec // 4)] + RWK, writes=[PSK[6]])
            if part == 1:
                S.dve(lambda e, i=i: e.tensor_tensor(out=logits[:, i, :], in0=bank(6)[:, 0:20], in1=rb_bc[:, :], op=ALU.add), reads=[PSK[6], "rb0", "rb1"], writes=["logits.%d" % i])

        TD = [int(v) for v in os.environ.get("KTAIL", "0,1,2,3,4,5,9,11,12,14").split(",")]
        TAIL = [(TD[0], tail_N), (TD[1], tail_T), (TD[2], lambda i: tail_Oq(i, 0)), (TD[3], lambda i: tail_Oq(i, 1)), (TD[4], lambda i: tail_Oq(i, 2)), (TD[5], lambda i: tail_Oq(i, 3)),
                (TD[6], tail_L), (TD[7], lambda i: tail_H(i, 0)), (TD[8], lambda i: tail_H(i, 1)), (TD[9], lambda i: (tail_R(i, 0), tail_R(i, 1)))]
        NSTEP = len(TAIL)
        done_steps = set()

        def emit_step(t, k):
            if t < 0 or (t, k) in done_steps:
                return
            for kk in range(k):
                emit_step(t, kk)
            emit_step(t - 1, k)
            if k == 1:
                emit_step(t - 1, 5)
            if k == 7:
                emit_step(t - 1, NSTEP - 1)
            if k == 0:
                for kk in range(NSTEP):
                    emit_step(t - 2, kk)
            done_steps.add((t, k))
            TAIL[k][1](t)

        pending = []
        def tick():
            keep = []
            for ent in pending:
                if ent[0] <= 0:
                    emit_step(ent[1], ent[2])
                else:
                    ent[0] -= 1
                    keep.append(ent)
            pending[:] = keep
        prev = None
        for gi, u in enumerate(units):
            emit_S(u, gi)
            emit_E(u, gi)
            if prev is not None:
                emit_V(prev[0], prev[1])
                if prev[0][0] != u[0]:
                    for k, (dly, fn) in enumerate(TAIL):
                        pending.append([dly, prev[0][0], k])
            tick()
            prev = (u, gi)
        if prev is not None:
            emit_V(prev[0], prev[1])
            for k, (dly, fn) in enumerate(TAIL):
                pending.append([dly, prev[0][0], k])
        while pending:
            tick()

        if dbg == "att" and s == 0:
            fence()
            RACC.reset()
            cvt = RACC.alloc([NT * 1024], BF16)
            d1 = dbg_out("dbg_hT", [128, 8 * SEQ]); d2 = dbg_out("dbg_logits", [128, NT * 20])
            cv2 = RACC.alloc([2 * SEQ], F32)
            for q in range(4):
                S.dve(lambda e, q=q: e.tensor_copy(cv2[:, :], hT[:, 2 * q:2 * q + 2, :].rearrange("p a b -> p (a b)")), reads=[], writes=["cv2"])
                outs.append(S.dma("sp", lambda e, q=q: e.dma_start(out=d1[:, 2 * q * SEQ:(2 * q + 2) * SEQ], in_=cv2[:, :]), reads=["cv2"]))
            outs.append(S.dma("sp", lambda e: e.dma_start(out=d2[:, :], in_=logits.rearrange("p t j -> p (t j)")), reads=["logits.%d" % i for i in range(int(os.environ.get("KATT_TILES", NT)))] if int(os.environ.get("KATT_STAGE", 9)) >= 7 else []))
            break


        fence()
        TT.reset(); RW.reset(); RACC.reset()
        S.dma("sp", lambda e: e.dma_start(out=lnG[:, :], in_=ln2_g[0:1, :].to_broadcast([128, 1024])), writes=["lnG"])
        S.dma("sp", lambda e: e.dma_start(out=lnB[:, :], in_=ln2_b[0:1, :].to_broadcast([128, 1024])), writes=["lnB"])
        LOGK = ["logits.%d" % i for i in range(NT)]
        lg = logits[:, :, 0:4]
        le4 = logits[:, :, 4:20].rearrange("p t (g j) -> p t g j", g=4)
        gmax = TT.alloc([NT], F32); goh = TT.alloc([NT, 4], F32); gex = TT.alloc([NT, 4], F32)
        gsum = TT.alloc([NT], F32); gval = TT.alloc([NT], F32)
        tmp16 = TT.alloc([NT, 4, 4], F32); esel = TT.alloc([NT, 4], F32)
        m1 = TT.alloc([NT], F32); oh1 = TT.alloc([NT, 4], F32); e2 = TT.alloc([NT, 4], F32)
        m2 = TT.alloc([NT], F32); oh2 = TT.alloc([NT, 4], F32); dd = TT.alloc([NT], F32)
        w1 = TT.alloc([NT], F32); w2 = TT.alloc([NT], F32); cw1 = TT.alloc([NT], F32); cw2 = TT.alloc([NT], F32)
        cj = TT.alloc([NT, 4], F32); cj2 = TT.alloc([NT, 4], F32)
        bc4 = lambda a: a.unsqueeze(2).to_broadcast([128, NT, 4])
        S.dve(lambda e: e.tensor_reduce(out=gmax[:, :], in_=lg, axis=AX.X, op=ALU.max), reads=LOGK, writes=["gmax"])
        S.dve(lambda e: e.tensor_tensor(out=goh[:, :, :], in0=lg, in1=bc4(gmax[:, :]), op=ALU.is_equal), reads=LOGK + ["gmax"], writes=["goh"])
        S.dve(lambda e: e.tensor_tensor(out=gex[:, :, :], in0=lg, in1=bc4(gmax[:, :]), op=ALU.subtract), reads=LOGK + ["gmax"], writes=["gex"])
        S.act(lambda e: e.activation(out=gex[:, :, :], in_=gex[:, :, :], func=AF.Exp), reads=["gex"], writes=["gex"])
        S.dve(lambda e: e.tensor_reduce(out=gsum[:, :], in_=gex[:, :, :], axis=AX.X, op=ALU.add), reads=["gex"], writes=["gsum"])
        S.dve(lambda e: e.reciprocal(gval[:, :], gsum[:, :]), reads=["gsum"], writes=["gval"])
        S.dve(lambda e: e.tensor_tensor(out=tmp16[:, :, :, :], in0=le4, in1=goh[:, :, :].unsqueeze(3).to_broadcast([128, NT, 4, 4]), op=ALU.mult), reads=LOGK + ["goh"], writes=["tmp16"])
        S.dve(lambda e: e.tensor_reduce(out=esel[:, :, :], in_=tmp16.rearrange("p t g j -> p t j g"), axis=AX.X, op=ALU.add), reads=["tmp16"], writes=["esel"])
        S.dve(lambda e: e.tensor_reduce(out=m1[:, :], in_=esel[:, :, :], axis=AX.X, op=ALU.max), reads=["esel"], writes=["m1"])
        S.dve(lambda e: e.tensor_tensor(out=oh1[:, :, :], in0=esel[:, :, :], in1=bc4(m1[:, :]), op=ALU.is_equal), reads=["esel", "m1"], writes=["oh1"])
        S.dve(lambda e: e.scalar_tensor_tensor(out=e2[:, :, :], in0=oh1[:, :, :], scalar=-1e30, in1=esel[:, :, :], op0=ALU.mult, op1=ALU.add), reads=["oh1", "esel"], writes=["e2"])
        S.dve(lambda e: e.tensor_reduce(out=m2[:, :], in_=e2[:, :, :], axis=AX.X, op=ALU.max), reads=["e2"], writes=["m2"])
        S.dve(lambda e: e.tensor_tensor(out=oh2[:, :, :], in0=e2[:, :, :], in1=bc4(m2[:, :]), op=ALU.is_equal), reads=["e2", "m2"], writes=["oh2"])
        S.dve(lambda e: e.tensor_tensor(out=dd[:, :], in0=m2[:, :], in1=m1[:, :], op=ALU.subtract), reads=["m1", "m2"], writes=["dd"])
        S.act(lambda e: e.activation(out=dd[:, :], in_=dd[:, :], func=AF.Exp), reads=["dd"], writes=["dd"])
        S.dve(lambda e: e.tensor_scalar(out=w1[:, :], in0=dd[:, :], scalar1=1.0, scalar2=None, op0=ALU.add), reads=["dd"], writes=["w1"])
        S.dve(lambda e: e.reciprocal(w1[:, :], w1[:, :]), reads=["w1"], writes=["w1"])
        S.dve(lambda e: e.tensor_tensor(out=w2[:, :], in0=dd[:, :], in1=w1[:, :], op=ALU.mult), reads=["dd", "w1"], writes=["w2"])
        S.dve(lambda e: e.tensor_tensor(out=cw1[:, :], in0=gval[:, :], in1=w1[:, :], op=ALU.mult), reads=["gval", "w1"], writes=["cw1"])
        S.dve(lambda e: e.tensor_tensor(out=cw2[:, :], in0=gval[:, :], in1=w2[:, :], op=ALU.mult), reads=["gval", "w2"], writes=["cw2"])
        S.dve(lambda e: e.tensor_tensor(out=cj[:, :, :], in0=oh1[:, :, :], in1=bc4(cw1[:, :]), op=ALU.mult), reads=["oh1", "cw1"], writes=["cj"])
        S.dve(lambda e: e.tensor_tensor(out=cj2[:, :, :], in0=oh2[:, :, :], in1=bc4(cw2[:, :]), op=ALU.mult), reads=["oh2", "cw2"], writes=["cj2"])
        S.dve(lambda e: e.tensor_tensor(out=cj[:, :, :], in0=cj[:, :, :], in1=cj2[:, :, :], op=ALU.add), reads=["cj", "cj2"], writes=["cj"])
        S.dve(lambda e: e.tensor_tensor(out=comb.rearrange("p t (g j) -> p t g j", g=4), in0=goh[:, :, :].unsqueeze(3).to_broadcast([128, NT, 4, 4]), in1=cj[:, :, :].unsqueeze(2).to_broadcast([128, NT, 4, 4]), op=ALU.mult),
              reads=["goh", "cj"], writes=["comb"])

        acc = RACC.alloc([NT, 1024], F32)
        sg = [TT.alloc([512], BF16) for _ in range(2)]
        actT = [TT.alloc([4, 512], BF16) for _ in range(2)]
        obuf = [TT.alloc([1024], F32) for _ in range(2)]
        bst2 = [TT.alloc([2, 6], F32) for _ in range(2)]
        mv2 = [TT.alloc([2], F32) for _ in range(2)]
        rs2 = [TT.alloc([1], F32) for _ in range(2)]
        for q in range(4):
            S.dma("sp", lambda e, q=q, s=s: e.dma_start(out=acc[:, 4 * q:4 * q + 4, :], in_=h_scr[s, q * 512:(q + 1) * 512, :].rearrange("(t p) d -> p t d", p=128)),
                  reads=["h_scr.%d" % t for t in range(4 * q, 4 * q + 4)], writes=["acc.%d" % t for t in range(4 * q, 4 * q + 4)])
        def ln2_stats(t):
            b2 = t % 2
            for c2 in range(2):
                S.dve(lambda e, c2=c2, t=t, b2=b2: e.bn_stats(bst2[b2][:, c2, :], acc[:, t, c2 * 512:(c2 + 1) * 512]), reads=["acc.%d" % t], writes=["bst2%d.%d" % (b2, c2)])
            S.dve(lambda e, b2=b2: e.bn_aggr(mv2[b2][:, :], bst2[b2][:, :, :]), reads=["bst2%d.0" % b2, "bst2%d.1" % b2], writes=["mv2%d" % b2])

        def ln2_apply(t):
            b2 = t % 2
            S.act(lambda e, b2=b2: e.activation(out=rs2[b2][:, :], in_=mv2[b2][:, 1:2], func=AF.Ln, bias=LN_EPS, scale=1.0), reads=["mv2%d" % b2], writes=["rs2%d" % b2])
            S.act(lambda e, b2=b2: e.activation(out=rs2[b2][:, :], in_=rs2[b2][:, :], func=AF.Exp, scale=-0.5), reads=["rs2%d" % b2], writes=["rs2%d" % b2])
            S.dve(lambda e, t=t, b2=b2: e.tensor_scalar(out=obuf[b2][:, :], in0=acc[:, t, :], scalar1=mv2[b2][:, 0:1], scalar2=rs2[b2][:, 0:1], op0=ALU.subtract, op1=ALU.mult),
                  reads=["acc.%d" % t, "mv2%d" % b2, "rs2%d" % b2], writes=["obuf%d" % b2])
            S.dve(lambda e, b2=b2: e.tensor_tensor(out=obuf[b2][:, :], in0=obuf[b2][:, :], in1=lnG[:, :], op=ALU.mult), reads=["obuf%d" % b2, "lnG"], writes=["obuf%d" % b2])
            S.dve(lambda e, b2=b2: e.tensor_tensor(out=obuf[b2][:, :], in0=obuf[b2][:, :], in1=lnB[:, :], op=ALU.add), reads=["obuf%d" % b2, "lnB"], writes=["obuf%d" % b2])
            outs.append(S.dma("sp", lambda e, t=t, b2=b2, s=s: e.dma_start(out=out[s, t * 128:(t + 1) * 128, :], in_=obuf[b2][:, :]), reads=["obuf%d" % b2], writes=["out.%d.%d" % (s, t)]))

        ln2_prev = []
        if NEXP > 1:
            load_expert(1)
        cg = 0; cd = 0
        for ex in range(NEXP):
            sl = ex % 2
            for tb in range(4):
                ab = (ex * 4 + tb) % 2
                hk = ["hT.%d.%d" % (i, hf) for i in range(4 * tb, 4 * tb + 4) for hf in range(2)]
                for fc in range(4):
                    pg = cg % 2; cg += 1
                    for k in range(8):
                        S.pe(lambda e, pg=pg, k=k, fc=fc, tb=tb, sl=sl: e.matmul(bank(pg)[:, :], lhsT=wgu[sl][:, k, fc * 128:(fc + 1) * 128], rhs=hT[:, k, tb * 512:(tb + 1) * 512], start=(k == 0), stop=(k == 7)),
                             reads=["wg.%d" % sl] + hk, writes=[PSK[pg]])
                    for k in range(8):
                        S.pe(lambda e, pg=pg, k=k, fc=fc, tb=tb, sl=sl: e.matmul(bank(2 + pg)[:, :], lhsT=wgu[sl][:, k, 512 + fc * 128:512 + (fc + 1) * 128], rhs=hT[:, k, tb * 512:(tb + 1) * 512], start=(k == 0), stop=(k == 7)),
                             reads=["wu.%d" % sl] + hk, writes=[PSK[2 + pg]])
                    S.act(lambda e, pg=pg: e.activation(out=sg[pg][:, :], in_=bank(pg)[:, :], func=AF.Silu), reads=[PSK[pg]], writes=["sg%d" % pg])
                    S.dve(lambda e, pg=pg, ab=ab, fc=fc: e.tensor_tensor(out=actT[ab][:, fc, :], in0=bank(2 + pg)[:, :], in1=sg[pg][:, :], op=ALU.mult),
                          reads=[PSK[2 + pg], "sg%d" % pg], writes=["actT%d.%d" % (ab, fc)])
                for tt in range(4):
                    t = tb * 4 + tt
                    pdi = 2 + (cd % 2); cd += 1
                    for half in range(2):
                        for fc in range(4):
                            S.pe(lambda e, pdi=pdi, half=half, fc=fc, ab=ab, tt=tt, sl=sl: e.matmul(pd[pdi][:, half * 512:(half + 1) * 512], lhsT=actT[ab][:, fc, tt * 128:(tt + 1) * 128], rhs=wdn[sl][:, fc, half * 512:(half + 1) * 512], start=(fc == 0), stop=(fc == 3)),
                                 reads=["actT%d.%d" % (ab, fc), "wd.%d" % sl], writes=[PSK[2 * pdi + half]])
                    S.dve(lambda e, pdi=pdi, t=t, ex=ex: e.scalar_tensor_tensor(out=acc[:, t, :], in0=pd[pdi][:, :], scalar=comb[:, t, ex:ex + 1], in1=acc[:, t, :], op0=ALU.mult, op1=ALU.add),
                          reads=[PSK[2 * pdi], PSK[2 * pdi + 1], "comb", "acc.%d" % t], writes=["acc.%d" % t])
                    if ex == NEXP - 1:
                        ln2_stats(t)
                        if ln2_prev:
                            ln2_apply(ln2_prev.pop())
                        ln2_prev.append(t)
            if ex + 2 < NEXP:
                load_expert(ex + 2)
        while ln2_prev:
            ln2_apply(ln2_prev.pop())
        if s + 1 < NSEQ:
            fence()
        if dbg == "one":
            break

    with nc.allow_non_contiguous_dma(reason="tiny constant loads"):
        st = S.emit(outs)
    return nc, st, dbg_t


_CACHE = {}


def _get_program():
    if "p" not in _CACHE:
        _CACHE["p"] = build_program(dbg=os.environ.get("KDBG", ""))
    return _CACHE["p"]


def kernel(**inputs):
    nc, st, dbg_t = _get_program()
    f = lambda a: np.ascontiguousarray(np.asarray(a, dtype=np.float32))
    x = f(inputs["x"])
    shared = {
        "w_in": f(inputs["w_in"])[0], "b_in": f(inputs["b_in"]).reshape(1, DIN),
        "conv_w": f(inputs["conv_w"])[0], "conv_b": f(inputs["conv_b"]).reshape(1, 1024),
        "a_log": f(inputs["a_log"]).reshape(1, 8), "d_skip": f(inputs["d_skip"]).reshape(1, 8),
        "ssd_norm_g": f(inputs["ssd_norm_g"]).reshape(1, 512), "w_out": f(inputs["w_out"])[0],
        "ln1_g": f(inputs["ln1_g"]).reshape(1, DM), "ln1_b": f(inputs["ln1_b"]).reshape(1, DM),
        "router_group_w": f(inputs["router_group_w"])[0], "router_group_b": f(inputs["router_group_b"]).reshape(1, 4),
        "router_expert_w": f(inputs["router_expert_w"])[0], "router_expert_b": f(inputs["router_expert_b"]).reshape(1, 16),
        "w_gate": f(inputs["w_gate"])[0], "w_up": f(inputs["w_up"])[0], "w_down": f(inputs["w_down"])[0],
        "ln2_g": f(inputs["ln2_g"]).reshape(1, DM), "ln2_b": f(inputs["ln2_b"]).reshape(1, DM),
    }
    ncores = int(os.environ.get("KCORES", NCORES))
    in_maps = []
    for c in range(ncores):
        m = dict(shared)
        m["x"] = np.ascontiguousarray(x[c * NSEQ:(c + 1) * NSEQ])
        in_maps.append(m)
    res = run_bass_kernel_spmd(nc, in_maps, core_ids=list(range(ncores)))
    if os.environ.get("KDBG", ""):
        _CACHE["dbg"] = res.results
    outp = np.concatenate([r["out"] for r in res.results], axis=0)
    return outp.astype(np.float32)
```

```python
import os
import numpy as np
import concourse.bass as bass
import concourse.mybir as mybir
from concourse.bass_utils import run_bass_kernel_spmd

F32 = mybir.dt.float32
BF16 = mybir.dt.bfloat16
U8 = mybir.dt.uint8
AF = mybir.ActivationFunctionType
ALU = mybir.AluOpType
AX = mybir.AxisListType

NCORES = 8
NSEQ = 2
SEQ = 2048
NT = 16
DM = 1024
DIN = 3088
ALPHA = float(2.0 ** 0.25)
LN_EPS = 1e-5
RMS_EPS = 1e-5
ATT_SCALE = 0.125
NEG = -30000.0


class Op:
    __slots__ = ("eng", "fn", "reads", "writes", "deps", "signal", "sigval", "dma", "gi", "nofence")


class Sched:
    COMPUTE = ("pe", "act", "dve", "pool")

    def __init__(self, nc, n_dma_sems=40):
        self.nc = nc
        self.h = {"pe": nc.tensor, "act": nc.scalar, "dve": nc.vector, "pool": nc.gpsimd, "sp": nc.sync}
        self.ops = []
        self.last_w = {}
        self.readers = {}
        self.n_dma_sems = n_dma_sems
        self.live_dma = []
        self.nfence = 0

    def add(self, eng, fn, reads=(), writes=(), dma=False, nofence=False):
        o = Op()
        o.eng = eng; o.fn = fn; o.reads = tuple(reads); o.writes = tuple(writes)
        o.deps = []; o.signal = False; o.sigval = None; o.dma = dma; o.gi = len(self.ops); o.nofence = nofence
        for r in o.reads:
            p = self.last_w.get(r)
            if p is not None:
                self._dep(o, p, True)
            if r.startswith("ps"):
                rd = self.readers.get(r)
                if rd:
                    for q in rd.values():
                        if q.eng != eng:
                            self._dep(o, q, True)
        for w in o.writes:
            p = self.last_w.get(w)
            if p is not None:
                self._dep(o, p, False)
            rd = self.readers.get(w)
            if rd:
                for q in rd.values():
                    self._dep(o, q, False)
        for r in o.reads:
            d = self.readers.setdefault(r, {})
            d[("dma", o.gi) if dma else eng] = o
        for w in o.writes:
            self.last_w[w] = o
            self.readers[w] = {}
        self.ops.append(o)
        if dma and not nofence:
            self.live_dma.append(o)
        return o

    def _dep(self, o, p, raw):
        if p is o:
            return
        if (not p.dma) and (not o.dma) and p.eng == o.eng:
            if o.eng == "pe":
                return
        o.deps.append(p)
        p.signal = True

    def pe(self, fn, reads=(), writes=()): return self.add("pe", fn, reads, writes)
    def act(self, fn, reads=(), writes=()): return self.add("act", fn, reads, writes)
    def dve(self, fn, reads=(), writes=()): return self.add("dve", fn, reads, writes)
    def pool(self, fn, reads=(), writes=()): return self.add("pool", fn, reads, writes)
    def dma(self, q, fn, reads=(), writes=(), nofence=False):
        return self.add(q, fn, reads, writes, dma=True, nofence=nofence)

    def fence(self, scratch):
        n = self.nfence; self.nfence += 1
        a_keys = []
        col = {"pe": None, "act": 0, "dve": 1, "pool": 2}
        for e in ("act", "dve", "pool"):
            k = "fenceA.%d.%s" % (n, e)
            c = col[e]
            if e == "act":
                self.add(e, (lambda eh, c=c: eh.activation(out=scratch[:, c:c + 1], in_=scratch[:, 8:9], func=AF.Copy)), reads=(), writes=(k,))
            else:
                self.add(e, (lambda eh, c=c: eh.memset(scratch[:, c:c + 1], 0.0)), reads=(), writes=(k,))
            a_keys.append(k)
        k = "fenceA.%d.pe" % n
        self.add("pe", (lambda eh: eh.matmul(self.fence_ps[0:1, 0:1], lhsT=self.fence_w[0:1, 0:1], rhs=self.fence_w[0:1, 0:1], start=True, stop=True)),
                 reads=(), writes=(k, "ps7"))
        a_keys.append(k)
        dmas = self.live_dma
        self.live_dma = []
        for e in ("act", "dve", "pool", "pe", "sp"):
            kb = "fenceB.%d.%s" % (n, e)
            if e == "act":
                o = self.add(e, (lambda eh: eh.activation(out=scratch[:, 3:4], in_=scratch[:, 8:9], func=AF.Copy)), reads=a_keys, writes=(kb,))
            elif e == "pe":
                o = self.add(e, (lambda eh: eh.matmul(self.fence_ps[0:1, 1:2], lhsT=self.fence_w[0:1, 0:1], rhs=self.fence_w[0:1, 0:1], start=True, stop=True)),
                             reads=a_keys, writes=(kb, "ps7"))
            elif e == "sp":
                o = self.add(e, (lambda eh: eh.nop()), reads=a_keys, writes=(kb,))
            else:
                c = 4 if e == "dve" else 5
                o = self.add(e, (lambda eh, c=c: eh.memset(scratch[:, c:c + 1], 0.0)), reads=a_keys, writes=(kb,))
            for d in dmas:
                o.deps.append(d)

    def emit(self, final_wait_ops=()):
        nc = self.nc
        esem = {e: nc.alloc_semaphore("s_" + e) for e in self.COMPUTE}
        dsems = [nc.alloc_semaphore("s_dma%d" % i) for i in range(self.n_dma_sems)]
        dtotal = [0] * self.n_dma_sems
        dlast = [None] * self.n_dma_sems
        ecount = {e: 0 for e in self.COMPUTE}
        nd = 0
        nq = {"sp": 0, "pool": 0}
        half = self.n_dma_sems // 2
        for o in self.ops:
            if o.dma:
                qi = nq[o.eng]; nq[o.eng] += 1; nd += 1
                i = (qi % half) + (0 if o.eng == "sp" else half)
                prev = dlast[i]
                if prev is not None:
                    o.deps.append(prev)
                dtotal[i] += 16
                o.sigval = (dsems[i], dtotal[i], 1000 + i)
                dlast[i] = o
            elif o.signal:
                ecount[o.eng] += 1
                o.sigval = (esem[o.eng], ecount[o.eng], o.eng)
        known = {e: {} for e in self.h}
        nwaits = 0
        for o in self.ops:
            eh = self.h[o.eng]
            kn = known[o.eng]
            need = {}
            for p in o.deps:
                s, v, key = p.sigval
                if kn.get(key, 0) >= v:
                    continue
                if key not in need or need[key][1] < v:
                    need[key] = (s, v)
            for key, (s, v) in need.items():
                eh.wait_ge(s, v)
                kn[key] = v
                nwaits += 1
            ins = o.fn(eh)
            if o.dma:
                ins.then_inc(o.sigval[0], 16)
            elif o.signal:
                ins.then_inc(o.sigval[0], 1)
        eh = self.h["sp"]
        for o in final_wait_ops:
            s, v, key = o.sigval
            eh.wait_ge(s, v)
        self.stats = dict(n_ops=len(self.ops), n_waits=nwaits, counts=dict(ecount), n_dma=nd)
        return self.stats


class Arena:
    def __init__(self, nc, name, nbytes):
        self.t = nc.alloc_sbuf_tensor(name, [128, nbytes], U8)
        self.n = nbytes
        self.off = 0

    def reset(self, off=0):
        self.off = off

    def alloc(self, shape, dtype, parts=128):
        esz = 2 if dtype == BF16 else 4
        n = esz
        for s in shape:
            n *= s
        off = (self.off + 31) // 32 * 32
        assert off + n <= self.n, (off, n, self.n)
        self.off = off + n
        flat = self.t[0:parts, off:off + n].bitcast(dtype)
        if len(shape) == 1:
            return flat
        names = " ".join("a%d" % i for i in range(len(shape)))
        kw = {"a%d" % i: shape[i] for i in range(1, len(shape))}
        return flat.rearrange("p (%s) -> p %s" % (names, names), **kw)


def build_program(dbg=False):
    nc = bass.Bass("TRN2", target_bir_lowering=False)
    S = Sched(nc)
    D = {}

    def din(name, shape):
        D[name] = nc.dram_tensor(name, list(shape), F32, kind="ExternalInput").ap()
        return D[name]

    x = din("x", [NSEQ, SEQ, DM])
    w_in = din("w_in", [DM, DIN])
    b_in = din("b_in", [1, DIN])
    conv_w = din("conv_w", [4, 1024])
    conv_b = din("conv_b", [1, 1024])
    a_log = din("a_log", [1, 8])
    d_skip = din("d_skip", [1, 8])
    ssd_g = din("ssd_norm_g", [1, 512])
    w_out = din("w_out", [DM, DM])
    ln1_g = din("ln1_g", [1, DM]); ln1_b = din("ln1_b", [1, DM])
    rg_w = din("router_group_w", [DM, 4]); rg_b = din("router_group_b", [1, 4])
    re_w = din("router_expert_w", [4, DM, 4]); re_b = din("router_expert_b", [1, 16])
    w_gate = din("w_gate", [16, DM, 512]); w_up = din("w_up", [16, DM, 512]); w_down = din("w_down", [16, 512, DM])
    ln2_g = din("ln2_g", [1, DM]); ln2_b = din("ln2_b", [1, DM])
    out = nc.dram_tensor("out", [NSEQ, SEQ, DM], F32, kind="ExternalOutput").ap()
    h_scr = nc.dram_tensor("h_scr", [NSEQ, SEQ, DM], F32).ap()
    dbg_t = {}

    def dbg_out(name, shape):
        dbg_t[name] = nc.dram_tensor(name, list(shape), F32, kind="ExternalOutput").ap()
        return dbg_t[name]

    CONST = Arena(nc, "CONST", 22 * 1024)
    RW = Arena(nc, "RW", 49408)
    RX = Arena(nc, "RX", 32768)
    RACC = Arena(nc, "RACC", 65536)
    MSSD = Arena(nc, "MSSD", 16384)
    TT = Arena(nc, "TT", nc.sbuf_bytes_remaining - 256)

    identB = CONST.alloc([128], BF16); identF = CONST.alloc([128], F32)
    Uf = CONST.alloc([128], F32); triU = CONST.alloc([128], BF16); maskneg = CONST.alloc([128], BF16)
    onesF = CONST.alloc([128], F32)
    ones_row = CONST.alloc([128], BF16, parts=1)
    bz_row = CONST.alloc([512], BF16, parts=1); bv_row = CONST.alloc([512], BF16, parts=1)
    bdtf = CONST.alloc([16], F32)
    bxbc = CONST.alloc([8], F32); bq = CONST.alloc([4], F32); bk = CONST.alloc([4], F32)
    convw = CONST.alloc([8, 4], F32); convb = CONST.alloc([8], F32)
    a_bc = CONST.alloc([8], F32); dskip_bc = CONST.alloc([8], F32)
    gssd_bc = CONST.alloc([512], F32)
    lnG = CONST.alloc([1024], F32); lnB = CONST.alloc([1024], F32)
    rw = CONST.alloc([8, 20], F32); rb_bc = CONST.alloc([20], F32)
    logits = CONST.alloc([NT, 20], F32); comb = CONST.alloc([NT, 16], F32)
    fsc = CONST.alloc([16], F32)
    S.fence_w = CONST.alloc([8], BF16)
    pd = [nc.alloc_psum_tensor("pd%d" % i, [128, 1024], F32) for i in range(4)]
    def bank(i):
        return pd[i // 2][:, (i % 2) * 512:(i % 2) * 512 + 512]
    def bankb(i):
        return bank(i).bitcast(BF16)
    PSK = ["ps%d" % i for i in range(8)]
    S.fence_ps = nc.alloc_sbuf_tensor("fence_dummy", [1, 8], F32)
    S.fence_ps = bank(7)[:, 504:512]

    def fence():
        S.fence(fsc)

    S.pool(lambda e: e.memset(fsc[:, :], 0.0), writes=["fsc"])
    S.pool(lambda e: e.memset(S.fence_w[:, :], 0.0), writes=["fence_w"])
    S.pool(lambda e: e.memset(identB[:, :], 1.0), writes=["identB"])
    S.pool(lambda e: e.affine_select(out=identB[:, :], in_=identB[:, :], pattern=[[-1, 128]], compare_op=ALU.is_equal, fill=0.0, base=0, channel_multiplier=1), reads=["identB"], writes=["identB"])
    S.pool(lambda e: e.memset(identF[:, :], 1.0), writes=["identF"])
    S.pool(lambda e: e.affine_select(out=identF[:, :], in_=identF[:, :], pattern=[[-1, 128]], compare_op=ALU.is_equal, fill=0.0, base=0, channel_multiplier=1), reads=["identF"], writes=["identF"])
    S.pool(lambda e: e.memset(Uf[:, :], 1.0), writes=["Uf"])
    S.pool(lambda e: e.affine_select(out=Uf[:, :], in_=Uf[:, :], pattern=[[1, 128]], compare_op=ALU.is_ge, fill=0.0, base=0, channel_multiplier=-1), reads=["Uf"], writes=["Uf"])
    S.pool(lambda e: e.memset(triU[:, :], 1.0), writes=["triU"])
    S.pool(lambda e: e.affine_select(out=triU[:, :], in_=triU[:, :], pattern=[[1, 128]], compare_op=ALU.is_ge, fill=0.0, base=0, channel_multiplier=-1), reads=["triU"], writes=["triU"])
    S.pool(lambda e: e.memset(maskneg[:, :], NEG), writes=["maskneg"])
    S.pool(lambda e: e.affine_select(out=maskneg[:, :], in_=maskneg[:, :], pattern=[[-1, 128]], compare_op=ALU.is_gt, fill=0.0, base=0, channel_multiplier=1), reads=["maskneg"], writes=["maskneg"])
    S.pool(lambda e: e.memset(onesF[:, :], 1.0), writes=["onesF"])
    S.pool(lambda e: e.memset(ones_row[:, :], 1.0), writes=["ones_row"])
    S.dma("pool", lambda e: e.dma_start(out=bz_row[:, :], in_=b_in[0:1, 0:512]), writes=["bz_row"])
    S.dma("pool", lambda e: e.dma_start(out=bv_row[:, :], in_=b_in[0:1, 2568:3080]), writes=["bv_row"])
    S.dma("sp", lambda e: e.dma_start(out=bdtf[:, 0:8], in_=b_in[0:1, 1536:1544].to_broadcast([128, 8])), writes=["bdtf0"])
    S.dma("sp", lambda e: e.dma_start(out=bdtf[:, 8:16], in_=b_in[0:1, 3080:3088].to_broadcast([128, 8])), writes=["bdtf1"])
    S.dma("sp", lambda e: e.dma_start(out=bxbc[:, :], in_=b_in[0, 512:1536].rearrange("(c p) -> p c", p=128)), writes=["bxbc"])
    S.dma("sp", lambda e: e.dma_start(out=bq[:, :], in_=b_in[0, 1544:2056].rearrange("(c p) -> p c", p=128)), writes=["bq"])
    S.dma("sp", lambda e: e.dma_start(out=bk[:, :], in_=b_in[0, 2056:2568].rearrange("(c p) -> p c", p=128)), writes=["bk"])
    for k in range(4):
        S.dma("sp", lambda e, k=k: e.dma_start(out=convw[:, :, k], in_=conv_w[k, :].rearrange("(c p) -> p c", p=128)), writes=["convw%d" % k])
    CONVW = ["convw%d" % k for k in range(4)]
    S.dma("sp", lambda e: e.dma_start(out=convb[:, :], in_=conv_b[0, :].rearrange("(c p) -> p c", p=128)), writes=["convb"])
    S.dma("sp", lambda e: e.dma_start(out=a_bc[:, :], in_=a_log[0:1, :].to_broadcast([128, 8])), writes=["a_bc"])
    S.act(lambda e: e.activation(out=a_bc[:, :], in_=a_bc[:, :], func=AF.Exp), reads=["a_bc"], writes=["a_bc"])
    S.dve(lambda e: e.tensor_scalar(out=a_bc[:, :], in0=a_bc[:, :], scalar1=-1.0, scalar2=None, op0=ALU.mult), reads=["a_bc"], writes=["a_bc"])
    S.dma("sp", lambda e: e.dma_start(out=dskip_bc[:, :], in_=d_skip[0:1, :].to_broadcast([128, 8])), writes=["dskip_bc"])
    S.dma("sp", lambda e: e.dma_start(out=gssd_bc[:, :], in_=ssd_g[0:1, :].to_broadcast([128, 512])), writes=["gssd_bc"])
    S.dma("sp", lambda e: e.dma_start(out=rw[:, :, 0:4], in_=rg_w.rearrange("(k p) j -> p k j", p=128)), writes=["rw0"])
    for g in range(4):
        S.dma("sp", lambda e, g=g: e.dma_start(out=rw[:, :, 4 + 4 * g:8 + 4 * g], in_=re_w[g].rearrange("(k p) j -> p k j", p=128)), writes=["rw%d" % (g + 1)])
    RWK = ["rw%d" % i for i in range(5)]
    S.dma("sp", lambda e: e.dma_start(out=rb_bc[:, 0:4], in_=rg_b[0:1, :].to_broadcast([128, 4])), writes=["rb0"])
    S.dma("sp", lambda e: e.dma_start(out=rb_bc[:, 4:20], in_=re_b[0:1, :].to_broadcast([128, 16])), writes=["rb1"])

    outs = []

    for s in range(NSEQ):
        RW.reset(); RX.reset(); RACC.reset(); MSSD.reset(); TT.reset()
        wgu = [None, None]; wdn = [None, None]
        wgu[0] = RW.alloc([8, 1024], BF16); wdn[0] = RW.alloc([4, 1024], BF16)
        wgu[1] = RW.alloc([8, 1024], BF16); wdn[1] = RW.alloc([4, 1024], BF16)
        RW.reset()
        wA = RW.alloc([8, 1544], BF16)
        xb = [RW.alloc([4, 1024], BF16) for _ in range(2)]
        xT = RX.alloc([8, SEQ], BF16)
        sz = RACC.alloc([NT, 512], BF16)
        xsB = RACC.alloc([NT, 768], BF16)
        BT = RACC.alloc([2, SEQ], BF16)
        CT = RACC.alloc([2, SEQ], BF16)
        xsT = MSSD.alloc([4, SEQ], BF16)
        dt_t = TT.alloc([NT, 8], F32)
        tt_mark = TT.off
        pre = [TT.alloc([SEQ + 3], BF16) for _ in range(2)]
        diagw = TT.alloc([8, 4, 128], BF16)
        dt_raw = TT.alloc([NT, 8], F32)

        w_in_v = w_in.rearrange("(k p) c -> p k c", p=128)
        S.dma("pool", lambda e, s=s: e.dma_start(out=xb[0][:, :, :], in_=x[s, 0:512, :].rearrange("(t p) d -> p t d", p=128)), writes=["xb0"])
        S.dma("pool", lambda e: e.dma_start(out=wA[:, :, 0:512], in_=w_in_v[:, :, 0:512]), writes=["wA.z"])
        S.dma("pool", lambda e: e.dma_start(out=wA[:, :, 1536:1544], in_=w_in_v[:, :, 1536:1544]), writes=["wA.dt"])
        S.dma("pool", lambda e, s=s: e.dma_start(out=xb[1][:, :, :], in_=x[s, 512:1024, :].rearrange("(t p) d -> p t d", p=128)), writes=["xb1"])
        for half in range(2):
            S.dma("pool", lambda e, half=half: e.dma_start(out=wA[:, 4 * half:4 * half + 4, 512:1536], in_=w_in_v[:, 4 * half:4 * half + 4, 512:1536]), writes=["wA.x%d" % half])
        for b in range(2):
            S.pool(lambda e, b=b: e.memset(pre[b][:, 0:3], 0.0), writes=["prepad%d" % b])
        ev = 0
        for blk in range(4):
            if blk >= 2:
                S.dma("pool", lambda e, blk=blk, s=s: e.dma_start(out=xb[blk % 2][:, :, :], in_=x[s, blk * 512:(blk + 1) * 512, :].rearrange("(t p) d -> p t d", p=128)),
                      writes=["xb%d" % (blk % 2)])
            for k in range(8):
                pb = (blk * 8 + k) % 2
                for t in range(4):
                    S.pe(lambda e, pb=pb, t=t, k=k, blk=blk: e.transpose(bankb(pb)[:, t * 128:(t + 1) * 128], xb[blk % 2][:, t, k * 128:(k + 1) * 128], identB[:, :]),
                         reads=["xb%d" % (blk % 2), "identB"], writes=[PSK[pb]])
                if ev % 2 == 0:
                    S.act(lambda e, pb=pb, k=k, blk=blk: e.copy(xT[:, k, blk * 512:(blk + 1) * 512], bankb(pb)[:, 0:512]), reads=[PSK[pb]], writes=["xT.%d.%d" % (k, blk)])
                else:
                    S.dve(lambda e, pb=pb, k=k, blk=blk: e.tensor_copy(xT[:, k, blk * 512:(blk + 1) * 512], bankb(pb)[:, 0:512]), reads=[PSK[pb]], writes=["xT.%d.%d" % (k, blk)])
                ev += 1
            for tt in range(4):
                t = blk * 4 + tt
                pz = 2 + (t % 2)
                xk = ["xT.%d.%d" % (k, blk) for k in range(8)]
                for k in range(8):
                    S.pe(lambda e, pz=pz, t=t, k=k: e.matmul(bank(pz)[:, :], lhsT=xT[:, k, t * 128:(t + 1) * 128], rhs=wA[:, k, 0:512], start=(k == 0), stop=False),
                         reads=[xk[k], "wA.z"], writes=[PSK[pz]])
                S.pe(lambda e, pz=pz: e.matmul(bank(pz)[:, :], lhsT=ones_row[0:1, :], rhs=bz_row[0:1, :], start=False, stop=True),
                     reads=["ones_row", "bz_row"], writes=[PSK[pz]])
                S.act(lambda e, pz=pz, t=t: e.activation(out=sz[:, t, :], in_=bank(pz)[:, :], func=AF.Silu), reads=[PSK[pz]], writes=["sz.%d" % t])
                for k in range(8):
                    S.pe(lambda e, t=t, k=k: e.matmul(bank(4)[:, t * 8:(t + 1) * 8], lhsT=xT[:, k, t * 128:(t + 1) * 128], rhs=wA[:, k, 1536:1544], start=(k == 0), stop=(k == 7)),
                         reads=[xk[k], "wA.dt"], writes=[PSK[4]])
        S.dve(lambda e: e.tensor_tensor(out=dt_raw[:, :, :], in0=bank(4)[:, 0:128].rearrange("p (t h) -> p t h", h=8), in1=bdtf[:, 0:8].unsqueeze(1).to_broadcast([128, NT, 8]), op=ALU.add),
              reads=[PSK[4], "bdtf0"], writes=["dt_raw"])
        for c in range(8):
            for k in range(4):
                S.dve(lambda e, c=c, k=k: e.tensor_scalar(out=diagw[:, c, k, :], in0=identB[:, :], scalar1=convw[:, c, k:k + 1], scalar2=None, op0=ALU.mult),
                      reads=["identB"] + CONVW, writes=["diagw.%d" % c])

        def a1_ip(c):
            pb_ = c % 2
            for blk in range(4):
                pc = 5 + (c * 4 + blk) % 2
                for k in range(8):
                    S.pe(lambda e, pc=pc, c=c, k=k, blk=blk: e.matmul(bank(pc)[:, :], lhsT=wA[:, k, 512 + c * 128:512 + (c + 1) * 128], rhs=xT[:, k, blk * 512:(blk + 1) * 512], start=(k == 0), stop=(k == 7)),
                         reads=["xT.%d.%d" % (k, blk), "wA.x%d" % (k // 4)], writes=[PSK[pc]])
                S.act(lambda e, pc=pc, c=c, blk=blk, pb_=pb_: e.activation(out=pre[pb_][:, 3 + blk * 512:3 + (blk + 1) * 512], in_=bank(pc)[:, :], func=AF.Identity, bias=bxbc[:, c:c + 1], scale=1.0),
                      reads=[PSK[pc], "bxbc"], writes=["pre%d.%d" % (pb_, blk)])

        def a1_conv(c):
            pb_ = c % 2
            if c < 4:
                dstt = xsT[:, c, :]; dk = "xsT.%d" % c
            elif c < 6:
                dstt = BT[:, c - 4, :]; dk = "BT.%d" % (c - 4)
            else:
                dstt = CT[:, c - 6, :]; dk = "CT.%d" % (c - 6)
            for blk in range(4):
                pc = 2 + (c * 4 + blk) % 2
                rk = ["pre%d.%d" % (pb_, blk), "diagw.%d" % c] + (["pre%d.%d" % (pb_, blk - 1)] if blk > 0 else ["prepad%d" % pb_])
                for k in range(4):
                    S.pe(lambda e, pc=pc, c=c, k=k, blk=blk, pb_=pb_: e.matmul(bank(pc)[:, :], lhsT=diagw[:, c, k, :], rhs=pre[pb_][:, blk * 512 + k:blk * 512 + k + 512], start=(k == 0), stop=(k == 3)),
                         reads=rk, writes=[PSK[pc]])
                S.act(lambda e, pc=pc, dstt=dstt, c=c, blk=blk: e.activation(out=dstt[:, blk * 512:(blk + 1) * 512], in_=bank(pc)[:, :], func=AF.Silu, bias=convb[:, c:c + 1], scale=1.0),
                      reads=[PSK[pc], "convb"], writes=[dk])

        a1_ip(0)
        for c in range(8):
            if c + 1 < 8:
                a1_ip(c + 1)
            a1_conv(c)
        for t in range(NT):
            pb = t % 2
            for c in range(6):
                src = xsT[:, c, t * 128:(t + 1) * 128] if c < 4 else BT[:, c - 4, t * 128:(t + 1) * 128]
                sk = "xsT.%d" % c if c < 4 else "BT.%d" % (c - 4)
                S.pe(lambda e, pb=pb, c=c, src=src: e.transpose(bankb(pb)[:, c * 128:(c + 1) * 128], src, identB[:, :]), reads=[sk, "identB"], writes=[PSK[pb]])
            if t % 2 == 0:
                S.dve(lambda e, pb=pb, t=t: e.tensor_copy(xsB[:, t, :], bankb(pb)[:, 0:768]), reads=[PSK[pb]], writes=["xsB.%d" % t])
            else:
                S.act(lambda e, pb=pb, t=t: e.copy(xsB[:, t, :], bankb(pb)[:, 0:768]), reads=[PSK[pb]], writes=["xsB.%d" % t])
        S.act(lambda e: e.activation(out=dt_t[:, :, :], in_=dt_raw[:, :, :], func=AF.Exp), reads=["dt_raw"], writes=["dt_t"])
        S.act(lambda e: e.activation(out=dt_t[:, :, :], in_=dt_t[:, :, :], func=AF.Ln, bias=1.0, scale=1.0), reads=["dt_t"], writes=["dt_t"])

        fence()
        MSSD.reset(); TT.reset(tt_mark); RW.reset()
        m_ssd = MSSD.alloc([NT, 512], BF16)
        da = TT.alloc([NT, 8], F32); acum = TT.alloc([NT, 8], F32); nacum = TT.alloc([NT, 8], F32)
        alast = TT.alloc([NT, 8], F32); dte = TT.alloc([NT, 8], F32); ea = TT.alloc([NT, 8], F32)
        cdec = TT.alloc([NT, 8], F32); dtdte = TT.alloc([NT, 8], F32)
        stT = TT.alloc([8, 64], F32); stTb = TT.alloc([8, 64], BF16)
        LT = [TT.alloc([128], BF16) for _ in range(4)]
        MT = [TT.alloc([128], BF16) for _ in range(4)]
        xdt = [RW.alloc([8, 64], BF16) for _ in range(2)]
        xdtd = [RW.alloc([8, 64], BF16) for _ in range(2)]
        t1 = [RW.alloc([8, 64], F32) for _ in range(2)]
        t2 = [RW.alloc([8, 64], F32) for _ in range(2)]
        yg = [RW.alloc([512], F32) for _ in range(2)]
        junk = TT.alloc([256], F32)
        ss = [TT.alloc([2], F32) for _ in range(2)]
        rstd = [TT.alloc([2], F32) for _ in range(2)]

        S.dve(lambda e: e.tensor_tensor(out=da[:, :, :], in0=dt_t[:, :, :], in1=a_bc[:, :].unsqueeze(1).to_broadcast([128, NT, 8]), op=ALU.mult), reads=["dt_t", "a_bc"], writes=["da"])
        daf = da.rearrange("p t h -> p (t h)")
        S.pe(lambda e: e.matmul(bank(5)[:, 0:128], lhsT=Uf[:, :], rhs=daf, start=True, stop=True), reads=["Uf", "da"], writes=[PSK[5]])
        S.pe(lambda e: e.matmul(bank(6)[:, 0:128], lhsT=onesF[:, :], rhs=daf, start=True, stop=True), reads=["onesF", "da"], writes=[PSK[6]])
        S.dve(lambda e: e.tensor_copy(acum.rearrange("p t h -> p (t h)"), bank(5)[:, 0:128]), reads=[PSK[5]], writes=["acum"])
        S.dve(lambda e: e.tensor_scalar(out=nacum.rearrange("p t h -> p (t h)"), in0=bank(5)[:, 0:128], scalar1=-1.0, scalar2=None, op0=ALU.mult), reads=[PSK[5]], writes=["nacum"])
        S.dve(lambda e: e.tensor_copy(alast.rearrange("p t h -> p (t h)"), bank(6)[:, 0:128]), reads=[PSK[6]], writes=["alast"])
        S.dve(lambda e: e.tensor_tensor(out=dte[:, :, :], in0=alast[:, :, :], in1=acum[:, :, :], op=ALU.subtract), reads=["alast", "acum"], writes=["dte"])
        S.act(lambda e: e.activation(out=dte[:, :, :], in_=dte[:, :, :], func=AF.Exp), reads=["dte"], writes=["dte"])
        S.act(lambda e: e.activation(out=ea[:, :, :], in_=acum[:, :, :], func=AF.Exp), reads=["acum"], writes=["ea"])
        S.act(lambda e: e.activation(out=cdec[:, :, :], in_=alast[:, :, :], func=AF.Exp), reads=["alast"], writes=["cdec"])
        S.dve(lambda e: e.tensor_tensor(out=dtdte[:, :, :], in0=dt_t[:, :, :], in1=dte[:, :, :], op=ALU.mult), reads=["dt_t", "dte"], writes=["dtdte"])
        S.pool(lambda e: e.memset(stT[:, :, :], 0.0), writes=["stT"])
        S.pool(lambda e: e.memset(stTb[:, :, :], 0.0), writes=["stTb"])

        def ssd_F(c):
            cs = slice(c * 128, (c + 1) * 128)
            b2 = c % 2
            py = 2 + b2
            xs_c = xsB[:, c, 0:512].rearrange("p (h d) -> p h d", h=8)
            S.pool(lambda e, c=c, b2=b2, xs_c=xs_c: e.tensor_tensor(out=xdt[b2][:, :, :], in0=xs_c, in1=dt_t[:, c, :].unsqueeze(2).to_broadcast([128, 8, 64]), op=ALU.mult),
                   reads=["xsB.%d" % c, "dt_t"], writes=["xdt%d" % b2])
            S.pool(lambda e, c=c, b2=b2, xs_c=xs_c: e.tensor_tensor(out=xdtd[b2][:, :, :], in0=xs_c, in1=dtdte[:, c, :].unsqueeze(2).to_broadcast([128, 8, 64]), op=ALU.mult),
                   reads=["xsB.%d" % c, "dtdte"], writes=["xdtd%d" % b2])
            S.pool(lambda e, c=c, b2=b2, xs_c=xs_c: e.tensor_tensor(out=t2[b2][:, :, :], in0=xs_c, in1=dskip_bc[:, :].unsqueeze(2).to_broadcast([128, 8, 64]), op=ALU.mult),
                   reads=["xsB.%d" % c, "dskip_bc"], writes=["t2%d" % b2])
            for g in range(2):
                S.pe(lambda e, g=g, cs=cs: e.matmul(bank(4)[:, g * 128:(g + 1) * 128], lhsT=BT[:, g, cs], rhs=CT[:, g, cs], start=True, stop=True),
                     reads=["BT.%d" % g, "CT.%d" % g], writes=[PSK[4]])
            for hh in range(2):
                pl = hh
                for h4 in range(4):
                    h = hh * 4 + h4
                    S.pe(lambda e, pl=pl, h4=h4, c=c, h=h: e.matmul(bank(pl)[:, h4 * 128:(h4 + 1) * 128], lhsT=da[:, c, h:h + 1].to_broadcast([128, 128]), rhs=Uf[:, :], start=True, stop=False),
                         reads=["da", "Uf"], writes=[PSK[pl]])
                    S.pe(lambda e, pl=pl, h4=h4: e.matmul(bank(pl)[:, h4 * 128:(h4 + 1) * 128], lhsT=identB[:, :], rhs=maskneg[:, :], start=False, stop=True),
                         reads=["identB", "maskneg"], writes=[PSK[pl]])
            for hh in range(2):
                pl = hh
                for h4 in range(4):
                    h = hh * 4 + h4
                    S.act(lambda e, pl=pl, h4=h4, c=c, h=h: e.activation(out=LT[h4][:, :], in_=bank(pl)[:, h4 * 128:(h4 + 1) * 128], func=AF.Exp, bias=nacum[:, c, h:h + 1], scale=1.0),
                          reads=[PSK[pl], "nacum"], writes=["LT%d" % h4])
                    g = h // 4
                    S.dve(lambda e, h4=h4, g=g: e.tensor_tensor(out=MT[h4][:, :], in0=bank(4)[:, g * 128:(g + 1) * 128], in1=LT[h4][:, :], op=ALU.mult),
                          reads=[PSK[4], "LT%d" % h4], writes=["MT%d" % h4])
                    S.pe(lambda e, h4=h4, h=h, b2=b2, py=py: e.matmul(bank(py)[:, h * 64:(h + 1) * 64], lhsT=MT[h4][:, :], rhs=xdt[b2][:, h, :], start=True, stop=True),
                         reads=["MT%d" % h4, "xdt%d" % b2], writes=[PSK[py]])

        def ssd_B(c):
            cs = slice(c * 128, (c + 1) * 128)
            b2 = c % 2
            py = 2 + b2
            if c > 0:
                for g in range(2):
                    S.pe(lambda e, g=g, cs=cs: e.matmul(bank(6)[:, g * 256:(g + 1) * 256], lhsT=CT[:, g, cs], rhs=stTb[:, 4 * g:4 * g + 4, :].rearrange("p h d -> p (h d)"), start=True, stop=True),
                         reads=["CT.%d" % g, "stTb"], writes=[PSK[6]])
            if c < NT - 1:
                for g in range(2):
                    S.pe(lambda e, g=g, c=c, b2=b2: e.matmul(bank(7)[:, g * 256:(g + 1) * 256], lhsT=xsB[:, c, 512 + g * 128:512 + (g + 1) * 128], rhs=xdtd[b2][:, 4 * g:4 * g + 4, :].rearrange("p h d -> p (h d)"), start=True, stop=True),
                         reads=["xsB.%d" % c, "xdtd%d" % b2], writes=[PSK[7]])
                S.dve(lambda e, c=c: e.tensor_tensor(out=stT[:, :, :], in0=stT[:, :, :], in1=cdec[:, c, :].unsqueeze(2).to_broadcast([128, 8, 64]), op=ALU.mult),
                      reads=["stT", "cdec"], writes=["stT"])
                S.dve(lambda e: e.tensor_tensor(out=stT[:, :, :], in0=bank(7)[:, 0:512].rearrange("p (h d) -> p h d", h=8), in1=stT[:, :, :], op=ALU.add),
                      reads=[PSK[7], "stT"], writes=["stT"])
            if c > 0:
                S.dve(lambda e, c=c, b2=b2: e.tensor_tensor(out=t1[b2][:, :, :], in0=bank(6)[:, :].rearrange("p (h d) -> p h d", h=8), in1=ea[:, c, :].unsqueeze(2).to_broadcast([128, 8, 64]), op=ALU.mult),
                      reads=[PSK[6], "ea"], writes=["t1%d" % b2])
            if c < NT - 1:
                S.act(lambda e: e.copy(stTb[:, :, :], stT[:, :, :]), reads=["stT"], writes=["stTb"])
            if c > 0:
                S.dve(lambda e, b2=b2, py=py: e.tensor_tensor(out=t1[b2][:, :, :], in0=bank(py)[:, :].rearrange("p (h d) -> p h d", h=8), in1=t1[b2][:, :, :], op=ALU.add),
                      reads=[PSK[py], "t1%d" % b2], writes=["t1%d" % b2])
                S.dve(lambda e, b2=b2: e.tensor_tensor(out=t1[b2][:, :, :], in0=t1[b2][:, :, :], in1=t2[b2][:, :, :], op=ALU.add),
                      reads=["t1%d" % b2, "t2%d" % b2], writes=["t1%d" % b2])
            else:
                S.dve(lambda e, b2=b2, py=py: e.tensor_tensor(out=t1[b2][:, :, :], in0=bank(py)[:, :].rearrange("p (h d) -> p h d", h=8), in1=t2[b2][:, :, :], op=ALU.add),
                      reads=[PSK[py], "t2%d" % b2], writes=["t1%d" % b2])
            S.dve(lambda e, c=c, b2=b2: e.tensor_tensor(out=yg[b2][:, :], in0=t1[b2].rearrange("p h d -> p (h d)"), in1=sz[:, c, :], op=ALU.mult),
                  reads=["t1%d" % b2, "sz.%d" % c], writes=["yg%d" % b2])
            for g in range(2):
                S.act(lambda e, g=g, b2=b2: e.activation(out=junk[:, :], in_=yg[b2][:, g * 256:(g + 1) * 256], func=AF.Square, accum_out=ss[b2][:, g:g + 1]),
                      reads=["yg%d" % b2], writes=["junk", "ss%d.%d" % (b2, g)])
            S.act(lambda e, b2=b2: e.activation(out=rstd[b2][:, :], in_=ss[b2][:, :], func=AF.Ln, bias=RMS_EPS, scale=1.0 / 256.0),
                  reads=["ss%d.0" % b2, "ss%d.1" % b2], writes=["rstd%d" % b2])
            S.act(lambda e, b2=b2: e.activation(out=rstd[b2][:, :], in_=rstd[b2][:, :], func=AF.Exp, scale=-0.5), reads=["rstd%d" % b2], writes=["rstd%d" % b2])
            for g in range(2):
                S.dve(lambda e, g=g, b2=b2, c=c: e.scalar_tensor_tensor(out=m_ssd[:, c, g * 256:(g + 1) * 256], in0=yg[b2][:, g * 256:(g + 1) * 256], scalar=rstd[b2][:, g:g + 1], in1=gssd_bc[:, g * 256:(g + 1) * 256], op0=ALU.mult, op1=ALU.mult),
                      reads=["yg%d" % b2, "rstd%d" % b2, "gssd_bc"], writes=["m_ssd.%d" % c])

        ssd_F(0)
        for c in range(NT):
            if c + 1 < NT:
                ssd_F(c + 1)
            ssd_B(c)

        if dbg == 'ssd' and s == 0:
            RX.reset()
            dm = dbg_out("dbg_mssd", [128, NT * 512])
            cvt = RX.alloc([NT * 512], F32) if dbg == 'ssd' else None
            S.dve(lambda e: e.tensor_copy(cvt[:, :], m_ssd.rearrange("p t d -> p (t d)")), reads=["m_ssd.%d" % c for c in range(NT)], writes=["cvt"])
            outs.append(S.dma("sp", lambda e: e.dma_start(out=dm[:, :], in_=cvt[:, :]), reads=["cvt"]))
        if dbg == "ssd":
            break

        fence()
        RW.reset(); RACC.reset(); TT.reset()
        wB = RW.alloc([8, 1544], BF16)
        wo = RW.alloc([8, 1024], BF16)
        ah = RW.alloc([1024], F32)
        hTf = RW.alloc([8, 128], F32)
        qT = RACC.alloc([4, SEQ], BF16)
        kT = RACC.alloc([4, SEQ], BF16)
        v_aug = RACC.alloc([NT, 8, 65], BF16)
        xres = [RACC.alloc([1024], F32) for _ in range(2)]
        hpre = [RACC.alloc([1024], F32), TT.alloc([1024], F32)]
        f_raw = TT.alloc([NT, 8], F32); Gc = TT.alloc([NT, 8], F32); tot = TT.alloc([NT, 8], F32)
        Pp = TT.alloc([NT, 8], F32); Gf = TT.alloc([NT, 8], F32); Gend = TT.alloc([NT, 8], F32)
        biasT = TT.alloc([8, NT, NT], F32)
        PT = [TT.alloc([8, 128], BF16) for _ in range(2)]
        yatt = [TT.alloc([8, 64], BF16) for _ in range(2)]
        rden = [TT.alloc([8], F32) for _ in range(2)]
        mT = [TT.alloc([8, 128], BF16)] * 2
        bst = [TT.alloc([2, 6], F32) for _ in range(2)]
        mv = [TT.alloc([2], F32) for _ in range(2)]
        rs1 = [TT.alloc([1], F32) for _ in range(2)]

        for half in range(2):
            S.dma("pool", lambda e, half=half: e.dma_start(out=wB[:, 4 * half:4 * half + 4, :], in_=w_in.rearrange("(k p) c -> p k c", p=128)[:, 4 * half:4 * half + 4, 1544:3088]),
                  writes=["wB"])
            S.dma("pool", lambda e, half=half: e.dma_start(out=wo[:, 4 * half:4 * half + 4, :], in_=w_out.rearrange("(k p) c -> p k c", p=128)[:, 4 * half:4 * half + 4, :]),
                  writes=["wo"])
        S.dma("sp", lambda e: e.dma_start(out=lnG[:, :], in_=ln1_g[0:1, :].to_broadcast([128, 1024])), writes=["lnG"])
        S.dma("sp", lambda e: e.dma_start(out=lnB[:, :], in_=ln1_b[0:1, :].to_broadcast([128, 1024])), writes=["lnB"])
        S.pool(lambda e: e.memset(v_aug[:, :, :, 64:65], 1.0), writes=["v_ones"])
        evq = 0
        for qk in range(2):
            dstT = qT if qk == 0 else kT
            bcol = bq if qk == 0 else bk
            nm = "qT" if qk == 0 else "kT"
            for p in range(4):
                for blk in range(4):
                    pq = evq % 4
                    for k in range(8):
                        S.pe(lambda e, pq=pq, k=k, p=p, blk=blk, qk=qk: e.matmul(bank(pq)[:, :], lhsT=wB[:, k, qk * 512 + p * 128:qk * 512 + (p + 1) * 128], rhs=xT[:, k, blk * 512:(blk + 1) * 512], start=(k == 0), stop=(k == 7)),
                             reads=["xT.%d.%d" % (k, blk), "wB"], writes=[PSK[pq]])
                    if evq % 2 == 0:
                        S.act(lambda e, pq=pq, p=p, blk=blk, dstT=dstT, bcol=bcol: e.activation(out=dstT[:, p, blk * 512:(blk + 1) * 512], in_=bank(pq)[:, :], func=AF.Identity, bias=bcol[:, p:p + 1], scale=1.0),
                              reads=[PSK[pq], "bq", "bk"], writes=["%s.%d.%d" % (nm, p, blk)])
                    else:
                        S.dve(lambda e, pq=pq, p=p, blk=blk, dstT=dstT, bcol=bcol: e.tensor_scalar(out=dstT[:, p, blk * 512:(blk + 1) * 512], in0=bank(pq)[:, :], scalar1=bcol[:, p:p + 1], scalar2=None, op0=ALU.add),
                              reads=[PSK[pq], "bq", "bk"], writes=["%s.%d.%d" % (nm, p, blk)])
                    evq += 1
        for t in range(NT):
            pv = 4 + (t % 2)
            blk = t // 4
            for k in range(8):
                S.pe(lambda e, pv=pv, t=t, k=k: e.matmul(bank(pv)[:, :], lhsT=xT[:, k, t * 128:(t + 1) * 128], rhs=wB[:, k, 1024:1536], start=(k == 0), stop=False),
                     reads=["xT.%d.%d" % (k, blk), "wB"], writes=[PSK[pv]])
            S.pe(lambda e, pv=pv: e.matmul(bank(pv)[:, :], lhsT=ones_row[0:1, :], rhs=bv_row[0:1, :], start=False, stop=True),
                 reads=["ones_row", "bv_row"], writes=[PSK[pv]])
            if t % 2 == 0:
                S.act(lambda e, pv=pv, t=t: e.copy(v_aug[:, t, :, 0:64], bank(pv)[:, :].rearrange("p (h d) -> p h d", h=8)), reads=[PSK[pv]], writes=["v.%d" % t])
            else:
                S.dve(lambda e, pv=pv, t=t: e.tensor_copy(v_aug[:, t, :, 0:64], bank(pv)[:, :].rearrange("p (h d) -> p h d", h=8)), reads=[PSK[pv]], writes=["v.%d" % t])
            for k in range(8):
                S.pe(lambda e, t=t, k=k: e.matmul(bank(6)[:, t * 8:(t + 1) * 8], lhsT=xT[:, k, t * 128:(t + 1) * 128], rhs=wB[:, k, 1536:1544], start=(k == 0), stop=(k == 7)),
                     reads=["xT.%d.%d" % (k, blk), "wB"], writes=[PSK[6]])
        S.dve(lambda e: e.tensor_tensor(out=f_raw[:, :, :], in0=bank(6)[:, 0:128].rearrange("p (t h) -> p t h", h=8), in1=bdtf[:, 8:16].unsqueeze(1).to_broadcast([128, NT, 8]), op=ALU.add),
              reads=[PSK[6], "bdtf1"], writes=["f_raw"])
        S.act(lambda e: e.activation(out=f_raw[:, :, :], in_=f_raw[:, :, :], func=AF.Exp, scale=-1.0), reads=["f_raw"], writes=["f_raw"])
        S.act(lambda e: e.activation(out=f_raw[:, :, :], in_=f_raw[:, :, :], func=AF.Ln, bias=1.0, scale=1.0), reads=["f_raw"], writes=["f_raw"])
        frf = f_raw.rearrange("p t h -> p (t h)")
        S.pe(lambda e: e.matmul(bank(7)[:, 0:128], lhsT=Uf[:, :], rhs=frf, start=True, stop=True), reads=["Uf", "f_raw"], writes=[PSK[7]])
        S.pe(lambda e: e.matmul(bank(7)[:, 128:256], lhsT=onesF[:, :], rhs=frf, start=True, stop=True), reads=["onesF", "f_raw"], writes=[PSK[7]])
        S.dve(lambda e: e.tensor_copy(Gc.rearrange("p t h -> p (t h)"), bank(7)[:, 0:128]), reads=[PSK[7]], writes=["Gc"])
        S.dve(lambda e: e.tensor_copy(tot.rearrange("p t h -> p (t h)"), bank(7)[:, 128:256]), reads=[PSK[7]], writes=["tot"])
        S.pool(lambda e: e.memset(Pp[:, 0, :], 0.0), writes=["Pp"])
        for t in range(1, NT):
            S.dve(lambda e, t=t: e.tensor_tensor(out=Pp[:, t, :], in0=Pp[:, t - 1, :], in1=tot[:, t - 1, :], op=ALU.add), reads=["Pp", "tot"], writes=["Pp"])
        S.dve(lambda e: e.tensor_tensor(out=Gf[:, :, :], in0=Gc[:, :, :], in1=Pp[:, :, :], op=ALU.add), reads=["Gc", "Pp"], writes=["Gf"])
        S.dve(lambda e: e.tensor_tensor(out=Gend[:, :, :], in0=tot[:, :, :], in1=Pp[:, :, :], op=ALU.add), reads=["tot", "Pp"], writes=["Gend"])
        for h in range(8):
            S.dve(lambda e, h=h: e.tensor_tensor(out=biasT[:, h, :, :], in0=Gf[:, :, h].unsqueeze(1).to_broadcast([128, NT, NT]), in1=Gend[:, :, h].unsqueeze(2).to_broadcast([128, NT, NT]), op=ALU.subtract),
                  reads=["Gf", "Gend"], writes=["biasT"])

        fence()
        RX.reset()
        hT = RX.alloc([8, SEQ], BF16)
        NEXP = int(os.environ.get("KNEXP", 16))

        def load_expert(ex):
            sl = ex % 2
            S.dma("pool", lambda e, ex=ex, sl=sl: e.dma_start(out=wgu[sl][:, :, 0:512], in_=w_gate[ex].rearrange("(k p) f -> p k f", p=128)), writes=["wg.%d" % sl], nofence=(ex == 0))
            S.dma("pool", lambda e, ex=ex, sl=sl: e.dma_start(out=wgu[sl][:, :, 512:1024], in_=w_up[ex].rearrange("(k p) f -> p k f", p=128)), writes=["wu.%d" % sl], nofence=(ex == 0))
            S.dma("pool", lambda e, ex=ex, sl=sl: e.dma_start(out=wdn[sl][:, :, :], in_=w_down[ex].rearrange("(k p) d -> p k d", p=128)), writes=["wd.%d" % sl], nofence=(ex == 0))


        load_expert(0)
        NTI = int(os.environ.get("KATT_TILES", NT))
        units = []
        for i in range(NTI):
            for p in range(4):
                for j0 in range(0, i + 1, 4):
                    units.append((i, p, list(range(j0, min(j0 + 4, i + 1)))))

        def emit_S(u, gi):
            i, p, js = u
            pb0 = 2 * (gi % 2)
            for jj, j in enumerate(js):
                for hh in range(2):
                    r0 = hh * 64
                    S.pe(lambda e, bk=pb0 + hh, jj=jj, j=j, p=p, r0=r0, i=i: e.matmul(bank(bk)[:, jj * 128:(jj + 1) * 128], lhsT=kT[r0:r0 + 64, p, j * 128:(j + 1) * 128], rhs=qT[r0:r0 + 64, p, i * 128:(i + 1) * 128], start=True, stop=True),
                         reads=["kT.%d.%d" % (p, j // 4), "qT.%d.%d" % (p, i // 4)], writes=[PSK[pb0 + hh]])

        def emit_E(u, gi):
            i, p, js = u
            pb0 = 2 * (gi % 2); pb = gi % 2
            for jj, j in enumerate(js):
                for hh in range(2):
                    h = 2 * p + hh
                    c8 = jj * 2 + hh
                    S.act(lambda e, bk=pb0 + hh, pb=pb, c8=c8, jj=jj, j=j, h=h, i=i: e.activation(out=PT[pb][:, c8, :], in_=bank(bk)[:, jj * 128:(jj + 1) * 128], func=AF.Exp, bias=biasT[:, h, i, j:j + 1], scale=ATT_SCALE),
                          reads=[PSK[pb0 + hh], "biasT"], writes=["PT%d.%d" % (pb, c8)])
                    if j == i:
                        S.dve(lambda e, pb=pb, c8=c8: e.tensor_tensor(out=PT[pb][:, c8, :], in0=PT[pb][:, c8, :], in1=triU[:, :], op=ALU.mult),
                              reads=["PT%d.%d" % (pb, c8), "triU"], writes=["PT%d.%d" % (pb, c8)])

        def emit_V(u, gi):
            i, p, js = u
            pb = gi % 2
            for jj, j in enumerate(js):
                for hh in range(2):
                    h = 2 * p + hh
                    c8 = jj * 2 + hh
                    po = 4 + h // 4; oc = (h % 4) * 65
                    S.pe(lambda e, pb=pb, c8=c8, j=j, h=h, po=po, oc=oc, i=i: e.matmul(bank(po)[:, oc:oc + 65], lhsT=PT[pb][:, c8, :], rhs=v_aug[:, j, h, :], start=(j == 0 and h % 4 == 0), stop=(j == i and h % 4 == 3)),
                         reads=["PT%d.%d" % (pb, c8), "v.%d" % j, "v_ones"], writes=[PSK[po]])

        def tail_N(i):
            b2 = i % 2
            S.dma("sp", lambda e, i=i, b2=b2, s=s: e.dma_start(out=xres[b2][:, :], in_=x[s, i * 128:(i + 1) * 128, :]), writes=["xres%d" % b2])
            for hb in range(2):
                ov = bank(4 + hb)[:, 0:260].rearrange("p (h d) -> p h d", h=4)
                S.dve(lambda e, hb=hb, ov=ov, b2=b2: e.reciprocal(rden[b2][:, 4 * hb:4 * hb + 4], ov[:, :, 64]), reads=[PSK[4 + hb]], writes=["rden%d.%d" % (b2, hb)])
                S.dve(lambda e, hb=hb, ov=ov, b2=b2: e.tensor_tensor(out=yatt[b2][:, 4 * hb:4 * hb + 4, :], in0=ov[:, :, 0:64], in1=rden[b2][:, 4 * hb:4 * hb + 4].unsqueeze(2).to_broadcast([128, 4, 64]), op=ALU.mult),
                      reads=[PSK[4 + hb], "rden%d.%d" % (b2, hb)], writes=["yatt%d.%d" % (b2, hb)])

        def tail_T(i):
            b2 = i % 2
            yf = yatt[b2].rearrange("p h d -> p (h d)")
            for ec in range(8):
                src = m_ssd[:, i, ec * 128:(ec + 1) * 128] if ec < 4 else yf[:, (ec - 4) * 128:(ec - 3) * 128]
                rk = ["m_ssd.%d" % i] if ec < 4 else ["yatt%d.%d" % (b2, (ec - 4) // 2)]
                S.pe(lambda e, ec=ec, src=src: e.transpose(bankb(6)[:, ec * 128:(ec + 1) * 128], src, identB[:, :]), reads=rk + ["identB"], writes=[PSK[6]])
            S.dve(lambda e, b2=b2: e.tensor_copy(mT[b2].rearrange("p a b -> p (a b)"), bankb(6)[:, 0:1024]), reads=[PSK[6]], writes=["mT"])

        def tail_Oq(i, q):
            b2 = i % 2
            half = q // 2
            hp = hpre[b2]
            for ec in range(4 * (q % 2), 4 * (q % 2) + 4):
                S.pe(lambda e, ec=ec, b2=b2, half=half: e.matmul(bank(7)[:, :], lhsT=mT[b2][:, ec, :], rhs=wo[:, ec, half * 512:(half + 1) * 512], start=(ec == 0), stop=(ec == 7)),
                     reads=["mT", "wo"], writes=[PSK[7]])
            if q % 2 == 1:
                S.dve(lambda e, hp=hp, b2=b2, half=half: e.scalar_tensor_tensor(out=hp[:, half * 512:(half + 1) * 512], in0=xres[b2][:, half * 512:(half + 1) * 512], scalar=ALPHA, in1=bank(7)[:, :], op0=ALU.mult, op1=ALU.add),
                      reads=["xres%d" % b2, PSK[7]], writes=["hpre%d.%d" % (b2, half)])
                S.dve(lambda e, hp=hp, half=half, b2=b2: e.bn_stats(bst[b2][:, half, :], hp[:, half * 512:(half + 1) * 512]), reads=["hpre%d.%d" % (b2, half)], writes=["bst%d.%d" % (b2, half)])
            if q == 3:
                S.dve(lambda e, b2=b2: e.bn_aggr(mv[b2][:, :], bst[b2][:, :, :]), reads=["bst%d.0" % b2, "bst%d.1" % b2], writes=["mv%d" % b2])

        def tail_L(i):
            b2 = i % 2
            hp = hpre[b2]
            HK = ["hpre%d.0" % b2, "hpre%d.1" % b2]
            S.act(lambda e, b2=b2: e.activation(out=rs1[b2][:, :], in_=mv[b2][:, 1:2], func=AF.Ln, bias=LN_EPS, scale=1.0), reads=["mv%d" % b2], writes=["rs1%d" % b2])
            S.act(lambda e, b2=b2: e.activation(out=rs1[b2][:, :], in_=rs1[b2][:, :], func=AF.Exp, scale=-0.5), reads=["rs1%d" % b2], writes=["rs1%d" % b2])
            S.dve(lambda e, hp=hp, b2=b2: e.tensor_scalar(out=hp[:, :], in0=hp[:, :], scalar1=mv[b2][:, 0:1], scalar2=rs1[b2][:, 0:1], op0=ALU.subtract, op1=ALU.mult),
                  reads=HK + ["mv%d" % b2, "rs1%d" % b2], writes=HK)
            S.dve(lambda e, hp=hp: e.tensor_tensor(out=hp[:, :], in0=hp[:, :], in1=lnG[:, :], op=ALU.mult), reads=HK + ["lnG"], writes=HK)
            S.dve(lambda e, hp=hp: e.tensor_tensor(out=hp[:, :], in0=hp[:, :], in1=lnB[:, :], op=ALU.add), reads=HK + ["lnB"], writes=HK)
            S.dve(lambda e, hp=hp: e.tensor_scalar(out=ah[:, :], in0=hp[:, :], scalar1=ALPHA, scalar2=None, op0=ALU.mult), reads=HK, writes=["ah"])
            S.dma("sp", lambda e, i=i, s=s: e.dma_start(out=h_scr[s, i * 128:(i + 1) * 128, :], in_=ah[:, :]), reads=["ah"], writes=["h_scr.%d" % i])

        def tail_H(i, half):
            b2 = i % 2
            hp = hpre[b2]
            for q4 in range(4):
                ec = half * 4 + q4
                S.pe(lambda e, q4=q4, ec=ec, hp=hp: e.transpose(bank(6)[:, q4 * 128:(q4 + 1) * 128], hp[:, ec * 128:(ec + 1) * 128], identF[:, :]), reads=["hpre%d.%d" % (b2, half), "identF"], writes=[PSK[6]])
            S.dve(lambda e, half=half, i=i: e.tensor_copy(hT[:, 4 * half:4 * half + 4, i * 128:(i + 1) * 128], bank(6)[:, :].rearrange("p (a b) -> p a b", a=4)), reads=[PSK[6]], writes=["hT.%d.%d" % (i, half)])
            S.dve(lambda e, half=half: e.tensor_copy(hTf[:, 4 * half:4 * half + 4, :], bank(6)[:, :].rearrange("p (a b) -> p a b", a=4)), reads=[PSK[6]], writes=["hTf.%d" % half])

        def tail_R(i, part):
            for ec in range(4 * part, 4 * part + 4):
                S.pe(lambda e, ec=ec: e.matmul(bank(6)[:, 0:20], lhsT=hTf[:, ec, :], rhs=rw[:, ec, :], start=(ec == 0), stop=(ec == 7)), reads=["hTf.%d" % (ec // 4)] + RWK, writes=[PSK[6]])
            if part == 1:
                S.dve(lambda e, i=i: e.tensor_tensor(out=logits[:, i, :], in0=bank(6)[:, 0:20], in1=rb_bc[:, :], op=ALU.add), reads=[PSK[6], "rb0", "rb1"], writes=["logits.%d" % i])

        TD = [int(v) for v in os.environ.get("KTAIL", "0,1,2,3,4,5,9,11,12,14").split(",")]
        TAIL = [(TD[0], tail_N), (TD[1], tail_T), (TD[2], lambda i: tail_Oq(i, 0)), (TD[3], lambda i: tail_Oq(i, 1)), (TD[4], lambda i: tail_Oq(i, 2)), (TD[5], lambda i: tail_Oq(i, 3)),
                (TD[6], tail_L), (TD[7], lambda i: tail_H(i, 0)), (TD[8], lambda i: tail_H(i, 1)), (TD[9], lambda i: (tail_R(i, 0), tail_R(i, 1)))]
        NSTEP = len(TAIL)
        done_steps = set()

        def emit_step(t, k):
            if t < 0 or (t, k) in done_steps:
                return
            for kk in range(k):
                emit_step(t, kk)
            emit_step(t - 1, k)
            if k == 1:
                emit_step(t - 1, 5)
            if k == 7:
                emit_step(t - 1, NSTEP - 1)
            if k == 0:
                for kk in range(NSTEP):
                    emit_step(t - 2, kk)
            done_steps.add((t, k))
            TAIL[k][1](t)

        pending = []
        def tick():
            keep = []
            for ent in pending:
                if ent[0] <= 0:
                    emit_step(ent[1], ent[2])
                else:
                    ent[0] -= 1
                    keep.append(ent)
            pending[:] = keep
        prev = None
        for gi, u in enumerate(units):
            emit_S(u, gi)
            emit_E(u, gi)
            if prev is not None:
                emit_V(prev[0], prev[1])
                if prev[0][0] != u[0]:
                    for k, (dly, fn) in enumerate(TAIL):
                        pending.append([dly, prev[0][0], k])
            tick()
            prev = (u, gi)
        if prev is not None:
            emit_V(prev[0], prev[1])
            for k, (dly, fn) in enumerate(TAIL):
                pending.append([dly, prev[0][0], k])
        while pending:
            tick()

        if dbg == "att" and s == 0:
            fence()
            RACC.reset()
            cvt = RACC.alloc([NT * 1024], BF16)
            d1 = dbg_out("dbg_hT", [128, 8 * SEQ]); d2 = dbg_out("dbg_logits", [128, NT * 20])
            cv2 = RACC.alloc([2 * SEQ], F32)
            for q in range(4):
                S.dve(lambda e, q=q: e.tensor_copy(cv2[:, :], hT[:, 2 * q:2 * q + 2, :].rearrange("p a b -> p (a b)")), reads=[], writes=["cv2"])
                outs.append(S.dma("sp", lambda e, q=q: e.dma_start(out=d1[:, 2 * q * SEQ:(2 * q + 2) * SEQ], in_=cv2[:, :]), reads=["cv2"]))
            outs.append(S.dma("sp", lambda e: e.dma_start(out=d2[:, :], in_=logits.rearrange("p t j -> p (t j)")), reads=["logits.%d" % i for i in range(int(os.environ.get("KATT_TILES", NT)))] if int(os.environ.get("KATT_STAGE", 9)) >= 7 else []))
            break


        fence()
        TT.reset(); RW.reset(); RACC.reset()
        S.dma("sp", lambda e: e.dma_start(out=lnG[:, :], in_=ln2_g[0:1, :].to_broadcast([128, 1024])), writes=["lnG"])
        S.dma("sp", lambda e: e.dma_start(out=lnB[:, :], in_=ln2_b[0:1, :].to_broadcast([128, 1024])), writes=["lnB"])
        LOGK = ["logits.%d" % i for i in range(NT)]
        lg = logits[:, :, 0:4]
        le4 = logits[:, :, 4:20].rearrange("p t (g j) -> p t g j", g=4)
        gmax = TT.alloc([NT], F32); goh = TT.alloc([NT, 4], F32); gex = TT.alloc([NT, 4], F32)
        gsum = TT.alloc([NT], F32); gval = TT.alloc([NT], F32)
        tmp16 = TT.alloc([NT, 4, 4], F32); esel = TT.alloc([NT, 4], F32)
        m1 = TT.alloc([NT], F32); oh1 = TT.alloc([NT, 4], F32); e2 = TT.alloc([NT, 4], F32)
        m2 = TT.alloc([NT], F32); oh2 = TT.alloc([NT, 4], F32); dd = TT.alloc([NT], F32)
        w1 = TT.alloc([NT], F32); w2 = TT.alloc([NT], F32); cw1 = TT.alloc([NT], F32); cw2 = TT.alloc([NT], F32)
        cj = TT.alloc([NT, 4], F32); cj2 = TT.alloc([NT, 4], F32)
        bc4 = lambda a: a.unsqueeze(2).to_broadcast([128, NT, 4])
        S.dve(lambda e: e.tensor_reduce(out=gmax[:, :], in_=lg, axis=AX.X, op=ALU.max), reads=LOGK, writes=["gmax"])
        S.dve(lambda e: e.tensor_tensor(out=goh[:, :, :], in0=lg, in1=bc4(gmax[:, :]), op=ALU.is_equal), reads=LOGK + ["gmax"], writes=["goh"])
        S.dve(lambda e: e.tensor_tensor(out=gex[:, :, :], in0=lg, in1=bc4(gmax[:, :]), op=ALU.subtract), reads=LOGK + ["gmax"], writes=["gex"])
        S.act(lambda e: e.activation(out=gex[:, :, :], in_=gex[:, :, :], func=AF.Exp), reads=["gex"], writes=["gex"])
        S.dve(lambda e: e.tensor_reduce(out=gsum[:, :], in_=gex[:, :, :], axis=AX.X, op=ALU.add), reads=["gex"], writes=["gsum"])
        S.dve(lambda e: e.reciprocal(gval[:, :], gsum[:, :]), reads=["gsum"], writes=["gval"])
        S.dve(lambda e: e.tensor_tensor(out=tmp16[:, :, :, :], in0=le4, in1=goh[:, :, :].unsqueeze(3).to_broadcast([128, NT, 4, 4]), op=ALU.mult), reads=LOGK + ["goh"], writes=["tmp16"])
        S.dve(lambda e: e.tensor_reduce(out=esel[:, :, :], in_=tmp16.rearrange("p t g j -> p t j g"), axis=AX.X, op=ALU.add), reads=["tmp16"], writes=["esel"])
        S.dve(lambda e: e.tensor_reduce(out=m1[:, :], in_=esel[:, :, :], axis=AX.X, op=ALU.max), reads=["esel"], writes=["m1"])
        S.dve(lambda e: e.tensor_tensor(out=oh1[:, :, :], in0=esel[:, :, :], in1=bc4(m1[:, :]), op=ALU.is_equal), reads=["esel", "m1"], writes=["oh1"])
        S.dve(lambda e: e.scalar_tensor_tensor(out=e2[:, :, :], in0=oh1[:, :, :], scalar=-1e30, in1=esel[:, :, :], op0=ALU.mult, op1=ALU.add), reads=["oh1", "esel"], writes=["e2"])
        S.dve(lambda e: e.tensor_reduce(out=m2[:, :], in_=e2[:, :, :], axis=AX.X, op=ALU.max), reads=["e2"], writes=["m2"])
        S.dve(lambda e: e.tensor_tensor(out=oh2[:, :, :], in0=e2[:, :, :], in1=bc4(m2[:, :]), op=ALU.is_equal), reads=["e2", "m2"], writes=["oh2"])
        S.dve(lambda e: e.tensor_tensor(out=dd[:, :], in0=m2[:, :], in1=m1[:, :], op=ALU.subtract), reads=["m1", "m2"], writes=["dd"])
        S.act(lambda e: e.activation(out=dd[:, :], in_=dd[:, :], func=AF.Exp), reads=["dd"], writes=["dd"])
        S.dve(lambda e: e.tensor_scalar(out=w1[:, :], in0=dd[:, :], scalar1=1.0, scalar2=None, op0=ALU.add), reads=["dd"], writes=["w1"])
        S.dve(lambda e: e.reciprocal(w1[:, :], w1[:, :]), reads=["w1"], writes=["w1"])
        S.dve(lambda e: e.tensor_tensor(out=w2[:, :], in0=dd[:, :], in1=w1[:, :], op=ALU.mult), reads=["dd", "w1"], writes=["w2"])
        S.dve(lambda e: e.tensor_tensor(out=cw1[:, :], in0=gval[:, :], in1=w1[:, :], op=ALU.mult), reads=["gval", "w1"], writes=["cw1"])
        S.dve(lambda e: e.tensor_tensor(out=cw2[:, :], in0=gval[:, :], in1=w2[:, :], op=ALU.mult), reads=["gval", "w2"], writes=["cw2"])
        S.dve(lambda e: e.tensor_tensor(out=cj[:, :, :], in0=oh1[:, :, :], in1=bc4(cw1[:, :]), op=ALU.mult), reads=["oh1", "cw1"], writes=["cj"])
        S.dve(lambda e: e.tensor_tensor(out=cj2[:, :, :], in0=oh2[:, :, :], in1=bc4(cw2[:, :]), op=ALU.mult), reads=["oh2", "cw2"], writes=["cj2"])
        S.dve(lambda e: e.tensor_tensor(out=cj[:, :, :], in0=cj[:, :, :], in1=cj2[:, :, :], op=ALU.add), reads=["cj", "cj2"], writes=["cj"])
        S.dve(lambda e: e.tensor_tensor(out=comb.rearrange("p t (g j) -> p t g j", g=4), in0=goh[:, :, :].unsqueeze(3).to_broadcast([128, NT, 4, 4]), in1=cj[:, :, :].unsqueeze(2).to_broadcast([128, NT, 4, 4]), op=ALU.mult),
              reads=["goh", "cj"], writes=["comb"])

        acc = RACC.alloc([NT, 1024], F32)
        sg = [TT.alloc([512], BF16) for _ in range(2)]
        actT = [TT.alloc([4, 512], BF16) for _ in range(2)]
        obuf = [TT.alloc([1024], F32) for _ in range(2)]
        bst2 = [TT.alloc([2, 6], F32) for _ in range(2)]
        mv2 = [TT.alloc([2], F32) for _ in range(2)]
        rs2 = [TT.alloc([1], F32) for _ in range(2)]
        for q in range(4):
            S.dma("sp", lambda e, q=q, s=s: e.dma_start(out=acc[:, 4 * q:4 * q + 4, :], in_=h_scr[s, q * 512:(q + 1) * 512, :].rearrange("(t p) d -> p t d", p=128)),
                  reads=["h_scr.%d" % t for t in range(4 * q, 4 * q + 4)], writes=["acc.%d" % t for t in range(4 * q, 4 * q + 4)])
        def ln2_stats(t):
            b2 = t % 2
            for c2 in range(2):
                S.dve(lambda e, c2=c2, t=t, b2=b2: e.bn_stats(bst2[b2][:, c2, :], acc[:, t, c2 * 512:(c2 + 1) * 512]), reads=["acc.%d" % t], writes=["bst2%d.%d" % (b2, c2)])
            S.dve(lambda e, b2=b2: e.bn_aggr(mv2[b2][:, :], bst2[b2][:, :, :]), reads=["bst2%d.0" % b2, "bst2%d.1" % b2], writes=["mv2%d" % b2])

        def ln2_apply(t):
            b2 = t % 2
            S.act(lambda e, b2=b2: e.activation(out=rs2[b2][:, :], in_=mv2[b2][:, 1:2], func=AF.Ln, bias=LN_EPS, scale=1.0), reads=["mv2%d" % b2], writes=["rs2%d" % b2])
            S.act(lambda e, b2=b2: e.activation(out=rs2[b2][:, :], in_=rs2[b2][:, :], func=AF.Exp, scale=-0.5), reads=["rs2%d" % b2], writes=["rs2%d" % b2])
            S.dve(lambda e, t=t, b2=b2: e.tensor_scalar(out=obuf[b2][:, :], in0=acc[:, t, :], scalar1=mv2[b2][:, 0:1], scalar2=rs2[b2][:, 0:1], op0=ALU.subtract, op1=ALU.mult),
                  reads=["acc.%d" % t, "mv2%d" % b2, "rs2%d" % b2], writes=["obuf%d" % b2])
            S.dve(lambda e, b2=b2: e.tensor_tensor(out=obuf[b2][:, :], in0=obuf[b2][:, :], in1=lnG[:, :], op=ALU.mult), reads=["obuf%d" % b2, "lnG"], writes=["obuf%d" % b2])
            S.dve(lambda e, b2=b2: e.tensor_tensor(out=obuf[b2][:, :], in0=obuf[b2][:, :], in1=lnB[:, :], op=ALU.add), reads=["obuf%d" % b2, "lnB"], writes=["obuf%d" % b2])
            outs.append(S.dma("sp", lambda e, t=t, b2=b2, s=s: e.dma_start(out=out[s, t * 128:(t + 1) * 128, :], in_=obuf[b2][:, :]), reads=["obuf%d" % b2], writes=["out.%d.%d" % (s, t)]))

        ln2_prev = []
        if NEXP > 1:
            load_expert(1)
        cg = 0; cd = 0
        for ex in range(NEXP):
            sl = ex % 2
            for tb in range(4):
                ab = (ex * 4 + tb) % 2
                hk = ["hT.%d.%d" % (i, hf) for i in range(4 * tb, 4 * tb + 4) for hf in range(2)]
                for fc in range(4):
                    pg = cg % 2; cg += 1
                    for k in range(8):
                        S.pe(lambda e, pg=pg, k=k, fc=fc, tb=tb, sl=sl: e.matmul(bank(pg)[:, :], lhsT=wgu[sl][:, k, fc * 128:(fc + 1) * 128], rhs=hT[:, k, tb * 512:(tb + 1) * 512], start=(k == 0), stop=(k == 7)),
                             reads=["wg.%d" % sl] + hk, writes=[PSK[pg]])
                    for k in range(8):
                        S.pe(lambda e, pg=pg, k=k, fc=fc, tb=tb, sl=sl: e.matmul(bank(2 + pg)[:, :], lhsT=wgu[sl][:, k, 512 + fc * 128:512 + (fc + 1) * 128], rhs=hT[:, k, tb * 512:(tb + 1) * 512], start=(k == 0), stop=(k == 7)),
                             reads=["wu.%d" % sl] + hk, writes=[PSK[2 + pg]])
                    S.act(lambda e, pg=pg: e.activation(out=sg[pg][:, :], in_=bank(pg)[:, :], func=AF.Silu), reads=[PSK[pg]], writes=["sg%d" % pg])
                    S.dve(lambda e, pg=pg, ab=ab, fc=fc: e.tensor_tensor(out=actT[ab][:, fc, :], in0=bank(2 + pg)[:, :], in1=sg[pg][:, :], op=ALU.mult),
                          reads=[PSK[2 + pg], "sg%d" % pg], writes=["actT%d.%d" % (ab, fc)])
                for tt in range(4):
                    t = tb * 4 + tt
                    pdi = 2 + (cd % 2); cd += 1
                    for half in range(2):
                        for fc in range(4):
                            S.pe(lambda e, pdi=pdi, half=half, fc=fc, ab=ab, tt=tt, sl=sl: e.matmul(pd[pdi][:, half * 512:(half + 1) * 512], lhsT=actT[ab][:, fc, tt * 128:(tt + 1) * 128], rhs=wdn[sl][:, fc, half * 512:(half + 1) * 512], start=(fc == 0), stop=(fc == 3)),
                                 reads=["actT%d.%d" % (ab, fc), "wd.%d" % sl], writes=[PSK[2 * pdi + half]])
                    S.dve(lambda e, pdi=pdi, t=t, ex=ex: e.scalar_tensor_tensor(out=acc[:, t, :], in0=pd[pdi][:, :], scalar=comb[:, t, ex:ex + 1], in1=acc[:, t, :], op0=ALU.mult, op1=ALU.add),
                          reads=[PSK[2 * pdi], PSK[2 * pdi + 1], "comb", "acc.%d" % t], writes=["acc.%d" % t])
                    if ex == NEXP - 1:
                        ln2_stats(t)
                        if ln2_prev:
                            ln2_apply(ln2_prev.pop())
                        ln2_prev.append(t)
            if ex + 2 < NEXP:
                load_expert(ex + 2)
        while ln2_prev:
            ln2_apply(ln2_prev.pop())
        if s + 1 < NSEQ:
            fence()
        if dbg == "one":
            break

    with nc.allow_non_contiguous_dma(reason="tiny constant loads"):
        st = S.emit(outs)
    return nc, st, dbg_t


_CACHE = {}


def _get_program():
    if "p" not in _CACHE:
        _CACHE["p"] = build_program(dbg=os.environ.get("KDBG", ""))
    return _CACHE["p"]


def kernel(**inputs):
    nc, st, dbg_t = _get_program()
    f = lambda a: np.ascontiguousarray(np.asarray(a, dtype=np.float32))
    x = f(inputs["x"])
    shared = {
        "w_in": f(inputs["w_in"])[0], "b_in": f(inputs["b_in"]).reshape(1, DIN),
        "conv_w": f(inputs["conv_w"])[0], "conv_b": f(inputs["conv_b"]).reshape(1, 1024),
        "a_log": f(inputs["a_log"]).reshape(1, 8), "d_skip": f(inputs["d_skip"]).reshape(1, 8),
        "ssd_norm_g": f(inputs["ssd_norm_g"]).reshape(1, 512), "w_out": f(inputs["w_out"])[0],
        "ln1_g": f(inputs["ln1_g"]).reshape(1, DM), "ln1_b": f(inputs["ln1_b"]).reshape(1, DM),
        "router_group_w": f(inputs["router_group_w"])[0], "router_group_b": f(inputs["router_group_b"]).reshape(1, 4),
        "router_expert_w": f(inputs["router_expert_w"])[0], "router_expert_b": f(inputs["router_expert_b"]).reshape(1, 16),
        "w_gate": f(inputs["w_gate"])[0], "w_up": f(inputs["w_up"])[0], "w_down": f(inputs["w_down"])[0],
        "ln2_g": f(inputs["ln2_g"]).reshape(1, DM), "ln2_b": f(inputs["ln2_b"]).reshape(1, DM),
    }
    ncores = int(os.environ.get("KCORES", NCORES))
    in_maps = []
    for c in range(ncores):
        m = dict(shared)
        m["x"] = np.ascontiguousarray(x[c * NSEQ:(c + 1) * NSEQ])
        in_maps.append(m)
    res = run_bass_kernel_spmd(nc, in_maps, core_ids=list(range(ncores)))
    if os.environ.get("KDBG", ""):
        _CACHE["dbg"] = res.results
    outp = np.concatenate([r["out"] for r in res.results], axis=0)
    return outp.astype(np.float32)
```

```python
import os
import numpy as np
import concourse.bass as bass
import concourse.mybir as mybir
from concourse.bass_utils import run_bass_kernel_spmd

F32 = mybir.dt.float32
BF16 = mybir.dt.bfloat16
U8 = mybir.dt.uint8
AF = mybir.ActivationFunctionType
ALU = mybir.AluOpType
AX = mybir.AxisListType

NCORES = 8
NSEQ = 2
SEQ = 2048
NT = 16
DM = 1024
DIN = 3088
ALPHA = float(2.0 ** 0.25)
LN_EPS = 1e-5
RMS_EPS = 1e-5
ATT_SCALE = 0.125
NEG = -30000.0


class Op:
    __slots__ = ("eng", "fn", "reads", "writes", "deps", "signal", "sigval", "dma", "gi", "nofence")


class Sched:
    COMPUTE = ("pe", "act", "dve", "pool")

    def __init__(self, nc, n_dma_sems=40):
        self.nc = nc
        self.h = {"pe": nc.tensor, "act": nc.scalar, "dve": nc.vector, "pool": nc.gpsimd, "sp": nc.sync}
        self.ops = []
        self.last_w = {}
        self.readers = {}
        self.n_dma_sems = n_dma_sems
        self.live_dma = []
        self.nfence = 0

    def add(self, eng, fn, reads=(), writes=(), dma=False, nofence=False):
        o = Op()
        o.eng = eng; o.fn = fn; o.reads = tuple(reads); o.writes = tuple(writes)
        o.deps = []; o.signal = False; o.sigval = None; o.dma = dma; o.gi = len(self.ops); o.nofence = nofence
        for r in o.reads:
            p = self.last_w.get(r)
            if p is not None:
                self._dep(o, p, True)
            if r.startswith("ps"):
                rd = self.readers.get(r)
                if rd:
                    for q in rd.values():
                        if q.eng != eng:
                            self._dep(o, q, True)
        for w in o.writes:
            p = self.last_w.get(w)
            if p is not None:
                self._dep(o, p, False)
            rd = self.readers.get(w)
            if rd:
                for q in rd.values():
                    self._dep(o, q, False)
        for r in o.reads:
            d = self.readers.setdefault(r, {})
            d[("dma", o.gi) if dma else eng] = o
        for w in o.writes:
            self.last_w[w] = o
            self.readers[w] = {}
        self.ops.append(o)
        if dma and not nofence:
            self.live_dma.append(o)
        return o

    def _dep(self, o, p, raw):
        if p is o:
            return
        if (not p.dma) and (not o.dma) and p.eng == o.eng:
            if o.eng == "pe":
                return
        o.deps.append(p)
        p.signal = True

    def pe(self, fn, reads=(), writes=()): return self.add("pe", fn, reads, writes)
    def act(self, fn, reads=(), writes=()): return self.add("act", fn, reads, writes)
    def dve(self, fn, reads=(), writes=()): return self.add("dve", fn, reads, writes)
    def pool(self, fn, reads=(), writes=()): return self.add("pool", fn, reads, writes)
    def dma(self, q, fn, reads=(), writes=(), nofence=False):
        return self.add(q, fn, reads, writes, dma=True, nofence=nofence)

    def fence(self, scratch):
        n = self.nfence; self.nfence += 1
        a_keys = []
        col = {"pe": None, "act": 0, "dve": 1, "pool": 2}
        for e in ("act", "dve", "pool"):
            k = "fenceA.%d.%s" % (n, e)
            c = col[e]
            if e == "act":
                self.add(e, (lambda eh, c=c: eh.activation(out=scratch[:, c:c + 1], in_=scratch[:, 8:9], func=AF.Copy)), reads=(), writes=(k,))
            else:
                self.add(e, (lambda eh, c=c: eh.memset(scratch[:, c:c + 1], 0.0)), reads=(), writes=(k,))
            a_keys.append(k)
        k = "fenceA.%d.pe" % n
        self.add("pe", (lambda eh: eh.matmul(self.fence_ps[0:1, 0:1], lhsT=self.fence_w[0:1, 0:1], rhs=self.fence_w[0:1, 0:1], start=True, stop=True)),
                 reads=(), writes=(k, "ps7"))
        a_keys.append(k)
        dmas = self.live_dma
        self.live_dma = []
        for e in ("act", "dve", "pool", "pe", "sp"):
            kb = "fenceB.%d.%s" % (n, e)
            if e == "act":
                o = self.add(e, (lambda eh: eh.activation(out=scratch[:, 3:4], in_=scratch[:, 8:9], func=AF.Copy)), reads=a_keys, writes=(kb,))
            elif e == "pe":
                o = self.add(e, (lambda eh: eh.matmul(self.fence_ps[0:1, 1:2], lhsT=self.fence_w[0:1, 0:1], rhs=self.fence_w[0:1, 0:1], start=True, stop=True)),
                             reads=a_keys, writes=(kb, "ps7"))
            elif e == "sp":
                o = self.add(e, (lambda eh: eh.nop()), reads=a_keys, writes=(kb,))
            else:
                c = 4 if e == "dve" else 5
                o = self.add(e, (lambda eh, c=c: eh.memset(scratch[:, c:c + 1], 0.0)), reads=a_keys, writes=(kb,))
            for d in dmas:
                o.deps.append(d)

    def emit(self, final_wait_ops=()):
        nc = self.nc
        esem = {e: nc.alloc_semaphore("s_" + e) for e in self.COMPUTE}
        dsems = [nc.alloc_semaphore("s_dma%d" % i) for i in range(self.n_dma_sems)]
        dtotal = [0] * self.n_dma_sems
        dlast = [None] * self.n_dma_sems
        ecount = {e: 0 for e in self.COMPUTE}
        nd = 0
        nq = {"sp": 0, "pool": 0}
        half = self.n_dma_sems // 2
        for o in self.ops:
            if o.dma:
                qi = nq[o.eng]; nq[o.eng] += 1; nd += 1
                i = (qi % half) + (0 if o.eng == "sp" else half)
                prev = dlast[i]
                if prev is not None:
                    o.deps.append(prev)
                dtotal[i] += 16
                o.sigval = (dsems[i], dtotal[i], 1000 + i)
                dlast[i] = o
            elif o.signal:
                ecount[o.eng] += 1
                o.sigval = (esem[o.eng], ecount[o.eng], o.eng)
        known = {e: {} for e in self.h}
        nwaits = 0
        for o in self.ops:
            eh = self.h[o.eng]
            kn = known[o.eng]
            need = {}
            for p in o.deps:
                s, v, key = p.sigval
                if kn.get(key, 0) >= v:
                    continue
                if key not in need or need[key][1] < v:
                    need[key] = (s, v)
            for key, (s, v) in need.items():
                eh.wait_ge(s, v)
                kn[key] = v
                nwaits += 1
            ins = o.fn(eh)
            if o.dma:
                ins.then_inc(o.sigval[0], 16)
            elif o.signal:
                ins.then_inc(o.sigval[0], 1)
        eh = self.h["sp"]
        for o in final_wait_ops:
            s, v, key = o.sigval
            eh.wait_ge(s, v)
        self.stats = dict(n_ops=len(self.ops), n_waits=nwaits, counts=dict(ecount), n_dma=nd)
        return self.stats


class Arena:
    def __init__(self, nc, name, nbytes):
        self.t = nc.alloc_sbuf_tensor(name, [128, nbytes], U8)
        self.n = nbytes
        self.off = 0

    def reset(self, off=0):
        self.off = off

    def alloc(self, shape, dtype, parts=128):
        esz = 2 if dtype == BF16 else 4
        n = esz
        for s in shape:
            n *= s
        off = (self.off + 31) // 32 * 32
        assert off + n <= self.n, (off, n, self.n)
        self.off = off + n
        flat = self.t[0:parts, off:off + n].bitcast(dtype)
        if len(shape) == 1:
            return flat
        names = " ".join("a%d" % i for i in range(len(shape)))
        kw = {"a%d" % i: shape[i] for i in range(1, len(shape))}
        return flat.rearrange("p (%s) -> p %s" % (names, names), **kw)


def build_program(dbg=False):
    nc = bass.Bass("TRN2", target_bir_lowering=False)
    S = Sched(nc)
    D = {}

    def din(name, shape):
        D[name] = nc.dram_tensor(name, list(shape), F32, kind="ExternalInput").ap()
        return D[name]

    x = din("x", [NSEQ, SEQ, DM])
    w_in = din("w_in", [DM, DIN])
    b_in = din("b_in", [1, DIN])
    conv_w = din("conv_w", [4, 1024])
    conv_b = din("conv_b", [1, 1024])
    a_log = din("a_log", [1, 8])
    d_skip = din("d_skip", [1, 8])
    ssd_g = din("ssd_norm_g", [1, 512])
    w_out = din("w_out", [DM, DM])
    ln1_g = din("ln1_g", [1, DM]); ln1_b = din("ln1_b", [1, DM])
    rg_w = din("router_group_w", [DM, 4]); rg_b = din("router_group_b", [1, 4])
    re_w = din("router_expert_w", [4, DM, 4]); re_b = din("router_expert_b", [1, 16])
    w_gate = din("w_gate", [16, DM, 512]); w_up = din("w_up", [16, DM, 512]); w_down = din("w_down", [16, 512, DM])
    ln2_g = din("ln2_g", [1, DM]); ln2_b = din("ln2_b", [1, DM])
    out = nc.dram_tensor("out", [NSEQ, SEQ, DM], F32, kind="ExternalOutput").ap()
    h_scr = nc.dram_tensor("h_scr", [NSEQ, SEQ, DM], F32).ap()
    dbg_t = {}

    def dbg_out(name, shape):
        dbg_t[name] = nc.dram_tensor(name, list(shape), F32, kind="ExternalOutput").ap()
        return dbg_t[name]

    CONST = Arena(nc, "CONST", 22 * 1024)
    RW = Arena(nc, "RW", 49408)
    RX = Arena(nc, "RX", 32768)
    RACC = Arena(nc, "RACC", 65536)
    MSSD = Arena(nc, "MSSD", 16384)
    TT = Arena(nc, "TT", nc.sbuf_bytes_remaining - 256)

    identB = CONST.alloc([128], BF16); identF = CONST.alloc([128], F32)
    Uf = CONST.alloc([128], F32); triU = CONST.alloc([128], BF16); maskneg = CONST.alloc([128], BF16)
    onesF = CONST.alloc([128], F32)
    ones_row = CONST.alloc([128], BF16, parts=1)
    bz_row = CONST.alloc([512], BF16, parts=1); bv_row = CONST.alloc([512], BF16, parts=1)
    bdtf = CONST.alloc([16], F32)
    bxbc = CONST.alloc([8], F32); bq = CONST.alloc([4], F32); bk = CONST.alloc([4], F32)
    convw = CONST.alloc([8, 4], F32); convb = CONST.alloc([8], F32)
    a_bc = CONST.alloc([8], F32); dskip_bc = CONST.alloc([8], F32)
    gssd_bc = CONST.alloc([512], F32)
    lnG = CONST.alloc([1024], F32); lnB = CONST.alloc([1024], F32)
    rw = CONST.alloc([8, 20], F32); rb_bc = CONST.alloc([20], F32)
    logits = CONST.alloc([NT, 20], F32); comb = CONST.alloc([NT, 16], F32)
    fsc = CONST.alloc([16], F32)
    S.fence_w = CONST.alloc([8], BF16)
    pd = [nc.alloc_psum_tensor("pd%d" % i, [128, 1024], F32) for i in range(4)]
    def bank(i):
        return pd[i // 2][:, (i % 2) * 512:(i % 2) * 512 + 512]
    def bankb(i):
        return bank(i).bitcast(BF16)
    PSK = ["ps%d" % i for i in range(8)]
    S.fence_ps = nc.alloc_sbuf_tensor("fence_dummy", [1, 8], F32)
    S.fence_ps = bank(7)[:, 504:512]

    def fence():
        S.fence(fsc)

    S.pool(lambda e: e.memset(fsc[:, :], 0.0), writes=["fsc"])
    S.pool(lambda e: e.memset(S.fence_w[:, :], 0.0), writes=["fence_w"])
    S.pool(lambda e: e.memset(identB[:, :], 1.0), writes=["identB"])
    S.pool(lambda e: e.affine_select(out=identB[:, :], in_=identB[:, :], pattern=[[-1, 128]], compare_op=ALU.is_equal, fill=0.0, base=0, channel_multiplier=1), reads=["identB"], writes=["identB"])
    S.pool(lambda e: e.memset(identF[:, :], 1.0), writes=["identF"])
    S.pool(lambda e: e.affine_select(out=identF[:, :], in_=identF[:, :], pattern=[[-1, 128]], compare_op=ALU.is_equal, fill=0.0, base=0, channel_multiplier=1), reads=["identF"], writes=["identF"])
    S.pool(lambda e: e.memset(Uf[:, :], 1.0), writes=["Uf"])
    S.pool(lambda e: e.affine_select(out=Uf[:, :], in_=Uf[:, :], pattern=[[1, 128]], compare_op=ALU.is_ge, fill=0.0, base=0, channel_multiplier=-1), reads=["Uf"], writes=["Uf"])
    S.pool(lambda e: e.memset(triU[:, :], 1.0), writes=["triU"])
    S.pool(lambda e: e.affine_select(out=triU[:, :], in_=triU[:, :], pattern=[[1, 128]], compare_op=ALU.is_ge, fill=0.0, base=0, channel_multiplier=-1), reads=["triU"], writes=["triU"])
    S.pool(lambda e: e.memset(maskneg[:, :], NEG), writes=["maskneg"])
    S.pool(lambda e: e.affine_select(out=maskneg[:, :], in_=maskneg[:, :], pattern=[[-1, 128]], compare_op=ALU.is_gt, fill=0.0, base=0, channel_multiplier=1), reads=["maskneg"], writes=["maskneg"])
    S.pool(lambda e: e.memset(onesF[:, :], 1.0), writes=["onesF"])
    S.pool(lambda e: e.memset(ones_row[:, :], 1.0), writes=["ones_row"])
    S.dma("pool", lambda e: e.dma_start(out=bz_row[:, :], in_=b_in[0:1, 0:512]), writes=["bz_row"])
    S.dma("pool", lambda e: e.dma_start(out=bv_row[:, :], in_=b_in[0:1, 2568:3080]), writes=["bv_row"])
    S.dma("sp", lambda e: e.dma_start(out=bdtf[:, 0:8], in_=b_in[0:1, 1536:1544].to_broadcast([128, 8])), writes=["bdtf0"])
    S.dma("sp", lambda e: e.dma_start(out=bdtf[:, 8:16], in_=b_in[0:1, 3080:3088].to_broadcast([128, 8])), writes=["bdtf1"])
    S.dma("sp", lambda e: e.dma_start(out=bxbc[:, :], in_=b_in[0, 512:1536].rearrange("(c p) -> p c", p=128)), writes=["bxbc"])
    S.dma("sp", lambda e: e.dma_start(out=bq[:, :], in_=b_in[0, 1544:2056].rearrange("(c p) -> p c", p=128)), writes=["bq"])
    S.dma("sp", lambda e: e.dma_start(out=bk[:, :], in_=b_in[0, 2056:2568].rearrange("(c p) -> p c", p=128)), writes=["bk"])
    for k in range(4):
        S.dma("sp", lambda e, k=k: e.dma_start(out=convw[:, :, k], in_=conv_w[k, :].rearrange("(c p) -> p c", p=128)), writes=["convw%d" % k])
    CONVW = ["convw%d" % k for k in range(4)]
    S.dma("sp", lambda e: e.dma_start(out=convb[:, :], in_=conv_b[0, :].rearrange("(c p) -> p c", p=128)), writes=["convb"])
    S.dma("sp", lambda e: e.dma_start(out=a_bc[:, :], in_=a_log[0:1, :].to_broadcast([128, 8])), writes=["a_bc"])
    S.act(lambda e: e.activation(out=a_bc[:, :], in_=a_bc[:, :], func=AF.Exp), reads=["a_bc"], writes=["a_bc"])
    S.dve(lambda e: e.tensor_scalar(out=a_bc[:, :], in0=a_bc[:, :], scalar1=-1.0, scalar2=None, op0=ALU.mult), reads=["a_bc"], writes=["a_bc"])
    S.dma("sp", lambda e: e.dma_start(out=dskip_bc[:, :], in_=d_skip[0:1, :].to_broadcast([128, 8])), writes=["dskip_bc"])
    S.dma("sp", lambda e: e.dma_start(out=gssd_bc[:, :], in_=ssd_g[0:1, :].to_broadcast([128, 512])), writes=["gssd_bc"])
    S.dma("sp", lambda e: e.dma_start(out=rw[:, :, 0:4], in_=rg_w.rearrange("(k p) j -> p k j", p=128)), writes=["rw0"])
    for g in range(4):
        S.dma("sp", lambda e, g=g: e.dma_start(out=rw[:, :, 4 + 4 * g:8 + 4 * g], in_=re_w[g].rearrange("(k p) j -> p k j", p=128)), writes=["rw%d" % (g + 1)])
    RWK = ["rw%d" % i for i in range(5)]
    S.dma("sp", lambda e: e.dma_start(out=rb_bc[:, 0:4], in_=rg_b[0:1, :].to_broadcast([128, 4])), writes=["rb0"])
    S.dma("sp", lambda e: e.dma_start(out=rb_bc[:, 4:20], in_=re_b[0:1, :].to_broadcast([128, 16])), writes=["rb1"])

    outs = []

    for s in range(NSEQ):
        RW.reset(); RX.reset(); RACC.reset(); MSSD.reset(); TT.reset()
        wgu = [None, None]; wdn = [None, None]
        wgu[0] = RW.alloc([8, 1024], BF16); wdn[0] = RW.alloc([4, 1024], BF16)
        wgu[1] = RW.alloc([8, 1024], BF16); wdn[1] = RW.alloc([4, 1024], BF16)
        RW.reset()
        wA = RW.alloc([8, 1544], BF16)
        xb = [RW.alloc([4, 1024], BF16) for _ in range(2)]
        xT = RX.alloc([8, SEQ], BF16)
        sz = RACC.alloc([NT, 512], BF16)
        xsB = RACC.alloc([NT, 768], BF16)
        BT = RACC.alloc([2, SEQ], BF16)
        CT = RACC.alloc([2, SEQ], BF16)
        xsT = MSSD.alloc([4, SEQ], BF16)
        dt_t = TT.alloc([NT, 8], F32)
        tt_mark = TT.off
        pre = [TT.alloc([SEQ + 3], BF16) for _ in range(2)]
        diagw = TT.alloc([8, 4, 128], BF16)
        dt_raw = TT.alloc([NT, 8], F32)

        w_in_v = w_in.rearrange("(k p) c -> p k c", p=128)
        S.dma("pool", lambda e, s=s: e.dma_start(out=xb[0][:, :, :], in_=x[s, 0:512, :].rearrange("(t p) d -> p t d", p=128)), writes=["xb0"])
        S.dma("pool", lambda e: e.dma_start(out=wA[:, :, 0:512], in_=w_in_v[:, :, 0:512]), writes=["wA.z"])
        S.dma("pool", lambda e: e.dma_start(out=wA[:, :, 1536:1544], in_=w_in_v[:, :, 1536:1544]), writes=["wA.dt"])
        S.dma("pool", lambda e, s=s: e.dma_start(out=xb[1][:, :, :], in_=x[s, 512:1024, :].rearrange("(t p) d -> p t d", p=128)), writes=["xb1"])
        for half in range(2):
            S.dma("pool", lambda e, half=half: e.dma_start(out=wA[:, 4 * half:4 * half + 4, 512:1536], in_=w_in_v[:, 4 * half:4 * half + 4, 512:1536]), writes=["wA.x%d" % half])
        for b in range(2):
            S.pool(lambda e, b=b: e.memset(pre[b][:, 0:3], 0.0), writes=["prepad%d" % b])
        ev = 0
        for blk in range(4):
            if blk >= 2:
                S.dma("pool", lambda e, blk=blk, s=s: e.dma_start(out=xb[blk % 2][:, :, :], in_=x[s, blk * 512:(blk + 1) * 512, :].rearrange("(t p) d -> p t d", p=128)),
                      writes=["xb%d" % (blk % 2)])
            for k in range(8):
                pb = (blk * 8 + k) % 2
                for t in range(4):
                    S.pe(lambda e, pb=pb, t=t, k=k, blk=blk: e.transpose(bankb(pb)[:, t * 128:(t + 1) * 128], xb[blk % 2][:, t, k * 128:(k + 1) * 128], identB[:, :]),
                         reads=["xb%d" % (blk % 2), "identB"], writes=[PSK[pb]])
                if ev % 2 == 0:
                    S.act(lambda e, pb=pb, k=k, blk=blk: e.copy(xT[:, k, blk * 512:(blk + 1) * 512], bankb(pb)[:, 0:512]), reads=[PSK[pb]], writes=["xT.%d.%d" % (k, blk)])
                else:
                    S.dve(lambda e, pb=pb, k=k, blk=blk: e.tensor_copy(xT[:, k, blk * 512:(blk + 1) * 512], bankb(pb)[:, 0:512]), reads=[PSK[pb]], writes=["xT.%d.%d" % (k, blk)])
                ev += 1
            for tt in range(4):
                t = blk * 4 + tt
                pz = 2 + (t % 2)
                xk = ["xT.%d.%d" % (k, blk) for k in range(8)]
                for k in range(8):
                    S.pe(lambda e, pz=pz, t=t, k=k: e.matmul(bank(pz)[:, :], lhsT=xT[:, k, t * 128:(t + 1) * 128], rhs=wA[:, k, 0:512], start=(k == 0), stop=False),
                         reads=[xk[k], "wA.z"], writes=[PSK[pz]])
                S.pe(lambda e, pz=pz: e.matmul(bank(pz)[:, :], lhsT=ones_row[0:1, :], rhs=bz_row[0:1, :], start=False, stop=True),
                     reads=["ones_row", "bz_row"], writes=[PSK[pz]])
                S.act(lambda e, pz=pz, t=t: e.activation(out=sz[:, t, :], in_=bank(pz)[:, :], func=AF.Silu), reads=[PSK[pz]], writes=["sz.%d" % t])
                for k in range(8):
                    S.pe(lambda e, t=t, k=k: e.matmul(bank(4)[:, t * 8:(t + 1) * 8], lhsT=xT[:, k, t * 128:(t + 1) * 128], rhs=wA[:, k, 1536:1544], start=(k == 0), stop=(k == 7)),
                         reads=[xk[k], "wA.dt"], writes=[PSK[4]])
        S.dve(lambda e: e.tensor_tensor(out=dt_raw[:, :, :], in0=bank(4)[:, 0:128].rearrange("p (t h) -> p t h", h=8), in1=bdtf[:, 0:8].unsqueeze(1).to_broadcast([128, NT, 8]), op=ALU.add),
              reads=[PSK[4], "bdtf0"], writes=["dt_raw"])
        for c in range(8):
            for k in range(4):
                S.dve(lambda e, c=c, k=k: e.tensor_scalar(out=diagw[:, c, k, :], in0=identB[:, :], scalar1=convw[:, c, k:k + 1], scalar2=None, op0=ALU.mult),
                      reads=["identB"] + CONVW, writes=["diagw.%d" % c])

        def a1_ip(c):
            pb_ = c % 2
            for blk in range(4):
                pc = 5 + (c * 4 + blk) % 2
                for k in range(8):
                    S.pe(lambda e, pc=pc, c=c, k=k, blk=blk: e.matmul(bank(pc)[:, :], lhsT=wA[:, k, 512 + c * 128:512 + (c + 1) * 128], rhs=xT[:, k, blk * 512:(blk + 1) * 512], start=(k == 0), stop=(k == 7)),
                         reads=["xT.%d.%d" % (k, blk), "wA.x%d" % (k // 4)], writes=[PSK[pc]])
                S.act(lambda e, pc=pc, c=c, blk=blk, pb_=pb_: e.activation(out=pre[pb_][:, 3 + blk * 512:3 + (blk + 1) * 512], in_=bank(pc)[:, :], func=AF.Identity, bias=bxbc[:, c:c + 1], scale=1.0),
                      reads=[PSK[pc], "bxbc"], writes=["pre%d.%d" % (pb_, blk)])

        def a1_conv(c):
            pb_ = c % 2
            if c < 4:
                dstt = xsT[:, c, :]; dk = "xsT.%d" % c
            elif c < 6:
                dstt = BT[:, c - 4, :]; dk = "BT.%d" % (c - 4)
            else:
                dstt = CT[:, c - 6, :]; dk = "CT.%d" % (c - 6)
            for blk in range(4):
                pc = 2 + (c * 4 + blk) % 2
                rk = ["pre%d.%d" % (pb_, blk), "diagw.%d" % c] + (["pre%d.%d" % (pb_, blk - 1)] if blk > 0 else ["prepad%d" % pb_])
                for k in range(4):
                    S.pe(lambda e, pc=pc, c=c, k=k, blk=blk, pb_=pb_: e.matmul(bank(pc)[:, :], lhsT=diagw[:, c, k, :], rhs=pre[pb_][:, blk * 512 + k:blk * 512 + k + 512], start=(k == 0), stop=(k == 3)),
                         reads=rk, writes=[PSK[pc]])
                S.act(lambda e, pc=pc, dstt=dstt, c=c, blk=blk: e.activation(out=dstt[:, blk * 512:(blk + 1) * 512], in_=bank(pc)[:, :], func=AF.Silu, bias=convb[:, c:c + 1], scale=1.0),
                      reads=[PSK[pc], "convb"], writes=[dk])

        a1_ip(0)
        for c in range(8):
            if c + 1 < 8:
                a1_ip(c + 1)
            a1_conv(c)
        for t in range(NT):
            pb = t % 2
            for c in range(6):
                src = xsT[:, c, t * 128:(t + 1) * 128] if c < 4 else BT[:, c - 4, t * 128:(t + 1) * 128]
                sk = "xsT.%d" % c if c < 4 else "BT.%d" % (c - 4)
                S.pe(lambda e, pb=pb, c=c, src=src: e.transpose(bankb(pb)[:, c * 128:(c + 1) * 128], src, identB[:, :]), reads=[sk, "identB"], writes=[PSK[pb]])
            if t % 2 == 0:
                S.dve(lambda e, pb=pb, t=t: e.tensor_copy(xsB[:, t, :], bankb(pb)[:, 0:768]), reads=[PSK[pb]], writes=["xsB.%d" % t])
            else:
                S.act(lambda e, pb=pb, t=t: e.copy(xsB[:, t, :], bankb(pb)[:, 0:768]), reads=[PSK[pb]], writes=["xsB.%d" % t])
        S.act(lambda e: e.activation(out=dt_t[:, :, :], in_=dt_raw[:, :, :], func=AF.Exp), reads=["dt_raw"], writes=["dt_t"])
        S.act(lambda e: e.activation(out=dt_t[:, :, :], in_=dt_t[:, :, :], func=AF.Ln, bias=1.0, scale=1.0), reads=["dt_t"], writes=["dt_t"])

        fence()
        MSSD.reset(); TT.reset(tt_mark)
        RW.reset()
        wB = RW.alloc([8, 1544], BF16)
        wo = RW.alloc([8, 1024], BF16)
        for half in range(2):
            S.dma("pool", lambda e, half=half: e.dma_start(out=wB[:, 4 * half:4 * half + 4, :], in_=w_in.rearrange("(k p) c -> p k c", p=128)[:, 4 * half:4 * half + 4, 1544:3088]),
                  writes=["wB"], nofence=True)
        for half in range(2):
            S.dma("pool", lambda e, half=half: e.dma_start(out=wo[:, 4 * half:4 * half + 4, :], in_=w_out.rearrange("(k p) c -> p k c", p=128)[:, 4 * half:4 * half + 4, :]),
                  writes=["wo"], nofence=True)
        m_ssd = MSSD.alloc([NT, 512], BF16)
        da = TT.alloc([NT, 8], F32); acum = TT.alloc([NT, 8], F32); nacum = TT.alloc([NT, 8], F32)
        alast = TT.alloc([NT, 8], F32); dte = TT.alloc([NT, 8], F32); ea = TT.alloc([NT, 8], F32)
        cdec = TT.alloc([NT, 8], F32); dtdte = TT.alloc([NT, 8], F32)
        stT = TT.alloc([8, 64], F32); stTb = TT.alloc([8, 64], BF16)
        LT = [TT.alloc([128], BF16) for _ in range(4)]
        MT = [TT.alloc([128], BF16) for _ in range(4)]
        xdt = [TT.alloc([8, 64], BF16) for _ in range(2)]
        xdtd = [TT.alloc([8, 64], BF16) for _ in range(2)]
        t1 = [TT.alloc([8, 64], F32)] * 2
        t2 = [TT.alloc([8, 64], F32) for _ in range(2)]
        yg = [TT.alloc([512], F32) for _ in range(2)]
        junk = TT.alloc([256], F32)
        ss = [TT.alloc([2], F32) for _ in range(2)]
        rstd = [TT.alloc([2], F32) for _ in range(2)]

        S.dve(lambda e: e.tensor_tensor(out=da[:, :, :], in0=dt_t[:, :, :], in1=a_bc[:, :].unsqueeze(1).to_broadcast([128, NT, 8]), op=ALU.mult), reads=["dt_t", "a_bc"], writes=["da"])
        daf = da.rearrange("p t h -> p (t h)")
        S.pe(lambda e: e.matmul(bank(5)[:, 0:128], lhsT=Uf[:, :], rhs=daf, start=True, stop=True), reads=["Uf", "da"], writes=[PSK[5]])
        S.pe(lambda e: e.matmul(bank(6)[:, 0:128], lhsT=onesF[:, :], rhs=daf, start=True, stop=True), reads=["onesF", "da"], writes=[PSK[6]])
        S.dve(lambda e: e.tensor_copy(acum.rearrange("p t h -> p (t h)"), bank(5)[:, 0:128]), reads=[PSK[5]], writes=["acum"])
        S.dve(lambda e: e.tensor_scalar(out=nacum.rearrange("p t h -> p (t h)"), in0=bank(5)[:, 0:128], scalar1=-1.0, scalar2=None, op0=ALU.mult), reads=[PSK[5]], writes=["nacum"])
        S.dve(lambda e: e.tensor_copy(alast.rearrange("p t h -> p (t h)"), bank(6)[:, 0:128]), reads=[PSK[6]], writes=["alast"])
        S.dve(lambda e: e.tensor_tensor(out=dte[:, :, :], in0=alast[:, :, :], in1=acum[:, :, :], op=ALU.subtract), reads=["alast", "acum"], writes=["dte"])
        S.act(lambda e: e.activation(out=dte[:, :, :], in_=dte[:, :, :], func=AF.Exp), reads=["dte"], writes=["dte"])
        S.act(lambda e: e.activation(out=ea[:, :, :], in_=acum[:, :, :], func=AF.Exp), reads=["acum"], writes=["ea"])
        S.act(lambda e: e.activation(out=cdec[:, :, :], in_=alast[:, :, :], func=AF.Exp), reads=["alast"], writes=["cdec"])
        S.dve(lambda e: e.tensor_tensor(out=dtdte[:, :, :], in0=dt_t[:, :, :], in1=dte[:, :, :], op=ALU.mult), reads=["dt_t", "dte"], writes=["dtdte"])
        S.pool(lambda e: e.memset(stT[:, :, :], 0.0), writes=["stT"])
        S.pool(lambda e: e.memset(stTb[:, :, :], 0.0), writes=["stTb"])

        def ssd_F(c):
            cs = slice(c * 128, (c + 1) * 128)
            b2 = c % 2
            py = 2 + b2
            xs_c = xsB[:, c, 0:512].rearrange("p (h d) -> p h d", h=8)
            S.pool(lambda e, c=c, b2=b2, xs_c=xs_c: e.tensor_tensor(out=xdt[b2][:, :, :], in0=xs_c, in1=dt_t[:, c, :].unsqueeze(2).to_broadcast([128, 8, 64]), op=ALU.mult),
                   reads=["xsB.%d" % c, "dt_t"], writes=["xdt%d" % b2])
            S.pool(lambda e, c=c, b2=b2, xs_c=xs_c: e.tensor_tensor(out=xdtd[b2][:, :, :], in0=xs_c, in1=dtdte[:, c, :].unsqueeze(2).to_broadcast([128, 8, 64]), op=ALU.mult),
                   reads=["xsB.%d" % c, "dtdte"], writes=["xdtd%d" % b2])
            S.pool(lambda e, c=c, b2=b2, xs_c=xs_c: e.tensor_tensor(out=t2[b2][:, :, :], in0=xs_c, in1=dskip_bc[:, :].unsqueeze(2).to_broadcast([128, 8, 64]), op=ALU.mult),
                   reads=["xsB.%d" % c, "dskip_bc"], writes=["t2%d" % b2])
            for g in range(2):
                S.pe(lambda e, g=g, cs=cs: e.matmul(bank(4)[:, g * 128:(g + 1) * 128], lhsT=BT[:, g, cs], rhs=CT[:, g, cs], start=True, stop=True),
                     reads=["BT.%d" % g, "CT.%d" % g], writes=[PSK[4]])
            for hh in range(2):
                pl = hh
                for h4 in range(4):
                    h = hh * 4 + h4
                    S.pe(lambda e, pl=pl, h4=h4, c=c, h=h: e.matmul(bank(pl)[:, h4 * 128:(h4 + 1) * 128], lhsT=da[:, c, h:h + 1].to_broadcast([128, 128]), rhs=Uf[:, :], start=True, stop=False),
                         reads=["da", "Uf"], writes=[PSK[pl]])
                    S.pe(lambda e, pl=pl, h4=h4: e.matmul(bank(pl)[:, h4 * 128:(h4 + 1) * 128], lhsT=identB[:, :], rhs=maskneg[:, :], start=False, stop=True),
                         reads=["identB", "maskneg"], writes=[PSK[pl]])
            for hh in range(2):
                pl = hh
                for h4 in range(4):
                    h = hh * 4 + h4
                    S.act(lambda e, pl=pl, h4=h4, c=c, h=h: e.activation(out=LT[h4][:, :], in_=bank(pl)[:, h4 * 128:(h4 + 1) * 128], func=AF.Exp, bias=nacum[:, c, h:h + 1], scale=1.0),
                          reads=[PSK[pl], "nacum"], writes=["LT%d" % h4])
                    g = h // 4
                    S.dve(lambda e, h4=h4, g=g: e.tensor_tensor(out=MT[h4][:, :], in0=bank(4)[:, g * 128:(g + 1) * 128], in1=LT[h4][:, :], op=ALU.mult),
                          reads=[PSK[4], "LT%d" % h4], writes=["MT%d" % h4])
                    S.pe(lambda e, h4=h4, h=h, b2=b2, py=py: e.matmul(bank(py)[:, h * 64:(h + 1) * 64], lhsT=MT[h4][:, :], rhs=xdt[b2][:, h, :], start=True, stop=True),
                         reads=["MT%d" % h4, "xdt%d" % b2], writes=[PSK[py]])

        def ssd_B(c):
            cs = slice(c * 128, (c + 1) * 128)
            b2 = c % 2
            py = 2 + b2
            if c > 0:
                for g in range(2):
                    S.pe(lambda e, g=g, cs=cs: e.matmul(bank(6)[:, g * 256:(g + 1) * 256], lhsT=CT[:, g, cs], rhs=stTb[:, 4 * g:4 * g + 4, :].rearrange("p h d -> p (h d)"), start=True, stop=True),
                         reads=["CT.%d" % g, "stTb"], writes=[PSK[6]])
            if c < NT - 1:
                for g in range(2):
                    S.pe(lambda e, g=g, c=c, b2=b2: e.matmul(bank(7)[:, g * 256:(g + 1) * 256], lhsT=xsB[:, c, 512 + g * 128:512 + (g + 1) * 128], rhs=xdtd[b2][:, 4 * g:4 * g + 4, :].rearrange("p h d -> p (h d)"), start=True, stop=True),
                         reads=["xsB.%d" % c, "xdtd%d" % b2], writes=[PSK[7]])
                S.dve(lambda e, c=c: e.tensor_tensor(out=stT[:, :, :], in0=stT[:, :, :], in1=cdec[:, c, :].unsqueeze(2).to_broadcast([128, 8, 64]), op=ALU.mult),
                      reads=["stT", "cdec"], writes=["stT"])
                S.dve(lambda e: e.tensor_tensor(out=stT[:, :, :], in0=bank(7)[:, 0:512].rearrange("p (h d) -> p h d", h=8), in1=stT[:, :, :], op=ALU.add),
                      reads=[PSK[7], "stT"], writes=["stT"])
            if c > 0:
                S.dve(lambda e, c=c, b2=b2: e.tensor_tensor(out=t1[b2][:, :, :], in0=bank(6)[:, :].rearrange("p (h d) -> p h d", h=8), in1=ea[:, c, :].unsqueeze(2).to_broadcast([128, 8, 64]), op=ALU.mult),
                      reads=[PSK[6], "ea"], writes=["t1"])
            if c < NT - 1:
                S.act(lambda e: e.copy(stTb[:, :, :], stT[:, :, :]), reads=["stT"], writes=["stTb"])
            if c > 0:
                S.dve(lambda e, b2=b2, py=py: e.tensor_tensor(out=t1[b2][:, :, :], in0=bank(py)[:, :].rearrange("p (h d) -> p h d", h=8), in1=t1[b2][:, :, :], op=ALU.add),
                      reads=[PSK[py], "t1"], writes=["t1"])
                S.dve(lambda e, b2=b2: e.tensor_tensor(out=t1[b2][:, :, :], in0=t1[b2][:, :, :], in1=t2[b2][:, :, :], op=ALU.add),
                      reads=["t1", "t2%d" % b2], writes=["t1"])
            else:
                S.dve(lambda e, b2=b2, py=py: e.tensor_tensor(out=t1[b2][:, :, :], in0=bank(py)[:, :].rearrange("p (h d) -> p h d", h=8), in1=t2[b2][:, :, :], op=ALU.add),
                      reads=[PSK[py], "t2%d" % b2], writes=["t1"])
            S.dve(lambda e, c=c, b2=b2: e.tensor_tensor(out=yg[b2][:, :], in0=t1[b2].rearrange("p h d -> p (h d)"), in1=sz[:, c, :], op=ALU.mult),
                  reads=["t1", "sz.%d" % c], writes=["yg%d" % b2])
            for g in range(2):
                S.act(lambda e, g=g, b2=b2: e.activation(out=junk[:, :], in_=yg[b2][:, g * 256:(g + 1) * 256], func=AF.Square, accum_out=ss[b2][:, g:g + 1]),
                      reads=["yg%d" % b2], writes=["junk", "ss%d.%d" % (b2, g)])
            S.act(lambda e, b2=b2: e.activation(out=rstd[b2][:, :], in_=ss[b2][:, :], func=AF.Ln, bias=RMS_EPS, scale=1.0 / 256.0),
                  reads=["ss%d.0" % b2, "ss%d.1" % b2], writes=["rstd%d" % b2])
            S.act(lambda e, b2=b2: e.activation(out=rstd[b2][:, :], in_=rstd[b2][:, :], func=AF.Exp, scale=-0.5), reads=["rstd%d" % b2], writes=["rstd%d" % b2])
            for g in range(2):
                S.dve(lambda e, g=g, b2=b2, c=c: e.scalar_tensor_tensor(out=m_ssd[:, c, g * 256:(g + 1) * 256], in0=yg[b2][:, g * 256:(g + 1) * 256], scalar=rstd[b2][:, g:g + 1], in1=gssd_bc[:, g * 256:(g + 1) * 256], op0=ALU.mult, op1=ALU.mult),
                      reads=["yg%d" % b2, "rstd%d" % b2, "gssd_bc"], writes=["m_ssd.%d" % c])

        ssd_F(0)
        for c in range(NT):
            if c + 1 < NT:
                ssd_F(c + 1)
            ssd_B(c)

        if dbg == 'ssd' and s == 0:
            RX.reset()
            dm = dbg_out("dbg_mssd", [128, NT * 512])
            cvt = RX.alloc([NT * 512], F32) if dbg == 'ssd' else None
            S.dve(lambda e: e.tensor_copy(cvt[:, :], m_ssd.rearrange("p t d -> p (t d)")), reads=["m_ssd.%d" % c for c in range(NT)], writes=["cvt"])
            outs.append(S.dma("sp", lambda e: e.dma_start(out=dm[:, :], in_=cvt[:, :]), reads=["cvt"]))
        if dbg == "ssd":
            break

        fence()
        RW.reset(); RACC.reset(); TT.reset()
        wB = RW.alloc([8, 1544], BF16)
        wo = RW.alloc([8, 1024], BF16)
        ah = RW.alloc([1024], F32)
        hTf = RW.alloc([8, 128], F32)
        qT = RACC.alloc([4, SEQ], BF16)
        kT = RACC.alloc([4, SEQ], BF16)
        v_aug = RACC.alloc([NT, 8, 65], BF16)
        xres = [RACC.alloc([1024], F32) for _ in range(2)]
        hpre = [RACC.alloc([1024], F32), TT.alloc([1024], F32)]
        f_raw = TT.alloc([NT, 8], F32); Gc = TT.alloc([NT, 8], F32); tot = TT.alloc([NT, 8], F32)
        Pp = TT.alloc([NT, 8], F32); Gf = TT.alloc([NT, 8], F32); Gend = TT.alloc([NT, 8], F32)
        biasT = TT.alloc([8, NT, NT], F32)
        PT = [TT.alloc([8, 128], BF16) for _ in range(2)]
        yatt = [TT.alloc([8, 64], BF16) for _ in range(2)]
        rden = [TT.alloc([8], F32) for _ in range(2)]
        mT = [TT.alloc([8, 128], BF16)] * 2
        bst = [TT.alloc([2, 6], F32) for _ in range(2)]
        mv = [TT.alloc([2], F32) for _ in range(2)]
        rs1 = [TT.alloc([1], F32) for _ in range(2)]

        S.dma("sp", lambda e: e.dma_start(out=lnG[:, :], in_=ln1_g[0:1, :].to_broadcast([128, 1024])), writes=["lnG"])
        S.dma("sp", lambda e: e.dma_start(out=lnB[:, :], in_=ln1_b[0:1, :].to_broadcast([128, 1024])), writes=["lnB"])
        S.pool(lambda e: e.memset(v_aug[:, :, :, 64:65], 1.0), writes=["v_ones"])
        evq = 0
        for qk in range(2):
            dstT = qT if qk == 0 else kT
            bcol = bq if qk == 0 else bk
            nm = "qT" if qk == 0 else "kT"
            for p in range(4):
                for blk in range(4):
                    pq = evq % 4
                    for k in range(8):
                        S.pe(lambda e, pq=pq, k=k, p=p, blk=blk, qk=qk: e.matmul(bank(pq)[:, :], lhsT=wB[:, k, qk * 512 + p * 128:qk * 512 + (p + 1) * 128], rhs=xT[:, k, blk * 512:(blk + 1) * 512], start=(k == 0), stop=(k == 7)),
                             reads=["xT.%d.%d" % (k, blk), "wB"], writes=[PSK[pq]])
                    if evq % 2 == 0:
                        S.act(lambda e, pq=pq, p=p, blk=blk, dstT=dstT, bcol=bcol: e.activation(out=dstT[:, p, blk * 512:(blk + 1) * 512], in_=bank(pq)[:, :], func=AF.Identity, bias=bcol[:, p:p + 1], scale=1.0),
                              reads=[PSK[pq], "bq", "bk"], writes=["%s.%d.%d" % (nm, p, blk)])
                    else:
                        S.dve(lambda e, pq=pq, p=p, blk=blk, dstT=dstT, bcol=bcol: e.tensor_scalar(out=dstT[:, p, blk * 512:(blk + 1) * 512], in0=bank(pq)[:, :], scalar1=bcol[:, p:p + 1], scalar2=None, op0=ALU.add),
                              reads=[PSK[pq], "bq", "bk"], writes=["%s.%d.%d" % (nm, p, blk)])
                    evq += 1
        for t in range(NT):
            pv = 4 + (t % 2)
            blk = t // 4
            for k in range(8):
                S.pe(lambda e, pv=pv, t=t, k=k: e.matmul(bank(pv)[:, :], lhsT=xT[:, k, t * 128:(t + 1) * 128], rhs=wB[:, k, 1024:1536], start=(k == 0), stop=False),
                     reads=["xT.%d.%d" % (k, blk), "wB"], writes=[PSK[pv]])
            S.pe(lambda e, pv=pv: e.matmul(bank(pv)[:, :], lhsT=ones_row[0:1, :], rhs=bv_row[0:1, :], start=False, stop=True),
                 reads=["ones_row", "bv_row"], writes=[PSK[pv]])
            if t % 2 == 0:
                S.act(lambda e, pv=pv, t=t: e.copy(v_aug[:, t, :, 0:64], bank(pv)[:, :].rearrange("p (h d) -> p h d", h=8)), reads=[PSK[pv]], writes=["v.%d" % t])
            else:
                S.dve(lambda e, pv=pv, t=t: e.tensor_copy(v_aug[:, t, :, 0:64], bank(pv)[:, :].rearrange("p (h d) -> p h d", h=8)), reads=[PSK[pv]], writes=["v.%d" % t])
            for k in range(8):
                S.pe(lambda e, t=t, k=k: e.matmul(bank(6)[:, t * 8:(t + 1) * 8], lhsT=xT[:, k, t * 128:(t + 1) * 128], rhs=wB[:, k, 1536:1544], start=(k == 0), stop=(k == 7)),
                     reads=["xT.%d.%d" % (k, blk), "wB"], writes=[PSK[6]])
        S.dve(lambda e: e.tensor_tensor(out=f_raw[:, :, :], in0=bank(6)[:, 0:128].rearrange("p (t h) -> p t h", h=8), in1=bdtf[:, 8:16].unsqueeze(1).to_broadcast([128, NT, 8]), op=ALU.add),
              reads=[PSK[6], "bdtf1"], writes=["f_raw"])
        S.act(lambda e: e.activation(out=f_raw[:, :, :], in_=f_raw[:, :, :], func=AF.Exp, scale=-1.0), reads=["f_raw"], writes=["f_raw"])
        S.act(lambda e: e.activation(out=f_raw[:, :, :], in_=f_raw[:, :, :], func=AF.Ln, bias=1.0, scale=1.0), reads=["f_raw"], writes=["f_raw"])
        frf = f_raw.rearrange("p t h -> p (t h)")
        S.pe(lambda e: e.matmul(bank(7)[:, 0:128], lhsT=Uf[:, :], rhs=frf, start=True, stop=True), reads=["Uf", "f_raw"], writes=[PSK[7]])
        S.pe(lambda e: e.matmul(bank(7)[:, 128:256], lhsT=onesF[:, :], rhs=frf, start=True, stop=True), reads=["onesF", "f_raw"], writes=[PSK[7]])
        S.dve(lambda e: e.tensor_copy(Gc.rearrange("p t h -> p (t h)"), bank(7)[:, 0:128]), reads=[PSK[7]], writes=["Gc"])
        S.dve(lambda e: e.tensor_copy(tot.rearrange("p t h -> p (t h)"), bank(7)[:, 128:256]), reads=[PSK[7]], writes=["tot"])
        S.pool(lambda e: e.memset(Pp[:, 0, :], 0.0), writes=["Pp"])
        for t in range(1, NT):
            S.dve(lambda e, t=t: e.tensor_tensor(out=Pp[:, t, :], in0=Pp[:, t - 1, :], in1=tot[:, t - 1, :], op=ALU.add), reads=["Pp", "tot"], writes=["Pp"])
        S.dve(lambda e: e.tensor_tensor(out=Gf[:, :, :], in0=Gc[:, :, :], in1=Pp[:, :, :], op=ALU.add), reads=["Gc", "Pp"], writes=["Gf"])
        S.dve(lambda e: e.tensor_tensor(out=Gend[:, :, :], in0=tot[:, :, :], in1=Pp[:, :, :], op=ALU.add), reads=["tot", "Pp"], writes=["Gend"])
        for h in range(8):
            S.dve(lambda e, h=h: e.tensor_tensor(out=biasT[:, h, :, :], in0=Gf[:, :, h].unsqueeze(1).to_broadcast([128, NT, NT]), in1=Gend[:, :, h].unsqueeze(2).to_broadcast([128, NT, NT]), op=ALU.subtract),
                  reads=["Gf", "Gend"], writes=["biasT"])

        fence()
        RX.reset()
        hT = RX.alloc([8, SEQ], BF16)
        NEXP = int(os.environ.get("KNEXP", 16))

        def load_expert(ex):
            sl = ex % 2
            S.dma("pool", lambda e, ex=ex, sl=sl: e.dma_start(out=wgu[sl][:, :, 0:512], in_=w_gate[ex].rearrange("(k p) f -> p k f", p=128)), writes=["wg.%d" % sl], nofence=(ex == 0))
            S.dma("pool", lambda e, ex=ex, sl=sl: e.dma_start(out=wgu[sl][:, :, 512:1024], in_=w_up[ex].rearrange("(k p) f -> p k f", p=128)), writes=["wu.%d" % sl], nofence=(ex == 0))
            S.dma("pool", lambda e, ex=ex, sl=sl: e.dma_start(out=wdn[sl][:, :, :], in_=w_down[ex].rearrange("(k p) d -> p k d", p=128)), writes=["wd.%d" % sl], nofence=(ex == 0))


        load_expert(0)
        NTI = int(os.environ.get("KATT_TILES", NT))
        units = []
        for i in range(NTI):
            for p in range(4):
                for j0 in range(0, i + 1, 4):
                    units.append((i, p, list(range(j0, min(j0 + 4, i + 1)))))

        def emit_S(u, gi):
            i, p, js = u
            pb0 = 2 * (gi % 2)
            for jj, j in enumerate(js):
                for hh in range(2):
                    r0 = hh * 64
                    S.pe(lambda e, bk=pb0 + hh, jj=jj, j=j, p=p, r0=r0, i=i: e.matmul(bank(bk)[:, jj * 128:(jj + 1) * 128], lhsT=kT[r0:r0 + 64, p, j * 128:(j + 1) * 128], rhs=qT[r0:r0 + 64, p, i * 128:(i + 1) * 128], start=True, stop=True),
                         reads=["kT.%d.%d" % (p, j // 4), "qT.%d.%d" % (p, i // 4)], writes=[PSK[pb0 + hh]])

        def emit_E(u, gi):
            i, p, js = u
            pb0 = 2 * (gi % 2); pb = gi % 2
            for jj, j in enumerate(js):
                for hh in range(2):
                    h = 2 * p + hh
                    c8 = jj * 2 + hh
                    S.act(lambda e, bk=pb0 + hh, pb=pb, c8=c8, jj=jj, j=j, h=h, i=i: e.activation(out=PT[pb][:, c8, :], in_=bank(bk)[:, jj * 128:(jj + 1) * 128], func=AF.Exp, bias=biasT[:, h, i, j:j + 1], scale=ATT_SCALE),
                          reads=[PSK[pb0 + hh], "biasT"], writes=["PT%d.%d" % (pb, c8)])
                    if j == i:
                        S.dve(lambda e, pb=pb, c8=c8: e.tensor_tensor(out=PT[pb][:, c8, :], in0=PT[pb][:, c8, :], in1=triU[:, :], op=ALU.mult),
                              reads=["PT%d.%d" % (pb, c8), "triU"], writes=["PT%d.%d" % (pb, c8)])

        def emit_V(u, gi):
            i, p, js = u
            pb = gi % 2
            for jj, j in enumerate(js):
                for hh in range(2):
                    h = 2 * p + hh
                    c8 = jj * 2 + hh
                    po = 4 + h // 4; oc = (h % 4) * 65
                    S.pe(lambda e, pb=pb, c8=c8, j=j, h=h, po=po, oc=oc, i=i: e.matmul(bank(po)[:, oc:oc + 65], lhsT=PT[pb][:, c8, :], rhs=v_aug[:, j, h, :], start=(j == 0 and h % 4 == 0), stop=(j == i and h % 4 == 3)),
                         reads=["PT%d.%d" % (pb, c8), "v.%d" % j, "v_ones"], writes=[PSK[po]])

        def tail_N(i):
            b2 = i % 2
            S.dma("sp", lambda e, i=i, b2=b2, s=s: e.dma_start(out=xres[b2][:, :], in_=x[s, i * 128:(i + 1) * 128, :]), writes=["xres%d" % b2])
            for hb in range(2):
                ov = bank(4 + hb)[:, 0:260].rearrange("p (h d) -> p h d", h=4)
                S.dve(lambda e, hb=hb, ov=ov, b2=b2: e.reciprocal(rden[b2][:, 4 * hb:4 * hb + 4], ov[:, :, 64]), reads=[PSK[4 + hb]], writes=["rden%d.%d" % (b2, hb)])
                S.dve(lambda e, hb=hb, ov=ov, b2=b2: e.tensor_tensor(out=yatt[b2][:, 4 * hb:4 * hb + 4, :], in0=ov[:, :, 0:64], in1=rden[b2][:, 4 * hb:4 * hb + 4].unsqueeze(2).to_broadcast([128, 4, 64]), op=ALU.mult),
                      reads=[PSK[4 + hb], "rden%d.%d" % (b2, hb)], writes=["yatt%d.%d" % (b2, hb)])

        def tail_T(i):
            b2 = i % 2
            yf = yatt[b2].rearrange("p h d -> p (h d)")
            for ec in range(8):
                src = m_ssd[:, i, ec * 128:(ec + 1) * 128] if ec < 4 else yf[:, (ec - 4) * 128:(ec - 3) * 128]
                rk = ["m_ssd.%d" % i] if ec < 4 else ["yatt%d.%d" % (b2, (ec - 4) // 2)]
                S.pe(lambda e, ec=ec, src=src: e.transpose(bankb(6)[:, ec * 128:(ec + 1) * 128], src, identB[:, :]), reads=rk + ["identB"], writes=[PSK[6]])
            S.dve(lambda e, b2=b2: e.tensor_copy(mT[b2].rearrange("p a b -> p (a b)"), bankb(6)[:, 0:1024]), reads=[PSK[6]], writes=["mT"])

        def tail_Oq(i, q):
            b2 = i % 2
            half = q // 2
            hp = hpre[b2]
            for ec in range(4 * (q % 2), 4 * (q % 2) + 4):
                S.pe(lambda e, ec=ec, b2=b2, half=half: e.matmul(bank(7)[:, :], lhsT=mT[b2][:, ec, :], rhs=wo[:, ec, half * 512:(half + 1) * 512], start=(ec == 0), stop=(ec == 7)),
                     reads=["mT", "wo"], writes=[PSK[7]])
            if q % 2 == 1:
                S.dve(lambda e, hp=hp, b2=b2, half=half: e.scalar_tensor_tensor(out=hp[:, half * 512:(half + 1) * 512], in0=xres[b2][:, half * 512:(half + 1) * 512], scalar=ALPHA, in1=bank(7)[:, :], op0=ALU.mult, op1=ALU.add),
                      reads=["xres%d" % b2, PSK[7]], writes=["hpre%d.%d" % (b2, half)])
                S.dve(lambda e, hp=hp, half=half, b2=b2: e.bn_stats(bst[b2][:, half, :], hp[:, half * 512:(half + 1) * 512]), reads=["hpre%d.%d" % (b2, half)], writes=["bst%d.%d" % (b2, half)])
            if q == 3:
                S.dve(lambda e, b2=b2: e.bn_aggr(mv[b2][:, :], bst[b2][:, :, :]), reads=["bst%d.0" % b2, "bst%d.1" % b2], writes=["mv%d" % b2])

        def tail_L(i):
            b2 = i % 2
            hp = hpre[b2]
            HK = ["hpre%d.0" % b2, "hpre%d.1" % b2]
            S.act(lambda e, b2=b2: e.activation(out=rs1[b2][:, :], in_=mv[b2][:, 1:2], func=AF.Ln, bias=LN_EPS, scale=1.0), reads=["mv%d" % b2], writes=["rs1%d" % b2])
            S.act(lambda e, b2=b2: e.activation(out=rs1[b2][:, :], in_=rs1[b2][:, :], func=AF.Exp, scale=-0.5), reads=["rs1%d" % b2], writes=["rs1%d" % b2])
            S.dve(lambda e, hp=hp, b2=b2: e.tensor_scalar(out=hp[:, :], in0=hp[:, :], scalar1=mv[b2][:, 0:1], scalar2=rs1[b2][:, 0:1], op0=ALU.subtract, op1=ALU.mult),
                  reads=HK + ["mv%d" % b2, "rs1%d" % b2], writes=HK)
            S.dve(lambda e, hp=hp: e.tensor_tensor(out=hp[:, :], in0=hp[:, :], in1=lnG[:, :], op=ALU.mult), reads=HK + ["lnG"], writes=HK)
            S.dve(lambda e, hp=hp: e.tensor_tensor(out=hp[:, :], in0=hp[:, :], in1=lnB[:, :], op=ALU.add), reads=HK + ["lnB"], writes=HK)
            S.dve(lambda e, hp=hp: e.tensor_scalar(out=ah[:, :], in0=hp[:, :], scalar1=ALPHA, scalar2=None, op0=ALU.mult), reads=HK, writes=["ah"])
            S.dma("sp", lambda e, i=i, s=s: e.dma_start(out=h_scr[s, i * 128:(i + 1) * 128, :], in_=ah[:, :]), reads=["ah"], writes=["h_scr.%d" % i])

        def tail_H(i, half):
            b2 = i % 2
            hp = hpre[b2]
            for q4 in range(4):
                ec = half * 4 + q4
                S.pe(lambda e, q4=q4, ec=ec, hp=hp: e.transpose(bank(6)[:, q4 * 128:(q4 + 1) * 128], hp[:, ec * 128:(ec + 1) * 128], identF[:, :]), reads=["hpre%d.%d" % (b2, half), "identF"], writes=[PSK[6]])
            S.dve(lambda e, half=half, i=i: e.tensor_copy(hT[:, 4 * half:4 * half + 4, i * 128:(i + 1) * 128], bank(6)[:, :].rearrange("p (a b) -> p a b", a=4)), reads=[PSK[6]], writes=["hT.%d.%d" % (i, half)])
            S.dve(lambda e, half=half: e.tensor_copy(hTf[:, 4 * half:4 * half + 4, :], bank(6)[:, :].rearrange("p (a b) -> p a b", a=4)), reads=[PSK[6]], writes=["hTf.%d" % half])

        def tail_R(i, part):
            for ec in range(4 * part, 4 * part + 4):
                S.pe(lambda e, ec=ec: e.matmul(bank(6)[:, 0:20], lhsT=hTf[:, ec, :], rhs=rw[:, ec, :], start=(ec == 0), stop=(ec == 7)), reads=["hTf.%d" % (ec // 4)] + RWK, writes=[PSK[6]])
            if part == 1:
                S.dve(lambda e, i=i: e.tensor_tensor(out=logits[:, i, :], in0=bank(6)[:, 0:20], in1=rb_bc[:, :], op=ALU.add), reads=[PSK[6], "rb0", "rb1"], writes=["logits.%d" % i])

        TD = [int(v) for v in os.environ.get("KTAIL", "0,1,2,3,4,5,9,11,12,14").split(",")]
        TAIL = [(TD[0], tail_N), (TD[1], tail_T), (TD[2], lambda i: tail_Oq(i, 0)), (TD[3], lambda i: tail_Oq(i, 1)), (TD[4], lambda i: tail_Oq(i, 2)), (TD[5], lambda i: tail_Oq(i, 3)),
                (TD[6], tail_L), (TD[7], lambda i: tail_H(i, 0)), (TD[8], lambda i: tail_H(i, 1)), (TD[9], lambda i: (tail_R(i, 0), tail_R(i, 1)))]
        NSTEP = len(TAIL)
        done_steps = set()

        def emit_step(t, k):
            if t < 0 or (t, k) in done_steps:
                return
            for kk in range(k):
                emit_step(t, kk)
            emit_step(t - 1, k)
            if k == 1:
                emit_step(t - 1, 5)
            if k == 7:
                emit_step(t - 1, NSTEP - 1)
            if k == 0:
                for kk in range(NSTEP):
                    emit_step(t - 2, kk)
            done_steps.add((t, k))
            TAIL[k][1](t)

        pending = []
        def tick():
            keep = []
            for ent in pending:
                if ent[0] <= 0:
                    emit_step(ent[1], ent[2])
                else:
                    ent[0] -= 1
                    keep.append(ent)
            pending[:] = keep
        prev = None
        for gi, u in enumerate(units):
            emit_S(u, gi)
            emit_E(u, gi)
            if prev is not None:
                emit_V(prev[0], prev[1])
                if prev[0][0] != u[0]:
                    for k, (dly, fn) in enumerate(TAIL):
                        pending.append([dly, prev[0][0], k])
            tick()
            prev = (u, gi)
        if prev is not None:
            emit_V(prev[0], prev[1])
            for k, (dly, fn) in enumerate(TAIL):
                pending.append([dly, prev[0][0], k])
        while pending:
            tick()

        if dbg == "att" and s == 0:
            fence()
            RACC.reset()
            cvt = RACC.alloc([NT * 1024], BF16)
            d1 = dbg_out("dbg_hT", [128, 8 * SEQ]); d2 = dbg_out("dbg_logits", [128, NT * 20])
            cv2 = RACC.alloc([2 * SEQ], F32)
            for q in range(4):
                S.dve(lambda e, q=q: e.tensor_copy(cv2[:, :], hT[:, 2 * q:2 * q + 2, :].rearrange("p a b -> p (a b)")), reads=[], writes=["cv2"])
                outs.append(S.dma("sp", lambda e, q=q: e.dma_start(out=d1[:, 2 * q * SEQ:(2 * q + 2) * SEQ], in_=cv2[:, :]), reads=["cv2"]))
            outs.append(S.dma("sp", lambda e: e.dma_start(out=d2[:, :], in_=logits.rearrange("p t j -> p (t j)")), reads=["logits.%d" % i for i in range(int(os.environ.get("KATT_TILES", NT)))] if int(os.environ.get("KATT_STAGE", 9)) >= 7 else []))
            break


        fence()
        TT.reset(); RW.reset(); RACC.reset()
        S.dma("sp", lambda e: e.dma_start(out=lnG[:, :], in_=ln2_g[0:1, :].to_broadcast([128, 1024])), writes=["lnG"])
        S.dma("sp", lambda e: e.dma_start(out=lnB[:, :], in_=ln2_b[0:1, :].to_broadcast([128, 1024])), writes=["lnB"])
        LOGK = ["logits.%d" % i for i in range(NT)]
        lg = logits[:, :, 0:4]
        le4 = logits[:, :, 4:20].rearrange("p t (g j) -> p t g j", g=4)
        gmax = TT.alloc([NT], F32); goh = TT.alloc([NT, 4], F32); gex = TT.alloc([NT, 4], F32)
        gsum = TT.alloc([NT], F32); gval = TT.alloc([NT], F32)
        tmp16 = TT.alloc([NT, 4, 4], F32); esel = TT.alloc([NT, 4], F32)
        m1 = TT.alloc([NT], F32); oh1 = TT.alloc([NT, 4], F32); e2 = TT.alloc([NT, 4], F32)
        m2 = TT.alloc([NT], F32); oh2 = TT.alloc([NT, 4], F32); dd = TT.alloc([NT], F32)
        w1 = TT.alloc([NT], F32); w2 = TT.alloc([NT], F32); cw1 = TT.alloc([NT], F32); cw2 = TT.alloc([NT], F32)
        cj = TT.alloc([NT, 4], F32); cj2 = TT.alloc([NT, 4], F32)
        bc4 = lambda a: a.unsqueeze(2).to_broadcast([128, NT, 4])
        S.dve(lambda e: e.tensor_reduce(out=gmax[:, :], in_=lg, axis=AX.X, op=ALU.max), reads=LOGK, writes=["gmax"])
        S.dve(lambda e: e.tensor_tensor(out=goh[:, :, :], in0=lg, in1=bc4(gmax[:, :]), op=ALU.is_equal), reads=LOGK + ["gmax"], writes=["goh"])
        S.dve(lambda e: e.tensor_tensor(out=gex[:, :, :], in0=lg, in1=bc4(gmax[:, :]), op=ALU.subtract), reads=LOGK + ["gmax"], writes=["gex"])
        S.act(lambda e: e.activation(out=gex[:, :, :], in_=gex[:, :, :], func=AF.Exp), reads=["gex"], writes=["gex"])
        S.dve(lambda e: e.tensor_reduce(out=gsum[:, :], in_=gex[:, :, :], axis=AX.X, op=ALU.add), reads=["gex"], writes=["gsum"])
        S.dve(lambda e: e.reciprocal(gval[:, :], gsum[:, :]), reads=["gsum"], writes=["gval"])
        S.dve(lambda e: e.tensor_tensor(out=tmp16[:, :, :, :], in0=le4, in1=goh[:, :, :].unsqueeze(3).to_broadcast([128, NT, 4, 4]), op=ALU.mult), reads=LOGK + ["goh"], writes=["tmp16"])
        S.dve(lambda e: e.tensor_reduce(out=esel[:, :, :], in_=tmp16.rearrange("p t g j -> p t j g"), axis=AX.X, op=ALU.add), reads=["tmp16"], writes=["esel"])
        S.dve(lambda e: e.tensor_reduce(out=m1[:, :], in_=esel[:, :, :], axis=AX.X, op=ALU.max), reads=["esel"], writes=["m1"])
        S.dve(lambda e: e.tensor_tensor(out=oh1[:, :, :], in0=esel[:, :, :], in1=bc4(m1[:, :]), op=ALU.is_equal), reads=["esel", "m1"], writes=["oh1"])
        S.dve(lambda e: e.scalar_tensor_tensor(out=e2[:, :, :], in0=oh1[:, :, :], scalar=-1e30, in1=esel[:, :, :], op0=ALU.mult, op1=ALU.add), reads=["oh1", "esel"], writes=["e2"])
        S.dve(lambda e: e.tensor_reduce(out=m2[:, :], in_=e2[:, :, :], axis=AX.X, op=ALU.max), reads=["e2"], writes=["m2"])
        S.dve(lambda e: e.tensor_tensor(out=oh2[:, :, :], in0=e2[:, :, :], in1=bc4(m2[:, :]), op=ALU.is_equal), reads=["e2", "m2"], writes=["oh2"])
        S.dve(lambda e: e.tensor_tensor(out=dd[:, :], in0=m2[:, :], in1=m1[:, :], op=ALU.subtract), reads=["m1", "m2"], writes=["dd"])
        S.act(lambda e: e.activation(out=dd[:, :], in_=dd[:, :], func=AF.Exp), reads=["dd"], writes=["dd"])
        S.dve(lambda e: e.tensor_scalar(out=w1[:, :], in0=dd[:, :], scalar1=1.0, scalar2=None, op0=ALU.add), reads=["dd"], writes=["w1"])
        S.dve(lambda e: e.reciprocal(w1[:, :], w1[:, :]), reads=["w1"], writes=["w1"])
        S.dve(lambda e: e.tensor_tensor(out=w2[:, :], in0=dd[:, :], in1=w1[:, :], op=ALU.mult), reads=["dd", "w1"], writes=["w2"])
        S.dve(lambda e: e.tensor_tensor(out=cw1[:, :], in0=gval[:, :], in1=w1[:, :], op=ALU.mult), reads=["gval", "w1"], writes=["cw1"])
        S.dve(lambda e: e.tensor_tensor(out=cw2[:, :], in0=gval[:, :], in1=w2[:, :], op=ALU.mult), reads=["gval", "w2"], writes=["cw2"])
        S.dve(lambda e: e.tensor_tensor(out=cj[:, :, :], in0=oh1[:, :, :], in1=bc4(cw1[:, :]), op=ALU.mult), reads=["oh1", "cw1"], writes=["cj"])
        S.dve(lambda e: e.tensor_tensor(out=cj2[:, :, :], in0=oh2[:, :, :], in1=bc4(cw2[:, :]), op=ALU.mult), reads=["oh2", "cw2"], writes=["cj2"])
        S.dve(lambda e: e.tensor_tensor(out=cj[:, :, :], in0=cj[:, :, :], in1=cj2[:, :, :], op=ALU.add), reads=["cj", "cj2"], writes=["cj"])
        S.dve(lambda e: e.tensor_tensor(out=comb.rearrange("p t (g j) -> p t g j", g=4), in0=goh[:, :, :].unsqueeze(3).to_broadcast([128, NT, 4, 4]), in1=cj[:, :, :].unsqueeze(2).to_broadcast([128, NT, 4, 4]), op=ALU.mult),
              reads=["goh", "cj"], writes=["comb"])

        acc = RACC.alloc([NT, 1024], F32)
        sg = [TT.alloc([512], BF16) for _ in range(2)]
        actT = [TT.alloc([4, 512], BF16) for _ in range(2)]
        obuf = [TT.alloc([1024], F32) for _ in range(2)]
        bst2 = [TT.alloc([2, 6], F32) for _ in range(2)]
        mv2 = [TT.alloc([2], F32) for _ in range(2)]
        rs2 = [TT.alloc([1], F32) for _ in range(2)]
        for q in range(4):
            S.dma("sp", lambda e, q=q, s=s: e.dma_start(out=acc[:, 4 * q:4 * q + 4, :], in_=h_scr[s, q * 512:(q + 1) * 512, :].rearrange("(t p) d -> p t d", p=128)),
                  reads=["h_scr.%d" % t for t in range(4 * q, 4 * q + 4)], writes=["acc.%d" % t for t in range(4 * q, 4 * q + 4)])
        def ln2_stats(t):
            b2 = t % 2
            for c2 in range(2):
                S.dve(lambda e, c2=c2, t=t, b2=b2: e.bn_stats(bst2[b2][:, c2, :], acc[:, t, c2 * 512:(c2 + 1) * 512]), reads=["acc.%d" % t], writes=["bst2%d.%d" % (b2, c2)])
            S.dve(lambda e, b2=b2: e.bn_aggr(mv2[b2][:, :], bst2[b2][:, :, :]), reads=["bst2%d.0" % b2, "bst2%d.1" % b2], writes=["mv2%d" % b2])

        def ln2_apply(t):
            b2 = t % 2
            S.act(lambda e, b2=b2: e.activation(out=rs2[b2][:, :], in_=mv2[b2][:, 1:2], func=AF.Ln, bias=LN_EPS, scale=1.0), reads=["mv2%d" % b2], writes=["rs2%d" % b2])
            S.act(lambda e, b2=b2: e.activation(out=rs2[b2][:, :], in_=rs2[b2][:, :], func=AF.Exp, scale=-0.5), reads=["rs2%d" % b2], writes=["rs2%d" % b2])
            S.dve(lambda e, t=t, b2=b2: e.tensor_scalar(out=obuf[b2][:, :], in0=acc[:, t, :], scalar1=mv2[b2][:, 0:1], scalar2=rs2[b2][:, 0:1], op0=ALU.subtract, op1=ALU.mult),
                  reads=["acc.%d" % t, "mv2%d" % b2, "rs2%d" % b2], writes=["obuf%d" % b2])
            S.dve(lambda e, b2=b2: e.tensor_tensor(out=obuf[b2][:, :], in0=obuf[b2][:, :], in1=lnG[:, :], op=ALU.mult), reads=["obuf%d" % b2, "lnG"], writes=["obuf%d" % b2])
            S.dve(lambda e, b2=b2: e.tensor_tensor(out=obuf[b2][:, :], in0=obuf[b2][:, :], in1=lnB[:, :], op=ALU.add), reads=["obuf%d" % b2, "lnB"], writes=["obuf%d" % b2])
            outs.append(S.dma("sp", lambda e, t=t, b2=b2, s=s: e.dma_start(out=out[s, t * 128:(t + 1) * 128, :], in_=obuf[b2][:, :]), reads=["obuf%d" % b2], writes=["out.%d.%d" % (s, t)]))

        ln2_prev = []
        if NEXP > 1:
            load_expert(1)
        cg = 0; cd = 0
        for ex in range(NEXP):
            sl = ex % 2
            for tb in range(4):
                ab = (ex * 4 + tb) % 2
                hk = ["hT.%d.%d" % (i, hf) for i in range(4 * tb, 4 * tb + 4) for hf in range(2)]
                for fc in range(4):
                    pg = cg % 2; cg += 1
                    for k in range(8):
                        S.pe(lambda e, pg=pg, k=k, fc=fc, tb=tb, sl=sl: e.matmul(bank(pg)[:, :], lhsT=wgu[sl][:, k, fc * 128:(fc + 1) * 128], rhs=hT[:, k, tb * 512:(tb + 1) * 512], start=(k == 0), stop=(k == 7)),
                             reads=["wg.%d" % sl] + hk, writes=[PSK[pg]])
                    for k in range(8):
                        S.pe(lambda e, pg=pg, k=k, fc=fc, tb=tb, sl=sl: e.matmul(bank(2 + pg)[:, :], lhsT=wgu[sl][:, k, 512 + fc * 128:512 + (fc + 1) * 128], rhs=hT[:, k, tb * 512:(tb + 1) * 512], start=(k == 0), stop=(k == 7)),
                             reads=["wu.%d" % sl] + hk, writes=[PSK[2 + pg]])
                    S.act(lambda e, pg=pg: e.activation(out=sg[pg][:, :], in_=bank(pg)[:, :], func=AF.Silu), reads=[PSK[pg]], writes=["sg%d" % pg])
                    S.dve(lambda e, pg=pg, ab=ab, fc=fc: e.tensor_tensor(out=actT[ab][:, fc, :], in0=bank(2 + pg)[:, :], in1=sg[pg][:, :], op=ALU.mult),
                          reads=[PSK[2 + pg], "sg%d" % pg], writes=["actT%d.%d" % (ab, fc)])
                for tt in range(4):
                    t = tb * 4 + tt
                    pdi = 2 + (cd % 2); cd += 1
                    for half in range(2):
                        for fc in range(4):
                            S.pe(lambda e, pdi=pdi, half=half, fc=fc, ab=ab, tt=tt, sl=sl: e.matmul(pd[pdi][:, half * 512:(half + 1) * 512], lhsT=actT[ab][:, fc, tt * 128:(tt + 1) * 128], rhs=wdn[sl][:, fc, half * 512:(half + 1) * 512], start=(fc == 0), stop=(fc == 3)),
                                 reads=["actT%d.%d" % (ab, fc), "wd.%d" % sl], writes=[PSK[2 * pdi + half]])
                    S.dve(lambda e, pdi=pdi, t=t, ex=ex: e.scalar_tensor_tensor(out=acc[:, t, :], in0=pd[pdi][:, :], scalar=comb[:, t, ex:ex + 1], in1=acc[:, t, :], op0=ALU.mult, op1=ALU.add),
                          reads=[PSK[2 * pdi], PSK[2 * pdi + 1], "comb", "acc.%d" % t], writes=["acc.%d" % t])
                    if ex == NEXP - 1:
                        ln2_stats(t)
                        if ln2_prev:
                            ln2_apply(ln2_prev.pop())
                        ln2_prev.append(t)
            if ex + 2 < NEXP:
                load_expert(ex + 2)
        while ln2_prev:
            ln2_apply(ln2_prev.pop())
        if s + 1 < NSEQ:
            fence()
        if dbg == "one":
            break

    with nc.allow_non_contiguous_dma(reason="tiny constant loads"):
        st = S.emit(outs)
    return nc, st, dbg_t


_CACHE = {}


def _get_program():
    if "p" not in _CACHE:
        _CACHE["p"] = build_program(dbg=os.environ.get("KDBG", ""))
    return _CACHE["p"]


def kernel(**inputs):
    nc, st, dbg_t = _get_program()
    f = lambda a: np.ascontiguousarray(np.asarray(a, dtype=np.float32))
    x = f(inputs["x"])
    shared = {
        "w_in": f(inputs["w_in"])[0], "b_in": f(inputs["b_in"]).reshape(1, DIN),
        "conv_w": f(inputs["conv_w"])[0], "conv_b": f(inputs["conv_b"]).reshape(1, 1024),
        "a_log": f(inputs["a_log"]).reshape(1, 8), "d_skip": f(inputs["d_skip"]).reshape(1, 8),
        "ssd_norm_g": f(inputs["ssd_norm_g"]).reshape(1, 512), "w_out": f(inputs["w_out"])[0],
        "ln1_g": f(inputs["ln1_g"]).reshape(1, DM), "ln1_b": f(inputs["ln1_b"]).reshape(1, DM),
        "router_group_w": f(inputs["router_group_w"])[0], "router_group_b": f(inputs["router_group_b"]).reshape(1, 4),
        "router_expert_w": f(inputs["router_expert_w"])[0], "router_expert_b": f(inputs["router_expert_b"]).reshape(1, 16),
        "w_gate": f(inputs["w_gate"])[0], "w_up": f(inputs["w_up"])[0], "w_down": f(inputs["w_down"])[0],
        "ln2_g": f(inputs["ln2_g"]).reshape(1, DM), "ln2_b": f(inputs["ln2_b"]).reshape(1, DM),
    }
    ncores = int(os.environ.get("KCORES", NCORES))
    in_maps = []
    for c in range(ncores):
        m = dict(shared)
        m["x"] = np.ascontiguousarray(x[c * NSEQ:(c + 1) * NSEQ])
        in_maps.append(m)
    res = run_bass_kernel_spmd(nc, in_maps, core_ids=list(range(ncores)))
    if os.environ.get("KDBG", ""):
        _CACHE["dbg"] = res.results
    outp = np.concatenate([r["out"] for r in res.results], axis=0)
    return outp.astype(np.float32)
```

```python
import os
import numpy as np
import concourse.bass as bass
import concourse.mybir as mybir
from concourse.bass_utils import run_bass_kernel_spmd

F32 = mybir.dt.float32
BF16 = mybir.dt.bfloat16
U8 = mybir.dt.uint8
AF = mybir.ActivationFunctionType
ALU = mybir.AluOpType
AX = mybir.AxisListType

NCORES = 8
NSEQ = 2
SEQ = 2048
NT = 16
DM = 1024
DIN = 3088
ALPHA = float(2.0 ** 0.25)
LN_EPS = 1e-5
RMS_EPS = 1e-5
ATT_SCALE = 0.125
NEG = -30000.0


class Op:
    __slots__ = ("eng", "fn", "reads", "writes", "deps", "signal", "sigval", "dma", "gi", "nofence", "fold")


class Sched:
    COMPUTE = ("pe", "act", "dve", "pool")

    def __init__(self, nc, n_dma_sems=40):
        self.nc = nc
        self.h = {"pe": nc.tensor, "act": nc.scalar, "dve": nc.vector, "pool": nc.gpsimd, "sp": nc.sync}
        self.ops = []
        self.last_w = {}
        self.readers = {}
        self.n_dma_sems = n_dma_sems
        self.live_dma = []
        self.nfence = 0
        self.fold_now = os.environ.get("KFOLD", "all") == "all"

    def add(self, eng, fn, reads=(), writes=(), dma=False, nofence=False):
        o = Op()
        o.eng = eng; o.fn = fn; o.reads = tuple(reads); o.writes = tuple(writes)
        o.deps = []; o.signal = False; o.sigval = None; o.dma = dma; o.gi = len(self.ops); o.nofence = nofence
        o.fold = self.fold_now
        for r in o.reads:
            p = self.last_w.get(r)
            if p is not None:
                self._dep(o, p, True)
            if r.startswith("ps"):
                rd = self.readers.get(r)
                if rd:
                    for q in rd.values():
                        if q.eng != eng:
                            self._dep(o, q, True)
        for w in o.writes:
            p = self.last_w.get(w)
            if p is not None:
                self._dep(o, p, False)
            rd = self.readers.get(w)
            if rd:
                for q in rd.values():
                    self._dep(o, q, False)
        for r in o.reads:
            d = self.readers.setdefault(r, {})
            d[("dma", o.gi) if dma else eng] = o
        for w in o.writes:
            self.last_w[w] = o
            self.readers[w] = {}
        self.ops.append(o)
        if dma and not nofence:
            self.live_dma.append(o)
        return o

    def _dep(self, o, p, raw):
        if p is o:
            return
        if (not p.dma) and (not o.dma) and p.eng == o.eng:
            if o.eng == "pe":
                return
        o.deps.append(p)
        p.signal = True

    def pe(self, fn, reads=(), writes=()): return self.add("pe", fn, reads, writes)
    def act(self, fn, reads=(), writes=()): return self.add("act", fn, reads, writes)
    def dve(self, fn, reads=(), writes=()): return self.add("dve", fn, reads, writes)
    def pool(self, fn, reads=(), writes=()): return self.add("pool", fn, reads, writes)
    def dma(self, q, fn, reads=(), writes=(), nofence=False):
        return self.add(q, fn, reads, writes, dma=True, nofence=nofence)

    def fence(self, scratch):
        n = self.nfence; self.nfence += 1
        a_keys = []
        col = {"pe": None, "act": 0, "dve": 1, "pool": 2}
        for e in ("act", "dve", "pool"):
            k = "fenceA.%d.%s" % (n, e)
            c = col[e]
            if e == "act":
                self.add(e, (lambda eh, c=c: eh.activation(out=scratch[:, c:c + 1], in_=scratch[:, 8:9], func=AF.Copy)), reads=(), writes=(k,))
            else:
                self.add(e, (lambda eh, c=c: eh.memset(scratch[:, c:c + 1], 0.0)), reads=(), writes=(k,))
            a_keys.append(k)
        k = "fenceA.%d.pe" % n
        self.add("pe", (lambda eh: eh.matmul(self.fence_ps[0:1, 0:1], lhsT=self.fence_w[0:1, 0:1], rhs=self.fence_w[0:1, 0:1], start=True, stop=True)),
                 reads=(), writes=(k, "ps7"))
        a_keys.append(k)
        dmas = self.live_dma
        self.live_dma = []
        for e in ("act", "dve", "pool", "pe", "sp"):
            kb = "fenceB.%d.%s" % (n, e)
            if e == "act":
                o = self.add(e, (lambda eh: eh.activation(out=scratch[:, 3:4], in_=scratch[:, 8:9], func=AF.Copy)), reads=a_keys, writes=(kb,))
            elif e == "pe":
                o = self.add(e, (lambda eh: eh.matmul(self.fence_ps[0:1, 1:2], lhsT=self.fence_w[0:1, 0:1], rhs=self.fence_w[0:1, 0:1], start=True, stop=True)),
                             reads=a_keys, writes=(kb, "ps7"))
            elif e == "sp":
                o = self.add(e, (lambda eh: eh.nop()), reads=a_keys, writes=(kb,))
            else:
                c = 4 if e == "dve" else 5
                o = self.add(e, (lambda eh, c=c: eh.memset(scratch[:, c:c + 1], 0.0)), reads=a_keys, writes=(kb,))
            for d in dmas:
                o.deps.append(d)

    def emit(self, final_wait_ops=()):
        nc = self.nc
        esem = {e: nc.alloc_semaphore("s_" + e) for e in self.COMPUTE}
        dsems = [nc.alloc_semaphore("s_dma%d" % i) for i in range(self.n_dma_sems)]
        dtotal = [0] * self.n_dma_sems
        dlast = [None] * self.n_dma_sems
        ecount = {e: 0 for e in self.COMPUTE}
        nd = 0
        nq = {"sp": 0, "pool": 0}
        half = self.n_dma_sems // 2
        for o in self.ops:
            if o.dma:
                qi = nq[o.eng]; nq[o.eng] += 1; nd += 1
                i = (qi % half) + (0 if o.eng == "sp" else half)
                prev = dlast[i]
                if prev is not None:
                    o.deps.append(prev)
                dtotal[i] += 16
                o.sigval = (dsems[i], dtotal[i], 1000 + i)
                dlast[i] = o
            elif o.signal:
                ecount[o.eng] += 1
                o.sigval = (esem[o.eng], ecount[o.eng], o.eng)
        known = {e: {} for e in self.h}
        nwaits = 0
        for o in self.ops:
            eh = self.h[o.eng]
            kn = known[o.eng]
            need = {}
            for p in o.deps:
                s, v, key = p.sigval
                if kn.get(key, 0) >= v:
                    continue
                if key not in need or need[key][1] < v:
                    need[key] = (s, v)
            items = list(need.items())
            fold = None
            if items and o.eng == "pe" and o.fold:
                fold = items.pop()
            for key, (s, v) in items:
                eh.wait_ge(s, v)
                kn[key] = v
                nwaits += 1
            ins = o.fn(eh)
            if fold is not None:
                key, (s, v) = fold
                ins._wait_ge(s, v)
                kn[key] = v
            if o.dma:
                ins.then_inc(o.sigval[0], 16)
            elif o.signal:
                ins.then_inc(o.sigval[0], 1)
        eh = self.h["sp"]
        for o in final_wait_ops:
            s, v, key = o.sigval
            eh.wait_ge(s, v)
        self.stats = dict(n_ops=len(self.ops), n_waits=nwaits, counts=dict(ecount), n_dma=nd)
        return self.stats


class Arena:
    def __init__(self, nc, name, nbytes):
        self.t = nc.alloc_sbuf_tensor(name, [128, nbytes], U8)
        self.n = nbytes
        self.off = 0

    def reset(self, off=0):
        self.off = off

    def alloc(self, shape, dtype, parts=128):
        esz = 2 if dtype == BF16 else 4
        n = esz
        for s in shape:
            n *= s
        off = (self.off + 31) // 32 * 32
        assert off + n <= self.n, (off, n, self.n)
        self.off = off + n
        flat = self.t[0:parts, off:off + n].bitcast(dtype)
        if len(shape) == 1:
            return flat
        names = " ".join("a%d" % i for i in range(len(shape)))
        kw = {"a%d" % i: shape[i] for i in range(1, len(shape))}
        return flat.rearrange("p (%s) -> p %s" % (names, names), **kw)


def build_program(dbg=False):
    nc = bass.Bass("TRN2", target_bir_lowering=False)
    S = Sched(nc)
    D = {}

    def din(name, shape):
        D[name] = nc.dram_tensor(name, list(shape), F32, kind="ExternalInput").ap()
        return D[name]

    x = din("x", [NSEQ, SEQ, DM])
    w_in = din("w_in", [DM, DIN])
    b_in = din("b_in", [1, DIN])
    conv_w = din("conv_w", [4, 1024])
    conv_b = din("conv_b", [1, 1024])
    a_log = din("a_log", [1, 8])
    d_skip = din("d_skip", [1, 8])
    ssd_g = din("ssd_norm_g", [1, 512])
    w_out = din("w_out", [DM, DM])
    ln1_g = din("ln1_g", [1, DM]); ln1_b = din("ln1_b", [1, DM])
    rg_w = din("router_group_w", [DM, 4]); rg_b = din("router_group_b", [1, 4])
    re_w = din("router_expert_w", [4, DM, 4]); re_b = din("router_expert_b", [1, 16])
    w_gate = din("w_gate", [16, DM, 512]); w_up = din("w_up", [16, DM, 512]); w_down = din("w_down", [16, 512, DM])
    ln2_g = din("ln2_g", [1, DM]); ln2_b = din("ln2_b", [1, DM])
    out = nc.dram_tensor("out", [NSEQ, SEQ, DM], F32, kind="ExternalOutput").ap()
    h_scr = nc.dram_tensor("h_scr", [NSEQ, SEQ, DM], F32).ap()
    dbg_t = {}

    def dbg_out(name, shape):
        dbg_t[name] = nc.dram_tensor(name, list(shape), F32, kind="ExternalOutput").ap()
        return dbg_t[name]

    CONST = Arena(nc, "CONST", 22 * 1024)
    RW = Arena(nc, "RW", 49408)
    RX = Arena(nc, "RX", 32768)
    RACC = Arena(nc, "RACC", 65536)
    MSSD = Arena(nc, "MSSD", 16384)
    TT = Arena(nc, "TT", nc.sbuf_bytes_remaining - 256)

    identB = CONST.alloc([128], BF16); identF = CONST.alloc([128], F32)
    Uf = CONST.alloc([128], F32); triU = CONST.alloc([128], BF16); maskneg = CONST.alloc([128], BF16)
    onesF = CONST.alloc([128], F32)
    ones_row = CONST.alloc([128], BF16, parts=1)
    bz_row = CONST.alloc([512], BF16, parts=1); bv_row = CONST.alloc([512], BF16, parts=1)
    bdtf = CONST.alloc([16], F32)
    bxbc = CONST.alloc([8], F32); bq = CONST.alloc([4], F32); bk = CONST.alloc([4], F32)
    convw = CONST.alloc([8, 4], F32); convb = CONST.alloc([8], F32)
    a_bc = CONST.alloc([8], F32); dskip_bc = CONST.alloc([8], F32)
    gssd_bc = CONST.alloc([512], F32)
    lnG = CONST.alloc([1024], F32); lnB = CONST.alloc([1024], F32)
    rw = CONST.alloc([8, 20], F32); rb_bc = CONST.alloc([20], F32)
    logits = CONST.alloc([NT, 20], F32); comb = CONST.alloc([NT, 16], F32)
    fsc = CONST.alloc([16], F32)
    S.fence_w = CONST.alloc([8], BF16)
    pd = [nc.alloc_psum_tensor("pd%d" % i, [128, 1024], F32) for i in range(4)]
    def bank(i):
        return pd[i // 2][:, (i % 2) * 512:(i % 2) * 512 + 512]
    def bankb(i):
        return bank(i).bitcast(BF16)
    PSK = ["ps%d" % i for i in range(8)]
    S.fence_ps = nc.alloc_sbuf_tensor("fence_dummy", [1, 8], F32)
    S.fence_ps = bank(7)[:, 504:512]

    def fence():
        S.fence(fsc)

    S.pool(lambda e: e.memset(fsc[:, :], 0.0), writes=["fsc"])
    S.pool(lambda e: e.memset(S.fence_w[:, :], 0.0), writes=["fence_w"])
    S.pool(lambda e: e.memset(identB[:, :], 1.0), writes=["identB"])
    S.pool(lambda e: e.affine_select(out=identB[:, :], in_=identB[:, :], pattern=[[-1, 128]], compare_op=ALU.is_equal, fill=0.0, base=0, channel_multiplier=1), reads=["identB"], writes=["identB"])
    S.pool(lambda e: e.memset(identF[:, :], 1.0), writes=["identF"])
    S.pool(lambda e: e.affine_select(out=identF[:, :], in_=identF[:, :], pattern=[[-1, 128]], compare_op=ALU.is_equal, fill=0.0, base=0, channel_multiplier=1), reads=["identF"], writes=["identF"])
    S.pool(lambda e: e.memset(Uf[:, :], 1.0), writes=["Uf"])
    S.pool(lambda e: e.affine_select(out=Uf[:, :], in_=Uf[:, :], pattern=[[1, 128]], compare_op=ALU.is_ge, fill=0.0, base=0, channel_multiplier=-1), reads=["Uf"], writes=["Uf"])
    S.pool(lambda e: e.memset(triU[:, :], 1.0), writes=["triU"])
    S.pool(lambda e: e.affine_select(out=triU[:, :], in_=triU[:, :], pattern=[[1, 128]], compare_op=ALU.is_ge, fill=0.0, base=0, channel_multiplier=-1), reads=["triU"], writes=["triU"])
    S.pool(lambda e: e.memset(maskneg[:, :], NEG), writes=["maskneg"])
    S.pool(lambda e: e.affine_select(out=maskneg[:, :], in_=maskneg[:, :], pattern=[[-1, 128]], compare_op=ALU.is_gt, fill=0.0, base=0, channel_multiplier=1), reads=["maskneg"], writes=["maskneg"])
    S.pool(lambda e: e.memset(onesF[:, :], 1.0), writes=["onesF"])
    S.pool(lambda e: e.memset(ones_row[:, :], 1.0), writes=["ones_row"])
    S.dma("pool", lambda e: e.dma_start(out=bz_row[:, :], in_=b_in[0:1, 0:512]), writes=["bz_row"])
    S.dma("pool", lambda e: e.dma_start(out=bv_row[:, :], in_=b_in[0:1, 2568:3080]), writes=["bv_row"])
    S.dma("sp", lambda e: e.dma_start(out=bdtf[:, 0:8], in_=b_in[0:1, 1536:1544].to_broadcast([128, 8])), writes=["bdtf0"])
    S.dma("sp", lambda e: e.dma_start(out=bdtf[:, 8:16], in_=b_in[0:1, 3080:3088].to_broadcast([128, 8])), writes=["bdtf1"])
    S.dma("sp", lambda e: e.dma_start(out=bxbc[:, :], in_=b_in[0, 512:1536].rearrange("(c p) -> p c", p=128)), writes=["bxbc"])
    S.dma("sp", lambda e: e.dma_start(out=bq[:, :], in_=b_in[0, 1544:2056].rearrange("(c p) -> p c", p=128)), writes=["bq"])
    S.dma("sp", lambda e: e.dma_start(out=bk[:, :], in_=b_in[0, 2056:2568].rearrange("(c p) -> p c", p=128)), writes=["bk"])
    for k in range(4):
        S.dma("sp", lambda e, k=k: e.dma_start(out=convw[:, :, k], in_=conv_w[k, :].rearrange("(c p) -> p c", p=128)), writes=["convw%d" % k])
    CONVW = ["convw%d" % k for k in range(4)]
    S.dma("sp", lambda e: e.dma_start(out=convb[:, :], in_=conv_b[0, :].rearrange("(c p) -> p c", p=128)), writes=["convb"])
    S.dma("sp", lambda e: e.dma_start(out=a_bc[:, :], in_=a_log[0:1, :].to_broadcast([128, 8])), writes=["a_bc"])
    S.act(lambda e: e.activation(out=a_bc[:, :], in_=a_bc[:, :], func=AF.Exp), reads=["a_bc"], writes=["a_bc"])
    S.dve(lambda e: e.tensor_scalar(out=a_bc[:, :], in0=a_bc[:, :], scalar1=-1.0, scalar2=None, op0=ALU.mult), reads=["a_bc"], writes=["a_bc"])
    S.dma("sp", lambda e: e.dma_start(out=dskip_bc[:, :], in_=d_skip[0:1, :].to_broadcast([128, 8])), writes=["dskip_bc"])
    S.dma("sp", lambda e: e.dma_start(out=gssd_bc[:, :], in_=ssd_g[0:1, :].to_broadcast([128, 512])), writes=["gssd_bc"])
    S.dma("sp", lambda e: e.dma_start(out=rw[:, :, 0:4], in_=rg_w.rearrange("(k p) j -> p k j", p=128)), writes=["rw0"])
    for g in range(4):
        S.dma("sp", lambda e, g=g: e.dma_start(out=rw[:, :, 4 + 4 * g:8 + 4 * g], in_=re_w[g].rearrange("(k p) j -> p k j", p=128)), writes=["rw%d" % (g + 1)])
    RWK = ["rw%d" % i for i in range(5)]
    S.dma("sp", lambda e: e.dma_start(out=rb_bc[:, 0:4], in_=rg_b[0:1, :].to_broadcast([128, 4])), writes=["rb0"])
    S.dma("sp", lambda e: e.dma_start(out=rb_bc[:, 4:20], in_=re_b[0:1, :].to_broadcast([128, 16])), writes=["rb1"])

    outs = []

    for s in range(NSEQ):
        RW.reset(); RX.reset(); RACC.reset(); MSSD.reset(); TT.reset()
        wgu = [None, None]; wdn = [None, None]
        wgu[0] = RW.alloc([8, 1024], BF16); wdn[0] = RW.alloc([4, 1024], BF16)
        wgu[1] = RW.alloc([8, 1024], BF16); wdn[1] = RW.alloc([4, 1024], BF16)
        RW.reset()
        wA = RW.alloc([8, 1544], BF16)
        xb = [RW.alloc([4, 1024], BF16) for _ in range(2)]
        xT = RX.alloc([8, SEQ], BF16)
        sz = RACC.alloc([NT, 512], BF16)
        xsB = RACC.alloc([NT, 768], BF16)
        BT = RACC.alloc([2, SEQ], BF16)
        CT = RACC.alloc([2, SEQ], BF16)
        xsT = MSSD.alloc([4, SEQ], BF16)
        dt_t = TT.alloc([NT, 8], F32)
        tt_mark = TT.off
        pre = [TT.alloc([SEQ + 3], BF16) for _ in range(2)]
        diagw = TT.alloc([8, 4, 128], BF16)
        dt_raw = TT.alloc([NT, 8], F32)

        w_in_v = w_in.rearrange("(k p) c -> p k c", p=128)
        S.dma("pool", lambda e, s=s: e.dma_start(out=xb[0][:, :, :], in_=x[s, 0:512, :].rearrange("(t p) d -> p t d", p=128)), writes=["xb0"])
        S.dma("pool", lambda e: e.dma_start(out=wA[:, :, 0:512], in_=w_in_v[:, :, 0:512]), writes=["wA.z"])
        S.dma("pool", lambda e: e.dma_start(out=wA[:, :, 1536:1544], in_=w_in_v[:, :, 1536:1544]), writes=["wA.dt"])
        S.dma("pool", lambda e, s=s: e.dma_start(out=xb[1][:, :, :], in_=x[s, 512:1024, :].rearrange("(t p) d -> p t d", p=128)), writes=["xb1"])
        for half in range(2):
            S.dma("pool", lambda e, half=half: e.dma_start(out=wA[:, 4 * half:4 * half + 4, 512:1536], in_=w_in_v[:, 4 * half:4 * half + 4, 512:1536]), writes=["wA.x%d" % half])
        for b in range(2):
            S.pool(lambda e, b=b: e.memset(pre[b][:, 0:3], 0.0), writes=["prepad%d" % b])
        ev = 0
        for blk in range(4):
            if blk >= 2:
                S.dma("pool", lambda e, blk=blk, s=s: e.dma_start(out=xb[blk % 2][:, :, :], in_=x[s, blk * 512:(blk + 1) * 512, :].rearrange("(t p) d -> p t d", p=128)),
                      writes=["xb%d" % (blk % 2)])
            for k in range(8):
                pb = (blk * 8 + k) % 2
                for t in range(4):
                    S.pe(lambda e, pb=pb, t=t, k=k, blk=blk: e.transpose(bankb(pb)[:, t * 128:(t + 1) * 128], xb[blk % 2][:, t, k * 128:(k + 1) * 128], identB[:, :]),
                         reads=["xb%d" % (blk % 2), "identB"], writes=[PSK[pb]])
                if ev % 2 == 0:
                    S.act(lambda e, pb=pb, k=k, blk=blk: e.copy(xT[:, k, blk * 512:(blk + 1) * 512], bankb(pb)[:, 0:512]), reads=[PSK[pb]], writes=["xT.%d.%d" % (k, blk)])
                else:
                    S.dve(lambda e, pb=pb, k=k, blk=blk: e.tensor_copy(xT[:, k, blk * 512:(blk + 1) * 512], bankb(pb)[:, 0:512]), reads=[PSK[pb]], writes=["xT.%d.%d" % (k, blk)])
                ev += 1
            for tt in range(4):
                t = blk * 4 + tt
                pz = 2 + (t % 2)
                xk = ["xT.%d.%d" % (k, blk) for k in range(8)]
                for k in range(8):
                    S.pe(lambda e, pz=pz, t=t, k=k: e.matmul(bank(pz)[:, :], lhsT=xT[:, k, t * 128:(t + 1) * 128], rhs=wA[:, k, 0:512], start=(k == 0), stop=False),
                         reads=[xk[k], "wA.z"], writes=[PSK[pz]])
                S.pe(lambda e, pz=pz: e.matmul(bank(pz)[:, :], lhsT=ones_row[0:1, :], rhs=bz_row[0:1, :], start=False, stop=True),
                     reads=["ones_row", "bz_row"], writes=[PSK[pz]])
                S.act(lambda e, pz=pz, t=t: e.activation(out=sz[:, t, :], in_=bank(pz)[:, :], func=AF.Silu), reads=[PSK[pz]], writes=["sz.%d" % t])
                for k in range(8):
                    S.pe(lambda e, t=t, k=k: e.matmul(bank(4)[:, t * 8:(t + 1) * 8], lhsT=xT[:, k, t * 128:(t + 1) * 128], rhs=wA[:, k, 1536:1544], start=(k == 0), stop=(k == 7)),
                         reads=[xk[k], "wA.dt"], writes=[PSK[4]])
        S.dve(lambda e: e.tensor_tensor(out=dt_raw[:, :, :], in0=bank(4)[:, 0:128].rearrange("p (t h) -> p t h", h=8), in1=bdtf[:, 0:8].unsqueeze(1).to_broadcast([128, NT, 8]), op=ALU.add),
              reads=[PSK[4], "bdtf0"], writes=["dt_raw"])
        for c in range(8):
            for k in range(4):
                S.dve(lambda e, c=c, k=k: e.tensor_scalar(out=diagw[:, c, k, :], in0=identB[:, :], scalar1=convw[:, c, k:k + 1], scalar2=None, op0=ALU.mult),
                      reads=["identB"] + CONVW, writes=["diagw.%d" % c])

        def a1_ip(c):
            pb_ = c % 2
            for blk in range(4):
                pc = 5 + (c * 4 + blk) % 2
                for k in range(8):
                    S.pe(lambda e, pc=pc, c=c, k=k, blk=blk: e.matmul(bank(pc)[:, :], lhsT=wA[:, k, 512 + c * 128:512 + (c + 1) * 128], rhs=xT[:, k, blk * 512:(blk + 1) * 512], start=(k == 0), stop=(k == 7)),
                         reads=["xT.%d.%d" % (k, blk), "wA.x%d" % (k // 4)], writes=[PSK[pc]])
                S.act(lambda e, pc=pc, c=c, blk=blk, pb_=pb_: e.activation(out=pre[pb_][:, 3 + blk * 512:3 + (blk + 1) * 512], in_=bank(pc)[:, :], func=AF.Identity, bias=bxbc[:, c:c + 1], scale=1.0),
                      reads=[PSK[pc], "bxbc"], writes=["pre%d.%d" % (pb_, blk)])

        def a1_conv(c):
            pb_ = c % 2
            if c < 4:
                dstt = xsT[:, c, :]; dk = "xsT.%d" % c
            elif c < 6:
                dstt = BT[:, c - 4, :]; dk = "BT.%d" % (c - 4)
            else:
                dstt = CT[:, c - 6, :]; dk = "CT.%d" % (c - 6)
            for blk in range(4):
                pc = 2 + (c * 4 + blk) % 2
                rk = ["pre%d.%d" % (pb_, blk), "diagw.%d" % c] + (["pre%d.%d" % (pb_, blk - 1)] if blk > 0 else ["prepad%d" % pb_])
                for k in range(4):
                    S.pe(lambda e, pc=pc, c=c, k=k, blk=blk, pb_=pb_: e.matmul(bank(pc)[:, :], lhsT=diagw[:, c, k, :], rhs=pre[pb_][:, blk * 512 + k:blk * 512 + k + 512], start=(k == 0), stop=(k == 3)),
                         reads=rk, writes=[PSK[pc]])
                S.act(lambda e, pc=pc, dstt=dstt, c=c, blk=blk: e.activation(out=dstt[:, blk * 512:(blk + 1) * 512], in_=bank(pc)[:, :], func=AF.Silu, bias=convb[:, c:c + 1], scale=1.0),
                      reads=[PSK[pc], "convb"], writes=[dk])

        a1_ip(0)
        for c in range(8):
            if c + 1 < 8:
                a1_ip(c + 1)
            a1_conv(c)
        for t in range(NT):
            pb = t % 2
            for c in range(6):
                src = xsT[:, c, t * 128:(t + 1) * 128] if c < 4 else BT[:, c - 4, t * 128:(t + 1) * 128]
                sk = "xsT.%d" % c if c < 4 else "BT.%d" % (c - 4)
                S.pe(lambda e, pb=pb, c=c, src=src: e.transpose(bankb(pb)[:, c * 128:(c + 1) * 128], src, identB[:, :]), reads=[sk, "identB"], writes=[PSK[pb]])
            if t % 2 == 0:
                S.dve(lambda e, pb=pb, t=t: e.tensor_copy(xsB[:, t, :], bankb(pb)[:, 0:768]), reads=[PSK[pb]], writes=["xsB.%d" % t])
            else:
                S.act(lambda e, pb=pb, t=t: e.copy(xsB[:, t, :], bankb(pb)[:, 0:768]), reads=[PSK[pb]], writes=["xsB.%d" % t])
        S.act(lambda e: e.activation(out=dt_t[:, :, :], in_=dt_raw[:, :, :], func=AF.Exp), reads=["dt_raw"], writes=["dt_t"])
        S.act(lambda e: e.activation(out=dt_t[:, :, :], in_=dt_t[:, :, :], func=AF.Ln, bias=1.0, scale=1.0), reads=["dt_t"], writes=["dt_t"])

        fence()
        MSSD.reset(); TT.reset(tt_mark)
        RW.reset()
        wB = RW.alloc([8, 1544], BF16)
        wo = RW.alloc([8, 1024], BF16)
        for half in range(2):
            S.dma("pool", lambda e, half=half: e.dma_start(out=wB[:, 4 * half:4 * half + 4, :], in_=w_in.rearrange("(k p) c -> p k c", p=128)[:, 4 * half:4 * half + 4, 1544:3088]),
                  writes=["wB"], nofence=True)
        for half in range(2):
            S.dma("pool", lambda e, half=half: e.dma_start(out=wo[:, 4 * half:4 * half + 4, :], in_=w_out.rearrange("(k p) c -> p k c", p=128)[:, 4 * half:4 * half + 4, :]),
                  writes=["wo"], nofence=True)
        m_ssd = MSSD.alloc([NT, 512], BF16)
        da = TT.alloc([NT, 8], F32); acum = TT.alloc([NT, 8], F32); nacum = TT.alloc([NT, 8], F32)
        alast = TT.alloc([NT, 8], F32); dte = TT.alloc([NT, 8], F32); ea = TT.alloc([NT, 8], F32)
        cdec = TT.alloc([NT, 8], F32); dtdte = TT.alloc([NT, 8], F32)
        stT = TT.alloc([8, 64], F32); stTb = TT.alloc([8, 64], BF16)
        LT = [TT.alloc([128], BF16) for _ in range(4)]
        MT = [TT.alloc([128], BF16) for _ in range(4)]
        xdt = [TT.alloc([8, 64], BF16) for _ in range(2)]
        xdtd = [TT.alloc([8, 64], BF16) for _ in range(2)]
        t1 = [TT.alloc([8, 64], F32)] * 2
        t2 = [TT.alloc([8, 64], F32) for _ in range(2)]
        yg = [TT.alloc([512], F32) for _ in range(2)]
        junk = TT.alloc([256], F32)
        ss = [TT.alloc([2], F32) for _ in range(2)]
        rstd = [TT.alloc([2], F32) for _ in range(2)]

        S.dve(lambda e: e.tensor_tensor(out=da[:, :, :], in0=dt_t[:, :, :], in1=a_bc[:, :].unsqueeze(1).to_broadcast([128, NT, 8]), op=ALU.mult), reads=["dt_t", "a_bc"], writes=["da"])
        daf = da.rearrange("p t h -> p (t h)")
        S.pe(lambda e: e.matmul(bank(5)[:, 0:128], lhsT=Uf[:, :], rhs=daf, start=True, stop=True), reads=["Uf", "da"], writes=[PSK[5]])
        S.pe(lambda e: e.matmul(bank(6)[:, 0:128], lhsT=onesF[:, :], rhs=daf, start=True, stop=True), reads=["onesF", "da"], writes=[PSK[6]])
        S.dve(lambda e: e.tensor_copy(acum.rearrange("p t h -> p (t h)"), bank(5)[:, 0:128]), reads=[PSK[5]], writes=["acum"])
        S.dve(lambda e: e.tensor_scalar(out=nacum.rearrange("p t h -> p (t h)"), in0=bank(5)[:, 0:128], scalar1=-1.0, scalar2=None, op0=ALU.mult), reads=[PSK[5]], writes=["nacum"])
        S.dve(lambda e: e.tensor_copy(alast.rearrange("p t h -> p (t h)"), bank(6)[:, 0:128]), reads=[PSK[6]], writes=["alast"])
        S.dve(lambda e: e.tensor_tensor(out=dte[:, :, :], in0=alast[:, :, :], in1=acum[:, :, :], op=ALU.subtract), reads=["alast", "acum"], writes=["dte"])
        S.act(lambda e: e.activation(out=dte[:, :, :], in_=dte[:, :, :], func=AF.Exp), reads=["dte"], writes=["dte"])
        S.act(lambda e: e.activation(out=ea[:, :, :], in_=acum[:, :, :], func=AF.Exp), reads=["acum"], writes=["ea"])
        S.act(lambda e: e.activation(out=cdec[:, :, :], in_=alast[:, :, :], func=AF.Exp), reads=["alast"], writes=["cdec"])
        S.dve(lambda e: e.tensor_tensor(out=dtdte[:, :, :], in0=dt_t[:, :, :], in1=dte[:, :, :], op=ALU.mult), reads=["dt_t", "dte"], writes=["dtdte"])
        S.pool(lambda e: e.memset(stT[:, :, :], 0.0), writes=["stT"])
        S.pool(lambda e: e.memset(stTb[:, :, :], 0.0), writes=["stTb"])

        def ssd_F(c):
            cs = slice(c * 128, (c + 1) * 128)
            b2 = c % 2
            py = 2 + b2
            xs_c = xsB[:, c, 0:512].rearrange("p (h d) -> p h d", h=8)
            S.pool(lambda e, c=c, b2=b2, xs_c=xs_c: e.tensor_tensor(out=xdt[b2][:, :, :], in0=xs_c, in1=dt_t[:, c, :].unsqueeze(2).to_broadcast([128, 8, 64]), op=ALU.mult),
                   reads=["xsB.%d" % c, "dt_t"], writes=["xdt%d" % b2])
            S.pool(lambda e, c=c, b2=b2, xs_c=xs_c: e.tensor_tensor(out=xdtd[b2][:, :, :], in0=xs_c, in1=dtdte[:, c, :].unsqueeze(2).to_broadcast([128, 8, 64]), op=ALU.mult),
                   reads=["xsB.%d" % c, "dtdte"], writes=["xdtd%d" % b2])
            S.pool(lambda e, c=c, b2=b2, xs_c=xs_c: e.tensor_tensor(out=t2[b2][:, :, :], in0=xs_c, in1=dskip_bc[:, :].unsqueeze(2).to_broadcast([128, 8, 64]), op=ALU.mult),
                   reads=["xsB.%d" % c, "dskip_bc"], writes=["t2%d" % b2])
            for g in range(2):
                S.pe(lambda e, g=g, cs=cs: e.matmul(bank(4)[:, g * 128:(g + 1) * 128], lhsT=BT[:, g, cs], rhs=CT[:, g, cs], start=True, stop=True),
                     reads=["BT.%d" % g, "CT.%d" % g], writes=[PSK[4]])
            for hh in range(2):
                pl = hh
                for h4 in range(4):
                    h = hh * 4 + h4
                    S.pe(lambda e, pl=pl, h4=h4, c=c, h=h: e.matmul(bank(pl)[:, h4 * 128:(h4 + 1) * 128], lhsT=da[:, c, h:h + 1].to_broadcast([128, 128]), rhs=Uf[:, :], start=True, stop=False),
                         reads=["da", "Uf"], writes=[PSK[pl]])
                    S.pe(lambda e, pl=pl, h4=h4: e.matmul(bank(pl)[:, h4 * 128:(h4 + 1) * 128], lhsT=identB[:, :], rhs=maskneg[:, :], start=False, stop=True),
                         reads=["identB", "maskneg"], writes=[PSK[pl]])
            for hh in range(2):
                pl = hh
                for h4 in range(4):
                    h = hh * 4 + h4
                    S.act(lambda e, pl=pl, h4=h4, c=c, h=h: e.activation(out=LT[h4][:, :], in_=bank(pl)[:, h4 * 128:(h4 + 1) * 128], func=AF.Exp, bias=nacum[:, c, h:h + 1], scale=1.0),
                          reads=[PSK[pl], "nacum"], writes=["LT%d" % h4])
                    g = h // 4
                    S.dve(lambda e, h4=h4, g=g: e.tensor_tensor(out=MT[h4][:, :], in0=bank(4)[:, g * 128:(g + 1) * 128], in1=LT[h4][:, :], op=ALU.mult),
                          reads=[PSK[4], "LT%d" % h4], writes=["MT%d" % h4])
                    S.pe(lambda e, h4=h4, h=h, b2=b2, py=py: e.matmul(bank(py)[:, h * 64:(h + 1) * 64], lhsT=MT[h4][:, :], rhs=xdt[b2][:, h, :], start=True, stop=True),
                         reads=["MT%d" % h4, "xdt%d" % b2], writes=[PSK[py]])

        def ssd_B(c):
            cs = slice(c * 128, (c + 1) * 128)
            b2 = c % 2
            py = 2 + b2
            if c > 0:
                for g in range(2):
                    S.pe(lambda e, g=g, cs=cs: e.matmul(bank(6)[:, g * 256:(g + 1) * 256], lhsT=CT[:, g, cs], rhs=stTb[:, 4 * g:4 * g + 4, :].rearrange("p h d -> p (h d)"), start=True, stop=True),
                         reads=["CT.%d" % g, "stTb"], writes=[PSK[6]])
            if c < NT - 1:
                for g in range(2):
                    S.pe(lambda e, g=g, c=c, b2=b2: e.matmul(bank(7)[:, g * 256:(g + 1) * 256], lhsT=xsB[:, c, 512 + g * 128:512 + (g + 1) * 128], rhs=xdtd[b2][:, 4 * g:4 * g + 4, :].rearrange("p h d -> p (h d)"), start=True, stop=True),
                         reads=["xsB.%d" % c, "xdtd%d" % b2], writes=[PSK[7]])
                S.dve(lambda e, c=c: e.tensor_tensor(out=stT[:, :, :], in0=stT[:, :, :], in1=cdec[:, c, :].unsqueeze(2).to_broadcast([128, 8, 64]), op=ALU.mult),
                      reads=["stT", "cdec"], writes=["stT"])
                S.dve(lambda e: e.tensor_tensor(out=stT[:, :, :], in0=bank(7)[:, 0:512].rearrange("p (h d) -> p h d", h=8), in1=stT[:, :, :], op=ALU.add),
                      reads=[PSK[7], "stT"], writes=["stT"])
            if c > 0:
                S.dve(lambda e, c=c, b2=b2: e.tensor_tensor(out=t1[b2][:, :, :], in0=bank(6)[:, :].rearrange("p (h d) -> p h d", h=8), in1=ea[:, c, :].unsqueeze(2).to_broadcast([128, 8, 64]), op=ALU.mult),
                      reads=[PSK[6], "ea"], writes=["t1"])
            if c < NT - 1:
                S.act(lambda e: e.copy(stTb[:, :, :], stT[:, :, :]), reads=["stT"], writes=["stTb"])
            if c > 0:
                S.dve(lambda e, b2=b2, py=py: e.tensor_tensor(out=t1[b2][:, :, :], in0=bank(py)[:, :].rearrange("p (h d) -> p h d", h=8), in1=t1[b2][:, :, :], op=ALU.add),
                      reads=[PSK[py], "t1"], writes=["t1"])
                S.dve(lambda e, b2=b2: e.tensor_tensor(out=t1[b2][:, :, :], in0=t1[b2][:, :, :], in1=t2[b2][:, :, :], op=ALU.add),
                      reads=["t1", "t2%d" % b2], writes=["t1"])
            else:
                S.dve(lambda e, b2=b2, py=py: e.tensor_tensor(out=t1[b2][:, :, :], in0=bank(py)[:, :].rearrange("p (h d) -> p h d", h=8), in1=t2[b2][:, :, :], op=ALU.add),
                      reads=[PSK[py], "t2%d" % b2], writes=["t1"])
            S.dve(lambda e, c=c, b2=b2: e.tensor_tensor(out=yg[b2][:, :], in0=t1[b2].rearrange("p h d -> p (h d)"), in1=sz[:, c, :], op=ALU.mult),
                  reads=["t1", "sz.%d" % c], writes=["yg%d" % b2])
            for g in range(2):
                S.act(lambda e, g=g, b2=b2: e.activation(out=junk[:, :], in_=yg[b2][:, g * 256:(g + 1) * 256], func=AF.Square, accum_out=ss[b2][:, g:g + 1]),
                      reads=["yg%d" % b2], writes=["junk", "ss%d.%d" % (b2, g)])
            S.act(lambda e, b2=b2: e.activation(out=rstd[b2][:, :], in_=ss[b2][:, :], func=AF.Ln, bias=RMS_EPS, scale=1.0 / 256.0),
                  reads=["ss%d.0" % b2, "ss%d.1" % b2], writes=["rstd%d" % b2])
            S.act(lambda e, b2=b2: e.activation(out=rstd[b2][:, :], in_=rstd[b2][:, :], func=AF.Exp, scale=-0.5), reads=["rstd%d" % b2], writes=["rstd%d" % b2])
            for g in range(2):
                S.dve(lambda e, g=g, b2=b2, c=c: e.scalar_tensor_tensor(out=m_ssd[:, c, g * 256:(g + 1) * 256], in0=yg[b2][:, g * 256:(g + 1) * 256], scalar=rstd[b2][:, g:g + 1], in1=gssd_bc[:, g * 256:(g + 1) * 256], op0=ALU.mult, op1=ALU.mult),
                      reads=["yg%d" % b2, "rstd%d" % b2, "gssd_bc"], writes=["m_ssd.%d" % c])

        ssd_F(0)
        for c in range(NT):
            if c + 1 < NT:
                ssd_F(c + 1)
            ssd_B(c)

        if dbg == 'ssd' and s == 0:
            RX.reset()
            dm = dbg_out("dbg_mssd", [128, NT * 512])
            cvt = RX.alloc([NT * 512], F32) if dbg == 'ssd' else None
            S.dve(lambda e: e.tensor_copy(cvt[:, :], m_ssd.rearrange("p t d -> p (t d)")), reads=["m_ssd.%d" % c for c in range(NT)], writes=["cvt"])
            outs.append(S.dma("sp", lambda e: e.dma_start(out=dm[:, :], in_=cvt[:, :]), reads=["cvt"]))
        if dbg == "ssd":
            break

        fence()
        RW.reset(); RACC.reset(); TT.reset()
        wB = RW.alloc([8, 1544], BF16)
        wo = RW.alloc([8, 1024], BF16)
        ah = RW.alloc([1024], F32)
        hTf = RW.alloc([8, 128], F32)
        qT = RACC.alloc([4, SEQ], BF16)
        kT = RACC.alloc([4, SEQ], BF16)
        v_aug = RACC.alloc([NT, 8, 65], BF16)
        xres = [RACC.alloc([1024], F32) for _ in range(2)]
        hpre = [RACC.alloc([1024], F32), TT.alloc([1024], F32)]
        f_raw = TT.alloc([NT, 8], F32); Gc = TT.alloc([NT, 8], F32); tot = TT.alloc([NT, 8], F32)
        Pp = TT.alloc([NT, 8], F32); Gf = TT.alloc([NT, 8], F32); Gend = TT.alloc([NT, 8], F32)
        biasT = TT.alloc([8, NT, NT], F32)
        PT = [TT.alloc([8, 128], BF16) for _ in range(2)]
        yatt = [TT.alloc([8, 64], BF16) for _ in range(2)]
        rden = [TT.alloc([8], F32) for _ in range(2)]
        mT = [TT.alloc([8, 128], BF16)] * 2
        bst = [TT.alloc([2, 6], F32) for _ in range(2)]
        mv = [TT.alloc([2], F32) for _ in range(2)]
        rs1 = [TT.alloc([1], F32) for _ in range(2)]

        S.dma("sp", lambda e: e.dma_start(out=lnG[:, :], in_=ln1_g[0:1, :].to_broadcast([128, 1024])), writes=["lnG"])
        S.dma("sp", lambda e: e.dma_start(out=lnB[:, :], in_=ln1_b[0:1, :].to_broadcast([128, 1024])), writes=["lnB"])
        S.pool(lambda e: e.memset(v_aug[:, :, :, 64:65], 1.0), writes=["v_ones"])
        evq = 0
        for qk in range(2):
            dstT = qT if qk == 0 else kT
            bcol = bq if qk == 0 else bk
            nm = "qT" if qk == 0 else "kT"
            for p in range(4):
                for blk in range(4):
                    pq = evq % 4
                    for k in range(8):
                        S.pe(lambda e, pq=pq, k=k, p=p, blk=blk, qk=qk: e.matmul(bank(pq)[:, :], lhsT=wB[:, k, qk * 512 + p * 128:qk * 512 + (p + 1) * 128], rhs=xT[:, k, blk * 512:(blk + 1) * 512], start=(k == 0), stop=(k == 7)),
                             reads=["xT.%d.%d" % (k, blk), "wB"], writes=[PSK[pq]])
                    if evq % 2 == 0:
                        S.act(lambda e, pq=pq, p=p, blk=blk, dstT=dstT, bcol=bcol: e.activation(out=dstT[:, p, blk * 512:(blk + 1) * 512], in_=bank(pq)[:, :], func=AF.Identity, bias=bcol[:, p:p + 1], scale=1.0),
                              reads=[PSK[pq], "bq", "bk"], writes=["%s.%d.%d" % (nm, p, blk)])
                    else:
                        S.dve(lambda e, pq=pq, p=p, blk=blk, dstT=dstT, bcol=bcol: e.tensor_scalar(out=dstT[:, p, blk * 512:(blk + 1) * 512], in0=bank(pq)[:, :], scalar1=bcol[:, p:p + 1], scalar2=None, op0=ALU.add),
                              reads=[PSK[pq], "bq", "bk"], writes=["%s.%d.%d" % (nm, p, blk)])
                    evq += 1
        for t in range(NT):
            pv = 4 + (t % 2)
            blk = t // 4
            for k in range(8):
                S.pe(lambda e, pv=pv, t=t, k=k: e.matmul(bank(pv)[:, :], lhsT=xT[:, k, t * 128:(t + 1) * 128], rhs=wB[:, k, 1024:1536], start=(k == 0), stop=False),
                     reads=["xT.%d.%d" % (k, blk), "wB"], writes=[PSK[pv]])
            S.pe(lambda e, pv=pv: e.matmul(bank(pv)[:, :], lhsT=ones_row[0:1, :], rhs=bv_row[0:1, :], start=False, stop=True),
                 reads=["ones_row", "bv_row"], writes=[PSK[pv]])
            if t % 2 == 0:
                S.act(lambda e, pv=pv, t=t: e.copy(v_aug[:, t, :, 0:64], bank(pv)[:, :].rearrange("p (h d) -> p h d", h=8)), reads=[PSK[pv]], writes=["v.%d" % t])
            else:
                S.dve(lambda e, pv=pv, t=t: e.tensor_copy(v_aug[:, t, :, 0:64], bank(pv)[:, :].rearrange("p (h d) -> p h d", h=8)), reads=[PSK[pv]], writes=["v.%d" % t])
            for k in range(8):
                S.pe(lambda e, t=t, k=k: e.matmul(bank(6)[:, t * 8:(t + 1) * 8], lhsT=xT[:, k, t * 128:(t + 1) * 128], rhs=wB[:, k, 1536:1544], start=(k == 0), stop=(k == 7)),
                     reads=["xT.%d.%d" % (k, blk), "wB"], writes=[PSK[6]])
        S.dve(lambda e: e.tensor_tensor(out=f_raw[:, :, :], in0=bank(6)[:, 0:128].rearrange("p (t h) -> p t h", h=8), in1=bdtf[:, 8:16].unsqueeze(1).to_broadcast([128, NT, 8]), op=ALU.add),
              reads=[PSK[6], "bdtf1"], writes=["f_raw"])
        S.act(lambda e: e.activation(out=f_raw[:, :, :], in_=f_raw[:, :, :], func=AF.Exp, scale=-1.0), reads=["f_raw"], writes=["f_raw"])
        S.act(lambda e: e.activation(out=f_raw[:, :, :], in_=f_raw[:, :, :], func=AF.Ln, bias=1.0, scale=1.0), reads=["f_raw"], writes=["f_raw"])
        frf = f_raw.rearrange("p t h -> p (t h)")
        S.pe(lambda e: e.matmul(bank(7)[:, 0:128], lhsT=Uf[:, :], rhs=frf, start=True, stop=True), reads=["Uf", "f_raw"], writes=[PSK[7]])
        S.pe(lambda e: e.matmul(bank(7)[:, 128:256], lhsT=onesF[:, :], rhs=frf, start=True, stop=True), reads=["onesF", "f_raw"], writes=[PSK[7]])
        S.dve(lambda e: e.tensor_copy(Gc.rearrange("p t h -> p (t h)"), bank(7)[:, 0:128]), reads=[PSK[7]], writes=["Gc"])
        S.dve(lambda e: e.tensor_copy(tot.rearrange("p t h -> p (t h)"), bank(7)[:, 128:256]), reads=[PSK[7]], writes=["tot"])
        S.pool(lambda e: e.memset(Pp[:, 0, :], 0.0), writes=["Pp"])
        for t in range(1, NT):
            S.dve(lambda e, t=t: e.tensor_tensor(out=Pp[:, t, :], in0=Pp[:, t - 1, :], in1=tot[:, t - 1, :], op=ALU.add), reads=["Pp", "tot"], writes=["Pp"])
        S.dve(lambda e: e.tensor_tensor(out=Gf[:, :, :], in0=Gc[:, :, :], in1=Pp[:, :, :], op=ALU.add), reads=["Gc", "Pp"], writes=["Gf"])
        S.dve(lambda e: e.tensor_tensor(out=Gend[:, :, :], in0=tot[:, :, :], in1=Pp[:, :, :], op=ALU.add), reads=["tot", "Pp"], writes=["Gend"])
        for h in range(8):
            S.dve(lambda e, h=h: e.tensor_tensor(out=biasT[:, h, :, :], in0=Gf[:, :, h].unsqueeze(1).to_broadcast([128, NT, NT]), in1=Gend[:, :, h].unsqueeze(2).to_broadcast([128, NT, NT]), op=ALU.subtract),
                  reads=["Gf", "Gend"], writes=["biasT"])

        fence()
        RX.reset()
        hT = RX.alloc([8, SEQ], BF16)
        NEXP = int(os.environ.get("KNEXP", 16))

        def load_expert(ex):
            sl = ex % 2
            S.dma("pool", lambda e, ex=ex, sl=sl: e.dma_start(out=wgu[sl][:, :, 0:512], in_=w_gate[ex].rearrange("(k p) f -> p k f", p=128)), writes=["wg.%d" % sl], nofence=(ex == 0))
            S.dma("pool", lambda e, ex=ex, sl=sl: e.dma_start(out=wgu[sl][:, :, 512:1024], in_=w_up[ex].rearrange("(k p) f -> p k f", p=128)), writes=["wu.%d" % sl], nofence=(ex == 0))
            S.dma("pool", lambda e, ex=ex, sl=sl: e.dma_start(out=wdn[sl][:, :, :], in_=w_down[ex].rearrange("(k p) d -> p k d", p=128)), writes=["wd.%d" % sl], nofence=(ex == 0))


        load_expert(0)
        NTI = int(os.environ.get("KATT_TILES", NT))
        S.fold_now = os.environ.get("KFOLD", "all") in ("attmoe", "all")
        units = []
        for i in range(NTI):
            for p in range(4):
                for j0 in range(0, i + 1, 4):
                    units.append((i, p, list(range(j0, min(j0 + 4, i + 1)))))

        def emit_S(u, gi):
            i, p, js = u
            pb0 = 2 * (gi % 2)
            for jj, j in enumerate(js):
                for hh in range(2):
                    r0 = hh * 64
                    S.pe(lambda e, bk=pb0 + hh, jj=jj, j=j, p=p, r0=r0, i=i: e.matmul(bank(bk)[:, jj * 128:(jj + 1) * 128], lhsT=kT[r0:r0 + 64, p, j * 128:(j + 1) * 128], rhs=qT[r0:r0 + 64, p, i * 128:(i + 1) * 128], start=True, stop=True),
                         reads=["kT.%d.%d" % (p, j // 4), "qT.%d.%d" % (p, i // 4)], writes=[PSK[pb0 + hh]])

        def emit_E(u, gi):
            i, p, js = u
            pb0 = 2 * (gi % 2); pb = gi % 2
            for jj, j in enumerate(js):
                for hh in range(2):
                    h = 2 * p + hh
                    c8 = jj * 2 + hh
                    S.act(lambda e, bk=pb0 + hh, pb=pb, c8=c8, jj=jj, j=j, h=h, i=i: e.activation(out=PT[pb][:, c8, :], in_=bank(bk)[:, jj * 128:(jj + 1) * 128], func=AF.Exp, bias=biasT[:, h, i, j:j + 1], scale=ATT_SCALE),
                          reads=[PSK[pb0 + hh], "biasT"], writes=["PT%d.%d" % (pb, c8)])
                    if j == i:
                        S.dve(lambda e, pb=pb, c8=c8: e.tensor_tensor(out=PT[pb][:, c8, :], in0=PT[pb][:, c8, :], in1=triU[:, :], op=ALU.mult),
                              reads=["PT%d.%d" % (pb, c8), "triU"], writes=["PT%d.%d" % (pb, c8)])

        def emit_V(u, gi):
            i, p, js = u
            pb = gi % 2
            for jj, j in enumerate(js):
                for hh in range(2):
                    h = 2 * p + hh
                    c8 = jj * 2 + hh
                    po = 4 + h // 4; oc = (h % 4) * 65
                    S.pe(lambda e, pb=pb, c8=c8, j=j, h=h, po=po, oc=oc, i=i: e.matmul(bank(po)[:, oc:oc + 65], lhsT=PT[pb][:, c8, :], rhs=v_aug[:, j, h, :], start=(j == 0 and h % 4 == 0), stop=(j == i and h % 4 == 3)),
                         reads=["PT%d.%d" % (pb, c8), "v.%d" % j, "v_ones"], writes=[PSK[po]])

        def tail_N(i):
            b2 = i % 2
            S.dma("sp", lambda e, i=i, b2=b2, s=s: e.dma_start(out=xres[b2][:, :], in_=x[s, i * 128:(i + 1) * 128, :]), writes=["xres%d" % b2])
            for hb in range(2):
                ov = bank(4 + hb)[:, 0:260].rearrange("p (h d) -> p h d", h=4)
                S.dve(lambda e, hb=hb, ov=ov, b2=b2: e.reciprocal(rden[b2][:, 4 * hb:4 * hb + 4], ov[:, :, 64]), reads=[PSK[4 + hb]], writes=["rden%d.%d" % (b2, hb)])
                S.dve(lambda e, hb=hb, ov=ov, b2=b2: e.tensor_tensor(out=yatt[b2][:, 4 * hb:4 * hb + 4, :], in0=ov[:, :, 0:64], in1=rden[b2][:, 4 * hb:4 * hb + 4].unsqueeze(2).to_broadcast([128, 4, 64]), op=ALU.mult),
                      reads=[PSK[4 + hb], "rden%d.%d" % (b2, hb)], writes=["yatt%d.%d" % (b2, hb)])

        def tail_T(i):
            b2 = i % 2
            yf = yatt[b2].rearrange("p h d -> p (h d)")
            for ec in range(8):
                src = m_ssd[:, i, ec * 128:(ec + 1) * 128] if ec < 4 else yf[:, (ec - 4) * 128:(ec - 3) * 128]
                rk = ["m_ssd.%d" % i] if ec < 4 else ["yatt%d.%d" % (b2, (ec - 4) // 2)]
                S.pe(lambda e, ec=ec, src=src: e.transpose(bankb(6)[:, ec * 128:(ec + 1) * 128], src, identB[:, :]), reads=rk + ["identB"], writes=[PSK[6]])
            S.dve(lambda e, b2=b2: e.tensor_copy(mT[b2].rearrange("p a b -> p (a b)"), bankb(6)[:, 0:1024]), reads=[PSK[6]], writes=["mT"])

        def tail_Oq(i, q):
            b2 = i % 2
            half = q // 2
            hp = hpre[b2]
            for ec in range(4 * (q % 2), 4 * (q % 2) + 4):
                S.pe(lambda e, ec=ec, b2=b2, half=half: e.matmul(bank(7)[:, :], lhsT=mT[b2][:, ec, :], rhs=wo[:, ec, half * 512:(half + 1) * 512], start=(ec == 0), stop=(ec == 7)),
                     reads=["mT", "wo"], writes=[PSK[7]])
            if q % 2 == 1:
                S.dve(lambda e, hp=hp, b2=b2, half=half: e.scalar_tensor_tensor(out=hp[:, half * 512:(half + 1) * 512], in0=xres[b2][:, half * 512:(half + 1) * 512], scalar=ALPHA, in1=bank(7)[:, :], op0=ALU.mult, op1=ALU.add),
                      reads=["xres%d" % b2, PSK[7]], writes=["hpre%d.%d" % (b2, half)])
                S.dve(lambda e, hp=hp, half=half, b2=b2: e.bn_stats(bst[b2][:, half, :], hp[:, half * 512:(half + 1) * 512]), reads=["hpre%d.%d" % (b2, half)], writes=["bst%d.%d" % (b2, half)])
            if q == 3:
                S.dve(lambda e, b2=b2: e.bn_aggr(mv[b2][:, :], bst[b2][:, :, :]), reads=["bst%d.0" % b2, "bst%d.1" % b2], writes=["mv%d" % b2])

        def tail_L(i):
            b2 = i % 2
            hp = hpre[b2]
            HK = ["hpre%d.0" % b2, "hpre%d.1" % b2]
            S.act(lambda e, b2=b2: e.activation(out=rs1[b2][:, :], in_=mv[b2][:, 1:2], func=AF.Ln, bias=LN_EPS, scale=1.0), reads=["mv%d" % b2], writes=["rs1%d" % b2])
            S.act(lambda e, b2=b2: e.activation(out=rs1[b2][:, :], in_=rs1[b2][:, :], func=AF.Exp, scale=-0.5), reads=["rs1%d" % b2], writes=["rs1%d" % b2])
            S.dve(lambda e, hp=hp, b2=b2: e.tensor_scalar(out=hp[:, :], in0=hp[:, :], scalar1=mv[b2][:, 0:1], scalar2=rs1[b2][:, 0:1], op0=ALU.subtract, op1=ALU.mult),
                  reads=HK + ["mv%d" % b2, "rs1%d" % b2], writes=HK)
            S.dve(lambda e, hp=hp: e.tensor_tensor(out=hp[:, :], in0=hp[:, :], in1=lnG[:, :], op=ALU.mult), reads=HK + ["lnG"], writes=HK)
            S.dve(lambda e, hp=hp: e.tensor_tensor(out=hp[:, :], in0=hp[:, :], in1=lnB[:, :], op=ALU.add), reads=HK + ["lnB"], writes=HK)
            S.dve(lambda e, hp=hp: e.tensor_scalar(out=ah[:, :], in0=hp[:, :], scalar1=ALPHA, scalar2=None, op0=ALU.mult), reads=HK, writes=["ah"])
            S.dma("sp", lambda e, i=i, s=s: e.dma_start(out=h_scr[s, i * 128:(i + 1) * 128, :], in_=ah[:, :]), reads=["ah"], writes=["h_scr.%d" % i])

        def tail_H(i, half):
            b2 = i % 2
            hp = hpre[b2]
            for q4 in range(4):
                ec = half * 4 + q4
                S.pe(lambda e, q4=q4, ec=ec, hp=hp: e.transpose(bank(6)[:, q4 * 128:(q4 + 1) * 128], hp[:, ec * 128:(ec + 1) * 128], identF[:, :]), reads=["hpre%d.%d" % (b2, half), "identF"], writes=[PSK[6]])
            S.dve(lambda e, half=half, i=i: e.tensor_copy(hT[:, 4 * half:4 * half + 4, i * 128:(i + 1) * 128], bank(6)[:, :].rearrange("p (a b) -> p a b", a=4)), reads=[PSK[6]], writes=["hT.%d.%d" % (i, half)])
            S.dve(lambda e, half=half: e.tensor_copy(hTf[:, 4 * half:4 * half + 4, :], bank(6)[:, :].rearrange("p (a b) -> p a b", a=4)), reads=[PSK[6]], writes=["hTf.%d" % half])

        def tail_R(i, part):
            for ec in range(4 * part, 4 * part + 4):
                S.pe(lambda e, ec=ec: e.matmul(bank(6)[:, 0:20], lhsT=hTf[:, ec, :], rhs=rw[:, ec, :], start=(ec == 0), stop=(ec == 7)), reads=["hTf.%d" % (ec // 4)] + RWK, writes=[PSK[6]])
            if part == 1:
                S.dve(lambda e, i=i: e.tensor_tensor(out=logits[:, i, :], in0=bank(6)[:, 0:20], in1=rb_bc[:, :], op=ALU.add), reads=[PSK[6], "rb0", "rb1"], writes=["logits.%d" % i])

        TD = [int(v) for v in os.environ.get("KTAIL", "0,1,2,3,4,5,9,11,12,14").split(",")]
        TAIL = [(TD[0], tail_N), (TD[1], tail_T), (TD[2], lambda i: tail_Oq(i, 0)), (TD[3], lambda i: tail_Oq(i, 1)), (TD[4], lambda i: tail_Oq(i, 2)), (TD[5], lambda i: tail_Oq(i, 3)),
                (TD[6], tail_L), (TD[7], lambda i: tail_H(i, 0)), (TD[8], lambda i: tail_H(i, 1)), (TD[9], lambda i: (tail_R(i, 0), tail_R(i, 1)))]
        NSTEP = len(TAIL)
        done_steps = set()

        def emit_step(t, k):
            if t < 0 or (t, k) in done_steps:
                return
            for kk in range(k):
                emit_step(t, kk)
            emit_step(t - 1, k)
            if k == 1:
                emit_step(t - 1, 5)
            if k == 7:
                emit_step(t - 1, NSTEP - 1)
            if k == 0:
                for kk in range(NSTEP):
                    emit_step(t - 2, kk)
            done_steps.add((t, k))
            TAIL[k][1](t)

        pending = []
        def tick():
            keep = []
            for ent in pending:
                if ent[0] <= 0:
                    emit_step(ent[1], ent[2])
                else:
                    ent[0] -= 1
                    keep.append(ent)
            pending[:] = keep
        prev = None
        for gi, u in enumerate(units):
            emit_S(u, gi)
            emit_E(u, gi)
            if prev is not None:
                emit_V(prev[0], prev[1])
                if prev[0][0] != u[0]:
                    for k, (dly, fn) in enumerate(TAIL):
                        pending.append([dly, prev[0][0], k])
            tick()
            prev = (u, gi)
        if prev is not None:
            emit_V(prev[0], prev[1])
            for k, (dly, fn) in enumerate(TAIL):
                pending.append([dly, prev[0][0], k])
        while pending:
            tick()

        if dbg == "att" and s == 0:
            fence()
            RACC.reset()
            cvt = RACC.alloc([NT * 1024], BF16)
            d1 = dbg_out("dbg_hT", [128, 8 * SEQ]); d2 = dbg_out("dbg_logits", [128, NT * 20])
            cv2 = RACC.alloc([2 * SEQ], F32)
            for q in range(4):
                S.dve(lambda e, q=q: e.tensor_copy(cv2[:, :], hT[:, 2 * q:2 * q + 2, :].rearrange("p a b -> p (a b)")), reads=[], writes=["cv2"])
                outs.append(S.dma("sp", lambda e, q=q: e.dma_start(out=d1[:, 2 * q * SEQ:(2 * q + 2) * SEQ], in_=cv2[:, :]), reads=["cv2"]))
            outs.append(S.dma("sp", lambda e: e.dma_start(out=d2[:, :], in_=logits.rearrange("p t j -> p (t j)")), reads=["logits.%d" % i for i in range(int(os.environ.get("KATT_TILES", NT)))] if int(os.environ.get("KATT_STAGE", 9)) >= 7 else []))
            break


        S.fold_now = os.environ.get("KFOLD", "all") == "all"
        fence()
        TT.reset(); RW.reset(); RACC.reset()
        S.dma("sp", lambda e: e.dma_start(out=lnG[:, :], in_=ln2_g[0:1, :].to_broadcast([128, 1024])), writes=["lnG"])
        S.dma("sp", lambda e: e.dma_start(out=lnB[:, :], in_=ln2_b[0:1, :].to_broadcast([128, 1024])), writes=["lnB"])
        LOGK = ["logits.%d" % i for i in range(NT)]
        lg = logits[:, :, 0:4]
        le4 = logits[:, :, 4:20].rearrange("p t (g j) -> p t g j", g=4)
        gmax = TT.alloc([NT], F32); goh = TT.alloc([NT, 4], F32); gex = TT.alloc([NT, 4], F32)
        gsum = TT.alloc([NT], F32); gval = TT.alloc([NT], F32)
        tmp16 = TT.alloc([NT, 4, 4], F32); esel = TT.alloc([NT, 4], F32)
        m1 = TT.alloc([NT], F32); oh1 = TT.alloc([NT, 4], F32); e2 = TT.alloc([NT, 4], F32)
        m2 = TT.alloc([NT], F32); oh2 = TT.alloc([NT, 4], F32); dd = TT.alloc([NT], F32)
        w1 = TT.alloc([NT], F32); w2 = TT.alloc([NT], F32); cw1 = TT.alloc([NT], F32); cw2 = TT.alloc([NT], F32)
        cj = TT.alloc([NT, 4], F32); cj2 = TT.alloc([NT, 4], F32)
        bc4 = lambda a: a.unsqueeze(2).to_broadcast([128, NT, 4])
        S.dve(lambda e: e.tensor_reduce(out=gmax[:, :], in_=lg, axis=AX.X, op=ALU.max), reads=LOGK, writes=["gmax"])
        S.dve(lambda e: e.tensor_tensor(out=goh[:, :, :], in0=lg, in1=bc4(gmax[:, :]), op=ALU.is_equal), reads=LOGK + ["gmax"], writes=["goh"])
        S.dve(lambda e: e.tensor_tensor(out=gex[:, :, :], in0=lg, in1=bc4(gmax[:, :]), op=ALU.subtract), reads=LOGK + ["gmax"], writes=["gex"])
        S.act(lambda e: e.activation(out=gex[:, :, :], in_=gex[:, :, :], func=AF.Exp), reads=["gex"], writes=["gex"])
        S.dve(lambda e: e.tensor_reduce(out=gsum[:, :], in_=gex[:, :, :], axis=AX.X, op=ALU.add), reads=["gex"], writes=["gsum"])
        S.dve(lambda e: e.reciprocal(gval[:, :], gsum[:, :]), reads=["gsum"], writes=["gval"])
        S.dve(lambda e: e.tensor_tensor(out=tmp16[:, :, :, :], in0=le4, in1=goh[:, :, :].unsqueeze(3).to_broadcast([128, NT, 4, 4]), op=ALU.mult), reads=LOGK + ["goh"], writes=["tmp16"])
        S.dve(lambda e: e.tensor_reduce(out=esel[:, :, :], in_=tmp16.rearrange("p t g j -> p t j g"), axis=AX.X, op=ALU.add), reads=["tmp16"], writes=["esel"])
        S.dve(lambda e: e.tensor_reduce(out=m1[:, :], in_=esel[:, :, :], axis=AX.X, op=ALU.max), reads=["esel"], writes=["m1"])
        S.dve(lambda e: e.tensor_tensor(out=oh1[:, :, :], in0=esel[:, :, :], in1=bc4(m1[:, :]), op=ALU.is_equal), reads=["esel", "m1"], writes=["oh1"])
        S.dve(lambda e: e.scalar_tensor_tensor(out=e2[:, :, :], in0=oh1[:, :, :], scalar=-1e30, in1=esel[:, :, :], op0=ALU.mult, op1=ALU.add), reads=["oh1", "esel"], writes=["e2"])
        S.dve(lambda e: e.tensor_reduce(out=m2[:, :], in_=e2[:, :, :], axis=AX.X, op=ALU.max), reads=["e2"], writes=["m2"])
        S.dve(lambda e: e.tensor_tensor(out=oh2[:, :, :], in0=e2[:, :, :], in1=bc4(m2[:, :]), op=ALU.is_equal), reads=["e2", "m2"], writes=["oh2"])
        S.dve(lambda e: e.tensor_tensor(out=dd[:, :], in0=m2[:, :], in1=m1[:, :], op=ALU.subtract), reads=["m1", "m2"], writes=["dd"])
        S.act(lambda e: e.activation(out=dd[:, :], in_=dd[:, :], func=AF.Exp), reads=["dd"], writes=["dd"])
        S.dve(lambda e: e.tensor_scalar(out=w1[:, :], in0=dd[:, :], scalar1=1.0, scalar2=None, op0=ALU.add), reads=["dd"], writes=["w1"])
        S.dve(lambda e: e.reciprocal(w1[:, :], w1[:, :]), reads=["w1"], writes=["w1"])
        S.dve(lambda e: e.tensor_tensor(out=w2[:, :], in0=dd[:, :], in1=w1[:, :], op=ALU.mult), reads=["dd", "w1"], writes=["w2"])
        S.dve(lambda e: e.tensor_tensor(out=cw1[:, :], in0=gval[:, :], in1=w1[:, :], op=ALU.mult), reads=["gval", "w1"], writes=["cw1"])
        S.dve(lambda e: e.tensor_tensor(out=cw2[:, :], in0=gval[:, :], in1=w2[:, :], op=ALU.mult), reads=["gval", "w2"], writes=["cw2"])
        S.dve(lambda e: e.tensor_tensor(out=cj[:, :, :], in0=oh1[:, :, :], in1=bc4(cw1[:, :]), op=ALU.mult), reads=["oh1", "cw1"], writes=["cj"])
        S.dve(lambda e: e.tensor_tensor(out=cj2[:, :, :], in0=oh2[:, :, :], in1=bc4(cw2[:, :]), op=ALU.mult), reads=["oh2", "cw2"], writes=["cj2"])
        S.dve(lambda e: e.tensor_tensor(out=cj[:, :, :], in0=cj[:, :, :], in1=cj2[:, :, :], op=ALU.add), reads=["cj", "cj2"], writes=["cj"])
        S.dve(lambda e: e.tensor_tensor(out=comb.rearrange("p t (g j) -> p t g j", g=4), in0=goh[:, :, :].unsqueeze(3).to_broadcast([128, NT, 4, 4]), in1=cj[:, :, :].unsqueeze(2).to_broadcast([128, NT, 4, 4]), op=ALU.mult),
              reads=["goh", "cj"], writes=["comb"])

        S.fold_now = os.environ.get("KFOLD", "all") in ("moe", "attmoe", "all")
        acc = RACC.alloc([NT, 1024], F32)
        sg = [TT.alloc([512], BF16) for _ in range(2)]
        actT = [TT.alloc([4, 512], BF16) for _ in range(2)]
        obuf = [TT.alloc([1024], F32) for _ in range(2)]
        bst2 = [TT.alloc([2, 6], F32) for _ in range(2)]
        mv2 = [TT.alloc([2], F32) for _ in range(2)]
        rs2 = [TT.alloc([1], F32) for _ in range(2)]
        for q in range(4):
            S.dma("sp", lambda e, q=q, s=s: e.dma_start(out=acc[:, 4 * q:4 * q + 4, :], in_=h_scr[s, q * 512:(q + 1) * 512, :].rearrange("(t p) d -> p t d", p=128)),
                  reads=["h_scr.%d" % t for t in range(4 * q, 4 * q + 4)], writes=["acc.%d" % t for t in range(4 * q, 4 * q + 4)])
        def ln2_stats(t):
            b2 = t % 2
            for c2 in range(2):
                S.dve(lambda e, c2=c2, t=t, b2=b2: e.bn_stats(bst2[b2][:, c2, :], acc[:, t, c2 * 512:(c2 + 1) * 512]), reads=["acc.%d" % t], writes=["bst2%d.%d" % (b2, c2)])
            S.dve(lambda e, b2=b2: e.bn_aggr(mv2[b2][:, :], bst2[b2][:, :, :]), reads=["bst2%d.0" % b2, "bst2%d.1" % b2], writes=["mv2%d" % b2])

        def ln2_apply(t):
            b2 = t % 2
            S.act(lambda e, b2=b2: e.activation(out=rs2[b2][:, :], in_=mv2[b2][:, 1:2], func=AF.Ln, bias=LN_EPS, scale=1.0), reads=["mv2%d" % b2], writes=["rs2%d" % b2])
            S.act(lambda e, b2=b2: e.activation(out=rs2[b2][:, :], in_=rs2[b2][:, :], func=AF.Exp, scale=-0.5), reads=["rs2%d" % b2], writes=["rs2%d" % b2])
            S.dve(lambda e, t=t, b2=b2: e.tensor_scalar(out=obuf[b2][:, :], in0=acc[:, t, :], scalar1=mv2[b2][:, 0:1], scalar2=rs2[b2][:, 0:1], op0=ALU.subtract, op1=ALU.mult),
                  reads=["acc.%d" % t, "mv2%d" % b2, "rs2%d" % b2], writes=["obuf%d" % b2])
            S.dve(lambda e, b2=b2: e.tensor_tensor(out=obuf[b2][:, :], in0=obuf[b2][:, :], in1=lnG[:, :], op=ALU.mult), reads=["obuf%d" % b2, "lnG"], writes=["obuf%d" % b2])
            S.dve(lambda e, b2=b2: e.tensor_tensor(out=obuf[b2][:, :], in0=obuf[b2][:, :], in1=lnB[:, :], op=ALU.add), reads=["obuf%d" % b2, "lnB"], writes=["obuf%d" % b2])
            outs.append(S.dma("sp", lambda e, t=t, b2=b2, s=s: e.dma_start(out=out[s, t * 128:(t + 1) * 128, :], in_=obuf[b2][:, :]), reads=["obuf%d" % b2], writes=["out.%d.%d" % (s, t)]))

        ln2_prev = []
        if NEXP > 1:
            load_expert(1)
        cg = 0; cd = 0
        for ex in range(NEXP):
            sl = ex % 2
            for tb in range(4):
                ab = (ex * 4 + tb) % 2
                hk = ["hT.%d.%d" % (i, hf) for i in range(4 * tb, 4 * tb + 4) for hf in range(2)]
                for fc in range(4):
                    pg = cg % 2; cg += 1
                    for k in range(8):
                        S.pe(lambda e, pg=pg, k=k, fc=fc, tb=tb, sl=sl: e.matmul(bank(pg)[:, :], lhsT=wgu[sl][:, k, fc * 128:(fc + 1) * 128], rhs=hT[:, k, tb * 512:(tb + 1) * 512], start=(k == 0), stop=(k == 7)),
                             reads=["wg.%d" % sl] + hk, writes=[PSK[pg]])
                    for k in range(8):
                        S.pe(lambda e, pg=pg, k=k, fc=fc, tb=tb, sl=sl: e.matmul(bank(2 + pg)[:, :], lhsT=wgu[sl][:, k, 512 + fc * 128:512 + (fc + 1) * 128], rhs=hT[:, k, tb * 512:(tb + 1) * 512], start=(k == 0), stop=(k == 7)),
                             reads=["wu.%d" % sl] + hk, writes=[PSK[2 + pg]])
                    S.act(lambda e, pg=pg: e.activation(out=sg[pg][:, :], in_=bank(pg)[:, :], func=AF.Silu), reads=[PSK[pg]], writes=["sg%d" % pg])
                    S.dve(lambda e, pg=pg, ab=ab, fc=fc: e.tensor_tensor(out=actT[ab][:, fc, :], in0=bank(2 + pg)[:, :], in1=sg[pg][:, :], op=ALU.mult),
                          reads=[PSK[2 + pg], "sg%d" % pg], writes=["actT%d.%d" % (ab, fc)])
                for tt in range(4):
                    t = tb * 4 + tt
                    pdi = 2 + (cd % 2); cd += 1
                    for half in range(2):
                        for fc in range(4):
                            S.pe(lambda e, pdi=pdi, half=half, fc=fc, ab=ab, tt=tt, sl=sl: e.matmul(pd[pdi][:, half * 512:(half + 1) * 512], lhsT=actT[ab][:, fc, tt * 128:(tt + 1) * 128], rhs=wdn[sl][:, fc, half * 512:(half + 1) * 512], start=(fc == 0), stop=(fc == 3)),
                                 reads=["actT%d.%d" % (ab, fc), "wd.%d" % sl], writes=[PSK[2 * pdi + half]])
                    S.dve(lambda e, pdi=pdi, t=t, ex=ex: e.scalar_tensor_tensor(out=acc[:, t, :], in0=pd[pdi][:, :], scalar=comb[:, t, ex:ex + 1], in1=acc[:, t, :], op0=ALU.mult, op1=ALU.add),
                          reads=[PSK[2 * pdi], PSK[2 * pdi + 1], "comb", "acc.%d" % t], writes=["acc.%d" % t])
                    if ex == NEXP - 1:
                        ln2_stats(t)
                        if ln2_prev:
                            ln2_apply(ln2_prev.pop())
                        ln2_prev.append(t)
            if ex + 2 < NEXP:
                load_expert(ex + 2)
        while ln2_prev:
            ln2_apply(ln2_prev.pop())
        S.fold_now = os.environ.get("KFOLD", "all") == "all"
        if s + 1 < NSEQ:
            fence()
        if dbg == "one":
            break

    with nc.allow_non_contiguous_dma(reason="tiny constant loads"):
        st = S.emit(outs)
    return nc, st, dbg_t


_CACHE = {}


def _get_program():
    if "p" not in _CACHE:
        _CACHE["p"] = build_program(dbg=os.environ.get("KDBG", ""))
    return _CACHE["p"]


def kernel(**inputs):
    nc, st, dbg_t = _get_program()
    f = lambda a: np.ascontiguousarray(np.asarray(a, dtype=np.float32))
    x = f(inputs["x"])
    shared = {
        "w_in": f(inputs["w_in"])[0], "b_in": f(inputs["b_in"]).reshape(1, DIN),
        "conv_w": f(inputs["conv_w"])[0], "conv_b": f(inputs["conv_b"]).reshape(1, 1024),
        "a_log": f(inputs["a_log"]).reshape(1, 8), "d_skip": f(inputs["d_skip"]).reshape(1, 8),
        "ssd_norm_g": f(inputs["ssd_norm_g"]).reshape(1, 512), "w_out": f(inputs["w_out"])[0],
        "ln1_g": f(inputs["ln1_g"]).reshape(1, DM), "ln1_b": f(inputs["ln1_b"]).reshape(1, DM),
        "router_group_w": f(inputs["router_group_w"])[0], "router_group_b": f(inputs["router_group_b"]).reshape(1, 4),
        "router_expert_w": f(inputs["router_expert_w"])[0], "router_expert_b": f(inputs["router_expert_b"]).reshape(1, 16),
        "w_gate": f(inputs["w_gate"])[0], "w_up": f(inputs["w_up"])[0], "w_down": f(inputs["w_down"])[0],
        "ln2_g": f(inputs["ln2_g"]).reshape(1, DM), "ln2_b": f(inputs["ln2_b"]).reshape(1, DM),
    }
    ncores = int(os.environ.get("KCORES", NCORES))
    in_maps = []
    for c in range(ncores):
        m = dict(shared)
        m["x"] = np.ascontiguousarray(x[c * NSEQ:(c + 1) * NSEQ])
        in_maps.append(m)
    res = run_bass_kernel_spmd(nc, in_maps, core_ids=list(range(ncores)))
    if os.environ.get("KDBG", ""):
        _CACHE["dbg"] = res.results
    outp = np.concatenate([r["out"] for r in res.results], axis=0)
    return outp.astype(np.float32)
```

```python
import os
import numpy as np
import concourse.bass as bass
import concourse.mybir as mybir
from concourse.bass_utils import run_bass_kernel_spmd

F32 = mybir.dt.float32
BF16 = mybir.dt.bfloat16
U8 = mybir.dt.uint8
AF = mybir.ActivationFunctionType
ALU = mybir.AluOpType
AX = mybir.AxisListType

NCORES = 8
NSEQ = 2
SEQ = 2048
NT = 16
DM = 1024
DIN = 3088
ALPHA = float(2.0 ** 0.25)
LN_EPS = 1e-5
RMS_EPS = 1e-5
ATT_SCALE = 0.125
NEG = -30000.0
FOLD_ENG = tuple(os.environ.get("KFOLDENG", "act,dve").split(","))


class Op:
    __slots__ = ("eng", "fn", "reads", "writes", "deps", "signal", "sigval", "dma", "gi", "nofence", "fold")


class Sched:
    COMPUTE = ("pe", "act", "dve", "pool")

    def __init__(self, nc, n_dma_sems=40):
        self.nc = nc
        self.h = {"pe": nc.tensor, "act": nc.scalar, "dve": nc.vector, "pool": nc.gpsimd, "sp": nc.sync}
        self.ops = []
        self.last_w = {}
        self.readers = {}
        self.n_dma_sems = n_dma_sems
        self.live_dma = []
        self.nfence = 0
        self.fold_now = os.environ.get("KFOLD", "all") == "all"

    def add(self, eng, fn, reads=(), writes=(), dma=False, nofence=False):
        o = Op()
        o.eng = eng; o.fn = fn; o.reads = tuple(reads); o.writes = tuple(writes)
        o.deps = []; o.signal = False; o.sigval = None; o.dma = dma; o.gi = len(self.ops); o.nofence = nofence
        o.fold = self.fold_now
        for r in o.reads:
            p = self.last_w.get(r)
            if p is not None:
                self._dep(o, p, True)
            if r.startswith("ps"):
                rd = self.readers.get(r)
                if rd:
                    for q in rd.values():
                        if q.eng != eng:
                            self._dep(o, q, True)
        for w in o.writes:
            p = self.last_w.get(w)
            if p is not None:
                self._dep(o, p, False)
            rd = self.readers.get(w)
            if rd:
                for q in rd.values():
                    self._dep(o, q, False)
        for r in o.reads:
            d = self.readers.setdefault(r, {})
            d[("dma", o.gi) if dma else eng] = o
        for w in o.writes:
            self.last_w[w] = o
            self.readers[w] = {}
        self.ops.append(o)
        if dma and not nofence:
            self.live_dma.append(o)
        return o

    def _dep(self, o, p, raw):
        if p is o:
            return
        if (not p.dma) and (not o.dma) and p.eng == o.eng:
            if o.eng == "pe":
                return
        o.deps.append(p)
        p.signal = True

    def pe(self, fn, reads=(), writes=()): return self.add("pe", fn, reads, writes)
    def act(self, fn, reads=(), writes=()): return self.add("act", fn, reads, writes)
    def dve(self, fn, reads=(), writes=()): return self.add("dve", fn, reads, writes)
    def pool(self, fn, reads=(), writes=()): return self.add("pool", fn, reads, writes)
    def dma(self, q, fn, reads=(), writes=(), nofence=False):
        return self.add(q, fn, reads, writes, dma=True, nofence=nofence)

    def fence(self, scratch):
        n = self.nfence; self.nfence += 1
        a_keys = []
        col = {"pe": None, "act": 0, "dve": 1, "pool": 2}
        for e in ("act", "dve", "pool"):
            k = "fenceA.%d.%s" % (n, e)
            c = col[e]
            if e == "act":
                self.add(e, (lambda eh, c=c: eh.activation(out=scratch[:, c:c + 1], in_=scratch[:, 8:9], func=AF.Copy)), reads=(), writes=(k,))
            else:
                self.add(e, (lambda eh, c=c: eh.memset(scratch[:, c:c + 1], 0.0)), reads=(), writes=(k,))
            a_keys.append(k)
        k = "fenceA.%d.pe" % n
        self.add("pe", (lambda eh: eh.matmul(self.fence_ps[0:1, 0:1], lhsT=self.fence_w[0:1, 0:1], rhs=self.fence_w[0:1, 0:1], start=True, stop=True)),
                 reads=(), writes=(k, "ps7"))
        a_keys.append(k)
        dmas = self.live_dma
        self.live_dma = []
        for e in ("act", "dve", "pool", "pe", "sp"):
            kb = "fenceB.%d.%s" % (n, e)
            if e == "act":
                o = self.add(e, (lambda eh: eh.activation(out=scratch[:, 3:4], in_=scratch[:, 8:9], func=AF.Copy)), reads=a_keys, writes=(kb,))
            elif e == "pe":
                o = self.add(e, (lambda eh: eh.matmul(self.fence_ps[0:1, 1:2], lhsT=self.fence_w[0:1, 0:1], rhs=self.fence_w[0:1, 0:1], start=True, stop=True)),
                             reads=a_keys, writes=(kb, "ps7"))
            elif e == "sp":
                o = self.add(e, (lambda eh: eh.nop()), reads=a_keys, writes=(kb,))
            else:
                c = 4 if e == "dve" else 5
                o = self.add(e, (lambda eh, c=c: eh.memset(scratch[:, c:c + 1], 0.0)), reads=a_keys, writes=(kb,))
            for d in dmas:
                o.deps.append(d)

    def emit(self, final_wait_ops=()):
        nc = self.nc
        esem = {e: nc.alloc_semaphore("s_" + e) for e in self.COMPUTE}
        dsems = [nc.alloc_semaphore("s_dma%d" % i) for i in range(self.n_dma_sems)]
        dtotal = [0] * self.n_dma_sems
        dlast = [None] * self.n_dma_sems
        ecount = {e: 0 for e in self.COMPUTE}
        nd = 0
        nq = {"sp": 0, "pool": 0}
        half = self.n_dma_sems // 2
        for o in self.ops:
            if o.dma:
                qi = nq[o.eng]; nq[o.eng] += 1; nd += 1
                i = (qi % half) + (0 if o.eng == "sp" else half)
                prev = dlast[i]
                if prev is not None:
                    o.deps.append(prev)
                dtotal[i] += 16
                o.sigval = (dsems[i], dtotal[i], 1000 + i)
                dlast[i] = o
            elif o.signal:
                ecount[o.eng] += 1
                o.sigval = (esem[o.eng], ecount[o.eng], o.eng)
        known = {e: {} for e in self.h}
        nwaits = 0
        for o in self.ops:
            eh = self.h[o.eng]
            kn = known[o.eng]
            need = {}
            for p in o.deps:
                s, v, key = p.sigval
                if kn.get(key, 0) >= v:
                    continue
                if key not in need or need[key][1] < v:
                    need[key] = (s, v)
            items = list(need.items())
            fold = None
            if items and o.fold and (o.eng == "pe" or (o.eng in FOLD_ENG and not o.dma)):
                fold = items.pop()
            for key, (s, v) in items:
                eh.wait_ge(s, v)
                kn[key] = v
                nwaits += 1
            ins = o.fn(eh)
            if fold is not None:
                key, (s, v) = fold
                ins._wait_ge(s, v)
                kn[key] = v
            if o.dma:
                ins.then_inc(o.sigval[0], 16)
            elif o.signal:
                ins.then_inc(o.sigval[0], 1)
        eh = self.h["sp"]
        for o in final_wait_ops:
            s, v, key = o.sigval
            eh.wait_ge(s, v)
        self.stats = dict(n_ops=len(self.ops), n_waits=nwaits, counts=dict(ecount), n_dma=nd)
        return self.stats


class Arena:
    def __init__(self, nc, name, nbytes):
        self.t = nc.alloc_sbuf_tensor(name, [128, nbytes], U8)
        self.n = nbytes
        self.off = 0

    def reset(self, off=0):
        self.off = off

    def alloc(self, shape, dtype, parts=128):
        esz = 2 if dtype == BF16 else 4
        n = esz
        for s in shape:
            n *= s
        off = (self.off + 31) // 32 * 32
        assert off + n <= self.n, (off, n, self.n)
        self.off = off + n
        flat = self.t[0:parts, off:off + n].bitcast(dtype)
        if len(shape) == 1:
            return flat
        names = " ".join("a%d" % i for i in range(len(shape)))
        kw = {"a%d" % i: shape[i] for i in range(1, len(shape))}
        return flat.rearrange("p (%s) -> p %s" % (names, names), **kw)


def build_program(dbg=False):
    nc = bass.Bass("TRN2", target_bir_lowering=False)
    S = Sched(nc)
    D = {}

    def din(name, shape):
        D[name] = nc.dram_tensor(name, list(shape), F32, kind="ExternalInput").ap()
        return D[name]

    x = din("x", [NSEQ, SEQ, DM])
    w_in = din("w_in", [DM, DIN])
    b_in = din("b_in", [1, DIN])
    conv_w = din("conv_w", [4, 1024])
    conv_b = din("conv_b", [1, 1024])
    a_log = din("a_log", [1, 8])
    d_skip = din("d_skip", [1, 8])
    ssd_g = din("ssd_norm_g", [1, 512])
    w_out = din("w_out", [DM, DM])
    ln1_g = din("ln1_g", [1, DM]); ln1_b = din("ln1_b", [1, DM])
    rg_w = din("router_group_w", [DM, 4]); rg_b = din("router_group_b", [1, 4])
    re_w = din("router_expert_w", [4, DM, 4]); re_b = din("router_expert_b", [1, 16])
    w_gate = din("w_gate", [16, DM, 512]); w_up = din("w_up", [16, DM, 512]); w_down = din("w_down", [16, 512, DM])
    ln2_g = din("ln2_g", [1, DM]); ln2_b = din("ln2_b", [1, DM])
    out = nc.dram_tensor("out", [NSEQ, SEQ, DM], F32, kind="ExternalOutput").ap()
    h_scr = nc.dram_tensor("h_scr", [NSEQ, SEQ, DM], F32).ap()
    dbg_t = {}

    def dbg_out(name, shape):
        dbg_t[name] = nc.dram_tensor(name, list(shape), F32, kind="ExternalOutput").ap()
        return dbg_t[name]

    CONST = Arena(nc, "CONST", 22 * 1024)
    RW = Arena(nc, "RW", 49408)
    RX = Arena(nc, "RX", 32768)
    RACC = Arena(nc, "RACC", 65536)
    MSSD = Arena(nc, "MSSD", 16384)
    TT = Arena(nc, "TT", nc.sbuf_bytes_remaining - 256)

    identB = CONST.alloc([128], BF16); identF = CONST.alloc([128], F32)
    Uf = CONST.alloc([128], F32); triU = CONST.alloc([128], BF16); maskneg = CONST.alloc([128], BF16)
    onesF = CONST.alloc([128], F32)
    ones_row = CONST.alloc([128], BF16, parts=1)
    bz_row = CONST.alloc([512], BF16, parts=1); bv_row = CONST.alloc([512], BF16, parts=1)
    bdtf = CONST.alloc([16], F32)
    bxbc = CONST.alloc([8], F32); bq = CONST.alloc([4], F32); bk = CONST.alloc([4], F32)
    convw = CONST.alloc([8, 4], F32); convb = CONST.alloc([8], F32)
    a_bc = CONST.alloc([8], F32); dskip_bc = CONST.alloc([8], F32)
    gssd_bc = CONST.alloc([512], F32)
    lnG = CONST.alloc([1024], F32); lnB = CONST.alloc([1024], F32)
    rw = CONST.alloc([8, 20], F32); rb_bc = CONST.alloc([20], F32)
    logits = CONST.alloc([NT, 20], F32); comb = CONST.alloc([NT, 16], F32)
    fsc = CONST.alloc([16], F32)
    S.fence_w = CONST.alloc([8], BF16)
    pd = [nc.alloc_psum_tensor("pd%d" % i, [128, 1024], F32) for i in range(4)]
    def bank(i):
        return pd[i // 2][:, (i % 2) * 512:(i % 2) * 512 + 512]
    def bankb(i):
        return bank(i).bitcast(BF16)
    PSK = ["ps%d" % i for i in range(8)]
    S.fence_ps = nc.alloc_sbuf_tensor("fence_dummy", [1, 8], F32)
    S.fence_ps = bank(7)[:, 504:512]

    def fence():
        S.fence(fsc)

    S.pool(lambda e: e.memset(fsc[:, :], 0.0), writes=["fsc"])
    S.pool(lambda e: e.memset(S.fence_w[:, :], 0.0), writes=["fence_w"])
    S.pool(lambda e: e.memset(identB[:, :], 1.0), writes=["identB"])
    S.pool(lambda e: e.affine_select(out=identB[:, :], in_=identB[:, :], pattern=[[-1, 128]], compare_op=ALU.is_equal, fill=0.0, base=0, channel_multiplier=1), reads=["identB"], writes=["identB"])
    S.pool(lambda e: e.memset(identF[:, :], 1.0), writes=["identF"])
    S.pool(lambda e: e.affine_select(out=identF[:, :], in_=identF[:, :], pattern=[[-1, 128]], compare_op=ALU.is_equal, fill=0.0, base=0, channel_multiplier=1), reads=["identF"], writes=["identF"])
    S.pool(lambda e: e.memset(Uf[:, :], 1.0), writes=["Uf"])
    S.pool(lambda e: e.affine_select(out=Uf[:, :], in_=Uf[:, :], pattern=[[1, 128]], compare_op=ALU.is_ge, fill=0.0, base=0, channel_multiplier=-1), reads=["Uf"], writes=["Uf"])
    S.pool(lambda e: e.memset(triU[:, :], 1.0), writes=["triU"])
    S.pool(lambda e: e.affine_select(out=triU[:, :], in_=triU[:, :], pattern=[[1, 128]], compare_op=ALU.is_ge, fill=0.0, base=0, channel_multiplier=-1), reads=["triU"], writes=["triU"])
    S.pool(lambda e: e.memset(maskneg[:, :], NEG), writes=["maskneg"])
    S.pool(lambda e: e.affine_select(out=maskneg[:, :], in_=maskneg[:, :], pattern=[[-1, 128]], compare_op=ALU.is_gt, fill=0.0, base=0, channel_multiplier=1), reads=["maskneg"], writes=["maskneg"])
    S.pool(lambda e: e.memset(onesF[:, :], 1.0), writes=["onesF"])
    S.pool(lambda e: e.memset(ones_row[:, :], 1.0), writes=["ones_row"])
    S.dma("pool", lambda e: e.dma_start(out=bz_row[:, :], in_=b_in[0:1, 0:512]), writes=["bz_row"])
    S.dma("pool", lambda e: e.dma_start(out=bv_row[:, :], in_=b_in[0:1, 2568:3080]), writes=["bv_row"])
    S.dma("sp", lambda e: e.dma_start(out=bdtf[:, 0:8], in_=b_in[0:1, 1536:1544].to_broadcast([128, 8])), writes=["bdtf0"])
    S.dma("sp", lambda e: e.dma_start(out=bdtf[:, 8:16], in_=b_in[0:1, 3080:3088].to_broadcast([128, 8])), writes=["bdtf1"])
    S.dma("sp", lambda e: e.dma_start(out=bxbc[:, :], in_=b_in[0, 512:1536].rearrange("(c p) -> p c", p=128)), writes=["bxbc"])
    S.dma("sp", lambda e: e.dma_start(out=bq[:, :], in_=b_in[0, 1544:2056].rearrange("(c p) -> p c", p=128)), writes=["bq"])
    S.dma("sp", lambda e: e.dma_start(out=bk[:, :], in_=b_in[0, 2056:2568].rearrange("(c p) -> p c", p=128)), writes=["bk"])
    for k in range(4):
        S.dma("sp", lambda e, k=k: e.dma_start(out=convw[:, :, k], in_=conv_w[k, :].rearrange("(c p) -> p c", p=128)), writes=["convw%d" % k])
    CONVW = ["convw%d" % k for k in range(4)]
    S.dma("sp", lambda e: e.dma_start(out=convb[:, :], in_=conv_b[0, :].rearrange("(c p) -> p c", p=128)), writes=["convb"])
    S.dma("sp", lambda e: e.dma_start(out=a_bc[:, :], in_=a_log[0:1, :].to_broadcast([128, 8])), writes=["a_bc"])
    S.act(lambda e: e.activation(out=a_bc[:, :], in_=a_bc[:, :], func=AF.Exp), reads=["a_bc"], writes=["a_bc"])
    S.dve(lambda e: e.tensor_scalar(out=a_bc[:, :], in0=a_bc[:, :], scalar1=-1.0, scalar2=None, op0=ALU.mult), reads=["a_bc"], writes=["a_bc"])
    S.dma("sp", lambda e: e.dma_start(out=dskip_bc[:, :], in_=d_skip[0:1, :].to_broadcast([128, 8])), writes=["dskip_bc"])
    S.dma("sp", lambda e: e.dma_start(out=gssd_bc[:, :], in_=ssd_g[0:1, :].to_broadcast([128, 512])), writes=["gssd_bc"])
    S.dma("sp", lambda e: e.dma_start(out=rw[:, :, 0:4], in_=rg_w.rearrange("(k p) j -> p k j", p=128)), writes=["rw0"])
    for g in range(4):
        S.dma("sp", lambda e, g=g: e.dma_start(out=rw[:, :, 4 + 4 * g:8 + 4 * g], in_=re_w[g].rearrange("(k p) j -> p k j", p=128)), writes=["rw%d" % (g + 1)])
    RWK = ["rw%d" % i for i in range(5)]
    S.dma("sp", lambda e: e.dma_start(out=rb_bc[:, 0:4], in_=rg_b[0:1, :].to_broadcast([128, 4])), writes=["rb0"])
    S.dma("sp", lambda e: e.dma_start(out=rb_bc[:, 4:20], in_=re_b[0:1, :].to_broadcast([128, 16])), writes=["rb1"])

    outs = []

    for s in range(NSEQ):
        RW.reset(); RX.reset(); RACC.reset(); MSSD.reset(); TT.reset()
        wgu = [None, None]; wdn = [None, None]
        wgu[0] = RW.alloc([8, 1024], BF16); wdn[0] = RW.alloc([4, 1024], BF16)
        wgu[1] = RW.alloc([8, 1024], BF16); wdn[1] = RW.alloc([4, 1024], BF16)
        RW.reset()
        wA = RW.alloc([8, 1544], BF16)
        xb = [RW.alloc([4, 1024], BF16) for _ in range(2)]
        xT = RX.alloc([8, SEQ], BF16)
        sz = RACC.alloc([NT, 512], BF16)
        xsB = RACC.alloc([NT, 768], BF16)
        BT = RACC.alloc([2, SEQ], BF16)
        CT = RACC.alloc([2, SEQ], BF16)
        xsT = MSSD.alloc([4, SEQ], BF16)
        dt_t = TT.alloc([NT, 8], F32)
        tt_mark = TT.off
        pre = [TT.alloc([SEQ + 3], BF16) for _ in range(2)]
        diagw = TT.alloc([8, 4, 128], BF16)
        dt_raw = TT.alloc([NT, 8], F32)

        w_in_v = w_in.rearrange("(k p) c -> p k c", p=128)
        S.dma("pool", lambda e, s=s: e.dma_start(out=xb[0][:, :, :], in_=x[s, 0:512, :].rearrange("(t p) d -> p t d", p=128)), writes=["xb0"])
        S.dma("pool", lambda e: e.dma_start(out=wA[:, :, 0:512], in_=w_in_v[:, :, 0:512]), writes=["wA.z"])
        S.dma("pool", lambda e: e.dma_start(out=wA[:, :, 1536:1544], in_=w_in_v[:, :, 1536:1544]), writes=["wA.dt"])
        S.dma("pool", lambda e, s=s: e.dma_start(out=xb[1][:, :, :], in_=x[s, 512:1024, :].rearrange("(t p) d -> p t d", p=128)), writes=["xb1"])
        for half in range(2):
            S.dma("pool", lambda e, half=half: e.dma_start(out=wA[:, 4 * half:4 * half + 4, 512:1536], in_=w_in_v[:, 4 * half:4 * half + 4, 512:1536]), writes=["wA.x%d" % half])
        for b in range(2):
            S.pool(lambda e, b=b: e.memset(pre[b][:, 0:3], 0.0), writes=["prepad%d" % b])
        ev = 0
        for blk in range(4):
            if blk >= 2:
                S.dma("pool", lambda e, blk=blk, s=s: e.dma_start(out=xb[blk % 2][:, :, :], in_=x[s, blk * 512:(blk + 1) * 512, :].rearrange("(t p) d -> p t d", p=128)),
                      writes=["xb%d" % (blk % 2)])
            for k in range(8):
                pb = (blk * 8 + k) % 2
                for t in range(4):
                    S.pe(lambda e, pb=pb, t=t, k=k, blk=blk: e.transpose(bankb(pb)[:, t * 128:(t + 1) * 128], xb[blk % 2][:, t, k * 128:(k + 1) * 128], identB[:, :]),
                         reads=["xb%d" % (blk % 2), "identB"], writes=[PSK[pb]])
                if ev % 2 == 0:
                    S.act(lambda e, pb=pb, k=k, blk=blk: e.copy(xT[:, k, blk * 512:(blk + 1) * 512], bankb(pb)[:, 0:512]), reads=[PSK[pb]], writes=["xT.%d.%d" % (k, blk)])
                else:
                    S.dve(lambda e, pb=pb, k=k, blk=blk: e.tensor_copy(xT[:, k, blk * 512:(blk + 1) * 512], bankb(pb)[:, 0:512]), reads=[PSK[pb]], writes=["xT.%d.%d" % (k, blk)])
                ev += 1
            for tt in range(4):
                t = blk * 4 + tt
                pz = 2 + (t % 2)
                xk = ["xT.%d.%d" % (k, blk) for k in range(8)]
                for k in range(8):
                    S.pe(lambda e, pz=pz, t=t, k=k: e.matmul(bank(pz)[:, :], lhsT=xT[:, k, t * 128:(t + 1) * 128], rhs=wA[:, k, 0:512], start=(k == 0), stop=False),
                         reads=[xk[k], "wA.z"], writes=[PSK[pz]])
                S.pe(lambda e, pz=pz: e.matmul(bank(pz)[:, :], lhsT=ones_row[0:1, :], rhs=bz_row[0:1, :], start=False, stop=True),
                     reads=["ones_row", "bz_row"], writes=[PSK[pz]])
                S.act(lambda e, pz=pz, t=t: e.activation(out=sz[:, t, :], in_=bank(pz)[:, :], func=AF.Silu), reads=[PSK[pz]], writes=["sz.%d" % t])
                for k in range(8):
                    S.pe(lambda e, t=t, k=k: e.matmul(bank(4)[:, t * 8:(t + 1) * 8], lhsT=xT[:, k, t * 128:(t + 1) * 128], rhs=wA[:, k, 1536:1544], start=(k == 0), stop=(k == 7)),
                         reads=[xk[k], "wA.dt"], writes=[PSK[4]])
        S.dve(lambda e: e.tensor_tensor(out=dt_raw[:, :, :], in0=bank(4)[:, 0:128].rearrange("p (t h) -> p t h", h=8), in1=bdtf[:, 0:8].unsqueeze(1).to_broadcast([128, NT, 8]), op=ALU.add),
              reads=[PSK[4], "bdtf0"], writes=["dt_raw"])
        for c in range(8):
            for k in range(4):
                S.dve(lambda e, c=c, k=k: e.tensor_scalar(out=diagw[:, c, k, :], in0=identB[:, :], scalar1=convw[:, c, k:k + 1], scalar2=None, op0=ALU.mult),
                      reads=["identB"] + CONVW, writes=["diagw.%d" % c])

        def a1_ip(c):
            pb_ = c % 2
            for blk in range(4):
                pc = 5 + (c * 4 + blk) % 2
                for k in range(8):
                    S.pe(lambda e, pc=pc, c=c, k=k, blk=blk: e.matmul(bank(pc)[:, :], lhsT=wA[:, k, 512 + c * 128:512 + (c + 1) * 128], rhs=xT[:, k, blk * 512:(blk + 1) * 512], start=(k == 0), stop=(k == 7)),
                         reads=["xT.%d.%d" % (k, blk), "wA.x%d" % (k // 4)], writes=[PSK[pc]])
                S.act(lambda e, pc=pc, c=c, blk=blk, pb_=pb_: e.activation(out=pre[pb_][:, 3 + blk * 512:3 + (blk + 1) * 512], in_=bank(pc)[:, :], func=AF.Identity, bias=bxbc[:, c:c + 1], scale=1.0),
                      reads=[PSK[pc], "bxbc"], writes=["pre%d.%d" % (pb_, blk)])

        def a1_conv(c):
            pb_ = c % 2
            if c < 4:
                dstt = xsT[:, c, :]; dk = "xsT.%d" % c
            elif c < 6:
                dstt = BT[:, c - 4, :]; dk = "BT.%d" % (c - 4)
            else:
                dstt = CT[:, c - 6, :]; dk = "CT.%d" % (c - 6)
            for blk in range(4):
                pc = 2 + (c * 4 + blk) % 2
                rk = ["pre%d.%d" % (pb_, blk), "diagw.%d" % c] + (["pre%d.%d" % (pb_, blk - 1)] if blk > 0 else ["prepad%d" % pb_])
                for k in range(4):
                    S.pe(lambda e, pc=pc, c=c, k=k, blk=blk, pb_=pb_: e.matmul(bank(pc)[:, :], lhsT=diagw[:, c, k, :], rhs=pre[pb_][:, blk * 512 + k:blk * 512 + k + 512], start=(k == 0), stop=(k == 3)),
                         reads=rk, writes=[PSK[pc]])
                S.act(lambda e, pc=pc, dstt=dstt, c=c, blk=blk: e.activation(out=dstt[:, blk * 512:(blk + 1) * 512], in_=bank(pc)[:, :], func=AF.Silu, bias=convb[:, c:c + 1], scale=1.0),
                      reads=[PSK[pc], "convb"], writes=[dk])

        a1_ip(0)
        for c in range(8):
            if c + 1 < 8:
                a1_ip(c + 1)
            a1_conv(c)
        for t in range(NT):
            pb = t % 2
            for c in range(6):
                src = xsT[:, c, t * 128:(t + 1) * 128] if c < 4 else BT[:, c - 4, t * 128:(t + 1) * 128]
                sk = "xsT.%d" % c if c < 4 else "BT.%d" % (c - 4)
                S.pe(lambda e, pb=pb, c=c, src=src: e.transpose(bankb(pb)[:, c * 128:(c + 1) * 128], src, identB[:, :]), reads=[sk, "identB"], writes=[PSK[pb]])
            if t % 2 == 0:
                S.dve(lambda e, pb=pb, t=t: e.tensor_copy(xsB[:, t, :], bankb(pb)[:, 0:768]), reads=[PSK[pb]], writes=["xsB.%d" % t])
            else:
                S.act(lambda e, pb=pb, t=t: e.copy(xsB[:, t, :], bankb(pb)[:, 0:768]), reads=[PSK[pb]], writes=["xsB.%d" % t])
        S.act(lambda e: e.activation(out=dt_t[:, :, :], in_=dt_raw[:, :, :], func=AF.Exp), reads=["dt_raw"], writes=["dt_t"])
        S.act(lambda e: e.activation(out=dt_t[:, :, :], in_=dt_t[:, :, :], func=AF.Ln, bias=1.0, scale=1.0), reads=["dt_t"], writes=["dt_t"])

        fence()
        MSSD.reset(); TT.reset(tt_mark)
        RW.reset()
        wB = RW.alloc([8, 1544], BF16)
        wo = RW.alloc([8, 1024], BF16)
        for half in range(2):
            S.dma("pool", lambda e, half=half: e.dma_start(out=wB[:, 4 * half:4 * half + 4, :], in_=w_in.rearrange("(k p) c -> p k c", p=128)[:, 4 * half:4 * half + 4, 1544:3088]),
                  writes=["wB"], nofence=True)
        for half in range(2):
            S.dma("pool", lambda e, half=half: e.dma_start(out=wo[:, 4 * half:4 * half + 4, :], in_=w_out.rearrange("(k p) c -> p k c", p=128)[:, 4 * half:4 * half + 4, :]),
                  writes=["wo"], nofence=True)
        m_ssd = MSSD.alloc([NT, 512], BF16)
        da = TT.alloc([NT, 8], F32); acum = TT.alloc([NT, 8], F32); nacum = TT.alloc([NT, 8], F32)
        alast = TT.alloc([NT, 8], F32); dte = TT.alloc([NT, 8], F32); ea = TT.alloc([NT, 8], F32)
        cdec = TT.alloc([NT, 8], F32); dtdte = TT.alloc([NT, 8], F32)
        stT = TT.alloc([8, 64], F32); stTb = TT.alloc([8, 64], BF16)
        LT = [TT.alloc([128], BF16) for _ in range(4)]
        MT = [TT.alloc([128], BF16) for _ in range(4)]
        xdt = [TT.alloc([8, 64], BF16) for _ in range(2)]
        xdtd = [TT.alloc([8, 64], BF16) for _ in range(2)]
        t1 = [TT.alloc([8, 64], F32)] * 2
        t2 = [TT.alloc([8, 64], F32) for _ in range(2)]
        yg = [TT.alloc([512], F32) for _ in range(2)]
        junk = TT.alloc([256], F32)
        ss = [TT.alloc([2], F32) for _ in range(2)]
        rstd = [TT.alloc([2], F32) for _ in range(2)]

        S.dve(lambda e: e.tensor_tensor(out=da[:, :, :], in0=dt_t[:, :, :], in1=a_bc[:, :].unsqueeze(1).to_broadcast([128, NT, 8]), op=ALU.mult), reads=["dt_t", "a_bc"], writes=["da"])
        daf = da.rearrange("p t h -> p (t h)")
        S.pe(lambda e: e.matmul(bank(5)[:, 0:128], lhsT=Uf[:, :], rhs=daf, start=True, stop=True), reads=["Uf", "da"], writes=[PSK[5]])
        S.pe(lambda e: e.matmul(bank(6)[:, 0:128], lhsT=onesF[:, :], rhs=daf, start=True, stop=True), reads=["onesF", "da"], writes=[PSK[6]])
        S.dve(lambda e: e.tensor_copy(acum.rearrange("p t h -> p (t h)"), bank(5)[:, 0:128]), reads=[PSK[5]], writes=["acum"])
        S.dve(lambda e: e.tensor_scalar(out=nacum.rearrange("p t h -> p (t h)"), in0=bank(5)[:, 0:128], scalar1=-1.0, scalar2=None, op0=ALU.mult), reads=[PSK[5]], writes=["nacum"])
        S.dve(lambda e: e.tensor_copy(alast.rearrange("p t h -> p (t h)"), bank(6)[:, 0:128]), reads=[PSK[6]], writes=["alast"])
        S.dve(lambda e: e.tensor_tensor(out=dte[:, :, :], in0=alast[:, :, :], in1=acum[:, :, :], op=ALU.subtract), reads=["alast", "acum"], writes=["dte"])
        S.act(lambda e: e.activation(out=dte[:, :, :], in_=dte[:, :, :], func=AF.Exp), reads=["dte"], writes=["dte"])
        S.act(lambda e: e.activation(out=ea[:, :, :], in_=acum[:, :, :], func=AF.Exp), reads=["acum"], writes=["ea"])
        S.act(lambda e: e.activation(out=cdec[:, :, :], in_=alast[:, :, :], func=AF.Exp), reads=["alast"], writes=["cdec"])
        S.dve(lambda e: e.tensor_tensor(out=dtdte[:, :, :], in0=dt_t[:, :, :], in1=dte[:, :, :], op=ALU.mult), reads=["dt_t", "dte"], writes=["dtdte"])
        S.pool(lambda e: e.memset(stT[:, :, :], 0.0), writes=["stT"])
        S.pool(lambda e: e.memset(stTb[:, :, :], 0.0), writes=["stTb"])

        def ssd_F(c):
            cs = slice(c * 128, (c + 1) * 128)
            b2 = c % 2
            py = 2 + b2
            xs_c = xsB[:, c, 0:512].rearrange("p (h d) -> p h d", h=8)
            S.pool(lambda e, c=c, b2=b2, xs_c=xs_c: e.tensor_tensor(out=xdt[b2][:, :, :], in0=xs_c, in1=dt_t[:, c, :].unsqueeze(2).to_broadcast([128, 8, 64]), op=ALU.mult),
                   reads=["xsB.%d" % c, "dt_t"], writes=["xdt%d" % b2])
            S.pool(lambda e, c=c, b2=b2, xs_c=xs_c: e.tensor_tensor(out=xdtd[b2][:, :, :], in0=xs_c, in1=dtdte[:, c, :].unsqueeze(2).to_broadcast([128, 8, 64]), op=ALU.mult),
                   reads=["xsB.%d" % c, "dtdte"], writes=["xdtd%d" % b2])
            S.pool(lambda e, c=c, b2=b2, xs_c=xs_c: e.tensor_tensor(out=t2[b2][:, :, :], in0=xs_c, in1=dskip_bc[:, :].unsqueeze(2).to_broadcast([128, 8, 64]), op=ALU.mult),
                   reads=["xsB.%d" % c, "dskip_bc"], writes=["t2%d" % b2])
            for g in range(2):
                S.pe(lambda e, g=g, cs=cs: e.matmul(bank(4)[:, g * 128:(g + 1) * 128], lhsT=BT[:, g, cs], rhs=CT[:, g, cs], start=True, stop=True),
                     reads=["BT.%d" % g, "CT.%d" % g], writes=[PSK[4]])
            for hh in range(2):
                pl = hh
                for h4 in range(4):
                    h = hh * 4 + h4
                    S.pe(lambda e, pl=pl, h4=h4, c=c, h=h: e.matmul(bank(pl)[:, h4 * 128:(h4 + 1) * 128], lhsT=da[:, c, h:h + 1].to_broadcast([128, 128]), rhs=Uf[:, :], start=True, stop=False),
                         reads=["da", "Uf"], writes=[PSK[pl]])
                    S.pe(lambda e, pl=pl, h4=h4: e.matmul(bank(pl)[:, h4 * 128:(h4 + 1) * 128], lhsT=identB[:, :], rhs=maskneg[:, :], start=False, stop=True),
                         reads=["identB", "maskneg"], writes=[PSK[pl]])
            for hh in range(2):
                pl = hh
                for h4 in range(4):
                    h = hh * 4 + h4
                    S.act(lambda e, pl=pl, h4=h4, c=c, h=h: e.activation(out=LT[h4][:, :], in_=bank(pl)[:, h4 * 128:(h4 + 1) * 128], func=AF.Exp, bias=nacum[:, c, h:h + 1], scale=1.0),
                          reads=[PSK[pl], "nacum"], writes=["LT%d" % h4])
                    g = h // 4
                    S.dve(lambda e, h4=h4, g=g: e.tensor_tensor(out=MT[h4][:, :], in0=bank(4)[:, g * 128:(g + 1) * 128], in1=LT[h4][:, :], op=ALU.mult),
                          reads=[PSK[4], "LT%d" % h4], writes=["MT%d" % h4])
                    S.pe(lambda e, h4=h4, h=h, b2=b2, py=py: e.matmul(bank(py)[:, h * 64:(h + 1) * 64], lhsT=MT[h4][:, :], rhs=xdt[b2][:, h, :], start=True, stop=True),
                         reads=["MT%d" % h4, "xdt%d" % b2], writes=[PSK[py]])

        def ssd_B(c):
            cs = slice(c * 128, (c + 1) * 128)
            b2 = c % 2
            py = 2 + b2
            if c > 0:
                for g in range(2):
                    S.pe(lambda e, g=g, cs=cs: e.matmul(bank(6)[:, g * 256:(g + 1) * 256], lhsT=CT[:, g, cs], rhs=stTb[:, 4 * g:4 * g + 4, :].rearrange("p h d -> p (h d)"), start=True, stop=True),
                         reads=["CT.%d" % g, "stTb"], writes=[PSK[6]])
            if c < NT - 1:
                for g in range(2):
                    S.pe(lambda e, g=g, c=c, b2=b2: e.matmul(bank(7)[:, g * 256:(g + 1) * 256], lhsT=xsB[:, c, 512 + g * 128:512 + (g + 1) * 128], rhs=xdtd[b2][:, 4 * g:4 * g + 4, :].rearrange("p h d -> p (h d)"), start=True, stop=True),
                         reads=["xsB.%d" % c, "xdtd%d" % b2], writes=[PSK[7]])
                S.dve(lambda e, c=c: e.tensor_tensor(out=stT[:, :, :], in0=stT[:, :, :], in1=cdec[:, c, :].unsqueeze(2).to_broadcast([128, 8, 64]), op=ALU.mult),
                      reads=["stT", "cdec"], writes=["stT"])
                S.dve(lambda e: e.tensor_tensor(out=stT[:, :, :], in0=bank(7)[:, 0:512].rearrange("p (h d) -> p h d", h=8), in1=stT[:, :, :], op=ALU.add),
                      reads=[PSK[7], "stT"], writes=["stT"])
            if c > 0:
                S.dve(lambda e, c=c, b2=b2: e.tensor_tensor(out=t1[b2][:, :, :], in0=bank(6)[:, :].rearrange("p (h d) -> p h d", h=8), in1=ea[:, c, :].unsqueeze(2).to_broadcast([128, 8, 64]), op=ALU.mult),
                      reads=[PSK[6], "ea"], writes=["t1"])
            if c < NT - 1:
                S.act(lambda e: e.copy(stTb[:, :, :], stT[:, :, :]), reads=["stT"], writes=["stTb"])
            if c > 0:
                S.dve(lambda e, b2=b2, py=py: e.tensor_tensor(out=t1[b2][:, :, :], in0=bank(py)[:, :].rearrange("p (h d) -> p h d", h=8), in1=t1[b2][:, :, :], op=ALU.add),
                      reads=[PSK[py], "t1"], writes=["t1"])
                S.dve(lambda e, b2=b2: e.tensor_tensor(out=t1[b2][:, :, :], in0=t1[b2][:, :, :], in1=t2[b2][:, :, :], op=ALU.add),
                      reads=["t1", "t2%d" % b2], writes=["t1"])
            else:
                S.dve(lambda e, b2=b2, py=py: e.tensor_tensor(out=t1[b2][:, :, :], in0=bank(py)[:, :].rearrange("p (h d) -> p h d", h=8), in1=t2[b2][:, :, :], op=ALU.add),
                      reads=[PSK[py], "t2%d" % b2], writes=["t1"])
            S.dve(lambda e, c=c, b2=b2: e.tensor_tensor(out=yg[b2][:, :], in0=t1[b2].rearrange("p h d -> p (h d)"), in1=sz[:, c, :], op=ALU.mult),
                  reads=["t1", "sz.%d" % c], writes=["yg%d" % b2])
            for g in range(2):
                S.act(lambda e, g=g, b2=b2: e.activation(out=junk[:, :], in_=yg[b2][:, g * 256:(g + 1) * 256], func=AF.Square, accum_out=ss[b2][:, g:g + 1]),
                      reads=["yg%d" % b2], writes=["junk", "ss%d.%d" % (b2, g)])
            S.act(lambda e, b2=b2: e.activation(out=rstd[b2][:, :], in_=ss[b2][:, :], func=AF.Ln, bias=RMS_EPS, scale=1.0 / 256.0),
                  reads=["ss%d.0" % b2, "ss%d.1" % b2], writes=["rstd%d" % b2])
            S.act(lambda e, b2=b2: e.activation(out=rstd[b2][:, :], in_=rstd[b2][:, :], func=AF.Exp, scale=-0.5), reads=["rstd%d" % b2], writes=["rstd%d" % b2])
            for g in range(2):
                S.dve(lambda e, g=g, b2=b2, c=c: e.scalar_tensor_tensor(out=m_ssd[:, c, g * 256:(g + 1) * 256], in0=yg[b2][:, g * 256:(g + 1) * 256], scalar=rstd[b2][:, g:g + 1], in1=gssd_bc[:, g * 256:(g + 1) * 256], op0=ALU.mult, op1=ALU.mult),
                      reads=["yg%d" % b2, "rstd%d" % b2, "gssd_bc"], writes=["m_ssd.%d" % c])

        ssd_F(0)
        for c in range(NT):
            if c + 1 < NT:
                ssd_F(c + 1)
            ssd_B(c)

        if dbg == 'ssd' and s == 0:
            RX.reset()
            dm = dbg_out("dbg_mssd", [128, NT * 512])
            cvt = RX.alloc([NT * 512], F32) if dbg == 'ssd' else None
            S.dve(lambda e: e.tensor_copy(cvt[:, :], m_ssd.rearrange("p t d -> p (t d)")), reads=["m_ssd.%d" % c for c in range(NT)], writes=["cvt"])
            outs.append(S.dma("sp", lambda e: e.dma_start(out=dm[:, :], in_=cvt[:, :]), reads=["cvt"]))
        if dbg == "ssd":
            break

        fence()
        RW.reset(); RACC.reset(); TT.reset()
        wB = RW.alloc([8, 1544], BF16)
        wo = RW.alloc([8, 1024], BF16)
        ah = RW.alloc([1024], F32)
        hTf = RW.alloc([8, 128], F32)
        qT = RACC.alloc([4, SEQ], BF16)
        kT = RACC.alloc([4, SEQ], BF16)
        v_aug = RACC.alloc([NT, 8, 65], BF16)
        xres = [RACC.alloc([1024], F32) for _ in range(2)]
        hpre = [RACC.alloc([1024], F32), TT.alloc([1024], F32)]
        f_raw = TT.alloc([NT, 8], F32); Gc = TT.alloc([NT, 8], F32); tot = TT.alloc([NT, 8], F32)
        Pp = TT.alloc([NT, 8], F32); Gf = TT.alloc([NT, 8], F32); Gend = TT.alloc([NT, 8], F32)
        biasT = TT.alloc([8, NT, NT], F32)
        PT = [TT.alloc([8, 128], BF16) for _ in range(2)]
        yatt = [TT.alloc([8, 64], BF16) for _ in range(2)]
        rden = [TT.alloc([8], F32) for _ in range(2)]
        mT = [TT.alloc([8, 128], BF16)] * 2
        bst = [TT.alloc([2, 6], F32) for _ in range(2)]
        mv = [TT.alloc([2], F32) for _ in range(2)]
        rs1 = [TT.alloc([1], F32) for _ in range(2)]

        S.dma("sp", lambda e: e.dma_start(out=lnG[:, :], in_=ln1_g[0:1, :].to_broadcast([128, 1024])), writes=["lnG"])
        S.dma("sp", lambda e: e.dma_start(out=lnB[:, :], in_=ln1_b[0:1, :].to_broadcast([128, 1024])), writes=["lnB"])
        S.pool(lambda e: e.memset(v_aug[:, :, :, 64:65], 1.0), writes=["v_ones"])
        evq = 0
        for qk in range(2):
            dstT = qT if qk == 0 else kT
            bcol = bq if qk == 0 else bk
            nm = "qT" if qk == 0 else "kT"
            for p in range(4):
                for blk in range(4):
                    pq = evq % 4
                    for k in range(8):
                        S.pe(lambda e, pq=pq, k=k, p=p, blk=blk, qk=qk: e.matmul(bank(pq)[:, :], lhsT=wB[:, k, qk * 512 + p * 128:qk * 512 + (p + 1) * 128], rhs=xT[:, k, blk * 512:(blk + 1) * 512], start=(k == 0), stop=(k == 7)),
                             reads=["xT.%d.%d" % (k, blk), "wB"], writes=[PSK[pq]])
                    if evq % 2 == 0:
                        S.act(lambda e, pq=pq, p=p, blk=blk, dstT=dstT, bcol=bcol: e.activation(out=dstT[:, p, blk * 512:(blk + 1) * 512], in_=bank(pq)[:, :], func=AF.Identity, bias=bcol[:, p:p + 1], scale=1.0),
                              reads=[PSK[pq], "bq", "bk"], writes=["%s.%d.%d" % (nm, p, blk)])
                    else:
                        S.dve(lambda e, pq=pq, p=p, blk=blk, dstT=dstT, bcol=bcol: e.tensor_scalar(out=dstT[:, p, blk * 512:(blk + 1) * 512], in0=bank(pq)[:, :], scalar1=bcol[:, p:p + 1], scalar2=None, op0=ALU.add),
                              reads=[PSK[pq], "bq", "bk"], writes=["%s.%d.%d" % (nm, p, blk)])
                    evq += 1
        for t in range(NT):
            pv = 4 + (t % 2)
            blk = t // 4
            for k in range(8):
                S.pe(lambda e, pv=pv, t=t, k=k: e.matmul(bank(pv)[:, :], lhsT=xT[:, k, t * 128:(t + 1) * 128], rhs=wB[:, k, 1024:1536], start=(k == 0), stop=False),
                     reads=["xT.%d.%d" % (k, blk), "wB"], writes=[PSK[pv]])
            S.pe(lambda e, pv=pv: e.matmul(bank(pv)[:, :], lhsT=ones_row[0:1, :], rhs=bv_row[0:1, :], start=False, stop=True),
                 reads=["ones_row", "bv_row"], writes=[PSK[pv]])
            if t % 2 == 0:
                S.act(lambda e, pv=pv, t=t: e.copy(v_aug[:, t, :, 0:64], bank(pv)[:, :].rearrange("p (h d) -> p h d", h=8)), reads=[PSK[pv]], writes=["v.%d" % t])
            else:
                S.dve(lambda e, pv=pv, t=t: e.tensor_copy(v_aug[:, t, :, 0:64], bank(pv)[:, :].rearrange("p (h d) -> p h d", h=8)), reads=[PSK[pv]], writes=["v.%d" % t])
            for k in range(8):
                S.pe(lambda e, t=t, k=k: e.matmul(bank(6)[:, t * 8:(t + 1) * 8], lhsT=xT[:, k, t * 128:(t + 1) * 128], rhs=wB[:, k, 1536:1544], start=(k == 0), stop=(k == 7)),
                     reads=["xT.%d.%d" % (k, blk), "wB"], writes=[PSK[6]])
        S.dve(lambda e: e.tensor_tensor(out=f_raw[:, :, :], in0=bank(6)[:, 0:128].rearrange("p (t h) -> p t h", h=8), in1=bdtf[:, 8:16].unsqueeze(1).to_broadcast([128, NT, 8]), op=ALU.add),
              reads=[PSK[6], "bdtf1"], writes=["f_raw"])
        S.act(lambda e: e.activation(out=f_raw[:, :, :], in_=f_raw[:, :, :], func=AF.Exp, scale=-1.0), reads=["f_raw"], writes=["f_raw"])
        S.act(lambda e: e.activation(out=f_raw[:, :, :], in_=f_raw[:, :, :], func=AF.Ln, bias=1.0, scale=1.0), reads=["f_raw"], writes=["f_raw"])
        frf = f_raw.rearrange("p t h -> p (t h)")
        S.pe(lambda e: e.matmul(bank(7)[:, 0:128], lhsT=Uf[:, :], rhs=frf, start=True, stop=True), reads=["Uf", "f_raw"], writes=[PSK[7]])
        S.pe(lambda e: e.matmul(bank(7)[:, 128:256], lhsT=onesF[:, :], rhs=frf, start=True, stop=True), reads=["onesF", "f_raw"], writes=[PSK[7]])
        S.dve(lambda e: e.tensor_copy(Gc.rearrange("p t h -> p (t h)"), bank(7)[:, 0:128]), reads=[PSK[7]], writes=["Gc"])
        S.dve(lambda e: e.tensor_copy(tot.rearrange("p t h -> p (t h)"), bank(7)[:, 128:256]), reads=[PSK[7]], writes=["tot"])
        S.pool(lambda e: e.memset(Pp[:, 0, :], 0.0), writes=["Pp"])
        for t in range(1, NT):
            S.dve(lambda e, t=t: e.tensor_tensor(out=Pp[:, t, :], in0=Pp[:, t - 1, :], in1=tot[:, t - 1, :], op=ALU.add), reads=["Pp", "tot"], writes=["Pp"])
        S.dve(lambda e: e.tensor_tensor(out=Gf[:, :, :], in0=Gc[:, :, :], in1=Pp[:, :, :], op=ALU.add), reads=["Gc", "Pp"], writes=["Gf"])
        S.dve(lambda e: e.tensor_tensor(out=Gend[:, :, :], in0=tot[:, :, :], in1=Pp[:, :, :], op=ALU.add), reads=["tot", "Pp"], writes=["Gend"])
        for h in range(8):
            S.dve(lambda e, h=h: e.tensor_tensor(out=biasT[:, h, :, :], in0=Gf[:, :, h].unsqueeze(1).to_broadcast([128, NT, NT]), in1=Gend[:, :, h].unsqueeze(2).to_broadcast([128, NT, NT]), op=ALU.subtract),
                  reads=["Gf", "Gend"], writes=["biasT"])

        fence()
        RX.reset()
        hT = RX.alloc([8, SEQ], BF16)
        NEXP = int(os.environ.get("KNEXP", 16))

        def load_expert(ex):
            sl = ex % 2
            S.dma("pool", lambda e, ex=ex, sl=sl: e.dma_start(out=wgu[sl][:, :, 0:512], in_=w_gate[ex].rearrange("(k p) f -> p k f", p=128)), writes=["wg.%d" % sl], nofence=(ex == 0))
            S.dma("pool", lambda e, ex=ex, sl=sl: e.dma_start(out=wgu[sl][:, :, 512:1024], in_=w_up[ex].rearrange("(k p) f -> p k f", p=128)), writes=["wu.%d" % sl], nofence=(ex == 0))
            S.dma("pool", lambda e, ex=ex, sl=sl: e.dma_start(out=wdn[sl][:, :, :], in_=w_down[ex].rearrange("(k p) d -> p k d", p=128)), writes=["wd.%d" % sl], nofence=(ex == 0))


        load_expert(0)
        NTI = int(os.environ.get("KATT_TILES", NT))
        S.fold_now = os.environ.get("KFOLD", "all") in ("attmoe", "all")
        units = []
        for i in range(NTI):
            for p in range(4):
                for j0 in range(0, i + 1, 4):
                    units.append((i, p, list(range(j0, min(j0 + 4, i + 1)))))

        def emit_S(u, gi):
            i, p, js = u
            pb0 = 2 * (gi % 2)
            for jj, j in enumerate(js):
                for hh in range(2):
                    r0 = hh * 64
                    S.pe(lambda e, bk=pb0 + hh, jj=jj, j=j, p=p, r0=r0, i=i: e.matmul(bank(bk)[:, jj * 128:(jj + 1) * 128], lhsT=kT[r0:r0 + 64, p, j * 128:(j + 1) * 128], rhs=qT[r0:r0 + 64, p, i * 128:(i + 1) * 128], start=True, stop=True),
                         reads=["kT.%d.%d" % (p, j // 4), "qT.%d.%d" % (p, i // 4)], writes=[PSK[pb0 + hh]])

        def emit_E(u, gi):
            i, p, js = u
            pb0 = 2 * (gi % 2); pb = gi % 2
            for jj, j in enumerate(js):
                for hh in range(2):
                    h = 2 * p + hh
                    c8 = jj * 2 + hh
                    S.act(lambda e, bk=pb0 + hh, pb=pb, c8=c8, jj=jj, j=j, h=h, i=i: e.activation(out=PT[pb][:, c8, :], in_=bank(bk)[:, jj * 128:(jj + 1) * 128], func=AF.Exp, bias=biasT[:, h, i, j:j + 1], scale=ATT_SCALE),
                          reads=[PSK[pb0 + hh], "biasT"], writes=["PT%d.%d" % (pb, c8)])
                    if j == i:
                        S.dve(lambda e, pb=pb, c8=c8: e.tensor_tensor(out=PT[pb][:, c8, :], in0=PT[pb][:, c8, :], in1=triU[:, :], op=ALU.mult),
                              reads=["PT%d.%d" % (pb, c8), "triU"], writes=["PT%d.%d" % (pb, c8)])

        def emit_V(u, gi):
            i, p, js = u
            pb = gi % 2
            for jj, j in enumerate(js):
                for hh in range(2):
                    h = 2 * p + hh
                    c8 = jj * 2 + hh
                    po = 4 + h // 4; oc = (h % 4) * 65
                    S.pe(lambda e, pb=pb, c8=c8, j=j, h=h, po=po, oc=oc, i=i: e.matmul(bank(po)[:, oc:oc + 65], lhsT=PT[pb][:, c8, :], rhs=v_aug[:, j, h, :], start=(j == 0 and h % 4 == 0), stop=(j == i and h % 4 == 3)),
                         reads=["PT%d.%d" % (pb, c8), "v.%d" % j, "v_ones"], writes=[PSK[po]])

        def tail_N(i):
            b2 = i % 2
            S.dma("sp", lambda e, i=i, b2=b2, s=s: e.dma_start(out=xres[b2][:, :], in_=x[s, i * 128:(i + 1) * 128, :]), writes=["xres%d" % b2])
            for hb in range(2):
                ov = bank(4 + hb)[:, 0:260].rearrange("p (h d) -> p h d", h=4)
                S.dve(lambda e, hb=hb, ov=ov, b2=b2: e.reciprocal(rden[b2][:, 4 * hb:4 * hb + 4], ov[:, :, 64]), reads=[PSK[4 + hb]], writes=["rden%d.%d" % (b2, hb)])
                S.dve(lambda e, hb=hb, ov=ov, b2=b2: e.tensor_tensor(out=yatt[b2][:, 4 * hb:4 * hb + 4, :], in0=ov[:, :, 0:64], in1=rden[b2][:, 4 * hb:4 * hb + 4].unsqueeze(2).to_broadcast([128, 4, 64]), op=ALU.mult),
                      reads=[PSK[4 + hb], "rden%d.%d" % (b2, hb)], writes=["yatt%d.%d" % (b2, hb)])

        def tail_T(i):
            b2 = i % 2
            yf = yatt[b2].rearrange("p h d -> p (h d)")
            for ec in range(8):
                src = m_ssd[:, i, ec * 128:(ec + 1) * 128] if ec < 4 else yf[:, (ec - 4) * 128:(ec - 3) * 128]
                rk = ["m_ssd.%d" % i] if ec < 4 else ["yatt%d.%d" % (b2, (ec - 4) // 2)]
                S.pe(lambda e, ec=ec, src=src: e.transpose(bankb(6)[:, ec * 128:(ec + 1) * 128], src, identB[:, :]), reads=rk + ["identB"], writes=[PSK[6]])
            S.dve(lambda e, b2=b2: e.tensor_copy(mT[b2].rearrange("p a b -> p (a b)"), bankb(6)[:, 0:1024]), reads=[PSK[6]], writes=["mT"])

        def tail_Oq(i, q):
            b2 = i % 2
            half = q // 2
            hp = hpre[b2]
            for ec in range(4 * (q % 2), 4 * (q % 2) + 4):
                S.pe(lambda e, ec=ec, b2=b2, half=half: e.matmul(bank(7)[:, :], lhsT=mT[b2][:, ec, :], rhs=wo[:, ec, half * 512:(half + 1) * 512], start=(ec == 0), stop=(ec == 7)),
                     reads=["mT", "wo"], writes=[PSK[7]])
            if q % 2 == 1:
                S.dve(lambda e, hp=hp, b2=b2, half=half: e.scalar_tensor_tensor(out=hp[:, half * 512:(half + 1) * 512], in0=xres[b2][:, half * 512:(half + 1) * 512], scalar=ALPHA, in1=bank(7)[:, :], op0=ALU.mult, op1=ALU.add),
                      reads=["xres%d" % b2, PSK[7]], writes=["hpre%d.%d" % (b2, half)])
                S.dve(lambda e, hp=hp, half=half, b2=b2: e.bn_stats(bst[b2][:, half, :], hp[:, half * 512:(half + 1) * 512]), reads=["hpre%d.%d" % (b2, half)], writes=["bst%d.%d" % (b2, half)])
            if q == 3:
                S.dve(lambda e, b2=b2: e.bn_aggr(mv[b2][:, :], bst[b2][:, :, :]), reads=["bst%d.0" % b2, "bst%d.1" % b2], writes=["mv%d" % b2])

        def tail_L(i):
            b2 = i % 2
            hp = hpre[b2]
            HK = ["hpre%d.0" % b2, "hpre%d.1" % b2]
            S.act(lambda e, b2=b2: e.activation(out=rs1[b2][:, :], in_=mv[b2][:, 1:2], func=AF.Ln, bias=LN_EPS, scale=1.0), reads=["mv%d" % b2], writes=["rs1%d" % b2])
            S.act(lambda e, b2=b2: e.activation(out=rs1[b2][:, :], in_=rs1[b2][:, :], func=AF.Exp, scale=-0.5), reads=["rs1%d" % b2], writes=["rs1%d" % b2])
            S.dve(lambda e, hp=hp, b2=b2: e.tensor_scalar(out=hp[:, :], in0=hp[:, :], scalar1=mv[b2][:, 0:1], scalar2=rs1[b2][:, 0:1], op0=ALU.subtract, op1=ALU.mult),
                  reads=HK + ["mv%d" % b2, "rs1%d" % b2], writes=HK)
            S.dve(lambda e, hp=hp: e.tensor_tensor(out=hp[:, :], in0=hp[:, :], in1=lnG[:, :], op=ALU.mult), reads=HK + ["lnG"], writes=HK)
            S.dve(lambda e, hp=hp: e.tensor_tensor(out=hp[:, :], in0=hp[:, :], in1=lnB[:, :], op=ALU.add), reads=HK + ["lnB"], writes=HK)
            S.dve(lambda e, hp=hp: e.tensor_scalar(out=ah[:, :], in0=hp[:, :], scalar1=ALPHA, scalar2=None, op0=ALU.mult), reads=HK, writes=["ah"])
            S.dma("sp", lambda e, i=i, s=s: e.dma_start(out=h_scr[s, i * 128:(i + 1) * 128, :], in_=ah[:, :]), reads=["ah"], writes=["h_scr.%d" % i])

        def tail_H(i, half):
            b2 = i % 2
            hp = hpre[b2]
            for q4 in range(4):
                ec = half * 4 + q4
                S.pe(lambda e, q4=q4, ec=ec, hp=hp: e.transpose(bank(6)[:, q4 * 128:(q4 + 1) * 128], hp[:, ec * 128:(ec + 1) * 128], identF[:, :]), reads=["hpre%d.%d" % (b2, half), "identF"], writes=[PSK[6]])
            S.dve(lambda e, half=half, i=i: e.tensor_copy(hT[:, 4 * half:4 * half + 4, i * 128:(i + 1) * 128], bank(6)[:, :].rearrange("p (a b) -> p a b", a=4)), reads=[PSK[6]], writes=["hT.%d.%d" % (i, half)])
            S.dve(lambda e, half=half: e.tensor_copy(hTf[:, 4 * half:4 * half + 4, :], bank(6)[:, :].rearrange("p (a b) -> p a b", a=4)), reads=[PSK[6]], writes=["hTf.%d" % half])

        def tail_R(i, part):
            for ec in range(4 * part, 4 * part + 4):
                S.pe(lambda e, ec=ec: e.matmul(bank(6)[:, 0:20], lhsT=hTf[:, ec, :], rhs=rw[:, ec, :], start=(ec == 0), stop=(ec == 7)), reads=["hTf.%d" % (ec // 4)] + RWK, writes=[PSK[6]])
            if part == 1:
                S.dve(lambda e, i=i: e.tensor_tensor(out=logits[:, i, :], in0=bank(6)[:, 0:20], in1=rb_bc[:, :], op=ALU.add), reads=[PSK[6], "rb0", "rb1"], writes=["logits.%d" % i])

        TD = [int(v) for v in os.environ.get("KTAIL", "0,1,2,3,4,5,9,11,12,14").split(",")]
        TAIL = [(TD[0], tail_N), (TD[1], tail_T), (TD[2], lambda i: tail_Oq(i, 0)), (TD[3], lambda i: tail_Oq(i, 1)), (TD[4], lambda i: tail_Oq(i, 2)), (TD[5], lambda i: tail_Oq(i, 3)),
                (TD[6], tail_L), (TD[7], lambda i: tail_H(i, 0)), (TD[8], lambda i: tail_H(i, 1)), (TD[9], lambda i: (tail_R(i, 0), tail_R(i, 1)))]
        NSTEP = len(TAIL)
        done_steps = set()

        def emit_step(t, k):
            if t < 0 or (t, k) in done_steps:
                return
            for kk in range(k):
                emit_step(t, kk)
            emit_step(t - 1, k)
            if k == 1:
                emit_step(t - 1, 5)
            if k == 7:
                emit_step(t - 1, NSTEP - 1)
            if k == 0:
                for kk in range(NSTEP):
                    emit_step(t - 2, kk)
            done_steps.add((t, k))
            TAIL[k][1](t)

        pending = []
        def tick():
            keep = []
            for ent in pending:
                if ent[0] <= 0:
                    emit_step(ent[1], ent[2])
                else:
                    ent[0] -= 1
                    keep.append(ent)
            pending[:] = keep
        prev = None
        for gi, u in enumerate(units):
            emit_S(u, gi)
            emit_E(u, gi)
            if prev is not None:
                emit_V(prev[0], prev[1])
                if prev[0][0] != u[0]:
                    for k, (dly, fn) in enumerate(TAIL):
                        pending.append([dly, prev[0][0], k])
            tick()
            prev = (u, gi)
        if prev is not None:
            emit_V(prev[0], prev[1])
            for k, (dly, fn) in enumerate(TAIL):
                pending.append([dly, prev[0][0], k])
        while pending:
            tick()

        if dbg == "att" and s == 0:
            fence()
            RACC.reset()
            cvt = RACC.alloc([NT * 1024], BF16)
            d1 = dbg_out("dbg_hT", [128, 8 * SEQ]); d2 = dbg_out("dbg_logits", [128, NT * 20])
            cv2 = RACC.alloc([2 * SEQ], F32)
            for q in range(4):
                S.dve(lambda e, q=q: e.tensor_copy(cv2[:, :], hT[:, 2 * q:2 * q + 2, :].rearrange("p a b -> p (a b)")), reads=[], writes=["cv2"])
                outs.append(S.dma("sp", lambda e, q=q: e.dma_start(out=d1[:, 2 * q * SEQ:(2 * q + 2) * SEQ], in_=cv2[:, :]), reads=["cv2"]))
            outs.append(S.dma("sp", lambda e: e.dma_start(out=d2[:, :], in_=logits.rearrange("p t j -> p (t j)")), reads=["logits.%d" % i for i in range(int(os.environ.get("KATT_TILES", NT)))] if int(os.environ.get("KATT_STAGE", 9)) >= 7 else []))
            break


        S.fold_now = os.environ.get("KFOLD", "all") == "all"
        fence()
        TT.reset(); RW.reset(); RACC.reset()
        S.dma("sp", lambda e: e.dma_start(out=lnG[:, :], in_=ln2_g[0:1, :].to_broadcast([128, 1024])), writes=["lnG"])
        S.dma("sp", lambda e: e.dma_start(out=lnB[:, :], in_=ln2_b[0:1, :].to_broadcast([128, 1024])), writes=["lnB"])
        LOGK = ["logits.%d" % i for i in range(NT)]
        lg = logits[:, :, 0:4]
        le4 = logits[:, :, 4:20].rearrange("p t (g j) -> p t g j", g=4)
        gmax = TT.alloc([NT], F32); goh = TT.alloc([NT, 4], F32); gex = TT.alloc([NT, 4], F32)
        gsum = TT.alloc([NT], F32); gval = TT.alloc([NT], F32)
        tmp16 = TT.alloc([NT, 4, 4], F32); esel = TT.alloc([NT, 4], F32)
        m1 = TT.alloc([NT], F32); oh1 = TT.alloc([NT, 4], F32); e2 = TT.alloc([NT, 4], F32)
        m2 = TT.alloc([NT], F32); oh2 = TT.alloc([NT, 4], F32); dd = TT.alloc([NT], F32)
        w1 = TT.alloc([NT], F32); w2 = TT.alloc([NT], F32); cw1 = TT.alloc([NT], F32); cw2 = TT.alloc([NT], F32)
        cj = TT.alloc([NT, 4], F32); cj2 = TT.alloc([NT, 4], F32)
        bc4 = lambda a: a.unsqueeze(2).to_broadcast([128, NT, 4])
        S.dve(lambda e: e.tensor_reduce(out=gmax[:, :], in_=lg, axis=AX.X, op=ALU.max), reads=LOGK, writes=["gmax"])
        S.dve(lambda e: e.tensor_tensor(out=goh[:, :, :], in0=lg, in1=bc4(gmax[:, :]), op=ALU.is_equal), reads=LOGK + ["gmax"], writes=["goh"])
        S.dve(lambda e: e.tensor_tensor(out=gex[:, :, :], in0=lg, in1=bc4(gmax[:, :]), op=ALU.subtract), reads=LOGK + ["gmax"], writes=["gex"])
        S.act(lambda e: e.activation(out=gex[:, :, :], in_=gex[:, :, :], func=AF.Exp), reads=["gex"], writes=["gex"])
        S.dve(lambda e: e.tensor_reduce(out=gsum[:, :], in_=gex[:, :, :], axis=AX.X, op=ALU.add), reads=["gex"], writes=["gsum"])
        S.dve(lambda e: e.reciprocal(gval[:, :], gsum[:, :]), reads=["gsum"], writes=["gval"])
        S.dve(lambda e: e.tensor_tensor(out=tmp16[:, :, :, :], in0=le4, in1=goh[:, :, :].unsqueeze(3).to_broadcast([128, NT, 4, 4]), op=ALU.mult), reads=LOGK + ["goh"], writes=["tmp16"])
        S.dve(lambda e: e.tensor_reduce(out=esel[:, :, :], in_=tmp16.rearrange("p t g j -> p t j g"), axis=AX.X, op=ALU.add), reads=["tmp16"], writes=["esel"])
        S.dve(lambda e: e.tensor_reduce(out=m1[:, :], in_=esel[:, :, :], axis=AX.X, op=ALU.max), reads=["esel"], writes=["m1"])
        S.dve(lambda e: e.tensor_tensor(out=oh1[:, :, :], in0=esel[:, :, :], in1=bc4(m1[:, :]), op=ALU.is_equal), reads=["esel", "m1"], writes=["oh1"])
        S.dve(lambda e: e.scalar_tensor_tensor(out=e2[:, :, :], in0=oh1[:, :, :], scalar=-1e30, in1=esel[:, :, :], op0=ALU.mult, op1=ALU.add), reads=["oh1", "esel"], writes=["e2"])
        S.dve(lambda e: e.tensor_reduce(out=m2[:, :], in_=e2[:, :, :], axis=AX.X, op=ALU.max), reads=["e2"], writes=["m2"])
        S.dve(lambda e: e.tensor_tensor(out=oh2[:, :, :], in0=e2[:, :, :], in1=bc4(m2[:, :]), op=ALU.is_equal), reads=["e2", "m2"], writes=["oh2"])
        S.dve(lambda e: e.tensor_tensor(out=dd[:, :], in0=m2[:, :], in1=m1[:, :], op=ALU.subtract), reads=["m1", "m2"], writes=["dd"])
        S.act(lambda e: e.activation(out=dd[:, :], in_=dd[:, :], func=AF.Exp), reads=["dd"], writes=["dd"])
        S.dve(lambda e: e.tensor_scalar(out=w1[:, :], in0=dd[:, :], scalar1=1.0, scalar2=None, op0=ALU.add), reads=["dd"], writes=["w1"])
        S.dve(lambda e: e.reciprocal(w1[:, :], w1[:, :]), reads=["w1"], writes=["w1"])
        S.dve(lambda e: e.tensor_tensor(out=w2[:, :], in0=dd[:, :], in1=w1[:, :], op=ALU.mult), reads=["dd", "w1"], writes=["w2"])
        S.dve(lambda e: e.tensor_tensor(out=cw1[:, :], in0=gval[:, :], in1=w1[:, :], op=ALU.mult), reads=["gval", "w1"], writes=["cw1"])
        S.dve(lambda e: e.tensor_tensor(out=cw2[:, :], in0=gval[:, :], in1=w2[:, :], op=ALU.mult), reads=["gval", "w2"], writes=["cw2"])
        S.dve(lambda e: e.tensor_tensor(out=cj[:, :, :], in0=oh1[:, :, :], in1=bc4(cw1[:, :]), op=ALU.mult), reads=["oh1", "cw1"], writes=["cj"])
        S.dve(lambda e: e.tensor_tensor(out=cj2[:, :, :], in0=oh2[:, :, :], in1=bc4(cw2[:, :]), op=ALU.mult), reads=["oh2", "cw2"], writes=["cj2"])
        S.dve(lambda e: e.tensor_tensor(out=cj[:, :, :], in0=cj[:, :, :], in1=cj2[:, :, :], op=ALU.add), reads=["cj", "cj2"], writes=["cj"])
        S.dve(lambda e: e.tensor_tensor(out=comb.rearrange("p t (g j) -> p t g j", g=4), in0=goh[:, :, :].unsqueeze(3).to_broadcast([128, NT, 4, 4]), in1=cj[:, :, :].unsqueeze(2).to_broadcast([128, NT, 4, 4]), op=ALU.mult),
              reads=["goh", "cj"], writes=["comb"])

        S.fold_now = os.environ.get("KFOLD", "all") in ("moe", "attmoe", "all")
        acc = RACC.alloc([NT, 1024], F32)
        sg = [TT.alloc([512], BF16) for _ in range(2)]
        actT = [TT.alloc([4, 512], BF16) for _ in range(2)]
        obuf = [TT.alloc([1024], F32) for _ in range(2)]
        bst2 = [TT.alloc([2, 6], F32) for _ in range(2)]
        mv2 = [TT.alloc([2], F32) for _ in range(2)]
        rs2 = [TT.alloc([1], F32) for _ in range(2)]
        for q in range(4):
            S.dma("sp", lambda e, q=q, s=s: e.dma_start(out=acc[:, 4 * q:4 * q + 4, :], in_=h_scr[s, q * 512:(q + 1) * 512, :].rearrange("(t p) d -> p t d", p=128)),
                  reads=["h_scr.%d" % t for t in range(4 * q, 4 * q + 4)], writes=["acc.%d" % t for t in range(4 * q, 4 * q + 4)])
        def ln2_stats(t):
            b2 = t % 2
            for c2 in range(2):
                S.dve(lambda e, c2=c2, t=t, b2=b2: e.bn_stats(bst2[b2][:, c2, :], acc[:, t, c2 * 512:(c2 + 1) * 512]), reads=["acc.%d" % t], writes=["bst2%d.%d" % (b2, c2)])
            S.dve(lambda e, b2=b2: e.bn_aggr(mv2[b2][:, :], bst2[b2][:, :, :]), reads=["bst2%d.0" % b2, "bst2%d.1" % b2], writes=["mv2%d" % b2])

        def ln2_apply(t):
            b2 = t % 2
            S.act(lambda e, b2=b2: e.activation(out=rs2[b2][:, :], in_=mv2[b2][:, 1:2], func=AF.Ln, bias=LN_EPS, scale=1.0), reads=["mv2%d" % b2], writes=["rs2%d" % b2])
            S.act(lambda e, b2=b2: e.activation(out=rs2[b2][:, :], in_=rs2[b2][:, :], func=AF.Exp, scale=-0.5), reads=["rs2%d" % b2], writes=["rs2%d" % b2])
            S.dve(lambda e, t=t, b2=b2: e.tensor_scalar(out=obuf[b2][:, :], in0=acc[:, t, :], scalar1=mv2[b2][:, 0:1], scalar2=rs2[b2][:, 0:1], op0=ALU.subtract, op1=ALU.mult),
                  reads=["acc.%d" % t, "mv2%d" % b2, "rs2%d" % b2], writes=["obuf%d" % b2])
            S.dve(lambda e, b2=b2: e.tensor_tensor(out=obuf[b2][:, :], in0=obuf[b2][:, :], in1=lnG[:, :], op=ALU.mult), reads=["obuf%d" % b2, "lnG"], writes=["obuf%d" % b2])
            S.dve(lambda e, b2=b2: e.tensor_tensor(out=obuf[b2][:, :], in0=obuf[b2][:, :], in1=lnB[:, :], op=ALU.add), reads=["obuf%d" % b2, "lnB"], writes=["obuf%d" % b2])
            outs.append(S.dma("sp", lambda e, t=t, b2=b2, s=s: e.dma_start(out=out[s, t * 128:(t + 1) * 128, :], in_=obuf[b2][:, :]), reads=["obuf%d" % b2], writes=["out.%d.%d" % (s, t)]))

        ln2_prev = []
        if NEXP > 1:
            load_expert(1)
        cg = 0; cd = 0
        for ex in range(NEXP):
            sl = ex % 2
            for tb in range(4):
                ab = (ex * 4 + tb) % 2
                hk = ["hT.%d.%d" % (i, hf) for i in range(4 * tb, 4 * tb + 4) for hf in range(2)]
                for fc in range(4):
                    pg = cg % 2; cg += 1
                    for k in range(8):
                        S.pe(lambda e, pg=pg, k=k, fc=fc, tb=tb, sl=sl: e.matmul(bank(pg)[:, :], lhsT=wgu[sl][:, k, fc * 128:(fc + 1) * 128], rhs=hT[:, k, tb * 512:(tb + 1) * 512], start=(k == 0), stop=(k == 7)),
                             reads=["wg.%d" % sl] + hk, writes=[PSK[pg]])
                    for k in range(8):
                        S.pe(lambda e, pg=pg, k=k, fc=fc, tb=tb, sl=sl: e.matmul(bank(2 + pg)[:, :], lhsT=wgu[sl][:, k, 512 + fc * 128:512 + (fc + 1) * 128], rhs=hT[:, k, tb * 512:(tb + 1) * 512], start=(k == 0), stop=(k == 7)),
                             reads=["wu.%d" % sl] + hk, writes=[PSK[2 + pg]])
                    S.act(lambda e, pg=pg: e.activation(out=sg[pg][:, :], in_=bank(pg)[:, :], func=AF.Silu), reads=[PSK[pg]], writes=["sg%d" % pg])
                    S.dve(lambda e, pg=pg, ab=ab, fc=fc: e.tensor_tensor(out=actT[ab][:, fc, :], in0=bank(2 + pg)[:, :], in1=sg[pg][:, :], op=ALU.mult),
                          reads=[PSK[2 + pg], "sg%d" % pg], writes=["actT%d.%d" % (ab, fc)])
                for tt in range(4):
                    t = tb * 4 + tt
                    pdi = 2 + (cd % 2); cd += 1
                    for half in range(2):
                        for fc in range(4):
                            S.pe(lambda e, pdi=pdi, half=half, fc=fc, ab=ab, tt=tt, sl=sl: e.matmul(pd[pdi][:, half * 512:(half + 1) * 512], lhsT=actT[ab][:, fc, tt * 128:(tt + 1) * 128], rhs=wdn[sl][:, fc, half * 512:(half + 1) * 512], start=(fc == 0), stop=(fc == 3)),
                                 reads=["actT%d.%d" % (ab, fc), "wd.%d" % sl], writes=[PSK[2 * pdi + half]])
                    S.dve(lambda e, pdi=pdi, t=t, ex=ex: e.scalar_tensor_tensor(out=acc[:, t, :], in0=pd[pdi][:, :], scalar=comb[:, t, ex:ex + 1], in1=acc[:, t, :], op0=ALU.mult, op1=ALU.add),
                          reads=[PSK[2 * pdi], PSK[2 * pdi + 1], "comb", "acc.%d" % t], writes=["acc.%d" % t])
                    if ex == NEXP - 1:
                        ln2_stats(t)
                        if ln2_prev:
                            ln2_apply(ln2_prev.pop())
                        ln2_prev.append(t)
            if ex + 2 < NEXP:
                load_expert(ex + 2)
        while ln2_prev:
            ln2_apply(ln2_prev.pop())
        S.fold_now = os.environ.get("KFOLD", "all") == "all"
        if s + 1 < NSEQ:
            fence()
        if dbg == "one":
            break

    with nc.allow_non_contiguous_dma(reason="tiny constant loads"):
        st = S.emit(outs)
    return nc, st, dbg_t


_CACHE = {}


def _get_program():
    if "p" not in _CACHE:
        _CACHE["p"] = build_program(dbg=os.environ.get("KDBG", ""))
    return _CACHE["p"]


def kernel(**inputs):
    nc, st, dbg_t = _get_program()
    f = lambda a: np.ascontiguousarray(np.asarray(a, dtype=np.float32))
    x = f(inputs["x"])
    shared = {
        "w_in": f(inputs["w_in"])[0], "b_in": f(inputs["b_in"]).reshape(1, DIN),
        "conv_w": f(inputs["conv_w"])[0], "conv_b": f(inputs["conv_b"]).reshape(1, 1024),
        "a_log": f(inputs["a_log"]).reshape(1, 8), "d_skip": f(inputs["d_skip"]).reshape(1, 8),
        "ssd_norm_g": f(inputs["ssd_norm_g"]).reshape(1, 512), "w_out": f(inputs["w_out"])[0],
        "ln1_g": f(inputs["ln1_g"]).reshape(1, DM), "ln1_b": f(inputs["ln1_b"]).reshape(1, DM),
        "router_group_w": f(inputs["router_group_w"])[0], "router_group_b": f(inputs["router_group_b"]).reshape(1, 4),
        "router_expert_w": f(inputs["router_expert_w"])[0], "router_expert_b": f(inputs["router_expert_b"]).reshape(1, 16),
        "w_gate": f(inputs["w_gate"])[0], "w_up": f(inputs["w_up"])[0], "w_down": f(inputs["w_down"])[0],
        "ln2_g": f(inputs["ln2_g"]).reshape(1, DM), "ln2_b": f(inputs["ln2_b"]).reshape(1, DM),
    }
    ncores = int(os.environ.get("KCORES", NCORES))
    in_maps = []
    for c in range(ncores):
        m = dict(shared)
        m["x"] = np.ascontiguousarray(x[c * NSEQ:(c + 1) * NSEQ])
        in_maps.append(m)
    res = run_bass_kernel_spmd(nc, in_maps, core_ids=list(range(ncores)))
    if os.environ.get("KDBG", ""):
        _CACHE["dbg"] = res.results
    outp = np.concatenate([r["out"] for r in res.results], axis=0)
    return outp.astype(np.float32)
```

```python
import os
import numpy as np
import concourse.bass as bass
import concourse.mybir as mybir
from concourse.bass_utils import run_bass_kernel_spmd

F32 = mybir.dt.float32
BF16 = mybir.dt.bfloat16
U8 = mybir.dt.uint8
AF = mybir.ActivationFunctionType
ALU = mybir.AluOpType
AX = mybir.AxisListType

NCORES = 8
NSEQ = 2
SEQ = 2048
NT = 16
DM = 1024
DIN = 3088
ALPHA = float(2.0 ** 0.25)
LN_EPS = 1e-5
RMS_EPS = 1e-5
ATT_SCALE = 0.125
NEG = -30000.0
FOLD_ENG = tuple(os.environ.get("KFOLDENG", "act,dve").split(","))


class Op:
    __slots__ = ("eng", "fn", "reads", "writes", "deps", "signal", "sigval", "dma", "gi", "nofence", "fold")


class Sched:
    COMPUTE = ("pe", "act", "dve", "pool")

    def __init__(self, nc, n_dma_sems=40):
        self.nc = nc
        self.h = {"pe": nc.tensor, "act": nc.scalar, "dve": nc.vector, "pool": nc.gpsimd, "sp": nc.sync}
        self.ops = []
        self.last_w = {}
        self.readers = {}
        self.n_dma_sems = n_dma_sems
        self.live_dma = []
        self.nfence = 0
        self.fold_now = os.environ.get("KFOLD", "all") == "all"

    def add(self, eng, fn, reads=(), writes=(), dma=False, nofence=False):
        o = Op()
        o.eng = eng; o.fn = fn; o.reads = tuple(reads); o.writes = tuple(writes)
        o.deps = []; o.signal = False; o.sigval = None; o.dma = dma; o.gi = len(self.ops); o.nofence = nofence
        o.fold = self.fold_now
        for r in o.reads:
            p = self.last_w.get(r)
            if p is not None:
                self._dep(o, p, True)
            if r.startswith("ps"):
                rd = self.readers.get(r)
                if rd:
                    for q in rd.values():
                        if q.eng != eng:
                            self._dep(o, q, True)
        for w in o.writes:
            p = self.last_w.get(w)
            if p is not None:
                self._dep(o, p, False)
            rd = self.readers.get(w)
            if rd:
                for q in rd.values():
                    self._dep(o, q, False)
        for r in o.reads:
            d = self.readers.setdefault(r, {})
            d[("dma", o.gi) if dma else eng] = o
        for w in o.writes:
            self.last_w[w] = o
            self.readers[w] = {}
        self.ops.append(o)
        if dma and not nofence:
            self.live_dma.append(o)
        return o

    def _dep(self, o, p, raw):
        if p is o:
            return
        if (not p.dma) and (not o.dma) and p.eng == o.eng:
            if o.eng == "pe":
                return
        o.deps.append(p)
        p.signal = True

    def pe(self, fn, reads=(), writes=()): return self.add("pe", fn, reads, writes)
    def act(self, fn, reads=(), writes=()): return self.add("act", fn, reads, writes)
    def dve(self, fn, reads=(), writes=()): return self.add("dve", fn, reads, writes)
    def pool(self, fn, reads=(), writes=()): return self.add("pool", fn, reads, writes)
    def dma(self, q, fn, reads=(), writes=(), nofence=False):
        return self.add(q, fn, reads, writes, dma=True, nofence=nofence)

    def fence(self, scratch):
        n = self.nfence; self.nfence += 1
        a_keys = []
        col = {"pe": None, "act": 0, "dve": 1, "pool": 2}
        for e in ("act", "dve", "pool"):
            k = "fenceA.%d.%s" % (n, e)
            c = col[e]
            if e == "act":
                self.add(e, (lambda eh, c=c: eh.activation(out=scratch[:, c:c + 1], in_=scratch[:, 8:9], func=AF.Copy)), reads=(), writes=(k,))
            else:
                self.add(e, (lambda eh, c=c: eh.memset(scratch[:, c:c + 1], 0.0)), reads=(), writes=(k,))
            a_keys.append(k)
        k = "fenceA.%d.pe" % n
        self.add("pe", (lambda eh: eh.matmul(self.fence_ps[0:1, 0:1], lhsT=self.fence_w[0:1, 0:1], rhs=self.fence_w[0:1, 0:1], start=True, stop=True)),
                 reads=(), writes=(k, "ps7"))
        a_keys.append(k)
        dmas = self.live_dma
        self.live_dma = []
        for e in ("act", "dve", "pool", "pe", "sp"):
            kb = "fenceB.%d.%s" % (n, e)
            if e == "act":
                o = self.add(e, (lambda eh: eh.activation(out=scratch[:, 3:4], in_=scratch[:, 8:9], func=AF.Copy)), reads=a_keys, writes=(kb,))
            elif e == "pe":
                o = self.add(e, (lambda eh: eh.matmul(self.fence_ps[0:1, 1:2], lhsT=self.fence_w[0:1, 0:1], rhs=self.fence_w[0:1, 0:1], start=True, stop=True)),
                             reads=a_keys, writes=(kb, "ps7"))
            elif e == "sp":
                o = self.add(e, (lambda eh: eh.nop()), reads=a_keys, writes=(kb,))
            else:
                c = 4 if e == "dve" else 5
                o = self.add(e, (lambda eh, c=c: eh.memset(scratch[:, c:c + 1], 0.0)), reads=a_keys, writes=(kb,))
            for d in dmas:
                o.deps.append(d)

    def emit(self, final_wait_ops=()):
        nc = self.nc
        esem = {e: nc.alloc_semaphore("s_" + e) for e in self.COMPUTE}
        dsems = [nc.alloc_semaphore("s_dma%d" % i) for i in range(self.n_dma_sems)]
        dtotal = [0] * self.n_dma_sems
        dlast = [None] * self.n_dma_sems
        ecount = {e: 0 for e in self.COMPUTE}
        nd = 0
        nq = {"sp": 0, "pool": 0}
        half = self.n_dma_sems // 2
        for o in self.ops:
            if o.dma:
                qi = nq[o.eng]; nq[o.eng] += 1; nd += 1
                i = (qi % half) + (0 if o.eng == "sp" else half)
                prev = dlast[i]
                if prev is not None:
                    o.deps.append(prev)
                dtotal[i] += 16
                o.sigval = (dsems[i], dtotal[i], 1000 + i)
                dlast[i] = o
            elif o.signal:
                ecount[o.eng] += 1
                o.sigval = (esem[o.eng], ecount[o.eng], o.eng)
        known = {e: {} for e in self.h}
        nwaits = 0
        snaps = {}
        for o in self.ops:
            eh = self.h[o.eng]
            kn = known[o.eng]
            deps = sorted(o.deps, key=lambda p: -p.gi)
            todo = []
            for p in deps:
                s, v, key = p.sigval
                if kn.get(key, 0) >= v:
                    continue
                todo.append((s, v, key))
                kn[key] = v
                sn = snaps.get(p.gi)
                if sn:
                    for k2, v2 in sn.items():
                        if kn.get(k2, 0) < v2:
                            kn[k2] = v2
            fold = None
            if todo and o.fold and (o.eng == "pe" or (o.eng in FOLD_ENG and not o.dma)):
                fold = todo.pop()
            for (s, v, key) in todo:
                eh.wait_ge(s, v)
                nwaits += 1
            ins = o.fn(eh)
            if fold is not None:
                ins._wait_ge(fold[0], fold[1])
            if o.dma:
                ins.then_inc(o.sigval[0], 16)
                snaps[o.gi] = dict(kn)
            elif o.signal:
                ins.then_inc(o.sigval[0], 1)
                snaps[o.gi] = dict(kn)
        eh = self.h["sp"]
        for o in final_wait_ops:
            s, v, key = o.sigval
            eh.wait_ge(s, v)
        self.stats = dict(n_ops=len(self.ops), n_waits=nwaits, counts=dict(ecount), n_dma=nd)
        return self.stats


class Arena:
    def __init__(self, nc, name, nbytes):
        self.t = nc.alloc_sbuf_tensor(name, [128, nbytes], U8)
        self.n = nbytes
        self.off = 0

    def reset(self, off=0):
        self.off = off

    def alloc(self, shape, dtype, parts=128):
        esz = 2 if dtype == BF16 else 4
        n = esz
        for s in shape:
            n *= s
        off = (self.off + 31) // 32 * 32
        assert off + n <= self.n, (off, n, self.n)
        self.off = off + n
        flat = self.t[0:parts, off:off + n].bitcast(dtype)
        if len(shape) == 1:
            return flat
        names = " ".join("a%d" % i for i in range(len(shape)))
        kw = {"a%d" % i: shape[i] for i in range(1, len(shape))}
        return flat.rearrange("p (%s) -> p %s" % (names, names), **kw)


def build_program(dbg=False):
    nc = bass.Bass("TRN2", target_bir_lowering=False)
    S = Sched(nc)
    D = {}

    def din(name, shape):
        D[name] = nc.dram_tensor(name, list(shape), F32, kind="ExternalInput").ap()
        return D[name]

    x = din("x", [NSEQ, SEQ, DM])
    w_in = din("w_in", [DM, DIN])
    b_in = din("b_in", [1, DIN])
    conv_w = din("conv_w", [4, 1024])
    conv_b = din("conv_b", [1, 1024])
    a_log = din("a_log", [1, 8])
    d_skip = din("d_skip", [1, 8])
    ssd_g = din("ssd_norm_g", [1, 512])
    w_out = din("w_out", [DM, DM])
    ln1_g = din("ln1_g", [1, DM]); ln1_b = din("ln1_b", [1, DM])
    rg_w = din("router_group_w", [DM, 4]); rg_b = din("router_group_b", [1, 4])
    re_w = din("router_expert_w", [4, DM, 4]); re_b = din("router_expert_b", [1, 16])
    w_gate = din("w_gate", [16, DM, 512]); w_up = din("w_up", [16, DM, 512]); w_down = din("w_down", [16, 512, DM])
    ln2_g = din("ln2_g", [1, DM]); ln2_b = din("ln2_b", [1, DM])
    out = nc.dram_tensor("out", [NSEQ, SEQ, DM], F32, kind="ExternalOutput").ap()
    h_scr = nc.dram_tensor("h_scr", [NSEQ, SEQ, DM], F32).ap()
    dbg_t = {}

    def dbg_out(name, shape):
        dbg_t[name] = nc.dram_tensor(name, list(shape), F32, kind="ExternalOutput").ap()
        return dbg_t[name]

    CONST = Arena(nc, "CONST", 22 * 1024)
    RW = Arena(nc, "RW", 49408)
    RX = Arena(nc, "RX", 32768)
    RACC = Arena(nc, "RACC", 65536)
    MSSD = Arena(nc, "MSSD", 16384)
    TT = Arena(nc, "TT", nc.sbuf_bytes_remaining - 256)

    identB = CONST.alloc([128], BF16); identF = CONST.alloc([128], F32)
    Uf = CONST.alloc([128], F32); triU = CONST.alloc([128], BF16); maskneg = CONST.alloc([128], BF16)
    onesF = CONST.alloc([128], F32)
    ones_row = CONST.alloc([128], BF16, parts=1)
    bz_row = CONST.alloc([512], BF16, parts=1); bv_row = CONST.alloc([512], BF16, parts=1)
    bdtf = CONST.alloc([16], F32)
    bxbc = CONST.alloc([8], F32); bq = CONST.alloc([4], F32); bk = CONST.alloc([4], F32)
    convw = CONST.alloc([8, 4], F32); convb = CONST.alloc([8], F32)
    a_bc = CONST.alloc([8], F32); dskip_bc = CONST.alloc([8], F32)
    gssd_bc = CONST.alloc([512], F32)
    lnG = CONST.alloc([1024], F32); lnB = CONST.alloc([1024], F32)
    rw = CONST.alloc([8, 20], F32); rb_bc = CONST.alloc([20], F32)
    logits = CONST.alloc([NT, 20], F32); comb = CONST.alloc([NT, 16], F32)
    fsc = CONST.alloc([16], F32)
    S.fence_w = CONST.alloc([8], BF16)
    pd = [nc.alloc_psum_tensor("pd%d" % i, [128, 1024], F32) for i in range(4)]
    def bank(i):
        return pd[i // 2][:, (i % 2) * 512:(i % 2) * 512 + 512]
    def bankb(i):
        return bank(i).bitcast(BF16)
    PSK = ["ps%d" % i for i in range(8)]
    S.fence_ps = nc.alloc_sbuf_tensor("fence_dummy", [1, 8], F32)
    S.fence_ps = bank(7)[:, 504:512]

    def fence():
        S.fence(fsc)

    S.pool(lambda e: e.memset(fsc[:, :], 0.0), writes=["fsc"])
    S.pool(lambda e: e.memset(S.fence_w[:, :], 0.0), writes=["fence_w"])
    S.pool(lambda e: e.memset(identB[:, :], 1.0), writes=["identB"])
    S.pool(lambda e: e.affine_select(out=identB[:, :], in_=identB[:, :], pattern=[[-1, 128]], compare_op=ALU.is_equal, fill=0.0, base=0, channel_multiplier=1), reads=["identB"], writes=["identB"])
    S.pool(lambda e: e.memset(identF[:, :], 1.0), writes=["identF"])
    S.pool(lambda e: e.affine_select(out=identF[:, :], in_=identF[:, :], pattern=[[-1, 128]], compare_op=ALU.is_equal, fill=0.0, base=0, channel_multiplier=1), reads=["identF"], writes=["identF"])
    S.pool(lambda e: e.memset(Uf[:, :], 1.0), writes=["Uf"])
    S.pool(lambda e: e.affine_select(out=Uf[:, :], in_=Uf[:, :], pattern=[[1, 128]], compare_op=ALU.is_ge, fill=0.0, base=0, channel_multiplier=-1), reads=["Uf"], writes=["Uf"])
    S.pool(lambda e: e.memset(triU[:, :], 1.0), writes=["triU"])
    S.pool(lambda e: e.affine_select(out=triU[:, :], in_=triU[:, :], pattern=[[1, 128]], compare_op=ALU.is_ge, fill=0.0, base=0, channel_multiplier=-1), reads=["triU"], writes=["triU"])
    S.pool(lambda e: e.memset(maskneg[:, :], NEG), writes=["maskneg"])
    S.pool(lambda e: e.affine_select(out=maskneg[:, :], in_=maskneg[:, :], pattern=[[-1, 128]], compare_op=ALU.is_gt, fill=0.0, base=0, channel_multiplier=1), reads=["maskneg"], writes=["maskneg"])
    S.pool(lambda e: e.memset(onesF[:, :], 1.0), writes=["onesF"])
    S.pool(lambda e: e.memset(ones_row[:, :], 1.0), writes=["ones_row"])
    S.dma("pool", lambda e: e.dma_start(out=bz_row[:, :], in_=b_in[0:1, 0:512]), writes=["bz_row"])
    S.dma("pool", lambda e: e.dma_start(out=bv_row[:, :], in_=b_in[0:1, 2568:3080]), writes=["bv_row"])
    S.dma("sp", lambda e: e.dma_start(out=bdtf[:, 0:8], in_=b_in[0:1, 1536:1544].to_broadcast([128, 8])), writes=["bdtf0"])
    S.dma("sp", lambda e: e.dma_start(out=bdtf[:, 8:16], in_=b_in[0:1, 3080:3088].to_broadcast([128, 8])), writes=["bdtf1"])
    S.dma("sp", lambda e: e.dma_start(out=bxbc[:, :], in_=b_in[0, 512:1536].rearrange("(c p) -> p c", p=128)), writes=["bxbc"])
    S.dma("sp", lambda e: e.dma_start(out=bq[:, :], in_=b_in[0, 1544:2056].rearrange("(c p) -> p c", p=128)), writes=["bq"])
    S.dma("sp", lambda e: e.dma_start(out=bk[:, :], in_=b_in[0, 2056:2568].rearrange("(c p) -> p c", p=128)), writes=["bk"])
    for k in range(4):
        S.dma("sp", lambda e, k=k: e.dma_start(out=convw[:, :, k], in_=conv_w[k, :].rearrange("(c p) -> p c", p=128)), writes=["convw%d" % k])
    CONVW = ["convw%d" % k for k in range(4)]
    S.dma("sp", lambda e: e.dma_start(out=convb[:, :], in_=conv_b[0, :].rearrange("(c p) -> p c", p=128)), writes=["convb"])
    S.dma("sp", lambda e: e.dma_start(out=a_bc[:, :], in_=a_log[0:1, :].to_broadcast([128, 8])), writes=["a_bc"])
    S.act(lambda e: e.activation(out=a_bc[:, :], in_=a_bc[:, :], func=AF.Exp), reads=["a_bc"], writes=["a_bc"])
    S.dve(lambda e: e.tensor_scalar(out=a_bc[:, :], in0=a_bc[:, :], scalar1=-1.0, scalar2=None, op0=ALU.mult), reads=["a_bc"], writes=["a_bc"])
    S.dma("sp", lambda e: e.dma_start(out=dskip_bc[:, :], in_=d_skip[0:1, :].to_broadcast([128, 8])), writes=["dskip_bc"])
    S.dma("sp", lambda e: e.dma_start(out=gssd_bc[:, :], in_=ssd_g[0:1, :].to_broadcast([128, 512])), writes=["gssd_bc"])
    S.dma("sp", lambda e: e.dma_start(out=rw[:, :, 0:4], in_=rg_w.rearrange("(k p) j -> p k j", p=128)), writes=["rw0"])
    for g in range(4):
        S.dma("sp", lambda e, g=g: e.dma_start(out=rw[:, :, 4 + 4 * g:8 + 4 * g], in_=re_w[g].rearrange("(k p) j -> p k j", p=128)), writes=["rw%d" % (g + 1)])
    RWK = ["rw%d" % i for i in range(5)]
    S.dma("sp", lambda e: e.dma_start(out=rb_bc[:, 0:4], in_=rg_b[0:1, :].to_broadcast([128, 4])), writes=["rb0"])
    S.dma("sp", lambda e: e.dma_start(out=rb_bc[:, 4:20], in_=re_b[0:1, :].to_broadcast([128, 16])), writes=["rb1"])

    outs = []

    for s in range(NSEQ):
        RW.reset(); RX.reset(); RACC.reset(); MSSD.reset(); TT.reset()
        wgu = [None, None]; wdn = [None, None]
        wgu[0] = RW.alloc([8, 1024], BF16); wdn[0] = RW.alloc([4, 1024], BF16)
        wgu[1] = RW.alloc([8, 1024], BF16); wdn[1] = RW.alloc([4, 1024], BF16)
        RW.reset()
        wA = RW.alloc([8, 1544], BF16)
        xb = [RW.alloc([4, 1024], BF16) for _ in range(2)]
        xT = RX.alloc([8, SEQ], BF16)
        sz = RACC.alloc([NT, 512], BF16)
        xsB = RACC.alloc([NT, 768], BF16)
        BT = RACC.alloc([2, SEQ], BF16)
        CT = RACC.alloc([2, SEQ], BF16)
        xsT = MSSD.alloc([4, SEQ], BF16)
        dt_t = TT.alloc([NT, 8], F32)
        tt_mark = TT.off
        pre = [TT.alloc([SEQ + 3], BF16) for _ in range(2)]
        diagw = TT.alloc([8, 4, 128], BF16)
        dt_raw = TT.alloc([NT, 8], F32)

        w_in_v = w_in.rearrange("(k p) c -> p k c", p=128)
        S.dma("pool", lambda e, s=s: e.dma_start(out=xb[0][:, :, :], in_=x[s, 0:512, :].rearrange("(t p) d -> p t d", p=128)), writes=["xb0"])
        S.dma("pool", lambda e: e.dma_start(out=wA[:, :, 0:512], in_=w_in_v[:, :, 0:512]), writes=["wA.z"])
        S.dma("pool", lambda e: e.dma_start(out=wA[:, :, 1536:1544], in_=w_in_v[:, :, 1536:1544]), writes=["wA.dt"])
        S.dma("pool", lambda e, s=s: e.dma_start(out=xb[1][:, :, :], in_=x[s, 512:1024, :].rearrange("(t p) d -> p t d", p=128)), writes=["xb1"])
        for half in range(2):
            S.dma("pool", lambda e, half=half: e.dma_start(out=wA[:, 4 * half:4 * half + 4, 512:1536], in_=w_in_v[:, 4 * half:4 * half + 4, 512:1536]), writes=["wA.x%d" % half])
        for b in range(2):
            S.pool(lambda e, b=b: e.memset(pre[b][:, 0:3], 0.0), writes=["prepad%d" % b])
        ev = 0
        for blk in range(4):
            if blk >= 2:
                S.dma("pool", lambda e, blk=blk, s=s: e.dma_start(out=xb[blk % 2][:, :, :], in_=x[s, blk * 512:(blk + 1) * 512, :].rearrange("(t p) d -> p t d", p=128)),
                      writes=["xb%d" % (blk % 2)])
            for k in range(8):
                pb = (blk * 8 + k) % 2
                for t in range(4):
                    S.pe(lambda e, pb=pb, t=t, k=k, blk=blk: e.transpose(bankb(pb)[:, t * 128:(t + 1) * 128], xb[blk % 2][:, t, k * 128:(k + 1) * 128], identB[:, :]),
                         reads=["xb%d" % (blk % 2), "identB"], writes=[PSK[pb]])
                if ev % 2 == 0:
                    S.act(lambda e, pb=pb, k=k, blk=blk: e.copy(xT[:, k, blk * 512:(blk + 1) * 512], bankb(pb)[:, 0:512]), reads=[PSK[pb]], writes=["xT.%d.%d" % (k, blk)])
                else:
                    S.dve(lambda e, pb=pb, k=k, blk=blk: e.tensor_copy(xT[:, k, blk * 512:(blk + 1) * 512], bankb(pb)[:, 0:512]), reads=[PSK[pb]], writes=["xT.%d.%d" % (k, blk)])
                ev += 1
            for tt in range(4):
                t = blk * 4 + tt
                pz = 2 + (t % 2)
                xk = ["xT.%d.%d" % (k, blk) for k in range(8)]
                for k in range(8):
                    S.pe(lambda e, pz=pz, t=t, k=k: e.matmul(bank(pz)[:, :], lhsT=xT[:, k, t * 128:(t + 1) * 128], rhs=wA[:, k, 0:512], start=(k == 0), stop=False),
                         reads=[xk[k], "wA.z"], writes=[PSK[pz]])
                S.pe(lambda e, pz=pz: e.matmul(bank(pz)[:, :], lhsT=ones_row[0:1, :], rhs=bz_row[0:1, :], start=False, stop=True),
                     reads=["ones_row", "bz_row"], writes=[PSK[pz]])
                S.act(lambda e, pz=pz, t=t: e.activation(out=sz[:, t, :], in_=bank(pz)[:, :], func=AF.Silu), reads=[PSK[pz]], writes=["sz.%d" % t])
                for k in range(8):
                    S.pe(lambda e, t=t, k=k: e.matmul(bank(4)[:, t * 8:(t + 1) * 8], lhsT=xT[:, k, t * 128:(t + 1) * 128], rhs=wA[:, k, 1536:1544], start=(k == 0), stop=(k == 7)),
                         reads=[xk[k], "wA.dt"], writes=[PSK[4]])
        S.dve(lambda e: e.tensor_tensor(out=dt_raw[:, :, :], in0=bank(4)[:, 0:128].rearrange("p (t h) -> p t h", h=8), in1=bdtf[:, 0:8].unsqueeze(1).to_broadcast([128, NT, 8]), op=ALU.add),
              reads=[PSK[4], "bdtf0"], writes=["dt_raw"])
        for c in range(8):
            for k in range(4):
                S.dve(lambda e, c=c, k=k: e.tensor_scalar(out=diagw[:, c, k, :], in0=identB[:, :], scalar1=convw[:, c, k:k + 1], scalar2=None, op0=ALU.mult),
                      reads=["identB"] + CONVW, writes=["diagw.%d" % c])

        def a1_ip(c):
            pb_ = c % 2
            for blk in range(4):
                pc = 5 + (c * 4 + blk) % 2
                for k in range(8):
                    S.pe(lambda e, pc=pc, c=c, k=k, blk=blk: e.matmul(bank(pc)[:, :], lhsT=wA[:, k, 512 + c * 128:512 + (c + 1) * 128], rhs=xT[:, k, blk * 512:(blk + 1) * 512], start=(k == 0), stop=(k == 7)),
                         reads=["xT.%d.%d" % (k, blk), "wA.x%d" % (k // 4)], writes=[PSK[pc]])
                S.act(lambda e, pc=pc, c=c, blk=blk, pb_=pb_: e.activation(out=pre[pb_][:, 3 + blk * 512:3 + (blk + 1) * 512], in_=bank(pc)[:, :], func=AF.Identity, bias=bxbc[:, c:c + 1], scale=1.0),
                      reads=[PSK[pc], "bxbc"], writes=["pre%d.%d" % (pb_, blk)])

        def a1_conv(c):
            pb_ = c % 2
            if c < 4:
                dstt = xsT[:, c, :]; dk = "xsT.%d" % c
            elif c < 6:
                dstt = BT[:, c - 4, :]; dk = "BT.%d" % (c - 4)
            else:
                dstt = CT[:, c - 6, :]; dk = "CT.%d" % (c - 6)
            for blk in range(4):
                pc = 2 + (c * 4 + blk) % 2
                rk = ["pre%d.%d" % (pb_, blk), "diagw.%d" % c] + (["pre%d.%d" % (pb_, blk - 1)] if blk > 0 else ["prepad%d" % pb_])
                for k in range(4):
                    S.pe(lambda e, pc=pc, c=c, k=k, blk=blk, pb_=pb_: e.matmul(bank(pc)[:, :], lhsT=diagw[:, c, k, :], rhs=pre[pb_][:, blk * 512 + k:blk * 512 + k + 512], start=(k == 0), stop=(k == 3)),
                         reads=rk, writes=[PSK[pc]])
                S.act(lambda e, pc=pc, dstt=dstt, c=c, blk=blk: e.activation(out=dstt[:, blk * 512:(blk + 1) * 512], in_=bank(pc)[:, :], func=AF.Silu, bias=convb[:, c:c + 1], scale=1.0),
                      reads=[PSK[pc], "convb"], writes=[dk])

        a1_ip(0)
        for c in range(8):
            if c + 1 < 8:
                a1_ip(c + 1)
            a1_conv(c)
        for t in range(NT):
            pb = t % 2
            for c in range(6):
                src = xsT[:, c, t * 128:(t + 1) * 128] if c < 4 else BT[:, c - 4, t * 128:(t + 1) * 128]
                sk = "xsT.%d" % c if c < 4 else "BT.%d" % (c - 4)
                S.pe(lambda e, pb=pb, c=c, src=src: e.transpose(bankb(pb)[:, c * 128:(c + 1) * 128], src, identB[:, :]), reads=[sk, "identB"], writes=[PSK[pb]])
            if t % 2 == 0:
                S.dve(lambda e, pb=pb, t=t: e.tensor_copy(xsB[:, t, :], bankb(pb)[:, 0:768]), reads=[PSK[pb]], writes=["xsB.%d" % t])
            else:
                S.act(lambda e, pb=pb, t=t: e.copy(xsB[:, t, :], bankb(pb)[:, 0:768]), reads=[PSK[pb]], writes=["xsB.%d" % t])
        S.act(lambda e: e.activation(out=dt_t[:, :, :], in_=dt_raw[:, :, :], func=AF.Exp), reads=["dt_raw"], writes=["dt_t"])
        S.act(lambda e: e.activation(out=dt_t[:, :, :], in_=dt_t[:, :, :], func=AF.Ln, bias=1.0, scale=1.0), reads=["dt_t"], writes=["dt_t"])

        fence()
        MSSD.reset(); TT.reset(tt_mark)
        RW.reset()
        wB = RW.alloc([8, 1544], BF16)
        wo = RW.alloc([8, 1024], BF16)
        for half in range(2):
            S.dma("pool", lambda e, half=half: e.dma_start(out=wB[:, 4 * half:4 * half + 4, :], in_=w_in.rearrange("(k p) c -> p k c", p=128)[:, 4 * half:4 * half + 4, 1544:3088]),
                  writes=["wB"], nofence=True)
        for half in range(2):
            S.dma("pool", lambda e, half=half: e.dma_start(out=wo[:, 4 * half:4 * half + 4, :], in_=w_out.rearrange("(k p) c -> p k c", p=128)[:, 4 * half:4 * half + 4, :]),
                  writes=["wo"], nofence=True)
        m_ssd = MSSD.alloc([NT, 512], BF16)
        da = TT.alloc([NT, 8], F32); acum = TT.alloc([NT, 8], F32); nacum = TT.alloc([NT, 8], F32)
        alast = TT.alloc([NT, 8], F32); dte = TT.alloc([NT, 8], F32); ea = TT.alloc([NT, 8], F32)
        cdec = TT.alloc([NT, 8], F32); dtdte = TT.alloc([NT, 8], F32)
        stT = TT.alloc([8, 64], F32); stTb = TT.alloc([8, 64], BF16)
        LT = [TT.alloc([128], BF16) for _ in range(4)]
        MT = [TT.alloc([128], BF16) for _ in range(4)]
        xdt = [TT.alloc([8, 64], BF16) for _ in range(2)]
        xdtd = [TT.alloc([8, 64], BF16) for _ in range(2)]
        t1 = [TT.alloc([8, 64], F32)] * 2
        t2 = [TT.alloc([8, 64], F32) for _ in range(2)]
        yg = [TT.alloc([512], F32) for _ in range(2)]
        junk = TT.alloc([256], F32)
        ss = [TT.alloc([2], F32) for _ in range(2)]
        rstd = [TT.alloc([2], F32) for _ in range(2)]

        S.dve(lambda e: e.tensor_tensor(out=da[:, :, :], in0=dt_t[:, :, :], in1=a_bc[:, :].unsqueeze(1).to_broadcast([128, NT, 8]), op=ALU.mult), reads=["dt_t", "a_bc"], writes=["da"])
        daf = da.rearrange("p t h -> p (t h)")
        S.pe(lambda e: e.matmul(bank(5)[:, 0:128], lhsT=Uf[:, :], rhs=daf, start=True, stop=True), reads=["Uf", "da"], writes=[PSK[5]])
        S.pe(lambda e: e.matmul(bank(6)[:, 0:128], lhsT=onesF[:, :], rhs=daf, start=True, stop=True), reads=["onesF", "da"], writes=[PSK[6]])
        S.dve(lambda e: e.tensor_copy(acum.rearrange("p t h -> p (t h)"), bank(5)[:, 0:128]), reads=[PSK[5]], writes=["acum"])
        S.dve(lambda e: e.tensor_scalar(out=nacum.rearrange("p t h -> p (t h)"), in0=bank(5)[:, 0:128], scalar1=-1.0, scalar2=None, op0=ALU.mult), reads=[PSK[5]], writes=["nacum"])
        S.dve(lambda e: e.tensor_copy(alast.rearrange("p t h -> p (t h)"), bank(6)[:, 0:128]), reads=[PSK[6]], writes=["alast"])
        S.dve(lambda e: e.tensor_tensor(out=dte[:, :, :], in0=alast[:, :, :], in1=acum[:, :, :], op=ALU.subtract), reads=["alast", "acum"], writes=["dte"])
        S.act(lambda e: e.activation(out=dte[:, :, :], in_=dte[:, :, :], func=AF.Exp), reads=["dte"], writes=["dte"])
        S.act(lambda e: e.activation(out=ea[:, :, :], in_=acum[:, :, :], func=AF.Exp), reads=["acum"], writes=["ea"])
        S.act(lambda e: e.activation(out=cdec[:, :, :], in_=alast[:, :, :], func=AF.Exp), reads=["alast"], writes=["cdec"])
        S.dve(lambda e: e.tensor_tensor(out=dtdte[:, :, :], in0=dt_t[:, :, :], in1=dte[:, :, :], op=ALU.mult), reads=["dt_t", "dte"], writes=["dtdte"])
        S.pool(lambda e: e.memset(stT[:, :, :], 0.0), writes=["stT"])
        S.pool(lambda e: e.memset(stTb[:, :, :], 0.0), writes=["stTb"])

        def ssd_F(c):
            cs = slice(c * 128, (c + 1) * 128)
            b2 = c % 2
            py = 2 + b2
            xs_c = xsB[:, c, 0:512].rearrange("p (h d) -> p h d", h=8)
            S.pool(lambda e, c=c, b2=b2, xs_c=xs_c: e.tensor_tensor(out=xdt[b2][:, :, :], in0=xs_c, in1=dt_t[:, c, :].unsqueeze(2).to_broadcast([128, 8, 64]), op=ALU.mult),
                   reads=["xsB.%d" % c, "dt_t"], writes=["xdt%d" % b2])
            S.pool(lambda e, c=c, b2=b2, xs_c=xs_c: e.tensor_tensor(out=xdtd[b2][:, :, :], in0=xs_c, in1=dtdte[:, c, :].unsqueeze(2).to_broadcast([128, 8, 64]), op=ALU.mult),
                   reads=["xsB.%d" % c, "dtdte"], writes=["xdtd%d" % b2])
            S.pool(lambda e, c=c, b2=b2, xs_c=xs_c: e.tensor_tensor(out=t2[b2][:, :, :], in0=xs_c, in1=dskip_bc[:, :].unsqueeze(2).to_broadcast([128, 8, 64]), op=ALU.mult),
                   reads=["xsB.%d" % c, "dskip_bc"], writes=["t2%d" % b2])
            for g in range(2):
                S.pe(lambda e, g=g, cs=cs: e.matmul(bank(4)[:, g * 128:(g + 1) * 128], lhsT=BT[:, g, cs], rhs=CT[:, g, cs], start=True, stop=True),
                     reads=["BT.%d" % g, "CT.%d" % g], writes=[PSK[4]])
            for hh in range(2):
                pl = hh
                for h4 in range(4):
                    h = hh * 4 + h4
                    S.pe(lambda e, pl=pl, h4=h4, c=c, h=h: e.matmul(bank(pl)[:, h4 * 128:(h4 + 1) * 128], lhsT=da[:, c, h:h + 1].to_broadcast([128, 128]), rhs=Uf[:, :], start=True, stop=False),
                         reads=["da", "Uf"], writes=[PSK[pl]])
                    S.pe(lambda e, pl=pl, h4=h4: e.matmul(bank(pl)[:, h4 * 128:(h4 + 1) * 128], lhsT=identB[:, :], rhs=maskneg[:, :], start=False, stop=True),
                         reads=["identB", "maskneg"], writes=[PSK[pl]])
            for hh in range(2):
                pl = hh
                for h4 in range(4):
                    h = hh * 4 + h4
                    S.act(lambda e, pl=pl, h4=h4, c=c, h=h: e.activation(out=LT[h4][:, :], in_=bank(pl)[:, h4 * 128:(h4 + 1) * 128], func=AF.Exp, bias=nacum[:, c, h:h + 1], scale=1.0),
                          reads=[PSK[pl], "nacum"], writes=["LT%d" % h4])
                    g = h // 4
                    S.dve(lambda e, h4=h4, g=g: e.tensor_tensor(out=MT[h4][:, :], in0=bank(4)[:, g * 128:(g + 1) * 128], in1=LT[h4][:, :], op=ALU.mult),
                          reads=[PSK[4], "LT%d" % h4], writes=["MT%d" % h4])
                    S.pe(lambda e, h4=h4, h=h, b2=b2, py=py: e.matmul(bank(py)[:, h * 64:(h + 1) * 64], lhsT=MT[h4][:, :], rhs=xdt[b2][:, h, :], start=True, stop=True),
                         reads=["MT%d" % h4, "xdt%d" % b2], writes=[PSK[py]])

        def ssd_B(c):
            cs = slice(c * 128, (c + 1) * 128)
            b2 = c % 2
            py = 2 + b2
            if c > 0:
                for g in range(2):
                    S.pe(lambda e, g=g, cs=cs: e.matmul(bank(6)[:, g * 256:(g + 1) * 256], lhsT=CT[:, g, cs], rhs=stTb[:, 4 * g:4 * g + 4, :].rearrange("p h d -> p (h d)"), start=True, stop=True),
                         reads=["CT.%d" % g, "stTb"], writes=[PSK[6]])
            if c < NT - 1:
                for g in range(2):
                    S.pe(lambda e, g=g, c=c, b2=b2: e.matmul(bank(7)[:, g * 256:(g + 1) * 256], lhsT=xsB[:, c, 512 + g * 128:512 + (g + 1) * 128], rhs=xdtd[b2][:, 4 * g:4 * g + 4, :].rearrange("p h d -> p (h d)"), start=True, stop=True),
                         reads=["xsB.%d" % c, "xdtd%d" % b2], writes=[PSK[7]])
                S.dve(lambda e, c=c: e.tensor_tensor(out=stT[:, :, :], in0=stT[:, :, :], in1=cdec[:, c, :].unsqueeze(2).to_broadcast([128, 8, 64]), op=ALU.mult),
                      reads=["stT", "cdec"], writes=["stT"])
                S.dve(lambda e: e.tensor_tensor(out=stT[:, :, :], in0=bank(7)[:, 0:512].rearrange("p (h d) -> p h d", h=8), in1=stT[:, :, :], op=ALU.add),
                      reads=[PSK[7], "stT"], writes=["stT"])
            if c > 0:
                S.dve(lambda e, c=c, b2=b2: e.tensor_tensor(out=t1[b2][:, :, :], in0=bank(6)[:, :].rearrange("p (h d) -> p h d", h=8), in1=ea[:, c, :].unsqueeze(2).to_broadcast([128, 8, 64]), op=ALU.mult),
                      reads=[PSK[6], "ea"], writes=["t1"])
            if c < NT - 1:
                S.act(lambda e: e.copy(stTb[:, :, :], stT[:, :, :]), reads=["stT"], writes=["stTb"])
            if c > 0:
                S.dve(lambda e, b2=b2, py=py: e.tensor_tensor(out=t1[b2][:, :, :], in0=bank(py)[:, :].rearrange("p (h d) -> p h d", h=8), in1=t1[b2][:, :, :], op=ALU.add),
                      reads=[PSK[py], "t1"], writes=["t1"])
                S.dve(lambda e, b2=b2: e.tensor_tensor(out=t1[b2][:, :, :], in0=t1[b2][:, :, :], in1=t2[b2][:, :, :], op=ALU.add),
                      reads=["t1", "t2%d" % b2], writes=["t1"])
            else:
                S.dve(lambda e, b2=b2, py=py: e.tensor_tensor(out=t1[b2][:, :, :], in0=bank(py)[:, :].rearrange("p (h d) -> p h d", h=8), in1=t2[b2][:, :, :], op=ALU.add),
                      reads=[PSK[py], "t2%d" % b2], writes=["t1"])
            S.dve(lambda e, c=c, b2=b2: e.tensor_tensor(out=yg[b2][:, :], in0=t1[b2].rearrange("p h d -> p (h d)"), in1=sz[:, c, :], op=ALU.mult),
                  reads=["t1", "sz.%d" % c], writes=["yg%d" % b2])
            for g in range(2):
                S.act(lambda e, g=g, b2=b2: e.activation(out=junk[:, :], in_=yg[b2][:, g * 256:(g + 1) * 256], func=AF.Square, accum_out=ss[b2][:, g:g + 1]),
                      reads=["yg%d" % b2], writes=["junk", "ss%d.%d" % (b2, g)])
            S.act(lambda e, b2=b2: e.activation(out=rstd[b2][:, :], in_=ss[b2][:, :], func=AF.Ln, bias=RMS_EPS, scale=1.0 / 256.0),
                  reads=["ss%d.0" % b2, "ss%d.1" % b2], writes=["rstd%d" % b2])
            S.act(lambda e, b2=b2: e.activation(out=rstd[b2][:, :], in_=rstd[b2][:, :], func=AF.Exp, scale=-0.5), reads=["rstd%d" % b2], writes=["rstd%d" % b2])
            for g in range(2):
                S.dve(lambda e, g=g, b2=b2, c=c: e.scalar_tensor_tensor(out=m_ssd[:, c, g * 256:(g + 1) * 256], in0=yg[b2][:, g * 256:(g + 1) * 256], scalar=rstd[b2][:, g:g + 1], in1=gssd_bc[:, g * 256:(g + 1) * 256], op0=ALU.mult, op1=ALU.mult),
                      reads=["yg%d" % b2, "rstd%d" % b2, "gssd_bc"], writes=["m_ssd.%d" % c])

        ssd_F(0)
        for c in range(NT):
            if c + 1 < NT:
                ssd_F(c + 1)
            ssd_B(c)

        if dbg == 'ssd' and s == 0:
            RX.reset()
            dm = dbg_out("dbg_mssd", [128, NT * 512])
            cvt = RX.alloc([NT * 512], F32) if dbg == 'ssd' else None
            S.dve(lambda e: e.tensor_copy(cvt[:, :], m_ssd.rearrange("p t d -> p (t d)")), reads=["m_ssd.%d" % c for c in range(NT)], writes=["cvt"])
            outs.append(S.dma("sp", lambda e: e.dma_start(out=dm[:, :], in_=cvt[:, :]), reads=["cvt"]))
        if dbg == "ssd":
            break

        fence()
        RW.reset(); RACC.reset(); TT.reset()
        wB = RW.alloc([8, 1544], BF16)
        wo = RW.alloc([8, 1024], BF16)
        ah = RW.alloc([1024], F32)
        hTf = RW.alloc([8, 128], F32)
        qT = RACC.alloc([4, SEQ], BF16)
        kT = RACC.alloc([4, SEQ], BF16)
        v_aug = RACC.alloc([NT, 8, 65], BF16)
        xres = [RACC.alloc([1024], F32) for _ in range(2)]
        hpre = [RACC.alloc([1024], F32), TT.alloc([1024], F32)]
        f_raw = TT.alloc([NT, 8], F32); Gc = TT.alloc([NT, 8], F32); tot = TT.alloc([NT, 8], F32)
        Pp = TT.alloc([NT, 8], F32); Gf = TT.alloc([NT, 8], F32); Gend = TT.alloc([NT, 8], F32)
        biasT = TT.alloc([8, NT, NT], F32)
        PT = [TT.alloc([8, 128], BF16) for _ in range(2)]
        yatt = [TT.alloc([8, 64], BF16) for _ in range(2)]
        rden = [TT.alloc([8], F32) for _ in range(2)]
        mT = [TT.alloc([8, 128], BF16)] * 2
        bst = [TT.alloc([2, 6], F32) for _ in range(2)]
        mv = [TT.alloc([2], F32) for _ in range(2)]
        rs1 = [TT.alloc([1], F32) for _ in range(2)]

        S.dma("sp", lambda e: e.dma_start(out=lnG[:, :], in_=ln1_g[0:1, :].to_broadcast([128, 1024])), writes=["lnG"])
        S.dma("sp", lambda e: e.dma_start(out=lnB[:, :], in_=ln1_b[0:1, :].to_broadcast([128, 1024])), writes=["lnB"])
        S.pool(lambda e: e.memset(v_aug[:, :, :, 64:65], 1.0), writes=["v_ones"])
        evq = 0
        for qk in range(2):
            dstT = qT if qk == 0 else kT
            bcol = bq if qk == 0 else bk
            nm = "qT" if qk == 0 else "kT"
            for p in range(4):
                for blk in range(4):
                    pq = evq % 4
                    for k in range(8):
                        S.pe(lambda e, pq=pq, k=k, p=p, blk=blk, qk=qk: e.matmul(bank(pq)[:, :], lhsT=wB[:, k, qk * 512 + p * 128:qk * 512 + (p + 1) * 128], rhs=xT[:, k, blk * 512:(blk + 1) * 512], start=(k == 0), stop=(k == 7)),
                             reads=["xT.%d.%d" % (k, blk), "wB"], writes=[PSK[pq]])
                    if evq % 2 == 0:
                        S.act(lambda e, pq=pq, p=p, blk=blk, dstT=dstT, bcol=bcol: e.activation(out=dstT[:, p, blk * 512:(blk + 1) * 512], in_=bank(pq)[:, :], func=AF.Identity, bias=bcol[:, p:p + 1], scale=1.0),
                              reads=[PSK[pq], "bq", "bk"], writes=["%s.%d.%d" % (nm, p, blk)])
                    else:
                        S.dve(lambda e, pq=pq, p=p, blk=blk, dstT=dstT, bcol=bcol: e.tensor_scalar(out=dstT[:, p, blk * 512:(blk + 1) * 512], in0=bank(pq)[:, :], scalar1=bcol[:, p:p + 1], scalar2=None, op0=ALU.add),
                              reads=[PSK[pq], "bq", "bk"], writes=["%s.%d.%d" % (nm, p, blk)])
                    evq += 1
        for t in range(NT):
            pv = 4 + (t % 2)
            blk = t // 4
            for k in range(8):
                S.pe(lambda e, pv=pv, t=t, k=k: e.matmul(bank(pv)[:, :], lhsT=xT[:, k, t * 128:(t + 1) * 128], rhs=wB[:, k, 1024:1536], start=(k == 0), stop=False),
                     reads=["xT.%d.%d" % (k, blk), "wB"], writes=[PSK[pv]])
            S.pe(lambda e, pv=pv: e.matmul(bank(pv)[:, :], lhsT=ones_row[0:1, :], rhs=bv_row[0:1, :], start=False, stop=True),
                 reads=["ones_row", "bv_row"], writes=[PSK[pv]])
            if t % 2 == 0:
                S.act(lambda e, pv=pv, t=t: e.copy(v_aug[:, t, :, 0:64], bank(pv)[:, :].rearrange("p (h d) -> p h d", h=8)), reads=[PSK[pv]], writes=["v.%d" % t])
            else:
                S.dve(lambda e, pv=pv, t=t: e.tensor_copy(v_aug[:, t, :, 0:64], bank(pv)[:, :].rearrange("p (h d) -> p h d", h=8)), reads=[PSK[pv]], writes=["v.%d" % t])
            for k in range(8):
                S.pe(lambda e, t=t, k=k: e.matmul(bank(6)[:, t * 8:(t + 1) * 8], lhsT=xT[:, k, t * 128:(t + 1) * 128], rhs=wB[:, k, 1536:1544], start=(k == 0), stop=(k == 7)),
                     reads=["xT.%d.%d" % (k, blk), "wB"], writes=[PSK[6]])
        S.dve(lambda e: e.tensor_tensor(out=f_raw[:, :, :], in0=bank(6)[:, 0:128].rearrange("p (t h) -> p t h", h=8), in1=bdtf[:, 8:16].unsqueeze(1).to_broadcast([128, NT, 8]), op=ALU.add),
              reads=[PSK[6], "bdtf1"], writes=["f_raw"])
        S.act(lambda e: e.activation(out=f_raw[:, :, :], in_=f_raw[:, :, :], func=AF.Exp, scale=-1.0), reads=["f_raw"], writes=["f_raw"])
        S.act(lambda e: e.activation(out=f_raw[:, :, :], in_=f_raw[:, :, :], func=AF.Ln, bias=1.0, scale=1.0), reads=["f_raw"], writes=["f_raw"])
        frf = f_raw.rearrange("p t h -> p (t h)")
        S.pe(lambda e: e.matmul(bank(7)[:, 0:128], lhsT=Uf[:, :], rhs=frf, start=True, stop=True), reads=["Uf", "f_raw"], writes=[PSK[7]])
        S.pe(lambda e: e.matmul(bank(7)[:, 128:256], lhsT=onesF[:, :], rhs=frf, start=True, stop=True), reads=["onesF", "f_raw"], writes=[PSK[7]])
        S.dve(lambda e: e.tensor_copy(Gc.rearrange("p t h -> p (t h)"), bank(7)[:, 0:128]), reads=[PSK[7]], writes=["Gc"])
        S.dve(lambda e: e.tensor_copy(tot.rearrange("p t h -> p (t h)"), bank(7)[:, 128:256]), reads=[PSK[7]], writes=["tot"])
        S.pool(lambda e: e.memset(Pp[:, 0, :], 0.0), writes=["Pp"])
        for t in range(1, NT):
            S.dve(lambda e, t=t: e.tensor_tensor(out=Pp[:, t, :], in0=Pp[:, t - 1, :], in1=tot[:, t - 1, :], op=ALU.add), reads=["Pp", "tot"], writes=["Pp"])
        S.dve(lambda e: e.tensor_tensor(out=Gf[:, :, :], in0=Gc[:, :, :], in1=Pp[:, :, :], op=ALU.add), reads=["Gc", "Pp"], writes=["Gf"])
        S.dve(lambda e: e.tensor_tensor(out=Gend[:, :, :], in0=tot[:, :, :], in1=Pp[:, :, :], op=ALU.add), reads=["tot", "Pp"], writes=["Gend"])
        for h in range(8):
            S.dve(lambda e, h=h: e.tensor_tensor(out=biasT[:, h, :, :], in0=Gf[:, :, h].unsqueeze(1).to_broadcast([128, NT, NT]), in1=Gend[:, :, h].unsqueeze(2).to_broadcast([128, NT, NT]), op=ALU.subtract),
                  reads=["Gf", "Gend"], writes=["biasT"])

        fence()
        RX.reset()
        hT = RX.alloc([8, SEQ], BF16)
        NEXP = int(os.environ.get("KNEXP", 16))

        def load_expert(ex):
            sl = ex % 2
            S.dma("pool", lambda e, ex=ex, sl=sl: e.dma_start(out=wgu[sl][:, :, 0:512], in_=w_gate[ex].rearrange("(k p) f -> p k f", p=128)), writes=["wg.%d" % sl], nofence=(ex == 0))
            S.dma("pool", lambda e, ex=ex, sl=sl: e.dma_start(out=wgu[sl][:, :, 512:1024], in_=w_up[ex].rearrange("(k p) f -> p k f", p=128)), writes=["wu.%d" % sl], nofence=(ex == 0))
            S.dma("pool", lambda e, ex=ex, sl=sl: e.dma_start(out=wdn[sl][:, :, :], in_=w_down[ex].rearrange("(k p) d -> p k d", p=128)), writes=["wd.%d" % sl], nofence=(ex == 0))


        load_expert(0)
        NTI = int(os.environ.get("KATT_TILES", NT))
        S.fold_now = os.environ.get("KFOLD", "all") in ("attmoe", "all")
        units = []
        for i in range(NTI):
            for p in range(4):
                for j0 in range(0, i + 1, 4):
                    units.append((i, p, list(range(j0, min(j0 + 4, i + 1)))))

        def emit_S(u, gi):
            i, p, js = u
            pb0 = 2 * (gi % 2)
            for jj, j in enumerate(js):
                for hh in range(2):
                    r0 = hh * 64
                    S.pe(lambda e, bk=pb0 + hh, jj=jj, j=j, p=p, r0=r0, i=i: e.matmul(bank(bk)[:, jj * 128:(jj + 1) * 128], lhsT=kT[r0:r0 + 64, p, j * 128:(j + 1) * 128], rhs=qT[r0:r0 + 64, p, i * 128:(i + 1) * 128], start=True, stop=True),
                         reads=["kT.%d.%d" % (p, j // 4), "qT.%d.%d" % (p, i // 4)], writes=[PSK[pb0 + hh]])

        def emit_E(u, gi):
            i, p, js = u
            pb0 = 2 * (gi % 2); pb = gi % 2
            for jj, j in enumerate(js):
                for hh in range(2):
                    h = 2 * p + hh
                    c8 = jj * 2 + hh
                    S.act(lambda e, bk=pb0 + hh, pb=pb, c8=c8, jj=jj, j=j, h=h, i=i: e.activation(out=PT[pb][:, c8, :], in_=bank(bk)[:, jj * 128:(jj + 1) * 128], func=AF.Exp, bias=biasT[:, h, i, j:j + 1], scale=ATT_SCALE),
                          reads=[PSK[pb0 + hh], "biasT"], writes=["PT%d.%d" % (pb, c8)])
                    if j == i:
                        S.dve(lambda e, pb=pb, c8=c8: e.tensor_tensor(out=PT[pb][:, c8, :], in0=PT[pb][:, c8, :], in1=triU[:, :], op=ALU.mult),
                              reads=["PT%d.%d" % (pb, c8), "triU"], writes=["PT%d.%d" % (pb, c8)])

        def emit_V(u, gi):
            i, p, js = u
            pb = gi % 2
            for jj, j in enumerate(js):
                for hh in range(2):
                    h = 2 * p + hh
                    c8 = jj * 2 + hh
                    po = 4 + h // 4; oc = (h % 4) * 65
                    S.pe(lambda e, pb=pb, c8=c8, j=j, h=h, po=po, oc=oc, i=i: e.matmul(bank(po)[:, oc:oc + 65], lhsT=PT[pb][:, c8, :], rhs=v_aug[:, j, h, :], start=(j == 0 and h % 4 == 0), stop=(j == i and h % 4 == 3)),
                         reads=["PT%d.%d" % (pb, c8), "v.%d" % j, "v_ones"], writes=[PSK[po]])

        def tail_N(i):
            b2 = i % 2
            S.dma("sp", lambda e, i=i, b2=b2, s=s: e.dma_start(out=xres[b2][:, :], in_=x[s, i * 128:(i + 1) * 128, :]), writes=["xres%d" % b2])
            for hb in range(2):
                ov = bank(4 + hb)[:, 0:260].rearrange("p (h d) -> p h d", h=4)
                S.dve(lambda e, hb=hb, ov=ov, b2=b2: e.reciprocal(rden[b2][:, 4 * hb:4 * hb + 4], ov[:, :, 64]), reads=[PSK[4 + hb]], writes=["rden%d.%d" % (b2, hb)])
                S.dve(lambda e, hb=hb, ov=ov, b2=b2: e.tensor_tensor(out=yatt[b2][:, 4 * hb:4 * hb + 4, :], in0=ov[:, :, 0:64], in1=rden[b2][:, 4 * hb:4 * hb + 4].unsqueeze(2).to_broadcast([128, 4, 64]), op=ALU.mult),
                      reads=[PSK[4 + hb], "rden%d.%d" % (b2, hb)], writes=["yatt%d.%d" % (b2, hb)])

        def tail_T(i):
            b2 = i % 2
            yf = yatt[b2].rearrange("p h d -> p (h d)")
            for ec in range(8):
                src = m_ssd[:, i, ec * 128:(ec + 1) * 128] if ec < 4 else yf[:, (ec - 4) * 128:(ec - 3) * 128]
                rk = ["m_ssd.%d" % i] if ec < 4 else ["yatt%d.%d" % (b2, (ec - 4) // 2)]
                S.pe(lambda e, ec=ec, src=src: e.transpose(bankb(6)[:, ec * 128:(ec + 1) * 128], src, identB[:, :]), reads=rk + ["identB"], writes=[PSK[6]])
            S.dve(lambda e, b2=b2: e.tensor_copy(mT[b2].rearrange("p a b -> p (a b)"), bankb(6)[:, 0:1024]), reads=[PSK[6]], writes=["mT"])

        def tail_Oq(i, q):
            b2 = i % 2
            half = q // 2
            hp = hpre[b2]
            for ec in range(4 * (q % 2), 4 * (q % 2) + 4):
                S.pe(lambda e, ec=ec, b2=b2, half=half: e.matmul(bank(7)[:, :], lhsT=mT[b2][:, ec, :], rhs=wo[:, ec, half * 512:(half + 1) * 512], start=(ec == 0), stop=(ec == 7)),
                     reads=["mT", "wo"], writes=[PSK[7]])
            if q % 2 == 1:
                S.dve(lambda e, hp=hp, b2=b2, half=half: e.scalar_tensor_tensor(out=hp[:, half * 512:(half + 1) * 512], in0=xres[b2][:, half * 512:(half + 1) * 512], scalar=ALPHA, in1=bank(7)[:, :], op0=ALU.mult, op1=ALU.add),
                      reads=["xres%d" % b2, PSK[7]], writes=["hpre%d.%d" % (b2, half)])
                S.dve(lambda e, hp=hp, half=half, b2=b2: e.bn_stats(bst[b2][:, half, :], hp[:, half * 512:(half + 1) * 512]), reads=["hpre%d.%d" % (b2, half)], writes=["bst%d.%d" % (b2, half)])
            if q == 3:
                S.dve(lambda e, b2=b2: e.bn_aggr(mv[b2][:, :], bst[b2][:, :, :]), reads=["bst%d.0" % b2, "bst%d.1" % b2], writes=["mv%d" % b2])

        def tail_L(i):
            b2 = i % 2
            hp = hpre[b2]
            HK = ["hpre%d.0" % b2, "hpre%d.1" % b2]
            S.act(lambda e, b2=b2: e.activation(out=rs1[b2][:, :], in_=mv[b2][:, 1:2], func=AF.Ln, bias=LN_EPS, scale=1.0), reads=["mv%d" % b2], writes=["rs1%d" % b2])
            S.act(lambda e, b2=b2: e.activation(out=rs1[b2][:, :], in_=rs1[b2][:, :], func=AF.Exp, scale=-0.5), reads=["rs1%d" % b2], writes=["rs1%d" % b2])
            S.dve(lambda e, hp=hp, b2=b2: e.tensor_scalar(out=hp[:, :], in0=hp[:, :], scalar1=mv[b2][:, 0:1], scalar2=rs1[b2][:, 0:1], op0=ALU.subtract, op1=ALU.mult),
                  reads=HK + ["mv%d" % b2, "rs1%d" % b2], writes=HK)
            S.dve(lambda e, hp=hp: e.tensor_tensor(out=hp[:, :], in0=hp[:, :], in1=lnG[:, :], op=ALU.mult), reads=HK + ["lnG"], writes=HK)
            S.dve(lambda e, hp=hp: e.tensor_tensor(out=hp[:, :], in0=hp[:, :], in1=lnB[:, :], op=ALU.add), reads=HK + ["lnB"], writes=HK)
            S.dve(lambda e, hp=hp: e.tensor_scalar(out=ah[:, :], in0=hp[:, :], scalar1=ALPHA, scalar2=None, op0=ALU.mult), reads=HK, writes=["ah"])
            S.dma("sp", lambda e, i=i, s=s: e.dma_start(out=h_scr[s, i * 128:(i + 1) * 128, :], in_=ah[:, :]), reads=["ah"], writes=["h_scr.%d" % i])

        def tail_H(i, half):
            b2 = i % 2
            hp = hpre[b2]
            for q4 in range(4):
                ec = half * 4 + q4
                S.pe(lambda e, q4=q4, ec=ec, hp=hp: e.transpose(bank(6)[:, q4 * 128:(q4 + 1) * 128], hp[:, ec * 128:(ec + 1) * 128], identF[:, :]), reads=["hpre%d.%d" % (b2, half), "identF"], writes=[PSK[6]])
            S.dve(lambda e, half=half, i=i: e.tensor_copy(hT[:, 4 * half:4 * half + 4, i * 128:(i + 1) * 128], bank(6)[:, :].rearrange("p (a b) -> p a b", a=4)), reads=[PSK[6]], writes=["hT.%d.%d" % (i, half)])
            S.dve(lambda e, half=half: e.tensor_copy(hTf[:, 4 * half:4 * half + 4, :], bank(6)[:, :].rearrange("p (a b) -> p a b", a=4)), reads=[PSK[6]], writes=["hTf.%d" % half])

        def tail_R(i, part):
            for ec in range(4 * part, 4 * part + 4):
                S.pe(lambda e, ec=ec: e.matmul(bank(6)[:, 0:20], lhsT=hTf[:, ec, :], rhs=rw[:, ec, :], start=(ec == 0), stop=(ec == 7)), reads=["hTf.%d" % (ec // 4)] + RWK, writes=[PSK[6]])
            if part == 1:
                S.dve(lambda e, i=i: e.tensor_tensor(out=logits[:, i, :], in0=bank(6)[:, 0:20], in1=rb_bc[:, :], op=ALU.add), reads=[PSK[6], "rb0", "rb1"], writes=["logits.%d" % i])

        TD = [int(v) for v in os.environ.get("KTAIL", "0,1,2,3,4,5,9,11,12,14").split(",")]
        TAIL = [(TD[0], tail_N), (TD[1], tail_T), (TD[2], lambda i: tail_Oq(i, 0)), (TD[3], lambda i: tail_Oq(i, 1)), (TD[4], lambda i: tail_Oq(i, 2)), (TD[5], lambda i: tail_Oq(i, 3)),
                (TD[6], tail_L), (TD[7], lambda i: tail_H(i, 0)), (TD[8], lambda i: tail_H(i, 1)), (TD[9], lambda i: (tail_R(i, 0), tail_R(i, 1)))]
        NSTEP = len(TAIL)
        done_steps = set()

        def emit_step(t, k):
            if t < 0 or (t, k) in done_steps:
                return
            for kk in range(k):
                emit_step(t, kk)
            emit_step(t - 1, k)
            if k == 1:
                emit_step(t - 1, 5)
            if k == 7:
                emit_step(t - 1, NSTEP - 1)
            if k == 0:
                for kk in range(NSTEP):
                    emit_step(t - 2, kk)
            done_steps.add((t, k))
            TAIL[k][1](t)

        pending = []
        def tick():
            keep = []
            for ent in pending:
                if ent[0] <= 0:
                    emit_step(ent[1], ent[2])
                else:
                    ent[0] -= 1
                    keep.append(ent)
            pending[:] = keep
        prev = None
        for gi, u in enumerate(units):
            emit_S(u, gi)
            emit_E(u, gi)
            if prev is not None:
                emit_V(prev[0], prev[1])
                if prev[0][0] != u[0]:
                    for k, (dly, fn) in enumerate(TAIL):
                        pending.append([dly, prev[0][0], k])
            tick()
            prev = (u, gi)
        if prev is not None:
            emit_V(prev[0], prev[1])
            for k, (dly, fn) in enumerate(TAIL):
                pending.append([dly, prev[0][0], k])
        while pending:
            tick()

        if dbg == "att" and s == 0:
            fence()
            RACC.reset()
            cvt = RACC.alloc([NT * 1024], BF16)
            d1 = dbg_out("dbg_hT", [128, 8 * SEQ]); d2 = dbg_out("dbg_logits", [128, NT * 20])
            cv2 = RACC.alloc([2 * SEQ], F32)
            for q in range(4):
                S.dve(lambda e, q=q: e.tensor_copy(cv2[:, :], hT[:, 2 * q:2 * q + 2, :].rearrange("p a b -> p (a b)")), reads=[], writes=["cv2"])
                outs.append(S.dma("sp", lambda e, q=q: e.dma_start(out=d1[:, 2 * q * SEQ:(2 * q + 2) * SEQ], in_=cv2[:, :]), reads=["cv2"]))
            outs.append(S.dma("sp", lambda e: e.dma_start(out=d2[:, :], in_=logits.rearrange("p t j -> p (t j)")), reads=["logits.%d" % i for i in range(int(os.environ.get("KATT_TILES", NT)))] if int(os.environ.get("KATT_STAGE", 9)) >= 7 else []))
            break


        S.fold_now = os.environ.get("KFOLD", "all") == "all"
        fence()
        TT.reset(); RW.reset(); RACC.reset()
        S.dma("sp", lambda e: e.dma_start(out=lnG[:, :], in_=ln2_g[0:1, :].to_broadcast([128, 1024])), writes=["lnG"])
        S.dma("sp", lambda e: e.dma_start(out=lnB[:, :], in_=ln2_b[0:1, :].to_broadcast([128, 1024])), writes=["lnB"])
        LOGK = ["logits.%d" % i for i in range(NT)]
        lg = logits[:, :, 0:4]
        le4 = logits[:, :, 4:20].rearrange("p t (g j) -> p t g j", g=4)
        gmax = TT.alloc([NT], F32); goh = TT.alloc([NT, 4], F32); gex = TT.alloc([NT, 4], F32)
        gsum = TT.alloc([NT], F32); gval = TT.alloc([NT], F32)
        tmp16 = TT.alloc([NT, 4, 4], F32); esel = TT.alloc([NT, 4], F32)
        m1 = TT.alloc([NT], F32); oh1 = TT.alloc([NT, 4], F32); e2 = TT.alloc([NT, 4], F32)
        m2 = TT.alloc([NT], F32); oh2 = TT.alloc([NT, 4], F32); dd = TT.alloc([NT], F32)
        w1 = TT.alloc([NT], F32); w2 = TT.alloc([NT], F32); cw1 = TT.alloc([NT], F32); cw2 = TT.alloc([NT], F32)
        cj = TT.alloc([NT, 4], F32); cj2 = TT.alloc([NT, 4], F32)
        bc4 = lambda a: a.unsqueeze(2).to_broadcast([128, NT, 4])
        S.dve(lambda e: e.tensor_reduce(out=gmax[:, :], in_=lg, axis=AX.X, op=ALU.max), reads=LOGK, writes=["gmax"])
        S.dve(lambda e: e.tensor_tensor(out=goh[:, :, :], in0=lg, in1=bc4(gmax[:, :]), op=ALU.is_equal), reads=LOGK + ["gmax"], writes=["goh"])
        S.dve(lambda e: e.tensor_tensor(out=gex[:, :, :], in0=lg, in1=bc4(gmax[:, :]), op=ALU.subtract), reads=LOGK + ["gmax"], writes=["gex"])
        S.act(lambda e: e.activation(out=gex[:, :, :], in_=gex[:, :, :], func=AF.Exp), reads=["gex"], writes=["gex"])
        S.dve(lambda e: e.tensor_reduce(out=gsum[:, :], in_=gex[:, :, :], axis=AX.X, op=ALU.add), reads=["gex"], writes=["gsum"])
        S.dve(lambda e: e.reciprocal(gval[:, :], gsum[:, :]), reads=["gsum"], writes=["gval"])
        S.dve(lambda e: e.tensor_tensor(out=tmp16[:, :, :, :], in0=le4, in1=goh[:, :, :].unsqueeze(3).to_broadcast([128, NT, 4, 4]), op=ALU.mult), reads=LOGK + ["goh"], writes=["tmp16"])
        S.dve(lambda e: e.tensor_reduce(out=esel[:, :, :], in_=tmp16.rearrange("p t g j -> p t j g"), axis=AX.X, op=ALU.add), reads=["tmp16"], writes=["esel"])
        S.dve(lambda e: e.tensor_reduce(out=m1[:, :], in_=esel[:, :, :], axis=AX.X, op=ALU.max), reads=["esel"], writes=["m1"])
        S.dve(lambda e: e.tensor_tensor(out=oh1[:, :, :], in0=esel[:, :, :], in1=bc4(m1[:, :]), op=ALU.is_equal), reads=["esel", "m1"], writes=["oh1"])
        S.dve(lambda e: e.scalar_tensor_tensor(out=e2[:, :, :], in0=oh1[:, :, :], scalar=-1e30, in1=esel[:, :, :], op0=ALU.mult, op1=ALU.add), reads=["oh1", "esel"], writes=["e2"])
        S.dve(lambda e: e.tensor_reduce(out=m2[:, :], in_=e2[:, :, :], axis=AX.X, op=ALU.max), reads=["e2"], writes=["m2"])
        S.dve(lambda e: e.tensor_tensor(out=oh2[:, :, :], in0=e2[:, :, :], in1=bc4(m2[:, :]), op=ALU.is_equal), reads=["e2", "m2"], writes=["oh2"])
        S.dve(lambda e: e.tensor_tensor(out=dd[:, :], in0=m2[:, :], in1=m1[:, :], op=ALU.subtract), reads=["m1", "m2"], writes=["dd"])
        S.act(lambda e: e.activation(out=dd[:, :], in_=dd[:, :], func=AF.Exp), reads=["dd"], writes=["dd"])
        S.dve(lambda e: e.tensor_scalar(out=w1[:, :], in0=dd[:, :], scalar1=1.0, scalar2=None, op0=ALU.add), reads=["dd"], writes=["w1"])
        S.dve(lambda e: e.reciprocal(w1[:, :], w1[:, :]), reads=["w1"], writes=["w1"])
        S.dve(lambda e: e.tensor_tensor(out=w2[:, :], in0=dd[:, :], in1=w1[:, :], op=ALU.mult), reads=["dd", "w1"], writes=["w2"])
        S.dve(lambda e: e.tensor_tensor(out=cw1[:, :], in0=gval[:, :], in1=w1[:, :], op=ALU.mult), reads=["gval", "w1"], writes=["cw1"])
        S.dve(lambda e: e.tensor_tensor(out=cw2[:, :], in0=gval[:, :], in1=w2[:, :], op=ALU.mult), reads=["gval", "w2"], writes=["cw2"])
        S.dve(lambda e: e.tensor_tensor(out=cj[:, :, :], in0=oh1[:, :, :], in1=bc4(cw1[:, :]), op=ALU.mult), reads=["oh1", "cw1"], writes=["cj"])
        S.dve(lambda e: e.tensor_tensor(out=cj2[:, :, :], in0=oh2[:, :, :], in1=bc4(cw2[:, :]), op=ALU.mult), reads=["oh2", "cw2"], writes=["cj2"])
        S.dve(lambda e: e.tensor_tensor(out=cj[:, :, :], in0=cj[:, :, :], in1=cj2[:, :, :], op=ALU.add), reads=["cj", "cj2"], writes=["cj"])
        S.dve(lambda e: e.tensor_tensor(out=comb.rearrange("p t (g j) -> p t g j", g=4), in0=goh[:, :, :].unsqueeze(3).to_broadcast([128, NT, 4, 4]), in1=cj[:, :, :].unsqueeze(2).to_broadcast([128, NT, 4, 4]), op=ALU.mult),
              reads=["goh", "cj"], writes=["comb"])

        S.fold_now = os.environ.get("KFOLD", "all") in ("moe", "attmoe", "all")
        acc = RACC.alloc([NT, 1024], F32)
        sg = [TT.alloc([512], BF16) for _ in range(2)]
        actT = [TT.alloc([4, 512], BF16) for _ in range(2)]
        obuf = [TT.alloc([1024], F32) for _ in range(2)]
        bst2 = [TT.alloc([2, 6], F32) for _ in range(2)]
        mv2 = [TT.alloc([2], F32) for _ in range(2)]
        rs2 = [TT.alloc([1], F32) for _ in range(2)]
        for q in range(4):
            S.dma("sp", lambda e, q=q, s=s: e.dma_start(out=acc[:, 4 * q:4 * q + 4, :], in_=h_scr[s, q * 512:(q + 1) * 512, :].rearrange("(t p) d -> p t d", p=128)),
                  reads=["h_scr.%d" % t for t in range(4 * q, 4 * q + 4)], writes=["acc.%d" % t for t in range(4 * q, 4 * q + 4)])
        def ln2_stats(t):
            b2 = t % 2
            for c2 in range(2):
                S.dve(lambda e, c2=c2, t=t, b2=b2: e.bn_stats(bst2[b2][:, c2, :], acc[:, t, c2 * 512:(c2 + 1) * 512]), reads=["acc.%d" % t], writes=["bst2%d.%d" % (b2, c2)])
            S.dve(lambda e, b2=b2: e.bn_aggr(mv2[b2][:, :], bst2[b2][:, :, :]), reads=["bst2%d.0" % b2, "bst2%d.1" % b2], writes=["mv2%d" % b2])

        def ln2_apply(t):
            b2 = t % 2
            S.act(lambda e, b2=b2: e.activation(out=rs2[b2][:, :], in_=mv2[b2][:, 1:2], func=AF.Ln, bias=LN_EPS, scale=1.0), reads=["mv2%d" % b2], writes=["rs2%d" % b2])
            S.act(lambda e, b2=b2: e.activation(out=rs2[b2][:, :], in_=rs2[b2][:, :], func=AF.Exp, scale=-0.5), reads=["rs2%d" % b2], writes=["rs2%d" % b2])
            S.dve(lambda e, t=t, b2=b2: e.tensor_scalar(out=obuf[b2][:, :], in0=acc[:, t, :], scalar1=mv2[b2][:, 0:1], scalar2=rs2[b2][:, 0:1], op0=ALU.subtract, op1=ALU.mult),
                  reads=["acc.%d" % t, "mv2%d" % b2, "rs2%d" % b2], writes=["obuf%d" % b2])
            S.dve(lambda e, b2=b2: e.tensor_tensor(out=obuf[b2][:, :], in0=obuf[b2][:, :], in1=lnG[:, :], op=ALU.mult), reads=["obuf%d" % b2, "lnG"], writes=["obuf%d" % b2])
            S.dve(lambda e, b2=b2: e.tensor_tensor(out=obuf[b2][:, :], in0=obuf[b2][:, :], in1=lnB[:, :], op=ALU.add), reads=["obuf%d" % b2, "lnB"], writes=["obuf%d" % b2])
            outs.append(S.dma("sp", lambda e, t=t, b2=b2, s=s: e.dma_start(out=out[s, t * 128:(t + 1) * 128, :], in_=obuf[b2][:, :]), reads=["obuf%d" % b2], writes=["out.%d.%d" % (s, t)]))

        ln2_prev = []
        if NEXP > 1:
            load_expert(1)
        cg = 0; cd = 0
        for ex in range(NEXP):
            sl = ex % 2
            for tb in range(4):
                ab = (ex * 4 + tb) % 2
                hk = ["hT.%d.%d" % (i, hf) for i in range(4 * tb, 4 * tb + 4) for hf in range(2)]
                for fc in range(4):
                    pg = cg % 2; cg += 1
                    for k in range(8):
                        S.pe(lambda e, pg=pg, k=k, fc=fc, tb=tb, sl=sl: e.matmul(bank(pg)[:, :], lhsT=wgu[sl][:, k, fc * 128:(fc + 1) * 128], rhs=hT[:, k, tb * 512:(tb + 1) * 512], start=(k == 0), stop=(k == 7)),
                             reads=["wg.%d" % sl] + hk, writes=[PSK[pg]])
                    for k in range(8):
                        S.pe(lambda e, pg=pg, k=k, fc=fc, tb=tb, sl=sl: e.matmul(bank(2 + pg)[:, :], lhsT=wgu[sl][:, k, 512 + fc * 128:512 + (fc + 1) * 128], rhs=hT[:, k, tb * 512:(tb + 1) * 512], start=(k == 0), stop=(k == 7)),
                             reads=["wu.%d" % sl] + hk, writes=[PSK[2 + pg]])
                    S.act(lambda e, pg=pg: e.activation(out=sg[pg][:, :], in_=bank(pg)[:, :], func=AF.Silu), reads=[PSK[pg]], writes=["sg%d" % pg])
                    S.dve(lambda e, pg=pg, ab=ab, fc=fc: e.tensor_tensor(out=actT[ab][:, fc, :], in0=bank(2 + pg)[:, :], in1=sg[pg][:, :], op=ALU.mult),
                          reads=[PSK[2 + pg], "sg%d" % pg], writes=["actT%d.%d" % (ab, fc)])
                for tt in range(4):
                    t = tb * 4 + tt
                    pdi = 2 + (cd % 2); cd += 1
                    for half in range(2):
                        for fc in range(4):
                            S.pe(lambda e, pdi=pdi, half=half, fc=fc, ab=ab, tt=tt, sl=sl: e.matmul(pd[pdi][:, half * 512:(half + 1) * 512], lhsT=actT[ab][:, fc, tt * 128:(tt + 1) * 128], rhs=wdn[sl][:, fc, half * 512:(half + 1) * 512], start=(fc == 0), stop=(fc == 3)),
                                 reads=["actT%d.%d" % (ab, fc), "wd.%d" % sl], writes=[PSK[2 * pdi + half]])
                    S.dve(lambda e, pdi=pdi, t=t, ex=ex: e.scalar_tensor_tensor(out=acc[:, t, :], in0=pd[pdi][:, :], scalar=comb[:, t, ex:ex + 1], in1=acc[:, t, :], op0=ALU.mult, op1=ALU.add),
                          reads=[PSK[2 * pdi], PSK[2 * pdi + 1], "comb", "acc.%d" % t], writes=["acc.%d" % t])
                    if ex == NEXP - 1:
                        ln2_stats(t)
                        if ln2_prev:
                            ln2_apply(ln2_prev.pop())
                        ln2_prev.append(t)
            if ex + 2 < NEXP:
                load_expert(ex + 2)
        while ln2_prev:
            ln2_apply(ln2_prev.pop())
        S.fold_now = os.environ.get("KFOLD", "all") == "all"
        if s + 1 < NSEQ:
            fence()
        if dbg == "one":
            break

    with nc.allow_non_contiguous_dma(reason="tiny constant loads"):
        st = S.emit(outs)
    return nc, st, dbg_t


_CACHE = {}


def _get_program():
    if "p" not in _CACHE:
        _CACHE["p"] = build_program(dbg=os.environ.get("KDBG", ""))
    return _CACHE["p"]


def kernel(**inputs):
    nc, st, dbg_t = _get_program()
    f = lambda a: np.ascontiguousarray(np.asarray(a, dtype=np.float32))
    x = f(inputs["x"])
    shared = {
        "w_in": f(inputs["w_in"])[0], "b_in": f(inputs["b_in"]).reshape(1, DIN),
        "conv_w": f(inputs["conv_w"])[0], "conv_b": f(inputs["conv_b"]).reshape(1, 1024),
        "a_log": f(inputs["a_log"]).reshape(1, 8), "d_skip": f(inputs["d_skip"]).reshape(1, 8),
        "ssd_norm_g": f(inputs["ssd_norm_g"]).reshape(1, 512), "w_out": f(inputs["w_out"])[0],
        "ln1_g": f(inputs["ln1_g"]).reshape(1, DM), "ln1_b": f(inputs["ln1_b"]).reshape(1, DM),
        "router_group_w": f(inputs["router_group_w"])[0], "router_group_b": f(inputs["router_group_b"]).reshape(1, 4),
        "router_expert_w": f(inputs["router_expert_w"])[0], "router_expert_b": f(inputs["router_expert_b"]).reshape(1, 16),
        "w_gate": f(inputs["w_gate"])[0], "w_up": f(inputs["w_up"])[0], "w_down": f(inputs["w_down"])[0],
        "ln2_g": f(inputs["ln2_g"]).reshape(1, DM), "ln2_b": f(inputs["ln2_b"]).reshape(1, DM),
    }
    ncores = int(os.environ.get("KCORES", NCORES))
    in_maps = []
    for c in range(ncores):
        m = dict(shared)
        m["x"] = np.ascontiguousarray(x[c * NSEQ:(c + 1) * NSEQ])
        in_maps.append(m)
    res = run_bass_kernel_spmd(nc, in_maps, core_ids=list(range(ncores)))
    if os.environ.get("KDBG", ""):
        _CACHE["dbg"] = res.results
    outp = np.concatenate([r["out"] for r in res.results], axis=0)
    return outp.astype(np.float32)
```

```python
import os
import numpy as np
import concourse.bass as bass
import concourse.mybir as mybir
from concourse.bass_utils import run_bass_kernel_spmd

F32 = mybir.dt.float32
BF16 = mybir.dt.bfloat16
U8 = mybir.dt.uint8
AF = mybir.ActivationFunctionType
ALU = mybir.AluOpType
AX = mybir.AxisListType

NCORES = 8
NSEQ = 2
SEQ = 2048
NT = 16
DM = 1024
DIN = 3088
ALPHA = float(2.0 ** 0.25)
LN_EPS = 1e-5
RMS_EPS = 1e-5
ATT_SCALE = 0.125
NEG = -30000.0
FOLD_ENG = tuple(os.environ.get("KFOLDENG", "act,dve").split(","))


class Op:
    __slots__ = ("eng", "fn", "reads", "writes", "deps", "signal", "sigval", "dma", "gi", "nofence", "fold")


class Sched:
    COMPUTE = ("pe", "act", "dve", "pool")

    def __init__(self, nc, n_dma_sems=40):
        self.nc = nc
        self.h = {"pe": nc.tensor, "act": nc.scalar, "dve": nc.vector, "pool": nc.gpsimd, "sp": nc.sync}
        self.ops = []
        self.last_w = {}
        self.readers = {}
        self.n_dma_sems = n_dma_sems
        self.live_dma = []
        self.nfence = 0
        self.fold_now = os.environ.get("KFOLD", "all") == "all"

    def add(self, eng, fn, reads=(), writes=(), dma=False, nofence=False):
        o = Op()
        o.eng = eng; o.fn = fn; o.reads = tuple(reads); o.writes = tuple(writes)
        o.deps = []; o.signal = False; o.sigval = None; o.dma = dma; o.gi = len(self.ops); o.nofence = nofence
        o.fold = self.fold_now
        for r in o.reads:
            p = self.last_w.get(r)
            if p is not None:
                self._dep(o, p, True)
            if r.startswith("ps"):
                rd = self.readers.get(r)
                if rd:
                    for q in rd.values():
                        if q.eng != eng:
                            self._dep(o, q, True)
        for w in o.writes:
            p = self.last_w.get(w)
            if p is not None:
                self._dep(o, p, False)
            rd = self.readers.get(w)
            if rd:
                for q in rd.values():
                    self._dep(o, q, False)
        for r in o.reads:
            d = self.readers.setdefault(r, {})
            d[("dma", o.gi) if dma else eng] = o
        for w in o.writes:
            self.last_w[w] = o
            self.readers[w] = {}
        self.ops.append(o)
        if dma and not nofence:
            self.live_dma.append(o)
        return o

    def _dep(self, o, p, raw):
        if p is o:
            return
        if (not p.dma) and (not o.dma) and p.eng == o.eng:
            if o.eng == "pe":
                return
        o.deps.append(p)
        p.signal = True

    def pe(self, fn, reads=(), writes=()): return self.add("pe", fn, reads, writes)
    def act(self, fn, reads=(), writes=()): return self.add("act", fn, reads, writes)
    def dve(self, fn, reads=(), writes=()): return self.add("dve", fn, reads, writes)
    def pool(self, fn, reads=(), writes=()): return self.add("pool", fn, reads, writes)
    def dma(self, q, fn, reads=(), writes=(), nofence=False):
        return self.add(q, fn, reads, writes, dma=True, nofence=nofence)

    def fence(self, scratch):
        n = self.nfence; self.nfence += 1
        a_keys = []
        col = {"pe": None, "act": 0, "dve": 1, "pool": 2}
        for e in ("act", "dve", "pool"):
            k = "fenceA.%d.%s" % (n, e)
            c = col[e]
            if e == "act":
                self.add(e, (lambda eh, c=c: eh.activation(out=scratch[:, c:c + 1], in_=scratch[:, 8:9], func=AF.Copy)), reads=(), writes=(k,))
            else:
                self.add(e, (lambda eh, c=c: eh.memset(scratch[:, c:c + 1], 0.0)), reads=(), writes=(k,))
            a_keys.append(k)
        k = "fenceA.%d.pe" % n
        self.add("pe", (lambda eh: eh.matmul(self.fence_ps[0:1, 0:1], lhsT=self.fence_w[0:1, 0:1], rhs=self.fence_w[0:1, 0:1], start=True, stop=True)),
                 reads=(), writes=(k, "ps7"))
        a_keys.append(k)
        dmas = self.live_dma
        self.live_dma = []
        for e in ("act", "dve", "pool", "pe", "sp"):
            kb = "fenceB.%d.%s" % (n, e)
            if e == "act":
                o = self.add(e, (lambda eh: eh.activation(out=scratch[:, 3:4], in_=scratch[:, 8:9], func=AF.Copy)), reads=a_keys, writes=(kb,))
            elif e == "pe":
                o = self.add(e, (lambda eh: eh.matmul(self.fence_ps[0:1, 1:2], lhsT=self.fence_w[0:1, 0:1], rhs=self.fence_w[0:1, 0:1], start=True, stop=True)),
                             reads=a_keys, writes=(kb, "ps7"))
            elif e == "sp":
                o = self.add(e, (lambda eh: eh.nop()), reads=a_keys, writes=(kb,))
            else:
                c = 4 if e == "dve" else 5
                o = self.add(e, (lambda eh, c=c: eh.memset(scratch[:, c:c + 1], 0.0)), reads=a_keys, writes=(kb,))
            for d in dmas:
                o.deps.append(d)

    def emit(self, final_wait_ops=()):
        nc = self.nc
        esem = {e: nc.alloc_semaphore("s_" + e) for e in self.COMPUTE}
        dsems = [nc.alloc_semaphore("s_dma%d" % i) for i in range(self.n_dma_sems)]
        dtotal = [0] * self.n_dma_sems
        dlast = [None] * self.n_dma_sems
        ecount = {e: 0 for e in self.COMPUTE}
        nd = 0
        nq = {"sp": 0, "pool": 0}
        half = self.n_dma_sems // 2
        for o in self.ops:
            if o.dma:
                qi = nq[o.eng]; nq[o.eng] += 1; nd += 1
                i = (qi % half) + (0 if o.eng == "sp" else half)
                prev = dlast[i]
                if prev is not None:
                    o.deps.append(prev)
                dtotal[i] += 16
                o.sigval = (dsems[i], dtotal[i], 1000 + i)
                dlast[i] = o
            elif o.signal:
                ecount[o.eng] += 1
                o.sigval = (esem[o.eng], ecount[o.eng], o.eng)
        known = {e: {} for e in self.h}
        nwaits = 0
        snaps = {}
        for o in self.ops:
            eh = self.h[o.eng]
            kn = known[o.eng]
            deps = sorted(o.deps, key=lambda p: -p.gi)
            todo = []
            for p in deps:
                s, v, key = p.sigval
                if kn.get(key, 0) >= v:
                    continue
                todo.append((s, v, key))
                kn[key] = v
                sn = snaps.get(p.gi)
                if sn:
                    for k2, v2 in sn.items():
                        if kn.get(k2, 0) < v2:
                            kn[k2] = v2
            fold = None
            if todo and o.fold and (o.eng == "pe" or (o.eng in FOLD_ENG and not o.dma)):
                fold = todo.pop()
            for (s, v, key) in todo:
                eh.wait_ge(s, v)
                nwaits += 1
            ins = o.fn(eh)
            if fold is not None:
                ins._wait_ge(fold[0], fold[1])
            if o.dma:
                ins.then_inc(o.sigval[0], 16)
                snaps[o.gi] = dict(kn)
            elif o.signal:
                ins.then_inc(o.sigval[0], 1)
                snaps[o.gi] = dict(kn)
        eh = self.h["sp"]
        for o in final_wait_ops:
            s, v, key = o.sigval
            eh.wait_ge(s, v)
        self.stats = dict(n_ops=len(self.ops), n_waits=nwaits, counts=dict(ecount), n_dma=nd)
        return self.stats


class Arena:
    def __init__(self, nc, name, nbytes):
        self.t = nc.alloc_sbuf_tensor(name, [128, nbytes], U8)
        self.n = nbytes
        self.off = 0

    def reset(self, off=0):
        self.off = off

    def alloc(self, shape, dtype, parts=128):
        esz = 2 if dtype == BF16 else 4
        n = esz
        for s in shape:
            n *= s
        off = (self.off + 31) // 32 * 32
        assert off + n <= self.n, (off, n, self.n)
        self.off = off + n
        flat = self.t[0:parts, off:off + n].bitcast(dtype)
        if len(shape) == 1:
            return flat
        names = " ".join("a%d" % i for i in range(len(shape)))
        kw = {"a%d" % i: shape[i] for i in range(1, len(shape))}
        return flat.rearrange("p (%s) -> p %s" % (names, names), **kw)


def build_program(dbg=False):
    nc = bass.Bass("TRN2", target_bir_lowering=False)
    S = Sched(nc)
    D = {}

    def din(name, shape):
        D[name] = nc.dram_tensor(name, list(shape), F32, kind="ExternalInput").ap()
        return D[name]

    x = din("x", [NSEQ, SEQ, DM])
    w_in = din("w_in", [DM, DIN])
    b_in = din("b_in", [1, DIN])
    conv_w = din("conv_w", [4, 1024])
    conv_b = din("conv_b", [1, 1024])
    a_log = din("a_log", [1, 8])
    d_skip = din("d_skip", [1, 8])
    ssd_g = din("ssd_norm_g", [1, 512])
    w_out = din("w_out", [DM, DM])
    ln1_g = din("ln1_g", [1, DM]); ln1_b = din("ln1_b", [1, DM])
    rg_w = din("router_group_w", [DM, 4]); rg_b = din("router_group_b", [1, 4])
    re_w = din("router_expert_w", [4, DM, 4]); re_b = din("router_expert_b", [1, 16])
    w_gate = din("w_gate", [16, DM, 512]); w_up = din("w_up", [16, DM, 512]); w_down = din("w_down", [16, 512, DM])
    ln2_g = din("ln2_g", [1, DM]); ln2_b = din("ln2_b", [1, DM])
    out = nc.dram_tensor("out", [NSEQ, SEQ, DM], F32, kind="ExternalOutput").ap()
    h_scr = nc.dram_tensor("h_scr", [NSEQ, SEQ, DM], F32).ap()
    dbg_t = {}

    def dbg_out(name, shape):
        dbg_t[name] = nc.dram_tensor(name, list(shape), F32, kind="ExternalOutput").ap()
        return dbg_t[name]

    CONST = Arena(nc, "CONST", 22 * 1024)
    RW = Arena(nc, "RW", 49408)
    RX = Arena(nc, "RX", 32768)
    RACC = Arena(nc, "RACC", 65536)
    MSSD = Arena(nc, "MSSD", 16384)
    TT = Arena(nc, "TT", nc.sbuf_bytes_remaining - 256)

    identB = CONST.alloc([128], BF16); identF = CONST.alloc([128], F32)
    Uf = CONST.alloc([128], F32); triU = CONST.alloc([128], BF16); maskneg = CONST.alloc([128], BF16)
    onesF = CONST.alloc([128], F32)
    ones_row = CONST.alloc([128], BF16, parts=1)
    bz_row = CONST.alloc([512], BF16, parts=1); bv_row = CONST.alloc([512], BF16, parts=1)
    bdtf = CONST.alloc([16], F32)
    bxbc = CONST.alloc([8], F32); bq = CONST.alloc([4], F32); bk = CONST.alloc([4], F32)
    convw = CONST.alloc([8, 4], F32); convb = CONST.alloc([8], F32)
    a_bc = CONST.alloc([8], F32); dskip_bc = CONST.alloc([8], F32)
    gssd_bc = CONST.alloc([512], F32)
    lnG = CONST.alloc([1024], F32); lnB = CONST.alloc([1024], F32)
    rw = CONST.alloc([8, 20], F32); rb_bc = CONST.alloc([20], F32)
    logits = CONST.alloc([NT, 20], F32); comb = CONST.alloc([NT, 16], F32)
    fsc = CONST.alloc([16], F32)
    S.fence_w = CONST.alloc([8], BF16)
    pd = [nc.alloc_psum_tensor("pd%d" % i, [128, 1024], F32) for i in range(4)]
    def bank(i):
        return pd[i // 2][:, (i % 2) * 512:(i % 2) * 512 + 512]
    def bankb(i):
        return bank(i).bitcast(BF16)
    PSK = ["ps%d" % i for i in range(8)]
    S.fence_ps = nc.alloc_sbuf_tensor("fence_dummy", [1, 8], F32)
    S.fence_ps = bank(7)[:, 504:512]

    def fence():
        S.fence(fsc)

    S.pool(lambda e: e.memset(fsc[:, :], 0.0), writes=["fsc"])
    S.pool(lambda e: e.memset(S.fence_w[:, :], 0.0), writes=["fence_w"])
    S.pool(lambda e: e.memset(identB[:, :], 1.0), writes=["identB"])
    S.pool(lambda e: e.affine_select(out=identB[:, :], in_=identB[:, :], pattern=[[-1, 128]], compare_op=ALU.is_equal, fill=0.0, base=0, channel_multiplier=1), reads=["identB"], writes=["identB"])
    S.pool(lambda e: e.memset(identF[:, :], 1.0), writes=["identF"])
    S.pool(lambda e: e.affine_select(out=identF[:, :], in_=identF[:, :], pattern=[[-1, 128]], compare_op=ALU.is_equal, fill=0.0, base=0, channel_multiplier=1), reads=["identF"], writes=["identF"])
    S.pool(lambda e: e.memset(Uf[:, :], 1.0), writes=["Uf"])
    S.pool(lambda e: e.affine_select(out=Uf[:, :], in_=Uf[:, :], pattern=[[1, 128]], compare_op=ALU.is_ge, fill=0.0, base=0, channel_multiplier=-1), reads=["Uf"], writes=["Uf"])
    S.pool(lambda e: e.memset(triU[:, :], 1.0), writes=["triU"])
    S.pool(lambda e: e.affine_select(out=triU[:, :], in_=triU[:, :], pattern=[[1, 128]], compare_op=ALU.is_ge, fill=0.0, base=0, channel_multiplier=-1), reads=["triU"], writes=["triU"])
    S.pool(lambda e: e.memset(maskneg[:, :], NEG), writes=["maskneg"])
    S.pool(lambda e: e.affine_select(out=maskneg[:, :], in_=maskneg[:, :], pattern=[[-1, 128]], compare_op=ALU.is_gt, fill=0.0, base=0, channel_multiplier=1), reads=["maskneg"], writes=["maskneg"])
    S.pool(lambda e: e.memset(onesF[:, :], 1.0), writes=["onesF"])
    S.pool(lambda e: e.memset(ones_row[:, :], 1.0), writes=["ones_row"])
    S.dma("pool", lambda e: e.dma_start(out=bz_row[:, :], in_=b_in[0:1, 0:512]), writes=["bz_row"])
    S.dma("pool", lambda e: e.dma_start(out=bv_row[:, :], in_=b_in[0:1, 2568:3080]), writes=["bv_row"])
    S.dma("sp", lambda e: e.dma_start(out=bdtf[:, 0:8], in_=b_in[0:1, 1536:1544].to_broadcast([128, 8])), writes=["bdtf0"])
    S.dma("sp", lambda e: e.dma_start(out=bdtf[:, 8:16], in_=b_in[0:1, 3080:3088].to_broadcast([128, 8])), writes=["bdtf1"])
    S.dma("sp", lambda e: e.dma_start(out=bxbc[:, :], in_=b_in[0, 512:1536].rearrange("(c p) -> p c", p=128)), writes=["bxbc"])
    S.dma("sp", lambda e: e.dma_start(out=bq[:, :], in_=b_in[0, 1544:2056].rearrange("(c p) -> p c", p=128)), writes=["bq"])
    S.dma("sp", lambda e: e.dma_start(out=bk[:, :], in_=b_in[0, 2056:2568].rearrange("(c p) -> p c", p=128)), writes=["bk"])
    for k in range(4):
        S.dma("sp", lambda e, k=k: e.dma_start(out=convw[:, :, k], in_=conv_w[k, :].rearrange("(c p) -> p c", p=128)), writes=["convw%d" % k])
    CONVW = ["convw%d" % k for k in range(4)]
    S.dma("sp", lambda e: e.dma_start(out=convb[:, :], in_=conv_b[0, :].rearrange("(c p) -> p c", p=128)), writes=["convb"])
    S.dma("sp", lambda e: e.dma_start(out=a_bc[:, :], in_=a_log[0:1, :].to_broadcast([128, 8])), writes=["a_bc"])
    S.act(lambda e: e.activation(out=a_bc[:, :], in_=a_bc[:, :], func=AF.Exp), reads=["a_bc"], writes=["a_bc"])
    S.dve(lambda e: e.tensor_scalar(out=a_bc[:, :], in0=a_bc[:, :], scalar1=-1.0, scalar2=None, op0=ALU.mult), reads=["a_bc"], writes=["a_bc"])
    S.dma("sp", lambda e: e.dma_start(out=dskip_bc[:, :], in_=d_skip[0:1, :].to_broadcast([128, 8])), writes=["dskip_bc"])
    S.dma("sp", lambda e: e.dma_start(out=gssd_bc[:, :], in_=ssd_g[0:1, :].to_broadcast([128, 512])), writes=["gssd_bc"])
    S.dma("sp", lambda e: e.dma_start(out=rw[:, :, 0:4], in_=rg_w.rearrange("(k p) j -> p k j", p=128)), writes=["rw0"])
    for g in range(4):
        S.dma("sp", lambda e, g=g: e.dma_start(out=rw[:, :, 4 + 4 * g:8 + 4 * g], in_=re_w[g].rearrange("(k p) j -> p k j", p=128)), writes=["rw%d" % (g + 1)])
    RWK = ["rw%d" % i for i in range(5)]
    S.dma("sp", lambda e: e.dma_start(out=rb_bc[:, 0:4], in_=rg_b[0:1, :].to_broadcast([128, 4])), writes=["rb0"])
    S.dma("sp", lambda e: e.dma_start(out=rb_bc[:, 4:20], in_=re_b[0:1, :].to_broadcast([128, 16])), writes=["rb1"])

    outs = []

    for s in range(NSEQ):
        RW.reset(); RX.reset(); RACC.reset(); MSSD.reset(); TT.reset()
        wgu = [None, None]; wdn = [None, None]
        wgu[0] = RW.alloc([8, 1024], BF16); wdn[0] = RW.alloc([4, 1024], BF16)
        wgu[1] = RW.alloc([8, 1024], BF16); wdn[1] = RW.alloc([4, 1024], BF16)
        RW.reset()
        wA = RW.alloc([8, 1544], BF16)
        xb = [RW.alloc([4, 1024], BF16) for _ in range(2)]
        xT = RX.alloc([8, SEQ], BF16)
        sz = RACC.alloc([NT, 512], BF16)
        xsB = RACC.alloc([NT, 768], BF16)
        BT = RACC.alloc([2, SEQ], BF16)
        CT = RACC.alloc([2, SEQ], BF16)
        xsT = MSSD.alloc([4, SEQ], BF16)
        dt_t = TT.alloc([NT, 8], F32)
        tt_mark = TT.off
        pre = [TT.alloc([SEQ + 3], BF16) for _ in range(2)]
        diagw = TT.alloc([8, 4, 128], BF16)
        dt_raw = TT.alloc([NT, 8], F32)

        w_in_v = w_in.rearrange("(k p) c -> p k c", p=128)
        S.dma("pool", lambda e, s=s: e.dma_start(out=xb[0][:, :, :], in_=x[s, 0:512, :].rearrange("(t p) d -> p t d", p=128)), writes=["xb0"])
        S.dma("pool", lambda e: e.dma_start(out=wA[:, :, 0:512], in_=w_in_v[:, :, 0:512]), writes=["wA.z"])
        S.dma("pool", lambda e: e.dma_start(out=wA[:, :, 1536:1544], in_=w_in_v[:, :, 1536:1544]), writes=["wA.dt"])
        S.dma("pool", lambda e, s=s: e.dma_start(out=xb[1][:, :, :], in_=x[s, 512:1024, :].rearrange("(t p) d -> p t d", p=128)), writes=["xb1"])
        for half in range(2):
            S.dma("pool", lambda e, half=half: e.dma_start(out=wA[:, 4 * half:4 * half + 4, 512:1536], in_=w_in_v[:, 4 * half:4 * half + 4, 512:1536]), writes=["wA.x%d" % half])
        for b in range(2):
            S.pool(lambda e, b=b: e.memset(pre[b][:, 0:3], 0.0), writes=["prepad%d" % b])
        ev = 0
        for blk in range(4):
            if blk >= 2:
                S.dma("pool", lambda e, blk=blk, s=s: e.dma_start(out=xb[blk % 2][:, :, :], in_=x[s, blk * 512:(blk + 1) * 512, :].rearrange("(t p) d -> p t d", p=128)),
                      writes=["xb%d" % (blk % 2)])
            for k in range(8):
                pb = (blk * 8 + k) % 2
                for t in range(4):
                    S.pe(lambda e, pb=pb, t=t, k=k, blk=blk: e.transpose(bankb(pb)[:, t * 128:(t + 1) * 128], xb[blk % 2][:, t, k * 128:(k + 1) * 128], identB[:, :]),
                         reads=["xb%d" % (blk % 2), "identB"], writes=[PSK[pb]])
                if ev % 2 == 0:
                    S.act(lambda e, pb=pb, k=k, blk=blk: e.copy(xT[:, k, blk * 512:(blk + 1) * 512], bankb(pb)[:, 0:512]), reads=[PSK[pb]], writes=["xT.%d.%d" % (k, blk)])
                else:
                    S.dve(lambda e, pb=pb, k=k, blk=blk: e.tensor_copy(xT[:, k, blk * 512:(blk + 1) * 512], bankb(pb)[:, 0:512]), reads=[PSK[pb]], writes=["xT.%d.%d" % (k, blk)])
                ev += 1
            for tt in range(4):
                t = blk * 4 + tt
                pz = 2 + (t % 2)
                xk = ["xT.%d.%d" % (k, blk) for k in range(8)]
                for k in range(8):
                    S.pe(lambda e, pz=pz, t=t, k=k: e.matmul(bank(pz)[:, :], lhsT=xT[:, k, t * 128:(t + 1) * 128], rhs=wA[:, k, 0:512], start=(k == 0), stop=False),
                         reads=[xk[k], "wA.z"], writes=[PSK[pz]])
                S.pe(lambda e, pz=pz: e.matmul(bank(pz)[:, :], lhsT=ones_row[0:1, :], rhs=bz_row[0:1, :], start=False, stop=True),
                     reads=["ones_row", "bz_row"], writes=[PSK[pz]])
                S.act(lambda e, pz=pz, t=t: e.activation(out=sz[:, t, :], in_=bank(pz)[:, :], func=AF.Silu), reads=[PSK[pz]], writes=["sz.%d" % t])
                for k in range(8):
                    S.pe(lambda e, t=t, k=k: e.matmul(bank(4)[:, t * 8:(t + 1) * 8], lhsT=xT[:, k, t * 128:(t + 1) * 128], rhs=wA[:, k, 1536:1544], start=(k == 0), stop=(k == 7)),
                         reads=[xk[k], "wA.dt"], writes=[PSK[4]])
        S.dve(lambda e: e.tensor_tensor(out=dt_raw[:, :, :], in0=bank(4)[:, 0:128].rearrange("p (t h) -> p t h", h=8), in1=bdtf[:, 0:8].unsqueeze(1).to_broadcast([128, NT, 8]), op=ALU.add),
              reads=[PSK[4], "bdtf0"], writes=["dt_raw"])
        for c in range(8):
            for k in range(4):
                S.dve(lambda e, c=c, k=k: e.tensor_scalar(out=diagw[:, c, k, :], in0=identB[:, :], scalar1=convw[:, c, k:k + 1], scalar2=None, op0=ALU.mult),
                      reads=["identB"] + CONVW, writes=["diagw.%d" % c])

        def a1_ip(c):
            pb_ = c % 2
            for blk in range(4):
                pc = 5 + (c * 4 + blk) % 2
                for k in range(8):
                    S.pe(lambda e, pc=pc, c=c, k=k, blk=blk: e.matmul(bank(pc)[:, :], lhsT=wA[:, k, 512 + c * 128:512 + (c + 1) * 128], rhs=xT[:, k, blk * 512:(blk + 1) * 512], start=(k == 0), stop=(k == 7)),
                         reads=["xT.%d.%d" % (k, blk), "wA.x%d" % (k // 4)], writes=[PSK[pc]])
                S.act(lambda e, pc=pc, c=c, blk=blk, pb_=pb_: e.activation(out=pre[pb_][:, 3 + blk * 512:3 + (blk + 1) * 512], in_=bank(pc)[:, :], func=AF.Identity, bias=bxbc[:, c:c + 1], scale=1.0),
                      reads=[PSK[pc], "bxbc"], writes=["pre%d.%d" % (pb_, blk)])

        def a1_conv(c):
            pb_ = c % 2
            if c < 4:
                dstt = xsT[:, c, :]; dk = "xsT.%d" % c
            elif c < 6:
                dstt = BT[:, c - 4, :]; dk = "BT.%d" % (c - 4)
            else:
                dstt = CT[:, c - 6, :]; dk = "CT.%d" % (c - 6)
            for blk in range(4):
                pc = 2 + (c * 4 + blk) % 2
                rk = ["pre%d.%d" % (pb_, blk), "diagw.%d" % c] + (["pre%d.%d" % (pb_, blk - 1)] if blk > 0 else ["prepad%d" % pb_])
                for k in range(4):
                    S.pe(lambda e, pc=pc, c=c, k=k, blk=blk, pb_=pb_: e.matmul(bank(pc)[:, :], lhsT=diagw[:, c, k, :], rhs=pre[pb_][:, blk * 512 + k:blk * 512 + k + 512], start=(k == 0), stop=(k == 3)),
                         reads=rk, writes=[PSK[pc]])
                S.act(lambda e, pc=pc, dstt=dstt, c=c, blk=blk: e.activation(out=dstt[:, blk * 512:(blk + 1) * 512], in_=bank(pc)[:, :], func=AF.Silu, bias=convb[:, c:c + 1], scale=1.0),
                      reads=[PSK[pc], "convb"], writes=[dk])

        a1_ip(0)
        for c in range(8):
            if c + 1 < 8:
                a1_ip(c + 1)
            a1_conv(c)
        for t in range(NT):
            pb = t % 2
            for c in range(6):
                src = xsT[:, c, t * 128:(t + 1) * 128] if c < 4 else BT[:, c - 4, t * 128:(t + 1) * 128]
                sk = "xsT.%d" % c if c < 4 else "BT.%d" % (c - 4)
                S.pe(lambda e, pb=pb, c=c, src=src: e.transpose(bankb(pb)[:, c * 128:(c + 1) * 128], src, identB[:, :]), reads=[sk, "identB"], writes=[PSK[pb]])
            if t % 2 == 0:
                S.dve(lambda e, pb=pb, t=t: e.tensor_copy(xsB[:, t, :], bankb(pb)[:, 0:768]), reads=[PSK[pb]], writes=["xsB.%d" % t])
            else:
                S.act(lambda e, pb=pb, t=t: e.copy(xsB[:, t, :], bankb(pb)[:, 0:768]), reads=[PSK[pb]], writes=["xsB.%d" % t])
        S.act(lambda e: e.activation(out=dt_t[:, :, :], in_=dt_raw[:, :, :], func=AF.Exp), reads=["dt_raw"], writes=["dt_t"])
        S.act(lambda e: e.activation(out=dt_t[:, :, :], in_=dt_t[:, :, :], func=AF.Ln, bias=1.0, scale=1.0), reads=["dt_t"], writes=["dt_t"])

        fence()
        MSSD.reset(); TT.reset(tt_mark)
        RW.reset()
        wB = RW.alloc([8, 1544], BF16)
        wo = RW.alloc([8, 1024], BF16)
        for half in range(2):
            S.dma("pool", lambda e, half=half: e.dma_start(out=wB[:, 4 * half:4 * half + 4, :], in_=w_in.rearrange("(k p) c -> p k c", p=128)[:, 4 * half:4 * half + 4, 1544:3088]),
                  writes=["wB"], nofence=True)
        for half in range(2):
            S.dma("pool", lambda e, half=half: e.dma_start(out=wo[:, 4 * half:4 * half + 4, :], in_=w_out.rearrange("(k p) c -> p k c", p=128)[:, 4 * half:4 * half + 4, :]),
                  writes=["wo"], nofence=True)
        m_ssd = MSSD.alloc([NT, 512], BF16)
        da = TT.alloc([NT, 8], F32); acum = TT.alloc([NT, 8], F32); nacum = TT.alloc([NT, 8], F32)
        alast = TT.alloc([NT, 8], F32); dte = TT.alloc([NT, 8], F32); ea = TT.alloc([NT, 8], F32)
        cdec = TT.alloc([NT, 8], F32); dtdte = TT.alloc([NT, 8], F32)
        stT = TT.alloc([8, 64], F32); stTb = TT.alloc([8, 64], BF16)
        LT = [TT.alloc([128], BF16) for _ in range(4)]
        MT = [TT.alloc([128], BF16) for _ in range(4)]
        xdt = [TT.alloc([8, 64], BF16) for _ in range(2)]
        xdtd = [TT.alloc([8, 64], BF16) for _ in range(2)]
        t1 = [TT.alloc([8, 64], F32)] * 2
        t2 = [TT.alloc([8, 64], F32) for _ in range(2)]
        yg = [TT.alloc([512], F32) for _ in range(2)]
        junk = TT.alloc([256], F32)
        ss = [TT.alloc([2], F32) for _ in range(2)]
        rstd = [TT.alloc([2], F32) for _ in range(2)]

        S.dve(lambda e: e.tensor_tensor(out=da[:, :, :], in0=dt_t[:, :, :], in1=a_bc[:, :].unsqueeze(1).to_broadcast([128, NT, 8]), op=ALU.mult), reads=["dt_t", "a_bc"], writes=["da"])
        daf = da.rearrange("p t h -> p (t h)")
        S.pe(lambda e: e.matmul(bank(5)[:, 0:128], lhsT=Uf[:, :], rhs=daf, start=True, stop=True), reads=["Uf", "da"], writes=[PSK[5]])
        S.pe(lambda e: e.matmul(bank(6)[:, 0:128], lhsT=onesF[:, :], rhs=daf, start=True, stop=True), reads=["onesF", "da"], writes=[PSK[6]])
        S.dve(lambda e: e.tensor_copy(acum.rearrange("p t h -> p (t h)"), bank(5)[:, 0:128]), reads=[PSK[5]], writes=["acum"])
        S.dve(lambda e: e.tensor_scalar(out=nacum.rearrange("p t h -> p (t h)"), in0=bank(5)[:, 0:128], scalar1=-1.0, scalar2=None, op0=ALU.mult), reads=[PSK[5]], writes=["nacum"])
        S.dve(lambda e: e.tensor_copy(alast.rearrange("p t h -> p (t h)"), bank(6)[:, 0:128]), reads=[PSK[6]], writes=["alast"])
        S.dve(lambda e: e.tensor_tensor(out=dte[:, :, :], in0=alast[:, :, :], in1=acum[:, :, :], op=ALU.subtract), reads=["alast", "acum"], writes=["dte"])
        S.act(lambda e: e.activation(out=dte[:, :, :], in_=dte[:, :, :], func=AF.Exp), reads=["dte"], writes=["dte"])
        S.act(lambda e: e.activation(out=ea[:, :, :], in_=acum[:, :, :], func=AF.Exp), reads=["acum"], writes=["ea"])
        S.act(lambda e: e.activation(out=cdec[:, :, :], in_=alast[:, :, :], func=AF.Exp), reads=["alast"], writes=["cdec"])
        S.dve(lambda e: e.tensor_tensor(out=dtdte[:, :, :], in0=dt_t[:, :, :], in1=dte[:, :, :], op=ALU.mult), reads=["dt_t", "dte"], writes=["dtdte"])
        S.pool(lambda e: e.memset(stT[:, :, :], 0.0), writes=["stT"])
        S.pool(lambda e: e.memset(stTb[:, :, :], 0.0), writes=["stTb"])

        def ssd_F(c):
            cs = slice(c * 128, (c + 1) * 128)
            b2 = c % 2
            py = 2 + b2
            xs_c = xsB[:, c, 0:512].rearrange("p (h d) -> p h d", h=8)
            S.pool(lambda e, c=c, b2=b2, xs_c=xs_c: e.tensor_tensor(out=xdt[b2][:, :, :], in0=xs_c, in1=dt_t[:, c, :].unsqueeze(2).to_broadcast([128, 8, 64]), op=ALU.mult),
                   reads=["xsB.%d" % c, "dt_t"], writes=["xdt%d" % b2])
            S.pool(lambda e, c=c, b2=b2, xs_c=xs_c: e.tensor_tensor(out=xdtd[b2][:, :, :], in0=xs_c, in1=dtdte[:, c, :].unsqueeze(2).to_broadcast([128, 8, 64]), op=ALU.mult),
                   reads=["xsB.%d" % c, "dtdte"], writes=["xdtd%d" % b2])
            S.pool(lambda e, c=c, b2=b2, xs_c=xs_c: e.tensor_tensor(out=t2[b2][:, :, :], in0=xs_c, in1=dskip_bc[:, :].unsqueeze(2).to_broadcast([128, 8, 64]), op=ALU.mult),
                   reads=["xsB.%d" % c, "dskip_bc"], writes=["t2%d" % b2])
            for g in range(2):
                S.pe(lambda e, g=g, cs=cs: e.matmul(bank(4)[:, g * 128:(g + 1) * 128], lhsT=BT[:, g, cs], rhs=CT[:, g, cs], start=True, stop=True),
                     reads=["BT.%d" % g, "CT.%d" % g], writes=[PSK[4]])
            for hh in range(2):
                pl = hh
                for h4 in range(4):
                    h = hh * 4 + h4
                    S.pe(lambda e, pl=pl, h4=h4, c=c, h=h: e.matmul(bank(pl)[:, h4 * 128:(h4 + 1) * 128], lhsT=da[:, c, h:h + 1].to_broadcast([128, 128]), rhs=Uf[:, :], start=True, stop=False),
                         reads=["da", "Uf"], writes=[PSK[pl]])
                    S.pe(lambda e, pl=pl, h4=h4: e.matmul(bank(pl)[:, h4 * 128:(h4 + 1) * 128], lhsT=identB[:, :], rhs=maskneg[:, :], start=False, stop=True),
                         reads=["identB", "maskneg"], writes=[PSK[pl]])
            for hh in range(2):
                pl = hh
                for h4 in range(4):
                    h = hh * 4 + h4
                    S.act(lambda e, pl=pl, h4=h4, c=c, h=h: e.activation(out=LT[h4][:, :], in_=bank(pl)[:, h4 * 128:(h4 + 1) * 128], func=AF.Exp, bias=nacum[:, c, h:h + 1], scale=1.0),
                          reads=[PSK[pl], "nacum"], writes=["LT%d" % h4])
                    g = h // 4
                    S.dve(lambda e, h4=h4, g=g: e.tensor_tensor(out=MT[h4][:, :], in0=bank(4)[:, g * 128:(g + 1) * 128], in1=LT[h4][:, :], op=ALU.mult),
                          reads=[PSK[4], "LT%d" % h4], writes=["MT%d" % h4])
                    S.pe(lambda e, h4=h4, h=h, b2=b2, py=py: e.matmul(bank(py)[:, h * 64:(h + 1) * 64], lhsT=MT[h4][:, :], rhs=xdt[b2][:, h, :], start=True, stop=True),
                         reads=["MT%d" % h4, "xdt%d" % b2], writes=[PSK[py]])

        def ssd_B(c):
            cs = slice(c * 128, (c + 1) * 128)
            b2 = c % 2
            py = 2 + b2
            if c > 0:
                for g in range(2):
                    S.pe(lambda e, g=g, cs=cs: e.matmul(bank(6)[:, g * 256:(g + 1) * 256], lhsT=CT[:, g, cs], rhs=stTb[:, 4 * g:4 * g + 4, :].rearrange("p h d -> p (h d)"), start=True, stop=True),
                         reads=["CT.%d" % g, "stTb"], writes=[PSK[6]])
            if c < NT - 1:
                for g in range(2):
                    S.pe(lambda e, g=g, c=c, b2=b2: e.matmul(bank(7)[:, g * 256:(g + 1) * 256], lhsT=xsB[:, c, 512 + g * 128:512 + (g + 1) * 128], rhs=xdtd[b2][:, 4 * g:4 * g + 4, :].rearrange("p h d -> p (h d)"), start=True, stop=True),
                         reads=["xsB.%d" % c, "xdtd%d" % b2], writes=[PSK[7]])
                S.dve(lambda e, c=c: e.tensor_tensor(out=stT[:, :, :], in0=stT[:, :, :], in1=cdec[:, c, :].unsqueeze(2).to_broadcast([128, 8, 64]), op=ALU.mult),
                      reads=["stT", "cdec"], writes=["stT"])
                S.dve(lambda e: e.tensor_tensor(out=stT[:, :, :], in0=bank(7)[:, 0:512].rearrange("p (h d) -> p h d", h=8), in1=stT[:, :, :], op=ALU.add),
                      reads=[PSK[7], "stT"], writes=["stT"])
            if c > 0:
                S.dve(lambda e, c=c, b2=b2: e.tensor_tensor(out=t1[b2][:, :, :], in0=bank(6)[:, :].rearrange("p (h d) -> p h d", h=8), in1=ea[:, c, :].unsqueeze(2).to_broadcast([128, 8, 64]), op=ALU.mult),
                      reads=[PSK[6], "ea"], writes=["t1"])
            if c < NT - 1:
                S.act(lambda e: e.copy(stTb[:, :, :], stT[:, :, :]), reads=["stT"], writes=["stTb"])
            if c > 0:
                S.dve(lambda e, b2=b2, py=py: e.tensor_tensor(out=t1[b2][:, :, :], in0=bank(py)[:, :].rearrange("p (h d) -> p h d", h=8), in1=t1[b2][:, :, :], op=ALU.add),
                      reads=[PSK[py], "t1"], writes=["t1"])
                S.dve(lambda e, b2=b2: e.tensor_tensor(out=t1[b2][:, :, :], in0=t1[b2][:, :, :], in1=t2[b2][:, :, :], op=ALU.add),
                      reads=["t1", "t2%d" % b2], writes=["t1"])
            else:
                S.dve(lambda e, b2=b2, py=py: e.tensor_tensor(out=t1[b2][:, :, :], in0=bank(py)[:, :].rearrange("p (h d) -> p h d", h=8), in1=t2[b2][:, :, :], op=ALU.add),
                      reads=[PSK[py], "t2%d" % b2], writes=["t1"])
            S.dve(lambda e, c=c, b2=b2: e.tensor_tensor(out=yg[b2][:, :], in0=t1[b2].rearrange("p h d -> p (h d)"), in1=sz[:, c, :], op=ALU.mult),
                  reads=["t1", "sz.%d" % c], writes=["yg%d" % b2])
            for g in range(2):
                S.act(lambda e, g=g, b2=b2: e.activation(out=junk[:, :], in_=yg[b2][:, g * 256:(g + 1) * 256], func=AF.Square, accum_out=ss[b2][:, g:g + 1]),
                      reads=["yg%d" % b2], writes=["junk", "ss%d.%d" % (b2, g)])
            S.act(lambda e, b2=b2: e.activation(out=rstd[b2][:, :], in_=ss[b2][:, :], func=AF.Ln, bias=RMS_EPS, scale=1.0 / 256.0),
                  reads=["ss%d.0" % b2, "ss%d.1" % b2], writes=["rstd%d" % b2])
            S.act(lambda e, b2=b2: e.activation(out=rstd[b2][:, :], in_=rstd[b2][:, :], func=AF.Exp, scale=-0.5), reads=["rstd%d" % b2], writes=["rstd%d" % b2])
            for g in range(2):
                S.dve(lambda e, g=g, b2=b2, c=c: e.scalar_tensor_tensor(out=m_ssd[:, c, g * 256:(g + 1) * 256], in0=yg[b2][:, g * 256:(g + 1) * 256], scalar=rstd[b2][:, g:g + 1], in1=gssd_bc[:, g * 256:(g + 1) * 256], op0=ALU.mult, op1=ALU.mult),
                      reads=["yg%d" % b2, "rstd%d" % b2, "gssd_bc"], writes=["m_ssd.%d" % c])

        ssd_F(0)
        for c in range(NT):
            if c + 1 < NT:
                ssd_F(c + 1)
            ssd_B(c)

        if dbg == 'ssd' and s == 0:
            RX.reset()
            dm = dbg_out("dbg_mssd", [128, NT * 512])
            cvt = RX.alloc([NT * 512], F32) if dbg == 'ssd' else None
            S.dve(lambda e: e.tensor_copy(cvt[:, :], m_ssd.rearrange("p t d -> p (t d)")), reads=["m_ssd.%d" % c for c in range(NT)], writes=["cvt"])
            outs.append(S.dma("sp", lambda e: e.dma_start(out=dm[:, :], in_=cvt[:, :]), reads=["cvt"]))
        if dbg == "ssd":
            break

        fence()
        RW.reset(); RACC.reset(); TT.reset()
        wB = RW.alloc([8, 1544], BF16)
        wo = RW.alloc([8, 1024], BF16)
        ah = RW.alloc([1024], F32)
        hTf = RW.alloc([8, 128], F32)
        qT = RACC.alloc([4, SEQ], BF16)
        kT = RACC.alloc([4, SEQ], BF16)
        v_aug = RACC.alloc([NT, 8, 65], BF16)
        xres = [RACC.alloc([1024], F32) for _ in range(2)]
        hpre = [RACC.alloc([1024], F32), TT.alloc([1024], F32)]
        f_raw = TT.alloc([NT, 8], F32); Gc = TT.alloc([NT, 8], F32); tot = TT.alloc([NT, 8], F32)
        Pp = TT.alloc([NT, 8], F32); Gf = TT.alloc([NT, 8], F32); Gend = TT.alloc([NT, 8], F32)
        biasT = TT.alloc([8, NT, NT], F32)
        PT = [TT.alloc([8, 128], BF16) for _ in range(2)]
        yatt = [TT.alloc([8, 64], BF16) for _ in range(2)]
        rden = [TT.alloc([8], F32) for _ in range(2)]
        mT = [TT.alloc([8, 128], BF16)] * 2
        bst = [TT.alloc([2, 6], F32) for _ in range(2)]
        mv = [TT.alloc([2], F32) for _ in range(2)]
        rs1 = [TT.alloc([1], F32) for _ in range(2)]

        S.dma("sp", lambda e: e.dma_start(out=lnG[:, :], in_=ln1_g[0:1, :].to_broadcast([128, 1024])), writes=["lnG"])
        S.dma("sp", lambda e: e.dma_start(out=lnB[:, :], in_=ln1_b[0:1, :].to_broadcast([128, 1024])), writes=["lnB"])
        S.pool(lambda e: e.memset(v_aug[:, :, :, 64:65], 1.0), writes=["v_ones"])
        for t in range(NT):
            pv = 4 + (t % 2)
            blk = t // 4
            for k in range(8):
                S.pe(lambda e, pv=pv, t=t, k=k: e.matmul(bank(pv)[:, :], lhsT=xT[:, k, t * 128:(t + 1) * 128], rhs=wB[:, k, 1024:1536], start=(k == 0), stop=False),
                     reads=["xT.%d.%d" % (k, blk), "wB"], writes=[PSK[pv]])
            S.pe(lambda e, pv=pv: e.matmul(bank(pv)[:, :], lhsT=ones_row[0:1, :], rhs=bv_row[0:1, :], start=False, stop=True),
                 reads=["ones_row", "bv_row"], writes=[PSK[pv]])
            if t % 2 == 0:
                S.act(lambda e, pv=pv, t=t: e.copy(v_aug[:, t, :, 0:64], bank(pv)[:, :].rearrange("p (h d) -> p h d", h=8)), reads=[PSK[pv]], writes=["v.%d" % t])
            else:
                S.dve(lambda e, pv=pv, t=t: e.tensor_copy(v_aug[:, t, :, 0:64], bank(pv)[:, :].rearrange("p (h d) -> p h d", h=8)), reads=[PSK[pv]], writes=["v.%d" % t])
            for k in range(8):
                S.pe(lambda e, t=t, k=k: e.matmul(bank(6)[:, t * 8:(t + 1) * 8], lhsT=xT[:, k, t * 128:(t + 1) * 128], rhs=wB[:, k, 1536:1544], start=(k == 0), stop=(k == 7)),
                     reads=["xT.%d.%d" % (k, blk), "wB"], writes=[PSK[6]])
        S.dve(lambda e: e.tensor_tensor(out=f_raw[:, :, :], in0=bank(6)[:, 0:128].rearrange("p (t h) -> p t h", h=8), in1=bdtf[:, 8:16].unsqueeze(1).to_broadcast([128, NT, 8]), op=ALU.add),
              reads=[PSK[6], "bdtf1"], writes=["f_raw"])
        S.act(lambda e: e.activation(out=f_raw[:, :, :], in_=f_raw[:, :, :], func=AF.Exp, scale=-1.0), reads=["f_raw"], writes=["f_raw"])
        S.act(lambda e: e.activation(out=f_raw[:, :, :], in_=f_raw[:, :, :], func=AF.Ln, bias=1.0, scale=1.0), reads=["f_raw"], writes=["f_raw"])
        def a2_cums():
            frf = f_raw.rearrange("p t h -> p (t h)")
            S.pe(lambda e: e.matmul(bank(7)[:, 0:128], lhsT=Uf[:, :], rhs=frf, start=True, stop=True), reads=["Uf", "f_raw"], writes=[PSK[7]])
            S.pe(lambda e: e.matmul(bank(7)[:, 128:256], lhsT=onesF[:, :], rhs=frf, start=True, stop=True), reads=["onesF", "f_raw"], writes=[PSK[7]])

        def a2_chain():
            S.dve(lambda e: e.tensor_copy(Gc.rearrange("p t h -> p (t h)"), bank(7)[:, 0:128]), reads=[PSK[7]], writes=["Gc"])
            S.dve(lambda e: e.tensor_copy(tot.rearrange("p t h -> p (t h)"), bank(7)[:, 128:256]), reads=[PSK[7]], writes=["tot"])
            S.pool(lambda e: e.memset(Pp[:, 0, :], 0.0), writes=["Pp"])
            for t in range(1, NT):
                S.dve(lambda e, t=t: e.tensor_tensor(out=Pp[:, t, :], in0=Pp[:, t - 1, :], in1=tot[:, t - 1, :], op=ALU.add), reads=["Pp", "tot"], writes=["Pp"])
            S.dve(lambda e: e.tensor_tensor(out=Gf[:, :, :], in0=Gc[:, :, :], in1=Pp[:, :, :], op=ALU.add), reads=["Gc", "Pp"], writes=["Gf"])
            S.dve(lambda e: e.tensor_tensor(out=Gend[:, :, :], in0=tot[:, :, :], in1=Pp[:, :, :], op=ALU.add), reads=["tot", "Pp"], writes=["Gend"])
            for h in range(8):
                S.dve(lambda e, h=h: e.tensor_tensor(out=biasT[:, h, :, :], in0=Gf[:, :, h].unsqueeze(1).to_broadcast([128, NT, NT]), in1=Gend[:, :, h].unsqueeze(2).to_broadcast([128, NT, NT]), op=ALU.subtract),
                      reads=["Gf", "Gend"], writes=["biasT"])

        evq = 0
        qk_groups = [(qk, p, blk) for qk in range(2) for p in range(4) for blk in range(4)]
        for gidx, (qk, p, blk) in enumerate(qk_groups):
            dstT = qT if qk == 0 else kT
            bcol = bq if qk == 0 else bk
            nm = "qT" if qk == 0 else "kT"
            pq = evq % 4
            for k in range(8):
                S.pe(lambda e, pq=pq, k=k, p=p, blk=blk, qk=qk: e.matmul(bank(pq)[:, :], lhsT=wB[:, k, qk * 512 + p * 128:qk * 512 + (p + 1) * 128], rhs=xT[:, k, blk * 512:(blk + 1) * 512], start=(k == 0), stop=(k == 7)),
                     reads=["xT.%d.%d" % (k, blk), "wB"], writes=[PSK[pq]])
            if evq % 2 == 0:
                S.act(lambda e, pq=pq, p=p, blk=blk, dstT=dstT, bcol=bcol: e.activation(out=dstT[:, p, blk * 512:(blk + 1) * 512], in_=bank(pq)[:, :], func=AF.Identity, bias=bcol[:, p:p + 1], scale=1.0),
                      reads=[PSK[pq], "bq", "bk"], writes=["%s.%d.%d" % (nm, p, blk)])
            else:
                S.dve(lambda e, pq=pq, p=p, blk=blk, dstT=dstT, bcol=bcol: e.tensor_scalar(out=dstT[:, p, blk * 512:(blk + 1) * 512], in0=bank(pq)[:, :], scalar1=bcol[:, p:p + 1], scalar2=None, op0=ALU.add),
                      reads=[PSK[pq], "bq", "bk"], writes=["%s.%d.%d" % (nm, p, blk)])
            evq += 1
            if gidx == 7:
                a2_cums()
            if gidx == 13:
                a2_chain()
        fence()
        RX.reset()
        hT = RX.alloc([8, SEQ], BF16)
        NEXP = int(os.environ.get("KNEXP", 16))

        def load_expert(ex):
            sl = ex % 2
            S.dma("pool", lambda e, ex=ex, sl=sl: e.dma_start(out=wgu[sl][:, :, 0:512], in_=w_gate[ex].rearrange("(k p) f -> p k f", p=128)), writes=["wg.%d" % sl], nofence=(ex == 0))
            S.dma("pool", lambda e, ex=ex, sl=sl: e.dma_start(out=wgu[sl][:, :, 512:1024], in_=w_up[ex].rearrange("(k p) f -> p k f", p=128)), writes=["wu.%d" % sl], nofence=(ex == 0))
            S.dma("pool", lambda e, ex=ex, sl=sl: e.dma_start(out=wdn[sl][:, :, :], in_=w_down[ex].rearrange("(k p) d -> p k d", p=128)), writes=["wd.%d" % sl], nofence=(ex == 0))


        load_expert(0)
        NTI = int(os.environ.get("KATT_TILES", NT))
        S.fold_now = os.environ.get("KFOLD", "all") in ("attmoe", "all")
        units = []
        for i in range(NTI):
            for p in range(4):
                for j0 in range(0, i + 1, 4):
                    units.append((i, p, list(range(j0, min(j0 + 4, i + 1)))))

        def emit_S(u, gi):
            i, p, js = u
            pb0 = 2 * (gi % 2)
            for jj, j in enumerate(js):
                for hh in range(2):
                    r0 = hh * 64
                    S.pe(lambda e, bk=pb0 + hh, jj=jj, j=j, p=p, r0=r0, i=i: e.matmul(bank(bk)[:, jj * 128:(jj + 1) * 128], lhsT=kT[r0:r0 + 64, p, j * 128:(j + 1) * 128], rhs=qT[r0:r0 + 64, p, i * 128:(i + 1) * 128], start=True, stop=True),
                         reads=["kT.%d.%d" % (p, j // 4), "qT.%d.%d" % (p, i // 4)], writes=[PSK[pb0 + hh]])

        def emit_E(u, gi):
            i, p, js = u
            pb0 = 2 * (gi % 2); pb = gi % 2
            for jj, j in enumerate(js):
                for hh in range(2):
                    h = 2 * p + hh
                    c8 = jj * 2 + hh
                    S.act(lambda e, bk=pb0 + hh, pb=pb, c8=c8, jj=jj, j=j, h=h, i=i: e.activation(out=PT[pb][:, c8, :], in_=bank(bk)[:, jj * 128:(jj + 1) * 128], func=AF.Exp, bias=biasT[:, h, i, j:j + 1], scale=ATT_SCALE),
                          reads=[PSK[pb0 + hh], "biasT"], writes=["PT%d.%d" % (pb, c8)])
                    if j == i:
                        S.dve(lambda e, pb=pb, c8=c8: e.tensor_tensor(out=PT[pb][:, c8, :], in0=PT[pb][:, c8, :], in1=triU[:, :], op=ALU.mult),
                              reads=["PT%d.%d" % (pb, c8), "triU"], writes=["PT%d.%d" % (pb, c8)])

        def emit_V(u, gi):
            i, p, js = u
            pb = gi % 2
            for jj, j in enumerate(js):
                for hh in range(2):
                    h = 2 * p + hh
                    c8 = jj * 2 + hh
                    po = 4 + h // 4; oc = (h % 4) * 65
                    S.pe(lambda e, pb=pb, c8=c8, j=j, h=h, po=po, oc=oc, i=i: e.matmul(bank(po)[:, oc:oc + 65], lhsT=PT[pb][:, c8, :], rhs=v_aug[:, j, h, :], start=(j == 0 and h % 4 == 0), stop=(j == i and h % 4 == 3)),
                         reads=["PT%d.%d" % (pb, c8), "v.%d" % j, "v_ones"], writes=[PSK[po]])

        def tail_N(i):
            b2 = i % 2
            S.dma("sp", lambda e, i=i, b2=b2, s=s: e.dma_start(out=xres[b2][:, :], in_=x[s, i * 128:(i + 1) * 128, :]), writes=["xres%d" % b2])
            for hb in range(2):
                ov = bank(4 + hb)[:, 0:260].rearrange("p (h d) -> p h d", h=4)
                S.dve(lambda e, hb=hb, ov=ov, b2=b2: e.reciprocal(rden[b2][:, 4 * hb:4 * hb + 4], ov[:, :, 64]), reads=[PSK[4 + hb]], writes=["rden%d.%d" % (b2, hb)])
                S.dve(lambda e, hb=hb, ov=ov, b2=b2: e.tensor_tensor(out=yatt[b2][:, 4 * hb:4 * hb + 4, :], in0=ov[:, :, 0:64], in1=rden[b2][:, 4 * hb:4 * hb + 4].unsqueeze(2).to_broadcast([128, 4, 64]), op=ALU.mult),
                      reads=[PSK[4 + hb], "rden%d.%d" % (b2, hb)], writes=["yatt%d.%d" % (b2, hb)])

        def tail_T(i):
            b2 = i % 2
            yf = yatt[b2].rearrange("p h d -> p (h d)")
            for ec in range(8):
                src = m_ssd[:, i, ec * 128:(ec + 1) * 128] if ec < 4 else yf[:, (ec - 4) * 128:(ec - 3) * 128]
                rk = ["m_ssd.%d" % i] if ec < 4 else ["yatt%d.%d" % (b2, (ec - 4) // 2)]
                S.pe(lambda e, ec=ec, src=src: e.transpose(bankb(6)[:, ec * 128:(ec + 1) * 128], src, identB[:, :]), reads=rk + ["identB"], writes=[PSK[6]])
            S.dve(lambda e, b2=b2: e.tensor_copy(mT[b2].rearrange("p a b -> p (a b)"), bankb(6)[:, 0:1024]), reads=[PSK[6]], writes=["mT"])

        def tail_Oq(i, q):
            b2 = i % 2
            half = q // 2
            hp = hpre[b2]
            for ec in range(4 * (q % 2), 4 * (q % 2) + 4):
                S.pe(lambda e, ec=ec, b2=b2, half=half: e.matmul(bank(7)[:, :], lhsT=mT[b2][:, ec, :], rhs=wo[:, ec, half * 512:(half + 1) * 512], start=(ec == 0), stop=(ec == 7)),
                     reads=["mT", "wo"], writes=[PSK[7]])
            if q % 2 == 1:
                S.dve(lambda e, hp=hp, b2=b2, half=half: e.scalar_tensor_tensor(out=hp[:, half * 512:(half + 1) * 512], in0=xres[b2][:, half * 512:(half + 1) * 512], scalar=ALPHA, in1=bank(7)[:, :], op0=ALU.mult, op1=ALU.add),
                      reads=["xres%d" % b2, PSK[7]], writes=["hpre%d.%d" % (b2, half)])
                S.dve(lambda e, hp=hp, half=half, b2=b2: e.bn_stats(bst[b2][:, half, :], hp[:, half * 512:(half + 1) * 512]), reads=["hpre%d.%d" % (b2, half)], writes=["bst%d.%d" % (b2, half)])
            if q == 3:
                S.dve(lambda e, b2=b2: e.bn_aggr(mv[b2][:, :], bst[b2][:, :, :]), reads=["bst%d.0" % b2, "bst%d.1" % b2], writes=["mv%d" % b2])

        def tail_L(i):
            b2 = i % 2
            hp = hpre[b2]
            HK = ["hpre%d.0" % b2, "hpre%d.1" % b2]
            S.act(lambda e, b2=b2: e.activation(out=rs1[b2][:, :], in_=mv[b2][:, 1:2], func=AF.Ln, bias=LN_EPS, scale=1.0), reads=["mv%d" % b2], writes=["rs1%d" % b2])
            S.act(lambda e, b2=b2: e.activation(out=rs1[b2][:, :], in_=rs1[b2][:, :], func=AF.Exp, scale=-0.5), reads=["rs1%d" % b2], writes=["rs1%d" % b2])
            S.dve(lambda e, hp=hp, b2=b2: e.tensor_scalar(out=hp[:, :], in0=hp[:, :], scalar1=mv[b2][:, 0:1], scalar2=rs1[b2][:, 0:1], op0=ALU.subtract, op1=ALU.mult),
                  reads=HK + ["mv%d" % b2, "rs1%d" % b2], writes=HK)
            S.dve(lambda e, hp=hp: e.tensor_tensor(out=hp[:, :], in0=hp[:, :], in1=lnG[:, :], op=ALU.mult), reads=HK + ["lnG"], writes=HK)
            S.dve(lambda e, hp=hp: e.tensor_tensor(out=hp[:, :], in0=hp[:, :], in1=lnB[:, :], op=ALU.add), reads=HK + ["lnB"], writes=HK)
            S.dve(lambda e, hp=hp: e.tensor_scalar(out=ah[:, :], in0=hp[:, :], scalar1=ALPHA, scalar2=None, op0=ALU.mult), reads=HK, writes=["ah"])
            S.dma("sp", lambda e, i=i, s=s: e.dma_start(out=h_scr[s, i * 128:(i + 1) * 128, :], in_=ah[:, :]), reads=["ah"], writes=["h_scr.%d" % i])

        def tail_H(i, half):
            b2 = i % 2
            hp = hpre[b2]
            for q4 in range(4):
                ec = half * 4 + q4
                S.pe(lambda e, q4=q4, ec=ec, hp=hp: e.transpose(bank(6)[:, q4 * 128:(q4 + 1) * 128], hp[:, ec * 128:(ec + 1) * 128], identF[:, :]), reads=["hpre%d.%d" % (b2, half), "identF"], writes=[PSK[6]])
            S.dve(lambda e, half=half, i=i: e.tensor_copy(hT[:, 4 * half:4 * half + 4, i * 128:(i + 1) * 128], bank(6)[:, :].rearrange("p (a b) -> p a b", a=4)), reads=[PSK[6]], writes=["hT.%d.%d" % (i, half)])
            S.dve(lambda e, half=half: e.tensor_copy(hTf[:, 4 * half:4 * half + 4, :], bank(6)[:, :].rearrange("p (a b) -> p a b", a=4)), reads=[PSK[6]], writes=["hTf.%d" % half])

        def tail_R(i, part):
            for ec in range(4 * part, 4 * part + 4):
                S.pe(lambda e, ec=ec: e.matmul(bank(6)[:, 0:20], lhsT=hTf[:, ec, :], rhs=rw[:, ec, :], start=(ec == 0), stop=(ec == 7)), reads=["hTf.%d" % (ec // 4)] + RWK, writes=[PSK[6]])
            if part == 1:
                S.dve(lambda e, i=i: e.tensor_tensor(out=logits[:, i, :], in0=bank(6)[:, 0:20], in1=rb_bc[:, :], op=ALU.add), reads=[PSK[6], "rb0", "rb1"], writes=["logits.%d" % i])

        TD = [int(v) for v in os.environ.get("KTAIL", "0,1,2,3,4,5,9,11,12,14").split(",")]
        TAIL = [(TD[0], tail_N), (TD[1], tail_T), (TD[2], lambda i: tail_Oq(i, 0)), (TD[3], lambda i: tail_Oq(i, 1)), (TD[4], lambda i: tail_Oq(i, 2)), (TD[5], lambda i: tail_Oq(i, 3)),
                (TD[6], tail_L), (TD[7], lambda i: tail_H(i, 0)), (TD[8], lambda i: tail_H(i, 1)), (TD[9], lambda i: (tail_R(i, 0), tail_R(i, 1)))]
        NSTEP = len(TAIL)
        done_steps = set()

        def emit_step(t, k):
            if t < 0 or (t, k) in done_steps:
                return
            for kk in range(k):
                emit_step(t, kk)
            emit_step(t - 1, k)
            if k == 1:
                emit_step(t - 1, 5)
            if k == 7:
                emit_step(t - 1, NSTEP - 1)
            if k == 0:
                for kk in range(NSTEP):
                    emit_step(t - 2, kk)
            done_steps.add((t, k))
            TAIL[k][1](t)

        pending = []
        def tick():
            keep = []
            for ent in pending:
                if ent[0] <= 0:
                    emit_step(ent[1], ent[2])
                else:
                    ent[0] -= 1
                    keep.append(ent)
            pending[:] = keep
        prev = None
        for gi, u in enumerate(units):
            emit_S(u, gi)
            emit_E(u, gi)
            if prev is not None:
                emit_V(prev[0], prev[1])
                if prev[0][0] != u[0]:
                    for k, (dly, fn) in enumerate(TAIL):
                        pending.append([dly, prev[0][0], k])
            tick()
            prev = (u, gi)
        if prev is not None:
            emit_V(prev[0], prev[1])
            for k, (dly, fn) in enumerate(TAIL):
                pending.append([dly, prev[0][0], k])
        while pending:
            tick()

        if dbg == "att" and s == 0:
            fence()
            RACC.reset()
            cvt = RACC.alloc([NT * 1024], BF16)
            d1 = dbg_out("dbg_hT", [128, 8 * SEQ]); d2 = dbg_out("dbg_logits", [128, NT * 20])
            cv2 = RACC.alloc([2 * SEQ], F32)
            for q in range(4):
                S.dve(lambda e, q=q: e.tensor_copy(cv2[:, :], hT[:, 2 * q:2 * q + 2, :].rearrange("p a b -> p (a b)")), reads=[], writes=["cv2"])
                outs.append(S.dma("sp", lambda e, q=q: e.dma_start(out=d1[:, 2 * q * SEQ:(2 * q + 2) * SEQ], in_=cv2[:, :]), reads=["cv2"]))
            outs.append(S.dma("sp", lambda e: e.dma_start(out=d2[:, :], in_=logits.rearrange("p t j -> p (t j)")), reads=["logits.%d" % i for i in range(int(os.environ.get("KATT_TILES", NT)))] if int(os.environ.get("KATT_STAGE", 9)) >= 7 else []))
            break


        S.fold_now = os.environ.get("KFOLD", "all") == "all"
        fence()
        TT.reset(); RW.reset(); RACC.reset()
        S.dma("sp", lambda e: e.dma_start(out=lnG[:, :], in_=ln2_g[0:1, :].to_broadcast([128, 1024])), writes=["lnG"])
        S.dma("sp", lambda e: e.dma_start(out=lnB[:, :], in_=ln2_b[0:1, :].to_broadcast([128, 1024])), writes=["lnB"])
        LOGK = ["logits.%d" % i for i in range(NT)]
        lg = logits[:, :, 0:4]
        le4 = logits[:, :, 4:20].rearrange("p t (g j) -> p t g j", g=4)
        gmax = TT.alloc([NT], F32); goh = TT.alloc([NT, 4], F32); gex = TT.alloc([NT, 4], F32)
        gsum = TT.alloc([NT], F32); gval = TT.alloc([NT], F32)
        tmp16 = TT.alloc([NT, 4, 4], F32); esel = TT.alloc([NT, 4], F32)
        m1 = TT.alloc([NT], F32); oh1 = TT.alloc([NT, 4], F32); e2 = TT.alloc([NT, 4], F32)
        m2 = TT.alloc([NT], F32); oh2 = TT.alloc([NT, 4], F32); dd = TT.alloc([NT], F32)
        w1 = TT.alloc([NT], F32); w2 = TT.alloc([NT], F32); cw1 = TT.alloc([NT], F32); cw2 = TT.alloc([NT], F32)
        cj = TT.alloc([NT, 4], F32); cj2 = TT.alloc([NT, 4], F32)
        bc4 = lambda a: a.unsqueeze(2).to_broadcast([128, NT, 4])
        S.dve(lambda e: e.tensor_reduce(out=gmax[:, :], in_=lg, axis=AX.X, op=ALU.max), reads=LOGK, writes=["gmax"])
        S.dve(lambda e: e.tensor_tensor(out=goh[:, :, :], in0=lg, in1=bc4(gmax[:, :]), op=ALU.is_equal), reads=LOGK + ["gmax"], writes=["goh"])
        S.dve(lambda e: e.tensor_tensor(out=gex[:, :, :], in0=lg, in1=bc4(gmax[:, :]), op=ALU.subtract), reads=LOGK + ["gmax"], writes=["gex"])
        S.act(lambda e: e.activation(out=gex[:, :, :], in_=gex[:, :, :], func=AF.Exp), reads=["gex"], writes=["gex"])
        S.dve(lambda e: e.tensor_reduce(out=gsum[:, :], in_=gex[:, :, :], axis=AX.X, op=ALU.add), reads=["gex"], writes=["gsum"])
        S.dve(lambda e: e.reciprocal(gval[:, :], gsum[:, :]), reads=["gsum"], writes=["gval"])
        S.dve(lambda e: e.tensor_tensor(out=tmp16[:, :, :, :], in0=le4, in1=goh[:, :, :].unsqueeze(3).to_broadcast([128, NT, 4, 4]), op=ALU.mult), reads=LOGK + ["goh"], writes=["tmp16"])
        S.dve(lambda e: e.tensor_reduce(out=esel[:, :, :], in_=tmp16.rearrange("p t g j -> p t j g"), axis=AX.X, op=ALU.add), reads=["tmp16"], writes=["esel"])
        S.dve(lambda e: e.tensor_reduce(out=m1[:, :], in_=esel[:, :, :], axis=AX.X, op=ALU.max), reads=["esel"], writes=["m1"])
        S.dve(lambda e: e.tensor_tensor(out=oh1[:, :, :], in0=esel[:, :, :], in1=bc4(m1[:, :]), op=ALU.is_equal), reads=["esel", "m1"], writes=["oh1"])
        S.dve(lambda e: e.scalar_tensor_tensor(out=e2[:, :, :], in0=oh1[:, :, :], scalar=-1e30, in1=esel[:, :, :], op0=ALU.mult, op1=ALU.add), reads=["oh1", "esel"], writes=["e2"])
        S.dve(lambda e: e.tensor_reduce(out=m2[:, :], in_=e2[:, :, :], axis=AX.X, op=ALU.max), reads=["e2"], writes=["m2"])
        S.dve(lambda e: e.tensor_tensor(out=oh2[:, :, :], in0=e2[:, :, :], in1=bc4(m2[:, :]), op=ALU.is_equal), reads=["e2", "m2"], writes=["oh2"])
        S.dve(lambda e: e.tensor_tensor(out=dd[:, :], in0=m2[:, :], in1=m1[:, :], op=ALU.subtract), reads=["m1", "m2"], writes=["dd"])
        S.act(lambda e: e.activation(out=dd[:, :], in_=dd[:, :], func=AF.Exp), reads=["dd"], writes=["dd"])
        S.dve(lambda e: e.tensor_scalar(out=w1[:, :], in0=dd[:, :], scalar1=1.0, scalar2=None, op0=ALU.add), reads=["dd"], writes=["w1"])
        S.dve(lambda e: e.reciprocal(w1[:, :], w1[:, :]), reads=["w1"], writes=["w1"])
        S.dve(lambda e: e.tensor_tensor(out=w2[:, :], in0=dd[:, :], in1=w1[:, :], op=ALU.mult), reads=["dd", "w1"], writes=["w2"])
        S.dve(lambda e: e.tensor_tensor(out=cw1[:, :], in0=gval[:, :], in1=w1[:, :], op=ALU.mult), reads=["gval", "w1"], writes=["cw1"])
        S.dve(lambda e: e.tensor_tensor(out=cw2[:, :], in0=gval[:, :], in1=w2[:, :], op=ALU.mult), reads=["gval", "w2"], writes=["cw2"])
        S.dve(lambda e: e.tensor_tensor(out=cj[:, :, :], in0=oh1[:, :, :], in1=bc4(cw1[:, :]), op=ALU.mult), reads=["oh1", "cw1"], writes=["cj"])
        S.dve(lambda e: e.tensor_tensor(out=cj2[:, :, :], in0=oh2[:, :, :], in1=bc4(cw2[:, :]), op=ALU.mult), reads=["oh2", "cw2"], writes=["cj2"])
        S.dve(lambda e: e.tensor_tensor(out=cj[:, :, :], in0=cj[:, :, :], in1=cj2[:, :, :], op=ALU.add), reads=["cj", "cj2"], writes=["cj"])
        S.dve(lambda e: e.tensor_tensor(out=comb.rearrange("p t (g j) -> p t g j", g=4), in0=goh[:, :, :].unsqueeze(3).to_broadcast([128, NT, 4, 4]), in1=cj[:, :, :].unsqueeze(2).to_broadcast([128, NT, 4, 4]), op=ALU.mult),
              reads=["goh", "cj"], writes=["comb"])

        S.fold_now = os.environ.get("KFOLD", "all") in ("moe", "attmoe", "all")
        acc = RACC.alloc([NT, 1024], F32)
        sg = [TT.alloc([512], BF16) for _ in range(2)]
        actT = [TT.alloc([4, 512], BF16) for _ in range(2)]
        obuf = [TT.alloc([1024], F32) for _ in range(2)]
        bst2 = [TT.alloc([2, 6], F32) for _ in range(2)]
        mv2 = [TT.alloc([2], F32) for _ in range(2)]
        rs2 = [TT.alloc([1], F32) for _ in range(2)]
        for q in range(4):
            S.dma("sp", lambda e, q=q, s=s: e.dma_start(out=acc[:, 4 * q:4 * q + 4, :], in_=h_scr[s, q * 512:(q + 1) * 512, :].rearrange("(t p) d -> p t d", p=128)),
                  reads=["h_scr.%d" % t for t in range(4 * q, 4 * q + 4)], writes=["acc.%d" % t for t in range(4 * q, 4 * q + 4)])
        def ln2_stats(t):
            b2 = t % 2
            for c2 in range(2):
                S.dve(lambda e, c2=c2, t=t, b2=b2: e.bn_stats(bst2[b2][:, c2, :], acc[:, t, c2 * 512:(c2 + 1) * 512]), reads=["acc.%d" % t], writes=["bst2%d.%d" % (b2, c2)])
            S.dve(lambda e, b2=b2: e.bn_aggr(mv2[b2][:, :], bst2[b2][:, :, :]), reads=["bst2%d.0" % b2, "bst2%d.1" % b2], writes=["mv2%d" % b2])

        def ln2_apply(t):
            b2 = t % 2
            S.act(lambda e, b2=b2: e.activation(out=rs2[b2][:, :], in_=mv2[b2][:, 1:2], func=AF.Ln, bias=LN_EPS, scale=1.0), reads=["mv2%d" % b2], writes=["rs2%d" % b2])
            S.act(lambda e, b2=b2: e.activation(out=rs2[b2][:, :], in_=rs2[b2][:, :], func=AF.Exp, scale=-0.5), reads=["rs2%d" % b2], writes=["rs2%d" % b2])
            S.dve(lambda e, t=t, b2=b2: e.tensor_scalar(out=obuf[b2][:, :], in0=acc[:, t, :], scalar1=mv2[b2][:, 0:1], scalar2=rs2[b2][:, 0:1], op0=ALU.subtract, op1=ALU.mult),
                  reads=["acc.%d" % t, "mv2%d" % b2, "rs2%d" % b2], writes=["obuf%d" % b2])
            S.dve(lambda e, b2=b2: e.tensor_tensor(out=obuf[b2][:, :], in0=obuf[b2][:, :], in1=lnG[:, :], op=ALU.mult), reads=["obuf%d" % b2, "lnG"], writes=["obuf%d" % b2])
            S.pool(lambda e, b2=b2: e.tensor_tensor(out=obuf[b2][:, :], in0=obuf[b2][:, :], in1=lnB[:, :], op=ALU.add), reads=["obuf%d" % b2, "lnB"], writes=["obuf%d" % b2])
            outs.append(S.dma("sp", lambda e, t=t, b2=b2, s=s: e.dma_start(out=out[s, t * 128:(t + 1) * 128, :], in_=obuf[b2][:, :]), reads=["obuf%d" % b2], writes=["out.%d.%d" % (s, t)]))

        ln2_prev = []
        if NEXP > 1:
            load_expert(1)
        cg = 0; cd = 0
        for ex in range(NEXP):
            sl = ex % 2
            for tb in range(4):
                ab = (ex * 4 + tb) % 2
                hk = ["hT.%d.%d" % (i, hf) for i in range(4 * tb, 4 * tb + 4) for hf in range(2)]
                for fc in range(4):
                    pg = cg % 2; cg += 1
                    for k in range(8):
                        S.pe(lambda e, pg=pg, k=k, fc=fc, tb=tb, sl=sl: e.matmul(bank(pg)[:, :], lhsT=wgu[sl][:, k, fc * 128:(fc + 1) * 128], rhs=hT[:, k, tb * 512:(tb + 1) * 512], start=(k == 0), stop=(k == 7)),
                             reads=["wg.%d" % sl] + hk, writes=[PSK[pg]])
                    for k in range(8):
                        S.pe(lambda e, pg=pg, k=k, fc=fc, tb=tb, sl=sl: e.matmul(bank(2 + pg)[:, :], lhsT=wgu[sl][:, k, 512 + fc * 128:512 + (fc + 1) * 128], rhs=hT[:, k, tb * 512:(tb + 1) * 512], start=(k == 0), stop=(k == 7)),
                             reads=["wu.%d" % sl] + hk, writes=[PSK[2 + pg]])
                    S.act(lambda e, pg=pg: e.activation(out=sg[pg][:, :], in_=bank(pg)[:, :], func=AF.Silu), reads=[PSK[pg]], writes=["sg%d" % pg])
                    S.dve(lambda e, pg=pg, ab=ab, fc=fc: e.tensor_tensor(out=actT[ab][:, fc, :], in0=bank(2 + pg)[:, :], in1=sg[pg][:, :], op=ALU.mult),
                          reads=[PSK[2 + pg], "sg%d" % pg], writes=["actT%d.%d" % (ab, fc)])
                for tt in range(4):
                    t = tb * 4 + tt
                    pdi = 2 + (cd % 2); cd += 1
                    for half in range(2):
                        for fc in range(4):
                            S.pe(lambda e, pdi=pdi, half=half, fc=fc, ab=ab, tt=tt, sl=sl: e.matmul(pd[pdi][:, half * 512:(half + 1) * 512], lhsT=actT[ab][:, fc, tt * 128:(tt + 1) * 128], rhs=wdn[sl][:, fc, half * 512:(half + 1) * 512], start=(fc == 0), stop=(fc == 3)),
                                 reads=["actT%d.%d" % (ab, fc), "wd.%d" % sl], writes=[PSK[2 * pdi + half]])
                    S.dve(lambda e, pdi=pdi, t=t, ex=ex: e.scalar_tensor_tensor(out=acc[:, t, :], in0=pd[pdi][:, :], scalar=comb[:, t, ex:ex + 1], in1=acc[:, t, :], op0=ALU.mult, op1=ALU.add),
                          reads=[PSK[2 * pdi], PSK[2 * pdi + 1], "comb", "acc.%d" % t], writes=["acc.%d" % t])
                    if ex == NEXP - 1:
                        ln2_stats(t)
                        if ln2_prev:
                            ln2_apply(ln2_prev.pop())
                        ln2_prev.append(t)
            if ex + 2 < NEXP:
                load_expert(ex + 2)
        while ln2_prev:
            ln2_apply(ln2_prev.pop())
        S.fold_now = os.environ.get("KFOLD", "all") == "all"
        if s + 1 < NSEQ:
            fence()
        if dbg == "one":
            break

    with nc.allow_non_contiguous_dma(reason="tiny constant loads"):
        st = S.emit(outs)
    return nc, st, dbg_t


_CACHE = {}


def _get_program():
    if "p" not in _CACHE:
        _CACHE["p"] = build_program(dbg=os.environ.get("KDBG", ""))
    return _CACHE["p"]


def kernel(**inputs):
    nc, st, dbg_t = _get_program()
    f = lambda a: np.ascontiguousarray(np.asarray(a, dtype=np.float32))
    x = f(inputs["x"])
    shared = {
        "w_in": f(inputs["w_in"])[0], "b_in": f(inputs["b_in"]).reshape(1, DIN),
        "conv_w": f(inputs["conv_w"])[0], "conv_b": f(inputs["conv_b"]).reshape(1, 1024),
        "a_log": f(inputs["a_log"]).reshape(1, 8), "d_skip": f(inputs["d_skip"]).reshape(1, 8),
        "ssd_norm_g": f(inputs["ssd_norm_g"]).reshape(1, 512), "w_out": f(inputs["w_out"])[0],
        "ln1_g": f(inputs["ln1_g"]).reshape(1, DM), "ln1_b": f(inputs["ln1_b"]).reshape(1, DM),
        "router_group_w": f(inputs["router_group_w"])[0], "router_group_b": f(inputs["router_group_b"]).reshape(1, 4),
        "router_expert_w": f(inputs["router_expert_w"])[0], "router_expert_b": f(inputs["router_expert_b"]).reshape(1, 16),
        "w_gate": f(inputs["w_gate"])[0], "w_up": f(inputs["w_up"])[0], "w_down": f(inputs["w_down"])[0],
        "ln2_g": f(inputs["ln2_g"]).reshape(1, DM), "ln2_b": f(inputs["ln2_b"]).reshape(1, DM),
    }
    ncores = int(os.environ.get("KCORES", NCORES))
    in_maps = []
    for c in range(ncores):
        m = dict(shared)
        m["x"] = np.ascontiguousarray(x[c * NSEQ:(c + 1) * NSEQ])
        in_maps.append(m)
    res = run_bass_kernel_spmd(nc, in_maps, core_ids=list(range(ncores)))
    if os.environ.get("KDBG", ""):
        _CACHE["dbg"] = res.results
    outp = np.concatenate([r["out"] for r in res.results], axis=0)
    return outp.astype(np.float32)
```

```python
import os
import numpy as np
import concourse.bass as bass
import concourse.mybir as mybir
from concourse.bass_utils import run_bass_kernel_spmd

F32 = mybir.dt.float32
BF16 = mybir.dt.bfloat16
U8 = mybir.dt.uint8
AF = mybir.ActivationFunctionType
ALU = mybir.AluOpType
AX = mybir.AxisListType

NCORES = 8
NSEQ = 2
SEQ = 2048
NT = 16
DM = 1024
DIN = 3088
ALPHA = float(2.0 ** 0.25)
LN_EPS = 1e-5
RMS_EPS = 1e-5
ATT_SCALE = 0.125
NEG = -30000.0
FOLD_ENG = tuple(os.environ.get("KFOLDENG", "act,dve").split(","))


class Op:
    __slots__ = ("eng", "fn", "reads", "writes", "deps", "signal", "sigval", "dma", "gi", "nofence", "fold")


class Sched:
    COMPUTE = ("pe", "act", "dve", "pool")

    def __init__(self, nc, n_dma_sems=40):
        self.nc = nc
        self.h = {"pe": nc.tensor, "act": nc.scalar, "dve": nc.vector, "pool": nc.gpsimd, "sp": nc.sync}
        self.ops = []
        self.last_w = {}
        self.readers = {}
        self.n_dma_sems = n_dma_sems
        self.live_dma = []
        self.nfence = 0
        self.fold_now = os.environ.get("KFOLD", "all") == "all"

    def add(self, eng, fn, reads=(), writes=(), dma=False, nofence=False):
        o = Op()
        o.eng = eng; o.fn = fn; o.reads = tuple(reads); o.writes = tuple(writes)
        o.deps = []; o.signal = False; o.sigval = None; o.dma = dma; o.gi = len(self.ops); o.nofence = nofence
        o.fold = self.fold_now
        for r in o.reads:
            p = self.last_w.get(r)
            if p is not None:
                self._dep(o, p, True)
            if r.startswith("ps"):
                rd = self.readers.get(r)
                if rd:
                    for q in rd.values():
                        if q.eng != eng:
                            self._dep(o, q, True)
        for w in o.writes:
            p = self.last_w.get(w)
            if p is not None:
                self._dep(o, p, False)
            rd = self.readers.get(w)
            if rd:
                for q in rd.values():
                    self._dep(o, q, False)
        for r in o.reads:
            d = self.readers.setdefault(r, {})
            d[("dma", o.gi) if dma else eng] = o
        for w in o.writes:
            self.last_w[w] = o
            self.readers[w] = {}
        self.ops.append(o)
        if dma and not nofence:
            self.live_dma.append(o)
        return o

    def _dep(self, o, p, raw):
        if p is o:
            return
        if (not p.dma) and (not o.dma) and p.eng == o.eng:
            if o.eng == "pe":
                return
        o.deps.append(p)
        p.signal = True

    def pe(self, fn, reads=(), writes=()): return self.add("pe", fn, reads, writes)
    def act(self, fn, reads=(), writes=()): return self.add("act", fn, reads, writes)
    def dve(self, fn, reads=(), writes=()): return self.add("dve", fn, reads, writes)
    def pool(self, fn, reads=(), writes=()): return self.add("pool", fn, reads, writes)
    def dma(self, q, fn, reads=(), writes=(), nofence=False):
        return self.add(q, fn, reads, writes, dma=True, nofence=nofence)

    def fence(self, scratch):
        n = self.nfence; self.nfence += 1
        a_keys = []
        col = {"pe": None, "act": 0, "dve": 1, "pool": 2}
        for e in ("act", "dve", "pool"):
            k = "fenceA.%d.%s" % (n, e)
            c = col[e]
            if e == "act":
                self.add(e, (lambda eh, c=c: eh.activation(out=scratch[:, c:c + 1], in_=scratch[:, 8:9], func=AF.Copy)), reads=(), writes=(k,))
            else:
                self.add(e, (lambda eh, c=c: eh.memset(scratch[:, c:c + 1], 0.0)), reads=(), writes=(k,))
            a_keys.append(k)
        k = "fenceA.%d.pe" % n
        self.add("pe", (lambda eh: eh.matmul(self.fence_ps[0:1, 0:1], lhsT=self.fence_w[0:1, 0:1], rhs=self.fence_w[0:1, 0:1], start=True, stop=True)),
                 reads=(), writes=(k, "ps7"))
        a_keys.append(k)
        dmas = self.live_dma
        self.live_dma = []
        for e in ("act", "dve", "pool", "pe", "sp"):
            kb = "fenceB.%d.%s" % (n, e)
            if e == "act":
                o = self.add(e, (lambda eh: eh.activation(out=scratch[:, 3:4], in_=scratch[:, 8:9], func=AF.Copy)), reads=a_keys, writes=(kb,))
            elif e == "pe":
                o = self.add(e, (lambda eh: eh.matmul(self.fence_ps[0:1, 1:2], lhsT=self.fence_w[0:1, 0:1], rhs=self.fence_w[0:1, 0:1], start=True, stop=True)),
                             reads=a_keys, writes=(kb, "ps7"))
            elif e == "sp":
                o = self.add(e, (lambda eh: eh.nop()), reads=a_keys, writes=(kb,))
            else:
                c = 4 if e == "dve" else 5
                o = self.add(e, (lambda eh, c=c: eh.memset(scratch[:, c:c + 1], 0.0)), reads=a_keys, writes=(kb,))
            for d in dmas:
                o.deps.append(d)

    def emit(self, final_wait_ops=()):
        nc = self.nc
        esem = {e: nc.alloc_semaphore("s_" + e) for e in self.COMPUTE}
        dsems = [nc.alloc_semaphore("s_dma%d" % i) for i in range(self.n_dma_sems)]
        dtotal = [0] * self.n_dma_sems
        dlast = [None] * self.n_dma_sems
        ecount = {e: 0 for e in self.COMPUTE}
        nd = 0
        nq = {"sp": 0, "pool": 0}
        half = self.n_dma_sems // 2
        for o in self.ops:
            if o.dma:
                qi = nq[o.eng]; nq[o.eng] += 1; nd += 1
                i = (qi % half) + (0 if o.eng == "sp" else half)
                prev = dlast[i]
                if prev is not None:
                    o.deps.append(prev)
                dtotal[i] += 16
                o.sigval = (dsems[i], dtotal[i], 1000 + i)
                dlast[i] = o
            elif o.signal:
                ecount[o.eng] += 1
                o.sigval = (esem[o.eng], ecount[o.eng], o.eng)
        known = {e: {} for e in self.h}
        nwaits = 0
        snaps = {}
        for o in self.ops:
            eh = self.h[o.eng]
            kn = known[o.eng]
            deps = sorted(o.deps, key=lambda p: -p.gi)
            todo = []
            for p in deps:
                s, v, key = p.sigval
                if kn.get(key, 0) >= v:
                    continue
                todo.append((s, v, key))
                kn[key] = v
                sn = snaps.get(p.gi)
                if sn:
                    for k2, v2 in sn.items():
                        if kn.get(k2, 0) < v2:
                            kn[k2] = v2
            fold = None
            if todo and o.fold and (o.eng == "pe" or (o.eng in FOLD_ENG and not o.dma)):
                fold = todo.pop()
            for (s, v, key) in todo:
                eh.wait_ge(s, v)
                nwaits += 1
            ins = o.fn(eh)
            if fold is not None:
                ins._wait_ge(fold[0], fold[1])
            if o.dma:
                ins.then_inc(o.sigval[0], 16)
                snaps[o.gi] = dict(kn)
            elif o.signal:
                ins.then_inc(o.sigval[0], 1)
                snaps[o.gi] = dict(kn)
        eh = self.h["sp"]
        for o in final_wait_ops:
            s, v, key = o.sigval
            eh.wait_ge(s, v)
        self.stats = dict(n_ops=len(self.ops), n_waits=nwaits, counts=dict(ecount), n_dma=nd)
        return self.stats


class Arena:
    def __init__(self, nc, name, nbytes):
        self.t = nc.alloc_sbuf_tensor(name, [128, nbytes], U8)
        self.n = nbytes
        self.off = 0

    def reset(self, off=0):
        self.off = off

    def alloc(self, shape, dtype, parts=128):
        esz = 2 if dtype == BF16 else 4
        n = esz
        for s in shape:
            n *= s
        off = (self.off + 31) // 32 * 32
        assert off + n <= self.n, (off, n, self.n)
        self.off = off + n
        flat = self.t[0:parts, off:off + n].bitcast(dtype)
        if len(shape) == 1:
            return flat
        names = " ".join("a%d" % i for i in range(len(shape)))
        kw = {"a%d" % i: shape[i] for i in range(1, len(shape))}
        return flat.rearrange("p (%s) -> p %s" % (names, names), **kw)


def build_program(dbg=False):
    nc = bass.Bass("TRN2", target_bir_lowering=False)
    S = Sched(nc)
    D = {}

    def din(name, shape):
        D[name] = nc.dram_tensor(name, list(shape), F32, kind="ExternalInput").ap()
        return D[name]

    x = din("x", [NSEQ, SEQ, DM])
    w_in = din("w_in", [DM, DIN])
    b_in = din("b_in", [1, DIN])
    conv_w = din("conv_w", [4, 1024])
    conv_b = din("conv_b", [1, 1024])
    a_log = din("a_log", [1, 8])
    d_skip = din("d_skip", [1, 8])
    ssd_g = din("ssd_norm_g", [1, 512])
    w_out = din("w_out", [DM, DM])
    ln1_g = din("ln1_g", [1, DM]); ln1_b = din("ln1_b", [1, DM])
    rg_w = din("router_group_w", [DM, 4]); rg_b = din("router_group_b", [1, 4])
    re_w = din("router_expert_w", [4, DM, 4]); re_b = din("router_expert_b", [1, 16])
    w_gate = din("w_gate", [16, DM, 512]); w_up = din("w_up", [16, DM, 512]); w_down = din("w_down", [16, 512, DM])
    ln2_g = din("ln2_g", [1, DM]); ln2_b = din("ln2_b", [1, DM])
    out = nc.dram_tensor("out", [NSEQ, SEQ, DM], F32, kind="ExternalOutput").ap()
    h_scr = nc.dram_tensor("h_scr", [NSEQ, SEQ, DM], F32).ap()
    dbg_t = {}

    def dbg_out(name, shape):
        dbg_t[name] = nc.dram_tensor(name, list(shape), F32, kind="ExternalOutput").ap()
        return dbg_t[name]

    CONST = Arena(nc, "CONST", 22 * 1024)
    RW = Arena(nc, "RW", 49408)
    RX = Arena(nc, "RX", 32768)
    RACC = Arena(nc, "RACC", 65536)
    MSSD = Arena(nc, "MSSD", 16384)
    TT = Arena(nc, "TT", nc.sbuf_bytes_remaining - 256)

    identB = CONST.alloc([128], BF16); identF = CONST.alloc([128], F32)
    Uf = CONST.alloc([128], F32); triU = CONST.alloc([128], BF16); maskneg = CONST.alloc([128], BF16)
    onesF = CONST.alloc([128], F32)
    ones_row = CONST.alloc([128], BF16, parts=1)
    bz_row = CONST.alloc([512], BF16, parts=1); bv_row = CONST.alloc([512], BF16, parts=1)
    bdtf = CONST.alloc([16], F32)
    bxbc = CONST.alloc([8], F32); bq = CONST.alloc([4], F32); bk = CONST.alloc([4], F32)
    convw = CONST.alloc([8, 4], F32); convb = CONST.alloc([8], F32)
    a_bc = CONST.alloc([8], F32); dskip_bc = CONST.alloc([8], F32)
    gssd_bc = CONST.alloc([512], F32)
    lnG = CONST.alloc([1024], F32); lnB = CONST.alloc([1024], F32)
    rw = CONST.alloc([8, 20], F32); rb_bc = CONST.alloc([20], F32)
    logits = CONST.alloc([NT, 20], F32); comb = CONST.alloc([NT, 16], F32)
    fsc = CONST.alloc([16], F32)
    S.fence_w = CONST.alloc([8], BF16)
    pd = [nc.alloc_psum_tensor("pd%d" % i, [128, 1024], F32) for i in range(4)]
    def bank(i):
        return pd[i // 2][:, (i % 2) * 512:(i % 2) * 512 + 512]
    def bankb(i):
        return bank(i).bitcast(BF16)
    PSK = ["ps%d" % i for i in range(8)]
    S.fence_ps = nc.alloc_sbuf_tensor("fence_dummy", [1, 8], F32)
    S.fence_ps = bank(7)[:, 504:512]

    def fence():
        S.fence(fsc)

    S.pool(lambda e: e.memset(fsc[:, :], 0.0), writes=["fsc"])
    S.pool(lambda e: e.memset(S.fence_w[:, :], 0.0), writes=["fence_w"])
    S.pool(lambda e: e.memset(identB[:, :], 1.0), writes=["identB"])
    S.pool(lambda e: e.affine_select(out=identB[:, :], in_=identB[:, :], pattern=[[-1, 128]], compare_op=ALU.is_equal, fill=0.0, base=0, channel_multiplier=1), reads=["identB"], writes=["identB"])
    S.pool(lambda e: e.memset(identF[:, :], 1.0), writes=["identF"])
    S.pool(lambda e: e.affine_select(out=identF[:, :], in_=identF[:, :], pattern=[[-1, 128]], compare_op=ALU.is_equal, fill=0.0, base=0, channel_multiplier=1), reads=["identF"], writes=["identF"])
    S.pool(lambda e: e.memset(Uf[:, :], 1.0), writes=["Uf"])
    S.pool(lambda e: e.affine_select(out=Uf[:, :], in_=Uf[:, :], pattern=[[1, 128]], compare_op=ALU.is_ge, fill=0.0, base=0, channel_multiplier=-1), reads=["Uf"], writes=["Uf"])
    S.pool(lambda e: e.memset(triU[:, :], 1.0), writes=["triU"])
    S.pool(lambda e: e.affine_select(out=triU[:, :], in_=triU[:, :], pattern=[[1, 128]], compare_op=ALU.is_ge, fill=0.0, base=0, channel_multiplier=-1), reads=["triU"], writes=["triU"])
    S.pool(lambda e: e.memset(maskneg[:, :], NEG), writes=["maskneg"])
    S.pool(lambda e: e.affine_select(out=maskneg[:, :], in_=maskneg[:, :], pattern=[[-1, 128]], compare_op=ALU.is_gt, fill=0.0, base=0, channel_multiplier=1), reads=["maskneg"], writes=["maskneg"])
    S.pool(lambda e: e.memset(onesF[:, :], 1.0), writes=["onesF"])
    S.pool(lambda e: e.memset(ones_row[:, :], 1.0), writes=["ones_row"])
    S.dma("pool", lambda e: e.dma_start(out=bz_row[:, :], in_=b_in[0:1, 0:512]), writes=["bz_row"])
    S.dma("pool", lambda e: e.dma_start(out=bv_row[:, :], in_=b_in[0:1, 2568:3080]), writes=["bv_row"])
    S.dma("sp", lambda e: e.dma_start(out=bdtf[:, 0:8], in_=b_in[0:1, 1536:1544].to_broadcast([128, 8])), writes=["bdtf0"])
    S.dma("sp", lambda e: e.dma_start(out=bdtf[:, 8:16], in_=b_in[0:1, 3080:3088].to_broadcast([128, 8])), writes=["bdtf1"])
    S.dma("sp", lambda e: e.dma_start(out=bxbc[:, :], in_=b_in[0, 512:1536].rearrange("(c p) -> p c", p=128)), writes=["bxbc"])
    S.dma("sp", lambda e: e.dma_start(out=bq[:, :], in_=b_in[0, 1544:2056].rearrange("(c p) -> p c", p=128)), writes=["bq"])
    S.dma("sp", lambda e: e.dma_start(out=bk[:, :], in_=b_in[0, 2056:2568].rearrange("(c p) -> p c", p=128)), writes=["bk"])
    for k in range(4):
        S.dma("sp", lambda e, k=k: e.dma_start(out=convw[:, :, k], in_=conv_w[k, :].rearrange("(c p) -> p c", p=128)), writes=["convw%d" % k])
    CONVW = ["convw%d" % k for k in range(4)]
    S.dma("sp", lambda e: e.dma_start(out=convb[:, :], in_=conv_b[0, :].rearrange("(c p) -> p c", p=128)), writes=["convb"])
    S.dma("sp", lambda e: e.dma_start(out=a_bc[:, :], in_=a_log[0:1, :].to_broadcast([128, 8])), writes=["a_bc"])
    S.act(lambda e: e.activation(out=a_bc[:, :], in_=a_bc[:, :], func=AF.Exp), reads=["a_bc"], writes=["a_bc"])
    S.dve(lambda e: e.tensor_scalar(out=a_bc[:, :], in0=a_bc[:, :], scalar1=-1.0, scalar2=None, op0=ALU.mult), reads=["a_bc"], writes=["a_bc"])
    S.dma("sp", lambda e: e.dma_start(out=dskip_bc[:, :], in_=d_skip[0:1, :].to_broadcast([128, 8])), writes=["dskip_bc"])
    S.dma("sp", lambda e: e.dma_start(out=gssd_bc[:, :], in_=ssd_g[0:1, :].to_broadcast([128, 512])), writes=["gssd_bc"])
    S.dma("sp", lambda e: e.dma_start(out=rw[:, :, 0:4], in_=rg_w.rearrange("(k p) j -> p k j", p=128)), writes=["rw0"])
    for g in range(4):
        S.dma("sp", lambda e, g=g: e.dma_start(out=rw[:, :, 4 + 4 * g:8 + 4 * g], in_=re_w[g].rearrange("(k p) j -> p k j", p=128)), writes=["rw%d" % (g + 1)])
    RWK = ["rw%d" % i for i in range(5)]
    S.dma("sp", lambda e: e.dma_start(out=rb_bc[:, 0:4], in_=rg_b[0:1, :].to_broadcast([128, 4])), writes=["rb0"])
    S.dma("sp", lambda e: e.dma_start(out=rb_bc[:, 4:20], in_=re_b[0:1, :].to_broadcast([128, 16])), writes=["rb1"])

    outs = []

    for s in range(NSEQ):
        RW.reset(); RX.reset(); RACC.reset(); MSSD.reset(); TT.reset()
        wgu = [None, None]; wdn = [None, None]
        wgu[0] = RW.alloc([8, 1024], BF16); wdn[0] = RW.alloc([4, 1024], BF16)
        wgu[1] = RW.alloc([8, 1024], BF16); wdn[1] = RW.alloc([4, 1024], BF16)
        RW.reset()
        wA = RW.alloc([8, 1544], BF16)
        xb = [RW.alloc([4, 1024], BF16) for _ in range(2)]
        xT = RX.alloc([8, SEQ], BF16)
        sz = RACC.alloc([NT, 512], BF16)
        xsB = RACC.alloc([NT, 768], BF16)
        BT = RACC.alloc([2, SEQ], BF16)
        CT = RACC.alloc([2, SEQ], BF16)
        xsT = MSSD.alloc([4, SEQ], BF16)
        dt_t = TT.alloc([NT, 8], F32)
        tt_mark = TT.off
        pre = [TT.alloc([SEQ + 3], BF16) for _ in range(2)]
        diagw = TT.alloc([8, 4, 128], BF16)
        dt_raw = TT.alloc([NT, 8], F32)

        w_in_v = w_in.rearrange("(k p) c -> p k c", p=128)
        S.dma("pool", lambda e, s=s: e.dma_start(out=xb[0][:, :, :], in_=x[s, 0:512, :].rearrange("(t p) d -> p t d", p=128)), writes=["xb0"])
        S.dma("pool", lambda e: e.dma_start(out=wA[:, :, 0:512], in_=w_in_v[:, :, 0:512]), writes=["wA.z"])
        S.dma("pool", lambda e: e.dma_start(out=wA[:, :, 1536:1544], in_=w_in_v[:, :, 1536:1544]), writes=["wA.dt"])
        S.dma("pool", lambda e, s=s: e.dma_start(out=xb[1][:, :, :], in_=x[s, 512:1024, :].rearrange("(t p) d -> p t d", p=128)), writes=["xb1"])
        for half in range(2):
            S.dma("pool", lambda e, half=half: e.dma_start(out=wA[:, 4 * half:4 * half + 4, 512:1536], in_=w_in_v[:, 4 * half:4 * half + 4, 512:1536]), writes=["wA.x%d" % half])
        for b in range(2):
            S.pool(lambda e, b=b: e.memset(pre[b][:, 0:3], 0.0), writes=["prepad%d" % b])
        ev = 0
        for blk in range(4):
            if blk >= 2:
                S.dma("pool", lambda e, blk=blk, s=s: e.dma_start(out=xb[blk % 2][:, :, :], in_=x[s, blk * 512:(blk + 1) * 512, :].rearrange("(t p) d -> p t d", p=128)),
                      writes=["xb%d" % (blk % 2)])
            for k in range(8):
                pb = (blk * 8 + k) % 2
                for t in range(4):
                    S.pe(lambda e, pb=pb, t=t, k=k, blk=blk: e.transpose(bankb(pb)[:, t * 128:(t + 1) * 128], xb[blk % 2][:, t, k * 128:(k + 1) * 128], identB[:, :]),
                         reads=["xb%d" % (blk % 2), "identB"], writes=[PSK[pb]])
                if ev % 2 == 0:
                    S.act(lambda e, pb=pb, k=k, blk=blk: e.copy(xT[:, k, blk * 512:(blk + 1) * 512], bankb(pb)[:, 0:512]), reads=[PSK[pb]], writes=["xT.%d.%d" % (k, blk)])
                else:
                    S.dve(lambda e, pb=pb, k=k, blk=blk: e.tensor_copy(xT[:, k, blk * 512:(blk + 1) * 512], bankb(pb)[:, 0:512]), reads=[PSK[pb]], writes=["xT.%d.%d" % (k, blk)])
                ev += 1
            for tt in range(4):
                t = blk * 4 + tt
                pz = 2 + (t % 2)
                xk = ["xT.%d.%d" % (k, blk) for k in range(8)]
                for k in range(8):
                    S.pe(lambda e, pz=pz, t=t, k=k: e.matmul(bank(pz)[:, :], lhsT=xT[:, k, t * 128:(t + 1) * 128], rhs=wA[:, k, 0:512], start=(k == 0), stop=False),
                         reads=[xk[k], "wA.z"], writes=[PSK[pz]])
                S.pe(lambda e, pz=pz: e.matmul(bank(pz)[:, :], lhsT=ones_row[0:1, :], rhs=bz_row[0:1, :], start=False, stop=True),
                     reads=["ones_row", "bz_row"], writes=[PSK[pz]])
                S.act(lambda e, pz=pz, t=t: e.activation(out=sz[:, t, :], in_=bank(pz)[:, :], func=AF.Silu), reads=[PSK[pz]], writes=["sz.%d" % t])
                for k in range(8):
                    S.pe(lambda e, t=t, k=k: e.matmul(bank(4)[:, t * 8:(t + 1) * 8], lhsT=xT[:, k, t * 128:(t + 1) * 128], rhs=wA[:, k, 1536:1544], start=(k == 0), stop=(k == 7)),
                         reads=[xk[k], "wA.dt"], writes=[PSK[4]])
        S.dve(lambda e: e.tensor_tensor(out=dt_raw[:, :, :], in0=bank(4)[:, 0:128].rearrange("p (t h) -> p t h", h=8), in1=bdtf[:, 0:8].unsqueeze(1).to_broadcast([128, NT, 8]), op=ALU.add),
              reads=[PSK[4], "bdtf0"], writes=["dt_raw"])
        for c in range(8):
            for k in range(4):
                S.dve(lambda e, c=c, k=k: e.tensor_scalar(out=diagw[:, c, k, :], in0=identB[:, :], scalar1=convw[:, c, k:k + 1], scalar2=None, op0=ALU.mult),
                      reads=["identB"] + CONVW, writes=["diagw.%d" % c])

        def a1_ip(c):
            pb_ = c % 2
            for blk in range(4):
                pc = 5 + (c * 4 + blk) % 2
                for k in range(8):
                    S.pe(lambda e, pc=pc, c=c, k=k, blk=blk: e.matmul(bank(pc)[:, :], lhsT=wA[:, k, 512 + c * 128:512 + (c + 1) * 128], rhs=xT[:, k, blk * 512:(blk + 1) * 512], start=(k == 0), stop=(k == 7)),
                         reads=["xT.%d.%d" % (k, blk), "wA.x%d" % (k // 4)], writes=[PSK[pc]])
                S.act(lambda e, pc=pc, c=c, blk=blk, pb_=pb_: e.activation(out=pre[pb_][:, 3 + blk * 512:3 + (blk + 1) * 512], in_=bank(pc)[:, :], func=AF.Identity, bias=bxbc[:, c:c + 1], scale=1.0),
                      reads=[PSK[pc], "bxbc"], writes=["pre%d.%d" % (pb_, blk)])

        def a1_conv(c):
            pb_ = c % 2
            if c < 4:
                dstt = xsT[:, c, :]; dk = "xsT.%d" % c
            elif c < 6:
                dstt = BT[:, c - 4, :]; dk = "BT.%d" % (c - 4)
            else:
                dstt = CT[:, c - 6, :]; dk = "CT.%d" % (c - 6)
            for blk in range(4):
                pc = 2 + (c * 4 + blk) % 2
                rk = ["pre%d.%d" % (pb_, blk), "diagw.%d" % c] + (["pre%d.%d" % (pb_, blk - 1)] if blk > 0 else ["prepad%d" % pb_])
                for k in range(4):
                    S.pe(lambda e, pc=pc, c=c, k=k, blk=blk, pb_=pb_: e.matmul(bank(pc)[:, :], lhsT=diagw[:, c, k, :], rhs=pre[pb_][:, blk * 512 + k:blk * 512 + k + 512], start=(k == 0), stop=(k == 3)),
                         reads=rk, writes=[PSK[pc]])
                S.act(lambda e, pc=pc, dstt=dstt, c=c, blk=blk: e.activation(out=dstt[:, blk * 512:(blk + 1) * 512], in_=bank(pc)[:, :], func=AF.Silu, bias=convb[:, c:c + 1], scale=1.0),
                      reads=[PSK[pc], "convb"], writes=[dk])

        a1_ip(0)
        for c in range(8):
            if c + 1 < 8:
                a1_ip(c + 1)
            a1_conv(c)
        for t in range(NT):
            pb = t % 2
            for c in range(6):
                src = xsT[:, c, t * 128:(t + 1) * 128] if c < 4 else BT[:, c - 4, t * 128:(t + 1) * 128]
                sk = "xsT.%d" % c if c < 4 else "BT.%d" % (c - 4)
                S.pe(lambda e, pb=pb, c=c, src=src: e.transpose(bankb(pb)[:, c * 128:(c + 1) * 128], src, identB[:, :]), reads=[sk, "identB"], writes=[PSK[pb]])
            if t % 2 == 0:
                S.dve(lambda e, pb=pb, t=t: e.tensor_copy(xsB[:, t, :], bankb(pb)[:, 0:768]), reads=[PSK[pb]], writes=["xsB.%d" % t])
            else:
                S.act(lambda e, pb=pb, t=t: e.copy(xsB[:, t, :], bankb(pb)[:, 0:768]), reads=[PSK[pb]], writes=["xsB.%d" % t])
        S.act(lambda e: e.activation(out=dt_t[:, :, :], in_=dt_raw[:, :, :], func=AF.Exp), reads=["dt_raw"], writes=["dt_t"])
        S.act(lambda e: e.activation(out=dt_t[:, :, :], in_=dt_t[:, :, :], func=AF.Ln, bias=1.0, scale=1.0), reads=["dt_t"], writes=["dt_t"])

        fence()
        MSSD.reset(); TT.reset(tt_mark)
        RW.reset()
        wB = RW.alloc([8, 1544], BF16)
        wo = RW.alloc([8, 1024], BF16)
        for half in range(2):
            S.dma("pool", lambda e, half=half: e.dma_start(out=wB[:, 4 * half:4 * half + 4, :], in_=w_in.rearrange("(k p) c -> p k c", p=128)[:, 4 * half:4 * half + 4, 1544:3088]),
                  writes=["wB"], nofence=True)
        for half in range(2):
            S.dma("pool", lambda e, half=half: e.dma_start(out=wo[:, 4 * half:4 * half + 4, :], in_=w_out.rearrange("(k p) c -> p k c", p=128)[:, 4 * half:4 * half + 4, :]),
                  writes=["wo"], nofence=True)
        m_ssd = MSSD.alloc([NT, 512], BF16)
        da = TT.alloc([NT, 8], F32); acum = TT.alloc([NT, 8], F32); nacum = TT.alloc([NT, 8], F32)
        alast = TT.alloc([NT, 8], F32); dte = TT.alloc([NT, 8], F32); ea = TT.alloc([NT, 8], F32)
        cdec = TT.alloc([NT, 8], F32); dtdte = TT.alloc([NT, 8], F32)
        stT = TT.alloc([8, 64], F32); stTb = TT.alloc([8, 64], BF16)
        LT = [TT.alloc([128], BF16) for _ in range(4)]
        MT = [TT.alloc([128], BF16) for _ in range(4)]
        xdt = [TT.alloc([8, 64], BF16) for _ in range(2)]
        xdtd = [TT.alloc([8, 64], BF16) for _ in range(2)]
        t1 = [TT.alloc([8, 64], F32)] * 2
        t2 = [TT.alloc([8, 64], F32) for _ in range(2)]
        yg = [TT.alloc([512], F32) for _ in range(2)]
        junk = TT.alloc([256], F32)
        ss = [TT.alloc([2], F32) for _ in range(2)]
        rstd = [TT.alloc([2], F32) for _ in range(2)]

        S.dve(lambda e: e.tensor_tensor(out=da[:, :, :], in0=dt_t[:, :, :], in1=a_bc[:, :].unsqueeze(1).to_broadcast([128, NT, 8]), op=ALU.mult), reads=["dt_t", "a_bc"], writes=["da"])
        daf = da.rearrange("p t h -> p (t h)")
        S.pe(lambda e: e.matmul(bank(5)[:, 0:128], lhsT=Uf[:, :], rhs=daf, start=True, stop=True), reads=["Uf", "da"], writes=[PSK[5]])
        S.pe(lambda e: e.matmul(bank(6)[:, 0:128], lhsT=onesF[:, :], rhs=daf, start=True, stop=True), reads=["onesF", "da"], writes=[PSK[6]])
        S.dve(lambda e: e.tensor_copy(acum.rearrange("p t h -> p (t h)"), bank(5)[:, 0:128]), reads=[PSK[5]], writes=["acum"])
        S.dve(lambda e: e.tensor_scalar(out=nacum.rearrange("p t h -> p (t h)"), in0=bank(5)[:, 0:128], scalar1=-1.0, scalar2=None, op0=ALU.mult), reads=[PSK[5]], writes=["nacum"])
        S.dve(lambda e: e.tensor_copy(alast.rearrange("p t h -> p (t h)"), bank(6)[:, 0:128]), reads=[PSK[6]], writes=["alast"])
        S.dve(lambda e: e.tensor_tensor(out=dte[:, :, :], in0=alast[:, :, :], in1=acum[:, :, :], op=ALU.subtract), reads=["alast", "acum"], writes=["dte"])
        S.act(lambda e: e.activation(out=dte[:, :, :], in_=dte[:, :, :], func=AF.Exp), reads=["dte"], writes=["dte"])
        S.act(lambda e: e.activation(out=ea[:, :, :], in_=acum[:, :, :], func=AF.Exp), reads=["acum"], writes=["ea"])
        S.act(lambda e: e.activation(out=cdec[:, :, :], in_=alast[:, :, :], func=AF.Exp), reads=["alast"], writes=["cdec"])
        S.dve(lambda e: e.tensor_tensor(out=dtdte[:, :, :], in0=dt_t[:, :, :], in1=dte[:, :, :], op=ALU.mult), reads=["dt_t", "dte"], writes=["dtdte"])
        S.pool(lambda e: e.memset(stT[:, :, :], 0.0), writes=["stT"])
        S.pool(lambda e: e.memset(stTb[:, :, :], 0.0), writes=["stTb"])

        def ssd_F(c):
            cs = slice(c * 128, (c + 1) * 128)
            b2 = c % 2
            py = 2 + b2
            xs_c = xsB[:, c, 0:512].rearrange("p (h d) -> p h d", h=8)
            S.pool(lambda e, c=c, b2=b2, xs_c=xs_c: e.tensor_tensor(out=xdt[b2][:, :, :], in0=xs_c, in1=dt_t[:, c, :].unsqueeze(2).to_broadcast([128, 8, 64]), op=ALU.mult),
                   reads=["xsB.%d" % c, "dt_t"], writes=["xdt%d" % b2])
            S.pool(lambda e, c=c, b2=b2, xs_c=xs_c: e.tensor_tensor(out=xdtd[b2][:, :, :], in0=xs_c, in1=dtdte[:, c, :].unsqueeze(2).to_broadcast([128, 8, 64]), op=ALU.mult),
                   reads=["xsB.%d" % c, "dtdte"], writes=["xdtd%d" % b2])
            S.pool(lambda e, c=c, b2=b2, xs_c=xs_c: e.tensor_tensor(out=t2[b2][:, :, :], in0=xs_c, in1=dskip_bc[:, :].unsqueeze(2).to_broadcast([128, 8, 64]), op=ALU.mult),
                   reads=["xsB.%d" % c, "dskip_bc"], writes=["t2%d" % b2])
            for g in range(2):
                S.pe(lambda e, g=g, cs=cs: e.matmul(bank(4)[:, g * 128:(g + 1) * 128], lhsT=BT[:, g, cs], rhs=CT[:, g, cs], start=True, stop=True),
                     reads=["BT.%d" % g, "CT.%d" % g], writes=[PSK[4]])
            for hh in range(2):
                pl = hh
                for h4 in range(4):
                    h = hh * 4 + h4
                    S.pe(lambda e, pl=pl, h4=h4, c=c, h=h: e.matmul(bank(pl)[:, h4 * 128:(h4 + 1) * 128], lhsT=da[:, c, h:h + 1].to_broadcast([128, 128]), rhs=Uf[:, :], start=True, stop=False),
                         reads=["da", "Uf"], writes=[PSK[pl]])
                    S.pe(lambda e, pl=pl, h4=h4: e.matmul(bank(pl)[:, h4 * 128:(h4 + 1) * 128], lhsT=identB[:, :], rhs=maskneg[:, :], start=False, stop=True),
                         reads=["identB", "maskneg"], writes=[PSK[pl]])
            for hh in range(2):
                pl = hh
                for h4 in range(4):
                    h = hh * 4 + h4
                    S.act(lambda e, pl=pl, h4=h4, c=c, h=h: e.activation(out=LT[h4][:, :], in_=bank(pl)[:, h4 * 128:(h4 + 1) * 128], func=AF.Exp, bias=nacum[:, c, h:h + 1], scale=1.0),
                          reads=[PSK[pl], "nacum"], writes=["LT%d" % h4])
                    g = h // 4
                    S.dve(lambda e, h4=h4, g=g: e.tensor_tensor(out=MT[h4][:, :], in0=bank(4)[:, g * 128:(g + 1) * 128], in1=LT[h4][:, :], op=ALU.mult),
                          reads=[PSK[4], "LT%d" % h4], writes=["MT%d" % h4])
                    S.pe(lambda e, h4=h4, h=h, b2=b2, py=py: e.matmul(bank(py)[:, h * 64:(h + 1) * 64], lhsT=MT[h4][:, :], rhs=xdt[b2][:, h, :], start=True, stop=True),
                         reads=["MT%d" % h4, "xdt%d" % b2], writes=[PSK[py]])

        def ssd_B(c):
            cs = slice(c * 128, (c + 1) * 128)
            b2 = c % 2
            py = 2 + b2
            if c > 0:
                for g in range(2):
                    S.pe(lambda e, g=g, cs=cs: e.matmul(bank(6)[:, g * 256:(g + 1) * 256], lhsT=CT[:, g, cs], rhs=stTb[:, 4 * g:4 * g + 4, :].rearrange("p h d -> p (h d)"), start=True, stop=True),
                         reads=["CT.%d" % g, "stTb"], writes=[PSK[6]])
            if c < NT - 1:
                for g in range(2):
                    S.pe(lambda e, g=g, c=c, b2=b2: e.matmul(bank(7)[:, g * 256:(g + 1) * 256], lhsT=xsB[:, c, 512 + g * 128:512 + (g + 1) * 128], rhs=xdtd[b2][:, 4 * g:4 * g + 4, :].rearrange("p h d -> p (h d)"), start=True, stop=True),
                         reads=["xsB.%d" % c, "xdtd%d" % b2], writes=[PSK[7]])
                S.dve(lambda e, c=c: e.tensor_tensor(out=stT[:, :, :], in0=stT[:, :, :], in1=cdec[:, c, :].unsqueeze(2).to_broadcast([128, 8, 64]), op=ALU.mult),
                      reads=["stT", "cdec"], writes=["stT"])
                S.dve(lambda e: e.tensor_tensor(out=stT[:, :, :], in0=bank(7)[:, 0:512].rearrange("p (h d) -> p h d", h=8), in1=stT[:, :, :], op=ALU.add),
                      reads=[PSK[7], "stT"], writes=["stT"])
            if c > 0:
                S.dve(lambda e, c=c, b2=b2: e.tensor_tensor(out=t1[b2][:, :, :], in0=bank(6)[:, :].rearrange("p (h d) -> p h d", h=8), in1=ea[:, c, :].unsqueeze(2).to_broadcast([128, 8, 64]), op=ALU.mult),
                      reads=[PSK[6], "ea"], writes=["t1"])
            if c < NT - 1:
                S.act(lambda e: e.copy(stTb[:, :, :], stT[:, :, :]), reads=["stT"], writes=["stTb"])
            if c > 0:
                S.dve(lambda e, b2=b2, py=py: e.tensor_tensor(out=t1[b2][:, :, :], in0=bank(py)[:, :].rearrange("p (h d) -> p h d", h=8), in1=t1[b2][:, :, :], op=ALU.add),
                      reads=[PSK[py], "t1"], writes=["t1"])
                S.dve(lambda e, b2=b2: e.tensor_tensor(out=t1[b2][:, :, :], in0=t1[b2][:, :, :], in1=t2[b2][:, :, :], op=ALU.add),
                      reads=["t1", "t2%d" % b2], writes=["t1"])
            else:
                S.dve(lambda e, b2=b2, py=py: e.tensor_tensor(out=t1[b2][:, :, :], in0=bank(py)[:, :].rearrange("p (h d) -> p h d", h=8), in1=t2[b2][:, :, :], op=ALU.add),
                      reads=[PSK[py], "t2%d" % b2], writes=["t1"])
            S.dve(lambda e, c=c, b2=b2: e.tensor_tensor(out=yg[b2][:, :], in0=t1[b2].rearrange("p h d -> p (h d)"), in1=sz[:, c, :], op=ALU.mult),
                  reads=["t1", "sz.%d" % c], writes=["yg%d" % b2])
            for g in range(2):
                S.act(lambda e, g=g, b2=b2: e.activation(out=junk[:, :], in_=yg[b2][:, g * 256:(g + 1) * 256], func=AF.Square, accum_out=ss[b2][:, g:g + 1]),
                      reads=["yg%d" % b2], writes=["junk", "ss%d.%d" % (b2, g)])
            S.act(lambda e, b2=b2: e.activation(out=rstd[b2][:, :], in_=ss[b2][:, :], func=AF.Ln, bias=RMS_EPS, scale=1.0 / 256.0),
                  reads=["ss%d.0" % b2, "ss%d.1" % b2], writes=["rstd%d" % b2])
            S.act(lambda e, b2=b2: e.activation(out=rstd[b2][:, :], in_=rstd[b2][:, :], func=AF.Exp, scale=-0.5), reads=["rstd%d" % b2], writes=["rstd%d" % b2])
            for g in range(2):
                S.dve(lambda e, g=g, b2=b2, c=c: e.scalar_tensor_tensor(out=m_ssd[:, c, g * 256:(g + 1) * 256], in0=yg[b2][:, g * 256:(g + 1) * 256], scalar=rstd[b2][:, g:g + 1], in1=gssd_bc[:, g * 256:(g + 1) * 256], op0=ALU.mult, op1=ALU.mult),
                      reads=["yg%d" % b2, "rstd%d" % b2, "gssd_bc"], writes=["m_ssd.%d" % c])

        ssd_F(0)
        for c in range(NT):
            if c + 1 < NT:
                ssd_F(c + 1)
            ssd_B(c)

        if dbg == 'ssd' and s == 0:
            RX.reset()
            dm = dbg_out("dbg_mssd", [128, NT * 512])
            cvt = RX.alloc([NT * 512], F32) if dbg == 'ssd' else None
            S.dve(lambda e: e.tensor_copy(cvt[:, :], m_ssd.rearrange("p t d -> p (t d)")), reads=["m_ssd.%d" % c for c in range(NT)], writes=["cvt"])
            outs.append(S.dma("sp", lambda e: e.dma_start(out=dm[:, :], in_=cvt[:, :]), reads=["cvt"]))
        if dbg == "ssd":
            break

        fence()
        RW.reset(); RACC.reset(); TT.reset()
        wB = RW.alloc([8, 1544], BF16)
        wo = RW.alloc([8, 1024], BF16)
        ah = RW.alloc([1024], F32)
        hTf = RW.alloc([8, 128], F32)
        qT = RACC.alloc([4, SEQ], BF16)
        kT = RACC.alloc([4, SEQ], BF16)
        v_aug = RACC.alloc([NT, 8, 65], BF16)
        xres = [RACC.alloc([1024], F32) for _ in range(2)]
        hpre = [RACC.alloc([1024], F32), TT.alloc([1024], F32)]
        tt_f0 = TT.off
        f_raw = TT.alloc([NT, 8], F32); Gc = TT.alloc([NT, 8], F32); tot = TT.alloc([NT, 8], F32)
        Pp = TT.alloc([NT, 8], F32); Gf = TT.alloc([NT, 8], F32); Gend = TT.alloc([NT, 8], F32)
        biasT = TT.alloc([8, NT, NT], F32)
        PT = [TT.alloc([8, 128], BF16) for _ in range(2)]
        yatt = [TT.alloc([8, 64], BF16) for _ in range(2)]
        _o = TT.off; TT.reset(tt_f0)
        Vs = [TT.alloc([4, 2, 65], BF16) for _ in range(2)]
        assert TT.off <= tt_f0 + 6 * 512
        TT.reset(_o)
        rden = [TT.alloc([8], F32) for _ in range(2)]
        mT = [TT.alloc([8, 128], BF16)] * 2
        bst = [TT.alloc([2, 6], F32) for _ in range(2)]
        mv = [TT.alloc([2], F32) for _ in range(2)]
        rs1 = [TT.alloc([1], F32) for _ in range(2)]

        S.dma("sp", lambda e: e.dma_start(out=lnG[:, :], in_=ln1_g[0:1, :].to_broadcast([128, 1024])), writes=["lnG"])
        S.dma("sp", lambda e: e.dma_start(out=lnB[:, :], in_=ln1_b[0:1, :].to_broadcast([128, 1024])), writes=["lnB"])
        S.pool(lambda e: e.memset(v_aug[:, :, :, 64:65], 1.0), writes=["v_ones"])
        for t in range(NT):
            pv = 4 + (t % 2)
            blk = t // 4
            for k in range(8):
                S.pe(lambda e, pv=pv, t=t, k=k: e.matmul(bank(pv)[:, :], lhsT=xT[:, k, t * 128:(t + 1) * 128], rhs=wB[:, k, 1024:1536], start=(k == 0), stop=False),
                     reads=["xT.%d.%d" % (k, blk), "wB"], writes=[PSK[pv]])
            S.pe(lambda e, pv=pv: e.matmul(bank(pv)[:, :], lhsT=ones_row[0:1, :], rhs=bv_row[0:1, :], start=False, stop=True),
                 reads=["ones_row", "bv_row"], writes=[PSK[pv]])
            if t % 2 == 0:
                S.act(lambda e, pv=pv, t=t: e.copy(v_aug[:, t, :, 0:64], bank(pv)[:, :].rearrange("p (h d) -> p h d", h=8)), reads=[PSK[pv]], writes=["v.%d" % t])
            else:
                S.dve(lambda e, pv=pv, t=t: e.tensor_copy(v_aug[:, t, :, 0:64], bank(pv)[:, :].rearrange("p (h d) -> p h d", h=8)), reads=[PSK[pv]], writes=["v.%d" % t])
            for k in range(8):
                S.pe(lambda e, t=t, k=k: e.matmul(bank(6)[:, t * 8:(t + 1) * 8], lhsT=xT[:, k, t * 128:(t + 1) * 128], rhs=wB[:, k, 1536:1544], start=(k == 0), stop=(k == 7)),
                     reads=["xT.%d.%d" % (k, blk), "wB"], writes=[PSK[6]])
        S.dve(lambda e: e.tensor_tensor(out=f_raw[:, :, :], in0=bank(6)[:, 0:128].rearrange("p (t h) -> p t h", h=8), in1=bdtf[:, 8:16].unsqueeze(1).to_broadcast([128, NT, 8]), op=ALU.add),
              reads=[PSK[6], "bdtf1"], writes=["f_raw"])
        S.act(lambda e: e.activation(out=f_raw[:, :, :], in_=f_raw[:, :, :], func=AF.Exp, scale=-1.0), reads=["f_raw"], writes=["f_raw"])
        S.act(lambda e: e.activation(out=f_raw[:, :, :], in_=f_raw[:, :, :], func=AF.Ln, bias=1.0, scale=1.0), reads=["f_raw"], writes=["f_raw"])
        def a2_cums():
            frf = f_raw.rearrange("p t h -> p (t h)")
            S.pe(lambda e: e.matmul(bank(7)[:, 0:128], lhsT=Uf[:, :], rhs=frf, start=True, stop=True), reads=["Uf", "f_raw"], writes=[PSK[7]])
            S.pe(lambda e: e.matmul(bank(7)[:, 128:256], lhsT=onesF[:, :], rhs=frf, start=True, stop=True), reads=["onesF", "f_raw"], writes=[PSK[7]])

        def a2_chain():
            S.dve(lambda e: e.tensor_copy(Gc.rearrange("p t h -> p (t h)"), bank(7)[:, 0:128]), reads=[PSK[7]], writes=["Gc"])
            S.dve(lambda e: e.tensor_copy(tot.rearrange("p t h -> p (t h)"), bank(7)[:, 128:256]), reads=[PSK[7]], writes=["tot"])
            S.pool(lambda e: e.memset(Pp[:, 0, :], 0.0), writes=["Pp"])
            for t in range(1, NT):
                S.dve(lambda e, t=t: e.tensor_tensor(out=Pp[:, t, :], in0=Pp[:, t - 1, :], in1=tot[:, t - 1, :], op=ALU.add), reads=["Pp", "tot"], writes=["Pp"])
            S.dve(lambda e: e.tensor_tensor(out=Gf[:, :, :], in0=Gc[:, :, :], in1=Pp[:, :, :], op=ALU.add), reads=["Gc", "Pp"], writes=["Gf"])
            S.dve(lambda e: e.tensor_tensor(out=Gend[:, :, :], in0=tot[:, :, :], in1=Pp[:, :, :], op=ALU.add), reads=["tot", "Pp"], writes=["Gend"])
            for h in range(8):
                S.dve(lambda e, h=h: e.tensor_tensor(out=biasT[:, h, :, :], in0=Gf[:, :, h].unsqueeze(1).to_broadcast([128, NT, NT]), in1=Gend[:, :, h].unsqueeze(2).to_broadcast([128, NT, NT]), op=ALU.subtract),
                      reads=["Gf", "Gend"], writes=["biasT"])
            bflat = biasT.rearrange("p h a b -> p (h a b)")
            S.dve(lambda e: e.tensor_scalar(out=bflat, in0=bflat, scalar1=0.0, scalar2=None, op0=ALU.min), reads=["biasT"], writes=["biasT"])
            S.act(lambda e: e.activation(out=bflat, in_=bflat, func=AF.Exp), reads=["biasT"], writes=["biasT"])

        evq = 0
        qk_groups = [(qk, p, blk) for qk in range(2) for p in range(4) for blk in range(4)]
        for gidx, (qk, p, blk) in enumerate(qk_groups):
            dstT = qT if qk == 0 else kT
            bcol = bq if qk == 0 else bk
            nm = "qT" if qk == 0 else "kT"
            pq = evq % 4
            for k in range(8):
                S.pe(lambda e, pq=pq, k=k, p=p, blk=blk, qk=qk: e.matmul(bank(pq)[:, :], lhsT=wB[:, k, qk * 512 + p * 128:qk * 512 + (p + 1) * 128], rhs=xT[:, k, blk * 512:(blk + 1) * 512], start=(k == 0), stop=(k == 7)),
                     reads=["xT.%d.%d" % (k, blk), "wB"], writes=[PSK[pq]])
            if evq % 2 == 0:
                S.act(lambda e, pq=pq, p=p, blk=blk, dstT=dstT, bcol=bcol: e.activation(out=dstT[:, p, blk * 512:(blk + 1) * 512], in_=bank(pq)[:, :], func=AF.Identity, bias=bcol[:, p:p + 1], scale=1.0),
                      reads=[PSK[pq], "bq", "bk"], writes=["%s.%d.%d" % (nm, p, blk)])
            else:
                S.dve(lambda e, pq=pq, p=p, blk=blk, dstT=dstT, bcol=bcol: e.tensor_scalar(out=dstT[:, p, blk * 512:(blk + 1) * 512], in0=bank(pq)[:, :], scalar1=bcol[:, p:p + 1], scalar2=None, op0=ALU.add),
                      reads=[PSK[pq], "bq", "bk"], writes=["%s.%d.%d" % (nm, p, blk)])
            evq += 1
            if gidx == 7:
                a2_cums()
            if gidx == 13:
                a2_chain()
        fence()
        RX.reset()
        hT = RX.alloc([8, SEQ], BF16)
        NEXP = int(os.environ.get("KNEXP", 16))

        def load_expert(ex):
            sl = ex % 2
            S.dma("pool", lambda e, ex=ex, sl=sl: e.dma_start(out=wgu[sl][:, :, 0:512], in_=w_gate[ex].rearrange("(k p) f -> p k f", p=128)), writes=["wg.%d" % sl], nofence=(ex == 0))
            S.dma("pool", lambda e, ex=ex, sl=sl: e.dma_start(out=wgu[sl][:, :, 512:1024], in_=w_up[ex].rearrange("(k p) f -> p k f", p=128)), writes=["wu.%d" % sl], nofence=(ex == 0))
            S.dma("pool", lambda e, ex=ex, sl=sl: e.dma_start(out=wdn[sl][:, :, :], in_=w_down[ex].rearrange("(k p) d -> p k d", p=128)), writes=["wd.%d" % sl], nofence=(ex == 0))


        load_expert(0)
        NTI = int(os.environ.get("KATT_TILES", NT))
        S.fold_now = os.environ.get("KFOLD", "all") in ("attmoe", "all")
        units = []
        for i in range(NTI):
            for p in range(4):
                for j0 in range(0, i + 1, 4):
                    units.append((i, p, list(range(j0, min(j0 + 4, i + 1)))))

        def emit_S(u, gi):
            i, p, js = u
            pb0 = 2 * (gi % 2)
            for jj, j in enumerate(js):
                for hh in range(2):
                    r0 = hh * 64
                    S.pe(lambda e, bk=pb0 + hh, jj=jj, j=j, p=p, r0=r0, i=i: e.matmul(bank(bk)[:, jj * 128:(jj + 1) * 128], lhsT=kT[r0:r0 + 64, p, j * 128:(j + 1) * 128], rhs=qT[r0:r0 + 64, p, i * 128:(i + 1) * 128], start=True, stop=True),
                         reads=["kT.%d.%d" % (p, j // 4), "qT.%d.%d" % (p, i // 4)], writes=[PSK[pb0 + hh]])

        def emit_E(u, gi):
            i, p, js = u
            pb0 = 2 * (gi % 2); pb = gi % 2
            n = len(js)
            for jj, j in enumerate(js):
                S.dve(lambda e, pb=pb, jj=jj, j=j, p=p, i=i: e.tensor_tensor(out=Vs[pb][:, jj, :, :], in0=v_aug[:, j, 2 * p:2 * p + 2, :], in1=biasT[:, 2 * p:2 * p + 2, i, j].unsqueeze(2).to_broadcast([128, 2, 65]), op=ALU.mult),
                      reads=["v.%d" % j, "v_ones", "biasT"], writes=["Vs%d.%d" % (pb, jj)])
            for hh in range(2):
                S.act(lambda e, bk=pb0 + hh, pb=pb, hh=hh, n=n: e.activation(out=PT[pb][:, hh * 4:hh * 4 + n, :].rearrange("p a b -> p (a b)"), in_=bank(bk)[:, 0:n * 128], func=AF.Exp, scale=ATT_SCALE),
                      reads=[PSK[pb0 + hh]], writes=["PT%d.h%d" % (pb, hh)])
                for jj, j in enumerate(js):
                    if j == i:
                        S.dve(lambda e, pb=pb, c8=hh * 4 + jj: e.tensor_tensor(out=PT[pb][:, c8, :], in0=PT[pb][:, c8, :], in1=triU[:, :], op=ALU.mult),
                              reads=["PT%d.h%d" % (pb, hh), "triU"], writes=["PT%d.h%d" % (pb, hh)])

        def emit_V(u, gi):
            i, p, js = u
            pb = gi % 2
            for jj, j in enumerate(js):
                for hh in range(2):
                    h = 2 * p + hh
                    po = 4 + h // 4; oc = (h % 4) * 65
                    S.pe(lambda e, pb=pb, c8=hh * 4 + jj, jj=jj, hh=hh, j=j, h=h, po=po, oc=oc, i=i: e.matmul(bank(po)[:, oc:oc + 65], lhsT=PT[pb][:, c8, :], rhs=Vs[pb][:, jj, hh, :], start=(j == 0 and h % 4 == 0), stop=(j == i and h % 4 == 3)),
                         reads=["PT%d.h%d" % (pb, hh), "Vs%d.%d" % (pb, jj)], writes=[PSK[po]])

        def tail_N(i):
            b2 = i % 2
            S.dma("sp", lambda e, i=i, b2=b2, s=s: e.dma_start(out=xres[b2][:, :], in_=x[s, i * 128:(i + 1) * 128, :]), writes=["xres%d" % b2])
            for hb in range(2):
                ov = bank(4 + hb)[:, 0:260].rearrange("p (h d) -> p h d", h=4)
                S.dve(lambda e, hb=hb, ov=ov, b2=b2: e.reciprocal(rden[b2][:, 4 * hb:4 * hb + 4], ov[:, :, 64]), reads=[PSK[4 + hb]], writes=["rden%d.%d" % (b2, hb)])
                S.dve(lambda e, hb=hb, ov=ov, b2=b2: e.tensor_tensor(out=yatt[b2][:, 4 * hb:4 * hb + 4, :], in0=ov[:, :, 0:64], in1=rden[b2][:, 4 * hb:4 * hb + 4].unsqueeze(2).to_broadcast([128, 4, 64]), op=ALU.mult),
                      reads=[PSK[4 + hb], "rden%d.%d" % (b2, hb)], writes=["yatt%d.%d" % (b2, hb)])

        def tail_T(i):
            b2 = i % 2
            yf = yatt[b2].rearrange("p h d -> p (h d)")
            for ec in range(8):
                src = m_ssd[:, i, ec * 128:(ec + 1) * 128] if ec < 4 else yf[:, (ec - 4) * 128:(ec - 3) * 128]
                rk = ["m_ssd.%d" % i] if ec < 4 else ["yatt%d.%d" % (b2, (ec - 4) // 2)]
                S.pe(lambda e, ec=ec, src=src: e.transpose(bankb(6)[:, ec * 128:(ec + 1) * 128], src, identB[:, :]), reads=rk + ["identB"], writes=[PSK[6]])
            S.dve(lambda e, b2=b2: e.tensor_copy(mT[b2].rearrange("p a b -> p (a b)"), bankb(6)[:, 0:1024]), reads=[PSK[6]], writes=["mT"])

        def tail_Oq(i, q):
            b2 = i % 2
            half = q // 2
            hp = hpre[b2]
            for ec in range(4 * (q % 2), 4 * (q % 2) + 4):
                S.pe(lambda e, ec=ec, b2=b2, half=half: e.matmul(bank(7)[:, :], lhsT=mT[b2][:, ec, :], rhs=wo[:, ec, half * 512:(half + 1) * 512], start=(ec == 0), stop=(ec == 7)),
                     reads=["mT", "wo"], writes=[PSK[7]])
            if q % 2 == 1:
                S.dve(lambda e, hp=hp, b2=b2, half=half: e.scalar_tensor_tensor(out=hp[:, half * 512:(half + 1) * 512], in0=xres[b2][:, half * 512:(half + 1) * 512], scalar=ALPHA, in1=bank(7)[:, :], op0=ALU.mult, op1=ALU.add),
                      reads=["xres%d" % b2, PSK[7]], writes=["hpre%d.%d" % (b2, half)])
                S.dve(lambda e, hp=hp, half=half, b2=b2: e.bn_stats(bst[b2][:, half, :], hp[:, half * 512:(half + 1) * 512]), reads=["hpre%d.%d" % (b2, half)], writes=["bst%d.%d" % (b2, half)])
            if q == 3:
                S.dve(lambda e, b2=b2: e.bn_aggr(mv[b2][:, :], bst[b2][:, :, :]), reads=["bst%d.0" % b2, "bst%d.1" % b2], writes=["mv%d" % b2])

        def tail_L(i):
            b2 = i % 2
            hp = hpre[b2]
            HK = ["hpre%d.0" % b2, "hpre%d.1" % b2]
            S.act(lambda e, b2=b2: e.activation(out=rs1[b2][:, :], in_=mv[b2][:, 1:2], func=AF.Ln, bias=LN_EPS, scale=1.0), reads=["mv%d" % b2], writes=["rs1%d" % b2])
            S.act(lambda e, b2=b2: e.activation(out=rs1[b2][:, :], in_=rs1[b2][:, :], func=AF.Exp, scale=-0.5), reads=["rs1%d" % b2], writes=["rs1%d" % b2])
            S.dve(lambda e, hp=hp, b2=b2: e.tensor_scalar(out=hp[:, :], in0=hp[:, :], scalar1=mv[b2][:, 0:1], scalar2=rs1[b2][:, 0:1], op0=ALU.subtract, op1=ALU.mult),
                  reads=HK + ["mv%d" % b2, "rs1%d" % b2], writes=HK)
            S.dve(lambda e, hp=hp: e.tensor_tensor(out=hp[:, :], in0=hp[:, :], in1=lnG[:, :], op=ALU.mult), reads=HK + ["lnG"], writes=HK)
            S.dve(lambda e, hp=hp: e.tensor_tensor(out=hp[:, :], in0=hp[:, :], in1=lnB[:, :], op=ALU.add), reads=HK + ["lnB"], writes=HK)
            S.dve(lambda e, hp=hp: e.tensor_scalar(out=ah[:, :], in0=hp[:, :], scalar1=ALPHA, scalar2=None, op0=ALU.mult), reads=HK, writes=["ah"])
            S.dma("sp", lambda e, i=i, s=s: e.dma_start(out=h_scr[s, i * 128:(i + 1) * 128, :], in_=ah[:, :]), reads=["ah"], writes=["h_scr.%d" % i])

        def tail_H(i, half):
            b2 = i % 2
            hp = hpre[b2]
            for q4 in range(4):
                ec = half * 4 + q4
                S.pe(lambda e, q4=q4, ec=ec, hp=hp: e.transpose(bank(6)[:, q4 * 128:(q4 + 1) * 128], hp[:, ec * 128:(ec + 1) * 128], identF[:, :]), reads=["hpre%d.%d" % (b2, half), "identF"], writes=[PSK[6]])
            S.dve(lambda e, half=half, i=i: e.tensor_copy(hT[:, 4 * half:4 * half + 4, i * 128:(i + 1) * 128], bank(6)[:, :].rearrange("p (a b) -> p a b", a=4)), reads=[PSK[6]], writes=["hT.%d.%d" % (i, half)])
            S.dve(lambda e, half=half: e.tensor_copy(hTf[:, 4 * half:4 * half + 4, :], bank(6)[:, :].rearrange("p (a b) -> p a b", a=4)), reads=[PSK[6]], writes=["hTf.%d" % half])

        def tail_R(i, part):
            for ec in range(4 * part, 4 * part + 4):
                S.pe(lambda e, ec=ec: e.matmul(bank(6)[:, 0:20], lhsT=hTf[:, ec, :], rhs=rw[:, ec, :], start=(ec == 0), stop=(ec == 7)), reads=["hTf.%d" % (ec // 4)] + RWK, writes=[PSK[6]])
            if part == 1:
                S.dve(lambda e, i=i: e.tensor_tensor(out=logits[:, i, :], in0=bank(6)[:, 0:20], in1=rb_bc[:, :], op=ALU.add), reads=[PSK[6], "rb0", "rb1"], writes=["logits.%d" % i])

        TD = [int(v) for v in os.environ.get("KTAIL", "0,1,2,3,4,5,9,11,12,14").split(",")]
        TAIL = [(TD[0], tail_N), (TD[1], tail_T), (TD[2], lambda i: tail_Oq(i, 0)), (TD[3], lambda i: tail_Oq(i, 1)), (TD[4], lambda i: tail_Oq(i, 2)), (TD[5], lambda i: tail_Oq(i, 3)),
                (TD[6], tail_L), (TD[7], lambda i: tail_H(i, 0)), (TD[8], lambda i: tail_H(i, 1)), (TD[9], lambda i: (tail_R(i, 0), tail_R(i, 1)))]
        NSTEP = len(TAIL)
        done_steps = set()

        def emit_step(t, k):
            if t < 0 or (t, k) in done_steps:
                return
            for kk in range(k):
                emit_step(t, kk)
            emit_step(t - 1, k)
            if k == 1:
                emit_step(t - 1, 5)
            if k == 7:
                emit_step(t - 1, NSTEP - 1)
            if k == 0:
                for kk in range(NSTEP):
                    emit_step(t - 2, kk)
            done_steps.add((t, k))
            TAIL[k][1](t)

        pending = []
        def tick():
            keep = []
            for ent in pending:
                if ent[0] <= 0:
                    emit_step(ent[1], ent[2])
                else:
                    ent[0] -= 1
                    keep.append(ent)
            pending[:] = keep
        prev = None
        for gi, u in enumerate(units):
            emit_S(u, gi)
            emit_E(u, gi)
            if prev is not None:
                emit_V(prev[0], prev[1])
                if prev[0][0] != u[0]:
                    for k, (dly, fn) in enumerate(TAIL):
                        pending.append([dly, prev[0][0], k])
            tick()
            prev = (u, gi)
        if prev is not None:
            emit_V(prev[0], prev[1])
            for k, (dly, fn) in enumerate(TAIL):
                pending.append([dly, prev[0][0], k])
        while pending:
            tick()

        if dbg == "att" and s == 0:
            fence()
            RACC.reset()
            cvt = RACC.alloc([NT * 1024], BF16)
            d1 = dbg_out("dbg_hT", [128, 8 * SEQ]); d2 = dbg_out("dbg_logits", [128, NT * 20])
            cv2 = RACC.alloc([2 * SEQ], F32)
            for q in range(4):
                S.dve(lambda e, q=q: e.tensor_copy(cv2[:, :], hT[:, 2 * q:2 * q + 2, :].rearrange("p a b -> p (a b)")), reads=[], writes=["cv2"])
                outs.append(S.dma("sp", lambda e, q=q: e.dma_start(out=d1[:, 2 * q * SEQ:(2 * q + 2) * SEQ], in_=cv2[:, :]), reads=["cv2"]))
            outs.append(S.dma("sp", lambda e: e.dma_start(out=d2[:, :], in_=logits.rearrange("p t j -> p (t j)")), reads=["logits.%d" % i for i in range(int(os.environ.get("KATT_TILES", NT)))] if int(os.environ.get("KATT_STAGE", 9)) >= 7 else []))
            break


        S.fold_now = os.environ.get("KFOLD", "all") == "all"
        fence()
        TT.reset(); RW.reset(); RACC.reset()
        S.dma("sp", lambda e: e.dma_start(out=lnG[:, :], in_=ln2_g[0:1, :].to_broadcast([128, 1024])), writes=["lnG"])
        S.dma("sp", lambda e: e.dma_start(out=lnB[:, :], in_=ln2_b[0:1, :].to_broadcast([128, 1024])), writes=["lnB"])
        LOGK = ["logits.%d" % i for i in range(NT)]
        lg = logits[:, :, 0:4]
        le4 = logits[:, :, 4:20].rearrange("p t (g j) -> p t g j", g=4)
        gmax = TT.alloc([NT], F32); goh = TT.alloc([NT, 4], F32); gex = TT.alloc([NT, 4], F32)
        gsum = TT.alloc([NT], F32); gval = TT.alloc([NT], F32)
        tmp16 = TT.alloc([NT, 4, 4], F32); esel = TT.alloc([NT, 4], F32)
        m1 = TT.alloc([NT], F32); oh1 = TT.alloc([NT, 4], F32); e2 = TT.alloc([NT, 4], F32)
        m2 = TT.alloc([NT], F32); oh2 = TT.alloc([NT, 4], F32); dd = TT.alloc([NT], F32)
        w1 = TT.alloc([NT], F32); w2 = TT.alloc([NT], F32); cw1 = TT.alloc([NT], F32); cw2 = TT.alloc([NT], F32)
        cj = TT.alloc([NT, 4], F32); cj2 = TT.alloc([NT, 4], F32)
        bc4 = lambda a: a.unsqueeze(2).to_broadcast([128, NT, 4])
        S.dve(lambda e: e.tensor_reduce(out=gmax[:, :], in_=lg, axis=AX.X, op=ALU.max), reads=LOGK, writes=["gmax"])
        S.dve(lambda e: e.tensor_tensor(out=goh[:, :, :], in0=lg, in1=bc4(gmax[:, :]), op=ALU.is_equal), reads=LOGK + ["gmax"], writes=["goh"])
        S.dve(lambda e: e.tensor_tensor(out=gex[:, :, :], in0=lg, in1=bc4(gmax[:, :]), op=ALU.subtract), reads=LOGK + ["gmax"], writes=["gex"])
        S.act(lambda e: e.activation(out=gex[:, :, :], in_=gex[:, :, :], func=AF.Exp), reads=["gex"], writes=["gex"])
        S.dve(lambda e: e.tensor_reduce(out=gsum[:, :], in_=gex[:, :, :], axis=AX.X, op=ALU.add), reads=["gex"], writes=["gsum"])
        S.dve(lambda e: e.reciprocal(gval[:, :], gsum[:, :]), reads=["gsum"], writes=["gval"])
        S.dve(lambda e: e.tensor_tensor(out=tmp16[:, :, :, :], in0=le4, in1=goh[:, :, :].unsqueeze(3).to_broadcast([128, NT, 4, 4]), op=ALU.mult), reads=LOGK + ["goh"], writes=["tmp16"])
        S.dve(lambda e: e.tensor_reduce(out=esel[:, :, :], in_=tmp16.rearrange("p t g j -> p t j g"), axis=AX.X, op=ALU.add), reads=["tmp16"], writes=["esel"])
        S.dve(lambda e: e.tensor_reduce(out=m1[:, :], in_=esel[:, :, :], axis=AX.X, op=ALU.max), reads=["esel"], writes=["m1"])
        S.dve(lambda e: e.tensor_tensor(out=oh1[:, :, :], in0=esel[:, :, :], in1=bc4(m1[:, :]), op=ALU.is_equal), reads=["esel", "m1"], writes=["oh1"])
        S.dve(lambda e: e.scalar_tensor_tensor(out=e2[:, :, :], in0=oh1[:, :, :], scalar=-1e30, in1=esel[:, :, :], op0=ALU.mult, op1=ALU.add), reads=["oh1", "esel"], writes=["e2"])
        S.dve(lambda e: e.tensor_reduce(out=m2[:, :], in_=e2[:, :, :], axis=AX.X, op=ALU.max), reads=["e2"], writes=["m2"])
        S.dve(lambda e: e.tensor_tensor(out=oh2[:, :, :], in0=e2[:, :, :], in1=bc4(m2[:, :]), op=ALU.is_equal), reads=["e2", "m2"], writes=["oh2"])
        S.dve(lambda e: e.tensor_tensor(out=dd[:, :], in0=m2[:, :], in1=m1[:, :], op=ALU.subtract), reads=["m1", "m2"], writes=["dd"])
        S.act(lambda e: e.activation(out=dd[:, :], in_=dd[:, :], func=AF.Exp), reads=["dd"], writes=["dd"])
        S.dve(lambda e: e.tensor_scalar(out=w1[:, :], in0=dd[:, :], scalar1=1.0, scalar2=None, op0=ALU.add), reads=["dd"], writes=["w1"])
        S.dve(lambda e: e.reciprocal(w1[:, :], w1[:, :]), reads=["w1"], writes=["w1"])
        S.dve(lambda e: e.tensor_tensor(out=w2[:, :], in0=dd[:, :], in1=w1[:, :], op=ALU.mult), reads=["dd", "w1"], writes=["w2"])
        S.dve(lambda e: e.tensor_tensor(out=cw1[:, :], in0=gval[:, :], in1=w1[:, :], op=ALU.mult), reads=["gval", "w1"], writes=["cw1"])
        S.dve(lambda e: e.tensor_tensor(out=cw2[:, :], in0=gval[:, :], in1=w2[:, :], op=ALU.mult), reads=["gval", "w2"], writes=["cw2"])
        S.dve(lambda e: e.tensor_tensor(out=cj[:, :, :], in0=oh1[:, :, :], in1=bc4(cw1[:, :]), op=ALU.mult), reads=["oh1", "cw1"], writes=["cj"])
        S.dve(lambda e: e.tensor_tensor(out=cj2[:, :, :], in0=oh2[:, :, :], in1=bc4(cw2[:, :]), op=ALU.mult), reads=["oh2", "cw2"], writes=["cj2"])
        S.dve(lambda e: e.tensor_tensor(out=cj[:, :, :], in0=cj[:, :, :], in1=cj2[:, :, :], op=ALU.add), reads=["cj", "cj2"], writes=["cj"])
        S.dve(lambda e: e.tensor_tensor(out=comb.rearrange("p t (g j) -> p t g j", g=4), in0=goh[:, :, :].unsqueeze(3).to_broadcast([128, NT, 4, 4]), in1=cj[:, :, :].unsqueeze(2).to_broadcast([128, NT, 4, 4]), op=ALU.mult),
              reads=["goh", "cj"], writes=["comb"])

        S.fold_now = os.environ.get("KFOLD", "all") in ("moe", "attmoe", "all")
        acc = RACC.alloc([NT, 1024], F32)
        sg = [TT.alloc([512], BF16) for _ in range(2)]
        actT = [TT.alloc([4, 512], BF16) for _ in range(2)]
        obuf = [TT.alloc([1024], F32) for _ in range(2)]
        bst2 = [TT.alloc([2, 6], F32) for _ in range(2)]
        mv2 = [TT.alloc([2], F32) for _ in range(2)]
        rs2 = [TT.alloc([1], F32) for _ in range(2)]
        for q in range(4):
            S.dma("sp", lambda e, q=q, s=s: e.dma_start(out=acc[:, 4 * q:4 * q + 4, :], in_=h_scr[s, q * 512:(q + 1) * 512, :].rearrange("(t p) d -> p t d", p=128)),
                  reads=["h_scr.%d" % t for t in range(4 * q, 4 * q + 4)], writes=["acc.%d" % t for t in range(4 * q, 4 * q + 4)])
        def ln2_stats(t):
            b2 = t % 2
            for c2 in range(2):
                S.dve(lambda e, c2=c2, t=t, b2=b2: e.bn_stats(bst2[b2][:, c2, :], acc[:, t, c2 * 512:(c2 + 1) * 512]), reads=["acc.%d" % t], writes=["bst2%d.%d" % (b2, c2)])
            S.dve(lambda e, b2=b2: e.bn_aggr(mv2[b2][:, :], bst2[b2][:, :, :]), reads=["bst2%d.0" % b2, "bst2%d.1" % b2], writes=["mv2%d" % b2])

        def ln2_apply(t):
            b2 = t % 2
            S.act(lambda e, b2=b2: e.activation(out=rs2[b2][:, :], in_=mv2[b2][:, 1:2], func=AF.Ln, bias=LN_EPS, scale=1.0), reads=["mv2%d" % b2], writes=["rs2%d" % b2])
            S.act(lambda e, b2=b2: e.activation(out=rs2[b2][:, :], in_=rs2[b2][:, :], func=AF.Exp, scale=-0.5), reads=["rs2%d" % b2], writes=["rs2%d" % b2])
            S.dve(lambda e, t=t, b2=b2: e.tensor_scalar(out=obuf[b2][:, :], in0=acc[:, t, :], scalar1=mv2[b2][:, 0:1], scalar2=rs2[b2][:, 0:1], op0=ALU.subtract, op1=ALU.mult),
                  reads=["acc.%d" % t, "mv2%d" % b2, "rs2%d" % b2], writes=["obuf%d" % b2])
            S.dve(lambda e, b2=b2: e.tensor_tensor(out=obuf[b2][:, :], in0=obuf[b2][:, :], in1=lnG[:, :], op=ALU.mult), reads=["obuf%d" % b2, "lnG"], writes=["obuf%d" % b2])
            S.pool(lambda e, b2=b2: e.tensor_tensor(out=obuf[b2][:, :], in0=obuf[b2][:, :], in1=lnB[:, :], op=ALU.add), reads=["obuf%d" % b2, "lnB"], writes=["obuf%d" % b2])
            outs.append(S.dma("sp", lambda e, t=t, b2=b2, s=s: e.dma_start(out=out[s, t * 128:(t + 1) * 128, :], in_=obuf[b2][:, :]), reads=["obuf%d" % b2], writes=["out.%d.%d" % (s, t)]))

        ln2_prev = []
        if NEXP > 1:
            load_expert(1)
        cg = 0; cd = 0
        for ex in range(NEXP):
            sl = ex % 2
            for tb in range(4):
                ab = (ex * 4 + tb) % 2
                hk = ["hT.%d.%d" % (i, hf) for i in range(4 * tb, 4 * tb + 4) for hf in range(2)]
                for fc in range(4):
                    pg = cg % 2; cg += 1
                    for k in range(8):
                        S.pe(lambda e, pg=pg, k=k, fc=fc, tb=tb, sl=sl: e.matmul(bank(pg)[:, :], lhsT=wgu[sl][:, k, fc * 128:(fc + 1) * 128], rhs=hT[:, k, tb * 512:(tb + 1) * 512], start=(k == 0), stop=(k == 7)),
                             reads=["wg.%d" % sl] + hk, writes=[PSK[pg]])
                    for k in range(8):
                        S.pe(lambda e, pg=pg, k=k, fc=fc, tb=tb, sl=sl: e.matmul(bank(2 + pg)[:, :], lhsT=wgu[sl][:, k, 512 + fc * 128:512 + (fc + 1) * 128], rhs=hT[:, k, tb * 512:(tb + 1) * 512], start=(k == 0), stop=(k == 7)),
                             reads=["wu.%d" % sl] + hk, writes=[PSK[2 + pg]])
                    S.act(lambda e, pg=pg: e.activation(out=sg[pg][:, :], in_=bank(pg)[:, :], func=AF.Silu), reads=[PSK[pg]], writes=["sg%d" % pg])
                    S.dve(lambda e, pg=pg, ab=ab, fc=fc: e.tensor_tensor(out=actT[ab][:, fc, :], in0=bank(2 + pg)[:, :], in1=sg[pg][:, :], op=ALU.mult),
                          reads=[PSK[2 + pg], "sg%d" % pg], writes=["actT%d.%d" % (ab, fc)])
                for tt in range(4):
                    t = tb * 4 + tt
                    pdi = 2 + (cd % 2); cd += 1
                    for half in range(2):
                        for fc in range(4):
                            S.pe(lambda e, pdi=pdi, half=half, fc=fc, ab=ab, tt=tt, sl=sl: e.matmul(pd[pdi][:, half * 512:(half + 1) * 512], lhsT=actT[ab][:, fc, tt * 128:(tt + 1) * 128], rhs=wdn[sl][:, fc, half * 512:(half + 1) * 512], start=(fc == 0), stop=(fc == 3)),
                                 reads=["actT%d.%d" % (ab, fc), "wd.%d" % sl], writes=[PSK[2 * pdi + half]])
                    S.dve(lambda e, pdi=pdi, t=t, ex=ex: e.scalar_tensor_tensor(out=acc[:, t, :], in0=pd[pdi][:, :], scalar=comb[:, t, ex:ex + 1], in1=acc[:, t, :], op0=ALU.mult, op1=ALU.add),
                          reads=[PSK[2 * pdi], PSK[2 * pdi + 1], "comb", "acc.%d" % t], writes=["acc.%d" % t])
                    if ex == NEXP - 1:
                        ln2_stats(t)
                        if ln2_prev:
                            ln2_apply(ln2_prev.pop())
                        ln2_prev.append(t)
            if ex + 2 < NEXP:
                load_expert(ex + 2)
        while ln2_prev:
            ln2_apply(ln2_prev.pop())
        S.fold_now = os.environ.get("KFOLD", "all") == "all"
        if s + 1 < NSEQ:
            fence()
        if dbg == "one":
            break

    with nc.allow_non_contiguous_dma(reason="tiny constant loads"):
        st = S.emit(outs)
    return nc, st, dbg_t


_CACHE = {}


def _get_program():
    if "p" not in _CACHE:
        _CACHE["p"] = build_program(dbg=os.environ.get("KDBG", ""))
    return _CACHE["p"]


def kernel(**inputs):
    nc, st, dbg_t = _get_program()
    f = lambda a: np.ascontiguousarray(np.asarray(a, dtype=np.float32))
    x = f(inputs["x"])
    shared = {
        "w_in": f(inputs["w_in"])[0], "b_in": f(inputs["b_in"]).reshape(1, DIN),
        "conv_w": f(inputs["conv_w"])[0], "conv_b": f(inputs["conv_b"]).reshape(1, 1024),
        "a_log": f(inputs["a_log"]).reshape(1, 8), "d_skip": f(inputs["d_skip"]).reshape(1, 8),
        "ssd_norm_g": f(inputs["ssd_norm_g"]).reshape(1, 512), "w_out": f(inputs["w_out"])[0],
        "ln1_g": f(inputs["ln1_g"]).reshape(1, DM), "ln1_b": f(inputs["ln1_b"]).reshape(1, DM),
        "router_group_w": f(inputs["router_group_w"])[0], "router_group_b": f(inputs["router_group_b"]).reshape(1, 4),
        "router_expert_w": f(inputs["router_expert_w"])[0], "router_expert_b": f(inputs["router_expert_b"]).reshape(1, 16),
        "w_gate": f(inputs["w_gate"])[0], "w_up": f(inputs["w_up"])[0], "w_down": f(inputs["w_down"])[0],
        "ln2_g": f(inputs["ln2_g"]).reshape(1, DM), "ln2_b": f(inputs["ln2_b"]).reshape(1, DM),
    }
    ncores = int(os.environ.get("KCORES", NCORES))
    in_maps = []
    for c in range(ncores):
        m = dict(shared)
        m["x"] = np.ascontiguousarray(x[c * NSEQ:(c + 1) * NSEQ])
        in_maps.append(m)
    res = run_bass_kernel_spmd(nc, in_maps, core_ids=list(range(ncores)))
    if os.environ.get("KDBG", ""):
        _CACHE["dbg"] = res.results
    outp = np.concatenate([r["out"] for r in res.results], axis=0)
    return outp.astype(np.float32)
```
